# Optimizing a Trainium2 kernel written in Bass

```python
import jax
import jax.numpy as jnp
from jax import lax
import numpy as np

D_MODEL = 1024
BATCH = 16
SEQ = 2048
DEPTH = 2

GRID_W = 64
CTX_LEN = 256

CONV_DIM = 512
CONV_WIDTH = 31
NA_HEADS = 8
NA_HEAD_DIM = 64
NA_DIM = NA_HEADS * NA_HEAD_DIM
NA_KH_MAX = 8
NA_KW = 16
C_HEADS = 16
C_KV_HEADS = 4
C_GROUP = C_HEADS // C_KV_HEADS
C_HEAD_DIM = 64
C_WINDOW = 128
C_BLOCK = 128
ROPE_BASE = 10000.0
N_GROUPS = 4
EXPERTS_PER_GROUP = 4
N_EXPERTS = N_GROUPS * EXPERTS_PER_GROUP
TOP_K = 2
D_EXPERT = 256

LN_EPS = 1e-5
NEG_INF = -1e30

kernel_name = 'hybrid_conv_natten_swa_hmoe_dit'


def layer_norm(x, g, b):
    xf = x.astype(jnp.float32)
    mu = jnp.mean(xf, axis=-1, keepdims=True)
    var = jnp.mean(jnp.square(xf - mu), axis=-1, keepdims=True)
    return ((xf - mu) * lax.rsqrt(var + LN_EPS)).astype(x.dtype) * g + b


def axial_rope_tables(n_tokens, head_dim):
    t = jnp.arange(n_tokens)
    row = (t // GRID_W).astype(jnp.float32)
    col = (t % GRID_W).astype(jnp.float32)
    axis_dim = head_dim // 2
    inv_freq = ROPE_BASE ** (-jnp.arange(0, axis_dim, 2, dtype=jnp.float32) / axis_dim)
    ang = jnp.concatenate([row[:, None] * inv_freq, col[:, None] * inv_freq], axis=-1)
    return jnp.cos(ang), jnp.sin(ang)


def apply_axial_rope(x, cos, sin):
    xr = x.astype(jnp.float32).reshape(x.shape[:-1] + (2, 2, -1))
    bshape = (1, x.shape[1]) + (1,) * (x.ndim - 3) + (2, cos.shape[-1] // 2)
    cs = cos.reshape(bshape)
    sn = sin.reshape(bshape)
    x1 = xr[..., 0, :]
    x2 = xr[..., 1, :]
    out = jnp.stack([x1 * cs - x2 * sn, x2 * cs + x1 * sn], axis=-2)
    return out.reshape(x.shape).astype(x.dtype)


def depthwise_conv(h, w, b):
    half = w.shape[0] // 2
    out = lax.conv_general_dilated(h, w[:, None, :], window_strides=(1,), padding=[(half, half)],
                                   dimension_numbers=('NWC', 'WIO', 'NWC'),
                                   feature_group_count=h.shape[-1])
    return out + b


def conformer_conv(a, conv_w, conv_b, g, b):
    h = a[..., :CONV_DIM] * jax.nn.sigmoid(a[..., CONV_DIM:])
    h = layer_norm(depthwise_conv(h, conv_w, conv_b), g, b)
    return jax.nn.silu(h)


def context_attention(q, k, v, sink=None):
    s = jnp.einsum('bqhgd,bkhd->bhgqk', q, k).astype(jnp.float32) * (q.shape[-1] ** -0.5)
    if sink is not None:
        sink_col = jnp.broadcast_to(sink.astype(jnp.float32)[None, :, :, None, None], s.shape[:-1] + (1,))
        s = jnp.concatenate([s, sink_col], axis=-1)
    p = jax.nn.softmax(s, axis=-1)[..., :k.shape[1]].astype(v.dtype)
    return jnp.einsum('bhgqk,bkhd->bqhgd', p, v)


def neighbourhood_attention(q, k, v, k_ctx, v_ctx, rpb, rows):
    bsz = q.shape[0]
    kh = min(NA_KH_MAX, rows)
    n_win = kh * GRID_W
    grid = lambda t: t.reshape(bsz, rows, GRID_W, NA_HEADS, NA_HEAD_DIM)
    qg, kg, vg = grid(q), grid(k), grid(v)
    r = np.arange(rows)
    row_start = np.clip(r - kh // 2, 0, rows - kh)
    row_off = row_start[:, None] + np.arange(kh)[None, :] - r[:, None] + NA_KH_MAX - 1
    j = np.arange(GRID_W)
    col_start = np.clip(j - NA_KW // 2, 0, GRID_W - NA_KW)
    col_in = (j[None, :] >= col_start[:, None]) & (j[None, :] < col_start[:, None] + NA_KW)
    col_off = np.clip(j[None, :] - j[:, None], -(NA_KW - 1), NA_KW - 1) + NA_KW - 1
    bias = rpb.astype(jnp.float32)[:, row_off][:, :, :, col_off]
    bias = jnp.where(col_in[None, None, None], bias, NEG_INF)
    bias = bias.transpose(1, 0, 3, 2, 4).reshape(rows, NA_HEADS, GRID_W, n_win)
    scale = NA_HEAD_DIM ** -0.5

    def row_step(args):
        start, q_row, b_row = args
        k_blk = lax.dynamic_slice_in_dim(kg, start, kh, axis=1).reshape(bsz, n_win, NA_HEADS, NA_HEAD_DIM)
        v_blk = lax.dynamic_slice_in_dim(vg, start, kh, axis=1).reshape(bsz, n_win, NA_HEADS, NA_HEAD_DIM)
        s_lat = jnp.einsum('bqhd,bkhd->bhqk', q_row, k_blk).astype(jnp.float32) * scale + b_row
        s_ctx = jnp.einsum('bqhd,bkhd->bhqk', q_row, k_ctx).astype(jnp.float32) * scale
        p = jax.nn.softmax(jnp.concatenate([s_lat, s_ctx], axis=-1), axis=-1).astype(v.dtype)
        return (jnp.einsum('bhqk,bkhd->bqhd', p[..., :n_win], v_blk)
                + jnp.einsum('bhqk,bkhd->bqhd', p[..., n_win:], v_ctx))

    out = lax.map(row_step, (jnp.asarray(row_start, jnp.int32), jnp.moveaxis(qg, 1, 0), bias))
    return jnp.moveaxis(out, 0, 1).reshape(bsz, rows * GRID_W, NA_HEADS, NA_HEAD_DIM)


def conv_na_mixer(u_ctx, u_lat, w_in, b_in, conv_w, conv_b, cln_g, cln_b, rpb, w_out, b_out, rows, need_ctx):
    bsz, n_lat, _ = u_lat.shape
    q0 = 2 * CONV_DIM
    kv0 = q0 + NA_DIM
    heads = lambda t: t.reshape(t.shape[:-1] + (NA_HEADS, NA_HEAD_DIM))
    a_lat, q_lat, k_lat, v_lat = jnp.split(u_lat @ w_in + b_in, [q0, kv0, kv0 + NA_DIM], axis=-1)
    k_ctx, v_ctx = jnp.split(u_ctx @ w_in[:, kv0:] + b_in[kv0:], 2, axis=-1)
    k_ctx, v_ctx = heads(k_ctx), heads(v_ctx)
    conv_lat = conformer_conv(a_lat, conv_w, conv_b, cln_g, cln_b)
    na_lat = neighbourhood_attention(heads(q_lat), heads(k_lat), heads(v_lat), k_ctx, v_ctx, rpb, rows)
    o_lat = jnp.concatenate([conv_lat, na_lat.reshape(bsz, n_lat, NA_DIM)], axis=-1) @ w_out + b_out
    o_ctx = None
    if need_ctx:
        a_ctx, q_ctx = jnp.split(u_ctx @ w_in[:, :kv0] + b_in[:kv0], [q0], axis=-1)
        conv_ctx = conformer_conv(a_ctx, conv_w, conv_b, cln_g, cln_b)
        na_ctx = context_attention(heads(q_ctx)[:, :, :, None, :], k_ctx, v_ctx)
        o_ctx = jnp.concatenate([conv_ctx, na_ctx.reshape(bsz, -1, NA_DIM)], axis=-1) @ w_out + b_out
    return o_ctx, o_lat


def banded_window_attention(q, k, v, k_ctx, v_ctx, sink):
    bsz, n, hkv, g, d = q.shape
    nb = n // C_BLOCK
    span = C_BLOCK + 2 * C_WINDOW
    pad = [(0, 0), (C_WINDOW, C_WINDOW), (0, 0), (0, 0)]
    kp = jnp.pad(k, pad)
    vp = jnp.pad(v, pad)
    q_blocks = jnp.moveaxis(q.reshape(bsz, nb, C_BLOCK, hkv, g, d), 1, 0)
    scale = d ** -0.5
    sink_col = jnp.broadcast_to(sink.astype(jnp.float32)[None, :, :, None, None], (bsz, hkv, g, C_BLOCK, 1))

    def block_step(args):
        b_idx, q_blk = args
        start = b_idx * C_BLOCK
        k_blk = lax.dynamic_slice_in_dim(kp, start, span, axis=1)
        v_blk = lax.dynamic_slice_in_dim(vp, start, span, axis=1)
        k_pos = start - C_WINDOW + jnp.arange(span)
        q_pos = start + jnp.arange(C_BLOCK)
        valid = (k_pos >= 0)[None, :] & (k_pos < n)[None, :] & (jnp.abs(q_pos[:, None] - k_pos[None, :]) <= C_WINDOW)
        s_lat = jnp.einsum('bqhgd,bkhd->bhgqk', q_blk, k_blk).astype(jnp.float32) * scale
        s_lat = jnp.where(valid, s_lat, NEG_INF)
        s_ctx = jnp.einsum('bqhgd,bkhd->bhgqk', q_blk, k_ctx).astype(jnp.float32) * scale
        p = jax.nn.softmax(jnp.concatenate([s_lat, s_ctx, sink_col], axis=-1), axis=-1).astype(v.dtype)
        return (jnp.einsum('bhgqk,bkhd->bqhgd', p[..., :span], v_blk)
                + jnp.einsum('bhgqk,bkhd->bqhgd', p[..., span:span + k_ctx.shape[1]], v_ctx))

    out = lax.map(block_step, (jnp.arange(nb, dtype=jnp.int32), q_blocks))
    return jnp.moveaxis(out, 0, 1).reshape(bsz, n, hkv, g, d)


def window_gqa_mixer(u_ctx, u_lat, w_in, sink, w_out, rope_cos, rope_sin, need_ctx):
    bsz, n_lat, _ = u_lat.shape
    q_cols = C_HEADS * C_HEAD_DIM
    kv_cols = C_KV_HEADS * C_HEAD_DIM
    qh = lambda t: t.reshape(t.shape[:-1] + (C_KV_HEADS, C_GROUP, C_HEAD_DIM))
    kvh = lambda t: t.reshape(t.shape[:-1] + (C_KV_HEADS, C_HEAD_DIM))
    q_lat, k_lat, v_lat = jnp.split(u_lat @ w_in, [q_cols, q_cols + kv_cols], axis=-1)
    k_ctx, v_ctx = jnp.split(u_ctx @ w_in[:, q_cols:], [kv_cols], axis=-1)
    k_ctx, v_ctx = kvh(k_ctx), kvh(v_ctx)
    q_lat = apply_axial_rope(qh(q_lat), rope_cos, rope_sin)
    k_lat = apply_axial_rope(kvh(k_lat), rope_cos, rope_sin)
    sink_hg = sink.reshape(C_KV_HEADS, C_GROUP)
    o_lat = banded_window_attention(q_lat, k_lat, kvh(v_lat), k_ctx, v_ctx, sink_hg)
    o_lat = o_lat.reshape(bsz, n_lat, q_cols) @ w_out
    o_ctx = None
    if need_ctx:
        q_ctx = qh(u_ctx @ w_in[:, :q_cols])
        o_ctx = context_attention(q_ctx, k_ctx, v_ctx, sink_hg).reshape(bsz, -1, q_cols) @ w_out
    return o_ctx, o_lat


def hier_moe(u, router_g, router_e, w_gate, w_up, w_down):
    t = u.shape[0]
    g_prob = jax.nn.softmax((u @ router_g).astype(jnp.float32), axis=-1)
    g_p, g_idx = lax.top_k(g_prob, 1)
    e_logits = (u @ router_e).astype(jnp.float32).reshape(t, N_GROUPS, EXPERTS_PER_GROUP)
    e_logits = jnp.take_along_axis(e_logits, g_idx[:, :, None], axis=1)[:, 0]
    e_top, e_idx = lax.top_k(e_logits, TOP_K)
    e_w = jax.nn.softmax(e_top, axis=-1) * g_p
    within = jnp.einsum('tk,tke->te', e_w, jax.nn.one_hot(e_idx, EXPERTS_PER_GROUP, dtype=jnp.float32))
    gates = (jax.nn.one_hot(g_idx[:, 0], N_GROUPS, dtype=jnp.float32)[:, :, None] * within[:, None, :]).astype(u.dtype)
    wg = w_gate.reshape(N_GROUPS, EXPERTS_PER_GROUP, D_MODEL, D_EXPERT)
    wu = w_up.reshape(N_GROUPS, EXPERTS_PER_GROUP, D_MODEL, D_EXPERT)
    wd = w_down.reshape(N_GROUPS, EXPERTS_PER_GROUP, D_EXPERT, D_MODEL)
    y = jnp.zeros_like(u)
    for g in range(N_GROUPS):
        h = jax.nn.silu(jnp.einsum('td,edf->tef', u, wg[g])) * jnp.einsum('td,edf->tef', u, wu[g])
        y = y + jnp.einsum('tef,efd->td', h * gates[:, g, :, None], wd[g])
    return y


def setup_inputs(seed: int = 0) -> dict:
    key = jax.random.key(seed)
    ks = iter(jax.random.split(key, 32))
    f32 = jnp.float32
    n_even = (DEPTH + 1) // 2
    n_odd = DEPTH // 2
    beta = (8.0 * DEPTH) ** -0.25
    ab_cols = 2 * CONV_DIM + 3 * NA_DIM
    gqa_cols = (C_HEADS + 2 * C_KV_HEADS) * C_HEAD_DIM

    def nrm(shape, scale):
        return jax.random.normal(next(ks), shape, f32) * scale

    return {
        'x': nrm((BATCH, SEQ, D_MODEL), 1.0),
        'c': nrm((BATCH, D_MODEL), 1.0),
        'ctx': nrm((BATCH, CTX_LEN, D_MODEL), 1.0),
        'c_ctx': nrm((D_MODEL,), 1.0),
        'ada_w': nrm((DEPTH, D_MODEL, 6 * D_MODEL), 0.5 * D_MODEL ** -0.5),
        'ada_b': nrm((DEPTH, 6 * D_MODEL), 0.02),
        'ln_g': 1.0 + nrm((DEPTH, 2, D_MODEL), 0.02),
        'ln_b': nrm((DEPTH, 2, D_MODEL), 0.02),
        'ab_w_in': nrm((n_even, D_MODEL, ab_cols), D_MODEL ** -0.5),
        'ab_b_in': nrm((n_even, ab_cols), 0.02),
        'conv_w': nrm((n_even, CONV_WIDTH, CONV_DIM), CONV_WIDTH ** -0.5),
        'conv_b': nrm((n_even, CONV_DIM), 0.02),
        'conv_ln_g': 1.0 + nrm((n_even, CONV_DIM), 0.02),
        'conv_ln_b': nrm((n_even, CONV_DIM), 0.02),
        'na_rpb': nrm((n_even, NA_HEADS, 2 * NA_KH_MAX - 1, 2 * NA_KW - 1), 0.1),
        'ab_w_out': nrm((n_even, CONV_DIM + NA_DIM, D_MODEL), beta * (CONV_DIM + NA_DIM) ** -0.5),
        'ab_b_out': nrm((n_even, D_MODEL), 0.02),
        'gqa_w_in': nrm((n_odd, D_MODEL, gqa_cols), D_MODEL ** -0.5),
        'gqa_sink': nrm((n_odd, C_HEADS), 0.5),
        'gqa_w_out': nrm((n_odd, C_HEADS * C_HEAD_DIM, D_MODEL), beta * (C_HEADS * C_HEAD_DIM) ** -0.5),
        'router_group': nrm((DEPTH, D_MODEL, N_GROUPS), D_MODEL ** -0.5),
        'router_expert': nrm((DEPTH, D_MODEL, N_EXPERTS), D_MODEL ** -0.5),
        'exp_w_gate': nrm((DEPTH, N_EXPERTS, D_MODEL, D_EXPERT), D_MODEL ** -0.5),
        'exp_w_up': nrm((DEPTH, N_EXPERTS, D_MODEL, D_EXPERT), D_MODEL ** -0.5),
        'exp_w_down': nrm((DEPTH, N_EXPERTS, D_EXPERT, D_MODEL), beta * D_EXPERT ** -0.5),
    }


def reference(x, c, ctx, c_ctx, ada_w, ada_b, ln_g, ln_b, ab_w_in, ab_b_in, conv_w, conv_b, conv_ln_g,
              conv_ln_b, na_rpb, ab_w_out, ab_b_out, gqa_w_in, gqa_sink, gqa_w_out, router_group,
              router_expert, exp_w_gate, exp_w_up, exp_w_down):
    alpha = (2.0 * DEPTH) ** 0.25
    bsz, n_lat, _ = x.shape
    n_ctx = ctx.shape[1]
    rows = n_lat // GRID_W
    rope_cos, rope_sin = axial_rope_tables(n_lat, C_HEAD_DIM)
    silu_c = jax.nn.silu(c)
    silu_cc = jax.nn.silu(c_ctx)
    h_lat, h_ctx = x, ctx
    for i in range(DEPTH):
        j = i // 2
        need_ctx = i < DEPTH - 1
        m_lat = jnp.split((silu_c @ ada_w[i] + ada_b[i])[:, None, :], 6, axis=-1)
        m_ctx = jnp.split(silu_cc @ ada_w[i] + ada_b[i], 6, axis=-1)
        u_lat = h_lat * (1.0 + m_lat[1]) + m_lat[0]
        u_ctx = h_ctx * (1.0 + m_ctx[1]) + m_ctx[0]
        if i % 2 == 0:
            o_ctx, o_lat = conv_na_mixer(u_ctx, u_lat, ab_w_in[j], ab_b_in[j], conv_w[j], conv_b[j],
                                         conv_ln_g[j], conv_ln_b[j], na_rpb[j], ab_w_out[j], ab_b_out[j],
                                         rows, need_ctx)
        else:
            o_ctx, o_lat = window_gqa_mixer(u_ctx, u_lat, gqa_w_in[j], gqa_sink[j], gqa_w_out[j],
                                            rope_cos, rope_sin, need_ctx)
        h_lat = layer_norm(alpha * h_lat + m_lat[2] * o_lat, ln_g[i, 0], ln_b[i, 0])
        t_lat = h_lat * (1.0 + m_lat[4]) + m_lat[3]
        if need_ctx:
            h_ctx = layer_norm(alpha * h_ctx + m_ctx[2] * o_ctx, ln_g[i, 0], ln_b[i, 0])
            t_ctx = h_ctx * (1.0 + m_ctx[4]) + m_ctx[3]
            tokens = jnp.concatenate([t_ctx, t_lat], axis=1)
        else:
            tokens = t_lat
        y = hier_moe(tokens.reshape(-1, D_MODEL), router_group[i], router_expert[i], exp_w_gate[i],
                     exp_w_up[i], exp_w_down[i]).reshape(bsz, -1, D_MODEL)
        h_lat = layer_norm(alpha * h_lat + m_lat[5] * y[:, y.shape[1] - n_lat:], ln_g[i, 1], ln_b[i, 1])
        if need_ctx:
            h_ctx = layer_norm(alpha * h_ctx + m_ctx[5] * y[:, :n_ctx], ln_g[i, 1], ln_b[i, 1])
    return h_lat
```

```python
import numpy as np
from contextlib import ExitStack
import concourse.bass as bass
import concourse.mybir as mybir
from concourse.bass_utils import run_bass_kernel_spmd

F32 = mybir.dt.float32
BF16 = mybir.dt.bfloat16
AF = mybir.ActivationFunctionType
ALU = mybir.AluOpType
AX = mybir.AxisListType

D = 1024
SEQ = 2048
NCTX = 256
NT = SEQ + NCTX
GW = 64
ALPHA = 4.0 ** 0.25
EPS = 1e-5
NEG = -1e30

ENGS = ("pe", "act", "dve", "pool", "sp")
N_DMA_SEMS = 40


class Res:
    __slots__ = ("name", "last_w", "readers")

    def __init__(self, name):
        self.name = name
        self.last_w = None
        self.readers = []


class Op:
    __slots__ = ("eng", "fn", "idx", "deps", "dma", "sig", "semval", "dsem", "dval", "dprev")

    def __init__(self, eng, fn, idx, dma):
        self.eng = eng
        self.fn = fn
        self.idx = idx
        self.deps = []
        self.dma = dma
        self.sig = False
        self.semval = 0
        self.dsem = -1
        self.dval = 0
        self.dprev = 0


class Sched:
    def __init__(self):
        self.ops = {e: [] for e in ENGS}
        self.ndma = 0
        self.dma_tot = [0] * N_DMA_SEMS
        self.last_dma = [None] * N_DMA_SEMS
        self._assigned = False

    def add(self, eng, fn, reads=(), writes=(), dma=False, extra=()):
        lst = self.ops[eng]
        op = Op(eng, fn, len(lst), dma)
        deps = {}
        for r in reads:
            if r.last_w is not None:
                deps[id(r.last_w)] = r.last_w
        for w in writes:
            if w.last_w is not None:
                deps[id(w.last_w)] = w.last_w
            for rd in w.readers:
                deps[id(rd)] = rd
        for x in extra:
            deps[id(x)] = x
        for r in reads:
            r.readers.append(op)
        for w in writes:
            w.last_w = op
            w.readers = []
        if dma:
            s = self.ndma % N_DMA_SEMS
            self.ndma += 1
            op.dsem = s
            op.dprev = self.dma_tot[s]
            self.dma_tot[s] += 16
            op.dval = self.dma_tot[s]
            self.last_dma[s] = op
        for d in deps.values():
            if d is op:
                continue
            if d.eng == eng and not d.dma and not dma:
                if eng == "pe":
                    continue
                if op.idx - d.idx > 2:
                    continue
            op.deps.append(d)
            if not d.dma:
                d.sig = True
        lst.append(op)
        return op

    def barrier(self):
        lasts = []
        for e in ENGS:
            for op in reversed(self.ops[e]):
                if not op.dma:
                    lasts.append(op)
                    break
        dl = [o for o in self.last_dma if o is not None]
        for e in ENGS:
            self.add(e, lambda eng: eng.nop(), extra=[o for o in lasts if o.eng != e] + dl)

    def emit_one(self, e, eng, esem, dsems):
        if not self._assigned:
            for ee in ENGS:
                c = 0
                for op in self.ops[ee]:
                    if op.sig and not op.dma:
                        c += 1
                        op.semval = c
            self._assigned = True
        seen = {}
        for op in self.ops[e]:
            need = {}
            for d in op.deps:
                if d.dma:
                    key = ("d", d.dsem)
                    val = d.dval
                else:
                    key = ("e", d.eng)
                    val = d.semval
                if val > need.get(key, 0):
                    need[key] = val
            if op.dma and op.dprev > 0:
                key = ("d", op.dsem)
                if op.dprev > need.get(key, 0):
                    need[key] = op.dprev
            for key, val in need.items():
                if seen.get(key, 0) >= val:
                    continue
                seen[key] = val
                sem = dsems[key[1]] if key[0] == "d" else esem[key[1]]
                eng.wait_ge(sem, val)
            ins = op.fn(eng)
            if op.dma:
                ins.then_inc(dsems[op.dsem], 16)
            elif op.sig:
                ins.then_inc(esem[e], 1)


def _sm_layout():
    off = {}
    n = 0
    for name, cols in (("ada_b0", 48), ("ada_b1", 48), ("ln_g", 32), ("ln_b", 32), ("b_in", 16),
                       ("conv_w", 124), ("conv_b", 4), ("cln_g", 4), ("cln_b", 4), ("b_out", 8), ("eps", 1)):
        off[name] = n
        n += cols
    return off, n


SMO, SMN = _sm_layout()


def _fm(v):
    v = np.asarray(v, np.float32)
    return np.ascontiguousarray(v.reshape(-1, 128).T)


def _gqa_qidx():
    idx = np.zeros(1024, np.int64)
    for c in range(8):
        m, j = divmod(c, 4)
        h0 = 8 * m + j
        h1 = 8 * m + 4 + j
        idx[c * 128:c * 128 + 64] = h0 * 64 + np.arange(64)
        idx[c * 128 + 64:c * 128 + 128] = h1 * 64 + np.arange(64)
    return idx


def _swap64(n):
    d = np.arange(n)
    dd = d % 64
    sw = np.where(dd % 32 < 16, dd + 16, dd - 16)
    return (d // 64) * 64 + sw


def _na_tiles(j):
    if j in (0, 1):
        return [0, 1, 2, 3]
    if j in (14, 15):
        return [12, 13, 14, 15]
    return [j - 2, j - 1, j, j + 1, j + 2]


def _na_tile_index(j):
    if j == 0:
        return 5
    if j == 1:
        return 9
    if j == 14:
        return 13
    if j == 15:
        return 17
    return 0


def _na_bias_table(rpb):
    rows = 32
    r = np.arange(rows)
    row_start = np.clip(r - 4, 0, rows - 8)
    jj = np.arange(GW)
    col_start = np.clip(jj - 8, 0, GW - 16)
    col_in = (jj[None, :] >= col_start[:, None]) & (jj[None, :] < col_start[:, None] + 16)
    col_off = np.clip(jj[None, :] - jj[:, None], -15, 15) + 15
    out = np.full((8, 21, 128, 128), NEG, np.float32)

    def tile(j, a):
        t = np.full((8, 128, 128), NEG, np.float32)
        for pk in range(2):
            rk = 2 * a + pk
            for pq in range(2):
                rq = 2 * j + pq
                if not (row_start[rq] <= rk < row_start[rq] + 8):
                    continue
                ro = rk - rq + 7
                blk = rpb[:, ro][:, col_off]
                blk = np.where(col_in[None], blk, np.float32(NEG))
                t[:, pk * 64:(pk + 1) * 64, pq * 64:(pq + 1) * 64] = blk.transpose(0, 2, 1)
        return t

    for i, a in enumerate(_na_tiles(5)):
        out[:, i] = tile(5, a)
    for j in (0, 1, 14, 15):
        base = _na_tile_index(j)
        for i, a in enumerate(_na_tiles(j)):
            out[:, base + i] = tile(j, a)
    return np.ascontiguousarray(out.transpose(0, 2, 1, 3))


def _rope_tables():
    t = np.arange(SEQ)
    row = (t // GW).astype(np.float32)
    col = (t % GW).astype(np.float32)
    inv = (np.float32(10000.0) ** (-np.arange(0, 32, 2, dtype=np.float32) / np.float32(32))).astype(np.float32)
    ang = np.concatenate([row[:, None] * inv, col[:, None] * inv], axis=-1).astype(np.float32)
    cos = np.cos(ang).astype(np.float32)
    sin = np.sin(ang).astype(np.float32)
    p = np.arange(128)
    d = p % 64
    ai = (d // 32) * 16 + d % 16
    sgn = np.where(d % 32 < 16, -1.0, 1.0).astype(np.float32)
    C = np.ascontiguousarray(cos[:, ai].T)
    S = np.ascontiguousarray((sin[:, ai] * sgn[None, :]).T)
    return C.astype(np.float32), S.astype(np.float32)


class _Stop(Exception):
    pass


def build(nlayers=2, nb=2, debug=False, stop=None):
    nc = bass.Bass("TRN2", target_bir_lowering=False)
    S = Sched()

    def din(name, shape):
        return nc.dram_tensor(name, list(shape), F32, kind="ExternalInput").ap()

    x2 = din("x2", [2, SEQ, D])
    ctx2 = din("ctx2", [2, NCTX, D])
    cvec = din("cvec", [128, 24])
    ada_w = din("ada_w", [2, D, 6 * D])
    smd = din("sm", [128, SMN])
    w_in0 = din("w_in0", [D, 2560])
    bvbc = din("bvbc", [128, 512])
    nab = din("nab", [8, 128, 21 * 128])
    w_out0 = din("w_out0", [D, D])
    wq1 = din("wq1", [D, 1024])
    wqs1 = din("wqs1", [D, 1024])
    wk1 = din("wk1", [D, 256])
    wks1 = din("wks1", [D, 256])
    wv1 = din("wv1", [D, 256])
    w_out1 = din("w_out1", [D, D])
    ropeC = din("ropeC", [128, SEQ])
    ropeS = din("ropeS", [128, SEQ])
    maskLU = din("maskLU", [128, 256])
    sinkbc = din("sinkbc", [128, 16])
    wr = din("wr", [2, 128, 160])
    sel = din("sel", [16, 2048])
    ident = din("ident", [128, 128])
    ewg = din("ewg", [2, 16, D, 256])
    ewu = din("ewu", [2, 16, D, 256])
    ewd = din("ewd", [2, 16, 256, D])
    outd = nc.dram_tensor("out", [2, SEQ, D], F32, kind="ExternalOutput").ap()
    Hs = nc.dram_tensor("Hs", [128, 8 * NT], F32, kind="Internal").ap()
    dbg = nc.dram_tensor("dbg", [128, 8 * NT], F32, kind="ExternalOutput").ap() if debug else None
    dbgb = nc.dram_tensor("dbgb", [128, 8 * NT], BF16, kind="ExternalOutput").ap() if debug else None
    Hs3 = Hs.rearrange("p (c t) -> p c t", c=8)

    es = ExitStack()
    cur = [16640]

    acache = {}

    def alloc(name, shape, dt, at=None):
        nbytes = int(np.prod(shape[1:])) * (4 if dt == F32 else 2)
        if at is None:
            at = cur[0]
            cur[0] = (at + nbytes + 63) // 64 * 64
        assert at + nbytes <= 229376, (name, at, nbytes)
        key = (name, at, tuple(shape))
        if key not in acache:
            acache[key] = nc.alloc_sbuf_tensor_at("%s_%d" % (name, len(acache)), list(shape), dt, offset=at)
        return acache[key]

    sm = alloc("sm", [128, SMN], F32)
    id32 = alloc("id32", [128, 128], F32)
    ones1k = alloc("ones1k", [128, 128], BF16)
    ones512 = alloc("ones512", [128, 128], BF16)
    csil = alloc("csil", [128, 24], BF16)
    cv32 = alloc("cv32", [128, 24], F32)
    mod = alloc("mod", [128, 2 * 144], F32)
    mp1 = alloc("mp1", [128, 2 * 144], F32)
    vecs = alloc("vecs", [128, 128], F32)
    selt = alloc("selt", [16, 2048], F32)
    wrt = alloc("wrt", [128, 320], F32)
    esink = alloc("esink", [128, 16], F32)
    R_const = Res("const")
    R_mod = Res("mod")
    R_vecs = Res("vecs")
    base0 = cur[0]

    PS = [es.enter_context(nc.psum_tensor("ps%d" % i, [128, 1024], F32)) for i in range(4)]
    RB = [Res("bank%d" % i) for i in range(8)]

    def bank(k):
        return PS[k // 2][:, (k % 2) * 512:(k % 2) * 512 + 512]

    esem = {e: es.enter_context(nc.semaphore("es_" + e)) for e in ENGS}
    dsems = [es.enter_context(nc.semaphore("ds%d" % i)) for i in range(N_DMA_SEMS)]

    def smc(name, j, n=1):
        o = SMO[name] + j
        return sm[:, o:o + n]

    def modc(l, k, ch, col):
        o = l * 144 + (k * 8 + ch) * 3 + col
        return mod[:, o:o + 1]

    def mp1c(l, k, ch, col):
        o = l * 144 + (k * 8 + ch) * 3 + col
        return mp1[:, o:o + 1]

    VK = {}

    def vslot(kind, ch):
        key = (kind, ch)
        if key not in VK:
            VK[key] = len(VK)
            assert len(VK) <= 128
        o = VK[key]
        return vecs[:, o:o + 1]

    S.add("sp", lambda e: e.dma_start(out=sm[:], in_=smd), writes=[R_const], dma=True)
    S.add("sp", lambda e: e.dma_start(out=id32[:], in_=ident), writes=[R_const], dma=True)
    S.add("sp", lambda e: e.dma_start(out=cv32[:], in_=cvec), writes=[R_const], dma=True)
    S.add("sp", lambda e: e.dma_start(out=selt[:], in_=sel), writes=[R_const], dma=True)
    S.add("sp", lambda e: e.dma_start(out=wrt[:].rearrange("p (l n) -> p l n", l=2), in_=wr.rearrange("l p n -> p l n")), writes=[R_const], dma=True)
    S.add("sp", lambda e: e.dma_start(out=esink[:], in_=sinkbc), writes=[R_const], dma=True)
    S.add("pool", lambda e: e.memset(ones1k[:], 1.0 / 1024.0), writes=[R_const])
    S.add("pool", lambda e: e.memset(ones512[:], 1.0 / 512.0), writes=[R_const])
    S.add("act", lambda e: e.activation(out=csil[:], in_=cv32[:], func=AF.Silu), reads=[R_const], writes=[R_const])
    S.add("act", lambda e: e.activation(out=esink[:], in_=esink[:], func=AF.Exp), reads=[R_const], writes=[R_const])

    adaw = [alloc("adaw%d" % i, [128, 8, 1024], BF16) for i in range(2)]
    R_adaw = [Res("adaw0"), Res("adaw1")]
    pi = 0
    for l in range(nlayers):
        awl = ada_w[l].rearrange("(kc p) n -> p kc n", p=128)
        for piece in range(6):
            s = pi % 2
            pi += 1
            S.add("pool", (lambda e, s=s, awl=awl, piece=piece: e.dma_start(out=adaw[s][:], in_=awl[:, :, piece * 1024:(piece + 1) * 1024])),
                  writes=[R_adaw[s]], dma=True)
            for oc8 in range(8):
                oc = piece * 8 + oc8
                for kc in range(8):
                    S.add("pe", (lambda e, s=s, oc=oc, oc8=oc8, kc=kc: e.matmul(bank(0)[:, oc * 3:oc * 3 + 3], lhsT=adaw[s][:, kc, oc8 * 128:(oc8 + 1) * 128],
                                                                                    rhs=csil[:, kc * 3:kc * 3 + 3], start=(kc == 0), stop=(kc == 7))),
                          reads=[R_adaw[s], R_const], writes=[RB[0]])
        ab = smc("ada_b%d" % l, 0, 48)
        S.add("dve", (lambda e, l=l, ab=ab: e.tensor_tensor(out=mod[:, l * 144:(l + 1) * 144].rearrange("p (a b) -> p a b", b=3),
                                                             in0=bank(0)[:, 0:144].rearrange("p (a b) -> p a b", b=3),
                                                             in1=ab.unsqueeze(2).broadcast_to([128, 48, 3]), op=ALU.add)),
              reads=[RB[0], R_const], writes=[R_mod])
        S.add("dve", (lambda e, l=l: e.tensor_scalar_add(out=mp1[:, l * 144:(l + 1) * 144], in0=mod[:, l * 144:(l + 1) * 144], scalar1=1.0)),
              reads=[R_mod], writes=[R_mod])
    S.barrier()
    cur[0] = base0

    A0 = cur[0]
    hres = alloc("hres", [128, 8, NT], F32)
    qT = alloc("qT", [128, 4, NT], BF16, at=A0)
    kT = alloc("kT", [128, 4, NT], BF16, at=A0 + 18432)
    vaug = alloc("vaug", [128, 18, 4, 192], BF16, at=A0 + 36864)
    qT1 = alloc("qT1", [128, 8, SEQ], BF16, at=A0)
    kT1 = alloc("kT1", [128, 2, NT], BF16, at=A0 + 32768)
    vaug1 = alloc("vaug1", [128, 18, 2, 192], BF16, at=A0 + 41984)
    rC = alloc("rC", [128, SEQ], F32, at=A0 + 55808)
    rS = alloc("rS", [128, SEQ], F32, at=A0 + 55808 + 8192)
    bvb = alloc("bvb", [128, 512], F32, at=A0 + 64512)
    idb = alloc("idb", [128, 128], BF16, at=A0 + 64512 + 2048)
    cmean = alloc("cmean", [128, 256], F32, at=A0 + 64512 + 2304)
    crstd = alloc("crstd", [128, 256], F32, at=A0 + 64512 + 3328)
    UT = alloc("UT", [128, 8, NT], BF16)
    oT = alloc("oT", [128, 8, NT], BF16)
    C0 = cur[0]
    RT = {}

    def rtok(name, t0, t1):
        out = []
        for tt in range(t0 // 128, (t1 + 127) // 128):
            key = (name, tt)
            if key not in RT:
                RT[key] = Res("%s_%d" % key)
            out.append(RT[key])
        return out

    R_hpad = [Res("hpad%d" % c) for c in range(4)]

    def derive_vecs(l, col, tag):
        ops = []
        for ch in range(8):
            g1 = smc("ln_g", (l * 2 + 0) * 8 + ch)
            b1 = smc("ln_b", (l * 2 + 0) * 8 + ch)
            g2 = smc("ln_g", (l * 2 + 1) * 8 + ch)
            b2 = smc("ln_b", (l * 2 + 1) * 8 + ch)
            S.add("dve", (lambda e, ch=ch, g1=g1: e.tensor_tensor(out=vslot((tag, "G4"), ch), in0=g1, in1=mp1c(l, 4, ch, col), op=ALU.mult)),
                  reads=[R_const, R_mod], writes=[R_vecs])
            S.add("dve", (lambda e, ch=ch, b1=b1: e.scalar_tensor_tensor(out=vslot((tag, "B4"), ch), in0=b1, scalar=mp1c(l, 4, ch, col), in1=modc(l, 3, ch, col),
                                                                         op0=ALU.mult, op1=ALU.add)),
                  reads=[R_const, R_mod], writes=[R_vecs])
            S.add("dve", (lambda e, ch=ch, g1=g1: e.tensor_scalar_mul(out=vslot((tag, "GA1"), ch), in0=g1, scalar1=ALPHA)), reads=[R_const], writes=[R_vecs])
            S.add("dve", (lambda e, ch=ch, b1=b1: e.tensor_scalar_mul(out=vslot((tag, "BA1"), ch), in0=b1, scalar1=ALPHA)), reads=[R_const], writes=[R_vecs])
            if l == 0:
                S.add("dve", (lambda e, ch=ch: e.tensor_tensor(out=vslot((tag, "M2B"), ch), in0=modc(l, 2, ch, col), in1=smc("b_out", ch), op=ALU.mult)),
                      reads=[R_const, R_mod], writes=[R_vecs])
                S.add("dve", (lambda e, ch=ch, g2=g2: e.tensor_tensor(out=vslot((tag, "GU"), ch), in0=g2, in1=mp1c(1, 1, ch, col), op=ALU.mult)),
                      reads=[R_const, R_mod], writes=[R_vecs])
                S.add("dve", (lambda e, ch=ch, b2=b2: e.scalar_tensor_tensor(out=vslot((tag, "BU"), ch), in0=b2, scalar=mp1c(1, 1, ch, col), in1=modc(1, 0, ch, col),
                                                                             op0=ALU.mult, op1=ALU.add)),
                      reads=[R_const, R_mod], writes=[R_vecs])

    def ln_stats(pre_ap, nch, N, ones_t, prebf, presq, mean_sb, rstd_sb, bk, r_pre, r_tmp):
        S.add("dve", lambda e: e.tensor_copy(out=prebf[:, 0:nch, 0:N], in_=pre_ap), reads=r_pre, writes=[r_tmp[0]])
        S.add("act", lambda e: e.activation(out=presq[:, 0:nch, 0:N], in_=pre_ap, func=AF.Square), reads=r_pre, writes=[r_tmp[1]])
        for c in range(nch):
            S.add("pe", (lambda e, c=c: e.matmul(bank(bk)[:, 0:N], lhsT=ones_t[:], rhs=prebf[:, c, 0:N], start=(c == 0), stop=(c == nch - 1))),
                  reads=[r_tmp[0], R_const], writes=[RB[bk]])
        for c in range(nch):
            S.add("pe", (lambda e, c=c: e.matmul(bank(bk)[:, 256:256 + N], lhsT=ones_t[:], rhs=presq[:, c, 0:N], start=(c == 0), stop=(c == nch - 1))),
                  reads=[r_tmp[1], R_const], writes=[RB[bk]])
        S.add("act", lambda e: e.copy(out=mean_sb[:, 0:N], in_=bank(bk)[:, 0:N]), reads=[RB[bk]], writes=[r_tmp[2]])
        S.add("dve", lambda e: e.tensor_tensor(out=rstd_sb[:, 0:N], in0=mean_sb[:, 0:N], in1=mean_sb[:, 0:N], op=ALU.mult), reads=[r_tmp[2]], writes=[r_tmp[3]])
        S.add("dve", lambda e: e.tensor_tensor(out=rstd_sb[:, 0:N], in0=bank(bk)[:, 256:256 + N], in1=rstd_sb[:, 0:N], op=ALU.subtract),
              reads=[RB[bk], r_tmp[3]], writes=[r_tmp[3]])
        S.add("act", lambda e: e.activation(out=rstd_sb[:, 0:N], in_=rstd_sb[:, 0:N], func=AF.Sqrt, bias=smc("eps", 0), scale=1.0),
              reads=[r_tmp[3], R_const], writes=[r_tmp[3]])
        S.add("dve", lambda e: e.reciprocal(out=rstd_sb[:, 0:N], in_=rstd_sb[:, 0:N]), reads=[r_tmp[3]], writes=[r_tmp[3]])

    def normalize(pre_ap, nch, N, mean_sb, rstd_sb, r_pre, r_tmp):
        S.add("dve", lambda e: e.tensor_tensor(out=pre_ap, in0=pre_ap, in1=mean_sb[:, 0:N].unsqueeze(1).broadcast_to([128, nch, N]), op=ALU.subtract),
              reads=r_pre + [r_tmp[2]], writes=r_pre)
        S.add("dve", lambda e: e.tensor_tensor(out=pre_ap, in0=pre_ap, in1=rstd_sb[:, 0:N].unsqueeze(1).broadcast_to([128, nch, N]), op=ALU.mult),
              reads=r_pre + [r_tmp[3]], writes=r_pre)

    aff_rr = [0]

    def affine(out_ap, in_ap, sc, bi, reads, writes, psum_in=False):
        k = aff_rr[0] % 2
        aff_rr[0] += 1
        if k == 0:
            S.add("act", lambda e: e.activation(out=out_ap, in_=in_ap, func=AF.Identity, bias=bi, scale=sc), reads=reads, writes=writes)
        else:
            S.add("dve" if k == 1 else "pool", lambda e: e.tensor_scalar(out=out_ap, in0=in_ap, scalar1=sc, scalar2=bi, op0=ALU.mult, op1=ALU.add),
                  reads=reads, writes=writes)

    def dump(name, src_ap3):
        if stop != name:
            return
        S.barrier()
        c, t = src_ap3.shape[1], src_ap3.shape[2]
        dst = dbg if src_ap3.dtype == F32 else dbgb
        for ci in range(c):
            S.add("sp", (lambda e, ci=ci: e.dma_start(out=dst[:, ci * t:(ci + 1) * t], in_=src_ap3[:, ci, :])), writes=[Res("dbgo")], dma=True)
        raise _Stop()

    try:
      for b in range(nb):
        for l in range(nlayers):
              lat_only = (l == nlayers - 1) and l == 1
              col = b
              S.barrier()
              derive_vecs(l, b, "lat")
              if l == 0:
                  derive_vecs(l, 2, "ctx")

              def V(kind, ch, t0):
                  return vslot((("ctx" if (t0 < NCTX and l == 0) else "lat"), kind), ch)

              def mcol(t0):
                  return 2 if t0 < NCTX else b

              cur[0] = C0
              if l == 0:
                  xin = [alloc("xin%d" % i, [128, D], F32) for i in range(2)]
                  hblk = [alloc("hblk%d" % i, [128, 8, 128], F32) for i in range(2)]
                  R_xin = [Res("xin0"), Res("xin1")]
                  R_hblk = [Res("hblk0"), Res("hblk1")]
                  for tt in range(18):
                      s = tt % 2
                      src = ctx2[b, tt * 128:(tt + 1) * 128, :] if tt < 2 else x2[b, (tt - 2) * 128:(tt - 1) * 128, :]
                      S.add("sp", (lambda e, s=s, src=src: e.dma_start(out=xin[s][:], in_=src)), writes=[R_xin[s]], dma=True)
                      for ch in range(8):
                          bk = (tt % 2) * 2 + ch // 4
                          S.add("pe", (lambda e, s=s, ch=ch, bk=bk: e.transpose(bank(bk)[:, (ch % 4) * 128:(ch % 4 + 1) * 128], xin[s][:, ch * 128:(ch + 1) * 128], id32[:])),
                                reads=[R_xin[s], R_const], writes=[RB[bk]])
                      for hf in range(2):
                          bk = (tt % 2) * 2 + hf
                          S.add("act", (lambda e, s=s, hf=hf, bk=bk: e.copy(out=hblk[s][:, hf * 4:(hf + 1) * 4, :], in_=bank(bk).rearrange("p (c t) -> p c t", c=4))),
                                reads=[RB[bk]], writes=[R_hblk[s]])
                      mc = mcol(tt * 128)
                      for ch in range(8):
                          bk = (tt % 2) * 2 + ch // 4
                          affine(UT[:, ch, tt * 128:(tt + 1) * 128], bank(bk)[:, (ch % 4) * 128:(ch % 4 + 1) * 128], mp1c(0, 1, ch, mc), modc(0, 0, ch, mc),
                                 [RB[bk], R_mod], rtok("UT", tt * 128, tt * 128 + 128), psum_in=True)
                      S.add("sp", (lambda e, s=s, tt=tt: e.dma_start(out=Hs3[:, :, tt * 128:(tt + 1) * 128], in_=hblk[s][:])), reads=[R_hblk[s]],
                            writes=rtok("Hs", tt * 128, tt * 128 + 128), dma=True)

              if l == 0:
                  dump("S0", UT[:])
              blocks512 = [(0, 256)] + [(256 + 512 * i, 512) for i in range(4)]
              blocks256 = [(256 * i, 256) for i in range(9)]
              if l == 1:
                  lat512 = [(256 + 512 * i, 512) for i in range(4)]
                  lat256 = [(256 * i, 256) for i in range(1, 9)]

              if l == 0:
                  S.barrier()
                  cur[0] = C0
                  wAB = alloc("wAB", [128, 8, 1536], BF16)
                  wA = alloc("wA", [128, 8, 1024], BF16, at=C0)
                  hpd = [alloc("hpd%d" % i, [128, 2368], BF16, at=C0 + 16384 + i * 4736) for i in range(2)]
                  dg0 = alloc("diag0", [128, 31, 128], BF16, at=C0 + 25856)
                  diag = [dg0, dg0]
                  cur[0] = C0 + 33792
                  sgt = [alloc("sgt%d" % i, [128, 512], F32) for i in range(2)]
                  czsq = alloc("czsq", [128, 4, 256], BF16)
                  cz = alloc("cz", [128, 4, 256], F32)
                  R_wAB, R_misc = Res("wAB"), Res("misc0")
                  R_hpd = [Res("hpd0"), Res("hpd1")]
                  R_dg = [Res("dg0")] * 2
                  R_sgt = [Res("sgt0"), Res("sgt1")]
                  R_cz, R_ct = Res("cz"), [Res("czbf"), Res("czsq"), Res("cmean"), Res("crstd")]
                  w0 = w_in0.rearrange("(kc p) n -> p kc n", p=128)
                  S.add("pool", lambda e: e.dma_start(out=wA[:], in_=w0[:, :, 0:1024]), writes=[R_wAB], dma=True)
                  S.add("pool", lambda e: e.dma_start(out=idb[:], in_=ident), writes=[R_misc], dma=True)
                  S.add("sp", lambda e: e.dma_start(out=bvb[:], in_=bvbc), writes=[R_misc], dma=True)
                  S.add("pool", lambda e: e.memset(hpd[0][:], 0.0), writes=[R_hpd[0]])
                  S.add("pool", lambda e: e.memset(hpd[1][:], 0.0), writes=[R_hpd[1]])
                  S.add("pool", lambda e: e.memset(vaug[:], 1.0), writes=rtok("vaug", 0, NT))

                  def hoff(t0):
                      return 15 + t0 if t0 < NCTX else 286 + 15 + (t0 - NCTX)

                  it = 0
                  for cc in range(4):
                      hs_ = cc % 2
                      for k in range(31):
                          S.add("dve", (lambda e, cc=cc, k=k, hs_=hs_: e.tensor_scalar_mul(out=diag[hs_][:, k, :], in0=idb[:], scalar1=smc("conv_w", cc * 31 + k))),
                                reads=[R_misc, R_const], writes=[R_dg[hs_]])
                      for (t0, N) in blocks512:
                          s = it % 2
                          it += 1
                          b1, b2 = 2 * s, 2 * s + 1
                          for kc in range(8):
                              S.add("pe", (lambda e, kc=kc, cc=cc, t0=t0, N=N, b1=b1: e.matmul(bank(b1)[:, 0:N], lhsT=wA[:, kc, cc * 128:(cc + 1) * 128], rhs=UT[:, kc, t0:t0 + N],
                                                                                             start=(kc == 0), stop=(kc == 7))),
                                    reads=[R_wAB] + rtok("UT", t0, t0 + N), writes=[RB[b1]])
                          for kc in range(8):
                              S.add("pe", (lambda e, kc=kc, cc=cc, t0=t0, N=N, b2=b2: e.matmul(bank(b2)[:, 0:N], lhsT=wA[:, kc, 512 + cc * 128:512 + (cc + 1) * 128], rhs=UT[:, kc, t0:t0 + N],
                                                                                             start=(kc == 0), stop=(kc == 7))),
                                    reads=[R_wAB] + rtok("UT", t0, t0 + N), writes=[RB[b2]])
                          S.add("act", (lambda e, s=s, cc=cc, N=N, b2=b2: e.activation(out=sgt[s][:, 0:N], in_=bank(b2)[:, 0:N], func=AF.Sigmoid, bias=smc("b_in", 4 + cc), scale=1.0)),
                                reads=[RB[b2], R_const], writes=[R_sgt[s]])
                          ho = hoff(t0)
                          S.add("dve", (lambda e, s=s, cc=cc, N=N, b1=b1, ho=ho, hs_=hs_: e.scalar_tensor_tensor(out=hpd[hs_][:, ho:ho + N], in0=bank(b1)[:, 0:N], scalar=smc("b_in", cc),
                                                                                                                 in1=sgt[s][:, 0:N], op0=ALU.add, op1=ALU.mult)),
                                reads=[RB[b1], R_sgt[s], R_const], writes=[R_hpd[hs_]])
                      for bi, (t0, N) in enumerate(blocks512):
                          ho = hoff(t0) - 15
                          bk = 4 + bi % 2
                          for k in range(31):
                              S.add("pe", (lambda e, k=k, ho=ho, N=N, bk=bk, hs_=hs_: e.matmul(bank(bk)[:, 0:N], lhsT=diag[hs_][:, k, :], rhs=hpd[hs_][:, ho + k:ho + k + N],
                                                                                           start=(k == 0), stop=(k == 30))),
                                    reads=[R_dg[hs_], R_hpd[hs_]], writes=[RB[bk]])
                          S.add("act", (lambda e, cc=cc, N=N, t0=t0, bk=bk: e.activation(out=oT[:, cc, t0:t0 + N], in_=bank(bk)[:, 0:N], func=AF.Identity, bias=smc("conv_b", cc), scale=1.0)),
                                reads=[RB[bk], R_const], writes=rtok("oT", t0, t0 + N))
                  dump("S1z", oT[:, 0:4, :])
                  dump("S1h", hpd[1][:].unsqueeze(1))
                  dump("S1d", diag[0][:])
                  S.add("pool", lambda e: e.dma_start(out=wAB[:], in_=w0[:, :, 1024:2560]), writes=[R_wAB] + R_hpd + [R_dg[0]], dma=True)
                  for (t0, N) in blocks256:
                      zin = oT[:, 0:4, t0:t0 + 256]
                      rz = rtok("oT", t0, t0 + 256)
                      S.add("act", (lambda e, zin=zin: e.activation(out=czsq[:], in_=zin, func=AF.Square)), reads=rz, writes=[R_ct[1]])
                      for c4 in range(4):
                          S.add("pe", (lambda e, c4=c4, t0=t0: e.matmul(bank(6)[:, 0:256], lhsT=ones512[:], rhs=oT[:, c4, t0:t0 + 256], start=(c4 == 0), stop=(c4 == 3))),
                                reads=rz + [R_const], writes=[RB[6]])
                      for c4 in range(4):
                          S.add("pe", (lambda e, c4=c4: e.matmul(bank(6)[:, 256:512], lhsT=ones512[:], rhs=czsq[:, c4, :], start=(c4 == 0), stop=(c4 == 3))),
                                reads=[R_ct[1], R_const], writes=[RB[6]])
                      S.add("act", lambda e: e.copy(out=cmean[:], in_=bank(6)[:, 0:256]), reads=[RB[6]], writes=[R_ct[2]])
                      S.add("dve", lambda e: e.tensor_tensor(out=crstd[:], in0=cmean[:], in1=cmean[:], op=ALU.mult), reads=[R_ct[2]], writes=[R_ct[3]])
                      S.add("dve", lambda e: e.tensor_tensor(out=crstd[:], in0=bank(6)[:, 256:512], in1=crstd[:], op=ALU.subtract), reads=[RB[6], R_ct[3]], writes=[R_ct[3]])
                      S.add("act", lambda e: e.activation(out=crstd[:], in_=crstd[:], func=AF.Sqrt, bias=smc("eps", 0), scale=1.0), reads=[R_ct[3], R_const], writes=[R_ct[3]])
                      S.add("dve", lambda e: e.reciprocal(out=crstd[:], in_=crstd[:]), reads=[R_ct[3]], writes=[R_ct[3]])
                      S.add("dve", (lambda e, zin=zin: e.tensor_tensor(out=cz[:], in0=zin, in1=cmean[:].unsqueeze(1).broadcast_to([128, 4, 256]), op=ALU.subtract)),
                            reads=rz + [R_ct[2]], writes=[R_cz])
                      S.add("dve", lambda e: e.tensor_tensor(out=cz[:], in0=cz[:], in1=crstd[:].unsqueeze(1).broadcast_to([128, 4, 256]), op=ALU.mult),
                            reads=[R_cz, R_ct[3]], writes=[R_cz])
                      for c4 in range(4):
                          S.add("act", (lambda e, c4=c4, t0=t0: e.activation(out=oT[:, c4, t0:t0 + 256], in_=cz[:, c4, :], func=AF.Silu, bias=smc("cln_b", c4), scale=smc("cln_g", c4))),
                                reads=[R_cz, R_const], writes=rz)
                  it = 0
                  for (t0, N) in blocks512:
                      for c in range(8):
                          bk = it % 4
                          it += 1
                          for kc in range(8):
                              S.add("pe", (lambda e, kc=kc, c=c, t0=t0, N=N, bk=bk: e.matmul(bank(bk)[:, 0:N], lhsT=wAB[:, kc, c * 128:(c + 1) * 128], rhs=UT[:, kc, t0:t0 + N],
                                                                                           start=(kc == 0), stop=(kc == 7))),
                                    reads=[R_wAB] + rtok("UT", t0, t0 + N), writes=[RB[bk]])
                          dst = qT if c < 4 else kT
                          S.add("act", (lambda e, c=c, t0=t0, N=N, bk=bk, dst=dst: e.activation(out=dst[:, c % 4, t0:t0 + N], in_=bank(bk)[:, 0:N], func=AF.Identity,
                                                                                              bias=smc("b_in", 8 + c), scale=1.0)),
                                reads=[RB[bk], R_const], writes=rtok("qk", t0, t0 + N))
                  for tt in range(18):
                      bk = it % 4
                      it += 1
                      for kc in range(8):
                          S.add("pe", (lambda e, kc=kc, tt=tt, bk=bk: e.matmul(bank(bk)[:, 0:512], lhsT=UT[:, kc, tt * 128:(tt + 1) * 128], rhs=wAB[:, kc, 1024:1536],
                                                                             start=(kc == 0), stop=(kc == 7))),
                                reads=[R_wAB] + rtok("UT", tt * 128, tt * 128 + 128), writes=[RB[bk]])
                      for x in range(2):
                          S.add("dve", (lambda e, tt=tt, bk=bk, x=x: e.tensor_tensor(out=vaug[:, tt, :, x * 128:x * 128 + 64],
                                                                                   in0=bank(bk).rearrange("p (c x d) -> p c x d", c=4, x=2)[:, :, x, :],
                                                                                   in1=bvb[:].rearrange("p (c x d) -> p c x d", c=4, x=2)[:, :, x, :], op=ALU.add)),
                                reads=[RB[bk], R_misc], writes=rtok("vaug", tt * 128, tt * 128 + 128))

                  dump("S1o", oT[:, 0:4, :])
                  dump("S1q", qT[:])
                  dump("S1k", kT[:])
                  dump("S1v", vaug[:].rearrange("p t c x -> p t (c x)"))
                  S.barrier()
                  cur[0] = C0
                  nabt = [alloc("nabt%d" % i, [128, 21, 128], F32) for i in range(2)]
                  sbt = [alloc("sbt%d" % i, [128, 640], F32) for i in range(2)]
                  PT = [alloc("PT%d" % i, [128, 896], BF16) for i in range(2)]
                  rec = [alloc("rec%d" % i, [128, 256], F32) for i in range(2)]
                  R_nabt = [Res("nabt0"), Res("nabt1")]
                  R_sbt = [Res("sbt0"), Res("sbt1")]
                  R_PT = [Res("PT0"), Res("PT1")]
                  R_rec = [Res("rec0"), Res("rec1")]
                  it = 0
                  for h in range(8):
                      c, sh = h // 2, h % 2
                      p0, p1 = sh * 64, sh * 64 + 64
                      nh0, nh1 = (0, 64) if sh == 0 else (64, 128)
                      dh0, dh1 = (64, 128) if sh == 0 else (0, 64)
                      hs = h % 2
                      S.add("sp", (lambda e, h=h, hs=hs: e.dma_start(out=nabt[hs][:], in_=nab[h].rearrange("p (a q) -> p a q", a=21))), writes=[R_nabt[hs]], dma=True)
                      vcol = sh * 64
                      s = it % 2
                      it += 1
                      sb0 = 2 * s
                      for i in range(2):
                          S.add("pe", (lambda e, i=i, c=c, p0=p0, p1=p1, sb0=sb0: e.matmul(bank(sb0)[:, i * 256:(i + 1) * 256], lhsT=kT[p0:p1, c, i * 128:(i + 1) * 128],
                                                                                          rhs=qT[p0:p1, c, 0:256], start=True, stop=True)),
                                reads=rtok("qk", 0, 256), writes=[RB[sb0]])
                      S.add("act", (lambda e, s=s, sb0=sb0: e.activation(out=PT[s][:, 0:512], in_=bank(sb0)[:, 0:512], func=AF.Exp, scale=0.125)), reads=[RB[sb0]], writes=[R_PT[s]])
                      ob = 4 + s
                      for i in range(2):
                          S.add("pe", (lambda e, i=i, c=c, s=s, ob=ob, vcol=vcol: e.matmul(bank(ob)[:, 0:256], lhsT=vaug[:, i, c, vcol:vcol + 128], rhs=PT[s][:, i * 256:(i + 1) * 256],
                                                                                          start=(i == 0), stop=(i == 1))),
                                reads=[R_PT[s]] + rtok("vaug", 0, 256), writes=[RB[ob]])
                      S.add("dve", (lambda e, s=s, ob=ob, dh0=dh0, dh1=dh1: e.reciprocal(out=rec[s][dh0:dh1, 0:256], in_=bank(ob)[dh0:dh1, 0:256])), reads=[RB[ob]], writes=[R_rec[s]])
                      S.add("dve", (lambda e, s=s, ob=ob, c=c, nh0=nh0, nh1=nh1, dh0=dh0, dh1=dh1: e.tensor_tensor(out=oT[nh0:nh1, 4 + c, 0:256], in0=bank(ob)[nh0:nh1, 0:256],
                                                                                                               in1=rec[s][dh0:dh1, 0:256], op=ALU.mult)),
                            reads=[RB[ob], R_rec[s]], writes=rtok("oT", 0, 256))
                      for j in range(16):
                          s = it % 2
                          it += 1
                          sb0 = 2 * s
                          tl = _na_tiles(j)
                          nl = len(tl)
                          ti0 = _na_tile_index(j)
                          q0 = NCTX + j * 128
                          ktoks = [NCTX + a * 128 for a in tl] + [0, 128]
                          for i, kt0 in enumerate(ktoks):
                              bk = sb0 + (i // 4)
                              S.add("pe", (lambda e, i=i, kt0=kt0, bk=bk, c=c, p0=p0, p1=p1, q0=q0: e.matmul(bank(bk)[:, (i % 4) * 128:(i % 4 + 1) * 128], lhsT=kT[p0:p1, c, kt0:kt0 + 128],
                                                                                                        rhs=qT[p0:p1, c, q0:q0 + 128], start=True, stop=True)),
                                    reads=rtok("qk", kt0, kt0 + 128) + rtok("qk", q0, q0 + 128), writes=[RB[bk]])
                          S.add("dve", (lambda e, s=s, nl=nl, ti0=ti0, hs=hs: e.scalar_tensor_tensor(out=sbt[s][:, 0:nl * 128], in0=PS[s][:, 0:nl * 128], scalar=0.125,
                                                                                                    in1=nabt[hs][:, ti0:ti0 + nl, :].rearrange("p a q -> p (a q)"),
                                                                                                    op0=ALU.mult, op1=ALU.add)),
                                reads=[RB[sb0], RB[sb0 + 1], R_nabt[hs]], writes=[R_sbt[s]])
                          S.add("act", (lambda e, s=s, nl=nl: e.activation(out=PT[s][:, 0:nl * 128], in_=sbt[s][:, 0:nl * 128], func=AF.Exp)), reads=[R_sbt[s]], writes=[R_PT[s]])
                          S.add("act", (lambda e, s=s, nl=nl: e.activation(out=PT[s][:, nl * 128:(nl + 2) * 128], in_=PS[s][:, nl * 128:(nl + 2) * 128], func=AF.Exp, scale=0.125)),
                                reads=[RB[sb0], RB[sb0 + 1]], writes=[R_PT[s]])
                          ob = 4 + s
                          for i, kt0 in enumerate(ktoks):
                              S.add("pe", (lambda e, i=i, kt0=kt0, c=c, s=s, ob=ob, vcol=vcol, nl=nl: e.matmul(bank(ob)[:, 0:128], lhsT=vaug[:, kt0 // 128, c, vcol:vcol + 128],
                                                                                                          rhs=PT[s][:, i * 128:(i + 1) * 128], start=(i == 0), stop=(i == nl + 1))),
                                    reads=[R_PT[s]] + rtok("vaug", kt0, kt0 + 128), writes=[RB[ob]])
                          S.add("dve", (lambda e, s=s, ob=ob, dh0=dh0, dh1=dh1: e.reciprocal(out=rec[s][dh0:dh1, 0:128], in_=bank(ob)[dh0:dh1, 0:128])), reads=[RB[ob]], writes=[R_rec[s]])
                          S.add("dve", (lambda e, s=s, ob=ob, c=c, q0=q0, nh0=nh0, nh1=nh1, dh0=dh0, dh1=dh1: e.tensor_tensor(out=oT[nh0:nh1, 4 + c, q0:q0 + 128], in0=bank(ob)[nh0:nh1, 0:128],
                                                                                                                     in1=rec[s][dh0:dh1, 0:128], op=ALU.mult)),
                                reads=[RB[ob], R_rec[s]], writes=rtok("oT", q0, q0 + 128))
                  dump("S3", oT[:, 4:8, :])
                  w_out_d = w_out0
                  tok_blocks = blocks256
              else:
                  S.barrier()
                  cur[0] = C0
                  wq = alloc("wq", [128, 8, 512], BF16)
                  wqs = alloc("wqs", [128, 8, 512], BF16)
                  wk = alloc("wk", [128, 8, 256], BF16)
                  wks = alloc("wks", [128, 8, 256], BF16)
                  wv = alloc("wv", [128, 8, 256], BF16)
                  rt = [alloc("rt%d" % i, [128, 2, 512], F32) for i in range(2)]
                  R_w1, R_rope = Res("w1"), Res("rope")
                  R_rt = [Res("rt0"), Res("rt1")]
                  R_wq = Res("wq")
                  for dst, src in ((wk, wk1), (wks, wks1), (wv, wv1)):
                      S.add("pool", (lambda e, dst=dst, src=src: e.dma_start(out=dst[:], in_=src.rearrange("(kc p) n -> p kc n", p=128))), writes=[R_w1], dma=True)
                  S.add("sp", lambda e: e.dma_start(out=rC[:], in_=ropeC), writes=[R_rope], dma=True)
                  S.add("sp", lambda e: e.dma_start(out=rS[:], in_=ropeS), writes=[R_rope], dma=True)
                  if True:
                      S.add("pool", lambda e: e.memset(vaug1[:], 1.0), writes=rtok("vaug", 0, NT))
                  it = 0
                  for half, (t0, N) in [(hf_, blk_) for hf_ in range(3) for blk_ in lat512]:
                      l0 = t0 - NCTX
                      if half < 2 and t0 == NCTX:
                          for dst, src in ((wq, wq1), (wqs, wqs1)):
                              S.add("pool", (lambda e, dst=dst, src=src, half=half: e.dma_start(out=dst[:], in_=src.rearrange("(kc p) n -> p kc n", p=128)[:, :, half * 512:(half + 1) * 512])),
                                    writes=[R_wq], dma=True)
                      for c in (range(half * 4, half * 4 + 4) if half < 2 else range(8, 10)):
                          s = it % 2
                          it += 1
                          b1, b2 = 2 * s, 2 * s + 1
                          wa, wb = (wq, wqs) if c < 8 else (wk, wks)
                          cc = (c % 4) if c < 8 else c - 8
                          for kc in range(8):
                              S.add("pe", (lambda e, kc=kc, cc=cc, wa=wa, t0=t0, N=N, b1=b1: e.matmul(bank(b1)[:, 0:N], lhsT=wa[:, kc, cc * 128:(cc + 1) * 128], rhs=UT[:, kc, t0:t0 + N],
                                                                                                 start=(kc == 0), stop=(kc == 7))),
                                    reads=[R_w1, R_wq] + rtok("UT", t0, t0 + N), writes=[RB[b1]])
                          for kc in range(8):
                              S.add("pe", (lambda e, kc=kc, cc=cc, wb=wb, t0=t0, N=N, b2=b2: e.matmul(bank(b2)[:, 0:N], lhsT=wb[:, kc, cc * 128:(cc + 1) * 128], rhs=UT[:, kc, t0:t0 + N],
                                                                                                 start=(kc == 0), stop=(kc == 7))),
                                    reads=[R_w1, R_wq] + rtok("UT", t0, t0 + N), writes=[RB[b2]])
                          S.add("dve", (lambda e, s=s, l0=l0, N=N, b1=b1: e.tensor_tensor(out=rt[s][:, 0, 0:N], in0=bank(b1)[:, 0:N], in1=rC[:, l0:l0 + N], op=ALU.mult)),
                                reads=[RB[b1], R_rope], writes=[R_rt[s]])
                          S.add("dve", (lambda e, s=s, l0=l0, N=N, b2=b2: e.tensor_tensor(out=rt[s][:, 1, 0:N], in0=bank(b2)[:, 0:N], in1=rS[:, l0:l0 + N], op=ALU.mult)),
                                reads=[RB[b2], R_rope], writes=[R_rt[s]])
                          if c < 8:
                              dst = qT1[:, c, l0:l0 + N]
                          else:
                              dst = kT1[:, c - 8, t0:t0 + N]
                          S.add("dve", (lambda e, s=s, N=N, dst=dst: e.tensor_tensor(out=dst, in0=rt[s][:, 0, 0:N], in1=rt[s][:, 1, 0:N], op=ALU.add)),
                                reads=[R_rt[s]], writes=rtok("qk", t0, t0 + N))
                  for cc in range(2):
                      s = it % 2
                      it += 1
                      b1 = 2 * s
                      for kc in range(8):
                          S.add("pe", (lambda e, kc=kc, cc=cc, b1=b1: e.matmul(bank(b1)[:, 0:256], lhsT=wk[:, kc, cc * 128:(cc + 1) * 128], rhs=UT[:, kc, 0:256], start=(kc == 0), stop=(kc == 7))),
                                reads=[R_w1] + rtok("UT", 0, 256), writes=[RB[b1]])
                      S.add("act", (lambda e, cc=cc, b1=b1: e.copy(out=kT1[:, cc, 0:256], in_=bank(b1)[:, 0:256])), reads=[RB[b1]], writes=rtok("qk", 0, 256))
                  for tt in range(18):
                      bk = 4 + tt % 2
                      for kc in range(8):
                          S.add("pe", (lambda e, kc=kc, tt=tt, bk=bk: e.matmul(bank(bk)[:, 0:256], lhsT=UT[:, kc, tt * 128:(tt + 1) * 128], rhs=wv[:, kc, :], start=(kc == 0), stop=(kc == 7))),
                                reads=[R_w1] + rtok("UT", tt * 128, tt * 128 + 128), writes=[RB[bk]])
                      for x in range(2):
                          S.add("act", (lambda e, tt=tt, bk=bk, x=x: e.copy(out=vaug1[:, tt, :, x * 128:x * 128 + 64],
                                                                          in_=bank(bk)[:, 0:256].rearrange("p (c x d) -> p c x d", c=2, x=2)[:, :, x, :])),
                                reads=[RB[bk]], writes=rtok("vaug", tt * 128, tt * 128 + 128))
                  S.barrier()
                  cur[0] = C0
                  mlu = alloc("mlu", [128, 256], F32)
                  S.add("sp", lambda e: e.dma_start(out=mlu[:], in_=maskLU), writes=[R_const], dma=True)
                  sbm = [alloc("sbm%d" % i, [128, 512], F32) for i in range(2)]
                  PT1 = [alloc("PT1_%d" % i, [128, 5, 512], BF16) for i in range(2)]
                  rec1 = [alloc("rec1_%d" % i, [128, 512], F32) for i in range(2)]
                  R_sbm = [Res("sbm0"), Res("sbm1")]
                  R_PT1 = [[Res("PT1_%d_%d" % (i, k)) for k in range(5)] for i in range(2)]
                  R_rec1 = [Res("rec1_0"), Res("rec1_1")]
                  it = 0
                  sbr = 0
                  mi = 0
                  for g in range(4):
                      m, sh = g // 2, g % 2
                      p0, p1 = sh * 64, sh * 64 + 64
                      nh0, nh1 = (0, 64) if sh == 0 else (64, 128)
                      dh0, dh1 = (64, 128) if sh == 0 else (0, 64)
                      vcol = sh * 64
                      for qb in range(16):
                          s = it % 2
                          it += 1
                          tiles = []
                          if qb > 0:
                              tiles.append((NCTX + (qb - 1) * 128, 0))
                          tiles.append((NCTX + qb * 128, None))
                          if qb < 15:
                              tiles.append((NCTX + (qb + 1) * 128, 1))
                          tiles += [(0, None), (128, None)]
                          nt = len(tiles)
                          for i, (kt0, mk) in enumerate(tiles):
                              bk = sbr % 4
                              sbr += 1
                              S.add("pe", (lambda e, kt0=kt0, bk=bk, m=m, p0=p0, p1=p1, qb=qb: e.matmul(bank(bk).rearrange("p (h q) -> p h q", h=4), lhsT=kT1[p0:p1, m, kt0:kt0 + 128],
                                                                                                   rhs=qT1[p0:p1, 4 * m:4 * m + 4, qb * 128:(qb + 1) * 128], start=True, stop=True)),
                                    reads=rtok("qk", kt0, kt0 + 128) + rtok("qk", NCTX + qb * 128, NCTX + qb * 128 + 128), writes=[RB[bk]])
                              if mk is None:
                                  S.add("act", (lambda e, s=s, i=i, bk=bk: e.activation(out=PT1[s][:, i, :], in_=bank(bk), func=AF.Exp, scale=0.125)), reads=[RB[bk]], writes=[R_PT1[s][i]])
                              else:
                                  ms = mi % 2
                                  mi += 1
                                  S.add("dve", (lambda e, ms=ms, mk=mk, bk=bk: e.scalar_tensor_tensor(out=sbm[ms][:].rearrange("p (h q) -> p h q", h=4), in0=bank(bk).rearrange("p (h q) -> p h q", h=4),
                                                                                                    scalar=0.125, in1=mlu[:, mk * 128:(mk + 1) * 128].unsqueeze(1).broadcast_to([128, 4, 128]),
                                                                                                    op0=ALU.mult, op1=ALU.add)),
                                        reads=[RB[bk], R_const], writes=[R_sbm[ms]])
                                  S.add("act", (lambda e, s=s, i=i, ms=ms: e.activation(out=PT1[s][:, i, :], in_=sbm[ms][:], func=AF.Exp)), reads=[R_sbm[ms]], writes=[R_PT1[s][i]])
                          ob = 4 + s
                          for i, (kt0, mk) in enumerate(tiles):
                              S.add("pe", (lambda e, i=i, kt0=kt0, s=s, ob=ob, m=m, vcol=vcol, nt=nt: e.matmul(bank(ob), lhsT=vaug1[:, kt0 // 128, m, vcol:vcol + 128], rhs=PT1[s][:, i, :],
                                                                                                          start=(i == 0), stop=(i == nt - 1))),
                                    reads=[R_PT1[s][i]] + rtok("vaug", kt0, kt0 + 128), writes=[RB[ob]])
                          S.add("dve", (lambda e, s=s, ob=ob, m=m, sh=sh, dh0=dh0, dh1=dh1: e.tensor_tensor(out=rec1[s][dh0:dh1, :].rearrange("p (h q) -> p h q", h=4),
                                                                                                        in0=bank(ob)[dh0:dh1, :].rearrange("p (h q) -> p h q", h=4),
                                                                                                        in1=esink[dh0:dh1, (m * 2 + sh) * 4:(m * 2 + sh) * 4 + 4].unsqueeze(2).broadcast_to([64, 4, 128]),
                                                                                                        op=ALU.add)),
                                reads=[RB[ob], R_const], writes=[R_rec1[s]])
                          S.add("dve", (lambda e, s=s, dh0=dh0, dh1=dh1: e.reciprocal(out=rec1[s][dh0:dh1, :], in_=rec1[s][dh0:dh1, :])), reads=[R_rec1[s]], writes=[R_rec1[s]])
                          q0 = NCTX + qb * 128
                          S.add("dve", (lambda e, s=s, ob=ob, m=m, q0=q0, nh0=nh0, nh1=nh1, dh0=dh0, dh1=dh1: e.tensor_tensor(out=oT[nh0:nh1, 4 * m:4 * m + 4, q0:q0 + 128],
                                                                                                                     in0=bank(ob)[nh0:nh1, :].rearrange("p (h q) -> p h q", h=4),
                                                                                                                     in1=rec1[s][dh0:dh1, :].rearrange("p (h q) -> p h q", h=4), op=ALU.mult)),
                                reads=[RB[ob], R_rec1[s]], writes=rtok("oT", q0, q0 + 128))
                  w_out_d = w_out1
                  tok_blocks = lat256

              S.barrier()
              cur[0] = C0
              gatesT = alloc("gatesT", [16, NT], F32)
              wo = alloc("wo", [128, 8, D], BF16)
              hold0_at = cur[0]
              hold = [alloc("hold%d" % i, [128, 8, 128], F32) for i in range(2)]
              pre = alloc("pre", [128, 8, 128], F32)
              prebf = alloc("prebf", [128, 8, 128], BF16)
              presq = alloc("presq", [128, 8, 128], BF16)
              t32 = alloc("t32", [128, 8, 128], F32)
              tmpo = [alloc("tmpo%d" % i, [128, 128], F32) for i in range(2)]
              mean_sb = alloc("mean_sb", [128, 128], F32)
              rstd_sb = alloc("rstd_sb", [128, 128], F32)
              lsb = alloc("lsb", [128, 18, 20], F32, at=hold0_at)
              rw = alloc("rw", [128, 18 * 96], F32, at=hold0_at + 1472)
              assert hold0_at + 1472 + 18 * 96 * 4 <= cur[0]
              R_wo = Res("wo")
              R_hold = [Res("hold0"), Res("hold1")]
              R_pre, R_t32 = Res("pre"), Res("t32")
              R_tmpo = [Res("tmpo0"), Res("tmpo1")]
              R_lt = [Res("prebf"), Res("presq"), Res("mean"), Res("rstd")]
              R_rout = Res("rout")
              S.add("pool", (lambda e, w_out_d=w_out_d: e.dma_start(out=wo[:], in_=w_out_d.rearrange("(kc p) n -> p kc n", p=128))), writes=[R_wo], dma=True)
              tiles4 = [t for (t0_, n_) in tok_blocks for t in range(t0_, t0_ + n_, 128)]
              for bi, t0 in enumerate(tiles4):
                  s = bi % 2
                  mc = mcol(t0)
                  tt = t0 // 128
                  S.add("sp", (lambda e, s=s, t0=t0: e.dma_start(out=hold[s][:], in_=Hs3[:, :, t0:t0 + 128])), reads=rtok("Hs", t0, t0 + 128), writes=[R_hold[s]], dma=True)
                  for oc in range(8):
                      bk = oc // 4
                      co = (oc % 4) * 128
                      for kc in range(8):
                          S.add("pe", (lambda e, kc=kc, oc=oc, bk=bk, co=co, t0=t0: e.matmul(bank(bk)[:, co:co + 128], lhsT=wo[:, kc, oc * 128:(oc + 1) * 128], rhs=oT[:, kc, t0:t0 + 128],
                                                                                           start=(kc == 0), stop=(kc == 7))),
                                reads=[R_wo] + rtok("oT", t0, t0 + 128), writes=[RB[bk]])
                  for oc in range(8):
                      bk = oc // 4
                      co = (oc % 4) * 128
                      ts = oc % 2
                      if l == 0:
                          vb = V("M2B", oc, t0)
                          S.add("act", (lambda e, oc=oc, bk=bk, co=co, ts=ts, mc=mc, vb=vb: e.activation(out=tmpo[ts][:], in_=bank(bk)[:, co:co + 128], func=AF.Identity,
                                                                                                   bias=vb, scale=modc(0, 2, oc, mc))),
                                reads=[RB[bk], R_mod, R_vecs], writes=[R_tmpo[ts]])
                      else:
                          S.add("act", (lambda e, oc=oc, bk=bk, co=co, ts=ts, mc=mc: e.activation(out=tmpo[ts][:], in_=bank(bk)[:, co:co + 128], func=AF.Identity,
                                                                                            scale=modc(1, 2, oc, mc))),
                                reads=[RB[bk], R_mod], writes=[R_tmpo[ts]])
                      S.add("dve", (lambda e, oc=oc, s=s, ts=ts: e.scalar_tensor_tensor(out=pre[:, oc, :], in0=hold[s][:, oc, :], scalar=ALPHA, in1=tmpo[ts][:], op0=ALU.mult, op1=ALU.add)),
                            reads=[R_hold[s], R_tmpo[ts]], writes=[R_pre])
                  ln_stats(pre[:], 8, 128, ones1k, prebf, presq, mean_sb, rstd_sb, 4, [R_pre], R_lt)
                  normalize(pre[:], 8, 128, mean_sb, rstd_sb, [R_pre], R_lt)
                  for ch in range(8):
                      affine(UT[:, ch, t0:t0 + 128], pre[:, ch, :], V("G4", ch, t0), V("B4", ch, t0), [R_pre, R_vecs], rtok("UT", t0, t0 + 128))
                      affine(t32[:, ch, :], pre[:, ch, :], V("G4", ch, t0), V("B4", ch, t0), [R_pre, R_vecs], [R_t32])
                      affine(hres[:, ch, t0:t0 + 128], pre[:, ch, :], V("GA1", ch, t0), V("BA1", ch, t0), [R_pre, R_vecs], rtok("hres", t0, t0 + 128))
                  for kc in range(8):
                      S.add("pe", (lambda e, kc=kc, tt=tt, l=l: e.matmul(bank(5)[:, tt * 20:tt * 20 + 20], lhsT=t32[:, kc, :], rhs=wrt[:, l * 160 + kc * 20:l * 160 + kc * 20 + 20],
                                                                        start=(kc == 0), stop=(kc == 7))),
                            reads=[R_t32, R_const], writes=[RB[5]])

              dump("S4", hres[:])
              dump("S4u", UT[:])
              S.barrier()
              T0 = tok_blocks[0][0] // 128
              T1 = 18
              nT = T1 - T0
              S.add("dve", lambda e: e.tensor_copy(out=lsb[:, T0:T1, :], in_=bank(5)[:, T0 * 20:T1 * 20].rearrange("p (t n) -> p t n", n=20)), reads=[RB[5]], writes=[R_rout])

              def rwv(i, n):
                  return rw[:, i * 18 * 4:(i * 18 * 4) + 18 * n].rearrange("p (t n) -> p t n", n=n)[:, T0:T1, :]

              def rop(fn):
                  S.add("dve", fn, reads=[R_rout], writes=[R_rout])

              lg = lsb[:, T0:T1, 0:4]
              le = lsb[:, T0:T1, 4:20].rearrange("p t (g x) -> p t g x", g=4)
              gmax, gsum, gp, m1, m2, dd, w1, w2 = [rwv(i, 1) for i in range(8)]
              gsh, gmask, elsel, mask1, el2, mask2, within, wa_ = [rwv(8 + i, 4) for i in range(8)]
              t44 = rw[:, 18 * 64:18 * 80].rearrange("p (t g x) -> p t g x", g=4, x=4)[:, T0:T1]
              gates = rw[:, 18 * 80:18 * 96].rearrange("p (t g x) -> p t g x", g=4, x=4)
              bc4 = lambda a: a.broadcast_to([128, nT, 4])
              rop(lambda e: e.tensor_reduce(out=gmax, in_=lg, axis=AX.X, op=ALU.max))
              rop(lambda e: e.tensor_tensor(out=gsh, in0=lg, in1=bc4(gmax), op=ALU.subtract))
              rop(lambda e: e.tensor_tensor(out=gmask, in0=lg, in1=bc4(gmax), op=ALU.is_equal))
              S.add("act", lambda e: e.activation(out=gsh, in_=gsh, func=AF.Exp), reads=[R_rout], writes=[R_rout])
              rop(lambda e: e.tensor_reduce(out=gsum, in_=gsh, axis=AX.X, op=ALU.add))
              rop(lambda e: e.reciprocal(out=gp, in_=gsum))
              rop(lambda e: e.tensor_tensor(out=t44, in0=le, in1=gmask.unsqueeze(3).broadcast_to([128, nT, 4, 4]), op=ALU.mult))
              rop(lambda e: e.tensor_reduce(out=elsel, in_=t44.rearrange("p t g x -> p t x g"), axis=AX.X, op=ALU.add))
              rop(lambda e: e.tensor_reduce(out=m1, in_=elsel, axis=AX.X, op=ALU.max))
              rop(lambda e: e.tensor_tensor(out=mask1, in0=elsel, in1=bc4(m1), op=ALU.is_equal))
              rop(lambda e: e.scalar_tensor_tensor(out=el2, in0=mask1, scalar=NEG, in1=elsel, op0=ALU.mult, op1=ALU.add))
              rop(lambda e: e.tensor_reduce(out=m2, in_=el2, axis=AX.X, op=ALU.max))
              rop(lambda e: e.tensor_tensor(out=mask2, in0=el2, in1=bc4(m2), op=ALU.is_equal))
              rop(lambda e: e.tensor_tensor(out=dd, in0=m2, in1=m1, op=ALU.subtract))
              S.add("act", lambda e: e.activation(out=dd, in_=dd, func=AF.Exp), reads=[R_rout], writes=[R_rout])
              rop(lambda e: e.tensor_scalar_add(out=w1, in0=dd, scalar1=1.0))
              rop(lambda e: e.reciprocal(out=w1, in_=w1))
              rop(lambda e: e.tensor_tensor(out=w1, in0=w1, in1=gp, op=ALU.mult))
              rop(lambda e: e.tensor_tensor(out=w2, in0=dd, in1=w1, op=ALU.mult))
              rop(lambda e: e.tensor_tensor(out=within, in0=mask1, in1=bc4(w1), op=ALU.mult))
              rop(lambda e: e.tensor_tensor(out=wa_, in0=mask2, in1=bc4(w2), op=ALU.mult))
              rop(lambda e: e.tensor_tensor(out=within, in0=within, in1=wa_, op=ALU.add))
              rop(lambda e: e.tensor_tensor(out=gates[:, T0:T1], in0=gmask.unsqueeze(3).broadcast_to([128, nT, 4, 4]), in1=within.unsqueeze(2).broadcast_to([128, nT, 4, 4]), op=ALU.mult))
              for tt in range(T0, T1):
                  bk = 6 + (tt // 4) % 2
                  S.add("pe", (lambda e, tt=tt, bk=bk: e.transpose(bank(bk)[0:16, (tt % 4) * 128:(tt % 4 + 1) * 128], gates[:, tt].rearrange("p g x -> p (g x)"), id32[:])),
                        reads=[R_rout, R_const], writes=[RB[bk]])
                  if tt % 4 == 3 or tt == T1 - 1:
                      ta = (tt // 4) * 4
                      ta0 = max(ta, T0)
                      S.add("act", (lambda e, bk=bk, ta=ta, ta0=ta0, tt=tt: e.copy(out=gatesT[0:16, ta0 * 128:(tt + 1) * 128], in_=bank(bk)[0:16, (ta0 - ta) * 128:(tt + 1 - ta) * 128])),
                            reads=[RB[bk]], writes=[R_rout])

              S.barrier()
              cur[0] = C0
              gatesT = alloc("gatesT", [16, NT], F32)
              wgs = [alloc("wgs%d" % i, [128, 8, 256], BF16) for i in range(2)]
              wus = [alloc("wus%d" % i, [128, 8, 256], BF16) for i in range(2)]
              wds = [alloc("wds%d" % i, [128, 2, D], BF16) for i in range(2)]
              sgm = [alloc("sgm%d" % i, [128, 2, 512], F32) for i in range(2)]
              hgm = [alloc("hgm%d" % i, [128, 2, 512], BF16) for i in range(2)]
              R_ew = [Res("ew0"), Res("ew1")]
              R_sgm = [Res("sgm0"), Res("sgm1")]
              R_hgm = [Res("hgm0"), Res("hgm1")]
              mblocks = blocks512 if l == 0 else lat512
              it = 0
              yb = 0
              for ex in range(16):
                  s = ex % 2
                  S.add("pool", (lambda e, s=s, ex=ex, l=l: e.dma_start(out=wgs[s][:], in_=ewg[l, ex].rearrange("(kc p) f -> p kc f", p=128))), writes=[R_ew[s]], dma=True)
                  S.add("pool", (lambda e, s=s, ex=ex, l=l: e.dma_start(out=wus[s][:], in_=ewu[l, ex].rearrange("(kc p) f -> p kc f", p=128))), writes=[R_ew[s]], dma=True)
                  S.add("pool", (lambda e, s=s, ex=ex, l=l: e.dma_start(out=wds[s][:], in_=ewd[l, ex].rearrange("(kc p) f -> p kc f", p=128))), writes=[R_ew[s]], dma=True)
                  for (t0, N) in mblocks:
                      q = it % 2
                      it += 1
                      mc = mcol(t0)
                      S.add("pe", (lambda e, ex=ex, t0=t0, N=N: e.matmul(bank(4)[:, 0:N], lhsT=selt[0:16, ex * 128:(ex + 1) * 128], rhs=gatesT[0:16, t0:t0 + N], start=True, stop=True)),
                            reads=[R_rout, R_const], writes=[RB[4]])
                      for oc in range(4):
                          wsrc = wgs[s] if oc < 2 else wus[s]
                          for kc in range(8):
                              S.add("pe", (lambda e, kc=kc, oc=oc, wsrc=wsrc, t0=t0, N=N: e.matmul(bank(oc)[:, 0:N], lhsT=wsrc[:, kc, (oc % 2) * 128:(oc % 2 + 1) * 128], rhs=UT[:, kc, t0:t0 + N],
                                                                                              start=(kc == 0), stop=(kc == 7))),
                                    reads=[R_ew[s]] + rtok("UT", t0, t0 + N), writes=[RB[oc]])
                      S.add("act", (lambda e, q=q, N=N: e.activation(out=sgm[q][:, :, 0:N], in_=PS[0][:].rearrange("p (j n) -> p j n", j=2)[:, :, 0:N], func=AF.Silu)),
                            reads=[RB[0], RB[1]], writes=[R_sgm[q]])
                      S.add("dve", (lambda e, q=q, N=N: e.tensor_tensor(out=sgm[q][:, :, 0:N], in0=sgm[q][:, :, 0:N], in1=PS[1][:].rearrange("p (j n) -> p j n", j=2)[:, :, 0:N], op=ALU.mult)),
                            reads=[RB[2], RB[3], R_sgm[q]], writes=[R_sgm[q]])
                      S.add("dve", (lambda e, q=q, N=N: e.tensor_tensor(out=hgm[q][:, :, 0:N], in0=sgm[q][:, :, 0:N], in1=bank(4)[:, 0:N].unsqueeze(1).broadcast_to([128, 2, N]), op=ALU.mult)),
                            reads=[RB[4], R_sgm[q]], writes=[R_hgm[q]])
                      for dc in range(8):
                          bk = 5 + yb % 3
                          yb += 1
                          for k2 in range(2):
                              S.add("pe", (lambda e, k2=k2, dc=dc, bk=bk, s=s, q=q, N=N: e.matmul(bank(bk)[:, 0:N], lhsT=wds[s][:, k2, dc * 128:(dc + 1) * 128], rhs=hgm[q][:, k2, 0:N],
                                                                                             start=(k2 == 0), stop=(k2 == 1))),
                                    reads=[R_ew[s], R_hgm[q]], writes=[RB[bk]])
                          S.add("dve", (lambda e, dc=dc, bk=bk, t0=t0, N=N, mc=mc, l=l: e.scalar_tensor_tensor(out=hres[:, dc, t0:t0 + N], in0=bank(bk)[:, 0:N], scalar=modc(l, 5, dc, mc),
                                                                                                       in1=hres[:, dc, t0:t0 + N], op0=ALU.mult, op1=ALU.add)),
                                reads=[RB[bk], R_mod] + rtok("hres", t0, t0 + N), writes=rtok("hres", t0, t0 + N))

              dump("S5", hres[:])
              S.barrier()
              cur[0] = C0
              pre2 = alloc("pre2", [128, 8, 256], BF16)
              presq2 = alloc("presq2", [128, 8, 256], BF16)
              mean2 = alloc("mean2", [128, 256], F32)
              rstd2 = alloc("rstd2", [128, 256], F32)
              otile = [alloc("otile%d" % i, [128, D], F32) for i in range(2)]
              R_l2 = [Res("pre2"), Res("presq2"), Res("mean2"), Res("rstd2")]
              R_ot = [Res("ot0"), Res("ot1")]
              oi = 0
              for bi, (t0, N) in enumerate(tok_blocks):
                  hap = hres[:, :, t0:t0 + 256]
                  rh = rtok("hres", t0, t0 + 256)
                  ln_stats(hap, 8, 256, ones1k, pre2, presq2, mean2, rstd2, 4, rh, R_l2)
                  normalize(hap, 8, 256, mean2, rstd2, rh, R_l2)
                  if l == 0:
                      for ch in range(8):
                          affine(UT[:, ch, t0:t0 + 256], hres[:, ch, t0:t0 + 256], V("GU", ch, t0), V("BU", ch, t0), rh + [R_vecs], rtok("UT", t0, t0 + 256))
                      for ch in range(8):
                          affine(hres[:, ch, t0:t0 + 256], hres[:, ch, t0:t0 + 256], smc("ln_g", 8 + ch), smc("ln_b", 8 + ch), rh + [R_const], rh)
                      S.add("sp", (lambda e, t0=t0: e.dma_start(out=Hs3[:, :, t0:t0 + 256], in_=hres[:, :, t0:t0 + 256])), reads=rh, writes=rtok("Hs", t0, t0 + 256), dma=True)
                      if debug and nlayers == 1:
                          S.add("sp", (lambda e, t0=t0: e.dma_start(out=dbg.rearrange("p (c t) -> p c t", c=8)[:, :, t0:t0 + 256], in_=hres[:, :, t0:t0 + 256])), reads=rh,
                                writes=[Res("dbgo")], dma=True)
                  else:
                      for ch in range(8):
                          affine(hres[:, ch, t0:t0 + 256], hres[:, ch, t0:t0 + 256], smc("ln_g", 24 + ch), smc("ln_b", 24 + ch), rh + [R_const], rh)
                      for hh in range(2):
                          tk = t0 + hh * 128
                          so = oi % 2
                          oi += 1
                          for ch in range(8):
                              bk = so * 2 + ch // 4
                              S.add("pe", (lambda e, ch=ch, bk=bk, tk=tk: e.transpose(bank(bk)[:, (ch % 4) * 128:(ch % 4 + 1) * 128], hres[:, ch, tk:tk + 128], id32[:])),
                                    reads=rh + [R_const], writes=[RB[bk]])
                          for hf in range(2):
                              bk = so * 2 + hf
                              S.add("act" if hf else "dve", (lambda e, so=so, hf=hf, bk=bk: (e.copy if hf else e.tensor_copy)(out=otile[so][:, hf * 512:(hf + 1) * 512], in_=bank(bk))),
                                    reads=[RB[bk]], writes=[R_ot[so]])
                          S.add("sp", (lambda e, so=so, tk=tk, b=b: e.dma_start(out=outd[b, tk - NCTX:tk - NCTX + 128, :], in_=otile[so][:])), reads=[R_ot[so]], writes=[Res("outw")], dma=True)
    except _Stop:
        pass
    S.barrier()

    with nc.Block() as block:
        @block.tensor
        def _(e):
            S.emit_one("pe", e, esem, dsems)

        @block.scalar
        def _(e):
            S.emit_one("act", e, esem, dsems)

        @block.vector
        def _(e):
            S.emit_one("dve", e, esem, dsems)

        @block.gpsimd
        def _(e):
            S.emit_one("pool", e, esem, dsems)

        @block.sync
        def _(e):
            S.emit_one("sp", e, esem, dsems)
    es.close()
    return nc


def _prep_shared(inp):
    f = lambda a: np.ascontiguousarray(np.asarray(a, np.float32))
    sm = np.zeros((128, SMN), np.float32)

    def put(name, arr):
        arr = np.asarray(arr, np.float32)
        sm[:, SMO[name]:SMO[name] + arr.shape[1]] = arr

    put("ada_b0", _fm(inp["ada_b"][0]))
    put("ada_b1", _fm(inp["ada_b"][1]))
    put("ln_g", np.concatenate([_fm(inp["ln_g"][l, k]) for l in range(2) for k in range(2)], axis=1))
    put("ln_b", np.concatenate([_fm(inp["ln_b"][l, k]) for l in range(2) for k in range(2)], axis=1))
    b_in = np.asarray(inp["ab_b_in"][0], np.float32)
    put("b_in", _fm(b_in[:2048]))
    cw = np.asarray(inp["conv_w"][0], np.float32)
    put("conv_w", np.ascontiguousarray(cw.T.reshape(4, 128, 31).transpose(1, 0, 2).reshape(128, 124)))
    put("conv_b", _fm(inp["conv_b"][0]))
    put("cln_g", _fm(inp["conv_ln_g"][0]))
    put("cln_b", _fm(inp["conv_ln_b"][0]))
    put("b_out", _fm(inp["ab_b_out"][0]))
    sm[:, SMO["eps"]] = EPS
    qidx = _gqa_qidx()
    gw = np.asarray(inp["gqa_w_in"][0], np.float32)
    wq = gw[:, :1024]
    wkk = gw[:, 1024:1280]
    wvv = gw[:, 1280:1536]
    C, Sg = _rope_tables()
    kk = np.arange(128)[:, None]
    qq = np.arange(128)[None, :]
    maskL = np.where(kk >= qq, 0.0, NEG).astype(np.float32)
    maskU = np.where(kk <= qq, 0.0, NEG).astype(np.float32)
    sink = np.asarray(inp["gqa_sink"][0], np.float32)
    sperm = np.array([8 * m + 4 * sh + j for m in range(2) for sh in range(2) for j in range(4)])
    sel = np.zeros((16, 16, 128), np.float32)
    for ex in range(16):
        sel[ex, ex, :] = 1.0
    wr = np.stack([np.concatenate([np.asarray(inp["router_group"][l], np.float32), np.asarray(inp["router_expert"][l], np.float32)], axis=1)
                   .reshape(8, 128, 20).transpose(1, 0, 2).reshape(128, 160) for l in range(2)])
    bv = b_in[2048:2560]
    shared = {
        "ada_w": f(inp["ada_w"]),
        "sm": sm,
        "w_in0": f(inp["ab_w_in"][0]),
        "bvbc": np.ascontiguousarray(np.broadcast_to(bv[None, :], (128, 512))),
        "nab": np.ascontiguousarray(_na_bias_table(np.asarray(inp["na_rpb"][0], np.float32)).reshape(8, 128, 21 * 128)),
        "w_out0": f(inp["ab_w_out"][0]),
        "wq1": f(wq[:, qidx]),
        "wqs1": f(wq[:, qidx][:, _swap64(1024)]),
        "wk1": f(wkk),
        "wks1": f(wkk[:, _swap64(256)]),
        "wv1": f(wvv),
        "w_out1": f(np.asarray(inp["gqa_w_out"][0], np.float32)[qidx, :]),
        "ropeC": C,
        "ropeS": Sg,
        "maskLU": np.ascontiguousarray(np.concatenate([maskL, maskU], axis=1)),
        "sinkbc": np.ascontiguousarray(np.broadcast_to(sink[sperm][None, :], (128, 16))),
        "wr": f(wr),
        "sel": np.ascontiguousarray(sel.reshape(16, 2048)),
        "ident": np.eye(128, dtype=np.float32),
        "ewg": f(inp["exp_w_gate"]),
        "ewu": f(inp["exp_w_up"]),
        "ewd": f(inp["exp_w_down"]),
    }
    return shared


def _core_inputs(inp, shared, i):
    x = np.asarray(inp["x"], np.float32)
    ctx = np.asarray(inp["ctx"], np.float32)
    c = np.asarray(inp["c"], np.float32)
    cc = np.stack([c[2 * i], c[2 * i + 1], np.asarray(inp["c_ctx"], np.float32)])
    cvec = np.ascontiguousarray(cc.reshape(3, 8, 128).transpose(2, 1, 0).reshape(128, 24))
    m = dict(shared)
    m["x2"] = np.ascontiguousarray(x[2 * i:2 * i + 2])
    m["ctx2"] = np.ascontiguousarray(ctx[2 * i:2 * i + 2])
    m["cvec"] = cvec
    return m


_NC_CACHE = {}


def kernel(**inputs):
    n = 8
    if "nc" not in _NC_CACHE:
        _NC_CACHE["nc"] = build()
    nc = _NC_CACHE["nc"]
    shared = _prep_shared(inputs)
    in_maps = [_core_inputs(inputs, shared, i) for i in range(n)]
    res = run_bass_kernel_spmd(nc, in_maps, core_ids=list(range(n)))
    out = np.concatenate([np.asarray(r["out"], np.float32) for r in res.results], axis=0)
    return out
```

```python
import numpy as np
from contextlib import ExitStack
import concourse.bass as bass
import concourse.mybir as mybir
from concourse.bass_utils import run_bass_kernel_spmd

F32 = mybir.dt.float32
BF16 = mybir.dt.bfloat16
AF = mybir.ActivationFunctionType
ALU = mybir.AluOpType
AX = mybir.AxisListType

D = 1024
SEQ = 2048
NCTX = 256
NT = SEQ + NCTX
GW = 64
ALPHA = 4.0 ** 0.25
EPS = 1e-5
NEG = -1e30

ENGS = ("pe", "act", "dve", "pool", "sp")
N_DMA_SEMS = 40


class Res:
    __slots__ = ("name", "last_w", "readers")

    def __init__(self, name):
        self.name = name
        self.last_w = None
        self.readers = []


class Op:
    __slots__ = ("eng", "fn", "idx", "deps", "dma", "sig", "semval", "dsem", "dval", "dprev")

    def __init__(self, eng, fn, idx, dma):
        self.eng = eng
        self.fn = fn
        self.idx = idx
        self.deps = []
        self.dma = dma
        self.sig = False
        self.semval = 0
        self.dsem = -1
        self.dval = 0
        self.dprev = 0


class Sched:
    def __init__(self):
        self.ops = {e: [] for e in ENGS}
        self.ndma = 0
        self.dma_tot = [0] * N_DMA_SEMS
        self.last_dma = [None] * N_DMA_SEMS
        self._assigned = False

    def add(self, eng, fn, reads=(), writes=(), dma=False, extra=()):
        lst = self.ops[eng]
        op = Op(eng, fn, len(lst), dma)
        deps = {}
        for r in reads:
            if r.last_w is not None:
                deps[id(r.last_w)] = r.last_w
        for w in writes:
            if w.last_w is not None:
                deps[id(w.last_w)] = w.last_w
            for rd in w.readers:
                deps[id(rd)] = rd
        for x in extra:
            deps[id(x)] = x
        for r in reads:
            r.readers.append(op)
        for w in writes:
            w.last_w = op
            w.readers = []
        if dma:
            s = self.ndma % N_DMA_SEMS
            self.ndma += 1
            op.dsem = s
            op.dprev = self.dma_tot[s]
            self.dma_tot[s] += 16
            op.dval = self.dma_tot[s]
            self.last_dma[s] = op
        for d in deps.values():
            if d is op:
                continue
            if d.eng == eng and not d.dma and not dma:
                if eng == "pe":
                    continue
                if op.idx - d.idx > 2:
                    continue
            op.deps.append(d)
            if not d.dma:
                d.sig = True
        lst.append(op)
        return op

    def barrier(self):
        lasts = []
        for e in ENGS:
            for op in reversed(self.ops[e]):
                if not op.dma:
                    lasts.append(op)
                    break
        dl = [o for o in self.last_dma if o is not None]
        for e in ENGS:
            self.add(e, lambda eng: eng.nop(), extra=[o for o in lasts if o.eng != e] + dl)

    def emit_one(self, e, eng, esem, dsems):
        if not self._assigned:
            for ee in ENGS:
                c = 0
                for op in self.ops[ee]:
                    if op.sig and not op.dma:
                        c += 1
                        op.semval = c
            self._assigned = True
        seen = {}
        for op in self.ops[e]:
            need = {}
            for d in op.deps:
                if d.dma:
                    key = ("d", d.dsem)
                    val = d.dval
                else:
                    key = ("e", d.eng)
                    val = d.semval
                if val > need.get(key, 0):
                    need[key] = val
            if op.dma and op.dprev > 0:
                key = ("d", op.dsem)
                if op.dprev > need.get(key, 0):
                    need[key] = op.dprev
            for key, val in need.items():
                if seen.get(key, 0) >= val:
                    continue
                seen[key] = val
                sem = dsems[key[1]] if key[0] == "d" else esem[key[1]]
                eng.wait_ge(sem, val)
            ins = op.fn(eng)
            if op.dma:
                ins.then_inc(dsems[op.dsem], 16)
            elif op.sig:
                ins.then_inc(esem[e], 1)


def _sm_layout():
    off = {}
    n = 0
    for name, cols in (("ada_b0", 48), ("ada_b1", 48), ("ln_g", 32), ("ln_b", 32), ("b_in", 16),
                       ("conv_w", 124), ("conv_b", 4), ("cln_g", 4), ("cln_b", 4), ("b_out", 8), ("eps", 1)):
        off[name] = n
        n += cols
    return off, n


SMO, SMN = _sm_layout()


def _fm(v):
    v = np.asarray(v, np.float32)
    return np.ascontiguousarray(v.reshape(-1, 128).T)


def _gqa_qidx():
    idx = np.zeros(1024, np.int64)
    for c in range(8):
        m, j = divmod(c, 4)
        h0 = 8 * m + j
        h1 = 8 * m + 4 + j
        idx[c * 128:c * 128 + 64] = h0 * 64 + np.arange(64)
        idx[c * 128 + 64:c * 128 + 128] = h1 * 64 + np.arange(64)
    return idx


def _swap64(n):
    d = np.arange(n)
    dd = d % 64
    sw = np.where(dd % 32 < 16, dd + 16, dd - 16)
    return (d // 64) * 64 + sw


def _na_tiles(j):
    if j in (0, 1):
        return [0, 1, 2, 3]
    if j in (14, 15):
        return [12, 13, 14, 15]
    return [j - 2, j - 1, j, j + 1, j + 2]


def _na_tile_index(j):
    if j == 0:
        return 5
    if j == 1:
        return 9
    if j == 14:
        return 13
    if j == 15:
        return 17
    return 0


def _na_bias_table(rpb):
    rows = 32
    r = np.arange(rows)
    row_start = np.clip(r - 4, 0, rows - 8)
    jj = np.arange(GW)
    col_start = np.clip(jj - 8, 0, GW - 16)
    col_in = (jj[None, :] >= col_start[:, None]) & (jj[None, :] < col_start[:, None] + 16)
    col_off = np.clip(jj[None, :] - jj[:, None], -15, 15) + 15
    out = np.full((8, 21, 128, 128), NEG, np.float32)

    def tile(j, a):
        t = np.full((8, 128, 128), NEG, np.float32)
        for pk in range(2):
            rk = 2 * a + pk
            for pq in range(2):
                rq = 2 * j + pq
                if not (row_start[rq] <= rk < row_start[rq] + 8):
                    continue
                ro = rk - rq + 7
                blk = rpb[:, ro][:, col_off]
                blk = np.where(col_in[None], blk, np.float32(NEG))
                t[:, pk * 64:(pk + 1) * 64, pq * 64:(pq + 1) * 64] = blk.transpose(0, 2, 1)
        return t

    for i, a in enumerate(_na_tiles(5)):
        out[:, i] = tile(5, a)
    for j in (0, 1, 14, 15):
        base = _na_tile_index(j)
        for i, a in enumerate(_na_tiles(j)):
            out[:, base + i] = tile(j, a)
    return np.ascontiguousarray(out.transpose(0, 2, 1, 3))


def _rope_tables():
    t = np.arange(SEQ)
    row = (t // GW).astype(np.float32)
    col = (t % GW).astype(np.float32)
    inv = (np.float32(10000.0) ** (-np.arange(0, 32, 2, dtype=np.float32) / np.float32(32))).astype(np.float32)
    ang = np.concatenate([row[:, None] * inv, col[:, None] * inv], axis=-1).astype(np.float32)
    cos = np.cos(ang).astype(np.float32)
    sin = np.sin(ang).astype(np.float32)
    p = np.arange(128)
    d = p % 64
    ai = (d // 32) * 16 + d % 16
    sgn = np.where(d % 32 < 16, -1.0, 1.0).astype(np.float32)
    C = np.ascontiguousarray(cos[:, ai].T)
    S = np.ascontiguousarray((sin[:, ai] * sgn[None, :]).T)
    return C.astype(np.float32), S.astype(np.float32)


class _Stop(Exception):
    pass


def build(nlayers=2, nb=2, debug=False, stop=None):
    nc = bass.Bass("TRN2", target_bir_lowering=False)
    S = Sched()

    def din(name, shape):
        return nc.dram_tensor(name, list(shape), F32, kind="ExternalInput").ap()

    x2 = din("x2", [2, SEQ, D])
    ctx2 = din("ctx2", [2, NCTX, D])
    cvec = din("cvec", [128, 24])
    ada_w = din("ada_w", [2, D, 6 * D])
    smd = din("sm", [128, SMN])
    w_in0 = din("w_in0", [D, 2560])
    bvbc = din("bvbc", [128, 512])
    nab = din("nab", [8, 128, 21 * 128])
    w_out0 = din("w_out0", [D, D])
    wq1 = din("wq1", [D, 1024])
    wqs1 = din("wqs1", [D, 1024])
    wk1 = din("wk1", [D, 256])
    wks1 = din("wks1", [D, 256])
    wv1 = din("wv1", [D, 256])
    w_out1 = din("w_out1", [D, D])
    ropeC = din("ropeC", [128, SEQ])
    ropeS = din("ropeS", [128, SEQ])
    maskLU = din("maskLU", [128, 256])
    sinkbc = din("sinkbc", [128, 16])
    wr = din("wr", [2, 128, 160])
    sel = din("sel", [16, 2048])
    ident = din("ident", [128, 128])
    ewg = din("ewg", [2, 16, D, 256])
    ewu = din("ewu", [2, 16, D, 256])
    ewd = din("ewd", [2, 16, 256, D])
    outd = nc.dram_tensor("out", [2, SEQ, D], F32, kind="ExternalOutput").ap()
    Hs = nc.dram_tensor("Hs", [128, 8 * NT], F32, kind="Internal").ap()
    dbg = nc.dram_tensor("dbg", [128, 8 * NT], F32, kind="ExternalOutput").ap() if debug else None
    dbgb = nc.dram_tensor("dbgb", [128, 8 * NT], BF16, kind="ExternalOutput").ap() if debug else None
    Hs3 = Hs.rearrange("p (c t) -> p c t", c=8)

    es = ExitStack()
    cur = [16640]

    acache = {}

    def alloc(name, shape, dt, at=None):
        nbytes = int(np.prod(shape[1:])) * (4 if dt == F32 else 2)
        if at is None:
            at = cur[0]
            cur[0] = (at + nbytes + 63) // 64 * 64
        assert at + nbytes <= 229376, (name, at, nbytes)
        key = (name, at, tuple(shape))
        if key not in acache:
            acache[key] = nc.alloc_sbuf_tensor_at("%s_%d" % (name, len(acache)), list(shape), dt, offset=at)
        return acache[key]

    sm = alloc("sm", [128, SMN], F32)
    id32 = alloc("id32", [128, 128], F32)
    ones1k = alloc("ones1k", [128, 128], BF16)
    ones512 = alloc("ones512", [128, 128], BF16)
    csil = alloc("csil", [128, 24], BF16)
    cv32 = alloc("cv32", [128, 24], F32)
    mod = alloc("mod", [128, 2 * 144], F32)
    mp1 = alloc("mp1", [128, 2 * 144], F32)
    vecs = alloc("vecs", [128, 128], F32)
    selt = alloc("selt", [16, 2048], F32)
    wrt = alloc("wrt", [128, 320], F32)
    esink = alloc("esink", [128, 16], F32)
    R_const = Res("const")
    R_mod = Res("mod")
    R_vecs = Res("vecs")
    base0 = cur[0]

    PS = [es.enter_context(nc.psum_tensor("ps%d" % i, [128, 1024], F32)) for i in range(4)]
    RB = [Res("bank%d" % i) for i in range(8)]

    def bank(k):
        return PS[k // 2][:, (k % 2) * 512:(k % 2) * 512 + 512]

    esem = {e: es.enter_context(nc.semaphore("es_" + e)) for e in ENGS}
    dsems = [es.enter_context(nc.semaphore("ds%d" % i)) for i in range(N_DMA_SEMS)]

    def smc(name, j, n=1):
        o = SMO[name] + j
        return sm[:, o:o + n]

    def modc(l, k, ch, col):
        o = l * 144 + (k * 8 + ch) * 3 + col
        return mod[:, o:o + 1]

    def mp1c(l, k, ch, col):
        o = l * 144 + (k * 8 + ch) * 3 + col
        return mp1[:, o:o + 1]

    VK = {}

    def vslot(kind, ch):
        key = (kind, ch)
        if key not in VK:
            VK[key] = len(VK)
            assert len(VK) <= 128
        o = VK[key]
        return vecs[:, o:o + 1]

    S.add("sp", lambda e: e.dma_start(out=sm[:], in_=smd), writes=[R_const], dma=True)
    S.add("sp", lambda e: e.dma_start(out=id32[:], in_=ident), writes=[R_const], dma=True)
    S.add("sp", lambda e: e.dma_start(out=cv32[:], in_=cvec), writes=[R_const], dma=True)
    S.add("sp", lambda e: e.dma_start(out=selt[:], in_=sel), writes=[R_const], dma=True)
    S.add("sp", lambda e: e.dma_start(out=wrt[:].rearrange("p (l n) -> p l n", l=2), in_=wr.rearrange("l p n -> p l n")), writes=[R_const], dma=True)
    S.add("sp", lambda e: e.dma_start(out=esink[:], in_=sinkbc), writes=[R_const], dma=True)
    S.add("pool", lambda e: e.memset(ones1k[:], 1.0 / 1024.0), writes=[R_const])
    S.add("pool", lambda e: e.memset(ones512[:], 1.0 / 512.0), writes=[R_const])
    S.add("act", lambda e: e.activation(out=csil[:], in_=cv32[:], func=AF.Silu), reads=[R_const], writes=[R_const])
    S.add("act", lambda e: e.activation(out=esink[:], in_=esink[:], func=AF.Exp), reads=[R_const], writes=[R_const])

    adaw = [alloc("adaw%d" % i, [128, 8, 1024], BF16) for i in range(2)]
    R_adaw = [Res("adaw0"), Res("adaw1")]
    pi = 0
    for l in range(nlayers):
        awl = ada_w[l].rearrange("(kc p) n -> p kc n", p=128)
        for piece in range(6):
            s = pi % 2
            pi += 1
            S.add("pool", (lambda e, s=s, awl=awl, piece=piece: e.dma_start(out=adaw[s][:], in_=awl[:, :, piece * 1024:(piece + 1) * 1024])),
                  writes=[R_adaw[s]], dma=True)
            for oc8 in range(8):
                oc = piece * 8 + oc8
                for kc in range(8):
                    S.add("pe", (lambda e, s=s, oc=oc, oc8=oc8, kc=kc: e.matmul(bank(0)[:, oc * 3:oc * 3 + 3], lhsT=adaw[s][:, kc, oc8 * 128:(oc8 + 1) * 128],
                                                                                    rhs=csil[:, kc * 3:kc * 3 + 3], start=(kc == 0), stop=(kc == 7))),
                          reads=[R_adaw[s], R_const], writes=[RB[0]])
        ab = smc("ada_b%d" % l, 0, 48)
        S.add("dve", (lambda e, l=l, ab=ab: e.tensor_tensor(out=mod[:, l * 144:(l + 1) * 144].rearrange("p (a b) -> p a b", b=3),
                                                             in0=bank(0)[:, 0:144].rearrange("p (a b) -> p a b", b=3),
                                                             in1=ab.unsqueeze(2).broadcast_to([128, 48, 3]), op=ALU.add)),
              reads=[RB[0], R_const], writes=[R_mod])
        S.add("dve", (lambda e, l=l: e.tensor_scalar_add(out=mp1[:, l * 144:(l + 1) * 144], in0=mod[:, l * 144:(l + 1) * 144], scalar1=1.0)),
              reads=[R_mod], writes=[R_mod])
    S.barrier()
    cur[0] = base0

    A0 = cur[0]
    hres = alloc("hres", [128, 8, NT], F32)
    qT = alloc("qT", [128, 4, NT], BF16, at=A0)
    kT = alloc("kT", [128, 4, NT], BF16, at=A0 + 18432)
    vaug = alloc("vaug", [128, 18, 4, 192], BF16, at=A0 + 36864)
    qT1 = alloc("qT1", [128, 8, SEQ], BF16, at=A0)
    kT1 = alloc("kT1", [128, 2, NT], BF16, at=A0 + 32768)
    vaug1 = alloc("vaug1", [128, 18, 2, 192], BF16, at=A0 + 41984)
    rC = alloc("rC", [128, SEQ], F32, at=A0 + 55808)
    rS = alloc("rS", [128, SEQ], F32, at=A0 + 55808 + 8192)
    bvb = alloc("bvb", [128, 512], F32, at=A0 + 64512)
    idb = alloc("idb", [128, 128], BF16, at=A0 + 64512 + 2048)
    cmean = alloc("cmean", [128, 256], F32, at=A0 + 64512 + 2304)
    crstd = alloc("crstd", [128, 256], F32, at=A0 + 64512 + 3328)
    UT = alloc("UT", [128, 8, NT], BF16)
    oT = alloc("oT", [128, 8, NT], BF16)
    C0 = cur[0]
    RT = {}

    def rtok(name, t0, t1):
        out = []
        for tt in range(t0 // 128, (t1 + 127) // 128):
            key = (name, tt)
            if key not in RT:
                RT[key] = Res("%s_%d" % key)
            out.append(RT[key])
        return out

    R_hpad = [Res("hpad%d" % c) for c in range(4)]

    def derive_vecs(l, col, tag):
        ops = []
        for ch in range(8):
            g1 = smc("ln_g", (l * 2 + 0) * 8 + ch)
            b1 = smc("ln_b", (l * 2 + 0) * 8 + ch)
            g2 = smc("ln_g", (l * 2 + 1) * 8 + ch)
            b2 = smc("ln_b", (l * 2 + 1) * 8 + ch)
            S.add("dve", (lambda e, ch=ch, g1=g1: e.tensor_tensor(out=vslot((tag, "G4"), ch), in0=g1, in1=mp1c(l, 4, ch, col), op=ALU.mult)),
                  reads=[R_const, R_mod], writes=[R_vecs])
            S.add("dve", (lambda e, ch=ch, b1=b1: e.scalar_tensor_tensor(out=vslot((tag, "B4"), ch), in0=b1, scalar=mp1c(l, 4, ch, col), in1=modc(l, 3, ch, col),
                                                                         op0=ALU.mult, op1=ALU.add)),
                  reads=[R_const, R_mod], writes=[R_vecs])
            S.add("dve", (lambda e, ch=ch, g1=g1: e.tensor_scalar_mul(out=vslot((tag, "GA1"), ch), in0=g1, scalar1=ALPHA)), reads=[R_const], writes=[R_vecs])
            S.add("dve", (lambda e, ch=ch, b1=b1: e.tensor_scalar_mul(out=vslot((tag, "BA1"), ch), in0=b1, scalar1=ALPHA)), reads=[R_const], writes=[R_vecs])
            if l == 0:
                S.add("dve", (lambda e, ch=ch: e.tensor_tensor(out=vslot((tag, "M2B"), ch), in0=modc(l, 2, ch, col), in1=smc("b_out", ch), op=ALU.mult)),
                      reads=[R_const, R_mod], writes=[R_vecs])
                S.add("dve", (lambda e, ch=ch, g2=g2: e.tensor_tensor(out=vslot((tag, "GU"), ch), in0=g2, in1=mp1c(1, 1, ch, col), op=ALU.mult)),
                      reads=[R_const, R_mod], writes=[R_vecs])
                S.add("dve", (lambda e, ch=ch, b2=b2: e.scalar_tensor_tensor(out=vslot((tag, "BU"), ch), in0=b2, scalar=mp1c(1, 1, ch, col), in1=modc(1, 0, ch, col),
                                                                             op0=ALU.mult, op1=ALU.add)),
                      reads=[R_const, R_mod], writes=[R_vecs])

    def ln_stats(pre_ap, nch, N, ones_t, prebf, presq, mean_sb, rstd_sb, bk, r_pre, r_tmp):
        S.add("dve", lambda e: e.tensor_copy(out=prebf[:, 0:nch, 0:N], in_=pre_ap), reads=r_pre, writes=[r_tmp[0]])
        S.add("act", lambda e: e.activation(out=presq[:, 0:nch, 0:N], in_=pre_ap, func=AF.Square), reads=r_pre, writes=[r_tmp[1]])
        for c in range(nch):
            S.add("pe", (lambda e, c=c: e.matmul(bank(bk)[:, 0:N], lhsT=ones_t[:], rhs=prebf[:, c, 0:N], start=(c == 0), stop=(c == nch - 1))),
                  reads=[r_tmp[0], R_const], writes=[RB[bk]])
        for c in range(nch):
            S.add("pe", (lambda e, c=c: e.matmul(bank(bk)[:, 256:256 + N], lhsT=ones_t[:], rhs=presq[:, c, 0:N], start=(c == 0), stop=(c == nch - 1))),
                  reads=[r_tmp[1], R_const], writes=[RB[bk]])
        S.add("act", lambda e: e.copy(out=mean_sb[:, 0:N], in_=bank(bk)[:, 0:N]), reads=[RB[bk]], writes=[r_tmp[2]])
        S.add("dve", lambda e: e.tensor_tensor(out=rstd_sb[:, 0:N], in0=mean_sb[:, 0:N], in1=mean_sb[:, 0:N], op=ALU.mult), reads=[r_tmp[2]], writes=[r_tmp[3]])
        S.add("dve", lambda e: e.tensor_tensor(out=rstd_sb[:, 0:N], in0=bank(bk)[:, 256:256 + N], in1=rstd_sb[:, 0:N], op=ALU.subtract),
              reads=[RB[bk], r_tmp[3]], writes=[r_tmp[3]])
        S.add("act", lambda e: e.activation(out=rstd_sb[:, 0:N], in_=rstd_sb[:, 0:N], func=AF.Sqrt, bias=smc("eps", 0), scale=1.0),
              reads=[r_tmp[3], R_const], writes=[r_tmp[3]])
        S.add("dve", lambda e: e.reciprocal(out=rstd_sb[:, 0:N], in_=rstd_sb[:, 0:N]), reads=[r_tmp[3]], writes=[r_tmp[3]])

    def normalize(pre_ap, nch, N, mean_sb, rstd_sb, r_pre, r_tmp):
        S.add("dve", lambda e: e.tensor_tensor(out=pre_ap, in0=pre_ap, in1=mean_sb[:, 0:N].unsqueeze(1).broadcast_to([128, nch, N]), op=ALU.subtract),
              reads=r_pre + [r_tmp[2]], writes=r_pre)
        S.add("dve", lambda e: e.tensor_tensor(out=pre_ap, in0=pre_ap, in1=rstd_sb[:, 0:N].unsqueeze(1).broadcast_to([128, nch, N]), op=ALU.mult),
              reads=r_pre + [r_tmp[3]], writes=r_pre)

    aff_rr = [0]

    def affine(out_ap, in_ap, sc, bi, reads, writes, psum_in=False):
        k = aff_rr[0] % 2
        aff_rr[0] += 1
        if k == 0:
            S.add("act", lambda e: e.activation(out=out_ap, in_=in_ap, func=AF.Identity, bias=bi, scale=sc), reads=reads, writes=writes)
        else:
            S.add("dve" if k == 1 else "pool", lambda e: e.tensor_scalar(out=out_ap, in0=in_ap, scalar1=sc, scalar2=bi, op0=ALU.mult, op1=ALU.add),
                  reads=reads, writes=writes)

    def dump(name, src_ap3):
        if stop != name:
            return
        S.barrier()
        c, t = src_ap3.shape[1], src_ap3.shape[2]
        dst = dbg if src_ap3.dtype == F32 else dbgb
        for ci in range(c):
            S.add("sp", (lambda e, ci=ci: e.dma_start(out=dst[:, ci * t:(ci + 1) * t], in_=src_ap3[:, ci, :])), writes=[Res("dbgo")], dma=True)
        raise _Stop()

    try:
      for b in range(nb):
        for l in range(nlayers):
              lat_only = (l == nlayers - 1) and l == 1
              col = b
              S.barrier()
              derive_vecs(l, b, "lat")
              if l == 0:
                  derive_vecs(l, 2, "ctx")

              def V(kind, ch, t0):
                  return vslot((("ctx" if (t0 < NCTX and l == 0) else "lat"), kind), ch)

              def mcol(t0):
                  return 2 if t0 < NCTX else b

              cur[0] = C0
              if l == 0:
                  xin = [alloc("xin%d" % i, [128, D], F32) for i in range(2)]
                  hblk = [alloc("hblk%d" % i, [128, 8, 128], F32) for i in range(2)]
                  R_xin = [Res("xin0"), Res("xin1")]
                  R_hblk = [Res("hblk0"), Res("hblk1")]
                  for tt in range(18):
                      s = tt % 2
                      src = ctx2[b, tt * 128:(tt + 1) * 128, :] if tt < 2 else x2[b, (tt - 2) * 128:(tt - 1) * 128, :]
                      S.add("sp", (lambda e, s=s, src=src: e.dma_start(out=xin[s][:], in_=src)), writes=[R_xin[s]], dma=True)
                      for ch in range(8):
                          bk = (tt % 2) * 2 + ch // 4
                          S.add("pe", (lambda e, s=s, ch=ch, bk=bk: e.transpose(bank(bk)[:, (ch % 4) * 128:(ch % 4 + 1) * 128], xin[s][:, ch * 128:(ch + 1) * 128], id32[:])),
                                reads=[R_xin[s], R_const], writes=[RB[bk]])
                      for hf in range(2):
                          bk = (tt % 2) * 2 + hf
                          S.add("act", (lambda e, s=s, hf=hf, bk=bk: e.copy(out=hblk[s][:, hf * 4:(hf + 1) * 4, :], in_=bank(bk).rearrange("p (c t) -> p c t", c=4))),
                                reads=[RB[bk]], writes=[R_hblk[s]])
                      mc = mcol(tt * 128)
                      for ch in range(8):
                          bk = (tt % 2) * 2 + ch // 4
                          affine(UT[:, ch, tt * 128:(tt + 1) * 128], bank(bk)[:, (ch % 4) * 128:(ch % 4 + 1) * 128], mp1c(0, 1, ch, mc), modc(0, 0, ch, mc),
                                 [RB[bk], R_mod], rtok("UT", tt * 128, tt * 128 + 128), psum_in=True)
                      S.add("sp", (lambda e, s=s, tt=tt: e.dma_start(out=Hs3[:, :, tt * 128:(tt + 1) * 128], in_=hblk[s][:])), reads=[R_hblk[s]],
                            writes=rtok("Hs", tt * 128, tt * 128 + 128), dma=True)

              if l == 0:
                  dump("S0", UT[:])
              blocks512 = [(0, 256)] + [(256 + 512 * i, 512) for i in range(4)]
              blocks256 = [(256 * i, 256) for i in range(9)]
              if l == 1:
                  lat512 = [(256 + 512 * i, 512) for i in range(4)]
                  lat256 = [(256 * i, 256) for i in range(1, 9)]

              if l == 0:
                  S.barrier()
                  cur[0] = C0
                  wAB = alloc("wAB", [128, 8, 1536], BF16)
                  wA = alloc("wA", [128, 8, 1024], BF16, at=C0)
                  hpd = [alloc("hpd%d" % i, [128, 2368], BF16, at=C0 + 16384 + i * 4736) for i in range(2)]
                  dg0 = alloc("diag0", [128, 31, 128], BF16, at=C0 + 25856)
                  diag = [dg0, dg0]
                  cur[0] = C0 + 33792
                  sgt = [alloc("sgt%d" % i, [128, 512], F32) for i in range(2)]
                  czsq = alloc("czsq", [128, 4, 256], BF16)
                  cz = alloc("cz", [128, 4, 256], F32)
                  R_wAB, R_misc = Res("wAB"), Res("misc0")
                  R_hpd = [Res("hpd0"), Res("hpd1")]
                  R_dg = [Res("dg0")] * 2
                  R_sgt = [Res("sgt0"), Res("sgt1")]
                  R_cz, R_ct = Res("cz"), [Res("czbf"), Res("czsq"), Res("cmean"), Res("crstd")]
                  w0 = w_in0.rearrange("(kc p) n -> p kc n", p=128)
                  S.add("pool", lambda e: e.dma_start(out=wA[:], in_=w0[:, :, 0:1024]), writes=[R_wAB], dma=True)
                  S.add("pool", lambda e: e.dma_start(out=idb[:], in_=ident), writes=[R_misc], dma=True)
                  S.add("sp", lambda e: e.dma_start(out=bvb[:], in_=bvbc), writes=[R_misc], dma=True)
                  S.add("pool", lambda e: e.memset(hpd[0][:], 0.0), writes=[R_hpd[0]])
                  S.add("pool", lambda e: e.memset(hpd[1][:], 0.0), writes=[R_hpd[1]])
                  S.add("pool", lambda e: e.memset(vaug[:], 1.0), writes=rtok("vaug", 0, NT))

                  def hoff(t0):
                      return 15 + t0 if t0 < NCTX else 286 + 15 + (t0 - NCTX)

                  it = 0
                  for cc in range(4):
                      hs_ = cc % 2
                      for k in range(31):
                          S.add("dve", (lambda e, cc=cc, k=k, hs_=hs_: e.tensor_scalar_mul(out=diag[hs_][:, k, :], in0=idb[:], scalar1=smc("conv_w", cc * 31 + k))),
                                reads=[R_misc, R_const], writes=[R_dg[hs_]])
                      for (t0, N) in blocks512:
                          s = it % 2
                          it += 1
                          b1, b2 = 2 * s, 2 * s + 1
                          for kc in range(8):
                              S.add("pe", (lambda e, kc=kc, cc=cc, t0=t0, N=N, b1=b1: e.matmul(bank(b1)[:, 0:N], lhsT=wA[:, kc, cc * 128:(cc + 1) * 128], rhs=UT[:, kc, t0:t0 + N],
                                                                                             start=(kc == 0), stop=(kc == 7))),
                                    reads=[R_wAB] + rtok("UT", t0, t0 + N), writes=[RB[b1]])
                          for kc in range(8):
                              S.add("pe", (lambda e, kc=kc, cc=cc, t0=t0, N=N, b2=b2: e.matmul(bank(b2)[:, 0:N], lhsT=wA[:, kc, 512 + cc * 128:512 + (cc + 1) * 128], rhs=UT[:, kc, t0:t0 + N],
                                                                                             start=(kc == 0), stop=(kc == 7))),
                                    reads=[R_wAB] + rtok("UT", t0, t0 + N), writes=[RB[b2]])
                          S.add("act", (lambda e, s=s, cc=cc, N=N, b2=b2: e.activation(out=sgt[s][:, 0:N], in_=bank(b2)[:, 0:N], func=AF.Sigmoid, bias=smc("b_in", 4 + cc), scale=1.0)),
                                reads=[RB[b2], R_const], writes=[R_sgt[s]])
                          ho = hoff(t0)
                          S.add("dve", (lambda e, s=s, cc=cc, N=N, b1=b1, ho=ho, hs_=hs_: e.scalar_tensor_tensor(out=hpd[hs_][:, ho:ho + N], in0=bank(b1)[:, 0:N], scalar=smc("b_in", cc),
                                                                                                                 in1=sgt[s][:, 0:N], op0=ALU.add, op1=ALU.mult)),
                                reads=[RB[b1], R_sgt[s], R_const], writes=[R_hpd[hs_]])
                      for bi, (t0, N) in enumerate(blocks512):
                          ho = hoff(t0) - 15
                          bk = 4 + bi % 2
                          for k in range(31):
                              S.add("pe", (lambda e, k=k, ho=ho, N=N, bk=bk, hs_=hs_: e.matmul(bank(bk)[:, 0:N], lhsT=diag[hs_][:, k, :], rhs=hpd[hs_][:, ho + k:ho + k + N],
                                                                                           start=(k == 0), stop=(k == 30))),
                                    reads=[R_dg[hs_], R_hpd[hs_]], writes=[RB[bk]])
                          S.add("act", (lambda e, cc=cc, N=N, t0=t0, bk=bk: e.activation(out=oT[:, cc, t0:t0 + N], in_=bank(bk)[:, 0:N], func=AF.Identity, bias=smc("conv_b", cc), scale=1.0)),
                                reads=[RB[bk], R_const], writes=rtok("oT", t0, t0 + N))
                  dump("S1z", oT[:, 0:4, :])
                  dump("S1h", hpd[1][:].unsqueeze(1))
                  dump("S1d", diag[0][:])
                  S.add("pool", lambda e: e.dma_start(out=wAB[:], in_=w0[:, :, 1024:2560]), writes=[R_wAB] + R_hpd + [R_dg[0]], dma=True)
                  for (t0, N) in blocks256:
                      zin = oT[:, 0:4, t0:t0 + 256]
                      rz = rtok("oT", t0, t0 + 256)
                      S.add("act", (lambda e, zin=zin: e.activation(out=czsq[:], in_=zin, func=AF.Square)), reads=rz, writes=[R_ct[1]])
                      for c4 in range(4):
                          S.add("pe", (lambda e, c4=c4, t0=t0: e.matmul(bank(6)[:, 0:256], lhsT=ones512[:], rhs=oT[:, c4, t0:t0 + 256], start=(c4 == 0), stop=(c4 == 3))),
                                reads=rz + [R_const], writes=[RB[6]])
                      for c4 in range(4):
                          S.add("pe", (lambda e, c4=c4: e.matmul(bank(6)[:, 256:512], lhsT=ones512[:], rhs=czsq[:, c4, :], start=(c4 == 0), stop=(c4 == 3))),
                                reads=[R_ct[1], R_const], writes=[RB[6]])
                      S.add("act", lambda e: e.copy(out=cmean[:], in_=bank(6)[:, 0:256]), reads=[RB[6]], writes=[R_ct[2]])
                      S.add("dve", lambda e: e.tensor_tensor(out=crstd[:], in0=cmean[:], in1=cmean[:], op=ALU.mult), reads=[R_ct[2]], writes=[R_ct[3]])
                      S.add("dve", lambda e: e.tensor_tensor(out=crstd[:], in0=bank(6)[:, 256:512], in1=crstd[:], op=ALU.subtract), reads=[RB[6], R_ct[3]], writes=[R_ct[3]])
                      S.add("act", lambda e: e.activation(out=crstd[:], in_=crstd[:], func=AF.Sqrt, bias=smc("eps", 0), scale=1.0), reads=[R_ct[3], R_const], writes=[R_ct[3]])
                      S.add("dve", lambda e: e.reciprocal(out=crstd[:], in_=crstd[:]), reads=[R_ct[3]], writes=[R_ct[3]])
                      S.add("dve", (lambda e, zin=zin: e.tensor_tensor(out=cz[:], in0=zin, in1=cmean[:].unsqueeze(1).broadcast_to([128, 4, 256]), op=ALU.subtract)),
                            reads=rz + [R_ct[2]], writes=[R_cz])
                      S.add("dve", lambda e: e.tensor_tensor(out=cz[:], in0=cz[:], in1=crstd[:].unsqueeze(1).broadcast_to([128, 4, 256]), op=ALU.mult),
                            reads=[R_cz, R_ct[3]], writes=[R_cz])
                      for c4 in range(4):
                          S.add("act", (lambda e, c4=c4, t0=t0: e.activation(out=oT[:, c4, t0:t0 + 256], in_=cz[:, c4, :], func=AF.Silu, bias=smc("cln_b", c4), scale=smc("cln_g", c4))),
                                reads=[R_cz, R_const], writes=rz)
                  it = 0
                  for (t0, N) in blocks512:
                      for c in range(8):
                          bk = it % 4
                          it += 1
                          for kc in range(8):
                              S.add("pe", (lambda e, kc=kc, c=c, t0=t0, N=N, bk=bk: e.matmul(bank(bk)[:, 0:N], lhsT=wAB[:, kc, c * 128:(c + 1) * 128], rhs=UT[:, kc, t0:t0 + N],
                                                                                           start=(kc == 0), stop=(kc == 7))),
                                    reads=[R_wAB] + rtok("UT", t0, t0 + N), writes=[RB[bk]])
                          dst = qT if c < 4 else kT
                          S.add("act", (lambda e, c=c, t0=t0, N=N, bk=bk, dst=dst: e.activation(out=dst[:, c % 4, t0:t0 + N], in_=bank(bk)[:, 0:N], func=AF.Identity,
                                                                                              bias=smc("b_in", 8 + c), scale=1.0)),
                                reads=[RB[bk], R_const], writes=rtok("qk", t0, t0 + N))
                  for tt in range(18):
                      bk = it % 4
                      it += 1
                      for kc in range(8):
                          S.add("pe", (lambda e, kc=kc, tt=tt, bk=bk: e.matmul(bank(bk)[:, 0:512], lhsT=UT[:, kc, tt * 128:(tt + 1) * 128], rhs=wAB[:, kc, 1024:1536],
                                                                             start=(kc == 0), stop=(kc == 7))),
                                reads=[R_wAB] + rtok("UT", tt * 128, tt * 128 + 128), writes=[RB[bk]])
                      for x in range(2):
                          S.add("dve", (lambda e, tt=tt, bk=bk, x=x: e.tensor_tensor(out=vaug[:, tt, :, x * 128:x * 128 + 64],
                                                                                   in0=bank(bk).rearrange("p (c x d) -> p c x d", c=4, x=2)[:, :, x, :],
                                                                                   in1=bvb[:].rearrange("p (c x d) -> p c x d", c=4, x=2)[:, :, x, :], op=ALU.add)),
                                reads=[RB[bk], R_misc], writes=rtok("vaug", tt * 128, tt * 128 + 128))

                  dump("S1o", oT[:, 0:4, :])
                  dump("S1q", qT[:])
                  dump("S1k", kT[:])
                  dump("S1v", vaug[:].rearrange("p t c x -> p t (c x)"))
                  S.barrier()
                  cur[0] = C0
                  nabt = [alloc("nabt%d" % i, [128, 21, 128], F32) for i in range(2)]
                  sbt = [alloc("sbt%d" % i, [128, 640], F32) for i in range(2)]
                  PT = [alloc("PT%d" % i, [128, 896], BF16) for i in range(2)]
                  rec = [alloc("rec%d" % i, [128, 256], F32) for i in range(2)]
                  R_nabt = [Res("nabt0"), Res("nabt1")]
                  R_sbt = [Res("sbt0"), Res("sbt1")]
                  R_PT = [Res("PT0"), Res("PT1")]
                  R_rec = [Res("rec0"), Res("rec1")]
                  it = 0
                  for h in range(8):
                      c, sh = h // 2, h % 2
                      p0, p1 = sh * 64, sh * 64 + 64
                      nh0, nh1 = (0, 64) if sh == 0 else (64, 128)
                      dh0, dh1 = (64, 128) if sh == 0 else (0, 64)
                      hs = h % 2
                      S.add("sp", (lambda e, h=h, hs=hs: e.dma_start(out=nabt[hs][:], in_=nab[h].rearrange("p (a q) -> p a q", a=21))), writes=[R_nabt[hs]], dma=True)
                      vcol = sh * 64
                      s = it % 2
                      it += 1
                      sb0 = 2 * s
                      for i in range(2):
                          S.add("pe", (lambda e, i=i, c=c, p0=p0, p1=p1, sb0=sb0: e.matmul(bank(sb0)[:, i * 256:(i + 1) * 256], lhsT=kT[p0:p1, c, i * 128:(i + 1) * 128],
                                                                                          rhs=qT[p0:p1, c, 0:256], start=True, stop=True)),
                                reads=rtok("qk", 0, 256), writes=[RB[sb0]])
                      S.add("act", (lambda e, s=s, sb0=sb0: e.activation(out=PT[s][:, 0:512], in_=bank(sb0)[:, 0:512], func=AF.Exp, scale=0.125)), reads=[RB[sb0]], writes=[R_PT[s]])
                      ob = 4 + s
                      for i in range(2):
                          S.add("pe", (lambda e, i=i, c=c, s=s, ob=ob, vcol=vcol: e.matmul(bank(ob)[:, 0:256], lhsT=vaug[:, i, c, vcol:vcol + 128], rhs=PT[s][:, i * 256:(i + 1) * 256],
                                                                                          start=(i == 0), stop=(i == 1))),
                                reads=[R_PT[s]] + rtok("vaug", 0, 256), writes=[RB[ob]])
                      S.add("dve", (lambda e, s=s, ob=ob, dh0=dh0, dh1=dh1: e.reciprocal(out=rec[s][dh0:dh1, 0:256], in_=bank(ob)[dh0:dh1, 0:256])), reads=[RB[ob]], writes=[R_rec[s]])
                      S.add("dve", (lambda e, s=s, ob=ob, c=c, nh0=nh0, nh1=nh1, dh0=dh0, dh1=dh1: e.tensor_tensor(out=oT[nh0:nh1, 4 + c, 0:256], in0=bank(ob)[nh0:nh1, 0:256],
                                                                                                               in1=rec[s][dh0:dh1, 0:256], op=ALU.mult)),
                            reads=[RB[ob], R_rec[s]], writes=rtok("oT", 0, 256))
                      for j in range(16):
                          s = it % 2
                          it += 1
                          sb0 = 2 * s
                          tl = _na_tiles(j)
                          nl = len(tl)
                          ti0 = _na_tile_index(j)
                          q0 = NCTX + j * 128
                          ktoks = [NCTX + a * 128 for a in tl] + [0, 128]
                          for i, kt0 in enumerate(ktoks):
                              bk = sb0 + (i // 4)
                              S.add("pe", (lambda e, i=i, kt0=kt0, bk=bk, c=c, p0=p0, p1=p1, q0=q0: e.matmul(bank(bk)[:, (i % 4) * 128:(i % 4 + 1) * 128], lhsT=kT[p0:p1, c, kt0:kt0 + 128],
                                                                                                        rhs=qT[p0:p1, c, q0:q0 + 128], start=True, stop=True)),
                                    reads=rtok("qk", kt0, kt0 + 128) + rtok("qk", q0, q0 + 128), writes=[RB[bk]])
                          S.add("dve", (lambda e, s=s, nl=nl, ti0=ti0, hs=hs: e.scalar_tensor_tensor(out=sbt[s][:, 0:nl * 128], in0=PS[s][:, 0:nl * 128], scalar=0.125,
                                                                                                    in1=nabt[hs][:, ti0:ti0 + nl, :].rearrange("p a q -> p (a q)"),
                                                                                                    op0=ALU.mult, op1=ALU.add)),
                                reads=[RB[sb0], RB[sb0 + 1], R_nabt[hs]], writes=[R_sbt[s]])
                          S.add("act", (lambda e, s=s, nl=nl: e.activation(out=PT[s][:, 0:nl * 128], in_=sbt[s][:, 0:nl * 128], func=AF.Exp)), reads=[R_sbt[s]], writes=[R_PT[s]])
                          S.add("act", (lambda e, s=s, nl=nl: e.activation(out=PT[s][:, nl * 128:(nl + 2) * 128], in_=PS[s][:, nl * 128:(nl + 2) * 128], func=AF.Exp, scale=0.125)),
                                reads=[RB[sb0], RB[sb0 + 1]], writes=[R_PT[s]])
                          ob = 4 + s
                          for i, kt0 in enumerate(ktoks):
                              S.add("pe", (lambda e, i=i, kt0=kt0, c=c, s=s, ob=ob, vcol=vcol, nl=nl: e.matmul(bank(ob)[:, 0:128], lhsT=vaug[:, kt0 // 128, c, vcol:vcol + 128],
                                                                                                          rhs=PT[s][:, i * 128:(i + 1) * 128], start=(i == 0), stop=(i == nl + 1))),
                                    reads=[R_PT[s]] + rtok("vaug", kt0, kt0 + 128), writes=[RB[ob]])
                          S.add("dve", (lambda e, s=s, ob=ob, dh0=dh0, dh1=dh1: e.reciprocal(out=rec[s][dh0:dh1, 0:128], in_=bank(ob)[dh0:dh1, 0:128])), reads=[RB[ob]], writes=[R_rec[s]])
                          S.add("dve", (lambda e, s=s, ob=ob, c=c, q0=q0, nh0=nh0, nh1=nh1, dh0=dh0, dh1=dh1: e.tensor_tensor(out=oT[nh0:nh1, 4 + c, q0:q0 + 128], in0=bank(ob)[nh0:nh1, 0:128],
                                                                                                                     in1=rec[s][dh0:dh1, 0:128], op=ALU.mult)),
                                reads=[RB[ob], R_rec[s]], writes=rtok("oT", q0, q0 + 128))
                  dump("S3", oT[:, 4:8, :])
                  w_out_d = w_out0
                  tok_blocks = blocks256
              else:
                  S.barrier()
                  cur[0] = C0
                  wq = alloc("wq", [128, 8, 512], BF16)
                  wqs = alloc("wqs", [128, 8, 512], BF16)
                  wk = alloc("wk", [128, 8, 256], BF16)
                  wks = alloc("wks", [128, 8, 256], BF16)
                  wv = alloc("wv", [128, 8, 256], BF16)
                  rt = [alloc("rt%d" % i, [128, 2, 512], F32) for i in range(2)]
                  R_w1, R_rope = Res("w1"), Res("rope")
                  R_rt = [Res("rt0"), Res("rt1")]
                  R_wq = Res("wq")
                  for dst, src in ((wk, wk1), (wks, wks1), (wv, wv1)):
                      S.add("pool", (lambda e, dst=dst, src=src: e.dma_start(out=dst[:], in_=src.rearrange("(kc p) n -> p kc n", p=128))), writes=[R_w1], dma=True)
                  S.add("sp", lambda e: e.dma_start(out=rC[:], in_=ropeC), writes=[R_rope], dma=True)
                  S.add("sp", lambda e: e.dma_start(out=rS[:], in_=ropeS), writes=[R_rope], dma=True)
                  if True:
                      S.add("pool", lambda e: e.memset(vaug1[:], 1.0), writes=rtok("vaug", 0, NT))
                  it = 0
                  for half, (t0, N) in [(hf_, blk_) for hf_ in range(3) for blk_ in lat512]:
                      l0 = t0 - NCTX
                      if half < 2 and t0 == NCTX:
                          for dst, src in ((wq, wq1), (wqs, wqs1)):
                              S.add("pool", (lambda e, dst=dst, src=src, half=half: e.dma_start(out=dst[:], in_=src.rearrange("(kc p) n -> p kc n", p=128)[:, :, half * 512:(half + 1) * 512])),
                                    writes=[R_wq], dma=True)
                      for c in (range(half * 4, half * 4 + 4) if half < 2 else range(8, 10)):
                          s = it % 2
                          it += 1
                          b1, b2 = 2 * s, 2 * s + 1
                          wa, wb = (wq, wqs) if c < 8 else (wk, wks)
                          cc = (c % 4) if c < 8 else c - 8
                          for kc in range(8):
                              S.add("pe", (lambda e, kc=kc, cc=cc, wa=wa, t0=t0, N=N, b1=b1: e.matmul(bank(b1)[:, 0:N], lhsT=wa[:, kc, cc * 128:(cc + 1) * 128], rhs=UT[:, kc, t0:t0 + N],
                                                                                                 start=(kc == 0), stop=(kc == 7))),
                                    reads=[R_w1, R_wq] + rtok("UT", t0, t0 + N), writes=[RB[b1]])
                          for kc in range(8):
                              S.add("pe", (lambda e, kc=kc, cc=cc, wb=wb, t0=t0, N=N, b2=b2: e.matmul(bank(b2)[:, 0:N], lhsT=wb[:, kc, cc * 128:(cc + 1) * 128], rhs=UT[:, kc, t0:t0 + N],
                                                                                                 start=(kc == 0), stop=(kc == 7))),
                                    reads=[R_w1, R_wq] + rtok("UT", t0, t0 + N), writes=[RB[b2]])
                          S.add("dve", (lambda e, s=s, l0=l0, N=N, b1=b1: e.tensor_tensor(out=rt[s][:, 0, 0:N], in0=bank(b1)[:, 0:N], in1=rC[:, l0:l0 + N], op=ALU.mult)),
                                reads=[RB[b1], R_rope], writes=[R_rt[s]])
                          S.add("dve", (lambda e, s=s, l0=l0, N=N, b2=b2: e.tensor_tensor(out=rt[s][:, 1, 0:N], in0=bank(b2)[:, 0:N], in1=rS[:, l0:l0 + N], op=ALU.mult)),
                                reads=[RB[b2], R_rope], writes=[R_rt[s]])
                          if c < 8:
                              dst = qT1[:, c, l0:l0 + N]
                          else:
                              dst = kT1[:, c - 8, t0:t0 + N]
                          S.add("dve", (lambda e, s=s, N=N, dst=dst: e.tensor_tensor(out=dst, in0=rt[s][:, 0, 0:N], in1=rt[s][:, 1, 0:N], op=ALU.add)),
                                reads=[R_rt[s]], writes=rtok("qk", t0, t0 + N))
                  for cc in range(2):
                      s = it % 2
                      it += 1
                      b1 = 2 * s
                      for kc in range(8):
                          S.add("pe", (lambda e, kc=kc, cc=cc, b1=b1: e.matmul(bank(b1)[:, 0:256], lhsT=wk[:, kc, cc * 128:(cc + 1) * 128], rhs=UT[:, kc, 0:256], start=(kc == 0), stop=(kc == 7))),
                                reads=[R_w1] + rtok("UT", 0, 256), writes=[RB[b1]])
                      S.add("act", (lambda e, cc=cc, b1=b1: e.copy(out=kT1[:, cc, 0:256], in_=bank(b1)[:, 0:256])), reads=[RB[b1]], writes=rtok("qk", 0, 256))
                  for tt in range(18):
                      bk = 4 + tt % 2
                      for kc in range(8):
                          S.add("pe", (lambda e, kc=kc, tt=tt, bk=bk: e.matmul(bank(bk)[:, 0:256], lhsT=UT[:, kc, tt * 128:(tt + 1) * 128], rhs=wv[:, kc, :], start=(kc == 0), stop=(kc == 7))),
                                reads=[R_w1] + rtok("UT", tt * 128, tt * 128 + 128), writes=[RB[bk]])
                      for x in range(2):
                          S.add("act", (lambda e, tt=tt, bk=bk, x=x: e.copy(out=vaug1[:, tt, :, x * 128:x * 128 + 64],
                                                                          in_=bank(bk)[:, 0:256].rearrange("p (c x d) -> p c x d", c=2, x=2)[:, :, x, :])),
                                reads=[RB[bk]], writes=rtok("vaug", tt * 128, tt * 128 + 128))
                  S.barrier()
                  cur[0] = C0
                  mlu = alloc("mlu", [128, 256], F32)
                  S.add("sp", lambda e: e.dma_start(out=mlu[:], in_=maskLU), writes=[R_const], dma=True)
                  sbm = [alloc("sbm%d" % i, [128, 512], F32) for i in range(2)]
                  PT1 = [alloc("PT1_%d" % i, [128, 5, 512], BF16) for i in range(2)]
                  rec1 = [alloc("rec1_%d" % i, [128, 512], F32) for i in range(2)]
                  R_sbm = [Res("sbm0"), Res("sbm1")]
                  R_PT1 = [[Res("PT1_%d_%d" % (i, k)) for k in range(5)] for i in range(2)]
                  R_rec1 = [Res("rec1_0"), Res("rec1_1")]
                  it = 0
                  sbr = 0
                  mi = 0
                  for g in range(4):
                      m, sh = g // 2, g % 2
                      p0, p1 = sh * 64, sh * 64 + 64
                      nh0, nh1 = (0, 64) if sh == 0 else (64, 128)
                      dh0, dh1 = (64, 128) if sh == 0 else (0, 64)
                      vcol = sh * 64
                      for qb in range(16):
                          s = it % 2
                          it += 1
                          tiles = []
                          if qb > 0:
                              tiles.append((NCTX + (qb - 1) * 128, 0))
                          tiles.append((NCTX + qb * 128, None))
                          if qb < 15:
                              tiles.append((NCTX + (qb + 1) * 128, 1))
                          tiles += [(0, None), (128, None)]
                          nt = len(tiles)
                          for i, (kt0, mk) in enumerate(tiles):
                              bk = sbr % 4
                              sbr += 1
                              S.add("pe", (lambda e, kt0=kt0, bk=bk, m=m, p0=p0, p1=p1, qb=qb: e.matmul(bank(bk).rearrange("p (h q) -> p h q", h=4), lhsT=kT1[p0:p1, m, kt0:kt0 + 128],
                                                                                                   rhs=qT1[p0:p1, 4 * m:4 * m + 4, qb * 128:(qb + 1) * 128], start=True, stop=True)),
                                    reads=rtok("qk", kt0, kt0 + 128) + rtok("qk", NCTX + qb * 128, NCTX + qb * 128 + 128), writes=[RB[bk]])
                              if mk is None:
                                  S.add("act", (lambda e, s=s, i=i, bk=bk: e.activation(out=PT1[s][:, i, :], in_=bank(bk), func=AF.Exp, scale=0.125)), reads=[RB[bk]], writes=[R_PT1[s][i]])
                              else:
                                  ms = mi % 2
                                  mi += 1
                                  S.add("dve", (lambda e, ms=ms, mk=mk, bk=bk: e.scalar_tensor_tensor(out=sbm[ms][:].rearrange("p (h q) -> p h q", h=4), in0=bank(bk).rearrange("p (h q) -> p h q", h=4),
                                                                                                    scalar=0.125, in1=mlu[:, mk * 128:(mk + 1) * 128].unsqueeze(1).broadcast_to([128, 4, 128]),
                                                                                                    op0=ALU.mult, op1=ALU.add)),
                                        reads=[RB[bk], R_const], writes=[R_sbm[ms]])
                                  S.add("act", (lambda e, s=s, i=i, ms=ms: e.activation(out=PT1[s][:, i, :], in_=sbm[ms][:], func=AF.Exp)), reads=[R_sbm[ms]], writes=[R_PT1[s][i]])
                          ob = 4 + s
                          for i, (kt0, mk) in enumerate(tiles):
                              S.add("pe", (lambda e, i=i, kt0=kt0, s=s, ob=ob, m=m, vcol=vcol, nt=nt: e.matmul(bank(ob), lhsT=vaug1[:, kt0 // 128, m, vcol:vcol + 128], rhs=PT1[s][:, i, :],
                                                                                                          start=(i == 0), stop=(i == nt - 1))),
                                    reads=[R_PT1[s][i]] + rtok("vaug", kt0, kt0 + 128), writes=[RB[ob]])
                          S.add("dve", (lambda e, s=s, ob=ob, m=m, sh=sh, dh0=dh0, dh1=dh1: e.tensor_tensor(out=rec1[s][dh0:dh1, :].rearrange("p (h q) -> p h q", h=4),
                                                                                                        in0=bank(ob)[dh0:dh1, :].rearrange("p (h q) -> p h q", h=4),
                                                                                                        in1=esink[dh0:dh1, (m * 2 + sh) * 4:(m * 2 + sh) * 4 + 4].unsqueeze(2).broadcast_to([64, 4, 128]),
                                                                                                        op=ALU.add)),
                                reads=[RB[ob], R_const], writes=[R_rec1[s]])
                          S.add("dve", (lambda e, s=s, dh0=dh0, dh1=dh1: e.reciprocal(out=rec1[s][dh0:dh1, :], in_=rec1[s][dh0:dh1, :])), reads=[R_rec1[s]], writes=[R_rec1[s]])
                          q0 = NCTX + qb * 128
                          S.add("dve", (lambda e, s=s, ob=ob, m=m, q0=q0, nh0=nh0, nh1=nh1, dh0=dh0, dh1=dh1: e.tensor_tensor(out=oT[nh0:nh1, 4 * m:4 * m + 4, q0:q0 + 128],
                                                                                                                     in0=bank(ob)[nh0:nh1, :].rearrange("p (h q) -> p h q", h=4),
                                                                                                                     in1=rec1[s][dh0:dh1, :].rearrange("p (h q) -> p h q", h=4), op=ALU.mult)),
                                reads=[RB[ob], R_rec1[s]], writes=rtok("oT", q0, q0 + 128))
                  w_out_d = w_out1
                  tok_blocks = lat256

              S.barrier()
              cur[0] = C0
              gatesT = alloc("gatesT", [16, NT], F32)
              wo = alloc("wo", [128, 8, D], BF16)
              hold0_at = cur[0]
              hold = [alloc("hold%d" % i, [128, 8, 128], F32) for i in range(2)]
              pre = alloc("pre", [128, 8, 128], F32)
              prebf = alloc("prebf", [128, 8, 128], BF16)
              presq = alloc("presq", [128, 8, 128], BF16)
              t32 = alloc("t32", [128, 8, 128], F32)
              tmpo = [alloc("tmpo%d" % i, [128, 128], F32) for i in range(2)]
              mean_sb = alloc("mean_sb", [128, 128], F32)
              rstd_sb = alloc("rstd_sb", [128, 128], F32)
              lsb = alloc("lsb", [128, 18, 20], F32, at=hold0_at)
              rw = alloc("rw", [128, 18 * 96], F32, at=hold0_at + 1472)
              assert hold0_at + 1472 + 18 * 96 * 4 <= cur[0]
              R_wo = Res("wo")
              R_hold = [Res("hold0"), Res("hold1")]
              R_pre, R_t32 = Res("pre"), Res("t32")
              R_tmpo = [Res("tmpo0"), Res("tmpo1")]
              R_lt = [Res("prebf"), Res("presq"), Res("mean"), Res("rstd")]
              R_rout = Res("rout")
              S.add("pool", (lambda e, w_out_d=w_out_d: e.dma_start(out=wo[:], in_=w_out_d.rearrange("(kc p) n -> p kc n", p=128))), writes=[R_wo], dma=True)
              tiles4 = [t for (t0_, n_) in tok_blocks for t in range(t0_, t0_ + n_, 128)]
              for bi, t0 in enumerate(tiles4):
                  s = bi % 2
                  mc = mcol(t0)
                  tt = t0 // 128
                  S.add("sp", (lambda e, s=s, t0=t0: e.dma_start(out=hold[s][:], in_=Hs3[:, :, t0:t0 + 128])), reads=rtok("Hs", t0, t0 + 128), writes=[R_hold[s]], dma=True)
                  for oc in range(8):
                      bk = oc // 4
                      co = (oc % 4) * 128
                      for kc in range(8):
                          S.add("pe", (lambda e, kc=kc, oc=oc, bk=bk, co=co, t0=t0: e.matmul(bank(bk)[:, co:co + 128], lhsT=wo[:, kc, oc * 128:(oc + 1) * 128], rhs=oT[:, kc, t0:t0 + 128],
                                                                                           start=(kc == 0), stop=(kc == 7))),
                                reads=[R_wo] + rtok("oT", t0, t0 + 128), writes=[RB[bk]])
                  for oc in range(8):
                      bk = oc // 4
                      co = (oc % 4) * 128
                      ts = oc % 2
                      if l == 0:
                          vb = V("M2B", oc, t0)
                          S.add("act", (lambda e, oc=oc, bk=bk, co=co, ts=ts, mc=mc, vb=vb: e.activation(out=tmpo[ts][:], in_=bank(bk)[:, co:co + 128], func=AF.Identity,
                                                                                                   bias=vb, scale=modc(0, 2, oc, mc))),
                                reads=[RB[bk], R_mod, R_vecs], writes=[R_tmpo[ts]])
                      else:
                          S.add("act", (lambda e, oc=oc, bk=bk, co=co, ts=ts, mc=mc: e.activation(out=tmpo[ts][:], in_=bank(bk)[:, co:co + 128], func=AF.Identity,
                                                                                            scale=modc(1, 2, oc, mc))),
                                reads=[RB[bk], R_mod], writes=[R_tmpo[ts]])
                      S.add("dve", (lambda e, oc=oc, s=s, ts=ts: e.scalar_tensor_tensor(out=pre[:, oc, :], in0=hold[s][:, oc, :], scalar=ALPHA, in1=tmpo[ts][:], op0=ALU.mult, op1=ALU.add)),
                            reads=[R_hold[s], R_tmpo[ts]], writes=[R_pre])
                  ln_stats(pre[:], 8, 128, ones1k, prebf, presq, mean_sb, rstd_sb, 4, [R_pre], R_lt)
                  normalize(pre[:], 8, 128, mean_sb, rstd_sb, [R_pre], R_lt)
                  for ch in range(8):
                      affine(UT[:, ch, t0:t0 + 128], pre[:, ch, :], V("G4", ch, t0), V("B4", ch, t0), [R_pre, R_vecs], rtok("UT", t0, t0 + 128))
                      affine(t32[:, ch, :], pre[:, ch, :], V("G4", ch, t0), V("B4", ch, t0), [R_pre, R_vecs], [R_t32])
                      affine(hres[:, ch, t0:t0 + 128], pre[:, ch, :], V("GA1", ch, t0), V("BA1", ch, t0), [R_pre, R_vecs], rtok("hres", t0, t0 + 128))
                  for kc in range(8):
                      S.add("pe", (lambda e, kc=kc, tt=tt, l=l: e.matmul(bank(5)[:, tt * 20:tt * 20 + 20], lhsT=t32[:, kc, :], rhs=wrt[:, l * 160 + kc * 20:l * 160 + kc * 20 + 20],
                                                                        start=(kc == 0), stop=(kc == 7))),
                            reads=[R_t32, R_const], writes=[RB[5]])

              dump("S4", hres[:])
              dump("S4u", UT[:])
              S.barrier()
              T0 = tok_blocks[0][0] // 128
              T1 = 18
              nT = T1 - T0
              S.add("dve", lambda e: e.tensor_copy(out=lsb[:, T0:T1, :], in_=bank(5)[:, T0 * 20:T1 * 20].rearrange("p (t n) -> p t n", n=20)), reads=[RB[5]], writes=[R_rout])

              def rwv(i, n):
                  return rw[:, i * 18 * 4:(i * 18 * 4) + 18 * n].rearrange("p (t n) -> p t n", n=n)[:, T0:T1, :]

              def rop(fn):
                  S.add("dve", fn, reads=[R_rout], writes=[R_rout])

              lg = lsb[:, T0:T1, 0:4]
              le = lsb[:, T0:T1, 4:20].rearrange("p t (g x) -> p t g x", g=4)
              gmax, gsum, gp, m1, m2, dd, w1, w2 = [rwv(i, 1) for i in range(8)]
              gsh, gmask, elsel, mask1, el2, mask2, within, wa_ = [rwv(8 + i, 4) for i in range(8)]
              t44 = rw[:, 18 * 64:18 * 80].rearrange("p (t g x) -> p t g x", g=4, x=4)[:, T0:T1]
              gates = rw[:, 18 * 80:18 * 96].rearrange("p (t g x) -> p t g x", g=4, x=4)
              bc4 = lambda a: a.broadcast_to([128, nT, 4])
              rop(lambda e: e.tensor_reduce(out=gmax, in_=lg, axis=AX.X, op=ALU.max))
              rop(lambda e: e.tensor_tensor(out=gsh, in0=lg, in1=bc4(gmax), op=ALU.subtract))
              rop(lambda e: e.tensor_tensor(out=gmask, in0=lg, in1=bc4(gmax), op=ALU.is_equal))
              S.add("act", lambda e: e.activation(out=gsh, in_=gsh, func=AF.Exp), reads=[R_rout], writes=[R_rout])
              rop(lambda e: e.tensor_reduce(out=gsum, in_=gsh, axis=AX.X, op=ALU.add))
              rop(lambda e: e.reciprocal(out=gp, in_=gsum))
              rop(lambda e: e.tensor_tensor(out=t44, in0=le, in1=gmask.unsqueeze(3).broadcast_to([128, nT, 4, 4]), op=ALU.mult))
              rop(lambda e: e.tensor_reduce(out=elsel, in_=t44.rearrange("p t g x -> p t x g"), axis=AX.X, op=ALU.add))
              rop(lambda e: e.tensor_reduce(out=m1, in_=elsel, axis=AX.X, op=ALU.max))
              rop(lambda e: e.tensor_tensor(out=mask1, in0=elsel, in1=bc4(m1), op=ALU.is_equal))
              rop(lambda e: e.scalar_tensor_tensor(out=el2, in0=mask1, scalar=NEG, in1=elsel, op0=ALU.mult, op1=ALU.add))
              rop(lambda e: e.tensor_reduce(out=m2, in_=el2, axis=AX.X, op=ALU.max))
              rop(lambda e: e.tensor_tensor(out=mask2, in0=el2, in1=bc4(m2), op=ALU.is_equal))
              rop(lambda e: e.tensor_tensor(out=dd, in0=m2, in1=m1, op=ALU.subtract))
              S.add("act", lambda e: e.activation(out=dd, in_=dd, func=AF.Exp), reads=[R_rout], writes=[R_rout])
              rop(lambda e: e.tensor_scalar_add(out=w1, in0=dd, scalar1=1.0))
              rop(lambda e: e.reciprocal(out=w1, in_=w1))
              rop(lambda e: e.tensor_tensor(out=w1, in0=w1, in1=gp, op=ALU.mult))
              rop(lambda e: e.tensor_tensor(out=w2, in0=dd, in1=w1, op=ALU.mult))
              rop(lambda e: e.tensor_tensor(out=within, in0=mask1, in1=bc4(w1), op=ALU.mult))
              rop(lambda e: e.tensor_tensor(out=wa_, in0=mask2, in1=bc4(w2), op=ALU.mult))
              rop(lambda e: e.tensor_tensor(out=within, in0=within, in1=wa_, op=ALU.add))
              rop(lambda e: e.tensor_tensor(out=gates[:, T0:T1], in0=gmask.unsqueeze(3).broadcast_to([128, nT, 4, 4]), in1=within.unsqueeze(2).broadcast_to([128, nT, 4, 4]), op=ALU.mult))
              for tt in range(T0, T1):
                  bk = 6 + (tt // 4) % 2
                  S.add("pe", (lambda e, tt=tt, bk=bk: e.transpose(bank(bk)[0:16, (tt % 4) * 128:(tt % 4 + 1) * 128], gates[:, tt].rearrange("p g x -> p (g x)"), id32[:])),
                        reads=[R_rout, R_const], writes=[RB[bk]])
                  if tt % 4 == 3 or tt == T1 - 1:
                      ta = (tt // 4) * 4
                      ta0 = max(ta, T0)
                      S.add("act", (lambda e, bk=bk, ta=ta, ta0=ta0, tt=tt: e.copy(out=gatesT[0:16, ta0 * 128:(tt + 1) * 128], in_=bank(bk)[0:16, (ta0 - ta) * 128:(tt + 1 - ta) * 128])),
                            reads=[RB[bk]], writes=[R_rout])

              S.barrier()
              cur[0] = C0
              gatesT = alloc("gatesT", [16, NT], F32)
              wgs = [alloc("wgs%d" % i, [128, 8, 256], BF16) for i in range(2)]
              wus = [alloc("wus%d" % i, [128, 8, 256], BF16) for i in range(2)]
              wds = [alloc("wds%d" % i, [128, 2, D], BF16) for i in range(2)]
              sgm = [alloc("sgm%d" % i, [128, 2, 512], F32) for i in range(2)]
              hgm = [alloc("hgm%d" % i, [128, 2, 512], BF16) for i in range(2)]
              R_ew = [Res("ew0"), Res("ew1")]
              R_sgm = [Res("sgm0"), Res("sgm1")]
              R_hgm = [Res("hgm0"), Res("hgm1")]
              mblocks = blocks512 if l == 0 else lat512
              it = 0
              ybc = [0]
              pend = [None]
              for ex in range(16):
                  s = ex % 2
                  S.add("pool", (lambda e, s=s, ex=ex, l=l: e.dma_start(out=wgs[s][:], in_=ewg[l, ex].rearrange("(kc p) f -> p kc f", p=128))), writes=[R_ew[s]], dma=True)
                  S.add("pool", (lambda e, s=s, ex=ex, l=l: e.dma_start(out=wus[s][:], in_=ewu[l, ex].rearrange("(kc p) f -> p kc f", p=128))), writes=[R_ew[s]], dma=True)
                  S.add("pool", (lambda e, s=s, ex=ex, l=l: e.dma_start(out=wds[s][:], in_=ewd[l, ex].rearrange("(kc p) f -> p kc f", p=128))), writes=[R_ew[s]], dma=True)
                  for (t0, N) in mblocks:
                      q = it % 2
                      it += 1
                      mc = mcol(t0)
                      S.add("pe", (lambda e, ex=ex, t0=t0, N=N: e.matmul(bank(4)[:, 0:N], lhsT=selt[0:16, ex * 128:(ex + 1) * 128], rhs=gatesT[0:16, t0:t0 + N], start=True, stop=True)),
                            reads=[R_rout, R_const], writes=[RB[4]])
                      for oc in range(4):
                          wsrc = wgs[s] if oc < 2 else wus[s]
                          for kc in range(8):
                              S.add("pe", (lambda e, kc=kc, oc=oc, wsrc=wsrc, t0=t0, N=N: e.matmul(bank(oc)[:, 0:N], lhsT=wsrc[:, kc, (oc % 2) * 128:(oc % 2 + 1) * 128], rhs=UT[:, kc, t0:t0 + N],
                                                                                              start=(kc == 0), stop=(kc == 7))),
                                    reads=[R_ew[s]] + rtok("UT", t0, t0 + N), writes=[RB[oc]])
                      S.add("act", (lambda e, q=q, N=N: e.activation(out=sgm[q][:, :, 0:N], in_=PS[0][:].rearrange("p (j n) -> p j n", j=2)[:, :, 0:N], func=AF.Silu)),
                            reads=[RB[0], RB[1]], writes=[R_sgm[q]])
                      S.add("dve", (lambda e, q=q, N=N: e.tensor_tensor(out=sgm[q][:, :, 0:N], in0=sgm[q][:, :, 0:N], in1=PS[1][:].rearrange("p (j n) -> p j n", j=2)[:, :, 0:N], op=ALU.mult)),
                            reads=[RB[2], RB[3], R_sgm[q]], writes=[R_sgm[q]])
                      S.add("dve", (lambda e, q=q, N=N: e.tensor_tensor(out=hgm[q][:, :, 0:N], in0=sgm[q][:, :, 0:N], in1=bank(4)[:, 0:N].unsqueeze(1).broadcast_to([128, 2, N]), op=ALU.mult)),
                            reads=[RB[4], R_sgm[q]], writes=[R_hgm[q]])
                      def emit_y(s=s, q=q, t0=t0, N=N, mc=mc, l=l):
                          for dc in range(8):
                              bk = 5 + ybc[0] % 3
                              ybc[0] += 1
                              for k2 in range(2):
                                  S.add("pe", (lambda e, k2=k2, dc=dc, bk=bk, s=s, q=q, N=N: e.matmul(bank(bk)[:, 0:N], lhsT=wds[s][:, k2, dc * 128:(dc + 1) * 128], rhs=hgm[q][:, k2, 0:N],
                                                                                                 start=(k2 == 0), stop=(k2 == 1))),
                                        reads=[R_ew[s], R_hgm[q]], writes=[RB[bk]])
                              S.add("dve", (lambda e, dc=dc, bk=bk, t0=t0, N=N, mc=mc, l=l: e.scalar_tensor_tensor(out=hres[:, dc, t0:t0 + N], in0=bank(bk)[:, 0:N], scalar=modc(l, 5, dc, mc),
                                                                                                              in1=hres[:, dc, t0:t0 + N], op0=ALU.mult, op1=ALU.add)),
                                    reads=[RB[bk], R_mod] + rtok("hres", t0, t0 + N), writes=rtok("hres", t0, t0 + N))
                      if pend[0] is not None:
                          pend[0]()
                      pend[0] = emit_y
              if pend[0] is not None:
                  pend[0]()
                  pend[0] = None

              dump("S5", hres[:])
              S.barrier()
              cur[0] = C0
              pre2 = alloc("pre2", [128, 8, 256], BF16)
              presq2 = alloc("presq2", [128, 8, 256], BF16)
              mean2 = alloc("mean2", [128, 256], F32)
              rstd2 = alloc("rstd2", [128, 256], F32)
              otile = [alloc("otile%d" % i, [128, D], F32) for i in range(2)]
              R_l2 = [Res("pre2"), Res("presq2"), Res("mean2"), Res("rstd2")]
              R_ot = [Res("ot0"), Res("ot1")]
              oi = 0
              for bi, (t0, N) in enumerate(tok_blocks):
                  hap = hres[:, :, t0:t0 + 256]
                  rh = rtok("hres", t0, t0 + 256)
                  ln_stats(hap, 8, 256, ones1k, pre2, presq2, mean2, rstd2, 4, rh, R_l2)
                  normalize(hap, 8, 256, mean2, rstd2, rh, R_l2)
                  if l == 0:
                      for ch in range(8):
                          affine(UT[:, ch, t0:t0 + 256], hres[:, ch, t0:t0 + 256], V("GU", ch, t0), V("BU", ch, t0), rh + [R_vecs], rtok("UT", t0, t0 + 256))
                      for ch in range(8):
                          affine(hres[:, ch, t0:t0 + 256], hres[:, ch, t0:t0 + 256], smc("ln_g", 8 + ch), smc("ln_b", 8 + ch), rh + [R_const], rh)
                      S.add("sp", (lambda e, t0=t0: e.dma_start(out=Hs3[:, :, t0:t0 + 256], in_=hres[:, :, t0:t0 + 256])), reads=rh, writes=rtok("Hs", t0, t0 + 256), dma=True)
                      if debug and nlayers == 1:
                          S.add("sp", (lambda e, t0=t0: e.dma_start(out=dbg.rearrange("p (c t) -> p c t", c=8)[:, :, t0:t0 + 256], in_=hres[:, :, t0:t0 + 256])), reads=rh,
                                writes=[Res("dbgo")], dma=True)
                  else:
                      for ch in range(8):
                          affine(hres[:, ch, t0:t0 + 256], hres[:, ch, t0:t0 + 256], smc("ln_g", 24 + ch), smc("ln_b", 24 + ch), rh + [R_const], rh)
                      for hh in range(2):
                          tk = t0 + hh * 128
                          so = oi % 2
                          oi += 1
                          for ch in range(8):
                              bk = so * 2 + ch // 4
                              S.add("pe", (lambda e, ch=ch, bk=bk, tk=tk: e.transpose(bank(bk)[:, (ch % 4) * 128:(ch % 4 + 1) * 128], hres[:, ch, tk:tk + 128], id32[:])),
                                    reads=rh + [R_const], writes=[RB[bk]])
                          for hf in range(2):
                              bk = so * 2 + hf
                              S.add("act" if hf else "dve", (lambda e, so=so, hf=hf, bk=bk: (e.copy if hf else e.tensor_copy)(out=otile[so][:, hf * 512:(hf + 1) * 512], in_=bank(bk))),
                                    reads=[RB[bk]], writes=[R_ot[so]])
                          S.add("sp", (lambda e, so=so, tk=tk, b=b: e.dma_start(out=outd[b, tk - NCTX:tk - NCTX + 128, :], in_=otile[so][:])), reads=[R_ot[so]], writes=[Res("outw")], dma=True)
    except _Stop:
        pass
    S.barrier()

    with nc.Block() as block:
        @block.tensor
        def _(e):
            S.emit_one("pe", e, esem, dsems)

        @block.scalar
        def _(e):
            S.emit_one("act", e, esem, dsems)

        @block.vector
        def _(e):
            S.emit_one("dve", e, esem, dsems)

        @block.gpsimd
        def _(e):
            S.emit_one("pool", e, esem, dsems)

        @block.sync
        def _(e):
            S.emit_one("sp", e, esem, dsems)
    es.close()
    return nc


def _prep_shared(inp):
    f = lambda a: np.ascontiguousarray(np.asarray(a, np.float32))
    sm = np.zeros((128, SMN), np.float32)

    def put(name, arr):
        arr = np.asarray(arr, np.float32)
        sm[:, SMO[name]:SMO[name] + arr.shape[1]] = arr

    put("ada_b0", _fm(inp["ada_b"][0]))
    put("ada_b1", _fm(inp["ada_b"][1]))
    put("ln_g", np.concatenate([_fm(inp["ln_g"][l, k]) for l in range(2) for k in range(2)], axis=1))
    put("ln_b", np.concatenate([_fm(inp["ln_b"][l, k]) for l in range(2) for k in range(2)], axis=1))
    b_in = np.asarray(inp["ab_b_in"][0], np.float32)
    put("b_in", _fm(b_in[:2048]))
    cw = np.asarray(inp["conv_w"][0], np.float32)
    put("conv_w", np.ascontiguousarray(cw.T.reshape(4, 128, 31).transpose(1, 0, 2).reshape(128, 124)))
    put("conv_b", _fm(inp["conv_b"][0]))
    put("cln_g", _fm(inp["conv_ln_g"][0]))
    put("cln_b", _fm(inp["conv_ln_b"][0]))
    put("b_out", _fm(inp["ab_b_out"][0]))
    sm[:, SMO["eps"]] = EPS
    qidx = _gqa_qidx()
    gw = np.asarray(inp["gqa_w_in"][0], np.float32)
    wq = gw[:, :1024]
    wkk = gw[:, 1024:1280]
    wvv = gw[:, 1280:1536]
    C, Sg = _rope_tables()
    kk = np.arange(128)[:, None]
    qq = np.arange(128)[None, :]
    maskL = np.where(kk >= qq, 0.0, NEG).astype(np.float32)
    maskU = np.where(kk <= qq, 0.0, NEG).astype(np.float32)
    sink = np.asarray(inp["gqa_sink"][0], np.float32)
    sperm = np.array([8 * m + 4 * sh + j for m in range(2) for sh in range(2) for j in range(4)])
    sel = np.zeros((16, 16, 128), np.float32)
    for ex in range(16):
        sel[ex, ex, :] = 1.0
    wr = np.stack([np.concatenate([np.asarray(inp["router_group"][l], np.float32), np.asarray(inp["router_expert"][l], np.float32)], axis=1)
                   .reshape(8, 128, 20).transpose(1, 0, 2).reshape(128, 160) for l in range(2)])
    bv = b_in[2048:2560]
    shared = {
        "ada_w": f(inp["ada_w"]),
        "sm": sm,
        "w_in0": f(inp["ab_w_in"][0]),
        "bvbc": np.ascontiguousarray(np.broadcast_to(bv[None, :], (128, 512))),
        "nab": np.ascontiguousarray(_na_bias_table(np.asarray(inp["na_rpb"][0], np.float32)).reshape(8, 128, 21 * 128)),
        "w_out0": f(inp["ab_w_out"][0]),
        "wq1": f(wq[:, qidx]),
        "wqs1": f(wq[:, qidx][:, _swap64(1024)]),
        "wk1": f(wkk),
        "wks1": f(wkk[:, _swap64(256)]),
        "wv1": f(wvv),
        "w_out1": f(np.asarray(inp["gqa_w_out"][0], np.float32)[qidx, :]),
        "ropeC": C,
        "ropeS": Sg,
        "maskLU": np.ascontiguousarray(np.concatenate([maskL, maskU], axis=1)),
        "sinkbc": np.ascontiguousarray(np.broadcast_to(sink[sperm][None, :], (128, 16))),
        "wr": f(wr),
        "sel": np.ascontiguousarray(sel.reshape(16, 2048)),
        "ident": np.eye(128, dtype=np.float32),
        "ewg": f(inp["exp_w_gate"]),
        "ewu": f(inp["exp_w_up"]),
        "ewd": f(inp["exp_w_down"]),
    }
    return shared


def _core_inputs(inp, shared, i):
    x = np.asarray(inp["x"], np.float32)
    ctx = np.asarray(inp["ctx"], np.float32)
    c = np.asarray(inp["c"], np.float32)
    cc = np.stack([c[2 * i], c[2 * i + 1], np.asarray(inp["c_ctx"], np.float32)])
    cvec = np.ascontiguousarray(cc.reshape(3, 8, 128).transpose(2, 1, 0).reshape(128, 24))
    m = dict(shared)
    m["x2"] = np.ascontiguousarray(x[2 * i:2 * i + 2])
    m["ctx2"] = np.ascontiguousarray(ctx[2 * i:2 * i + 2])
    m["cvec"] = cvec
    return m


_NC_CACHE = {}


def kernel(**inputs):
    n = 8
    if "nc" not in _NC_CACHE:
        _NC_CACHE["nc"] = build()
    nc = _NC_CACHE["nc"]
    shared = _prep_shared(inputs)
    in_maps = [_core_inputs(inputs, shared, i) for i in range(n)]
    res = run_bass_kernel_spmd(nc, in_maps, core_ids=list(range(n)))
    out = np.concatenate([np.asarray(r["out"], np.float32) for r in res.results], axis=0)
    return out
```

```python
import numpy as np
from contextlib import ExitStack
import concourse.bass as bass
import concourse.mybir as mybir
from concourse.bass_utils import run_bass_kernel_spmd

F32 = mybir.dt.float32
BF16 = mybir.dt.bfloat16
AF = mybir.ActivationFunctionType
ALU = mybir.AluOpType
AX = mybir.AxisListType

D = 1024
SEQ = 2048
NCTX = 256
NT = SEQ + NCTX
GW = 64
ALPHA = 4.0 ** 0.25
EPS = 1e-5
NEG = -1e30

ENGS = ("pe", "act", "dve", "pool", "sp")
N_DMA_SEMS = 40


class Res:
    __slots__ = ("name", "last_w", "readers")

    def __init__(self, name):
        self.name = name
        self.last_w = None
        self.readers = []


class Op:
    __slots__ = ("eng", "fn", "idx", "deps", "dma", "sig", "semval", "dsem", "dval", "dprev")

    def __init__(self, eng, fn, idx, dma):
        self.eng = eng
        self.fn = fn
        self.idx = idx
        self.deps = []
        self.dma = dma
        self.sig = False
        self.semval = 0
        self.dsem = -1
        self.dval = 0
        self.dprev = 0


class Sched:
    def __init__(self):
        self.ops = {e: [] for e in ENGS}
        self.ndma = 0
        self.dma_tot = [0] * N_DMA_SEMS
        self.last_dma = [None] * N_DMA_SEMS
        self._assigned = False

    def add(self, eng, fn, reads=(), writes=(), dma=False, extra=()):
        lst = self.ops[eng]
        op = Op(eng, fn, len(lst), dma)
        deps = {}
        for r in reads:
            if r.last_w is not None:
                deps[id(r.last_w)] = r.last_w
        for w in writes:
            if w.last_w is not None:
                deps[id(w.last_w)] = w.last_w
            for rd in w.readers:
                deps[id(rd)] = rd
        for x in extra:
            deps[id(x)] = x
        for r in reads:
            r.readers.append(op)
        for w in writes:
            w.last_w = op
            w.readers = []
        if dma:
            s = self.ndma % N_DMA_SEMS
            self.ndma += 1
            op.dsem = s
            op.dprev = self.dma_tot[s]
            self.dma_tot[s] += 16
            op.dval = self.dma_tot[s]
            self.last_dma[s] = op
        for d in deps.values():
            if d is op:
                continue
            if d.eng == eng and not d.dma and not dma:
                if eng == "pe":
                    continue
                if op.idx - d.idx > 2:
                    continue
            op.deps.append(d)
            if not d.dma:
                d.sig = True
        lst.append(op)
        return op

    def barrier(self):
        lasts = []
        for e in ENGS:
            for op in reversed(self.ops[e]):
                if not op.dma:
                    lasts.append(op)
                    break
        dl = [o for o in self.last_dma if o is not None]
        for e in ENGS:
            self.add(e, lambda eng: eng.nop(), extra=[o for o in lasts if o.eng != e] + dl)

    def emit_one(self, e, eng, esem, dsems):
        if not self._assigned:
            for ee in ENGS:
                c = 0
                for op in self.ops[ee]:
                    if op.sig and not op.dma:
                        c += 1
                        op.semval = c
            self._assigned = True
        seen = {}
        for op in self.ops[e]:
            need = {}
            for d in op.deps:
                if d.dma:
                    key = ("d", d.dsem)
                    val = d.dval
                else:
                    key = ("e", d.eng)
                    val = d.semval
                if val > need.get(key, 0):
                    need[key] = val
            if op.dma and op.dprev > 0:
                key = ("d", op.dsem)
                if op.dprev > need.get(key, 0):
                    need[key] = op.dprev
            for key, val in need.items():
                if seen.get(key, 0) >= val:
                    continue
                seen[key] = val
                sem = dsems[key[1]] if key[0] == "d" else esem[key[1]]
                eng.wait_ge(sem, val)
            ins = op.fn(eng)
            if op.dma:
                ins.then_inc(dsems[op.dsem], 16)
            elif op.sig:
                ins.then_inc(esem[e], 1)


def _sm_layout():
    off = {}
    n = 0
    for name, cols in (("ada_b0", 48), ("ada_b1", 48), ("ln_g", 32), ("ln_b", 32), ("b_in", 16),
                       ("conv_w", 124), ("conv_b", 4), ("cln_g", 4), ("cln_b", 4), ("b_out", 8), ("eps", 1)):
        off[name] = n
        n += cols
    return off, n


SMO, SMN = _sm_layout()


def _fm(v):
    v = np.asarray(v, np.float32)
    return np.ascontiguousarray(v.reshape(-1, 128).T)


def _gqa_qidx():
    idx = np.zeros(1024, np.int64)
    for c in range(8):
        m, j = divmod(c, 4)
        h0 = 8 * m + j
        h1 = 8 * m + 4 + j
        idx[c * 128:c * 128 + 64] = h0 * 64 + np.arange(64)
        idx[c * 128 + 64:c * 128 + 128] = h1 * 64 + np.arange(64)
    return idx


def _swap64(n):
    d = np.arange(n)
    dd = d % 64
    sw = np.where(dd % 32 < 16, dd + 16, dd - 16)
    return (d // 64) * 64 + sw


def _na_tiles(j):
    if j in (0, 1):
        return [0, 1, 2, 3]
    if j in (14, 15):
        return [12, 13, 14, 15]
    return [j - 2, j - 1, j, j + 1, j + 2]


def _na_tile_index(j):
    if j == 0:
        return 5
    if j == 1:
        return 9
    if j == 14:
        return 13
    if j == 15:
        return 17
    return 0


def _na_bias_table(rpb):
    rows = 32
    r = np.arange(rows)
    row_start = np.clip(r - 4, 0, rows - 8)
    jj = np.arange(GW)
    col_start = np.clip(jj - 8, 0, GW - 16)
    col_in = (jj[None, :] >= col_start[:, None]) & (jj[None, :] < col_start[:, None] + 16)
    col_off = np.clip(jj[None, :] - jj[:, None], -15, 15) + 15
    out = np.full((8, 21, 128, 128), NEG, np.float32)

    def tile(j, a):
        t = np.full((8, 128, 128), NEG, np.float32)
        for pk in range(2):
            rk = 2 * a + pk
            for pq in range(2):
                rq = 2 * j + pq
                if not (row_start[rq] <= rk < row_start[rq] + 8):
                    continue
                ro = rk - rq + 7
                blk = rpb[:, ro][:, col_off]
                blk = np.where(col_in[None], blk, np.float32(NEG))
                t[:, pk * 64:(pk + 1) * 64, pq * 64:(pq + 1) * 64] = blk.transpose(0, 2, 1)
        return t

    for i, a in enumerate(_na_tiles(5)):
        out[:, i] = tile(5, a)
    for j in (0, 1, 14, 15):
        base = _na_tile_index(j)
        for i, a in enumerate(_na_tiles(j)):
            out[:, base + i] = tile(j, a)
    return np.ascontiguousarray(out.transpose(0, 2, 1, 3))


def _rope_tables():
    t = np.arange(SEQ)
    row = (t // GW).astype(np.float32)
    col = (t % GW).astype(np.float32)
    inv = (np.float32(10000.0) ** (-np.arange(0, 32, 2, dtype=np.float32) / np.float32(32))).astype(np.float32)
    ang = np.concatenate([row[:, None] * inv, col[:, None] * inv], axis=-1).astype(np.float32)
    cos = np.cos(ang).astype(np.float32)
    sin = np.sin(ang).astype(np.float32)
    p = np.arange(128)
    d = p % 64
    ai = (d // 32) * 16 + d % 16
    sgn = np.where(d % 32 < 16, -1.0, 1.0).astype(np.float32)
    C = np.ascontiguousarray(cos[:, ai].T)
    S = np.ascontiguousarray((sin[:, ai] * sgn[None, :]).T)
    return C.astype(np.float32), S.astype(np.float32)


class _Stop(Exception):
    pass


def build(nlayers=2, nb=2, debug=False, stop=None):
    nc = bass.Bass("TRN2", target_bir_lowering=False)
    S = Sched()

    def din(name, shape):
        return nc.dram_tensor(name, list(shape), F32, kind="ExternalInput").ap()

    x2 = din("x2", [2, SEQ, D])
    ctx2 = din("ctx2", [2, NCTX, D])
    cvec = din("cvec", [128, 24])
    ada_w = din("ada_w", [2, D, 6 * D])
    smd = din("sm", [128, SMN])
    w_in0 = din("w_in0", [D, 2560])
    bvbc = din("bvbc", [128, 512])
    nab = din("nab", [8, 128, 21 * 128])
    w_out0 = din("w_out0", [D, D])
    wq1 = din("wq1", [D, 1024])
    wqs1 = din("wqs1", [D, 1024])
    wk1 = din("wk1", [D, 256])
    wks1 = din("wks1", [D, 256])
    wv1 = din("wv1", [D, 256])
    w_out1 = din("w_out1", [D, D])
    ropeC = din("ropeC", [128, SEQ])
    ropeS = din("ropeS", [128, SEQ])
    maskLU = din("maskLU", [128, 256])
    sinkbc = din("sinkbc", [128, 16])
    wr = din("wr", [2, 128, 160])
    sel = din("sel", [16, 2048])
    ident = din("ident", [128, 128])
    ewg = din("ewg", [2, 16, D, 256])
    ewu = din("ewu", [2, 16, D, 256])
    ewd = din("ewd", [2, 16, 256, D])
    outd = nc.dram_tensor("out", [2, SEQ, D], F32, kind="ExternalOutput").ap()
    Hs = nc.dram_tensor("Hs", [128, 8 * NT], F32, kind="Internal").ap()
    dbg = nc.dram_tensor("dbg", [128, 8 * NT], F32, kind="ExternalOutput").ap() if debug else None
    dbgb = nc.dram_tensor("dbgb", [128, 8 * NT], BF16, kind="ExternalOutput").ap() if debug else None
    Hs3 = Hs.rearrange("p (c t) -> p c t", c=8)

    es = ExitStack()
    cur = [16640]

    acache = {}

    def alloc(name, shape, dt, at=None):
        nbytes = int(np.prod(shape[1:])) * (4 if dt == F32 else 2)
        if at is None:
            at = cur[0]
            cur[0] = (at + nbytes + 63) // 64 * 64
        assert at + nbytes <= 229376, (name, at, nbytes)
        key = (name, at, tuple(shape))
        if key not in acache:
            acache[key] = nc.alloc_sbuf_tensor_at("%s_%d" % (name, len(acache)), list(shape), dt, offset=at)
        return acache[key]

    sm = alloc("sm", [128, SMN], F32)
    id32 = alloc("id32", [128, 128], F32)
    ones1k = alloc("ones1k", [128, 128], BF16)
    ones512 = alloc("ones512", [128, 128], BF16)
    csil = alloc("csil", [128, 24], BF16)
    cv32 = alloc("cv32", [128, 24], F32)
    mod = alloc("mod", [128, 2 * 144], F32)
    mp1 = alloc("mp1", [128, 2 * 144], F32)
    vecs = alloc("vecs", [128, 128], F32)
    selt = alloc("selt", [16, 2048], F32)
    wrt = alloc("wrt", [128, 320], F32)
    esink = alloc("esink", [128, 16], F32)
    R_const = Res("const")
    R_mod = Res("mod")
    R_vecs = Res("vecs")
    base0 = cur[0]

    PS = [es.enter_context(nc.psum_tensor("ps%d" % i, [128, 1024], F32)) for i in range(4)]
    RB = [Res("bank%d" % i) for i in range(8)]

    def bank(k):
        return PS[k // 2][:, (k % 2) * 512:(k % 2) * 512 + 512]

    esem = {e: es.enter_context(nc.semaphore("es_" + e)) for e in ENGS}
    dsems = [es.enter_context(nc.semaphore("ds%d" % i)) for i in range(N_DMA_SEMS)]

    def smc(name, j, n=1):
        o = SMO[name] + j
        return sm[:, o:o + n]

    def modc(l, k, ch, col):
        o = l * 144 + (k * 8 + ch) * 3 + col
        return mod[:, o:o + 1]

    def mp1c(l, k, ch, col):
        o = l * 144 + (k * 8 + ch) * 3 + col
        return mp1[:, o:o + 1]

    VK = {}

    def vslot(kind, ch):
        key = (kind, ch)
        if key not in VK:
            VK[key] = len(VK)
            assert len(VK) <= 128
        o = VK[key]
        return vecs[:, o:o + 1]

    S.add("sp", lambda e: e.dma_start(out=sm[:], in_=smd), writes=[R_const], dma=True)
    S.add("sp", lambda e: e.dma_start(out=id32[:], in_=ident), writes=[R_const], dma=True)
    S.add("sp", lambda e: e.dma_start(out=cv32[:], in_=cvec), writes=[R_const], dma=True)
    S.add("sp", lambda e: e.dma_start(out=selt[:], in_=sel), writes=[R_const], dma=True)
    S.add("sp", lambda e: e.dma_start(out=wrt[:].rearrange("p (l n) -> p l n", l=2), in_=wr.rearrange("l p n -> p l n")), writes=[R_const], dma=True)
    S.add("sp", lambda e: e.dma_start(out=esink[:], in_=sinkbc), writes=[R_const], dma=True)
    S.add("pool", lambda e: e.memset(ones1k[:], 1.0 / 1024.0), writes=[R_const])
    S.add("pool", lambda e: e.memset(ones512[:], 1.0 / 512.0), writes=[R_const])
    S.add("act", lambda e: e.activation(out=csil[:], in_=cv32[:], func=AF.Silu), reads=[R_const], writes=[R_const])
    S.add("act", lambda e: e.activation(out=esink[:], in_=esink[:], func=AF.Exp), reads=[R_const], writes=[R_const])

    adaw = [alloc("adaw%d" % i, [128, 8, 1024], BF16) for i in range(2)]
    R_adaw = [Res("adaw0"), Res("adaw1")]
    pi = 0
    for l in range(nlayers):
        awl = ada_w[l].rearrange("(kc p) n -> p kc n", p=128)
        for piece in range(6):
            s = pi % 2
            pi += 1
            S.add("pool", (lambda e, s=s, awl=awl, piece=piece: e.dma_start(out=adaw[s][:], in_=awl[:, :, piece * 1024:(piece + 1) * 1024])),
                  writes=[R_adaw[s]], dma=True)
            for oc8 in range(8):
                oc = piece * 8 + oc8
                for kc in range(8):
                    S.add("pe", (lambda e, s=s, oc=oc, oc8=oc8, kc=kc: e.matmul(bank(0)[:, oc * 3:oc * 3 + 3], lhsT=adaw[s][:, kc, oc8 * 128:(oc8 + 1) * 128],
                                                                                    rhs=csil[:, kc * 3:kc * 3 + 3], start=(kc == 0), stop=(kc == 7))),
                          reads=[R_adaw[s], R_const], writes=[RB[0]])
        ab = smc("ada_b%d" % l, 0, 48)
        S.add("dve", (lambda e, l=l, ab=ab: e.tensor_tensor(out=mod[:, l * 144:(l + 1) * 144].rearrange("p (a b) -> p a b", b=3),
                                                             in0=bank(0)[:, 0:144].rearrange("p (a b) -> p a b", b=3),
                                                             in1=ab.unsqueeze(2).broadcast_to([128, 48, 3]), op=ALU.add)),
              reads=[RB[0], R_const], writes=[R_mod])
        S.add("dve", (lambda e, l=l: e.tensor_scalar_add(out=mp1[:, l * 144:(l + 1) * 144], in0=mod[:, l * 144:(l + 1) * 144], scalar1=1.0)),
              reads=[R_mod], writes=[R_mod])
    S.barrier()
    cur[0] = base0

    A0 = cur[0]
    hres = alloc("hres", [128, 8, NT], F32)
    qT = alloc("qT", [128, 4, NT], BF16, at=A0)
    kT = alloc("kT", [128, 4, NT], BF16, at=A0 + 18432)
    vaug = alloc("vaug", [128, 18, 4, 192], BF16, at=A0 + 36864)
    qT1 = alloc("qT1", [128, 8, SEQ], BF16, at=A0)
    kT1 = alloc("kT1", [128, 2, NT], BF16, at=A0 + 32768)
    vaug1 = alloc("vaug1", [128, 18, 2, 192], BF16, at=A0 + 41984)
    rC = alloc("rC", [128, SEQ], F32, at=A0 + 55808)
    rS = alloc("rS", [128, SEQ], F32, at=A0 + 55808 + 8192)
    bvb = alloc("bvb", [128, 512], F32, at=A0 + 64512)
    idb = alloc("idb", [128, 128], BF16, at=A0 + 64512 + 2048)
    cmean = alloc("cmean", [128, 256], F32, at=A0 + 64512 + 2304)
    crstd = alloc("crstd", [128, 256], F32, at=A0 + 64512 + 3328)
    UT = alloc("UT", [128, 8, NT], BF16)
    oT = alloc("oT", [128, 8, NT], BF16)
    C0 = cur[0]
    RT = {}

    def rtok(name, t0, t1):
        out = []
        for tt in range(t0 // 128, (t1 + 127) // 128):
            key = (name, tt)
            if key not in RT:
                RT[key] = Res("%s_%d" % key)
            out.append(RT[key])
        return out

    R_hpad = [Res("hpad%d" % c) for c in range(4)]

    def derive_vecs(l, col, tag):
        ops = []
        for ch in range(8):
            g1 = smc("ln_g", (l * 2 + 0) * 8 + ch)
            b1 = smc("ln_b", (l * 2 + 0) * 8 + ch)
            g2 = smc("ln_g", (l * 2 + 1) * 8 + ch)
            b2 = smc("ln_b", (l * 2 + 1) * 8 + ch)
            S.add("dve", (lambda e, ch=ch, g1=g1: e.tensor_tensor(out=vslot((tag, "G4"), ch), in0=g1, in1=mp1c(l, 4, ch, col), op=ALU.mult)),
                  reads=[R_const, R_mod], writes=[R_vecs])
            S.add("dve", (lambda e, ch=ch, b1=b1: e.scalar_tensor_tensor(out=vslot((tag, "B4"), ch), in0=b1, scalar=mp1c(l, 4, ch, col), in1=modc(l, 3, ch, col),
                                                                         op0=ALU.mult, op1=ALU.add)),
                  reads=[R_const, R_mod], writes=[R_vecs])
            S.add("dve", (lambda e, ch=ch, g1=g1: e.tensor_scalar_mul(out=vslot((tag, "GA1"), ch), in0=g1, scalar1=ALPHA)), reads=[R_const], writes=[R_vecs])
            S.add("dve", (lambda e, ch=ch, b1=b1: e.tensor_scalar_mul(out=vslot((tag, "BA1"), ch), in0=b1, scalar1=ALPHA)), reads=[R_const], writes=[R_vecs])
            if l == 0:
                S.add("dve", (lambda e, ch=ch: e.tensor_tensor(out=vslot((tag, "M2B"), ch), in0=modc(l, 2, ch, col), in1=smc("b_out", ch), op=ALU.mult)),
                      reads=[R_const, R_mod], writes=[R_vecs])
                S.add("dve", (lambda e, ch=ch, g2=g2: e.tensor_tensor(out=vslot((tag, "GU"), ch), in0=g2, in1=mp1c(1, 1, ch, col), op=ALU.mult)),
                      reads=[R_const, R_mod], writes=[R_vecs])
                S.add("dve", (lambda e, ch=ch, b2=b2: e.scalar_tensor_tensor(out=vslot((tag, "BU"), ch), in0=b2, scalar=mp1c(1, 1, ch, col), in1=modc(1, 0, ch, col),
                                                                             op0=ALU.mult, op1=ALU.add)),
                      reads=[R_const, R_mod], writes=[R_vecs])

    def ln_stats(pre_ap, nch, N, ones_t, prebf, presq, mean_sb, rstd_sb, bk, r_pre, r_tmp):
        S.add("dve", lambda e: e.tensor_copy(out=prebf[:, 0:nch, 0:N], in_=pre_ap), reads=r_pre, writes=[r_tmp[0]])
        S.add("act", lambda e: e.activation(out=presq[:, 0:nch, 0:N], in_=pre_ap, func=AF.Square), reads=r_pre, writes=[r_tmp[1]])
        for c in range(nch):
            S.add("pe", (lambda e, c=c: e.matmul(bank(bk)[:, 0:N], lhsT=ones_t[:], rhs=prebf[:, c, 0:N], start=(c == 0), stop=(c == nch - 1))),
                  reads=[r_tmp[0], R_const], writes=[RB[bk]])
        for c in range(nch):
            S.add("pe", (lambda e, c=c: e.matmul(bank(bk)[:, 256:256 + N], lhsT=ones_t[:], rhs=presq[:, c, 0:N], start=(c == 0), stop=(c == nch - 1))),
                  reads=[r_tmp[1], R_const], writes=[RB[bk]])
        S.add("act", lambda e: e.copy(out=mean_sb[:, 0:N], in_=bank(bk)[:, 0:N]), reads=[RB[bk]], writes=[r_tmp[2]])
        S.add("dve", lambda e: e.tensor_tensor(out=rstd_sb[:, 0:N], in0=mean_sb[:, 0:N], in1=mean_sb[:, 0:N], op=ALU.mult), reads=[r_tmp[2]], writes=[r_tmp[3]])
        S.add("dve", lambda e: e.tensor_tensor(out=rstd_sb[:, 0:N], in0=bank(bk)[:, 256:256 + N], in1=rstd_sb[:, 0:N], op=ALU.subtract),
              reads=[RB[bk], r_tmp[3]], writes=[r_tmp[3]])
        S.add("act", lambda e: e.activation(out=rstd_sb[:, 0:N], in_=rstd_sb[:, 0:N], func=AF.Sqrt, bias=smc("eps", 0), scale=1.0),
              reads=[r_tmp[3], R_const], writes=[r_tmp[3]])
        S.add("dve", lambda e: e.reciprocal(out=rstd_sb[:, 0:N], in_=rstd_sb[:, 0:N]), reads=[r_tmp[3]], writes=[r_tmp[3]])

    def normalize(pre_ap, nch, N, mean_sb, rstd_sb, r_pre, r_tmp):
        S.add("dve", lambda e: e.tensor_tensor(out=pre_ap, in0=pre_ap, in1=mean_sb[:, 0:N].unsqueeze(1).broadcast_to([128, nch, N]), op=ALU.subtract),
              reads=r_pre + [r_tmp[2]], writes=r_pre)
        S.add("dve", lambda e: e.tensor_tensor(out=pre_ap, in0=pre_ap, in1=rstd_sb[:, 0:N].unsqueeze(1).broadcast_to([128, nch, N]), op=ALU.mult),
              reads=r_pre + [r_tmp[3]], writes=r_pre)

    aff_rr = [0]

    def affine(out_ap, in_ap, sc, bi, reads, writes, psum_in=False):
        k = aff_rr[0] % 2
        aff_rr[0] += 1
        if k == 0:
            S.add("act", lambda e: e.activation(out=out_ap, in_=in_ap, func=AF.Identity, bias=bi, scale=sc), reads=reads, writes=writes)
        else:
            S.add("dve" if k == 1 else "pool", lambda e: e.tensor_scalar(out=out_ap, in0=in_ap, scalar1=sc, scalar2=bi, op0=ALU.mult, op1=ALU.add),
                  reads=reads, writes=writes)

    def dump(name, src_ap3):
        if stop != name and stop != "%s@%d" % (name, cur_l[0]):
            return
        S.barrier()
        c, t = src_ap3.shape[1], src_ap3.shape[2]
        dst = dbg if src_ap3.dtype == F32 else dbgb
        for ci in range(c):
            S.add("sp", (lambda e, ci=ci: e.dma_start(out=dst[:, ci * t:(ci + 1) * t], in_=src_ap3[:, ci, :])), writes=[Res("dbgo")], dma=True)
        raise _Stop()

    cur_l = [0]
    try:
      for b in range(nb):
        for l in range(nlayers):
              cur_l[0] = l
              lat_only = (l == nlayers - 1) and l == 1
              col = b
              S.barrier()
              derive_vecs(l, b, "lat")
              if l == 0:
                  derive_vecs(l, 2, "ctx")

              def V(kind, ch, t0):
                  return vslot((("ctx" if (t0 < NCTX and l == 0) else "lat"), kind), ch)

              def mcol(t0):
                  return 2 if t0 < NCTX else b

              cur[0] = C0
              if l == 0:
                  xin = [alloc("xin%d" % i, [128, D], F32) for i in range(2)]
                  hblk = [alloc("hblk%d" % i, [128, 8, 128], F32) for i in range(2)]
                  R_xin = [Res("xin0"), Res("xin1")]
                  R_hblk = [Res("hblk0"), Res("hblk1")]
                  for tt in range(18):
                      s = tt % 2
                      src = ctx2[b, tt * 128:(tt + 1) * 128, :] if tt < 2 else x2[b, (tt - 2) * 128:(tt - 1) * 128, :]
                      S.add("sp", (lambda e, s=s, src=src: e.dma_start(out=xin[s][:], in_=src)), writes=[R_xin[s]], dma=True)
                      for ch in range(8):
                          bk = (tt % 2) * 2 + ch // 4
                          S.add("pe", (lambda e, s=s, ch=ch, bk=bk: e.transpose(bank(bk)[:, (ch % 4) * 128:(ch % 4 + 1) * 128], xin[s][:, ch * 128:(ch + 1) * 128], id32[:])),
                                reads=[R_xin[s], R_const], writes=[RB[bk]])
                      for hf in range(2):
                          bk = (tt % 2) * 2 + hf
                          S.add("act", (lambda e, s=s, hf=hf, bk=bk: e.copy(out=hblk[s][:, hf * 4:(hf + 1) * 4, :], in_=bank(bk).rearrange("p (c t) -> p c t", c=4))),
                                reads=[RB[bk]], writes=[R_hblk[s]])
                      mc = mcol(tt * 128)
                      for ch in range(8):
                          bk = (tt % 2) * 2 + ch // 4
                          affine(UT[:, ch, tt * 128:(tt + 1) * 128], bank(bk)[:, (ch % 4) * 128:(ch % 4 + 1) * 128], mp1c(0, 1, ch, mc), modc(0, 0, ch, mc),
                                 [RB[bk], R_mod], rtok("UT", tt * 128, tt * 128 + 128), psum_in=True)
                      S.add("sp", (lambda e, s=s, tt=tt: e.dma_start(out=Hs3[:, :, tt * 128:(tt + 1) * 128], in_=hblk[s][:])), reads=[R_hblk[s]],
                            writes=rtok("Hs", tt * 128, tt * 128 + 128), dma=True)

              if l == 0:
                  dump("S0", UT[:])
              blocks512 = [(0, 256)] + [(256 + 512 * i, 512) for i in range(4)]
              blocks256 = [(256 * i, 256) for i in range(9)]
              if l == 1:
                  lat512 = [(256 + 512 * i, 512) for i in range(4)]
                  lat256 = [(256 * i, 256) for i in range(1, 9)]

              if l == 0:
                  S.barrier()
                  cur[0] = C0
                  wAB = alloc("wAB", [128, 8, 1536], BF16)
                  wA = alloc("wA", [128, 8, 1024], BF16, at=C0)
                  hpd = [alloc("hpd%d" % i, [128, 2368], BF16, at=C0 + 16384 + i * 4736) for i in range(2)]
                  dg0 = alloc("diag0", [128, 31, 128], BF16, at=C0 + 25856)
                  diag = [dg0, dg0]
                  cur[0] = C0 + 33792
                  sgt = [alloc("sgt%d" % i, [128, 512], F32) for i in range(2)]
                  czsq = alloc("czsq", [128, 4, 256], BF16)
                  cz = alloc("cz", [128, 4, 256], F32)
                  R_wAB, R_misc = Res("wAB"), Res("misc0")
                  R_hpd = [Res("hpd0"), Res("hpd1")]
                  R_dg = [Res("dg0")] * 2
                  R_sgt = [Res("sgt0"), Res("sgt1")]
                  R_cz, R_ct = Res("cz"), [Res("czbf"), Res("czsq"), Res("cmean"), Res("crstd")]
                  w0 = w_in0.rearrange("(kc p) n -> p kc n", p=128)
                  S.add("pool", lambda e: e.dma_start(out=wA[:], in_=w0[:, :, 0:1024]), writes=[R_wAB], dma=True)
                  S.add("pool", lambda e: e.dma_start(out=idb[:], in_=ident), writes=[R_misc], dma=True)
                  S.add("sp", lambda e: e.dma_start(out=bvb[:], in_=bvbc), writes=[R_misc], dma=True)
                  S.add("pool", lambda e: e.memset(hpd[0][:], 0.0), writes=[R_hpd[0]])
                  S.add("pool", lambda e: e.memset(hpd[1][:], 0.0), writes=[R_hpd[1]])
                  S.add("pool", lambda e: e.memset(vaug[:], 1.0), writes=rtok("vaug", 0, NT))

                  def hoff(t0):
                      return 15 + t0 if t0 < NCTX else 286 + 15 + (t0 - NCTX)

                  it = 0
                  for cc in range(4):
                      hs_ = cc % 2
                      for k in range(31):
                          S.add("dve", (lambda e, cc=cc, k=k, hs_=hs_: e.tensor_scalar_mul(out=diag[hs_][:, k, :], in0=idb[:], scalar1=smc("conv_w", cc * 31 + k))),
                                reads=[R_misc, R_const], writes=[R_dg[hs_]])
                      for (t0, N) in blocks512:
                          s = it % 2
                          it += 1
                          b1, b2 = 2 * s, 2 * s + 1
                          for kc in range(8):
                              S.add("pe", (lambda e, kc=kc, cc=cc, t0=t0, N=N, b1=b1: e.matmul(bank(b1)[:, 0:N], lhsT=wA[:, kc, cc * 128:(cc + 1) * 128], rhs=UT[:, kc, t0:t0 + N],
                                                                                             start=(kc == 0), stop=(kc == 7))),
                                    reads=[R_wAB] + rtok("UT", t0, t0 + N), writes=[RB[b1]])
                          for kc in range(8):
                              S.add("pe", (lambda e, kc=kc, cc=cc, t0=t0, N=N, b2=b2: e.matmul(bank(b2)[:, 0:N], lhsT=wA[:, kc, 512 + cc * 128:512 + (cc + 1) * 128], rhs=UT[:, kc, t0:t0 + N],
                                                                                             start=(kc == 0), stop=(kc == 7))),
                                    reads=[R_wAB] + rtok("UT", t0, t0 + N), writes=[RB[b2]])
                          S.add("act", (lambda e, s=s, cc=cc, N=N, b2=b2: e.activation(out=sgt[s][:, 0:N], in_=bank(b2)[:, 0:N], func=AF.Sigmoid, bias=smc("b_in", 4 + cc), scale=1.0)),
                                reads=[RB[b2], R_const], writes=[R_sgt[s]])
                          ho = hoff(t0)
                          S.add("dve", (lambda e, s=s, cc=cc, N=N, b1=b1, ho=ho, hs_=hs_: e.scalar_tensor_tensor(out=hpd[hs_][:, ho:ho + N], in0=bank(b1)[:, 0:N], scalar=smc("b_in", cc),
                                                                                                                 in1=sgt[s][:, 0:N], op0=ALU.add, op1=ALU.mult)),
                                reads=[RB[b1], R_sgt[s], R_const], writes=[R_hpd[hs_]])
                      for bi, (t0, N) in enumerate(blocks512):
                          ho = hoff(t0) - 15
                          bk = 4 + bi % 2
                          for k in range(31):
                              S.add("pe", (lambda e, k=k, ho=ho, N=N, bk=bk, hs_=hs_: e.matmul(bank(bk)[:, 0:N], lhsT=diag[hs_][:, k, :], rhs=hpd[hs_][:, ho + k:ho + k + N],
                                                                                           start=(k == 0), stop=(k == 30))),
                                    reads=[R_dg[hs_], R_hpd[hs_]], writes=[RB[bk]])
                          S.add("act", (lambda e, cc=cc, N=N, t0=t0, bk=bk: e.activation(out=oT[:, cc, t0:t0 + N], in_=bank(bk)[:, 0:N], func=AF.Identity, bias=smc("conv_b", cc), scale=1.0)),
                                reads=[RB[bk], R_const], writes=rtok("oT", t0, t0 + N))
                  dump("S1z", oT[:, 0:4, :])
                  dump("S1h", hpd[1][:].unsqueeze(1))
                  dump("S1d", diag[0][:])
                  S.add("pool", lambda e: e.dma_start(out=wAB[:], in_=w0[:, :, 1024:2560]), writes=[R_wAB] + R_hpd + [R_dg[0]], dma=True)
                  for (t0, N) in blocks256:
                      zin = oT[:, 0:4, t0:t0 + 256]
                      rz = rtok("oT", t0, t0 + 256)
                      S.add("act", (lambda e, zin=zin: e.activation(out=czsq[:], in_=zin, func=AF.Square)), reads=rz, writes=[R_ct[1]])
                      for c4 in range(4):
                          S.add("pe", (lambda e, c4=c4, t0=t0: e.matmul(bank(6)[:, 0:256], lhsT=ones512[:], rhs=oT[:, c4, t0:t0 + 256], start=(c4 == 0), stop=(c4 == 3))),
                                reads=rz + [R_const], writes=[RB[6]])
                      for c4 in range(4):
                          S.add("pe", (lambda e, c4=c4: e.matmul(bank(6)[:, 256:512], lhsT=ones512[:], rhs=czsq[:, c4, :], start=(c4 == 0), stop=(c4 == 3))),
                                reads=[R_ct[1], R_const], writes=[RB[6]])
                      S.add("act", lambda e: e.copy(out=cmean[:], in_=bank(6)[:, 0:256]), reads=[RB[6]], writes=[R_ct[2]])
                      S.add("dve", lambda e: e.tensor_tensor(out=crstd[:], in0=cmean[:], in1=cmean[:], op=ALU.mult), reads=[R_ct[2]], writes=[R_ct[3]])
                      S.add("dve", lambda e: e.tensor_tensor(out=crstd[:], in0=bank(6)[:, 256:512], in1=crstd[:], op=ALU.subtract), reads=[RB[6], R_ct[3]], writes=[R_ct[3]])
                      S.add("act", lambda e: e.activation(out=crstd[:], in_=crstd[:], func=AF.Sqrt, bias=smc("eps", 0), scale=1.0), reads=[R_ct[3], R_const], writes=[R_ct[3]])
                      S.add("dve", lambda e: e.reciprocal(out=crstd[:], in_=crstd[:]), reads=[R_ct[3]], writes=[R_ct[3]])
                      S.add("dve", (lambda e, zin=zin: e.tensor_tensor(out=cz[:], in0=zin, in1=cmean[:].unsqueeze(1).broadcast_to([128, 4, 256]), op=ALU.subtract)),
                            reads=rz + [R_ct[2]], writes=[R_cz])
                      S.add("dve", lambda e: e.tensor_tensor(out=cz[:], in0=cz[:], in1=crstd[:].unsqueeze(1).broadcast_to([128, 4, 256]), op=ALU.mult),
                            reads=[R_cz, R_ct[3]], writes=[R_cz])
                      for c4 in range(4):
                          S.add("act", (lambda e, c4=c4, t0=t0: e.activation(out=oT[:, c4, t0:t0 + 256], in_=cz[:, c4, :], func=AF.Silu, bias=smc("cln_b", c4), scale=smc("cln_g", c4))),
                                reads=[R_cz, R_const], writes=rz)
                  it = 0
                  for (t0, N) in blocks512:
                      for c in range(8):
                          bk = it % 4
                          it += 1
                          for kc in range(8):
                              S.add("pe", (lambda e, kc=kc, c=c, t0=t0, N=N, bk=bk: e.matmul(bank(bk)[:, 0:N], lhsT=wAB[:, kc, c * 128:(c + 1) * 128], rhs=UT[:, kc, t0:t0 + N],
                                                                                           start=(kc == 0), stop=(kc == 7))),
                                    reads=[R_wAB] + rtok("UT", t0, t0 + N), writes=[RB[bk]])
                          dst = qT if c < 4 else kT
                          S.add("act", (lambda e, c=c, t0=t0, N=N, bk=bk, dst=dst: e.activation(out=dst[:, c % 4, t0:t0 + N], in_=bank(bk)[:, 0:N], func=AF.Identity,
                                                                                              bias=smc("b_in", 8 + c), scale=1.0)),
                                reads=[RB[bk], R_const], writes=rtok("qk", t0, t0 + N))
                  for tt in range(18):
                      bk = it % 4
                      it += 1
                      for kc in range(8):
                          S.add("pe", (lambda e, kc=kc, tt=tt, bk=bk: e.matmul(bank(bk)[:, 0:512], lhsT=UT[:, kc, tt * 128:(tt + 1) * 128], rhs=wAB[:, kc, 1024:1536],
                                                                             start=(kc == 0), stop=(kc == 7))),
                                reads=[R_wAB] + rtok("UT", tt * 128, tt * 128 + 128), writes=[RB[bk]])
                      for x in range(2):
                          S.add("dve", (lambda e, tt=tt, bk=bk, x=x: e.tensor_tensor(out=vaug[:, tt, :, x * 128:x * 128 + 64],
                                                                                   in0=bank(bk).rearrange("p (c x d) -> p c x d", c=4, x=2)[:, :, x, :],
                                                                                   in1=bvb[:].rearrange("p (c x d) -> p c x d", c=4, x=2)[:, :, x, :], op=ALU.add)),
                                reads=[RB[bk], R_misc], writes=rtok("vaug", tt * 128, tt * 128 + 128))

                  dump("S1o", oT[:, 0:4, :])
                  dump("S1q", qT[:])
                  dump("S1k", kT[:])
                  dump("S1v", vaug[:].rearrange("p t c x -> p t (c x)"))
                  S.barrier()
                  cur[0] = C0
                  nabt = [alloc("nabt%d" % i, [128, 21, 128], F32) for i in range(2)]
                  sbt = [alloc("sbt%d" % i, [128, 640], F32) for i in range(2)]
                  PT = [alloc("PT%d" % i, [128, 896], BF16) for i in range(2)]
                  rec = [alloc("rec%d" % i, [128, 256], F32) for i in range(2)]
                  R_nabt = [Res("nabt0"), Res("nabt1")]
                  R_sbt = [Res("sbt0"), Res("sbt1")]
                  R_PT = [Res("PT0"), Res("PT1")]
                  R_rec = [Res("rec0"), Res("rec1")]
                  it = 0
                  for h in range(8):
                      c, sh = h // 2, h % 2
                      p0, p1 = sh * 64, sh * 64 + 64
                      nh0, nh1 = (0, 64) if sh == 0 else (64, 128)
                      dh0, dh1 = (64, 128) if sh == 0 else (0, 64)
                      hs = h % 2
                      S.add("sp", (lambda e, h=h, hs=hs: e.dma_start(out=nabt[hs][:], in_=nab[h].rearrange("p (a q) -> p a q", a=21))), writes=[R_nabt[hs]], dma=True)
                      vcol = sh * 64
                      s = it % 2
                      it += 1
                      sb0 = 2 * s
                      for i in range(2):
                          S.add("pe", (lambda e, i=i, c=c, p0=p0, p1=p1, sb0=sb0: e.matmul(bank(sb0)[:, i * 256:(i + 1) * 256], lhsT=kT[p0:p1, c, i * 128:(i + 1) * 128],
                                                                                          rhs=qT[p0:p1, c, 0:256], start=True, stop=True)),
                                reads=rtok("qk", 0, 256), writes=[RB[sb0]])
                      S.add("act", (lambda e, s=s, sb0=sb0: e.activation(out=PT[s][:, 0:512], in_=bank(sb0)[:, 0:512], func=AF.Exp, scale=0.125)), reads=[RB[sb0]], writes=[R_PT[s]])
                      ob = 4 + s
                      for i in range(2):
                          S.add("pe", (lambda e, i=i, c=c, s=s, ob=ob, vcol=vcol: e.matmul(bank(ob)[:, 0:256], lhsT=vaug[:, i, c, vcol:vcol + 128], rhs=PT[s][:, i * 256:(i + 1) * 256],
                                                                                          start=(i == 0), stop=(i == 1))),
                                reads=[R_PT[s]] + rtok("vaug", 0, 256), writes=[RB[ob]])
                      S.add("dve", (lambda e, s=s, ob=ob, dh0=dh0, dh1=dh1: e.reciprocal(out=rec[s][dh0:dh1, 0:256], in_=bank(ob)[dh0:dh1, 0:256])), reads=[RB[ob]], writes=[R_rec[s]])
                      S.add("dve", (lambda e, s=s, ob=ob, c=c, nh0=nh0, nh1=nh1, dh0=dh0, dh1=dh1: e.tensor_tensor(out=oT[nh0:nh1, 4 + c, 0:256], in0=bank(ob)[nh0:nh1, 0:256],
                                                                                                               in1=rec[s][dh0:dh1, 0:256], op=ALU.mult)),
                            reads=[RB[ob], R_rec[s]], writes=rtok("oT", 0, 256))
                      for j in range(16):
                          s = it % 2
                          it += 1
                          sb0 = 2 * s
                          tl = _na_tiles(j)
                          nl = len(tl)
                          ti0 = _na_tile_index(j)
                          q0 = NCTX + j * 128
                          ktoks = [NCTX + a * 128 for a in tl] + [0, 128]
                          for i, kt0 in enumerate(ktoks):
                              bk = sb0 + (i // 4)
                              S.add("pe", (lambda e, i=i, kt0=kt0, bk=bk, c=c, p0=p0, p1=p1, q0=q0: e.matmul(bank(bk)[:, (i % 4) * 128:(i % 4 + 1) * 128], lhsT=kT[p0:p1, c, kt0:kt0 + 128],
                                                                                                        rhs=qT[p0:p1, c, q0:q0 + 128], start=True, stop=True)),
                                    reads=rtok("qk", kt0, kt0 + 128) + rtok("qk", q0, q0 + 128), writes=[RB[bk]])
                          S.add("dve", (lambda e, s=s, nl=nl, ti0=ti0, hs=hs: e.scalar_tensor_tensor(out=sbt[s][:, 0:nl * 128], in0=PS[s][:, 0:nl * 128], scalar=0.125,
                                                                                                    in1=nabt[hs][:, ti0:ti0 + nl, :].rearrange("p a q -> p (a q)"),
                                                                                                    op0=ALU.mult, op1=ALU.add)),
                                reads=[RB[sb0], RB[sb0 + 1], R_nabt[hs]], writes=[R_sbt[s]])
                          S.add("act", (lambda e, s=s, nl=nl: e.activation(out=PT[s][:, 0:nl * 128], in_=sbt[s][:, 0:nl * 128], func=AF.Exp)), reads=[R_sbt[s]], writes=[R_PT[s]])
                          S.add("act", (lambda e, s=s, nl=nl: e.activation(out=PT[s][:, nl * 128:(nl + 2) * 128], in_=PS[s][:, nl * 128:(nl + 2) * 128], func=AF.Exp, scale=0.125)),
                                reads=[RB[sb0], RB[sb0 + 1]], writes=[R_PT[s]])
                          ob = 4 + s
                          for i, kt0 in enumerate(ktoks):
                              S.add("pe", (lambda e, i=i, kt0=kt0, c=c, s=s, ob=ob, vcol=vcol, nl=nl: e.matmul(bank(ob)[:, 0:128], lhsT=vaug[:, kt0 // 128, c, vcol:vcol + 128],
                                                                                                          rhs=PT[s][:, i * 128:(i + 1) * 128], start=(i == 0), stop=(i == nl + 1))),
                                    reads=[R_PT[s]] + rtok("vaug", kt0, kt0 + 128), writes=[RB[ob]])
                          S.add("dve", (lambda e, s=s, ob=ob, dh0=dh0, dh1=dh1: e.reciprocal(out=rec[s][dh0:dh1, 0:128], in_=bank(ob)[dh0:dh1, 0:128])), reads=[RB[ob]], writes=[R_rec[s]])
                          S.add("dve", (lambda e, s=s, ob=ob, c=c, q0=q0, nh0=nh0, nh1=nh1, dh0=dh0, dh1=dh1: e.tensor_tensor(out=oT[nh0:nh1, 4 + c, q0:q0 + 128], in0=bank(ob)[nh0:nh1, 0:128],
                                                                                                                     in1=rec[s][dh0:dh1, 0:128], op=ALU.mult)),
                                reads=[RB[ob], R_rec[s]], writes=rtok("oT", q0, q0 + 128))
                  dump("S3", oT[:, 4:8, :])
                  w_out_d = w_out0
                  tok_blocks = blocks256
              else:
                  S.barrier()
                  cur[0] = C0
                  wq = alloc("wq", [128, 8, 512], BF16)
                  wqs = alloc("wqs", [128, 8, 512], BF16)
                  wk = alloc("wk", [128, 8, 256], BF16)
                  wks = alloc("wks", [128, 8, 256], BF16)
                  wv = alloc("wv", [128, 8, 256], BF16)
                  rt = [alloc("rt%d" % i, [128, 2, 512], F32) for i in range(2)]
                  R_w1, R_rope = Res("w1"), Res("rope")
                  R_rt = [Res("rt0"), Res("rt1")]
                  R_wq = Res("wq")
                  for dst, src in ((wk, wk1), (wks, wks1), (wv, wv1)):
                      S.add("pool", (lambda e, dst=dst, src=src: e.dma_start(out=dst[:], in_=src.rearrange("(kc p) n -> p kc n", p=128))), writes=[R_w1], dma=True)
                  S.add("sp", lambda e: e.dma_start(out=rC[:], in_=ropeC), writes=[R_rope], dma=True)
                  S.add("sp", lambda e: e.dma_start(out=rS[:], in_=ropeS), writes=[R_rope], dma=True)
                  if True:
                      S.add("pool", lambda e: e.memset(vaug1[:], 1.0), writes=rtok("vaug", 0, NT))
                  it = 0
                  for half, (t0, N) in [(hf_, blk_) for hf_ in range(3) for blk_ in lat512]:
                      l0 = t0 - NCTX
                      if half < 2 and t0 == NCTX:
                          for dst, src in ((wq, wq1), (wqs, wqs1)):
                              S.add("pool", (lambda e, dst=dst, src=src, half=half: e.dma_start(out=dst[:], in_=src.rearrange("(kc p) n -> p kc n", p=128)[:, :, half * 512:(half + 1) * 512])),
                                    writes=[R_wq], dma=True)
                      for c in (range(half * 4, half * 4 + 4) if half < 2 else range(8, 10)):
                          s = it % 2
                          it += 1
                          b1, b2 = 2 * s, 2 * s + 1
                          wa, wb = (wq, wqs) if c < 8 else (wk, wks)
                          cc = (c % 4) if c < 8 else c - 8
                          for kc in range(8):
                              S.add("pe", (lambda e, kc=kc, cc=cc, wa=wa, t0=t0, N=N, b1=b1: e.matmul(bank(b1)[:, 0:N], lhsT=wa[:, kc, cc * 128:(cc + 1) * 128], rhs=UT[:, kc, t0:t0 + N],
                                                                                                 start=(kc == 0), stop=(kc == 7))),
                                    reads=[R_w1, R_wq] + rtok("UT", t0, t0 + N), writes=[RB[b1]])
                          for kc in range(8):
                              S.add("pe", (lambda e, kc=kc, cc=cc, wb=wb, t0=t0, N=N, b2=b2: e.matmul(bank(b2)[:, 0:N], lhsT=wb[:, kc, cc * 128:(cc + 1) * 128], rhs=UT[:, kc, t0:t0 + N],
                                                                                                 start=(kc == 0), stop=(kc == 7))),
                                    reads=[R_w1, R_wq] + rtok("UT", t0, t0 + N), writes=[RB[b2]])
                          S.add("dve", (lambda e, s=s, l0=l0, N=N, b1=b1: e.tensor_tensor(out=rt[s][:, 0, 0:N], in0=bank(b1)[:, 0:N], in1=rC[:, l0:l0 + N], op=ALU.mult)),
                                reads=[RB[b1], R_rope], writes=[R_rt[s]])
                          S.add("dve", (lambda e, s=s, l0=l0, N=N, b2=b2: e.tensor_tensor(out=rt[s][:, 1, 0:N], in0=bank(b2)[:, 0:N], in1=rS[:, l0:l0 + N], op=ALU.mult)),
                                reads=[RB[b2], R_rope], writes=[R_rt[s]])
                          if c < 8:
                              dst = qT1[:, c, l0:l0 + N]
                          else:
                              dst = kT1[:, c - 8, t0:t0 + N]
                          S.add("dve", (lambda e, s=s, N=N, dst=dst: e.tensor_tensor(out=dst, in0=rt[s][:, 0, 0:N], in1=rt[s][:, 1, 0:N], op=ALU.add)),
                                reads=[R_rt[s]], writes=rtok("qk", t0, t0 + N))
                  for cc in range(2):
                      s = it % 2
                      it += 1
                      b1 = 2 * s
                      for kc in range(8):
                          S.add("pe", (lambda e, kc=kc, cc=cc, b1=b1: e.matmul(bank(b1)[:, 0:256], lhsT=wk[:, kc, cc * 128:(cc + 1) * 128], rhs=UT[:, kc, 0:256], start=(kc == 0), stop=(kc == 7))),
                                reads=[R_w1] + rtok("UT", 0, 256), writes=[RB[b1]])
                      S.add("act", (lambda e, cc=cc, b1=b1: e.copy(out=kT1[:, cc, 0:256], in_=bank(b1)[:, 0:256])), reads=[RB[b1]], writes=rtok("qk", 0, 256))
                  for tt in range(18):
                      bk = 4 + tt % 2
                      for kc in range(8):
                          S.add("pe", (lambda e, kc=kc, tt=tt, bk=bk: e.matmul(bank(bk)[:, 0:256], lhsT=UT[:, kc, tt * 128:(tt + 1) * 128], rhs=wv[:, kc, :], start=(kc == 0), stop=(kc == 7))),
                                reads=[R_w1] + rtok("UT", tt * 128, tt * 128 + 128), writes=[RB[bk]])
                      for x in range(2):
                          S.add("act", (lambda e, tt=tt, bk=bk, x=x: e.copy(out=vaug1[:, tt, :, x * 128:x * 128 + 64],
                                                                          in_=bank(bk)[:, 0:256].rearrange("p (c x d) -> p c x d", c=2, x=2)[:, :, x, :])),
                                reads=[RB[bk]], writes=rtok("vaug", tt * 128, tt * 128 + 128))
                  dump("P1", kT1[:])
                  S.barrier()
                  cur[0] = C0
                  mlu = alloc("mlu", [128, 256], F32)
                  S.add("sp", lambda e: e.dma_start(out=mlu[:], in_=maskLU), writes=[R_const], dma=True)
                  sbm = [alloc("sbm%d" % i, [128, 512], F32) for i in range(2)]
                  PT1 = [alloc("PT1_%d" % i, [128, 5, 512], BF16) for i in range(2)]
                  rec1 = [alloc("rec1_%d" % i, [128, 512], F32) for i in range(2)]
                  R_sbm = [Res("sbm0"), Res("sbm1")]
                  R_PT1 = [[Res("PT1_%d_%d" % (i, k)) for k in range(5)] for i in range(2)]
                  R_rec1 = [Res("rec1_0"), Res("rec1_1")]
                  it = 0
                  sbr = 0
                  mi = 0
                  for g in range(4):
                      m, sh = g // 2, g % 2
                      p0, p1 = sh * 64, sh * 64 + 64
                      nh0, nh1 = (0, 64) if sh == 0 else (64, 128)
                      dh0, dh1 = (64, 128) if sh == 0 else (0, 64)
                      vcol = sh * 64
                      for qb in range(16):
                          s = it % 2
                          it += 1
                          tiles = []
                          if qb > 0:
                              tiles.append((NCTX + (qb - 1) * 128, 0))
                          tiles.append((NCTX + qb * 128, None))
                          if qb < 15:
                              tiles.append((NCTX + (qb + 1) * 128, 1))
                          tiles += [(0, None), (128, None)]
                          nt = len(tiles)
                          for i, (kt0, mk) in enumerate(tiles):
                              bk = sbr % 4
                              sbr += 1
                              S.add("pe", (lambda e, kt0=kt0, bk=bk, m=m, p0=p0, p1=p1, qb=qb: e.matmul(bank(bk).rearrange("p (h q) -> p h q", h=4), lhsT=kT1[p0:p1, m, kt0:kt0 + 128],
                                                                                                   rhs=qT1[p0:p1, 4 * m:4 * m + 4, qb * 128:(qb + 1) * 128], start=True, stop=True)),
                                    reads=rtok("qk", kt0, kt0 + 128) + rtok("qk", NCTX + qb * 128, NCTX + qb * 128 + 128), writes=[RB[bk]])
                              if mk is None:
                                  S.add("act", (lambda e, s=s, i=i, bk=bk: e.activation(out=PT1[s][:, i, :], in_=bank(bk), func=AF.Exp, scale=0.125)), reads=[RB[bk]], writes=[R_PT1[s][i]])
                              else:
                                  ms = mi % 2
                                  mi += 1
                                  S.add("dve", (lambda e, ms=ms, mk=mk, bk=bk: e.scalar_tensor_tensor(out=sbm[ms][:].rearrange("p (h q) -> p h q", h=4), in0=bank(bk).rearrange("p (h q) -> p h q", h=4),
                                                                                                    scalar=0.125, in1=mlu[:, mk * 128:(mk + 1) * 128].unsqueeze(1).broadcast_to([128, 4, 128]),
                                                                                                    op0=ALU.mult, op1=ALU.add)),
                                        reads=[RB[bk], R_const], writes=[R_sbm[ms]])
                                  S.add("act", (lambda e, s=s, i=i, ms=ms: e.activation(out=PT1[s][:, i, :], in_=sbm[ms][:], func=AF.Exp)), reads=[R_sbm[ms]], writes=[R_PT1[s][i]])
                          ob = 4 + s
                          for i, (kt0, mk) in enumerate(tiles):
                              S.add("pe", (lambda e, i=i, kt0=kt0, s=s, ob=ob, m=m, vcol=vcol, nt=nt: e.matmul(bank(ob), lhsT=vaug1[:, kt0 // 128, m, vcol:vcol + 128], rhs=PT1[s][:, i, :],
                                                                                                          start=(i == 0), stop=(i == nt - 1))),
                                    reads=[R_PT1[s][i]] + rtok("vaug", kt0, kt0 + 128), writes=[RB[ob]])
                          S.add("dve", (lambda e, s=s, ob=ob, m=m, sh=sh, dh0=dh0, dh1=dh1: e.tensor_tensor(out=rec1[s][dh0:dh1, :].rearrange("p (h q) -> p h q", h=4),
                                                                                                        in0=bank(ob)[dh0:dh1, :].rearrange("p (h q) -> p h q", h=4),
                                                                                                        in1=esink[dh0:dh1, (m * 2 + sh) * 4:(m * 2 + sh) * 4 + 4].unsqueeze(2).broadcast_to([64, 4, 128]),
                                                                                                        op=ALU.add)),
                                reads=[RB[ob], R_const], writes=[R_rec1[s]])
                          S.add("dve", (lambda e, s=s, dh0=dh0, dh1=dh1: e.reciprocal(out=rec1[s][dh0:dh1, :], in_=rec1[s][dh0:dh1, :])), reads=[R_rec1[s]], writes=[R_rec1[s]])
                          q0 = NCTX + qb * 128
                          S.add("dve", (lambda e, s=s, ob=ob, m=m, q0=q0, nh0=nh0, nh1=nh1, dh0=dh0, dh1=dh1: e.tensor_tensor(out=oT[nh0:nh1, 4 * m:4 * m + 4, q0:q0 + 128],
                                                                                                                     in0=bank(ob)[nh0:nh1, :].rearrange("p (h q) -> p h q", h=4),
                                                                                                                     in1=rec1[s][dh0:dh1, :].rearrange("p (h q) -> p h q", h=4), op=ALU.mult)),
                                reads=[RB[ob], R_rec1[s]], writes=rtok("oT", q0, q0 + 128))
                  dump("A1", oT[:])
                  w_out_d = w_out1
                  tok_blocks = lat256

              S.barrier()
              cur[0] = C0
              gatesT = alloc("gatesT", [16, NT], F32)
              wo = alloc("wo", [128, 8, D], BF16)
              hold0_at = cur[0]
              hold = [alloc("hold%d" % i, [128, 8, 128], F32) for i in range(2)]
              pre = alloc("pre", [128, 8, 128], F32)
              prebf = alloc("prebf", [128, 8, 128], BF16)
              presq = alloc("presq", [128, 8, 128], BF16)
              t32 = alloc("t32", [128, 8, 128], F32)
              tmpo = [alloc("tmpo%d" % i, [128, 128], F32) for i in range(2)]
              mean_sb = alloc("mean_sb", [128, 128], F32)
              rstd_sb = alloc("rstd_sb", [128, 128], F32)
              lsb = alloc("lsb", [128, 18, 20], F32, at=hold0_at)
              rw = alloc("rw", [128, 18 * 96], F32, at=hold0_at + 1472)
              assert hold0_at + 1472 + 18 * 96 * 4 <= cur[0]
              R_wo = Res("wo")
              R_hold = [Res("hold0"), Res("hold1")]
              R_pre, R_t32 = Res("pre"), Res("t32")
              R_tmpo = [Res("tmpo0"), Res("tmpo1")]
              R_lt = [Res("prebf"), Res("presq"), Res("mean"), Res("rstd")]
              R_rout = Res("rout")
              S.add("pool", (lambda e, w_out_d=w_out_d: e.dma_start(out=wo[:], in_=w_out_d.rearrange("(kc p) n -> p kc n", p=128))), writes=[R_wo], dma=True)
              tiles4 = [t for (t0_, n_) in tok_blocks for t in range(t0_, t0_ + n_, 128)]
              for bi, t0 in enumerate(tiles4):
                  s = bi % 2
                  mc = mcol(t0)
                  tt = t0 // 128
                  S.add("sp", (lambda e, s=s, t0=t0: e.dma_start(out=hold[s][:], in_=Hs3[:, :, t0:t0 + 128])), reads=rtok("Hs", t0, t0 + 128), writes=[R_hold[s]], dma=True)
                  for oc in range(8):
                      bk = oc // 4
                      co = (oc % 4) * 128
                      for kc in range(8):
                          S.add("pe", (lambda e, kc=kc, oc=oc, bk=bk, co=co, t0=t0: e.matmul(bank(bk)[:, co:co + 128], lhsT=wo[:, kc, oc * 128:(oc + 1) * 128], rhs=oT[:, kc, t0:t0 + 128],
                                                                                           start=(kc == 0), stop=(kc == 7))),
                                reads=[R_wo] + rtok("oT", t0, t0 + 128), writes=[RB[bk]])
                  for oc in range(8):
                      bk = oc // 4
                      co = (oc % 4) * 128
                      ts = oc % 2
                      if l == 0:
                          vb = V("M2B", oc, t0)
                          S.add("act", (lambda e, oc=oc, bk=bk, co=co, ts=ts, mc=mc, vb=vb: e.activation(out=tmpo[ts][:], in_=bank(bk)[:, co:co + 128], func=AF.Identity,
                                                                                                   bias=vb, scale=modc(0, 2, oc, mc))),
                                reads=[RB[bk], R_mod, R_vecs], writes=[R_tmpo[ts]])
                      else:
                          S.add("act", (lambda e, oc=oc, bk=bk, co=co, ts=ts, mc=mc: e.activation(out=tmpo[ts][:], in_=bank(bk)[:, co:co + 128], func=AF.Identity,
                                                                                            scale=modc(1, 2, oc, mc))),
                                reads=[RB[bk], R_mod], writes=[R_tmpo[ts]])
                      S.add("dve", (lambda e, oc=oc, s=s, ts=ts: e.scalar_tensor_tensor(out=pre[:, oc, :], in0=hold[s][:, oc, :], scalar=ALPHA, in1=tmpo[ts][:], op0=ALU.mult, op1=ALU.add)),
                            reads=[R_hold[s], R_tmpo[ts]], writes=[R_pre])
                  ln_stats(pre[:], 8, 128, ones1k, prebf, presq, mean_sb, rstd_sb, 4, [R_pre], R_lt)
                  normalize(pre[:], 8, 128, mean_sb, rstd_sb, [R_pre], R_lt)
                  for ch in range(8):
                      affine(UT[:, ch, t0:t0 + 128], pre[:, ch, :], V("G4", ch, t0), V("B4", ch, t0), [R_pre, R_vecs], rtok("UT", t0, t0 + 128))
                      affine(t32[:, ch, :], pre[:, ch, :], V("G4", ch, t0), V("B4", ch, t0), [R_pre, R_vecs], [R_t32])
                      affine(hres[:, ch, t0:t0 + 128], pre[:, ch, :], V("GA1", ch, t0), V("BA1", ch, t0), [R_pre, R_vecs], rtok("hres%d" % ch, t0, t0 + 128))
                  for kc in range(8):
                      S.add("pe", (lambda e, kc=kc, tt=tt, l=l: e.matmul(bank(5)[:, tt * 20:tt * 20 + 20], lhsT=t32[:, kc, :], rhs=wrt[:, l * 160 + kc * 20:l * 160 + kc * 20 + 20],
                                                                        start=(kc == 0), stop=(kc == 7))),
                            reads=[R_t32, R_const], writes=[RB[5]])

              dump("S4", hres[:])
              dump("S4u", UT[:])
              S.barrier()
              T0 = tok_blocks[0][0] // 128
              T1 = 18
              nT = T1 - T0
              S.add("dve", lambda e: e.tensor_copy(out=lsb[:, T0:T1, :], in_=bank(5)[:, T0 * 20:T1 * 20].rearrange("p (t n) -> p t n", n=20)), reads=[RB[5]], writes=[R_rout])

              def rwv(i, n):
                  return rw[:, i * 18 * 4:(i * 18 * 4) + 18 * n].rearrange("p (t n) -> p t n", n=n)[:, T0:T1, :]

              def rop(fn):
                  S.add("dve", fn, reads=[R_rout], writes=[R_rout])

              lg = lsb[:, T0:T1, 0:4]
              le = lsb[:, T0:T1, 4:20].rearrange("p t (g x) -> p t g x", g=4)
              gmax, gsum, gp, m1, m2, dd, w1, w2 = [rwv(i, 1) for i in range(8)]
              gsh, gmask, elsel, mask1, el2, mask2, within, wa_ = [rwv(8 + i, 4) for i in range(8)]
              t44 = rw[:, 18 * 64:18 * 80].rearrange("p (t g x) -> p t g x", g=4, x=4)[:, T0:T1]
              gates = rw[:, 18 * 80:18 * 96].rearrange("p (t g x) -> p t g x", g=4, x=4)
              bc4 = lambda a: a.broadcast_to([128, nT, 4])
              rop(lambda e: e.tensor_reduce(out=gmax, in_=lg, axis=AX.X, op=ALU.max))
              rop(lambda e: e.tensor_tensor(out=gsh, in0=lg, in1=bc4(gmax), op=ALU.subtract))
              rop(lambda e: e.tensor_tensor(out=gmask, in0=lg, in1=bc4(gmax), op=ALU.is_equal))
              S.add("act", lambda e: e.activation(out=gsh, in_=gsh, func=AF.Exp), reads=[R_rout], writes=[R_rout])
              rop(lambda e: e.tensor_reduce(out=gsum, in_=gsh, axis=AX.X, op=ALU.add))
              rop(lambda e: e.reciprocal(out=gp, in_=gsum))
              rop(lambda e: e.tensor_tensor(out=t44, in0=le, in1=gmask.unsqueeze(3).broadcast_to([128, nT, 4, 4]), op=ALU.mult))
              rop(lambda e: e.tensor_reduce(out=elsel, in_=t44.rearrange("p t g x -> p t x g"), axis=AX.X, op=ALU.add))
              rop(lambda e: e.tensor_reduce(out=m1, in_=elsel, axis=AX.X, op=ALU.max))
              rop(lambda e: e.tensor_tensor(out=mask1, in0=elsel, in1=bc4(m1), op=ALU.is_equal))
              rop(lambda e: e.scalar_tensor_tensor(out=el2, in0=mask1, scalar=NEG, in1=elsel, op0=ALU.mult, op1=ALU.add))
              rop(lambda e: e.tensor_reduce(out=m2, in_=el2, axis=AX.X, op=ALU.max))
              rop(lambda e: e.tensor_tensor(out=mask2, in0=el2, in1=bc4(m2), op=ALU.is_equal))
              rop(lambda e: e.tensor_tensor(out=dd, in0=m2, in1=m1, op=ALU.subtract))
              S.add("act", lambda e: e.activation(out=dd, in_=dd, func=AF.Exp), reads=[R_rout], writes=[R_rout])
              rop(lambda e: e.tensor_scalar_add(out=w1, in0=dd, scalar1=1.0))
              rop(lambda e: e.reciprocal(out=w1, in_=w1))
              rop(lambda e: e.tensor_tensor(out=w1, in0=w1, in1=gp, op=ALU.mult))
              rop(lambda e: e.tensor_tensor(out=w2, in0=dd, in1=w1, op=ALU.mult))
              rop(lambda e: e.tensor_tensor(out=within, in0=mask1, in1=bc4(w1), op=ALU.mult))
              rop(lambda e: e.tensor_tensor(out=wa_, in0=mask2, in1=bc4(w2), op=ALU.mult))
              rop(lambda e: e.tensor_tensor(out=within, in0=within, in1=wa_, op=ALU.add))
              rop(lambda e: e.tensor_tensor(out=gates[:, T0:T1], in0=gmask.unsqueeze(3).broadcast_to([128, nT, 4, 4]), in1=within.unsqueeze(2).broadcast_to([128, nT, 4, 4]), op=ALU.mult))
              for tt in range(T0, T1):
                  bk = 6 + (tt // 4) % 2
                  S.add("pe", (lambda e, tt=tt, bk=bk: e.transpose(bank(bk)[0:16, (tt % 4) * 128:(tt % 4 + 1) * 128], gates[:, tt].rearrange("p g x -> p (g x)"), id32[:])),
                        reads=[R_rout, R_const], writes=[RB[bk]])
                  if tt % 4 == 3 or tt == T1 - 1:
                      ta = (tt // 4) * 4
                      ta0 = max(ta, T0)
                      S.add("act", (lambda e, bk=bk, ta=ta, ta0=ta0, tt=tt: e.copy(out=gatesT[0:16, ta0 * 128:(tt + 1) * 128], in_=bank(bk)[0:16, (ta0 - ta) * 128:(tt + 1 - ta) * 128])),
                            reads=[RB[bk]], writes=[R_rout])

              S.barrier()
              cur[0] = C0
              gatesT = alloc("gatesT", [16, NT], F32)
              wgs = [alloc("wgs%d" % i, [128, 8, 256], BF16) for i in range(2)]
              wus = [alloc("wus%d" % i, [128, 8, 256], BF16) for i in range(2)]
              wds = [alloc("wds%d" % i, [128, 2, D], BF16) for i in range(2)]
              sgm0 = alloc("sgm0", [128, 2, 512], F32)
              sgm = [sgm0, sgm0]
              tmpy = [alloc("tmpy%d" % i, [128, 512], F32) for i in range(2)]
              R_tmpy = [Res("tmpy0"), Res("tmpy1")]
              hgm = [alloc("hgm%d" % i, [128, 2, 512], BF16) for i in range(2)]
              R_ewg = [Res("ewg0"), Res("ewg1")]
              R_ewu = [Res("ewu0"), Res("ewu1")]
              R_ewd = [Res("ewd0"), Res("ewd1")]

              def load_expert(ex, l=l):
                  s = ex % 2
                  S.add("pool", (lambda e: e.dma_start(out=wgs[s][:], in_=ewg[l, ex].rearrange("(kc p) f -> p kc f", p=128))), writes=[R_ewg[s]], dma=True)
                  S.add("pool", (lambda e: e.dma_start(out=wus[s][:], in_=ewu[l, ex].rearrange("(kc p) f -> p kc f", p=128))), writes=[R_ewu[s]], dma=True)
                  S.add("pool", (lambda e: e.dma_start(out=wds[s][:], in_=ewd[l, ex].rearrange("(kc p) f -> p kc f", p=128))), writes=[R_ewd[s]], dma=True)

              load_expert(0)
              R_sgm = [Res("sgm0")] * 2
              R_hgm = [Res("hgm0"), Res("hgm1")]
              mblocks = blocks512 if l == 0 else lat512
              it = 0
              ybc = [0]
              pend = [None]
              for ex in range(16):
                  s = ex % 2
                  for (t0, N) in mblocks:
                      q = it % 2
                      it += 1
                      mc = mcol(t0)
                      S.add("pe", (lambda e, ex=ex, t0=t0, N=N: e.matmul(bank(4)[:, 0:N], lhsT=selt[0:16, ex * 128:(ex + 1) * 128], rhs=gatesT[0:16, t0:t0 + N], start=True, stop=True)),
                            reads=[R_rout, R_const], writes=[RB[4]])
                      for oc in range(4):
                          wsrc = wgs[s] if oc < 2 else wus[s]
                          for kc in range(8):
                              S.add("pe", (lambda e, kc=kc, oc=oc, wsrc=wsrc, t0=t0, N=N: e.matmul(bank(oc)[:, 0:N], lhsT=wsrc[:, kc, (oc % 2) * 128:(oc % 2 + 1) * 128], rhs=UT[:, kc, t0:t0 + N],
                                                                                              start=(kc == 0), stop=(kc == 7))),
                                    reads=[R_ewg[s], R_ewu[s]] + rtok("UT", t0, t0 + N), writes=[RB[oc]])
                      S.add("act", (lambda e, q=q, N=N: e.activation(out=sgm[q][:, :, 0:N], in_=PS[0][:].rearrange("p (j n) -> p j n", j=2)[:, :, 0:N], func=AF.Silu)),
                            reads=[RB[0], RB[1]], writes=[R_sgm[q]])
                      S.add("dve", (lambda e, q=q, N=N: e.tensor_tensor(out=sgm[q][:, :, 0:N], in0=sgm[q][:, :, 0:N], in1=PS[1][:].rearrange("p (j n) -> p j n", j=2)[:, :, 0:N], op=ALU.mult)),
                            reads=[RB[2], RB[3], R_sgm[q]], writes=[R_sgm[q]])
                      S.add("dve", (lambda e, q=q, N=N: e.tensor_tensor(out=hgm[q][:, :, 0:N], in0=sgm[q][:, :, 0:N], in1=bank(4)[:, 0:N].unsqueeze(1).broadcast_to([128, 2, N]), op=ALU.mult)),
                            reads=[RB[4], R_sgm[q]], writes=[R_hgm[q]])
                      def emit_y(s=s, q=q, t0=t0, N=N, mc=mc, l=l):
                          for dc in range(8):
                              bk = 5 + ybc[0] % 3
                              ybc[0] += 1
                              for k2 in range(2):
                                  S.add("pe", (lambda e, k2=k2, dc=dc, bk=bk, s=s, q=q, N=N: e.matmul(bank(bk)[:, 0:N], lhsT=wds[s][:, k2, dc * 128:(dc + 1) * 128], rhs=hgm[q][:, k2, 0:N],
                                                                                                 start=(k2 == 0), stop=(k2 == 1))),
                                        reads=[R_ewd[s], R_hgm[q]], writes=[RB[bk]])
                              ty = ybc[0] % 2
                              if dc < 4:
                                  S.add("act", (lambda e, dc=dc, bk=bk, N=N, mc=mc, l=l, ty=ty: e.activation(out=tmpy[ty][:, 0:N], in_=bank(bk)[:, 0:N], func=AF.Identity, scale=modc(l, 5, dc, mc))),
                                        reads=[RB[bk], R_mod], writes=[R_tmpy[ty]])
                                  S.add("pool", (lambda e, dc=dc, t0=t0, N=N, ty=ty: e.tensor_tensor(out=hres[:, dc, t0:t0 + N], in0=hres[:, dc, t0:t0 + N], in1=tmpy[ty][:, 0:N], op=ALU.add)),
                                        reads=[R_tmpy[ty]] + rtok("hres%d" % dc, t0, t0 + N), writes=rtok("hres%d" % dc, t0, t0 + N))
                              else:
                                  S.add("dve", (lambda e, dc=dc, bk=bk, t0=t0, N=N, mc=mc, l=l: e.scalar_tensor_tensor(out=hres[:, dc, t0:t0 + N], in0=bank(bk)[:, 0:N], scalar=modc(l, 5, dc, mc),
                                                                                                                  in1=hres[:, dc, t0:t0 + N], op0=ALU.mult, op1=ALU.add)),
                                        reads=[RB[bk], R_mod] + rtok("hres%d" % dc, t0, t0 + N), writes=rtok("hres%d" % dc, t0, t0 + N))
                      if pend[0] is not None:
                          pend[0]()
                      pend[0] = emit_y
                      if t0 == mblocks[0][0] and ex + 1 < 16:
                          load_expert(ex + 1)
              if pend[0] is not None:
                  pend[0]()
                  pend[0] = None

              dump("S5", hres[:])
              S.barrier()
              cur[0] = C0
              pre2 = alloc("pre2", [128, 8, 256], BF16)
              presq2 = alloc("presq2", [128, 8, 256], BF16)
              mean2 = alloc("mean2", [128, 256], F32)
              rstd2 = alloc("rstd2", [128, 256], F32)
              otile = [alloc("otile%d" % i, [128, D], F32) for i in range(2)]
              R_l2 = [Res("pre2"), Res("presq2"), Res("mean2"), Res("rstd2")]
              R_ot = [Res("ot0"), Res("ot1")]
              oi = 0
              for bi, (t0, N) in enumerate(tok_blocks):
                  hap = hres[:, :, t0:t0 + 256]
                  rh = [r_ for ch_ in range(8) for r_ in rtok("hres%d" % ch_, t0, t0 + 256)]
                  ln_stats(hap, 8, 256, ones1k, pre2, presq2, mean2, rstd2, 4, rh, R_l2)
                  normalize(hap, 8, 256, mean2, rstd2, rh, R_l2)
                  if l == 0:
                      for ch in range(8):
                          affine(UT[:, ch, t0:t0 + 256], hres[:, ch, t0:t0 + 256], V("GU", ch, t0), V("BU", ch, t0), rh + [R_vecs], rtok("UT", t0, t0 + 256))
                      for ch in range(8):
                          affine(hres[:, ch, t0:t0 + 256], hres[:, ch, t0:t0 + 256], smc("ln_g", 8 + ch), smc("ln_b", 8 + ch), rh + [R_const], rh)
                      S.add("sp", (lambda e, t0=t0: e.dma_start(out=Hs3[:, :, t0:t0 + 256], in_=hres[:, :, t0:t0 + 256])), reads=rh, writes=rtok("Hs", t0, t0 + 256), dma=True)
                      if debug and nlayers == 1:
                          S.add("sp", (lambda e, t0=t0: e.dma_start(out=dbg.rearrange("p (c t) -> p c t", c=8)[:, :, t0:t0 + 256], in_=hres[:, :, t0:t0 + 256])), reads=rh,
                                writes=[Res("dbgo")], dma=True)
                  else:
                      for ch in range(8):
                          affine(hres[:, ch, t0:t0 + 256], hres[:, ch, t0:t0 + 256], smc("ln_g", 24 + ch), smc("ln_b", 24 + ch), rh + [R_const], rh)
                      for hh in range(2):
                          tk = t0 + hh * 128
                          so = oi % 2
                          oi += 1
                          for ch in range(8):
                              bk = so * 2 + ch // 4
                              S.add("pe", (lambda e, ch=ch, bk=bk, tk=tk: e.transpose(bank(bk)[:, (ch % 4) * 128:(ch % 4 + 1) * 128], hres[:, ch, tk:tk + 128], id32[:])),
                                    reads=rh + [R_const], writes=[RB[bk]])
                          for hf in range(2):
                              bk = so * 2 + hf
                              S.add("act" if hf else "dve", (lambda e, so=so, hf=hf, bk=bk: (e.copy if hf else e.tensor_copy)(out=otile[so][:, hf * 512:(hf + 1) * 512], in_=bank(bk))),
                                    reads=[RB[bk]], writes=[R_ot[so]])
                          S.add("sp", (lambda e, so=so, tk=tk, b=b: e.dma_start(out=outd[b, tk - NCTX:tk - NCTX + 128, :], in_=otile[so][:])), reads=[R_ot[so]], writes=[Res("outw")], dma=True)
    except _Stop:
        pass
    S.barrier()

    with nc.Block() as block:
        @block.tensor
        def _(e):
            S.emit_one("pe", e, esem, dsems)

        @block.scalar
        def _(e):
            S.emit_one("act", e, esem, dsems)

        @block.vector
        def _(e):
            S.emit_one("dve", e, esem, dsems)

        @block.gpsimd
        def _(e):
            S.emit_one("pool", e, esem, dsems)

        @block.sync
        def _(e):
            S.emit_one("sp", e, esem, dsems)
    es.close()
    return nc


def _prep_shared(inp):
    f = lambda a: np.ascontiguousarray(np.asarray(a, np.float32))
    sm = np.zeros((128, SMN), np.float32)

    def put(name, arr):
        arr = np.asarray(arr, np.float32)
        sm[:, SMO[name]:SMO[name] + arr.shape[1]] = arr

    put("ada_b0", _fm(inp["ada_b"][0]))
    put("ada_b1", _fm(inp["ada_b"][1]))
    put("ln_g", np.concatenate([_fm(inp["ln_g"][l, k]) for l in range(2) for k in range(2)], axis=1))
    put("ln_b", np.concatenate([_fm(inp["ln_b"][l, k]) for l in range(2) for k in range(2)], axis=1))
    b_in = np.asarray(inp["ab_b_in"][0], np.float32)
    put("b_in", _fm(b_in[:2048]))
    cw = np.asarray(inp["conv_w"][0], np.float32)
    put("conv_w", np.ascontiguousarray(cw.T.reshape(4, 128, 31).transpose(1, 0, 2).reshape(128, 124)))
    put("conv_b", _fm(inp["conv_b"][0]))
    put("cln_g", _fm(inp["conv_ln_g"][0]))
    put("cln_b", _fm(inp["conv_ln_b"][0]))
    put("b_out", _fm(inp["ab_b_out"][0]))
    sm[:, SMO["eps"]] = EPS
    qidx = _gqa_qidx()
    gw = np.asarray(inp["gqa_w_in"][0], np.float32)
    wq = gw[:, :1024]
    wkk = gw[:, 1024:1280]
    wvv = gw[:, 1280:1536]
    C, Sg = _rope_tables()
    kk = np.arange(128)[:, None]
    qq = np.arange(128)[None, :]
    maskL = np.where(kk >= qq, 0.0, NEG).astype(np.float32)
    maskU = np.where(kk <= qq, 0.0, NEG).astype(np.float32)
    sink = np.asarray(inp["gqa_sink"][0], np.float32)
    sperm = np.array([8 * m + 4 * sh + j for m in range(2) for sh in range(2) for j in range(4)])
    sel = np.zeros((16, 16, 128), np.float32)
    for ex in range(16):
        sel[ex, ex, :] = 1.0
    wr = np.stack([np.concatenate([np.asarray(inp["router_group"][l], np.float32), np.asarray(inp["router_expert"][l], np.float32)], axis=1)
                   .reshape(8, 128, 20).transpose(1, 0, 2).reshape(128, 160) for l in range(2)])
    bv = b_in[2048:2560]
    shared = {
        "ada_w": f(inp["ada_w"]),
        "sm": sm,
        "w_in0": f(inp["ab_w_in"][0]),
        "bvbc": np.ascontiguousarray(np.broadcast_to(bv[None, :], (128, 512))),
        "nab": np.ascontiguousarray(_na_bias_table(np.asarray(inp["na_rpb"][0], np.float32)).reshape(8, 128, 21 * 128)),
        "w_out0": f(inp["ab_w_out"][0]),
        "wq1": f(wq[:, qidx]),
        "wqs1": f(wq[:, qidx][:, _swap64(1024)]),
        "wk1": f(wkk),
        "wks1": f(wkk[:, _swap64(256)]),
        "wv1": f(wvv),
        "w_out1": f(np.asarray(inp["gqa_w_out"][0], np.float32)[qidx, :]),
        "ropeC": C,
        "ropeS": Sg,
        "maskLU": np.ascontiguousarray(np.concatenate([maskL, maskU], axis=1)),
        "sinkbc": np.ascontiguousarray(np.broadcast_to(sink[sperm][None, :], (128, 16))),
        "wr": f(wr),
        "sel": np.ascontiguousarray(sel.reshape(16, 2048)),
        "ident": np.eye(128, dtype=np.float32),
        "ewg": f(inp["exp_w_gate"]),
        "ewu": f(inp["exp_w_up"]),
        "ewd": f(inp["exp_w_down"]),
    }
    return shared


def _core_inputs(inp, shared, i):
    x = np.asarray(inp["x"], np.float32)
    ctx = np.asarray(inp["ctx"], np.float32)
    c = np.asarray(inp["c"], np.float32)
    cc = np.stack([c[2 * i], c[2 * i + 1], np.asarray(inp["c_ctx"], np.float32)])
    cvec = np.ascontiguousarray(cc.reshape(3, 8, 128).transpose(2, 1, 0).reshape(128, 24))
    m = dict(shared)
    m["x2"] = np.ascontiguousarray(x[2 * i:2 * i + 2])
    m["ctx2"] = np.ascontiguousarray(ctx[2 * i:2 * i + 2])
    m["cvec"] = cvec
    return m


_NC_CACHE = {}


def kernel(**inputs):
    n = 8
    if "nc" not in _NC_CACHE:
        _NC_CACHE["nc"] = build()
    nc = _NC_CACHE["nc"]
    shared = _prep_shared(inputs)
    in_maps = [_core_inputs(inputs, shared, i) for i in range(n)]
    res = run_bass_kernel_spmd(nc, in_maps, core_ids=list(range(n)))
    out = np.concatenate([np.asarray(r["out"], np.float32) for r in res.results], axis=0)
    return out
```

```python
import numpy as np
from contextlib import ExitStack
import concourse.bass as bass
import concourse.mybir as mybir
from concourse.bass_utils import run_bass_kernel_spmd

F32 = mybir.dt.float32
BF16 = mybir.dt.bfloat16
AF = mybir.ActivationFunctionType
ALU = mybir.AluOpType
AX = mybir.AxisListType

D = 1024
SEQ = 2048
NCTX = 256
NT = SEQ + NCTX
GW = 64
ALPHA = 4.0 ** 0.25
EPS = 1e-5
NEG = -1e30

ENGS = ("pe", "act", "dve", "pool", "sp")
N_DMA_SEMS = 40


class Res:
    __slots__ = ("name", "last_w", "readers")

    def __init__(self, name):
        self.name = name
        self.last_w = None
        self.readers = []


class Op:
    __slots__ = ("eng", "fn", "idx", "deps", "dma", "sig", "semval", "dsem", "dval", "dprev")

    def __init__(self, eng, fn, idx, dma):
        self.eng = eng
        self.fn = fn
        self.idx = idx
        self.deps = []
        self.dma = dma
        self.sig = False
        self.semval = 0
        self.dsem = -1
        self.dval = 0
        self.dprev = 0


class Sched:
    def __init__(self):
        self.ops = {e: [] for e in ENGS}
        self.ndma = 0
        self.dma_tot = [0] * N_DMA_SEMS
        self.last_dma = [None] * N_DMA_SEMS
        self._assigned = False

    def add(self, eng, fn, reads=(), writes=(), dma=False, extra=()):
        lst = self.ops[eng]
        op = Op(eng, fn, len(lst), dma)
        deps = {}
        for r in reads:
            if r.last_w is not None:
                deps[id(r.last_w)] = r.last_w
        for w in writes:
            if w.last_w is not None:
                deps[id(w.last_w)] = w.last_w
            for rd in w.readers:
                deps[id(rd)] = rd
        for x in extra:
            deps[id(x)] = x
        for r in reads:
            r.readers.append(op)
        for w in writes:
            w.last_w = op
            w.readers = []
        if dma:
            s = self.ndma % N_DMA_SEMS
            self.ndma += 1
            op.dsem = s
            op.dprev = self.dma_tot[s]
            self.dma_tot[s] += 16
            op.dval = self.dma_tot[s]
            self.last_dma[s] = op
        for d in deps.values():
            if d is op:
                continue
            if d.eng == eng and not d.dma and not dma:
                if eng == "pe":
                    continue
                if op.idx - d.idx > 2:
                    continue
            op.deps.append(d)
            if not d.dma:
                d.sig = True
        lst.append(op)
        return op

    def barrier(self):
        lasts = []
        for e in ENGS:
            for op in reversed(self.ops[e]):
                if not op.dma:
                    lasts.append(op)
                    break
        dl = [o for o in self.last_dma if o is not None]
        for e in ENGS:
            self.add(e, lambda eng: eng.nop(), extra=[o for o in lasts if o.eng != e] + dl)

    def emit_one(self, e, eng, esem, dsems):
        if not self._assigned:
            for ee in ENGS:
                c = 0
                for op in self.ops[ee]:
                    if op.sig and not op.dma:
                        c += 1
                        op.semval = c
            self._assigned = True
        seen = {}
        for op in self.ops[e]:
            need = {}
            for d in op.deps:
                if d.dma:
                    key = ("d", d.dsem)
                    val = d.dval
                else:
                    key = ("e", d.eng)
                    val = d.semval
                if val > need.get(key, 0):
                    need[key] = val
            if op.dma and op.dprev > 0:
                key = ("d", op.dsem)
                if op.dprev > need.get(key, 0):
                    need[key] = op.dprev
            for key, val in need.items():
                if seen.get(key, 0) >= val:
                    continue
                seen[key] = val
                sem = dsems[key[1]] if key[0] == "d" else esem[key[1]]
                eng.wait_ge(sem, val)
            ins = op.fn(eng)
            if op.dma:
                ins.then_inc(dsems[op.dsem], 16)
            elif op.sig:
                ins.then_inc(esem[e], 1)


def _sm_layout():
    off = {}
    n = 0
    for name, cols in (("ada_b0", 48), ("ada_b1", 48), ("ln_g", 32), ("ln_b", 32), ("b_in", 16),
                       ("conv_w", 124), ("conv_b", 4), ("cln_g", 4), ("cln_b", 4), ("b_out", 8), ("eps", 1)):
        off[name] = n
        n += cols
    return off, n


SMO, SMN = _sm_layout()


def _fm(v):
    v = np.asarray(v, np.float32)
    return np.ascontiguousarray(v.reshape(-1, 128).T)


def _gqa_qidx():
    idx = np.zeros(1024, np.int64)
    for c in range(8):
        m, j = divmod(c, 4)
        h0 = 8 * m + j
        h1 = 8 * m + 4 + j
        idx[c * 128:c * 128 + 64] = h0 * 64 + np.arange(64)
        idx[c * 128 + 64:c * 128 + 128] = h1 * 64 + np.arange(64)
    return idx


def _swap64(n):
    d = np.arange(n)
    dd = d % 64
    sw = np.where(dd % 32 < 16, dd + 16, dd - 16)
    return (d // 64) * 64 + sw


def _na_tiles(j):
    if j in (0, 1):
        return [0, 1, 2, 3]
    if j in (14, 15):
        return [12, 13, 14, 15]
    return [j - 2, j - 1, j, j + 1, j + 2]


def _na_tile_index(j):
    if j == 0:
        return 5
    if j == 1:
        return 9
    if j == 14:
        return 13
    if j == 15:
        return 17
    return 0


def _na_bias_table(rpb):
    rows = 32
    r = np.arange(rows)
    row_start = np.clip(r - 4, 0, rows - 8)
    jj = np.arange(GW)
    col_start = np.clip(jj - 8, 0, GW - 16)
    col_in = (jj[None, :] >= col_start[:, None]) & (jj[None, :] < col_start[:, None] + 16)
    col_off = np.clip(jj[None, :] - jj[:, None], -15, 15) + 15
    out = np.full((8, 21, 128, 128), NEG, np.float32)

    def tile(j, a):
        t = np.full((8, 128, 128), NEG, np.float32)
        for pk in range(2):
            rk = 2 * a + pk
            for pq in range(2):
                rq = 2 * j + pq
                if not (row_start[rq] <= rk < row_start[rq] + 8):
                    continue
                ro = rk - rq + 7
                blk = rpb[:, ro][:, col_off]
                blk = np.where(col_in[None], blk, np.float32(NEG))
                t[:, pk * 64:(pk + 1) * 64, pq * 64:(pq + 1) * 64] = blk.transpose(0, 2, 1)
        return t

    for i, a in enumerate(_na_tiles(5)):
        out[:, i] = tile(5, a)
    for j in (0, 1, 14, 15):
        base = _na_tile_index(j)
        for i, a in enumerate(_na_tiles(j)):
            out[:, base + i] = tile(j, a)
    return np.ascontiguousarray(out.transpose(0, 2, 1, 3))


def _rope_tables():
    t = np.arange(SEQ)
    row = (t // GW).astype(np.float32)
    col = (t % GW).astype(np.float32)
    inv = (np.float32(10000.0) ** (-np.arange(0, 32, 2, dtype=np.float32) / np.float32(32))).astype(np.float32)
    ang = np.concatenate([row[:, None] * inv, col[:, None] * inv], axis=-1).astype(np.float32)
    cos = np.cos(ang).astype(np.float32)
    sin = np.sin(ang).astype(np.float32)
    p = np.arange(128)
    d = p % 64
    ai = (d // 32) * 16 + d % 16
    sgn = np.where(d % 32 < 16, -1.0, 1.0).astype(np.float32)
    C = np.ascontiguousarray(cos[:, ai].T)
    S = np.ascontiguousarray((sin[:, ai] * sgn[None, :]).T)
    return C.astype(np.float32), S.astype(np.float32)


class _Stop(Exception):
    pass


def build(nlayers=2, nb=2, debug=False, stop=None):
    nc = bass.Bass("TRN2", target_bir_lowering=False)
    S = Sched()

    def din(name, shape):
        return nc.dram_tensor(name, list(shape), F32, kind="ExternalInput").ap()

    x2 = din("x2", [2, SEQ, D])
    ctx2 = din("ctx2", [2, NCTX, D])
    cvec = din("cvec", [128, 24])
    ada_w = din("ada_w", [2, D, 6 * D])
    smd = din("sm", [128, SMN])
    w_in0 = din("w_in0", [D, 2560])
    bvbc = din("bvbc", [128, 512])
    nab = din("nab", [8, 128, 21 * 128])
    w_out0 = din("w_out0", [D, D])
    wq1 = din("wq1", [D, 1024])
    wqs1 = din("wqs1", [D, 1024])
    wk1 = din("wk1", [D, 256])
    wks1 = din("wks1", [D, 256])
    wv1 = din("wv1", [D, 256])
    w_out1 = din("w_out1", [D, D])
    ropeC = din("ropeC", [128, SEQ])
    ropeS = din("ropeS", [128, SEQ])
    maskLU = din("maskLU", [128, 256])
    sinkbc = din("sinkbc", [128, 16])
    wr = din("wr", [2, 128, 160])
    sel = din("sel", [16, 2048])
    ident = din("ident", [128, 128])
    ewg = din("ewg", [2, 16, D, 256])
    ewu = din("ewu", [2, 16, D, 256])
    ewd = din("ewd", [2, 16, 256, D])
    outd = nc.dram_tensor("out", [2, SEQ, D], F32, kind="ExternalOutput").ap()
    Hs = nc.dram_tensor("Hs", [128, 8 * NT], F32, kind="Internal").ap()
    dbg = nc.dram_tensor("dbg", [128, 8 * NT], F32, kind="ExternalOutput").ap() if debug else None
    dbgb = nc.dram_tensor("dbgb", [128, 8 * NT], BF16, kind="ExternalOutput").ap() if debug else None
    Hs3 = Hs.rearrange("p (c t) -> p c t", c=8)

    es = ExitStack()
    cur = [16640]

    acache = {}

    def alloc(name, shape, dt, at=None):
        nbytes = int(np.prod(shape[1:])) * (4 if dt == F32 else 2)
        if at is None:
            at = cur[0]
            cur[0] = (at + nbytes + 63) // 64 * 64
        assert at + nbytes <= 229376, (name, at, nbytes)
        key = (name, at, tuple(shape))
        if key not in acache:
            acache[key] = nc.alloc_sbuf_tensor_at("%s_%d" % (name, len(acache)), list(shape), dt, offset=at)
        return acache[key]

    sm = alloc("sm", [128, SMN], F32)
    id32 = alloc("id32", [128, 128], F32)
    ones1k = alloc("ones1k", [128, 128], BF16)
    ones512 = alloc("ones512", [128, 128], BF16)
    csil = alloc("csil", [128, 24], BF16)
    cv32 = alloc("cv32", [128, 24], F32)
    mod = alloc("mod", [128, 2 * 144], F32)
    mp1 = alloc("mp1", [128, 2 * 144], F32)
    vecs = alloc("vecs", [128, 128], F32)
    selt = alloc("selt", [16, 2048], F32)
    wrt = alloc("wrt", [128, 320], F32)
    esink = alloc("esink", [128, 16], F32)
    R_const = Res("const")
    R_mod = Res("mod")
    R_vecs = Res("vecs")
    base0 = cur[0]

    PS = [es.enter_context(nc.psum_tensor("ps%d" % i, [128, 1024], F32)) for i in range(4)]
    RB = [Res("bank%d" % i) for i in range(8)]

    def bank(k):
        return PS[k // 2][:, (k % 2) * 512:(k % 2) * 512 + 512]

    esem = {e: es.enter_context(nc.semaphore("es_" + e)) for e in ENGS}
    dsems = [es.enter_context(nc.semaphore("ds%d" % i)) for i in range(N_DMA_SEMS)]

    def smc(name, j, n=1):
        o = SMO[name] + j
        return sm[:, o:o + n]

    def modc(l, k, ch, col):
        o = l * 144 + (k * 8 + ch) * 3 + col
        return mod[:, o:o + 1]

    def mp1c(l, k, ch, col):
        o = l * 144 + (k * 8 + ch) * 3 + col
        return mp1[:, o:o + 1]

    VK = {}

    def vslot(kind, ch):
        key = (kind, ch)
        if key not in VK:
            VK[key] = len(VK)
            assert len(VK) <= 128
        o = VK[key]
        return vecs[:, o:o + 1]

    S.add("sp", lambda e: e.dma_start(out=sm[:], in_=smd), writes=[R_const], dma=True)
    S.add("sp", lambda e: e.dma_start(out=id32[:], in_=ident), writes=[R_const], dma=True)
    S.add("sp", lambda e: e.dma_start(out=cv32[:], in_=cvec), writes=[R_const], dma=True)
    S.add("sp", lambda e: e.dma_start(out=selt[:], in_=sel), writes=[R_const], dma=True)
    S.add("sp", lambda e: e.dma_start(out=wrt[:].rearrange("p (l n) -> p l n", l=2), in_=wr.rearrange("l p n -> p l n")), writes=[R_const], dma=True)
    S.add("sp", lambda e: e.dma_start(out=esink[:], in_=sinkbc), writes=[R_const], dma=True)
    S.add("pool", lambda e: e.memset(ones1k[:], 1.0 / 1024.0), writes=[R_const])
    S.add("pool", lambda e: e.memset(ones512[:], 1.0 / 512.0), writes=[R_const])
    S.add("act", lambda e: e.activation(out=csil[:], in_=cv32[:], func=AF.Silu), reads=[R_const], writes=[R_const])
    S.add("act", lambda e: e.activation(out=esink[:], in_=esink[:], func=AF.Exp), reads=[R_const], writes=[R_const])

    adaw = [alloc("adaw%d" % i, [128, 8, 1024], BF16) for i in range(2)]
    R_adaw = [Res("adaw0"), Res("adaw1")]
    pi = 0
    for l in range(nlayers):
        awl = ada_w[l].rearrange("(kc p) n -> p kc n", p=128)
        for piece in range(6):
            s = pi % 2
            pi += 1
            S.add("pool", (lambda e, s=s, awl=awl, piece=piece: e.dma_start(out=adaw[s][:], in_=awl[:, :, piece * 1024:(piece + 1) * 1024])),
                  writes=[R_adaw[s]], dma=True)
            for oc8 in range(8):
                oc = piece * 8 + oc8
                for kc in range(8):
                    S.add("pe", (lambda e, s=s, oc=oc, oc8=oc8, kc=kc: e.matmul(bank(0)[:, oc * 3:oc * 3 + 3], lhsT=adaw[s][:, kc, oc8 * 128:(oc8 + 1) * 128],
                                                                                    rhs=csil[:, kc * 3:kc * 3 + 3], start=(kc == 0), stop=(kc == 7))),
                          reads=[R_adaw[s], R_const], writes=[RB[0]])
        ab = smc("ada_b%d" % l, 0, 48)
        S.add("dve", (lambda e, l=l, ab=ab: e.tensor_tensor(out=mod[:, l * 144:(l + 1) * 144].rearrange("p (a b) -> p a b", b=3),
                                                             in0=bank(0)[:, 0:144].rearrange("p (a b) -> p a b", b=3),
                                                             in1=ab.unsqueeze(2).broadcast_to([128, 48, 3]), op=ALU.add)),
              reads=[RB[0], R_const], writes=[R_mod])
        S.add("dve", (lambda e, l=l: e.tensor_scalar_add(out=mp1[:, l * 144:(l + 1) * 144], in0=mod[:, l * 144:(l + 1) * 144], scalar1=1.0)),
              reads=[R_mod], writes=[R_mod])
    S.barrier()
    cur[0] = base0

    A0 = cur[0]
    hres = alloc("hres", [128, 8, NT], F32)
    qT = alloc("qT", [128, 4, NT], BF16, at=A0)
    kT = alloc("kT", [128, 4, NT], BF16, at=A0 + 18432)
    vaug = alloc("vaug", [128, 18, 4, 192], BF16, at=A0 + 36864)
    qT1 = alloc("qT1", [128, 8, SEQ], BF16, at=A0)
    kT1 = alloc("kT1", [128, 2, NT], BF16, at=A0 + 32768)
    vaug1 = alloc("vaug1", [128, 18, 2, 192], BF16, at=A0 + 41984)
    rC = alloc("rC", [128, SEQ], F32, at=A0 + 55808)
    rS = alloc("rS", [128, SEQ], F32, at=A0 + 55808 + 8192)
    bvb = alloc("bvb", [128, 512], F32, at=A0 + 64512)
    idb = alloc("idb", [128, 128], BF16, at=A0 + 64512 + 2048)
    cmean = alloc("cmean", [128, 256], F32, at=A0 + 64512 + 2304)
    crstd = alloc("crstd", [128, 256], F32, at=A0 + 64512 + 3328)
    UT = alloc("UT", [128, 8, NT], BF16)
    OT0 = cur[0]
    oT = alloc("oT", [128, 8, NT], BF16)
    C0 = cur[0]
    RT = {}

    def rtok(name, t0, t1):
        out = []
        for tt in range(t0 // 128, (t1 + 127) // 128):
            key = (name, tt)
            if key not in RT:
                RT[key] = Res("%s_%d" % key)
            out.append(RT[key])
        return out

    R_hpad = [Res("hpad%d" % c) for c in range(4)]

    def derive_vecs(l, col, tag):
        ops = []
        for ch in range(8):
            g1 = smc("ln_g", (l * 2 + 0) * 8 + ch)
            b1 = smc("ln_b", (l * 2 + 0) * 8 + ch)
            g2 = smc("ln_g", (l * 2 + 1) * 8 + ch)
            b2 = smc("ln_b", (l * 2 + 1) * 8 + ch)
            S.add("dve", (lambda e, ch=ch, g1=g1: e.tensor_tensor(out=vslot((tag, "G4"), ch), in0=g1, in1=mp1c(l, 4, ch, col), op=ALU.mult)),
                  reads=[R_const, R_mod], writes=[R_vecs])
            S.add("dve", (lambda e, ch=ch, b1=b1: e.scalar_tensor_tensor(out=vslot((tag, "B4"), ch), in0=b1, scalar=mp1c(l, 4, ch, col), in1=modc(l, 3, ch, col),
                                                                         op0=ALU.mult, op1=ALU.add)),
                  reads=[R_const, R_mod], writes=[R_vecs])
            S.add("dve", (lambda e, ch=ch, g1=g1: e.tensor_scalar_mul(out=vslot((tag, "GA1"), ch), in0=g1, scalar1=ALPHA)), reads=[R_const], writes=[R_vecs])
            S.add("dve", (lambda e, ch=ch, b1=b1: e.tensor_scalar_mul(out=vslot((tag, "BA1"), ch), in0=b1, scalar1=ALPHA)), reads=[R_const], writes=[R_vecs])
            if l == 0:
                S.add("dve", (lambda e, ch=ch: e.tensor_tensor(out=vslot((tag, "M2B"), ch), in0=modc(l, 2, ch, col), in1=smc("b_out", ch), op=ALU.mult)),
                      reads=[R_const, R_mod], writes=[R_vecs])
                S.add("dve", (lambda e, ch=ch, g2=g2: e.tensor_tensor(out=vslot((tag, "GU"), ch), in0=g2, in1=mp1c(1, 1, ch, col), op=ALU.mult)),
                      reads=[R_const, R_mod], writes=[R_vecs])
                S.add("dve", (lambda e, ch=ch, b2=b2: e.scalar_tensor_tensor(out=vslot((tag, "BU"), ch), in0=b2, scalar=mp1c(1, 1, ch, col), in1=modc(1, 0, ch, col),
                                                                             op0=ALU.mult, op1=ALU.add)),
                      reads=[R_const, R_mod], writes=[R_vecs])

    def ln_stats(pre_ap, nch, N, ones_t, prebf, presq, mean_sb, rstd_sb, bk, r_pre, r_tmp):
        S.add("dve", lambda e: e.tensor_copy(out=prebf[:, 0:nch, 0:N], in_=pre_ap), reads=r_pre, writes=[r_tmp[0]])
        S.add("act", lambda e: e.activation(out=presq[:, 0:nch, 0:N], in_=pre_ap, func=AF.Square), reads=r_pre, writes=[r_tmp[1]])
        for c in range(nch):
            S.add("pe", (lambda e, c=c: e.matmul(bank(bk)[:, 0:N], lhsT=ones_t[:], rhs=prebf[:, c, 0:N], start=(c == 0), stop=(c == nch - 1))),
                  reads=[r_tmp[0], R_const], writes=[RB[bk]])
        for c in range(nch):
            S.add("pe", (lambda e, c=c: e.matmul(bank(bk)[:, 256:256 + N], lhsT=ones_t[:], rhs=presq[:, c, 0:N], start=(c == 0), stop=(c == nch - 1))),
                  reads=[r_tmp[1], R_const], writes=[RB[bk]])
        S.add("act", lambda e: e.copy(out=mean_sb[:, 0:N], in_=bank(bk)[:, 0:N]), reads=[RB[bk]], writes=[r_tmp[2]])
        S.add("dve", lambda e: e.tensor_tensor(out=rstd_sb[:, 0:N], in0=mean_sb[:, 0:N], in1=mean_sb[:, 0:N], op=ALU.mult), reads=[r_tmp[2]], writes=[r_tmp[3]])
        S.add("dve", lambda e: e.tensor_tensor(out=rstd_sb[:, 0:N], in0=bank(bk)[:, 256:256 + N], in1=rstd_sb[:, 0:N], op=ALU.subtract),
              reads=[RB[bk], r_tmp[3]], writes=[r_tmp[3]])
        S.add("act", lambda e: e.activation(out=rstd_sb[:, 0:N], in_=rstd_sb[:, 0:N], func=AF.Sqrt, bias=smc("eps", 0), scale=1.0),
              reads=[r_tmp[3], R_const], writes=[r_tmp[3]])
        S.add("dve", lambda e: e.reciprocal(out=rstd_sb[:, 0:N], in_=rstd_sb[:, 0:N]), reads=[r_tmp[3]], writes=[r_tmp[3]])

    def normalize(pre_ap, nch, N, mean_sb, rstd_sb, r_pre, r_tmp):
        S.add("dve", lambda e: e.tensor_tensor(out=pre_ap, in0=pre_ap, in1=mean_sb[:, 0:N].unsqueeze(1).broadcast_to([128, nch, N]), op=ALU.subtract),
              reads=r_pre + [r_tmp[2]], writes=r_pre)
        S.add("dve", lambda e: e.tensor_tensor(out=pre_ap, in0=pre_ap, in1=rstd_sb[:, 0:N].unsqueeze(1).broadcast_to([128, nch, N]), op=ALU.mult),
              reads=r_pre + [r_tmp[3]], writes=r_pre)

    aff_rr = [0]

    def affine(out_ap, in_ap, sc, bi, reads, writes, psum_in=False):
        k = aff_rr[0] % 2
        aff_rr[0] += 1
        if k == 0:
            S.add("act", lambda e: e.activation(out=out_ap, in_=in_ap, func=AF.Identity, bias=bi, scale=sc), reads=reads, writes=writes)
        else:
            S.add("dve" if k == 1 else "pool", lambda e: e.tensor_scalar(out=out_ap, in0=in_ap, scalar1=sc, scalar2=bi, op0=ALU.mult, op1=ALU.add),
                  reads=reads, writes=writes)

    def dump(name, src_ap3):
        if stop != name and stop != "%s@%d" % (name, cur_l[0]):
            return
        S.barrier()
        c, t = src_ap3.shape[1], src_ap3.shape[2]
        dst = dbg if src_ap3.dtype == F32 else dbgb
        for ci in range(c):
            S.add("sp", (lambda e, ci=ci: e.dma_start(out=dst[:, ci * t:(ci + 1) * t], in_=src_ap3[:, ci, :])), writes=[Res("dbgo")], dma=True)
        raise _Stop()

    cur_l = [0]
    try:
      for b in range(nb):
        for l in range(nlayers):
              cur_l[0] = l
              lat_only = (l == nlayers - 1) and l == 1
              col = b
              S.barrier()
              derive_vecs(l, b, "lat")
              if l == 0:
                  derive_vecs(l, 2, "ctx")

              def V(kind, ch, t0):
                  return vslot((("ctx" if (t0 < NCTX and l == 0) else "lat"), kind), ch)

              def mcol(t0):
                  return 2 if t0 < NCTX else b

              cur[0] = C0
              if l == 0:
                  xin = [alloc("xin%d" % i, [128, D], F32) for i in range(2)]
                  hblk = [alloc("hblk%d" % i, [128, 8, 128], F32) for i in range(2)]
                  R_xin = [Res("xin0"), Res("xin1")]
                  R_hblk = [Res("hblk0"), Res("hblk1")]
                  for tt in range(18):
                      s = tt % 2
                      src = ctx2[b, tt * 128:(tt + 1) * 128, :] if tt < 2 else x2[b, (tt - 2) * 128:(tt - 1) * 128, :]
                      S.add("sp", (lambda e, s=s, src=src: e.dma_start(out=xin[s][:], in_=src)), writes=[R_xin[s]], dma=True)
                      for ch in range(8):
                          bk = (tt % 2) * 2 + ch // 4
                          S.add("pe", (lambda e, s=s, ch=ch, bk=bk: e.transpose(bank(bk)[:, (ch % 4) * 128:(ch % 4 + 1) * 128], xin[s][:, ch * 128:(ch + 1) * 128], id32[:])),
                                reads=[R_xin[s], R_const], writes=[RB[bk]])
                      for hf in range(2):
                          bk = (tt % 2) * 2 + hf
                          S.add("act", (lambda e, s=s, hf=hf, bk=bk: e.copy(out=hblk[s][:, hf * 4:(hf + 1) * 4, :], in_=bank(bk).rearrange("p (c t) -> p c t", c=4))),
                                reads=[RB[bk]], writes=[R_hblk[s]])
                      mc = mcol(tt * 128)
                      for ch in range(8):
                          bk = (tt % 2) * 2 + ch // 4
                          affine(UT[:, ch, tt * 128:(tt + 1) * 128], bank(bk)[:, (ch % 4) * 128:(ch % 4 + 1) * 128], mp1c(0, 1, ch, mc), modc(0, 0, ch, mc),
                                 [RB[bk], R_mod], rtok("UT", tt * 128, tt * 128 + 128), psum_in=True)
                      S.add("sp", (lambda e, s=s, tt=tt: e.dma_start(out=Hs3[:, :, tt * 128:(tt + 1) * 128], in_=hblk[s][:])), reads=[R_hblk[s]],
                            writes=rtok("Hs", tt * 128, tt * 128 + 128), dma=True)

              if l == 0:
                  dump("S0", UT[:])
              blocks512 = [(0, 256)] + [(256 + 512 * i, 512) for i in range(4)]
              blocks256 = [(256 * i, 256) for i in range(9)]
              if l == 1:
                  lat512 = [(256 + 512 * i, 512) for i in range(4)]
                  lat256 = [(256 * i, 256) for i in range(1, 9)]

              if l == 0:
                  S.barrier()
                  cur[0] = C0
                  wAB = alloc("wAB", [128, 8, 1536], BF16)
                  wA = alloc("wA", [128, 8, 1024], BF16, at=C0)
                  hpd = [alloc("hpd%d" % i, [128, 2368], BF16, at=C0 + 16384 + i * 4736) for i in range(2)]
                  dg0 = alloc("diag0", [128, 31, 128], BF16, at=C0 + 25856)
                  diag = [dg0, dg0]
                  cur[0] = C0 + 33792
                  sgt = [alloc("sgt%d" % i, [128, 512], F32) for i in range(2)]
                  czsq = alloc("czsq", [128, 4, 256], BF16)
                  cz = alloc("cz", [128, 4, 256], F32)
                  R_wAB, R_misc = Res("wAB"), Res("misc0")
                  R_hpd = [Res("hpd0"), Res("hpd1")]
                  R_dg = [Res("dg0")] * 2
                  R_sgt = [Res("sgt0"), Res("sgt1")]
                  R_cz, R_ct = Res("cz"), [Res("czbf"), Res("czsq"), Res("cmean"), Res("crstd")]
                  w0 = w_in0.rearrange("(kc p) n -> p kc n", p=128)
                  S.add("pool", lambda e: e.dma_start(out=wA[:], in_=w0[:, :, 0:1024]), writes=[R_wAB], dma=True)
                  S.add("pool", lambda e: e.dma_start(out=idb[:], in_=ident), writes=[R_misc], dma=True)
                  S.add("sp", lambda e: e.dma_start(out=bvb[:], in_=bvbc), writes=[R_misc], dma=True)
                  S.add("pool", lambda e: e.memset(hpd[0][:], 0.0), writes=[R_hpd[0]])
                  S.add("pool", lambda e: e.memset(hpd[1][:], 0.0), writes=[R_hpd[1]])
                  S.add("pool", lambda e: e.memset(vaug[:], 1.0), writes=rtok("vaug", 0, NT))

                  def hoff(t0):
                      return 15 + t0 if t0 < NCTX else 286 + 15 + (t0 - NCTX)

                  it = 0
                  for cc in range(4):
                      hs_ = cc % 2
                      for k in range(31):
                          S.add("dve", (lambda e, cc=cc, k=k, hs_=hs_: e.tensor_scalar_mul(out=diag[hs_][:, k, :], in0=idb[:], scalar1=smc("conv_w", cc * 31 + k))),
                                reads=[R_misc, R_const], writes=[R_dg[hs_]])
                      for (t0, N) in blocks512:
                          s = it % 2
                          it += 1
                          b1, b2 = 2 * s, 2 * s + 1
                          for kc in range(8):
                              S.add("pe", (lambda e, kc=kc, cc=cc, t0=t0, N=N, b1=b1: e.matmul(bank(b1)[:, 0:N], lhsT=wA[:, kc, cc * 128:(cc + 1) * 128], rhs=UT[:, kc, t0:t0 + N],
                                                                                             start=(kc == 0), stop=(kc == 7))),
                                    reads=[R_wAB] + rtok("UT", t0, t0 + N), writes=[RB[b1]])
                          for kc in range(8):
                              S.add("pe", (lambda e, kc=kc, cc=cc, t0=t0, N=N, b2=b2: e.matmul(bank(b2)[:, 0:N], lhsT=wA[:, kc, 512 + cc * 128:512 + (cc + 1) * 128], rhs=UT[:, kc, t0:t0 + N],
                                                                                             start=(kc == 0), stop=(kc == 7))),
                                    reads=[R_wAB] + rtok("UT", t0, t0 + N), writes=[RB[b2]])
                          S.add("act", (lambda e, s=s, cc=cc, N=N, b2=b2: e.activation(out=sgt[s][:, 0:N], in_=bank(b2)[:, 0:N], func=AF.Sigmoid, bias=smc("b_in", 4 + cc), scale=1.0)),
                                reads=[RB[b2], R_const], writes=[R_sgt[s]])
                          ho = hoff(t0)
                          S.add("dve", (lambda e, s=s, cc=cc, N=N, b1=b1, ho=ho, hs_=hs_: e.scalar_tensor_tensor(out=hpd[hs_][:, ho:ho + N], in0=bank(b1)[:, 0:N], scalar=smc("b_in", cc),
                                                                                                                 in1=sgt[s][:, 0:N], op0=ALU.add, op1=ALU.mult)),
                                reads=[RB[b1], R_sgt[s], R_const], writes=[R_hpd[hs_]])
                      for bi, (t0, N) in enumerate(blocks512):
                          ho = hoff(t0) - 15
                          bk = 4 + bi % 2
                          for k in range(31):
                              S.add("pe", (lambda e, k=k, ho=ho, N=N, bk=bk, hs_=hs_: e.matmul(bank(bk)[:, 0:N], lhsT=diag[hs_][:, k, :], rhs=hpd[hs_][:, ho + k:ho + k + N],
                                                                                           start=(k == 0), stop=(k == 30))),
                                    reads=[R_dg[hs_], R_hpd[hs_]], writes=[RB[bk]])
                          S.add("act", (lambda e, cc=cc, N=N, t0=t0, bk=bk: e.activation(out=oT[:, cc, t0:t0 + N], in_=bank(bk)[:, 0:N], func=AF.Identity, bias=smc("conv_b", cc), scale=1.0)),
                                reads=[RB[bk], R_const], writes=rtok("oT", t0, t0 + N))
                  dump("S1z", oT[:, 0:4, :])
                  dump("S1h", hpd[1][:].unsqueeze(1))
                  dump("S1d", diag[0][:])
                  S.add("pool", lambda e: e.dma_start(out=wAB[:], in_=w0[:, :, 1024:2560]), writes=[R_wAB] + R_hpd + [R_dg[0]], dma=True)
                  for (t0, N) in blocks256:
                      zin = oT[:, 0:4, t0:t0 + 256]
                      rz = rtok("oT", t0, t0 + 256)
                      S.add("act", (lambda e, zin=zin: e.activation(out=czsq[:], in_=zin, func=AF.Square)), reads=rz, writes=[R_ct[1]])
                      for c4 in range(4):
                          S.add("pe", (lambda e, c4=c4, t0=t0: e.matmul(bank(6)[:, 0:256], lhsT=ones512[:], rhs=oT[:, c4, t0:t0 + 256], start=(c4 == 0), stop=(c4 == 3))),
                                reads=rz + [R_const], writes=[RB[6]])
                      for c4 in range(4):
                          S.add("pe", (lambda e, c4=c4: e.matmul(bank(6)[:, 256:512], lhsT=ones512[:], rhs=czsq[:, c4, :], start=(c4 == 0), stop=(c4 == 3))),
                                reads=[R_ct[1], R_const], writes=[RB[6]])
                      S.add("act", lambda e: e.copy(out=cmean[:], in_=bank(6)[:, 0:256]), reads=[RB[6]], writes=[R_ct[2]])
                      S.add("dve", lambda e: e.tensor_tensor(out=crstd[:], in0=cmean[:], in1=cmean[:], op=ALU.mult), reads=[R_ct[2]], writes=[R_ct[3]])
                      S.add("dve", lambda e: e.tensor_tensor(out=crstd[:], in0=bank(6)[:, 256:512], in1=crstd[:], op=ALU.subtract), reads=[RB[6], R_ct[3]], writes=[R_ct[3]])
                      S.add("act", lambda e: e.activation(out=crstd[:], in_=crstd[:], func=AF.Sqrt, bias=smc("eps", 0), scale=1.0), reads=[R_ct[3], R_const], writes=[R_ct[3]])
                      S.add("dve", lambda e: e.reciprocal(out=crstd[:], in_=crstd[:]), reads=[R_ct[3]], writes=[R_ct[3]])
                      S.add("dve", (lambda e, zin=zin: e.tensor_tensor(out=cz[:], in0=zin, in1=cmean[:].unsqueeze(1).broadcast_to([128, 4, 256]), op=ALU.subtract)),
                            reads=rz + [R_ct[2]], writes=[R_cz])
                      S.add("dve", lambda e: e.tensor_tensor(out=cz[:], in0=cz[:], in1=crstd[:].unsqueeze(1).broadcast_to([128, 4, 256]), op=ALU.mult),
                            reads=[R_cz, R_ct[3]], writes=[R_cz])
                      for c4 in range(4):
                          S.add("act", (lambda e, c4=c4, t0=t0: e.activation(out=oT[:, c4, t0:t0 + 256], in_=cz[:, c4, :], func=AF.Silu, bias=smc("cln_b", c4), scale=smc("cln_g", c4))),
                                reads=[R_cz, R_const], writes=rz)
                  it = 0
                  for (t0, N) in blocks512:
                      for c in range(8):
                          bk = it % 4
                          it += 1
                          for kc in range(8):
                              S.add("pe", (lambda e, kc=kc, c=c, t0=t0, N=N, bk=bk: e.matmul(bank(bk)[:, 0:N], lhsT=wAB[:, kc, c * 128:(c + 1) * 128], rhs=UT[:, kc, t0:t0 + N],
                                                                                           start=(kc == 0), stop=(kc == 7))),
                                    reads=[R_wAB] + rtok("UT", t0, t0 + N), writes=[RB[bk]])
                          dst = qT if c < 4 else kT
                          S.add("act", (lambda e, c=c, t0=t0, N=N, bk=bk, dst=dst: e.activation(out=dst[:, c % 4, t0:t0 + N], in_=bank(bk)[:, 0:N], func=AF.Identity,
                                                                                              bias=smc("b_in", 8 + c), scale=1.0)),
                                reads=[RB[bk], R_const], writes=rtok("qk", t0, t0 + N))
                  for tt in range(18):
                      bk = it % 4
                      it += 1
                      for kc in range(8):
                          S.add("pe", (lambda e, kc=kc, tt=tt, bk=bk: e.matmul(bank(bk)[:, 0:512], lhsT=UT[:, kc, tt * 128:(tt + 1) * 128], rhs=wAB[:, kc, 1024:1536],
                                                                             start=(kc == 0), stop=(kc == 7))),
                                reads=[R_wAB] + rtok("UT", tt * 128, tt * 128 + 128), writes=[RB[bk]])
                      for x in range(2):
                          S.add("dve", (lambda e, tt=tt, bk=bk, x=x: e.tensor_tensor(out=vaug[:, tt, :, x * 128:x * 128 + 64],
                                                                                   in0=bank(bk).rearrange("p (c x d) -> p c x d", c=4, x=2)[:, :, x, :],
                                                                                   in1=bvb[:].rearrange("p (c x d) -> p c x d", c=4, x=2)[:, :, x, :], op=ALU.add)),
                                reads=[RB[bk], R_misc], writes=rtok("vaug", tt * 128, tt * 128 + 128))

                  dump("S1o", oT[:, 0:4, :])
                  dump("S1q", qT[:])
                  dump("S1k", kT[:])
                  dump("S1v", vaug[:].rearrange("p t c x -> p t (c x)"))
                  S.barrier()
                  cur[0] = C0
                  nabt = [alloc("nabt%d" % i, [128, 21, 128], F32) for i in range(2)]
                  sbt = [alloc("sbt%d" % i, [128, 640], F32) for i in range(2)]
                  PT = [alloc("PT%d" % i, [128, 896], BF16) for i in range(2)]
                  rec = [alloc("rec%d" % i, [128, 256], F32) for i in range(2)]
                  R_nabt = [Res("nabt0"), Res("nabt1")]
                  R_sbt = [Res("sbt0"), Res("sbt1")]
                  R_PT = [Res("PT0"), Res("PT1")]
                  R_rec = [Res("rec0"), Res("rec1")]
                  it = 0
                  for h in range(8):
                      c, sh = h // 2, h % 2
                      p0, p1 = sh * 64, sh * 64 + 64
                      nh0, nh1 = (0, 64) if sh == 0 else (64, 128)
                      dh0, dh1 = (64, 128) if sh == 0 else (0, 64)
                      hs = h % 2
                      S.add("sp", (lambda e, h=h, hs=hs: e.dma_start(out=nabt[hs][:], in_=nab[h].rearrange("p (a q) -> p a q", a=21))), writes=[R_nabt[hs]], dma=True)
                      vcol = sh * 64
                      s = it % 2
                      it += 1
                      sb0 = 2 * s
                      for i in range(2):
                          S.add("pe", (lambda e, i=i, c=c, p0=p0, p1=p1, sb0=sb0: e.matmul(bank(sb0)[:, i * 256:(i + 1) * 256], lhsT=kT[p0:p1, c, i * 128:(i + 1) * 128],
                                                                                          rhs=qT[p0:p1, c, 0:256], start=True, stop=True)),
                                reads=rtok("qk", 0, 256), writes=[RB[sb0]])
                      S.add("act", (lambda e, s=s, sb0=sb0: e.activation(out=PT[s][:, 0:512], in_=bank(sb0)[:, 0:512], func=AF.Exp, scale=0.125)), reads=[RB[sb0]], writes=[R_PT[s]])
                      ob = 4 + s
                      for i in range(2):
                          S.add("pe", (lambda e, i=i, c=c, s=s, ob=ob, vcol=vcol: e.matmul(bank(ob)[:, 0:256], lhsT=vaug[:, i, c, vcol:vcol + 128], rhs=PT[s][:, i * 256:(i + 1) * 256],
                                                                                          start=(i == 0), stop=(i == 1))),
                                reads=[R_PT[s]] + rtok("vaug", 0, 256), writes=[RB[ob]])
                      S.add("dve", (lambda e, s=s, ob=ob, dh0=dh0, dh1=dh1: e.reciprocal(out=rec[s][dh0:dh1, 0:256], in_=bank(ob)[dh0:dh1, 0:256])), reads=[RB[ob]], writes=[R_rec[s]])
                      S.add("dve", (lambda e, s=s, ob=ob, c=c, nh0=nh0, nh1=nh1, dh0=dh0, dh1=dh1: e.tensor_tensor(out=oT[nh0:nh1, 4 + c, 0:256], in0=bank(ob)[nh0:nh1, 0:256],
                                                                                                               in1=rec[s][dh0:dh1, 0:256], op=ALU.mult)),
                            reads=[RB[ob], R_rec[s]], writes=rtok("oT", 0, 256))
                      for j in range(16):
                          s = it % 2
                          it += 1
                          sb0 = 2 * s
                          tl = _na_tiles(j)
                          nl = len(tl)
                          ti0 = _na_tile_index(j)
                          q0 = NCTX + j * 128
                          ktoks = [NCTX + a * 128 for a in tl] + [0, 128]
                          for i, kt0 in enumerate(ktoks):
                              bk = sb0 + (i // 4)
                              S.add("pe", (lambda e, i=i, kt0=kt0, bk=bk, c=c, p0=p0, p1=p1, q0=q0: e.matmul(bank(bk)[:, (i % 4) * 128:(i % 4 + 1) * 128], lhsT=kT[p0:p1, c, kt0:kt0 + 128],
                                                                                                        rhs=qT[p0:p1, c, q0:q0 + 128], start=True, stop=True)),
                                    reads=rtok("qk", kt0, kt0 + 128) + rtok("qk", q0, q0 + 128), writes=[RB[bk]])
                          S.add("dve", (lambda e, s=s, nl=nl, ti0=ti0, hs=hs: e.scalar_tensor_tensor(out=sbt[s][:, 0:nl * 128], in0=PS[s][:, 0:nl * 128], scalar=0.125,
                                                                                                    in1=nabt[hs][:, ti0:ti0 + nl, :].rearrange("p a q -> p (a q)"),
                                                                                                    op0=ALU.mult, op1=ALU.add)),
                                reads=[RB[sb0], RB[sb0 + 1], R_nabt[hs]], writes=[R_sbt[s]])
                          S.add("act", (lambda e, s=s, nl=nl: e.activation(out=PT[s][:, 0:nl * 128], in_=sbt[s][:, 0:nl * 128], func=AF.Exp)), reads=[R_sbt[s]], writes=[R_PT[s]])
                          S.add("act", (lambda e, s=s, nl=nl: e.activation(out=PT[s][:, nl * 128:(nl + 2) * 128], in_=PS[s][:, nl * 128:(nl + 2) * 128], func=AF.Exp, scale=0.125)),
                                reads=[RB[sb0], RB[sb0 + 1]], writes=[R_PT[s]])
                          ob = 4 + s
                          for i, kt0 in enumerate(ktoks):
                              S.add("pe", (lambda e, i=i, kt0=kt0, c=c, s=s, ob=ob, vcol=vcol, nl=nl: e.matmul(bank(ob)[:, 0:128], lhsT=vaug[:, kt0 // 128, c, vcol:vcol + 128],
                                                                                                          rhs=PT[s][:, i * 128:(i + 1) * 128], start=(i == 0), stop=(i == nl + 1))),
                                    reads=[R_PT[s]] + rtok("vaug", kt0, kt0 + 128), writes=[RB[ob]])
                          S.add("dve", (lambda e, s=s, ob=ob, dh0=dh0, dh1=dh1: e.reciprocal(out=rec[s][dh0:dh1, 0:128], in_=bank(ob)[dh0:dh1, 0:128])), reads=[RB[ob]], writes=[R_rec[s]])
                          S.add("dve", (lambda e, s=s, ob=ob, c=c, q0=q0, nh0=nh0, nh1=nh1, dh0=dh0, dh1=dh1: e.tensor_tensor(out=oT[nh0:nh1, 4 + c, q0:q0 + 128], in0=bank(ob)[nh0:nh1, 0:128],
                                                                                                                     in1=rec[s][dh0:dh1, 0:128], op=ALU.mult)),
                                reads=[RB[ob], R_rec[s]], writes=rtok("oT", q0, q0 + 128))
                  dump("S3", oT[:, 4:8, :])
                  w_out_d = w_out0
                  tok_blocks = blocks256
              else:
                  S.barrier()
                  cur[0] = C0
                  wq = alloc("wq", [128, 8, 512], BF16)
                  wqs = alloc("wqs", [128, 8, 512], BF16)
                  wk = alloc("wk", [128, 8, 256], BF16)
                  wks = alloc("wks", [128, 8, 256], BF16)
                  wv = alloc("wv", [128, 8, 256], BF16)
                  rt = [alloc("rt%d" % i, [128, 2, 512], F32) for i in range(2)]
                  R_w1, R_rope = Res("w1"), Res("rope")
                  R_rt = [Res("rt0"), Res("rt1")]
                  R_wq = Res("wq")
                  for dst, src in ((wk, wk1), (wks, wks1), (wv, wv1)):
                      S.add("pool", (lambda e, dst=dst, src=src: e.dma_start(out=dst[:], in_=src.rearrange("(kc p) n -> p kc n", p=128))), writes=[R_w1], dma=True)
                  S.add("sp", lambda e: e.dma_start(out=rC[:], in_=ropeC), writes=[R_rope], dma=True)
                  S.add("sp", lambda e: e.dma_start(out=rS[:], in_=ropeS), writes=[R_rope], dma=True)
                  if True:
                      S.add("pool", lambda e: e.memset(vaug1[:], 1.0), writes=rtok("vaug", 0, NT))
                  it = 0
                  for half, (t0, N) in [(hf_, blk_) for hf_ in range(3) for blk_ in lat512]:
                      l0 = t0 - NCTX
                      if half < 2 and t0 == NCTX:
                          for dst, src in ((wq, wq1), (wqs, wqs1)):
                              S.add("pool", (lambda e, dst=dst, src=src, half=half: e.dma_start(out=dst[:], in_=src.rearrange("(kc p) n -> p kc n", p=128)[:, :, half * 512:(half + 1) * 512])),
                                    writes=[R_wq], dma=True)
                      for c in (range(half * 4, half * 4 + 4) if half < 2 else range(8, 10)):
                          s = it % 2
                          it += 1
                          b1, b2 = 2 * s, 2 * s + 1
                          wa, wb = (wq, wqs) if c < 8 else (wk, wks)
                          cc = (c % 4) if c < 8 else c - 8
                          for kc in range(8):
                              S.add("pe", (lambda e, kc=kc, cc=cc, wa=wa, t0=t0, N=N, b1=b1: e.matmul(bank(b1)[:, 0:N], lhsT=wa[:, kc, cc * 128:(cc + 1) * 128], rhs=UT[:, kc, t0:t0 + N],
                                                                                                 start=(kc == 0), stop=(kc == 7))),
                                    reads=[R_w1, R_wq] + rtok("UT", t0, t0 + N), writes=[RB[b1]])
                          for kc in range(8):
                              S.add("pe", (lambda e, kc=kc, cc=cc, wb=wb, t0=t0, N=N, b2=b2: e.matmul(bank(b2)[:, 0:N], lhsT=wb[:, kc, cc * 128:(cc + 1) * 128], rhs=UT[:, kc, t0:t0 + N],
                                                                                                 start=(kc == 0), stop=(kc == 7))),
                                    reads=[R_w1, R_wq] + rtok("UT", t0, t0 + N), writes=[RB[b2]])
                          S.add("dve", (lambda e, s=s, l0=l0, N=N, b1=b1: e.tensor_tensor(out=rt[s][:, 0, 0:N], in0=bank(b1)[:, 0:N], in1=rC[:, l0:l0 + N], op=ALU.mult)),
                                reads=[RB[b1], R_rope], writes=[R_rt[s]])
                          S.add("dve", (lambda e, s=s, l0=l0, N=N, b2=b2: e.tensor_tensor(out=rt[s][:, 1, 0:N], in0=bank(b2)[:, 0:N], in1=rS[:, l0:l0 + N], op=ALU.mult)),
                                reads=[RB[b2], R_rope], writes=[R_rt[s]])
                          if c < 8:
                              dst = qT1[:, c, l0:l0 + N]
                          else:
                              dst = kT1[:, c - 8, t0:t0 + N]
                          S.add("dve", (lambda e, s=s, N=N, dst=dst: e.tensor_tensor(out=dst, in0=rt[s][:, 0, 0:N], in1=rt[s][:, 1, 0:N], op=ALU.add)),
                                reads=[R_rt[s]], writes=rtok("qk", t0, t0 + N))
                  for cc in range(2):
                      s = it % 2
                      it += 1
                      b1 = 2 * s
                      for kc in range(8):
                          S.add("pe", (lambda e, kc=kc, cc=cc, b1=b1: e.matmul(bank(b1)[:, 0:256], lhsT=wk[:, kc, cc * 128:(cc + 1) * 128], rhs=UT[:, kc, 0:256], start=(kc == 0), stop=(kc == 7))),
                                reads=[R_w1] + rtok("UT", 0, 256), writes=[RB[b1]])
                      S.add("act", (lambda e, cc=cc, b1=b1: e.copy(out=kT1[:, cc, 0:256], in_=bank(b1)[:, 0:256])), reads=[RB[b1]], writes=rtok("qk", 0, 256))
                  for tt in range(18):
                      bk = 4 + tt % 2
                      for kc in range(8):
                          S.add("pe", (lambda e, kc=kc, tt=tt, bk=bk: e.matmul(bank(bk)[:, 0:256], lhsT=UT[:, kc, tt * 128:(tt + 1) * 128], rhs=wv[:, kc, :], start=(kc == 0), stop=(kc == 7))),
                                reads=[R_w1] + rtok("UT", tt * 128, tt * 128 + 128), writes=[RB[bk]])
                      for x in range(2):
                          S.add("act", (lambda e, tt=tt, bk=bk, x=x: e.copy(out=vaug1[:, tt, :, x * 128:x * 128 + 64],
                                                                          in_=bank(bk)[:, 0:256].rearrange("p (c x d) -> p c x d", c=2, x=2)[:, :, x, :])),
                                reads=[RB[bk]], writes=rtok("vaug", tt * 128, tt * 128 + 128))
                  dump("P1", kT1[:])
                  S.barrier()
                  cur[0] = C0
                  mlu = alloc("mlu", [128, 256], F32)
                  S.add("sp", lambda e: e.dma_start(out=mlu[:], in_=maskLU), writes=[R_const], dma=True)
                  sbm = [alloc("sbm%d" % i, [128, 512], F32) for i in range(2)]
                  PT1 = [alloc("PT1_%d" % i, [128, 5, 512], BF16) for i in range(2)]
                  rec1 = [alloc("rec1_%d" % i, [128, 512], F32) for i in range(2)]
                  R_sbm = [Res("sbm0"), Res("sbm1")]
                  R_PT1 = [[Res("PT1_%d_%d" % (i, k)) for k in range(5)] for i in range(2)]
                  R_rec1 = [Res("rec1_0"), Res("rec1_1")]
                  it = 0
                  sbr = 0
                  mi = 0
                  for g in range(4):
                      m, sh = g // 2, g % 2
                      p0, p1 = sh * 64, sh * 64 + 64
                      nh0, nh1 = (0, 64) if sh == 0 else (64, 128)
                      dh0, dh1 = (64, 128) if sh == 0 else (0, 64)
                      vcol = sh * 64
                      for qb in range(16):
                          s = it % 2
                          it += 1
                          tiles = []
                          if qb > 0:
                              tiles.append((NCTX + (qb - 1) * 128, 0))
                          tiles.append((NCTX + qb * 128, None))
                          if qb < 15:
                              tiles.append((NCTX + (qb + 1) * 128, 1))
                          tiles += [(0, None), (128, None)]
                          nt = len(tiles)
                          for i, (kt0, mk) in enumerate(tiles):
                              bk = sbr % 4
                              sbr += 1
                              S.add("pe", (lambda e, kt0=kt0, bk=bk, m=m, p0=p0, p1=p1, qb=qb: e.matmul(bank(bk).rearrange("p (h q) -> p h q", h=4), lhsT=kT1[p0:p1, m, kt0:kt0 + 128],
                                                                                                   rhs=qT1[p0:p1, 4 * m:4 * m + 4, qb * 128:(qb + 1) * 128], start=True, stop=True)),
                                    reads=rtok("qk", kt0, kt0 + 128) + rtok("qk", NCTX + qb * 128, NCTX + qb * 128 + 128), writes=[RB[bk]])
                              if mk is None:
                                  S.add("act", (lambda e, s=s, i=i, bk=bk: e.activation(out=PT1[s][:, i, :], in_=bank(bk), func=AF.Exp, scale=0.125)), reads=[RB[bk]], writes=[R_PT1[s][i]])
                              else:
                                  ms = mi % 2
                                  mi += 1
                                  S.add("dve", (lambda e, ms=ms, mk=mk, bk=bk: e.scalar_tensor_tensor(out=sbm[ms][:].rearrange("p (h q) -> p h q", h=4), in0=bank(bk).rearrange("p (h q) -> p h q", h=4),
                                                                                                    scalar=0.125, in1=mlu[:, mk * 128:(mk + 1) * 128].unsqueeze(1).broadcast_to([128, 4, 128]),
                                                                                                    op0=ALU.mult, op1=ALU.add)),
                                        reads=[RB[bk], R_const], writes=[R_sbm[ms]])
                                  S.add("act", (lambda e, s=s, i=i, ms=ms: e.activation(out=PT1[s][:, i, :], in_=sbm[ms][:], func=AF.Exp)), reads=[R_sbm[ms]], writes=[R_PT1[s][i]])
                          ob = 4 + s
                          for i, (kt0, mk) in enumerate(tiles):
                              S.add("pe", (lambda e, i=i, kt0=kt0, s=s, ob=ob, m=m, vcol=vcol, nt=nt: e.matmul(bank(ob), lhsT=vaug1[:, kt0 // 128, m, vcol:vcol + 128], rhs=PT1[s][:, i, :],
                                                                                                          start=(i == 0), stop=(i == nt - 1))),
                                    reads=[R_PT1[s][i]] + rtok("vaug", kt0, kt0 + 128), writes=[RB[ob]])
                          S.add("dve", (lambda e, s=s, ob=ob, m=m, sh=sh, dh0=dh0, dh1=dh1: e.tensor_tensor(out=rec1[s][dh0:dh1, :].rearrange("p (h q) -> p h q", h=4),
                                                                                                        in0=bank(ob)[dh0:dh1, :].rearrange("p (h q) -> p h q", h=4),
                                                                                                        in1=esink[dh0:dh1, (m * 2 + sh) * 4:(m * 2 + sh) * 4 + 4].unsqueeze(2).broadcast_to([64, 4, 128]),
                                                                                                        op=ALU.add)),
                                reads=[RB[ob], R_const], writes=[R_rec1[s]])
                          S.add("dve", (lambda e, s=s, dh0=dh0, dh1=dh1: e.reciprocal(out=rec1[s][dh0:dh1, :], in_=rec1[s][dh0:dh1, :])), reads=[R_rec1[s]], writes=[R_rec1[s]])
                          q0 = NCTX + qb * 128
                          S.add("dve", (lambda e, s=s, ob=ob, m=m, q0=q0, nh0=nh0, nh1=nh1, dh0=dh0, dh1=dh1: e.tensor_tensor(out=oT[nh0:nh1, 4 * m:4 * m + 4, q0:q0 + 128],
                                                                                                                     in0=bank(ob)[nh0:nh1, :].rearrange("p (h q) -> p h q", h=4),
                                                                                                                     in1=rec1[s][dh0:dh1, :].rearrange("p (h q) -> p h q", h=4), op=ALU.mult)),
                                reads=[RB[ob], R_rec1[s]], writes=rtok("oT", q0, q0 + 128))
                  dump("A1", oT[:])
                  w_out_d = w_out1
                  tok_blocks = lat256

              S.barrier()
              cur[0] = C0
              gatesT = alloc("gatesT", [16, NT], F32)
              wo = alloc("wo", [128, 8, D], BF16)
              hold0_at = cur[0]
              hold = [alloc("hold%d" % i, [128, 8, 128], F32) for i in range(2)]
              pre = alloc("pre", [128, 8, 128], F32)
              prebf = alloc("prebf", [128, 8, 128], BF16)
              presq = alloc("presq", [128, 8, 128], BF16)
              t32 = alloc("t32", [128, 8, 128], F32)
              tmpo = [alloc("tmpo%d" % i, [128, 128], F32) for i in range(2)]
              mean_sb = alloc("mean_sb", [128, 128], F32)
              rstd_sb = alloc("rstd_sb", [128, 128], F32)
              lsb = alloc("lsb", [128, 18, 20], F32, at=hold0_at)
              rw = alloc("rw", [128, 18 * 96], F32, at=hold0_at + 1472)
              assert hold0_at + 1472 + 18 * 96 * 4 <= cur[0]
              R_wo = Res("wo")
              R_hold = [Res("hold0"), Res("hold1")]
              R_pre, R_t32 = Res("pre"), Res("t32")
              R_tmpo = [Res("tmpo0"), Res("tmpo1")]
              R_lt = [Res("prebf"), Res("presq"), Res("mean"), Res("rstd")]
              R_rout = Res("rout")
              S.add("pool", (lambda e, w_out_d=w_out_d: e.dma_start(out=wo[:], in_=w_out_d.rearrange("(kc p) n -> p kc n", p=128))), writes=[R_wo], dma=True)
              tiles4 = [t for (t0_, n_) in tok_blocks for t in range(t0_, t0_ + n_, 128)]
              for bi, t0 in enumerate(tiles4):
                  s = bi % 2
                  mc = mcol(t0)
                  tt = t0 // 128
                  S.add("sp", (lambda e, s=s, t0=t0: e.dma_start(out=hold[s][:], in_=Hs3[:, :, t0:t0 + 128])), reads=rtok("Hs", t0, t0 + 128), writes=[R_hold[s]], dma=True)
                  for oc in range(8):
                      bk = oc // 4
                      co = (oc % 4) * 128
                      for kc in range(8):
                          S.add("pe", (lambda e, kc=kc, oc=oc, bk=bk, co=co, t0=t0: e.matmul(bank(bk)[:, co:co + 128], lhsT=wo[:, kc, oc * 128:(oc + 1) * 128], rhs=oT[:, kc, t0:t0 + 128],
                                                                                           start=(kc == 0), stop=(kc == 7))),
                                reads=[R_wo] + rtok("oT", t0, t0 + 128), writes=[RB[bk]])
                  for oc in range(8):
                      bk = oc // 4
                      co = (oc % 4) * 128
                      ts = oc % 2
                      if l == 0:
                          vb = V("M2B", oc, t0)
                          S.add("act", (lambda e, oc=oc, bk=bk, co=co, ts=ts, mc=mc, vb=vb: e.activation(out=tmpo[ts][:], in_=bank(bk)[:, co:co + 128], func=AF.Identity,
                                                                                                   bias=vb, scale=modc(0, 2, oc, mc))),
                                reads=[RB[bk], R_mod, R_vecs], writes=[R_tmpo[ts]])
                      else:
                          S.add("act", (lambda e, oc=oc, bk=bk, co=co, ts=ts, mc=mc: e.activation(out=tmpo[ts][:], in_=bank(bk)[:, co:co + 128], func=AF.Identity,
                                                                                            scale=modc(1, 2, oc, mc))),
                                reads=[RB[bk], R_mod], writes=[R_tmpo[ts]])
                      S.add("dve", (lambda e, oc=oc, s=s, ts=ts: e.scalar_tensor_tensor(out=pre[:, oc, :], in0=hold[s][:, oc, :], scalar=ALPHA, in1=tmpo[ts][:], op0=ALU.mult, op1=ALU.add)),
                            reads=[R_hold[s], R_tmpo[ts]], writes=[R_pre])
                  ln_stats(pre[:], 8, 128, ones1k, prebf, presq, mean_sb, rstd_sb, 4, [R_pre], R_lt)
                  normalize(pre[:], 8, 128, mean_sb, rstd_sb, [R_pre], R_lt)
                  for ch in range(8):
                      affine(UT[:, ch, t0:t0 + 128], pre[:, ch, :], V("G4", ch, t0), V("B4", ch, t0), [R_pre, R_vecs], rtok("UT", t0, t0 + 128))
                      affine(t32[:, ch, :], pre[:, ch, :], V("G4", ch, t0), V("B4", ch, t0), [R_pre, R_vecs], [R_t32])
                      affine(hres[:, ch, t0:t0 + 128], pre[:, ch, :], V("GA1", ch, t0), V("BA1", ch, t0), [R_pre, R_vecs], rtok("hres%d" % ch, t0, t0 + 128))
                  for kc in range(8):
                      S.add("pe", (lambda e, kc=kc, tt=tt, l=l: e.matmul(bank(5)[:, tt * 20:tt * 20 + 20], lhsT=t32[:, kc, :], rhs=wrt[:, l * 160 + kc * 20:l * 160 + kc * 20 + 20],
                                                                        start=(kc == 0), stop=(kc == 7))),
                            reads=[R_t32, R_const], writes=[RB[5]])

              dump("S4", hres[:])
              dump("S4u", UT[:])
              S.barrier()
              T0 = tok_blocks[0][0] // 128
              T1 = 18
              nT = T1 - T0
              S.add("dve", lambda e: e.tensor_copy(out=lsb[:, T0:T1, :], in_=bank(5)[:, T0 * 20:T1 * 20].rearrange("p (t n) -> p t n", n=20)), reads=[RB[5]], writes=[R_rout])

              def rwv(i, n):
                  return rw[:, i * 18 * 4:(i * 18 * 4) + 18 * n].rearrange("p (t n) -> p t n", n=n)[:, T0:T1, :]

              def rop(fn):
                  S.add("dve", fn, reads=[R_rout], writes=[R_rout])

              lg = lsb[:, T0:T1, 0:4]
              le = lsb[:, T0:T1, 4:20].rearrange("p t (g x) -> p t g x", g=4)
              gmax, gsum, gp, m1, m2, dd, w1, w2 = [rwv(i, 1) for i in range(8)]
              gsh, gmask, elsel, mask1, el2, mask2, within, wa_ = [rwv(8 + i, 4) for i in range(8)]
              t44 = rw[:, 18 * 64:18 * 80].rearrange("p (t g x) -> p t g x", g=4, x=4)[:, T0:T1]
              gates = rw[:, 18 * 80:18 * 96].rearrange("p (t g x) -> p t g x", g=4, x=4)
              bc4 = lambda a: a.broadcast_to([128, nT, 4])
              rop(lambda e: e.tensor_reduce(out=gmax, in_=lg, axis=AX.X, op=ALU.max))
              rop(lambda e: e.tensor_tensor(out=gsh, in0=lg, in1=bc4(gmax), op=ALU.subtract))
              rop(lambda e: e.tensor_tensor(out=gmask, in0=lg, in1=bc4(gmax), op=ALU.is_equal))
              S.add("act", lambda e: e.activation(out=gsh, in_=gsh, func=AF.Exp), reads=[R_rout], writes=[R_rout])
              rop(lambda e: e.tensor_reduce(out=gsum, in_=gsh, axis=AX.X, op=ALU.add))
              rop(lambda e: e.reciprocal(out=gp, in_=gsum))
              rop(lambda e: e.tensor_tensor(out=t44, in0=le, in1=gmask.unsqueeze(3).broadcast_to([128, nT, 4, 4]), op=ALU.mult))
              rop(lambda e: e.tensor_reduce(out=elsel, in_=t44.rearrange("p t g x -> p t x g"), axis=AX.X, op=ALU.add))
              rop(lambda e: e.tensor_reduce(out=m1, in_=elsel, axis=AX.X, op=ALU.max))
              rop(lambda e: e.tensor_tensor(out=mask1, in0=elsel, in1=bc4(m1), op=ALU.is_equal))
              rop(lambda e: e.scalar_tensor_tensor(out=el2, in0=mask1, scalar=NEG, in1=elsel, op0=ALU.mult, op1=ALU.add))
              rop(lambda e: e.tensor_reduce(out=m2, in_=el2, axis=AX.X, op=ALU.max))
              rop(lambda e: e.tensor_tensor(out=mask2, in0=el2, in1=bc4(m2), op=ALU.is_equal))
              rop(lambda e: e.tensor_tensor(out=dd, in0=m2, in1=m1, op=ALU.subtract))
              S.add("act", lambda e: e.activation(out=dd, in_=dd, func=AF.Exp), reads=[R_rout], writes=[R_rout])
              rop(lambda e: e.tensor_scalar_add(out=w1, in0=dd, scalar1=1.0))
              rop(lambda e: e.reciprocal(out=w1, in_=w1))
              rop(lambda e: e.tensor_tensor(out=w1, in0=w1, in1=gp, op=ALU.mult))
              rop(lambda e: e.tensor_tensor(out=w2, in0=dd, in1=w1, op=ALU.mult))
              rop(lambda e: e.tensor_tensor(out=within, in0=mask1, in1=bc4(w1), op=ALU.mult))
              rop(lambda e: e.tensor_tensor(out=wa_, in0=mask2, in1=bc4(w2), op=ALU.mult))
              rop(lambda e: e.tensor_tensor(out=within, in0=within, in1=wa_, op=ALU.add))
              rop(lambda e: e.tensor_tensor(out=gates[:, T0:T1], in0=gmask.unsqueeze(3).broadcast_to([128, nT, 4, 4]), in1=within.unsqueeze(2).broadcast_to([128, nT, 4, 4]), op=ALU.mult))
              for tt in range(T0, T1):
                  bk = 6 + (tt // 4) % 2
                  S.add("pe", (lambda e, tt=tt, bk=bk: e.transpose(bank(bk)[0:16, (tt % 4) * 128:(tt % 4 + 1) * 128], gates[:, tt].rearrange("p g x -> p (g x)"), id32[:])),
                        reads=[R_rout, R_const], writes=[RB[bk]])
                  if tt % 4 == 3 or tt == T1 - 1:
                      ta = (tt // 4) * 4
                      ta0 = max(ta, T0)
                      S.add("act", (lambda e, bk=bk, ta=ta, ta0=ta0, tt=tt: e.copy(out=gatesT[0:16, ta0 * 128:(tt + 1) * 128], in_=bank(bk)[0:16, (ta0 - ta) * 128:(tt + 1 - ta) * 128])),
                            reads=[RB[bk]], writes=[R_rout])

              S.barrier()
              cur[0] = C0
              gatesT = alloc("gatesT", [16, NT], F32)
              wgs = [alloc("wgs%d" % i, [128, 8, 256], BF16) for i in range(2)]
              wus = [alloc("wus%d" % i, [128, 8, 256], BF16) for i in range(2)]
              wds = [alloc("wds%d" % i, [128, 2, D], BF16) for i in range(2)]
              sgm = [alloc("sgm0", [128, 2, 512], F32)]
              hgm = [alloc("hgm%d" % i, [128, 2, 512], BF16) for i in range(4)]
              o_ = OT0
              for i in range(2, 4):
                  wgs.append(alloc("wgs%d" % i, [128, 8, 256], BF16, at=o_)); o_ += 4096
                  wus.append(alloc("wus%d" % i, [128, 8, 256], BF16, at=o_)); o_ += 4096
                  wds.append(alloc("wds%d" % i, [128, 2, D], BF16, at=o_)); o_ += 4096
              sgm.append(alloc("sgm1", [128, 2, 512], F32, at=o_)); o_ += 4096
              gsb = []
              for i in range(2):
                  gsb.append(alloc("gsb%d" % i, [128, 512], F32, at=o_)); o_ += 2048
              assert o_ <= OT0 + 36864
              R_ewg = [Res("ewg%d" % i) for i in range(4)]
              R_ewu = [Res("ewu%d" % i) for i in range(4)]
              R_ewd = [Res("ewd%d" % i) for i in range(4)]
              R_sgm = [Res("sgm0"), Res("sgm1")]
              R_hgm = [Res("hgm%d" % i) for i in range(4)]
              R_gsb = [Res("gsb0"), Res("gsb1")]
              mblocks = blocks512 if l == 0 else lat512

              def load_expert(ex, slot, l=l):
                  S.add("pool", (lambda e: e.dma_start(out=wgs[slot][:], in_=ewg[l, ex].rearrange("(kc p) f -> p kc f", p=128))), writes=[R_ewg[slot]], dma=True)
                  S.add("pool", (lambda e: e.dma_start(out=wus[slot][:], in_=ewu[l, ex].rearrange("(kc p) f -> p kc f", p=128))), writes=[R_ewu[slot]], dma=True)
                  S.add("pool", (lambda e: e.dma_start(out=wds[slot][:], in_=ewd[l, ex].rearrange("(kc p) f -> p kc f", p=128))), writes=[R_ewd[slot]], dma=True)

              def emit_front(ex, slot, t0, N, si):
                  S.add("pe", (lambda e: e.matmul(bank(4)[:, 0:N], lhsT=selt[0:16, ex * 128:(ex + 1) * 128], rhs=gatesT[0:16, t0:t0 + N], start=True, stop=True)),
                        reads=[R_rout, R_const], writes=[RB[4]])
                  S.add("act", (lambda e: e.copy(out=gsb[si][:, 0:N], in_=bank(4)[:, 0:N])), reads=[RB[4]], writes=[R_gsb[si]])
                  for oc in range(4):
                      wsrc = wgs[slot] if oc < 2 else wus[slot]
                      rw_ = R_ewg[slot] if oc < 2 else R_ewu[slot]
                      for kc in range(8):
                          S.add("pe", (lambda e, kc=kc, oc=oc, wsrc=wsrc: e.matmul(bank(oc)[:, 0:N], lhsT=wsrc[:, kc, (oc % 2) * 128:(oc % 2 + 1) * 128], rhs=UT[:, kc, t0:t0 + N],
                                                                                    start=(kc == 0), stop=(kc == 7))),
                                reads=[rw_] + rtok("UT", t0, t0 + N), writes=[RB[oc]])
                  S.add("act", (lambda e: e.activation(out=sgm[si][:, :, 0:N], in_=PS[0][:].rearrange("p (j n) -> p j n", j=2)[:, :, 0:N], func=AF.Silu)),
                        reads=[RB[0], RB[1]], writes=[R_sgm[si]])
                  S.add("dve", (lambda e: e.tensor_tensor(out=sgm[si][:, :, 0:N], in0=sgm[si][:, :, 0:N], in1=PS[1][:].rearrange("p (j n) -> p j n", j=2)[:, :, 0:N], op=ALU.mult)),
                        reads=[RB[2], RB[3], R_sgm[si]], writes=[R_sgm[si]])

              def emit_gate(si, q, N):
                  S.add("dve", (lambda e: e.tensor_tensor(out=hgm[q][:, :, 0:N], in0=sgm[si][:, :, 0:N], in1=gsb[si][:, 0:N].unsqueeze(1).broadcast_to([128, 2, N]), op=ALU.mult)),
                        reads=[R_gsb[si], R_sgm[si]], writes=[R_hgm[q]])

              ybc = [0]

              def make_yhalf(half, slots, qs, t0, N, mc, l=l):
                  def f():
                      for dc in range(half * 4, half * 4 + 4):
                          bk = 5 + ybc[0] % 3
                          ybc[0] += 1
                          for j in range(2):
                              for k2 in range(2):
                                  S.add("pe", (lambda e, j=j, k2=k2, dc=dc, bk=bk: e.matmul(bank(bk)[:, 0:N], lhsT=wds[slots[j]][:, k2, dc * 128:(dc + 1) * 128], rhs=hgm[qs[j]][:, k2, 0:N],
                                                                                           start=(j == 0 and k2 == 0), stop=(j == 1 and k2 == 1))),
                                        reads=[R_ewd[slots[j]], R_hgm[qs[j]]], writes=[RB[bk]])
                          S.add("dve", (lambda e, dc=dc, bk=bk: e.scalar_tensor_tensor(out=hres[:, dc, t0:t0 + N], in0=bank(bk)[:, 0:N], scalar=modc(l, 5, dc, mc),
                                                                                      in1=hres[:, dc, t0:t0 + N], op0=ALU.mult, op1=ALU.add)),
                                reads=[RB[bk], R_mod] + rtok("hres%d" % dc, t0, t0 + N), writes=rtok("hres%d" % dc, t0, t0 + N))
                  return f

              load_expert(0, 0)
              load_expert(1, 1)
              pendA = pendB = None
              it = 0
              for pr in range(8):
                  slots = (2 * (pr % 2), 2 * (pr % 2) + 1)
                  for bi, (t0, N) in enumerate(mblocks):
                      qs = ((it % 2) * 2, (it % 2) * 2 + 1)
                      it += 1
                      mc = mcol(t0)
                      emit_front(2 * pr, slots[0], t0, N, 0)
                      if pendA is not None:
                          pendA()
                      emit_gate(0, qs[0], N)
                      emit_front(2 * pr + 1, slots[1], t0, N, 1)
                      if pendB is not None:
                          pendB()
                      emit_gate(1, qs[1], N)
                      pendA = make_yhalf(0, slots, qs, t0, N, mc)
                      pendB = make_yhalf(1, slots, qs, t0, N, mc)
                      if bi == 0 and pr + 1 < 8:
                          nslots = (2 * ((pr + 1) % 2), 2 * ((pr + 1) % 2) + 1)
                          load_expert(2 * pr + 2, nslots[0])
                          load_expert(2 * pr + 3, nslots[1])
              pendA()
              pendB()

              dump("S5", hres[:])
              S.barrier()
              cur[0] = C0
              pre2 = alloc("pre2", [128, 8, 256], BF16)
              presq2 = alloc("presq2", [128, 8, 256], BF16)
              mean2 = alloc("mean2", [128, 256], F32)
              rstd2 = alloc("rstd2", [128, 256], F32)
              otile = [alloc("otile%d" % i, [128, D], F32) for i in range(2)]
              R_l2 = [Res("pre2"), Res("presq2"), Res("mean2"), Res("rstd2")]
              R_ot = [Res("ot0"), Res("ot1")]
              oi = 0
              for bi, (t0, N) in enumerate(tok_blocks):
                  hap = hres[:, :, t0:t0 + 256]
                  rh = [r_ for ch_ in range(8) for r_ in rtok("hres%d" % ch_, t0, t0 + 256)]
                  ln_stats(hap, 8, 256, ones1k, pre2, presq2, mean2, rstd2, 4, rh, R_l2)
                  normalize(hap, 8, 256, mean2, rstd2, rh, R_l2)
                  if l == 0:
                      for ch in range(8):
                          affine(UT[:, ch, t0:t0 + 256], hres[:, ch, t0:t0 + 256], V("GU", ch, t0), V("BU", ch, t0), rh + [R_vecs], rtok("UT", t0, t0 + 256))
                      for ch in range(8):
                          affine(hres[:, ch, t0:t0 + 256], hres[:, ch, t0:t0 + 256], smc("ln_g", 8 + ch), smc("ln_b", 8 + ch), rh + [R_const], rh)
                      S.add("sp", (lambda e, t0=t0: e.dma_start(out=Hs3[:, :, t0:t0 + 256], in_=hres[:, :, t0:t0 + 256])), reads=rh, writes=rtok("Hs", t0, t0 + 256), dma=True)
                      if debug and nlayers == 1:
                          S.add("sp", (lambda e, t0=t0: e.dma_start(out=dbg.rearrange("p (c t) -> p c t", c=8)[:, :, t0:t0 + 256], in_=hres[:, :, t0:t0 + 256])), reads=rh,
                                writes=[Res("dbgo")], dma=True)
                  else:
                      for ch in range(8):
                          affine(hres[:, ch, t0:t0 + 256], hres[:, ch, t0:t0 + 256], smc("ln_g", 24 + ch), smc("ln_b", 24 + ch), rh + [R_const], rh)
                      for hh in range(2):
                          tk = t0 + hh * 128
                          so = oi % 2
                          oi += 1
                          for ch in range(8):
                              bk = so * 2 + ch // 4
                              S.add("pe", (lambda e, ch=ch, bk=bk, tk=tk: e.transpose(bank(bk)[:, (ch % 4) * 128:(ch % 4 + 1) * 128], hres[:, ch, tk:tk + 128], id32[:])),
                                    reads=rh + [R_const], writes=[RB[bk]])
                          for hf in range(2):
                              bk = so * 2 + hf
                              S.add("act" if hf else "dve", (lambda e, so=so, hf=hf, bk=bk: (e.copy if hf else e.tensor_copy)(out=otile[so][:, hf * 512:(hf + 1) * 512], in_=bank(bk))),
                                    reads=[RB[bk]], writes=[R_ot[so]])
                          S.add("sp", (lambda e, so=so, tk=tk, b=b: e.dma_start(out=outd[b, tk - NCTX:tk - NCTX + 128, :], in_=otile[so][:])), reads=[R_ot[so]], writes=[Res("outw")], dma=True)
    except _Stop:
        pass
    S.barrier()

    with nc.Block() as block:
        @block.tensor
        def _(e):
            S.emit_one("pe", e, esem, dsems)

        @block.scalar
        def _(e):
            S.emit_one("act", e, esem, dsems)

        @block.vector
        def _(e):
            S.emit_one("dve", e, esem, dsems)

        @block.gpsimd
        def _(e):
            S.emit_one("pool", e, esem, dsems)

        @block.sync
        def _(e):
            S.emit_one("sp", e, esem, dsems)
    es.close()
    return nc


def _prep_shared(inp):
    f = lambda a: np.ascontiguousarray(np.asarray(a, np.float32))
    sm = np.zeros((128, SMN), np.float32)

    def put(name, arr):
        arr = np.asarray(arr, np.float32)
        sm[:, SMO[name]:SMO[name] + arr.shape[1]] = arr

    put("ada_b0", _fm(inp["ada_b"][0]))
    put("ada_b1", _fm(inp["ada_b"][1]))
    put("ln_g", np.concatenate([_fm(inp["ln_g"][l, k]) for l in range(2) for k in range(2)], axis=1))
    put("ln_b", np.concatenate([_fm(inp["ln_b"][l, k]) for l in range(2) for k in range(2)], axis=1))
    b_in = np.asarray(inp["ab_b_in"][0], np.float32)
    put("b_in", _fm(b_in[:2048]))
    cw = np.asarray(inp["conv_w"][0], np.float32)
    put("conv_w", np.ascontiguousarray(cw.T.reshape(4, 128, 31).transpose(1, 0, 2).reshape(128, 124)))
    put("conv_b", _fm(inp["conv_b"][0]))
    put("cln_g", _fm(inp["conv_ln_g"][0]))
    put("cln_b", _fm(inp["conv_ln_b"][0]))
    put("b_out", _fm(inp["ab_b_out"][0]))
    sm[:, SMO["eps"]] = EPS
    qidx = _gqa_qidx()
    gw = np.asarray(inp["gqa_w_in"][0], np.float32)
    wq = gw[:, :1024]
    wkk = gw[:, 1024:1280]
    wvv = gw[:, 1280:1536]
    C, Sg = _rope_tables()
    kk = np.arange(128)[:, None]
    qq = np.arange(128)[None, :]
    maskL = np.where(kk >= qq, 0.0, NEG).astype(np.float32)
    maskU = np.where(kk <= qq, 0.0, NEG).astype(np.float32)
    sink = np.asarray(inp["gqa_sink"][0], np.float32)
    sperm = np.array([8 * m + 4 * sh + j for m in range(2) for sh in range(2) for j in range(4)])
    sel = np.zeros((16, 16, 128), np.float32)
    for ex in range(16):
        sel[ex, ex, :] = 1.0
    wr = np.stack([np.concatenate([np.asarray(inp["router_group"][l], np.float32), np.asarray(inp["router_expert"][l], np.float32)], axis=1)
                   .reshape(8, 128, 20).transpose(1, 0, 2).reshape(128, 160) for l in range(2)])
    bv = b_in[2048:2560]
    shared = {
        "ada_w": f(inp["ada_w"]),
        "sm": sm,
        "w_in0": f(inp["ab_w_in"][0]),
        "bvbc": np.ascontiguousarray(np.broadcast_to(bv[None, :], (128, 512))),
        "nab": np.ascontiguousarray(_na_bias_table(np.asarray(inp["na_rpb"][0], np.float32)).reshape(8, 128, 21 * 128)),
        "w_out0": f(inp["ab_w_out"][0]),
        "wq1": f(wq[:, qidx]),
        "wqs1": f(wq[:, qidx][:, _swap64(1024)]),
        "wk1": f(wkk),
        "wks1": f(wkk[:, _swap64(256)]),
        "wv1": f(wvv),
        "w_out1": f(np.asarray(inp["gqa_w_out"][0], np.float32)[qidx, :]),
        "ropeC": C,
        "ropeS": Sg,
        "maskLU": np.ascontiguousarray(np.concatenate([maskL, maskU], axis=1)),
        "sinkbc": np.ascontiguousarray(np.broadcast_to(sink[sperm][None, :], (128, 16))),
        "wr": f(wr),
        "sel": np.ascontiguousarray(sel.reshape(16, 2048)),
        "ident": np.eye(128, dtype=np.float32),
        "ewg": f(inp["exp_w_gate"]),
        "ewu": f(inp["exp_w_up"]),
        "ewd": f(inp["exp_w_down"]),
    }
    return shared


def _core_inputs(inp, shared, i):
    x = np.asarray(inp["x"], np.float32)
    ctx = np.asarray(inp["ctx"], np.float32)
    c = np.asarray(inp["c"], np.float32)
    cc = np.stack([c[2 * i], c[2 * i + 1], np.asarray(inp["c_ctx"], np.float32)])
    cvec = np.ascontiguousarray(cc.reshape(3, 8, 128).transpose(2, 1, 0).reshape(128, 24))
    m = dict(shared)
    m["x2"] = np.ascontiguousarray(x[2 * i:2 * i + 2])
    m["ctx2"] = np.ascontiguousarray(ctx[2 * i:2 * i + 2])
    m["cvec"] = cvec
    return m


_NC_CACHE = {}


def kernel(**inputs):
    n = 8
    if "nc" not in _NC_CACHE:
        _NC_CACHE["nc"] = build()
    nc = _NC_CACHE["nc"]
    shared = _prep_shared(inputs)
    in_maps = [_core_inputs(inputs, shared, i) for i in range(n)]
    res = run_bass_kernel_spmd(nc, in_maps, core_ids=list(range(n)))
    out = np.concatenate([np.asarray(r["out"], np.float32) for r in res.results], axis=0)
    return out
```

```python
import numpy as np
from contextlib import ExitStack
import concourse.bass as bass
import concourse.mybir as mybir
from concourse.bass_utils import run_bass_kernel_spmd

F32 = mybir.dt.float32
BF16 = mybir.dt.bfloat16
AF = mybir.ActivationFunctionType
ALU = mybir.AluOpType
AX = mybir.AxisListType

D = 1024
SEQ = 2048
NCTX = 256
NT = SEQ + NCTX
GW = 64
ALPHA = 4.0 ** 0.25
EPS = 1e-5
NEG = -1e30

ENGS = ("pe", "act", "dve", "pool", "sp")
N_DMA_SEMS = 40


class Res:
    __slots__ = ("name", "last_w", "readers")

    def __init__(self, name):
        self.name = name
        self.last_w = None
        self.readers = []


class Op:
    __slots__ = ("eng", "fn", "idx", "deps", "dma", "sig", "semval", "dsem", "dval", "dprev")

    def __init__(self, eng, fn, idx, dma):
        self.eng = eng
        self.fn = fn
        self.idx = idx
        self.deps = []
        self.dma = dma
        self.sig = False
        self.semval = 0
        self.dsem = -1
        self.dval = 0
        self.dprev = 0


class Sched:
    def __init__(self):
        self.ops = {e: [] for e in ENGS}
        self.ndma = 0
        self.dma_tot = [0] * N_DMA_SEMS
        self.last_dma = [None] * N_DMA_SEMS
        self._assigned = False

    def add(self, eng, fn, reads=(), writes=(), dma=False, extra=()):
        lst = self.ops[eng]
        op = Op(eng, fn, len(lst), dma)
        deps = {}
        for r in reads:
            if r.last_w is not None:
                deps[id(r.last_w)] = r.last_w
        for w in writes:
            if w.last_w is not None:
                deps[id(w.last_w)] = w.last_w
            for rd in w.readers:
                deps[id(rd)] = rd
        for x in extra:
            deps[id(x)] = x
        for r in reads:
            r.readers.append(op)
        for w in writes:
            w.last_w = op
            w.readers = []
        if dma:
            s = self.ndma % N_DMA_SEMS
            self.ndma += 1
            op.dsem = s
            op.dprev = self.dma_tot[s]
            self.dma_tot[s] += 16
            op.dval = self.dma_tot[s]
            self.last_dma[s] = op
        for d in deps.values():
            if d is op:
                continue
            if d.eng == eng and not d.dma and not dma:
                if eng == "pe":
                    continue
                if op.idx - d.idx > 2:
                    continue
            op.deps.append(d)
            if not d.dma:
                d.sig = True
        lst.append(op)
        return op

    def barrier(self):
        lasts = []
        for e in ENGS:
            for op in reversed(self.ops[e]):
                if not op.dma:
                    lasts.append(op)
                    break
        dl = [o for o in self.last_dma if o is not None]
        for e in ENGS:
            self.add(e, lambda eng: eng.nop(), extra=[o for o in lasts if o.eng != e] + dl)

    def emit_one(self, e, eng, esem, dsems):
        if not self._assigned:
            for ee in ENGS:
                c = 0
                for op in self.ops[ee]:
                    if op.sig and not op.dma:
                        c += 1
                        op.semval = c
            self._assigned = True
        seen = {}
        for op in self.ops[e]:
            need = {}
            for d in op.deps:
                if d.dma:
                    key = ("d", d.dsem)
                    val = d.dval
                else:
                    key = ("e", d.eng)
                    val = d.semval
                if val > need.get(key, 0):
                    need[key] = val
            if op.dma and op.dprev > 0:
                key = ("d", op.dsem)
                if op.dprev > need.get(key, 0):
                    need[key] = op.dprev
            for key, val in need.items():
                if seen.get(key, 0) >= val:
                    continue
                seen[key] = val
                sem = dsems[key[1]] if key[0] == "d" else esem[key[1]]
                eng.wait_ge(sem, val)
            ins = op.fn(eng)
            if op.dma:
                ins.then_inc(dsems[op.dsem], 16)
            elif op.sig:
                ins.then_inc(esem[e], 1)


def _sm_layout():
    off = {}
    n = 0
    for name, cols in (("ada_b0", 48), ("ada_b1", 48), ("ln_g", 32), ("ln_b", 32), ("b_in", 16),
                       ("conv_w", 124), ("conv_b", 4), ("cln_g", 4), ("cln_b", 4), ("b_out", 8), ("eps", 1)):
        off[name] = n
        n += cols
    return off, n


SMO, SMN = _sm_layout()


def _fm(v):
    v = np.asarray(v, np.float32)
    return np.ascontiguousarray(v.reshape(-1, 128).T)


def _gqa_qidx():
    idx = np.zeros(1024, np.int64)
    for c in range(8):
        m, j = divmod(c, 4)
        h0 = 8 * m + j
        h1 = 8 * m + 4 + j
        idx[c * 128:c * 128 + 64] = h0 * 64 + np.arange(64)
        idx[c * 128 + 64:c * 128 + 128] = h1 * 64 + np.arange(64)
    return idx


def _swap64(n):
    d = np.arange(n)
    dd = d % 64
    sw = np.where(dd % 32 < 16, dd + 16, dd - 16)
    return (d // 64) * 64 + sw


def _na_tiles(j):
    if j in (0, 1):
        return [0, 1, 2, 3]
    if j in (14, 15):
        return [12, 13, 14, 15]
    return [j - 2, j - 1, j, j + 1, j + 2]


def _na_tile_index(j):
    if j == 0:
        return 5
    if j == 1:
        return 9
    if j == 14:
        return 13
    if j == 15:
        return 17
    return 0


def _na_bias_table(rpb):
    rows = 32
    r = np.arange(rows)
    row_start = np.clip(r - 4, 0, rows - 8)
    jj = np.arange(GW)
    col_start = np.clip(jj - 8, 0, GW - 16)
    col_in = (jj[None, :] >= col_start[:, None]) & (jj[None, :] < col_start[:, None] + 16)
    col_off = np.clip(jj[None, :] - jj[:, None], -15, 15) + 15
    out = np.full((8, 21, 128, 128), NEG, np.float32)

    def tile(j, a):
        t = np.full((8, 128, 128), NEG, np.float32)
        for pk in range(2):
            rk = 2 * a + pk
            for pq in range(2):
                rq = 2 * j + pq
                if not (row_start[rq] <= rk < row_start[rq] + 8):
                    continue
                ro = rk - rq + 7
                blk = rpb[:, ro][:, col_off]
                blk = np.where(col_in[None], blk, np.float32(NEG))
                t[:, pk * 64:(pk + 1) * 64, pq * 64:(pq + 1) * 64] = blk.transpose(0, 2, 1)
        return t

    for i, a in enumerate(_na_tiles(5)):
        out[:, i] = tile(5, a)
    for j in (0, 1, 14, 15):
        base = _na_tile_index(j)
        for i, a in enumerate(_na_tiles(j)):
            out[:, base + i] = tile(j, a)
    return np.ascontiguousarray(out.transpose(0, 2, 1, 3))


def _rope_tables():
    t = np.arange(SEQ)
    row = (t // GW).astype(np.float32)
    col = (t % GW).astype(np.float32)
    inv = (np.float32(10000.0) ** (-np.arange(0, 32, 2, dtype=np.float32) / np.float32(32))).astype(np.float32)
    ang = np.concatenate([row[:, None] * inv, col[:, None] * inv], axis=-1).astype(np.float32)
    cos = np.cos(ang).astype(np.float32)
    sin = np.sin(ang).astype(np.float32)
    p = np.arange(128)
    d = p % 64
    ai = (d // 32) * 16 + d % 16
    sgn = np.where(d % 32 < 16, -1.0, 1.0).astype(np.float32)
    C = np.ascontiguousarray(cos[:, ai].T)
    S = np.ascontiguousarray((sin[:, ai] * sgn[None, :]).T)
    return C.astype(np.float32), S.astype(np.float32)


class _Stop(Exception):
    pass


def build(nlayers=2, nb=2, debug=False, stop=None):
    nc = bass.Bass("TRN2", target_bir_lowering=False)
    S = Sched()

    def din(name, shape):
        return nc.dram_tensor(name, list(shape), F32, kind="ExternalInput").ap()

    x2 = din("x2", [2, SEQ, D])
    ctx2 = din("ctx2", [2, NCTX, D])
    cvec = din("cvec", [128, 24])
    ada_w = din("ada_w", [2, D, 6 * D])
    smd = din("sm", [128, SMN])
    w_in0 = din("w_in0", [D, 2560])
    bvbc = din("bvbc", [128, 512])
    nab = din("nab", [8, 128, 21 * 128])
    w_out0 = din("w_out0", [D, D])
    wq1 = din("wq1", [D, 1024])
    wqs1 = din("wqs1", [D, 1024])
    wk1 = din("wk1", [D, 256])
    wks1 = din("wks1", [D, 256])
    wv1 = din("wv1", [D, 256])
    w_out1 = din("w_out1", [D, D])
    ropeC = din("ropeC", [128, SEQ])
    ropeS = din("ropeS", [128, SEQ])
    maskLU = din("maskLU", [128, 256])
    sinkbc = din("sinkbc", [128, 16])
    wr = din("wr", [2, 128, 160])
    sel = din("sel", [16, 2048])
    ident = din("ident", [128, 128])
    ewg = din("ewg", [2, 16, D, 256])
    ewu = din("ewu", [2, 16, D, 256])
    ewd = din("ewd", [2, 16, 256, D])
    outd = nc.dram_tensor("out", [2, SEQ, D], F32, kind="ExternalOutput").ap()
    Hs = nc.dram_tensor("Hs", [128, 8 * NT], F32, kind="Internal").ap()
    dbg = nc.dram_tensor("dbg", [128, 8 * NT], F32, kind="ExternalOutput").ap() if debug else None
    dbgb = nc.dram_tensor("dbgb", [128, 8 * NT], BF16, kind="ExternalOutput").ap() if debug else None
    Hs3 = Hs.rearrange("p (c t) -> p c t", c=8)

    es = ExitStack()
    cur = [16640]

    acache = {}

    def alloc(name, shape, dt, at=None):
        nbytes = int(np.prod(shape[1:])) * (4 if dt == F32 else 2)
        if at is None:
            at = cur[0]
            cur[0] = (at + nbytes + 63) // 64 * 64
        assert at + nbytes <= 229376, (name, at, nbytes)
        key = (name, at, tuple(shape))
        if key not in acache:
            acache[key] = nc.alloc_sbuf_tensor_at("%s_%d" % (name, len(acache)), list(shape), dt, offset=at)
        return acache[key]

    sm = alloc("sm", [128, SMN], F32)
    id32 = alloc("id32", [128, 128], F32)
    ones1k = alloc("ones1k", [128, 128], BF16)
    ones512 = alloc("ones512", [128, 128], BF16)
    csil = alloc("csil", [128, 24], BF16)
    cv32 = alloc("cv32", [128, 24], F32)
    mod = alloc("mod", [128, 2 * 144], F32)
    mp1 = alloc("mp1", [128, 2 * 144], F32)
    vecs = alloc("vecs", [128, 128], F32)
    selt = alloc("selt", [16, 2048], F32)
    wrt = alloc("wrt", [128, 320], F32)
    esink = alloc("esink", [128, 16], F32)
    R_const = Res("const")
    R_mod = Res("mod")
    R_vecs = Res("vecs")
    base0 = cur[0]

    PS = [es.enter_context(nc.psum_tensor("ps%d" % i, [128, 1024], F32)) for i in range(4)]
    RB = [Res("bank%d" % i) for i in range(8)]

    def bank(k):
        return PS[k // 2][:, (k % 2) * 512:(k % 2) * 512 + 512]

    esem = {e: es.enter_context(nc.semaphore("es_" + e)) for e in ENGS}
    dsems = [es.enter_context(nc.semaphore("ds%d" % i)) for i in range(N_DMA_SEMS)]

    def smc(name, j, n=1):
        o = SMO[name] + j
        return sm[:, o:o + n]

    def modc(l, k, ch, col):
        o = l * 144 + (k * 8 + ch) * 3 + col
        return mod[:, o:o + 1]

    def mp1c(l, k, ch, col):
        o = l * 144 + (k * 8 + ch) * 3 + col
        return mp1[:, o:o + 1]

    VK = {}

    def vslot(kind, ch):
        key = (kind, ch)
        if key not in VK:
            VK[key] = len(VK)
            assert len(VK) <= 128
        o = VK[key]
        return vecs[:, o:o + 1]

    S.add("sp", lambda e: e.dma_start(out=sm[:], in_=smd), writes=[R_const], dma=True)
    S.add("sp", lambda e: e.dma_start(out=id32[:], in_=ident), writes=[R_const], dma=True)
    S.add("sp", lambda e: e.dma_start(out=cv32[:], in_=cvec), writes=[R_const], dma=True)
    S.add("sp", lambda e: e.dma_start(out=selt[:], in_=sel), writes=[R_const], dma=True)
    S.add("sp", lambda e: e.dma_start(out=wrt[:].rearrange("p (l n) -> p l n", l=2), in_=wr.rearrange("l p n -> p l n")), writes=[R_const], dma=True)
    S.add("sp", lambda e: e.dma_start(out=esink[:], in_=sinkbc), writes=[R_const], dma=True)
    S.add("pool", lambda e: e.memset(ones1k[:], 1.0 / 1024.0), writes=[R_const])
    S.add("pool", lambda e: e.memset(ones512[:], 1.0 / 512.0), writes=[R_const])
    S.add("act", lambda e: e.activation(out=csil[:], in_=cv32[:], func=AF.Silu), reads=[R_const], writes=[R_const])
    S.add("act", lambda e: e.activation(out=esink[:], in_=esink[:], func=AF.Exp), reads=[R_const], writes=[R_const])

    adaw = [alloc("adaw%d" % i, [128, 8, 1024], BF16) for i in range(2)]
    R_adaw = [Res("adaw0"), Res("adaw1")]
    pi = 0
    for l in range(nlayers):
        awl = ada_w[l].rearrange("(kc p) n -> p kc n", p=128)
        for piece in range(6):
            s = pi % 2
            pi += 1
            S.add("pool", (lambda e, s=s, awl=awl, piece=piece: e.dma_start(out=adaw[s][:], in_=awl[:, :, piece * 1024:(piece + 1) * 1024])),
                  writes=[R_adaw[s]], dma=True)
            for oc8 in range(8):
                oc = piece * 8 + oc8
                for kc in range(8):
                    S.add("pe", (lambda e, s=s, oc=oc, oc8=oc8, kc=kc: e.matmul(bank(0)[:, oc * 3:oc * 3 + 3], lhsT=adaw[s][:, kc, oc8 * 128:(oc8 + 1) * 128],
                                                                                    rhs=csil[:, kc * 3:kc * 3 + 3], start=(kc == 0), stop=(kc == 7))),
                          reads=[R_adaw[s], R_const], writes=[RB[0]])
        ab = smc("ada_b%d" % l, 0, 48)
        S.add("dve", (lambda e, l=l, ab=ab: e.tensor_tensor(out=mod[:, l * 144:(l + 1) * 144].rearrange("p (a b) -> p a b", b=3),
                                                             in0=bank(0)[:, 0:144].rearrange("p (a b) -> p a b", b=3),
                                                             in1=ab.unsqueeze(2).broadcast_to([128, 48, 3]), op=ALU.add)),
              reads=[RB[0], R_const], writes=[R_mod])
        S.add("dve", (lambda e, l=l: e.tensor_scalar_add(out=mp1[:, l * 144:(l + 1) * 144], in0=mod[:, l * 144:(l + 1) * 144], scalar1=1.0)),
              reads=[R_mod], writes=[R_mod])
    S.barrier()
    cur[0] = base0

    A0 = cur[0]
    hres = alloc("hres", [128, 8, NT], F32)
    qT = alloc("qT", [128, 4, NT], BF16, at=A0)
    kT = alloc("kT", [128, 4, NT], BF16, at=A0 + 18432)
    vaug = alloc("vaug", [128, 18, 4, 192], BF16, at=A0 + 36864)
    qT1 = alloc("qT1", [128, 8, SEQ], BF16, at=A0)
    kT1 = alloc("kT1", [128, 2, NT], BF16, at=A0 + 32768)
    vaug1 = alloc("vaug1", [128, 18, 2, 192], BF16, at=A0 + 41984)
    rC = alloc("rC", [128, SEQ], F32, at=A0 + 55808)
    rS = alloc("rS", [128, SEQ], F32, at=A0 + 55808 + 8192)
    bvb = alloc("bvb", [128, 512], F32, at=A0 + 64512)
    idb = alloc("idb", [128, 128], BF16, at=A0 + 64512 + 2048)
    cmean = alloc("cmean", [128, 256], F32, at=A0 + 64512 + 2304)
    crstd = alloc("crstd", [128, 256], F32, at=A0 + 64512 + 3328)
    UT = alloc("UT", [128, 8, NT], BF16)
    OT0 = cur[0]
    oT = alloc("oT", [128, 8, NT], BF16)
    C0 = cur[0]
    RT = {}

    def rtok(name, t0, t1):
        out = []
        for tt in range(t0 // 128, (t1 + 127) // 128):
            key = (name, tt)
            if key not in RT:
                RT[key] = Res("%s_%d" % key)
            out.append(RT[key])
        return out

    R_hpad = [Res("hpad%d" % c) for c in range(4)]

    def derive_vecs(l, col, tag):
        ops = []
        for ch in range(8):
            g1 = smc("ln_g", (l * 2 + 0) * 8 + ch)
            b1 = smc("ln_b", (l * 2 + 0) * 8 + ch)
            g2 = smc("ln_g", (l * 2 + 1) * 8 + ch)
            b2 = smc("ln_b", (l * 2 + 1) * 8 + ch)
            S.add("dve", (lambda e, ch=ch, g1=g1: e.tensor_tensor(out=vslot((tag, "G4"), ch), in0=g1, in1=mp1c(l, 4, ch, col), op=ALU.mult)),
                  reads=[R_const, R_mod], writes=[R_vecs])
            S.add("dve", (lambda e, ch=ch, b1=b1: e.scalar_tensor_tensor(out=vslot((tag, "B4"), ch), in0=b1, scalar=mp1c(l, 4, ch, col), in1=modc(l, 3, ch, col),
                                                                         op0=ALU.mult, op1=ALU.add)),
                  reads=[R_const, R_mod], writes=[R_vecs])
            S.add("dve", (lambda e, ch=ch, g1=g1: e.tensor_scalar_mul(out=vslot((tag, "GA1"), ch), in0=g1, scalar1=ALPHA)), reads=[R_const], writes=[R_vecs])
            S.add("dve", (lambda e, ch=ch, b1=b1: e.tensor_scalar_mul(out=vslot((tag, "BA1"), ch), in0=b1, scalar1=ALPHA)), reads=[R_const], writes=[R_vecs])
            if l == 0:
                S.add("dve", (lambda e, ch=ch: e.tensor_tensor(out=vslot((tag, "M2B"), ch), in0=modc(l, 2, ch, col), in1=smc("b_out", ch), op=ALU.mult)),
                      reads=[R_const, R_mod], writes=[R_vecs])
                S.add("dve", (lambda e, ch=ch, g2=g2: e.tensor_tensor(out=vslot((tag, "GU"), ch), in0=g2, in1=mp1c(1, 1, ch, col), op=ALU.mult)),
                      reads=[R_const, R_mod], writes=[R_vecs])
                S.add("dve", (lambda e, ch=ch, b2=b2: e.scalar_tensor_tensor(out=vslot((tag, "BU"), ch), in0=b2, scalar=mp1c(1, 1, ch, col), in1=modc(1, 0, ch, col),
                                                                             op0=ALU.mult, op1=ALU.add)),
                      reads=[R_const, R_mod], writes=[R_vecs])

    def ln_stats(pre_ap, nch, N, ones_t, prebf, presq, mean_sb, rstd_sb, bk, r_pre, r_tmp):
        S.add("dve", lambda e: e.tensor_copy(out=prebf[:, 0:nch, 0:N], in_=pre_ap), reads=r_pre, writes=[r_tmp[0]])
        S.add("act", lambda e: e.activation(out=presq[:, 0:nch, 0:N], in_=pre_ap, func=AF.Square), reads=r_pre, writes=[r_tmp[1]])
        for c in range(nch):
            S.add("pe", (lambda e, c=c: e.matmul(bank(bk)[:, 0:N], lhsT=ones_t[:], rhs=prebf[:, c, 0:N], start=(c == 0), stop=(c == nch - 1))),
                  reads=[r_tmp[0], R_const], writes=[RB[bk]])
        for c in range(nch):
            S.add("pe", (lambda e, c=c: e.matmul(bank(bk)[:, 256:256 + N], lhsT=ones_t[:], rhs=presq[:, c, 0:N], start=(c == 0), stop=(c == nch - 1))),
                  reads=[r_tmp[1], R_const], writes=[RB[bk]])
        S.add("act", lambda e: e.copy(out=mean_sb[:, 0:N], in_=bank(bk)[:, 0:N]), reads=[RB[bk]], writes=[r_tmp[2]])
        S.add("dve", lambda e: e.tensor_tensor(out=rstd_sb[:, 0:N], in0=mean_sb[:, 0:N], in1=mean_sb[:, 0:N], op=ALU.mult), reads=[r_tmp[2]], writes=[r_tmp[3]])
        S.add("dve", lambda e: e.tensor_tensor(out=rstd_sb[:, 0:N], in0=bank(bk)[:, 256:256 + N], in1=rstd_sb[:, 0:N], op=ALU.subtract),
              reads=[RB[bk], r_tmp[3]], writes=[r_tmp[3]])
        S.add("act", lambda e: e.activation(out=rstd_sb[:, 0:N], in_=rstd_sb[:, 0:N], func=AF.Sqrt, bias=smc("eps", 0), scale=1.0),
              reads=[r_tmp[3], R_const], writes=[r_tmp[3]])
        S.add("dve", lambda e: e.reciprocal(out=rstd_sb[:, 0:N], in_=rstd_sb[:, 0:N]), reads=[r_tmp[3]], writes=[r_tmp[3]])

    def normalize(pre_ap, nch, N, mean_sb, rstd_sb, r_pre, r_tmp):
        S.add("dve", lambda e: e.tensor_tensor(out=pre_ap, in0=pre_ap, in1=mean_sb[:, 0:N].unsqueeze(1).broadcast_to([128, nch, N]), op=ALU.subtract),
              reads=r_pre + [r_tmp[2]], writes=r_pre)
        S.add("dve", lambda e: e.tensor_tensor(out=pre_ap, in0=pre_ap, in1=rstd_sb[:, 0:N].unsqueeze(1).broadcast_to([128, nch, N]), op=ALU.mult),
              reads=r_pre + [r_tmp[3]], writes=r_pre)

    aff_rr = [0]

    def affine(out_ap, in_ap, sc, bi, reads, writes, psum_in=False):
        k = aff_rr[0] % 2
        aff_rr[0] += 1
        if k == 0:
            S.add("act", lambda e: e.activation(out=out_ap, in_=in_ap, func=AF.Identity, bias=bi, scale=sc), reads=reads, writes=writes)
        else:
            S.add("dve" if k == 1 else "pool", lambda e: e.tensor_scalar(out=out_ap, in0=in_ap, scalar1=sc, scalar2=bi, op0=ALU.mult, op1=ALU.add),
                  reads=reads, writes=writes)

    def dump(name, src_ap3):
        if stop != name and stop != "%s@%d" % (name, cur_l[0]):
            return
        S.barrier()
        c, t = src_ap3.shape[1], src_ap3.shape[2]
        dst = dbg if src_ap3.dtype == F32 else dbgb
        for ci in range(c):
            S.add("sp", (lambda e, ci=ci: e.dma_start(out=dst[:, ci * t:(ci + 1) * t], in_=src_ap3[:, ci, :])), writes=[Res("dbgo")], dma=True)
        raise _Stop()

    cur_l = [0]
    try:
      for b in range(nb):
        for l in range(nlayers):
              cur_l[0] = l
              lat_only = (l == nlayers - 1) and l == 1
              col = b
              S.barrier()
              derive_vecs(l, b, "lat")
              if l == 0:
                  derive_vecs(l, 2, "ctx")

              def V(kind, ch, t0):
                  return vslot((("ctx" if (t0 < NCTX and l == 0) else "lat"), kind), ch)

              def mcol(t0):
                  return 2 if t0 < NCTX else b

              cur[0] = C0
              if l == 0:
                  xin = [alloc("xin%d" % i, [128, D], F32) for i in range(2)]
                  hblk = [alloc("hblk%d" % i, [128, 8, 128], F32) for i in range(2)]
                  R_xin = [Res("xin0"), Res("xin1")]
                  R_hblk = [Res("hblk0"), Res("hblk1")]
                  for tt in range(18):
                      s = tt % 2
                      src = ctx2[b, tt * 128:(tt + 1) * 128, :] if tt < 2 else x2[b, (tt - 2) * 128:(tt - 1) * 128, :]
                      S.add("sp", (lambda e, s=s, src=src: e.dma_start(out=xin[s][:], in_=src)), writes=[R_xin[s]], dma=True)
                      for ch in range(8):
                          bk = (tt % 2) * 2 + ch // 4
                          S.add("pe", (lambda e, s=s, ch=ch, bk=bk: e.transpose(bank(bk)[:, (ch % 4) * 128:(ch % 4 + 1) * 128], xin[s][:, ch * 128:(ch + 1) * 128], id32[:])),
                                reads=[R_xin[s], R_const], writes=[RB[bk]])
                      for hf in range(2):
                          bk = (tt % 2) * 2 + hf
                          S.add("act", (lambda e, s=s, hf=hf, bk=bk: e.copy(out=hblk[s][:, hf * 4:(hf + 1) * 4, :], in_=bank(bk).rearrange("p (c t) -> p c t", c=4))),
                                reads=[RB[bk]], writes=[R_hblk[s]])
                      mc = mcol(tt * 128)
                      for ch in range(8):
                          bk = (tt % 2) * 2 + ch // 4
                          affine(UT[:, ch, tt * 128:(tt + 1) * 128], bank(bk)[:, (ch % 4) * 128:(ch % 4 + 1) * 128], mp1c(0, 1, ch, mc), modc(0, 0, ch, mc),
                                 [RB[bk], R_mod], rtok("UT", tt * 128, tt * 128 + 128), psum_in=True)
                      S.add("sp", (lambda e, s=s, tt=tt: e.dma_start(out=Hs3[:, :, tt * 128:(tt + 1) * 128], in_=hblk[s][:])), reads=[R_hblk[s]],
                            writes=rtok("Hs", tt * 128, tt * 128 + 128), dma=True)

              if l == 0:
                  dump("S0", UT[:])
              blocks512 = [(0, 256)] + [(256 + 512 * i, 512) for i in range(4)]
              blocks256 = [(256 * i, 256) for i in range(9)]
              if l == 1:
                  lat512 = [(256 + 512 * i, 512) for i in range(4)]
                  lat256 = [(256 * i, 256) for i in range(1, 9)]

              if l == 0:
                  S.barrier()
                  cur[0] = C0
                  wAB = alloc("wAB", [128, 8, 1536], BF16)
                  wA = alloc("wA", [128, 8, 1024], BF16, at=C0)
                  hpd = [alloc("hpd%d" % i, [128, 2368], BF16, at=C0 + 16384 + i * 4736) for i in range(2)]
                  dg0 = alloc("diag0", [128, 31, 128], BF16, at=C0 + 25856)
                  diag = [dg0, dg0]
                  cur[0] = C0 + 33792
                  sgt = [alloc("sgt%d" % i, [128, 512], F32) for i in range(2)]
                  czsq = alloc("czsq", [128, 4, 256], BF16)
                  cz = alloc("cz", [128, 4, 256], F32)
                  R_wAB, R_misc = Res("wAB"), Res("misc0")
                  R_hpd = [Res("hpd0"), Res("hpd1")]
                  R_dg = [Res("dg0")] * 2
                  R_sgt = [Res("sgt0"), Res("sgt1")]
                  R_cz, R_ct = Res("cz"), [Res("czbf"), Res("czsq"), Res("cmean"), Res("crstd")]
                  w0 = w_in0.rearrange("(kc p) n -> p kc n", p=128)
                  S.add("pool", lambda e: e.dma_start(out=wA[:], in_=w0[:, :, 0:1024]), writes=[R_wAB], dma=True)
                  S.add("pool", lambda e: e.dma_start(out=idb[:], in_=ident), writes=[R_misc], dma=True)
                  S.add("sp", lambda e: e.dma_start(out=bvb[:], in_=bvbc), writes=[R_misc], dma=True)
                  S.add("pool", lambda e: e.memset(hpd[0][:], 0.0), writes=[R_hpd[0]])
                  S.add("pool", lambda e: e.memset(hpd[1][:], 0.0), writes=[R_hpd[1]])
                  S.add("pool", lambda e: e.memset(vaug[:], 1.0), writes=rtok("vaug", 0, NT))

                  def hoff(t0):
                      return 15 + t0 if t0 < NCTX else 286 + 15 + (t0 - NCTX)

                  it = 0
                  for cc in range(4):
                      hs_ = cc % 2
                      for k in range(31):
                          S.add("dve", (lambda e, cc=cc, k=k, hs_=hs_: e.tensor_scalar_mul(out=diag[hs_][:, k, :], in0=idb[:], scalar1=smc("conv_w", cc * 31 + k))),
                                reads=[R_misc, R_const], writes=[R_dg[hs_]])
                      for (t0, N) in blocks512:
                          s = it % 2
                          it += 1
                          b1, b2 = 2 * s, 2 * s + 1
                          for kc in range(8):
                              S.add("pe", (lambda e, kc=kc, cc=cc, t0=t0, N=N, b1=b1: e.matmul(bank(b1)[:, 0:N], lhsT=wA[:, kc, cc * 128:(cc + 1) * 128], rhs=UT[:, kc, t0:t0 + N],
                                                                                             start=(kc == 0), stop=(kc == 7))),
                                    reads=[R_wAB] + rtok("UT", t0, t0 + N), writes=[RB[b1]])
                          for kc in range(8):
                              S.add("pe", (lambda e, kc=kc, cc=cc, t0=t0, N=N, b2=b2: e.matmul(bank(b2)[:, 0:N], lhsT=wA[:, kc, 512 + cc * 128:512 + (cc + 1) * 128], rhs=UT[:, kc, t0:t0 + N],
                                                                                             start=(kc == 0), stop=(kc == 7))),
                                    reads=[R_wAB] + rtok("UT", t0, t0 + N), writes=[RB[b2]])
                          S.add("act", (lambda e, s=s, cc=cc, N=N, b2=b2: e.activation(out=sgt[s][:, 0:N], in_=bank(b2)[:, 0:N], func=AF.Sigmoid, bias=smc("b_in", 4 + cc), scale=1.0)),
                                reads=[RB[b2], R_const], writes=[R_sgt[s]])
                          ho = hoff(t0)
                          S.add("dve", (lambda e, s=s, cc=cc, N=N, b1=b1, ho=ho, hs_=hs_: e.scalar_tensor_tensor(out=hpd[hs_][:, ho:ho + N], in0=bank(b1)[:, 0:N], scalar=smc("b_in", cc),
                                                                                                                 in1=sgt[s][:, 0:N], op0=ALU.add, op1=ALU.mult)),
                                reads=[RB[b1], R_sgt[s], R_const], writes=[R_hpd[hs_]])
                      for bi, (t0, N) in enumerate(blocks512):
                          ho = hoff(t0) - 15
                          bk = 4 + bi % 2
                          for k in range(31):
                              S.add("pe", (lambda e, k=k, ho=ho, N=N, bk=bk, hs_=hs_: e.matmul(bank(bk)[:, 0:N], lhsT=diag[hs_][:, k, :], rhs=hpd[hs_][:, ho + k:ho + k + N],
                                                                                           start=(k == 0), stop=(k == 30))),
                                    reads=[R_dg[hs_], R_hpd[hs_]], writes=[RB[bk]])
                          S.add("act", (lambda e, cc=cc, N=N, t0=t0, bk=bk: e.activation(out=oT[:, cc, t0:t0 + N], in_=bank(bk)[:, 0:N], func=AF.Identity, bias=smc("conv_b", cc), scale=1.0)),
                                reads=[RB[bk], R_const], writes=rtok("oT", t0, t0 + N))
                  dump("S1z", oT[:, 0:4, :])
                  dump("S1h", hpd[1][:].unsqueeze(1))
                  dump("S1d", diag[0][:])
                  S.add("pool", lambda e: e.dma_start(out=wAB[:], in_=w0[:, :, 1024:2560]), writes=[R_wAB] + R_hpd + [R_dg[0]], dma=True)
                  for (t0, N) in blocks256:
                      zin = oT[:, 0:4, t0:t0 + 256]
                      rz = rtok("oT", t0, t0 + 256)
                      S.add("act", (lambda e, zin=zin: e.activation(out=czsq[:], in_=zin, func=AF.Square)), reads=rz, writes=[R_ct[1]])
                      for c4 in range(4):
                          S.add("pe", (lambda e, c4=c4, t0=t0: e.matmul(bank(6)[:, 0:256], lhsT=ones512[:], rhs=oT[:, c4, t0:t0 + 256], start=(c4 == 0), stop=(c4 == 3))),
                                reads=rz + [R_const], writes=[RB[6]])
                      for c4 in range(4):
                          S.add("pe", (lambda e, c4=c4: e.matmul(bank(6)[:, 256:512], lhsT=ones512[:], rhs=czsq[:, c4, :], start=(c4 == 0), stop=(c4 == 3))),
                                reads=[R_ct[1], R_const], writes=[RB[6]])
                      S.add("act", lambda e: e.copy(out=cmean[:], in_=bank(6)[:, 0:256]), reads=[RB[6]], writes=[R_ct[2]])
                      S.add("dve", lambda e: e.tensor_tensor(out=crstd[:], in0=cmean[:], in1=cmean[:], op=ALU.mult), reads=[R_ct[2]], writes=[R_ct[3]])
                      S.add("dve", lambda e: e.tensor_tensor(out=crstd[:], in0=bank(6)[:, 256:512], in1=crstd[:], op=ALU.subtract), reads=[RB[6], R_ct[3]], writes=[R_ct[3]])
                      S.add("act", lambda e: e.activation(out=crstd[:], in_=crstd[:], func=AF.Sqrt, bias=smc("eps", 0), scale=1.0), reads=[R_ct[3], R_const], writes=[R_ct[3]])
                      S.add("dve", lambda e: e.reciprocal(out=crstd[:], in_=crstd[:]), reads=[R_ct[3]], writes=[R_ct[3]])
                      S.add("dve", (lambda e, zin=zin: e.tensor_tensor(out=cz[:], in0=zin, in1=cmean[:].unsqueeze(1).broadcast_to([128, 4, 256]), op=ALU.subtract)),
                            reads=rz + [R_ct[2]], writes=[R_cz])
                      S.add("dve", lambda e: e.tensor_tensor(out=cz[:], in0=cz[:], in1=crstd[:].unsqueeze(1).broadcast_to([128, 4, 256]), op=ALU.mult),
                            reads=[R_cz, R_ct[3]], writes=[R_cz])
                      for c4 in range(4):
                          S.add("act", (lambda e, c4=c4, t0=t0: e.activation(out=oT[:, c4, t0:t0 + 256], in_=cz[:, c4, :], func=AF.Silu, bias=smc("cln_b", c4), scale=smc("cln_g", c4))),
                                reads=[R_cz, R_const], writes=rz)
                  it = 0
                  for (t0, N) in blocks512:
                      for c in range(8):
                          bk = it % 4
                          it += 1
                          for kc in range(8):
                              S.add("pe", (lambda e, kc=kc, c=c, t0=t0, N=N, bk=bk: e.matmul(bank(bk)[:, 0:N], lhsT=wAB[:, kc, c * 128:(c + 1) * 128], rhs=UT[:, kc, t0:t0 + N],
                                                                                           start=(kc == 0), stop=(kc == 7))),
                                    reads=[R_wAB] + rtok("UT", t0, t0 + N), writes=[RB[bk]])
                          dst = qT if c < 4 else kT
                          S.add("act", (lambda e, c=c, t0=t0, N=N, bk=bk, dst=dst: e.activation(out=dst[:, c % 4, t0:t0 + N], in_=bank(bk)[:, 0:N], func=AF.Identity,
                                                                                              bias=smc("b_in", 8 + c), scale=1.0)),
                                reads=[RB[bk], R_const], writes=rtok("qk", t0, t0 + N))
                  for tt in range(18):
                      bk = it % 4
                      it += 1
                      for kc in range(8):
                          S.add("pe", (lambda e, kc=kc, tt=tt, bk=bk: e.matmul(bank(bk)[:, 0:512], lhsT=UT[:, kc, tt * 128:(tt + 1) * 128], rhs=wAB[:, kc, 1024:1536],
                                                                             start=(kc == 0), stop=(kc == 7))),
                                reads=[R_wAB] + rtok("UT", tt * 128, tt * 128 + 128), writes=[RB[bk]])
                      for x in range(2):
                          S.add("dve", (lambda e, tt=tt, bk=bk, x=x: e.tensor_tensor(out=vaug[:, tt, :, x * 128:x * 128 + 64],
                                                                                   in0=bank(bk).rearrange("p (c x d) -> p c x d", c=4, x=2)[:, :, x, :],
                                                                                   in1=bvb[:].rearrange("p (c x d) -> p c x d", c=4, x=2)[:, :, x, :], op=ALU.add)),
                                reads=[RB[bk], R_misc], writes=rtok("vaug", tt * 128, tt * 128 + 128))

                  dump("S1o", oT[:, 0:4, :])
                  dump("S1q", qT[:])
                  dump("S1k", kT[:])
                  dump("S1v", vaug[:].rearrange("p t c x -> p t (c x)"))
                  S.barrier()
                  cur[0] = C0
                  nabt = [alloc("nabt%d" % i, [128, 21, 128], F32) for i in range(2)]
                  sbt = [alloc("sbt%d" % i, [128, 640], F32) for i in range(2)]
                  PT = [alloc("PT%d" % i, [128, 896], BF16) for i in range(2)]
                  rec = [alloc("rec%d" % i, [128, 256], F32) for i in range(2)]
                  R_nabt = [Res("nabt0"), Res("nabt1")]
                  R_sbt = [Res("sbt0"), Res("sbt1")]
                  R_PT = [Res("PT0"), Res("PT1")]
                  R_rec = [Res("rec0"), Res("rec1")]
                  it = 0
                  na_pend = [None]
                  for h in range(8):
                      if na_pend[0] is not None:
                          na_pend[0]()
                          na_pend[0] = None
                      c, sh = h // 2, h % 2
                      p0, p1 = sh * 64, sh * 64 + 64
                      nh0, nh1 = (0, 64) if sh == 0 else (64, 128)
                      dh0, dh1 = (64, 128) if sh == 0 else (0, 64)
                      hs = h % 2
                      S.add("sp", (lambda e, h=h, hs=hs: e.dma_start(out=nabt[hs][:], in_=nab[h].rearrange("p (a q) -> p a q", a=21))), writes=[R_nabt[hs]], dma=True)
                      vcol = sh * 64
                      s = it % 2
                      it += 1
                      sb0 = 2 * s
                      for i in range(2):
                          S.add("pe", (lambda e, i=i, c=c, p0=p0, p1=p1, sb0=sb0: e.matmul(bank(sb0)[:, i * 256:(i + 1) * 256], lhsT=kT[p0:p1, c, i * 128:(i + 1) * 128],
                                                                                          rhs=qT[p0:p1, c, 0:256], start=True, stop=True)),
                                reads=rtok("qk", 0, 256), writes=[RB[sb0]])
                      S.add("act", (lambda e, s=s, sb0=sb0: e.activation(out=PT[s][:, 0:512], in_=bank(sb0)[:, 0:512], func=AF.Exp, scale=0.125)), reads=[RB[sb0]], writes=[R_PT[s]])
                      ob = 4 + s
                      for i in range(2):
                          S.add("pe", (lambda e, i=i, c=c, s=s, ob=ob, vcol=vcol: e.matmul(bank(ob)[:, 0:256], lhsT=vaug[:, i, c, vcol:vcol + 128], rhs=PT[s][:, i * 256:(i + 1) * 256],
                                                                                          start=(i == 0), stop=(i == 1))),
                                reads=[R_PT[s]] + rtok("vaug", 0, 256), writes=[RB[ob]])
                      S.add("dve", (lambda e, s=s, ob=ob, dh0=dh0, dh1=dh1: e.reciprocal(out=rec[s][dh0:dh1, 0:256], in_=bank(ob)[dh0:dh1, 0:256])), reads=[RB[ob]], writes=[R_rec[s]])
                      S.add("dve", (lambda e, s=s, ob=ob, c=c, nh0=nh0, nh1=nh1, dh0=dh0, dh1=dh1: e.tensor_tensor(out=oT[nh0:nh1, 4 + c, 0:256], in0=bank(ob)[nh0:nh1, 0:256],
                                                                                                               in1=rec[s][dh0:dh1, 0:256], op=ALU.mult)),
                            reads=[RB[ob], R_rec[s]], writes=rtok("oT", 0, 256))
                      for j in range(16):
                          s = it % 2
                          it += 1
                          sb0 = 2 * s
                          tl = _na_tiles(j)
                          nl = len(tl)
                          ti0 = _na_tile_index(j)
                          q0 = NCTX + j * 128
                          ktoks = [NCTX + a * 128 for a in tl] + [0, 128]
                          for i, kt0 in enumerate(ktoks):
                              bk = sb0 + (i // 4)
                              S.add("pe", (lambda e, i=i, kt0=kt0, bk=bk, c=c, p0=p0, p1=p1, q0=q0: e.matmul(bank(bk)[:, (i % 4) * 128:(i % 4 + 1) * 128], lhsT=kT[p0:p1, c, kt0:kt0 + 128],
                                                                                                        rhs=qT[p0:p1, c, q0:q0 + 128], start=True, stop=True)),
                                    reads=rtok("qk", kt0, kt0 + 128) + rtok("qk", q0, q0 + 128), writes=[RB[bk]])
                          S.add("dve", (lambda e, s=s, nl=nl, ti0=ti0, hs=hs: e.scalar_tensor_tensor(out=sbt[s][:, 0:nl * 128], in0=PS[s][:, 0:nl * 128], scalar=0.125,
                                                                                                    in1=nabt[hs][:, ti0:ti0 + nl, :].rearrange("p a q -> p (a q)"),
                                                                                                    op0=ALU.mult, op1=ALU.add)),
                                reads=[RB[sb0], RB[sb0 + 1], R_nabt[hs]], writes=[R_sbt[s]])
                          S.add("act", (lambda e, s=s, nl=nl: e.activation(out=PT[s][:, 0:nl * 128], in_=sbt[s][:, 0:nl * 128], func=AF.Exp)), reads=[R_sbt[s]], writes=[R_PT[s]])
                          S.add("act", (lambda e, s=s, nl=nl: e.activation(out=PT[s][:, nl * 128:(nl + 2) * 128], in_=PS[s][:, nl * 128:(nl + 2) * 128], func=AF.Exp, scale=0.125)),
                                reads=[RB[sb0], RB[sb0 + 1]], writes=[R_PT[s]])
                          ob = 4 + s

                          def na_back(ktoks=ktoks, c=c, s=s, ob=ob, vcol=vcol, nl=nl, q0=q0, nh0=nh0, nh1=nh1, dh0=dh0, dh1=dh1):
                              for i, kt0 in enumerate(ktoks):
                                  S.add("pe", (lambda e, i=i, kt0=kt0: e.matmul(bank(ob)[:, 0:128], lhsT=vaug[:, kt0 // 128, c, vcol:vcol + 128],
                                                                                rhs=PT[s][:, i * 128:(i + 1) * 128], start=(i == 0), stop=(i == nl + 1))),
                                        reads=[R_PT[s]] + rtok("vaug", kt0, kt0 + 128), writes=[RB[ob]])
                              S.add("act", (lambda e: e.activation(out=rec[s][dh0:dh1, 0:128], in_=bank(ob)[dh0:dh1, 0:128], func=AF.Ln)), reads=[RB[ob]], writes=[R_rec[s]])
                              S.add("act", (lambda e: e.activation(out=rec[s][dh0:dh1, 0:128], in_=rec[s][dh0:dh1, 0:128], func=AF.Exp, scale=-1.0)), reads=[R_rec[s]], writes=[R_rec[s]])
                              S.add("dve", (lambda e: e.tensor_tensor(out=oT[nh0:nh1, 4 + c, q0:q0 + 128], in0=bank(ob)[nh0:nh1, 0:128],
                                                                      in1=rec[s][dh0:dh1, 0:128], op=ALU.mult)),
                                    reads=[RB[ob], R_rec[s]], writes=rtok("oT", q0, q0 + 128))
                          if na_pend[0] is not None:
                              na_pend[0]()
                          na_pend[0] = na_back
                  if na_pend[0] is not None:
                      na_pend[0]()
                      na_pend[0] = None
                  dump("S3", oT[:, 4:8, :])
                  w_out_d = w_out0
                  tok_blocks = blocks256
              else:
                  S.barrier()
                  cur[0] = C0
                  wq = alloc("wq", [128, 8, 512], BF16)
                  wqs = alloc("wqs", [128, 8, 512], BF16)
                  wk = alloc("wk", [128, 8, 256], BF16)
                  wks = alloc("wks", [128, 8, 256], BF16)
                  wv = alloc("wv", [128, 8, 256], BF16)
                  rt = [alloc("rt%d" % i, [128, 2, 512], F32) for i in range(2)]
                  R_w1, R_rope = Res("w1"), Res("rope")
                  R_rt = [Res("rt0"), Res("rt1")]
                  R_wq = Res("wq")
                  for dst, src in ((wk, wk1), (wks, wks1), (wv, wv1)):
                      S.add("pool", (lambda e, dst=dst, src=src: e.dma_start(out=dst[:], in_=src.rearrange("(kc p) n -> p kc n", p=128))), writes=[R_w1], dma=True)
                  S.add("sp", lambda e: e.dma_start(out=rC[:], in_=ropeC), writes=[R_rope], dma=True)
                  S.add("sp", lambda e: e.dma_start(out=rS[:], in_=ropeS), writes=[R_rope], dma=True)
                  if True:
                      S.add("pool", lambda e: e.memset(vaug1[:], 1.0), writes=rtok("vaug", 0, NT))
                  it = 0
                  for half, (t0, N) in [(hf_, blk_) for hf_ in range(3) for blk_ in lat512]:
                      l0 = t0 - NCTX
                      if half < 2 and t0 == NCTX:
                          for dst, src in ((wq, wq1), (wqs, wqs1)):
                              S.add("pool", (lambda e, dst=dst, src=src, half=half: e.dma_start(out=dst[:], in_=src.rearrange("(kc p) n -> p kc n", p=128)[:, :, half * 512:(half + 1) * 512])),
                                    writes=[R_wq], dma=True)
                      for c in (range(half * 4, half * 4 + 4) if half < 2 else range(8, 10)):
                          s = it % 2
                          it += 1
                          b1, b2 = 2 * s, 2 * s + 1
                          wa, wb = (wq, wqs) if c < 8 else (wk, wks)
                          cc = (c % 4) if c < 8 else c - 8
                          for kc in range(8):
                              S.add("pe", (lambda e, kc=kc, cc=cc, wa=wa, t0=t0, N=N, b1=b1: e.matmul(bank(b1)[:, 0:N], lhsT=wa[:, kc, cc * 128:(cc + 1) * 128], rhs=UT[:, kc, t0:t0 + N],
                                                                                                 start=(kc == 0), stop=(kc == 7))),
                                    reads=[R_w1, R_wq] + rtok("UT", t0, t0 + N), writes=[RB[b1]])
                          for kc in range(8):
                              S.add("pe", (lambda e, kc=kc, cc=cc, wb=wb, t0=t0, N=N, b2=b2: e.matmul(bank(b2)[:, 0:N], lhsT=wb[:, kc, cc * 128:(cc + 1) * 128], rhs=UT[:, kc, t0:t0 + N],
                                                                                                 start=(kc == 0), stop=(kc == 7))),
                                    reads=[R_w1, R_wq] + rtok("UT", t0, t0 + N), writes=[RB[b2]])
                          S.add("dve", (lambda e, s=s, l0=l0, N=N, b1=b1: e.tensor_tensor(out=rt[s][:, 0, 0:N], in0=bank(b1)[:, 0:N], in1=rC[:, l0:l0 + N], op=ALU.mult)),
                                reads=[RB[b1], R_rope], writes=[R_rt[s]])
                          S.add("dve", (lambda e, s=s, l0=l0, N=N, b2=b2: e.tensor_tensor(out=rt[s][:, 1, 0:N], in0=bank(b2)[:, 0:N], in1=rS[:, l0:l0 + N], op=ALU.mult)),
                                reads=[RB[b2], R_rope], writes=[R_rt[s]])
                          if c < 8:
                              dst = qT1[:, c, l0:l0 + N]
                          else:
                              dst = kT1[:, c - 8, t0:t0 + N]
                          S.add("dve", (lambda e, s=s, N=N, dst=dst: e.tensor_tensor(out=dst, in0=rt[s][:, 0, 0:N], in1=rt[s][:, 1, 0:N], op=ALU.add)),
                                reads=[R_rt[s]], writes=rtok("qk", t0, t0 + N))
                  for cc in range(2):
                      s = it % 2
                      it += 1
                      b1 = 2 * s
                      for kc in range(8):
                          S.add("pe", (lambda e, kc=kc, cc=cc, b1=b1: e.matmul(bank(b1)[:, 0:256], lhsT=wk[:, kc, cc * 128:(cc + 1) * 128], rhs=UT[:, kc, 0:256], start=(kc == 0), stop=(kc == 7))),
                                reads=[R_w1] + rtok("UT", 0, 256), writes=[RB[b1]])
                      S.add("act", (lambda e, cc=cc, b1=b1: e.copy(out=kT1[:, cc, 0:256], in_=bank(b1)[:, 0:256])), reads=[RB[b1]], writes=rtok("qk", 0, 256))
                  for tt in range(18):
                      bk = 4 + tt % 2
                      for kc in range(8):
                          S.add("pe", (lambda e, kc=kc, tt=tt, bk=bk: e.matmul(bank(bk)[:, 0:256], lhsT=UT[:, kc, tt * 128:(tt + 1) * 128], rhs=wv[:, kc, :], start=(kc == 0), stop=(kc == 7))),
                                reads=[R_w1] + rtok("UT", tt * 128, tt * 128 + 128), writes=[RB[bk]])
                      for x in range(2):
                          S.add("act", (lambda e, tt=tt, bk=bk, x=x: e.copy(out=vaug1[:, tt, :, x * 128:x * 128 + 64],
                                                                          in_=bank(bk)[:, 0:256].rearrange("p (c x d) -> p c x d", c=2, x=2)[:, :, x, :])),
                                reads=[RB[bk]], writes=rtok("vaug", tt * 128, tt * 128 + 128))
                  dump("P1", kT1[:])
                  S.barrier()
                  cur[0] = C0
                  mlu = alloc("mlu", [128, 256], F32)
                  S.add("sp", lambda e: e.dma_start(out=mlu[:], in_=maskLU), writes=[R_const], dma=True)
                  sbm = [alloc("sbm%d" % i, [128, 512], F32) for i in range(2)]
                  PT1 = [alloc("PT1_%d" % i, [128, 5, 512], BF16) for i in range(2)]
                  rec1 = [alloc("rec1_%d" % i, [128, 512], F32) for i in range(2)]
                  R_sbm = [Res("sbm0"), Res("sbm1")]
                  R_PT1 = [[Res("PT1_%d_%d" % (i, k)) for k in range(5)] for i in range(2)]
                  R_rec1 = [Res("rec1_0"), Res("rec1_1")]
                  it = 0
                  sbr = 0
                  mi = 0
                  gq_pend = [None]
                  for g in range(4):
                      m, sh = g // 2, g % 2
                      p0, p1 = sh * 64, sh * 64 + 64
                      nh0, nh1 = (0, 64) if sh == 0 else (64, 128)
                      dh0, dh1 = (64, 128) if sh == 0 else (0, 64)
                      vcol = sh * 64
                      for qb in range(16):
                          s = it % 2
                          it += 1
                          tiles = []
                          if qb > 0:
                              tiles.append((NCTX + (qb - 1) * 128, 0))
                          tiles.append((NCTX + qb * 128, None))
                          if qb < 15:
                              tiles.append((NCTX + (qb + 1) * 128, 1))
                          tiles += [(0, None), (128, None)]
                          nt = len(tiles)
                          for i, (kt0, mk) in enumerate(tiles):
                              bk = sbr % 4
                              sbr += 1
                              S.add("pe", (lambda e, kt0=kt0, bk=bk, m=m, p0=p0, p1=p1, qb=qb: e.matmul(bank(bk).rearrange("p (h q) -> p h q", h=4), lhsT=kT1[p0:p1, m, kt0:kt0 + 128],
                                                                                                   rhs=qT1[p0:p1, 4 * m:4 * m + 4, qb * 128:(qb + 1) * 128], start=True, stop=True)),
                                    reads=rtok("qk", kt0, kt0 + 128) + rtok("qk", NCTX + qb * 128, NCTX + qb * 128 + 128), writes=[RB[bk]])
                              if mk is None:
                                  S.add("act", (lambda e, s=s, i=i, bk=bk: e.activation(out=PT1[s][:, i, :], in_=bank(bk), func=AF.Exp, scale=0.125)), reads=[RB[bk]], writes=[R_PT1[s][i]])
                              else:
                                  ms = mi % 2
                                  mi += 1
                                  S.add("dve", (lambda e, ms=ms, mk=mk, bk=bk: e.scalar_tensor_tensor(out=sbm[ms][:].rearrange("p (h q) -> p h q", h=4), in0=bank(bk).rearrange("p (h q) -> p h q", h=4),
                                                                                                    scalar=0.125, in1=mlu[:, mk * 128:(mk + 1) * 128].unsqueeze(1).broadcast_to([128, 4, 128]),
                                                                                                    op0=ALU.mult, op1=ALU.add)),
                                        reads=[RB[bk], R_const], writes=[R_sbm[ms]])
                                  S.add("act", (lambda e, s=s, i=i, ms=ms: e.activation(out=PT1[s][:, i, :], in_=sbm[ms][:], func=AF.Exp)), reads=[R_sbm[ms]], writes=[R_PT1[s][i]])
                          ob = 4 + s
                          q0 = NCTX + qb * 128

                          def gq_back(tiles=tiles, s=s, ob=ob, m=m, sh=sh, vcol=vcol, nt=nt, q0=q0, nh0=nh0, nh1=nh1, dh0=dh0, dh1=dh1):
                              for i, (kt0, mk) in enumerate(tiles):
                                  S.add("pe", (lambda e, i=i, kt0=kt0: e.matmul(bank(ob), lhsT=vaug1[:, kt0 // 128, m, vcol:vcol + 128], rhs=PT1[s][:, i, :],
                                                                                start=(i == 0), stop=(i == nt - 1))),
                                        reads=[R_PT1[s][i]] + rtok("vaug", kt0, kt0 + 128), writes=[RB[ob]])
                              S.add("dve", (lambda e: e.tensor_tensor(out=rec1[s][dh0:dh1, :].rearrange("p (h q) -> p h q", h=4),
                                                                      in0=bank(ob)[dh0:dh1, :].rearrange("p (h q) -> p h q", h=4),
                                                                      in1=esink[dh0:dh1, (m * 2 + sh) * 4:(m * 2 + sh) * 4 + 4].unsqueeze(2).broadcast_to([64, 4, 128]),
                                                                      op=ALU.add)),
                                    reads=[RB[ob], R_const], writes=[R_rec1[s]])
                              S.add("act", (lambda e: e.activation(out=rec1[s][dh0:dh1, :], in_=rec1[s][dh0:dh1, :], func=AF.Ln)), reads=[R_rec1[s]], writes=[R_rec1[s]])
                              S.add("act", (lambda e: e.activation(out=rec1[s][dh0:dh1, :], in_=rec1[s][dh0:dh1, :], func=AF.Exp, scale=-1.0)), reads=[R_rec1[s]], writes=[R_rec1[s]])
                              S.add("dve", (lambda e: e.tensor_tensor(out=oT[nh0:nh1, 4 * m:4 * m + 4, q0:q0 + 128],
                                                                      in0=bank(ob)[nh0:nh1, :].rearrange("p (h q) -> p h q", h=4),
                                                                      in1=rec1[s][dh0:dh1, :].rearrange("p (h q) -> p h q", h=4), op=ALU.mult)),
                                    reads=[RB[ob], R_rec1[s]], writes=rtok("oT", q0, q0 + 128))
                          if gq_pend[0] is not None:
                              gq_pend[0]()
                          gq_pend[0] = gq_back
                  gq_pend[0]()
                  gq_pend[0] = None
                  dump("A1", oT[:])
                  w_out_d = w_out1
                  tok_blocks = lat256

              S.barrier()
              cur[0] = C0
              gatesT = alloc("gatesT", [16, NT], F32)
              wo = alloc("wo", [128, 8, D], BF16)
              hold0_at = cur[0]
              hold = [alloc("hold%d" % i, [128, 8, 128], F32) for i in range(2)]
              pre = alloc("pre", [128, 8, 128], F32)
              prebf = alloc("prebf", [128, 8, 128], BF16)
              presq = alloc("presq", [128, 8, 128], BF16)
              t32 = alloc("t32", [128, 8, 128], F32)
              tmpo = [alloc("tmpo%d" % i, [128, 128], F32) for i in range(2)]
              mean_sb = alloc("mean_sb", [128, 128], F32)
              rstd_sb = alloc("rstd_sb", [128, 128], F32)
              lsb = alloc("lsb", [128, 18, 20], F32, at=hold0_at)
              rw = alloc("rw", [128, 18 * 96], F32, at=hold0_at + 1472)
              assert hold0_at + 1472 + 18 * 96 * 4 <= cur[0]
              R_wo = Res("wo")
              R_hold = [Res("hold0"), Res("hold1")]
              R_pre, R_t32 = Res("pre"), Res("t32")
              R_tmpo = [Res("tmpo0"), Res("tmpo1")]
              R_lt = [Res("prebf"), Res("presq"), Res("mean"), Res("rstd")]
              R_rout = Res("rout")
              S.add("pool", (lambda e, w_out_d=w_out_d: e.dma_start(out=wo[:], in_=w_out_d.rearrange("(kc p) n -> p kc n", p=128))), writes=[R_wo], dma=True)
              tiles4 = [t for (t0_, n_) in tok_blocks for t in range(t0_, t0_ + n_, 128)]
              for bi, t0 in enumerate(tiles4):
                  s = bi % 2
                  mc = mcol(t0)
                  tt = t0 // 128
                  S.add("sp", (lambda e, s=s, t0=t0: e.dma_start(out=hold[s][:], in_=Hs3[:, :, t0:t0 + 128])), reads=rtok("Hs", t0, t0 + 128), writes=[R_hold[s]], dma=True)
                  for oc in range(8):
                      bk = oc // 4
                      co = (oc % 4) * 128
                      for kc in range(8):
                          S.add("pe", (lambda e, kc=kc, oc=oc, bk=bk, co=co, t0=t0: e.matmul(bank(bk)[:, co:co + 128], lhsT=wo[:, kc, oc * 128:(oc + 1) * 128], rhs=oT[:, kc, t0:t0 + 128],
                                                                                           start=(kc == 0), stop=(kc == 7))),
                                reads=[R_wo] + rtok("oT", t0, t0 + 128), writes=[RB[bk]])
                  for oc in range(8):
                      bk = oc // 4
                      co = (oc % 4) * 128
                      ts = oc % 2
                      if l == 0:
                          vb = V("M2B", oc, t0)
                          S.add("act", (lambda e, oc=oc, bk=bk, co=co, ts=ts, mc=mc, vb=vb: e.activation(out=tmpo[ts][:], in_=bank(bk)[:, co:co + 128], func=AF.Identity,
                                                                                                   bias=vb, scale=modc(0, 2, oc, mc))),
                                reads=[RB[bk], R_mod, R_vecs], writes=[R_tmpo[ts]])
                      else:
                          S.add("act", (lambda e, oc=oc, bk=bk, co=co, ts=ts, mc=mc: e.activation(out=tmpo[ts][:], in_=bank(bk)[:, co:co + 128], func=AF.Identity,
                                                                                            scale=modc(1, 2, oc, mc))),
                                reads=[RB[bk], R_mod], writes=[R_tmpo[ts]])
                      S.add("dve", (lambda e, oc=oc, s=s, ts=ts: e.scalar_tensor_tensor(out=pre[:, oc, :], in0=hold[s][:, oc, :], scalar=ALPHA, in1=tmpo[ts][:], op0=ALU.mult, op1=ALU.add)),
                            reads=[R_hold[s], R_tmpo[ts]], writes=[R_pre])
                  ln_stats(pre[:], 8, 128, ones1k, prebf, presq, mean_sb, rstd_sb, 4, [R_pre], R_lt)
                  normalize(pre[:], 8, 128, mean_sb, rstd_sb, [R_pre], R_lt)
                  for ch in range(8):
                      affine(UT[:, ch, t0:t0 + 128], pre[:, ch, :], V("G4", ch, t0), V("B4", ch, t0), [R_pre, R_vecs], rtok("UT", t0, t0 + 128))
                      affine(t32[:, ch, :], pre[:, ch, :], V("G4", ch, t0), V("B4", ch, t0), [R_pre, R_vecs], [R_t32])
                      affine(hres[:, ch, t0:t0 + 128], pre[:, ch, :], V("GA1", ch, t0), V("BA1", ch, t0), [R_pre, R_vecs], rtok("hres%d" % ch, t0, t0 + 128))
                  for kc in range(8):
                      S.add("pe", (lambda e, kc=kc, tt=tt, l=l: e.matmul(bank(5)[:, tt * 20:tt * 20 + 20], lhsT=t32[:, kc, :], rhs=wrt[:, l * 160 + kc * 20:l * 160 + kc * 20 + 20],
                                                                        start=(kc == 0), stop=(kc == 7))),
                            reads=[R_t32, R_const], writes=[RB[5]])

              dump("S4", hres[:])
              dump("S4u", UT[:])
              S.barrier()
              T0 = tok_blocks[0][0] // 128
              T1 = 18
              nT = T1 - T0
              S.add("dve", lambda e: e.tensor_copy(out=lsb[:, T0:T1, :], in_=bank(5)[:, T0 * 20:T1 * 20].rearrange("p (t n) -> p t n", n=20)), reads=[RB[5]], writes=[R_rout])

              def rwv(i, n):
                  return rw[:, i * 18 * 4:(i * 18 * 4) + 18 * n].rearrange("p (t n) -> p t n", n=n)[:, T0:T1, :]

              def rop(fn):
                  S.add("dve", fn, reads=[R_rout], writes=[R_rout])

              lg = lsb[:, T0:T1, 0:4]
              le = lsb[:, T0:T1, 4:20].rearrange("p t (g x) -> p t g x", g=4)
              gmax, gsum, gp, m1, m2, dd, w1, w2 = [rwv(i, 1) for i in range(8)]
              gsh, gmask, elsel, mask1, el2, mask2, within, wa_ = [rwv(8 + i, 4) for i in range(8)]
              t44 = rw[:, 18 * 64:18 * 80].rearrange("p (t g x) -> p t g x", g=4, x=4)[:, T0:T1]
              gates = rw[:, 18 * 80:18 * 96].rearrange("p (t g x) -> p t g x", g=4, x=4)
              bc4 = lambda a: a.broadcast_to([128, nT, 4])
              rop(lambda e: e.tensor_reduce(out=gmax, in_=lg, axis=AX.X, op=ALU.max))
              rop(lambda e: e.tensor_tensor(out=gsh, in0=lg, in1=bc4(gmax), op=ALU.subtract))
              rop(lambda e: e.tensor_tensor(out=gmask, in0=lg, in1=bc4(gmax), op=ALU.is_equal))
              S.add("act", lambda e: e.activation(out=gsh, in_=gsh, func=AF.Exp), reads=[R_rout], writes=[R_rout])
              rop(lambda e: e.tensor_reduce(out=gsum, in_=gsh, axis=AX.X, op=ALU.add))
              rop(lambda e: e.reciprocal(out=gp, in_=gsum))
              rop(lambda e: e.tensor_tensor(out=t44, in0=le, in1=gmask.unsqueeze(3).broadcast_to([128, nT, 4, 4]), op=ALU.mult))
              rop(lambda e: e.tensor_reduce(out=elsel, in_=t44.rearrange("p t g x -> p t x g"), axis=AX.X, op=ALU.add))
              rop(lambda e: e.tensor_reduce(out=m1, in_=elsel, axis=AX.X, op=ALU.max))
              rop(lambda e: e.tensor_tensor(out=mask1, in0=elsel, in1=bc4(m1), op=ALU.is_equal))
              rop(lambda e: e.scalar_tensor_tensor(out=el2, in0=mask1, scalar=NEG, in1=elsel, op0=ALU.mult, op1=ALU.add))
              rop(lambda e: e.tensor_reduce(out=m2, in_=el2, axis=AX.X, op=ALU.max))
              rop(lambda e: e.tensor_tensor(out=mask2, in0=el2, in1=bc4(m2), op=ALU.is_equal))
              rop(lambda e: e.tensor_tensor(out=dd, in0=m2, in1=m1, op=ALU.subtract))
              S.add("act", lambda e: e.activation(out=dd, in_=dd, func=AF.Exp), reads=[R_rout], writes=[R_rout])
              rop(lambda e: e.tensor_scalar_add(out=w1, in0=dd, scalar1=1.0))
              rop(lambda e: e.reciprocal(out=w1, in_=w1))
              rop(lambda e: e.tensor_tensor(out=w1, in0=w1, in1=gp, op=ALU.mult))
              rop(lambda e: e.tensor_tensor(out=w2, in0=dd, in1=w1, op=ALU.mult))
              rop(lambda e: e.tensor_tensor(out=within, in0=mask1, in1=bc4(w1), op=ALU.mult))
              rop(lambda e: e.tensor_tensor(out=wa_, in0=mask2, in1=bc4(w2), op=ALU.mult))
              rop(lambda e: e.tensor_tensor(out=within, in0=within, in1=wa_, op=ALU.add))
              rop(lambda e: e.tensor_tensor(out=gates[:, T0:T1], in0=gmask.unsqueeze(3).broadcast_to([128, nT, 4, 4]), in1=within.unsqueeze(2).broadcast_to([128, nT, 4, 4]), op=ALU.mult))
              for tt in range(T0, T1):
                  bk = 6 + (tt // 4) % 2
                  S.add("pe", (lambda e, tt=tt, bk=bk: e.transpose(bank(bk)[0:16, (tt % 4) * 128:(tt % 4 + 1) * 128], gates[:, tt].rearrange("p g x -> p (g x)"), id32[:])),
                        reads=[R_rout, R_const], writes=[RB[bk]])
                  if tt % 4 == 3 or tt == T1 - 1:
                      ta = (tt // 4) * 4
                      ta0 = max(ta, T0)
                      S.add("act", (lambda e, bk=bk, ta=ta, ta0=ta0, tt=tt: e.copy(out=gatesT[0:16, ta0 * 128:(tt + 1) * 128], in_=bank(bk)[0:16, (ta0 - ta) * 128:(tt + 1 - ta) * 128])),
                            reads=[RB[bk]], writes=[R_rout])

              S.barrier()
              cur[0] = C0
              gatesT = alloc("gatesT", [16, NT], F32)
              wgs = [alloc("wgs%d" % i, [128, 8, 256], BF16) for i in range(2)]
              wus = [alloc("wus%d" % i, [128, 8, 256], BF16) for i in range(2)]
              wds = [alloc("wds%d" % i, [128, 2, D], BF16) for i in range(2)]
              sgm = [alloc("sgm0", [128, 2, 512], F32)]
              hgm = [alloc("hgm%d" % i, [128, 2, 512], BF16) for i in range(4)]
              o_ = OT0
              for i in range(2, 4):
                  wgs.append(alloc("wgs%d" % i, [128, 8, 256], BF16, at=o_)); o_ += 4096
                  wus.append(alloc("wus%d" % i, [128, 8, 256], BF16, at=o_)); o_ += 4096
                  wds.append(alloc("wds%d" % i, [128, 2, D], BF16, at=o_)); o_ += 4096
              sgm.append(alloc("sgm1", [128, 2, 512], F32, at=o_)); o_ += 4096
              gsb = []
              for i in range(2):
                  gsb.append(alloc("gsb%d" % i, [128, 512], F32, at=o_)); o_ += 2048
              assert o_ <= OT0 + 36864
              R_ewg = [Res("ewg%d" % i) for i in range(4)]
              R_ewu = [Res("ewu%d" % i) for i in range(4)]
              R_ewd = [Res("ewd%d" % i) for i in range(4)]
              R_sgm = [Res("sgm0"), Res("sgm1")]
              R_hgm = [Res("hgm%d" % i) for i in range(4)]
              R_gsb = [Res("gsb0"), Res("gsb1")]
              mblocks = blocks512 if l == 0 else lat512

              def load_expert(ex, slot, l=l):
                  S.add("pool", (lambda e: e.dma_start(out=wgs[slot][:], in_=ewg[l, ex].rearrange("(kc p) f -> p kc f", p=128))), writes=[R_ewg[slot]], dma=True)
                  S.add("pool", (lambda e: e.dma_start(out=wus[slot][:], in_=ewu[l, ex].rearrange("(kc p) f -> p kc f", p=128))), writes=[R_ewu[slot]], dma=True)
                  S.add("pool", (lambda e: e.dma_start(out=wds[slot][:], in_=ewd[l, ex].rearrange("(kc p) f -> p kc f", p=128))), writes=[R_ewd[slot]], dma=True)

              def emit_front(ex, slot, t0, N, si):
                  S.add("pe", (lambda e: e.matmul(bank(4)[:, 0:N], lhsT=selt[0:16, ex * 128:(ex + 1) * 128], rhs=gatesT[0:16, t0:t0 + N], start=True, stop=True)),
                        reads=[R_rout, R_const], writes=[RB[4]])
                  S.add("act", (lambda e: e.copy(out=gsb[si][:, 0:N], in_=bank(4)[:, 0:N])), reads=[RB[4]], writes=[R_gsb[si]])
                  for oc in range(4):
                      wsrc = wgs[slot] if oc < 2 else wus[slot]
                      rw_ = R_ewg[slot] if oc < 2 else R_ewu[slot]
                      for kc in range(8):
                          S.add("pe", (lambda e, kc=kc, oc=oc, wsrc=wsrc: e.matmul(bank(oc)[:, 0:N], lhsT=wsrc[:, kc, (oc % 2) * 128:(oc % 2 + 1) * 128], rhs=UT[:, kc, t0:t0 + N],
                                                                                    start=(kc == 0), stop=(kc == 7))),
                                reads=[rw_] + rtok("UT", t0, t0 + N), writes=[RB[oc]])
                  S.add("act", (lambda e: e.activation(out=sgm[si][:, :, 0:N], in_=PS[0][:].rearrange("p (j n) -> p j n", j=2)[:, :, 0:N], func=AF.Silu)),
                        reads=[RB[0], RB[1]], writes=[R_sgm[si]])
                  S.add("dve", (lambda e: e.tensor_tensor(out=sgm[si][:, :, 0:N], in0=sgm[si][:, :, 0:N], in1=PS[1][:].rearrange("p (j n) -> p j n", j=2)[:, :, 0:N], op=ALU.mult)),
                        reads=[RB[2], RB[3], R_sgm[si]], writes=[R_sgm[si]])

              def emit_gate(si, q, N):
                  S.add("dve", (lambda e: e.tensor_tensor(out=hgm[q][:, :, 0:N], in0=sgm[si][:, :, 0:N], in1=gsb[si][:, 0:N].unsqueeze(1).broadcast_to([128, 2, N]), op=ALU.mult)),
                        reads=[R_gsb[si], R_sgm[si]], writes=[R_hgm[q]])

              ybc = [0]

              def make_yhalf(half, slots, qs, t0, N, mc, l=l):
                  def f():
                      for dc in range(half * 4, half * 4 + 4):
                          bk = 5 + ybc[0] % 3
                          ybc[0] += 1
                          for j in range(2):
                              for k2 in range(2):
                                  S.add("pe", (lambda e, j=j, k2=k2, dc=dc, bk=bk: e.matmul(bank(bk)[:, 0:N], lhsT=wds[slots[j]][:, k2, dc * 128:(dc + 1) * 128], rhs=hgm[qs[j]][:, k2, 0:N],
                                                                                           start=(j == 0 and k2 == 0), stop=(j == 1 and k2 == 1))),
                                        reads=[R_ewd[slots[j]], R_hgm[qs[j]]], writes=[RB[bk]])
                          S.add("dve", (lambda e, dc=dc, bk=bk: e.scalar_tensor_tensor(out=hres[:, dc, t0:t0 + N], in0=bank(bk)[:, 0:N], scalar=modc(l, 5, dc, mc),
                                                                                      in1=hres[:, dc, t0:t0 + N], op0=ALU.mult, op1=ALU.add)),
                                reads=[RB[bk], R_mod] + rtok("hres%d" % dc, t0, t0 + N), writes=rtok("hres%d" % dc, t0, t0 + N))
                  return f

              load_expert(0, 0)
              load_expert(1, 1)
              pendA = pendB = None
              it = 0
              for pr in range(8):
                  slots = (2 * (pr % 2), 2 * (pr % 2) + 1)
                  for bi, (t0, N) in enumerate(mblocks):
                      qs = ((it % 2) * 2, (it % 2) * 2 + 1)
                      it += 1
                      mc = mcol(t0)
                      emit_front(2 * pr, slots[0], t0, N, 0)
                      if pendA is not None:
                          pendA()
                      emit_gate(0, qs[0], N)
                      emit_front(2 * pr + 1, slots[1], t0, N, 1)
                      if pendB is not None:
                          pendB()
                      emit_gate(1, qs[1], N)
                      pendA = make_yhalf(0, slots, qs, t0, N, mc)
                      pendB = make_yhalf(1, slots, qs, t0, N, mc)
                      if bi == 0 and pr + 1 < 8:
                          nslots = (2 * ((pr + 1) % 2), 2 * ((pr + 1) % 2) + 1)
                          load_expert(2 * pr + 2, nslots[0])
                          load_expert(2 * pr + 3, nslots[1])
              pendA()
              pendB()

              dump("S5", hres[:])
              S.barrier()
              cur[0] = C0
              pre2 = alloc("pre2", [128, 8, 256], BF16)
              presq2 = alloc("presq2", [128, 8, 256], BF16)
              mean2 = alloc("mean2", [128, 256], F32)
              rstd2 = alloc("rstd2", [128, 256], F32)
              otile = [alloc("otile%d" % i, [128, D], F32) for i in range(2)]
              R_l2 = [Res("pre2"), Res("presq2"), Res("mean2"), Res("rstd2")]
              R_ot = [Res("ot0"), Res("ot1")]
              oi = 0
              for bi, (t0, N) in enumerate(tok_blocks):
                  hap = hres[:, :, t0:t0 + 256]
                  rh = [r_ for ch_ in range(8) for r_ in rtok("hres%d" % ch_, t0, t0 + 256)]
                  ln_stats(hap, 8, 256, ones1k, pre2, presq2, mean2, rstd2, 4, rh, R_l2)
                  normalize(hap, 8, 256, mean2, rstd2, rh, R_l2)
                  if l == 0:
                      for ch in range(8):
                          affine(UT[:, ch, t0:t0 + 256], hres[:, ch, t0:t0 + 256], V("GU", ch, t0), V("BU", ch, t0), rh + [R_vecs], rtok("UT", t0, t0 + 256))
                      for ch in range(8):
                          affine(hres[:, ch, t0:t0 + 256], hres[:, ch, t0:t0 + 256], smc("ln_g", 8 + ch), smc("ln_b", 8 + ch), rh + [R_const], rh)
                      S.add("sp", (lambda e, t0=t0: e.dma_start(out=Hs3[:, :, t0:t0 + 256], in_=hres[:, :, t0:t0 + 256])), reads=rh, writes=rtok("Hs", t0, t0 + 256), dma=True)
                      if debug and nlayers == 1:
                          S.add("sp", (lambda e, t0=t0: e.dma_start(out=dbg.rearrange("p (c t) -> p c t", c=8)[:, :, t0:t0 + 256], in_=hres[:, :, t0:t0 + 256])), reads=rh,
                                writes=[Res("dbgo")], dma=True)
                  else:
                      for ch in range(8):
                          affine(hres[:, ch, t0:t0 + 256], hres[:, ch, t0:t0 + 256], smc("ln_g", 24 + ch), smc("ln_b", 24 + ch), rh + [R_const], rh)
                      for hh in range(2):
                          tk = t0 + hh * 128
                          so = oi % 2
                          oi += 1
                          for ch in range(8):
                              bk = so * 2 + ch // 4
                              S.add("pe", (lambda e, ch=ch, bk=bk, tk=tk: e.transpose(bank(bk)[:, (ch % 4) * 128:(ch % 4 + 1) * 128], hres[:, ch, tk:tk + 128], id32[:])),
                                    reads=rh + [R_const], writes=[RB[bk]])
                          for hf in range(2):
                              bk = so * 2 + hf
                              S.add("act" if hf else "dve", (lambda e, so=so, hf=hf, bk=bk: (e.copy if hf else e.tensor_copy)(out=otile[so][:, hf * 512:(hf + 1) * 512], in_=bank(bk))),
                                    reads=[RB[bk]], writes=[R_ot[so]])
                          S.add("sp", (lambda e, so=so, tk=tk, b=b: e.dma_start(out=outd[b, tk - NCTX:tk - NCTX + 128, :], in_=otile[so][:])), reads=[R_ot[so]], writes=[Res("outw")], dma=True)
    except _Stop:
        pass
    S.barrier()

    with nc.Block() as block:
        @block.tensor
        def _(e):
            S.emit_one("pe", e, esem, dsems)

        @block.scalar
        def _(e):
            S.emit_one("act", e, esem, dsems)

        @block.vector
        def _(e):
            S.emit_one("dve", e, esem, dsems)

        @block.gpsimd
        def _(e):
            S.emit_one("pool", e, esem, dsems)

        @block.sync
        def _(e):
            S.emit_one("sp", e, esem, dsems)
    es.close()
    return nc


def _prep_shared(inp):
    f = lambda a: np.ascontiguousarray(np.asarray(a, np.float32))
    sm = np.zeros((128, SMN), np.float32)

    def put(name, arr):
        arr = np.asarray(arr, np.float32)
        sm[:, SMO[name]:SMO[name] + arr.shape[1]] = arr

    put("ada_b0", _fm(inp["ada_b"][0]))
    put("ada_b1", _fm(inp["ada_b"][1]))
    put("ln_g", np.concatenate([_fm(inp["ln_g"][l, k]) for l in range(2) for k in range(2)], axis=1))
    put("ln_b", np.concatenate([_fm(inp["ln_b"][l, k]) for l in range(2) for k in range(2)], axis=1))
    b_in = np.asarray(inp["ab_b_in"][0], np.float32)
    put("b_in", _fm(b_in[:2048]))
    cw = np.asarray(inp["conv_w"][0], np.float32)
    put("conv_w", np.ascontiguousarray(cw.T.reshape(4, 128, 31).transpose(1, 0, 2).reshape(128, 124)))
    put("conv_b", _fm(inp["conv_b"][0]))
    put("cln_g", _fm(inp["conv_ln_g"][0]))
    put("cln_b", _fm(inp["conv_ln_b"][0]))
    put("b_out", _fm(inp["ab_b_out"][0]))
    sm[:, SMO["eps"]] = EPS
    qidx = _gqa_qidx()
    gw = np.asarray(inp["gqa_w_in"][0], np.float32)
    wq = gw[:, :1024]
    wkk = gw[:, 1024:1280]
    wvv = gw[:, 1280:1536]
    C, Sg = _rope_tables()
    kk = np.arange(128)[:, None]
    qq = np.arange(128)[None, :]
    maskL = np.where(kk >= qq, 0.0, NEG).astype(np.float32)
    maskU = np.where(kk <= qq, 0.0, NEG).astype(np.float32)
    sink = np.asarray(inp["gqa_sink"][0], np.float32)
    sperm = np.array([8 * m + 4 * sh + j for m in range(2) for sh in range(2) for j in range(4)])
    sel = np.zeros((16, 16, 128), np.float32)
    for ex in range(16):
        sel[ex, ex, :] = 1.0
    wr = np.stack([np.concatenate([np.asarray(inp["router_group"][l], np.float32), np.asarray(inp["router_expert"][l], np.float32)], axis=1)
                   .reshape(8, 128, 20).transpose(1, 0, 2).reshape(128, 160) for l in range(2)])
    bv = b_in[2048:2560]
    shared = {
        "ada_w": f(inp["ada_w"]),
        "sm": sm,
        "w_in0": f(inp["ab_w_in"][0]),
        "bvbc": np.ascontiguousarray(np.broadcast_to(bv[None, :], (128, 512))),
        "nab": np.ascontiguousarray(_na_bias_table(np.asarray(inp["na_rpb"][0], np.float32)).reshape(8, 128, 21 * 128)),
        "w_out0": f(inp["ab_w_out"][0]),
        "wq1": f(wq[:, qidx]),
        "wqs1": f(wq[:, qidx][:, _swap64(1024)]),
        "wk1": f(wkk),
        "wks1": f(wkk[:, _swap64(256)]),
        "wv1": f(wvv),
        "w_out1": f(np.asarray(inp["gqa_w_out"][0], np.float32)[qidx, :]),
        "ropeC": C,
        "ropeS": Sg,
        "maskLU": np.ascontiguousarray(np.concatenate([maskL, maskU], axis=1)),
        "sinkbc": np.ascontiguousarray(np.broadcast_to(sink[sperm][None, :], (128, 16))),
        "wr": f(wr),
        "sel": np.ascontiguousarray(sel.reshape(16, 2048)),
        "ident": np.eye(128, dtype=np.float32),
        "ewg": f(inp["exp_w_gate"]),
        "ewu": f(inp["exp_w_up"]),
        "ewd": f(inp["exp_w_down"]),
    }
    return shared


def _core_inputs(inp, shared, i):
    x = np.asarray(inp["x"], np.float32)
    ctx = np.asarray(inp["ctx"], np.float32)
    c = np.asarray(inp["c"], np.float32)
    cc = np.stack([c[2 * i], c[2 * i + 1], np.asarray(inp["c_ctx"], np.float32)])
    cvec = np.ascontiguousarray(cc.reshape(3, 8, 128).transpose(2, 1, 0).reshape(128, 24))
    m = dict(shared)
    m["x2"] = np.ascontiguousarray(x[2 * i:2 * i + 2])
    m["ctx2"] = np.ascontiguousarray(ctx[2 * i:2 * i + 2])
    m["cvec"] = cvec
    return m


_NC_CACHE = {}


def kernel(**inputs):
    n = 8
    if "nc" not in _NC_CACHE:
        _NC_CACHE["nc"] = build()
    nc = _NC_CACHE["nc"]
    shared = _prep_shared(inputs)
    in_maps = [_core_inputs(inputs, shared, i) for i in range(n)]
    res = run_bass_kernel_spmd(nc, in_maps, core_ids=list(range(n)))
    out = np.concatenate([np.asarray(r["out"], np.float32) for r in res.results], axis=0)
    return out
```

```python
import numpy as np
from contextlib import ExitStack
import concourse.bass as bass
import concourse.mybir as mybir
from concourse.bass_utils import run_bass_kernel_spmd

F32 = mybir.dt.float32
BF16 = mybir.dt.bfloat16
AF = mybir.ActivationFunctionType
ALU = mybir.AluOpType
AX = mybir.AxisListType

D = 1024
SEQ = 2048
NCTX = 256
NT = SEQ + NCTX
GW = 64
ALPHA = 4.0 ** 0.25
EPS = 1e-5
NEG = -1e30

ENGS = ("pe", "act", "dve", "pool", "sp")
N_DMA_SEMS = 40


class Res:
    __slots__ = ("name", "last_w", "readers")

    def __init__(self, name):
        self.name = name
        self.last_w = None
        self.readers = []


class Op:
    __slots__ = ("eng", "fn", "idx", "deps", "dma", "sig", "semval", "dsem", "dval", "dprev")

    def __init__(self, eng, fn, idx, dma):
        self.eng = eng
        self.fn = fn
        self.idx = idx
        self.deps = []
        self.dma = dma
        self.sig = False
        self.semval = 0
        self.dsem = -1
        self.dval = 0
        self.dprev = 0


class Sched:
    def __init__(self):
        self.ops = {e: [] for e in ENGS}
        self.ndma = 0
        self.dma_tot = [0] * N_DMA_SEMS
        self.last_dma = [None] * N_DMA_SEMS
        self._assigned = False

    def add(self, eng, fn, reads=(), writes=(), dma=False, extra=()):
        lst = self.ops[eng]
        op = Op(eng, fn, len(lst), dma)
        deps = {}
        for r in reads:
            if r.last_w is not None:
                deps[id(r.last_w)] = r.last_w
        for w in writes:
            if w.last_w is not None:
                deps[id(w.last_w)] = w.last_w
            for rd in w.readers:
                deps[id(rd)] = rd
        for x in extra:
            deps[id(x)] = x
        for r in reads:
            r.readers.append(op)
        for w in writes:
            w.last_w = op
            w.readers = []
        if dma:
            s = self.ndma % N_DMA_SEMS
            self.ndma += 1
            op.dsem = s
            op.dprev = self.dma_tot[s]
            self.dma_tot[s] += 16
            op.dval = self.dma_tot[s]
            self.last_dma[s] = op
        for d in deps.values():
            if d is op:
                continue
            if d.eng == eng and not d.dma and not dma:
                if eng == "pe":
                    continue
                if op.idx - d.idx > 2:
                    continue
            op.deps.append(d)
            if not d.dma:
                d.sig = True
        lst.append(op)
        return op

    def barrier(self):
        lasts = []
        for e in ENGS:
            for op in reversed(self.ops[e]):
                if not op.dma:
                    lasts.append(op)
                    break
        dl = [o for o in self.last_dma if o is not None]
        for e in ENGS:
            self.add(e, lambda eng: eng.nop(), extra=[o for o in lasts if o.eng != e] + dl)

    def emit_one(self, e, eng, esem, dsems):
        if not self._assigned:
            for ee in ENGS:
                c = 0
                for op in self.ops[ee]:
                    if op.sig and not op.dma:
                        c += 1
                        op.semval = c
            self._assigned = True
        seen = {}
        for op in self.ops[e]:
            need = {}
            for d in op.deps:
                if d.dma:
                    key = ("d", d.dsem)
                    val = d.dval
                else:
                    key = ("e", d.eng)
                    val = d.semval
                if val > need.get(key, 0):
                    need[key] = val
            if op.dma and op.dprev > 0:
                key = ("d", op.dsem)
                if op.dprev > need.get(key, 0):
                    need[key] = op.dprev
            for key, val in need.items():
                if seen.get(key, 0) >= val:
                    continue
                seen[key] = val
                sem = dsems[key[1]] if key[0] == "d" else esem[key[1]]
                eng.wait_ge(sem, val)
            ins = op.fn(eng)
            if op.dma:
                ins.then_inc(dsems[op.dsem], 16)
            elif op.sig:
                ins.then_inc(esem[e], 1)


def _sm_layout():
    off = {}
    n = 0
    for name, cols in (("ada_b0", 48), ("ada_b1", 48), ("ln_g", 32), ("ln_b", 32), ("b_in", 16),
                       ("conv_w", 124), ("conv_b", 4), ("cln_g", 4), ("cln_b", 4), ("b_out", 8), ("eps", 1)):
        off[name] = n
        n += cols
    return off, n


SMO, SMN = _sm_layout()


def _fm(v):
    v = np.asarray(v, np.float32)
    return np.ascontiguousarray(v.reshape(-1, 128).T)


def _gqa_qidx():
    idx = np.zeros(1024, np.int64)
    for c in range(8):
        m, j = divmod(c, 4)
        h0 = 8 * m + j
        h1 = 8 * m + 4 + j
        idx[c * 128:c * 128 + 64] = h0 * 64 + np.arange(64)
        idx[c * 128 + 64:c * 128 + 128] = h1 * 64 + np.arange(64)
    return idx


def _swap64(n):
    d = np.arange(n)
    dd = d % 64
    sw = np.where(dd % 32 < 16, dd + 16, dd - 16)
    return (d // 64) * 64 + sw


def _na_tiles(j):
    if j in (0, 1):
        return [0, 1, 2, 3]
    if j in (14, 15):
        return [12, 13, 14, 15]
    return [j - 2, j - 1, j, j + 1, j + 2]


def _na_tile_index(j):
    if j == 0:
        return 5
    if j == 1:
        return 9
    if j == 14:
        return 13
    if j == 15:
        return 17
    return 0


def _na_bias_table(rpb):
    rows = 32
    r = np.arange(rows)
    row_start = np.clip(r - 4, 0, rows - 8)
    jj = np.arange(GW)
    col_start = np.clip(jj - 8, 0, GW - 16)
    col_in = (jj[None, :] >= col_start[:, None]) & (jj[None, :] < col_start[:, None] + 16)
    col_off = np.clip(jj[None, :] - jj[:, None], -15, 15) + 15
    out = np.full((8, 21, 128, 128), NEG, np.float32)

    def tile(j, a):
        t = np.full((8, 128, 128), NEG, np.float32)
        for pk in range(2):
            rk = 2 * a + pk
            for pq in range(2):
                rq = 2 * j + pq
                if not (row_start[rq] <= rk < row_start[rq] + 8):
                    continue
                ro = rk - rq + 7
                blk = rpb[:, ro][:, col_off]
                blk = np.where(col_in[None], blk, np.float32(NEG))
                t[:, pk * 64:(pk + 1) * 64, pq * 64:(pq + 1) * 64] = blk.transpose(0, 2, 1)
        return t

    for i, a in enumerate(_na_tiles(5)):
        out[:, i] = tile(5, a)
    for j in (0, 1, 14, 15):
        base = _na_tile_index(j)
        for i, a in enumerate(_na_tiles(j)):
            out[:, base + i] = tile(j, a)
    return np.ascontiguousarray(out.transpose(0, 2, 1, 3))


def _rope_tables():
    t = np.arange(SEQ)
    row = (t // GW).astype(np.float32)
    col = (t % GW).astype(np.float32)
    inv = (np.float32(10000.0) ** (-np.arange(0, 32, 2, dtype=np.float32) / np.float32(32))).astype(np.float32)
    ang = np.concatenate([row[:, None] * inv, col[:, None] * inv], axis=-1).astype(np.float32)
    cos = np.cos(ang).astype(np.float32)
    sin = np.sin(ang).astype(np.float32)
    p = np.arange(128)
    d = p % 64
    ai = (d // 32) * 16 + d % 16
    sgn = np.where(d % 32 < 16, -1.0, 1.0).astype(np.float32)
    C = np.ascontiguousarray(cos[:, ai].T)
    S = np.ascontiguousarray((sin[:, ai] * sgn[None, :]).T)
    return C.astype(np.float32), S.astype(np.float32)


class _Stop(Exception):
    pass


def build(nlayers=2, nb=2, debug=False, stop=None):
    nc = bass.Bass("TRN2", target_bir_lowering=False)
    S = Sched()

    def din(name, shape):
        return nc.dram_tensor(name, list(shape), F32, kind="ExternalInput").ap()

    x2 = din("x2", [2, SEQ, D])
    ctx2 = din("ctx2", [2, NCTX, D])
    cvec = din("cvec", [128, 24])
    ada_w = din("ada_w", [2, D, 6 * D])
    smd = din("sm", [128, SMN])
    w_in0 = din("w_in0", [D, 2560])
    bvbc = din("bvbc", [128, 512])
    nab = din("nab", [8, 128, 21 * 128])
    w_out0 = din("w_out0", [D, D])
    wq1 = din("wq1", [D, 1024])
    wqs1 = din("wqs1", [D, 1024])
    wk1 = din("wk1", [D, 256])
    wks1 = din("wks1", [D, 256])
    wv1 = din("wv1", [D, 256])
    w_out1 = din("w_out1", [D, D])
    ropeC = din("ropeC", [128, SEQ])
    ropeS = din("ropeS", [128, SEQ])
    maskLU = din("maskLU", [128, 256])
    sinkbc = din("sinkbc", [128, 16])
    wr = din("wr", [2, 128, 160])
    sel = din("sel", [16, 2048])
    ident = din("ident", [128, 128])
    ewg = din("ewg", [2, 16, D, 256])
    ewu = din("ewu", [2, 16, D, 256])
    ewd = din("ewd", [2, 16, 256, D])
    outd = nc.dram_tensor("out", [2, SEQ, D], F32, kind="ExternalOutput").ap()
    Hs = nc.dram_tensor("Hs", [128, 8 * NT], F32, kind="Internal").ap()
    dbg = nc.dram_tensor("dbg", [128, 8 * NT], F32, kind="ExternalOutput").ap() if debug else None
    dbgb = nc.dram_tensor("dbgb", [128, 8 * NT], BF16, kind="ExternalOutput").ap() if debug else None
    Hs3 = Hs.rearrange("p (c t) -> p c t", c=8)

    es = ExitStack()
    cur = [16640]

    acache = {}

    def alloc(name, shape, dt, at=None):
        nbytes = int(np.prod(shape[1:])) * (4 if dt == F32 else 2)
        if at is None:
            at = cur[0]
            cur[0] = (at + nbytes + 63) // 64 * 64
        assert at + nbytes <= 229376, (name, at, nbytes)
        key = (name, at, tuple(shape))
        if key not in acache:
            acache[key] = nc.alloc_sbuf_tensor_at("%s_%d" % (name, len(acache)), list(shape), dt, offset=at)
        return acache[key]

    sm = alloc("sm", [128, SMN], F32)
    id32 = alloc("id32", [128, 128], F32)
    ones1k = alloc("ones1k", [128, 128], BF16)
    ones512 = alloc("ones512", [128, 128], BF16)
    csil = alloc("csil", [128, 24], BF16)
    cv32 = alloc("cv32", [128, 24], F32)
    mod = alloc("mod", [128, 2 * 144], F32)
    mp1 = alloc("mp1", [128, 2 * 144], F32)
    vecs = alloc("vecs", [128, 128], F32)
    selt = alloc("selt", [16, 2048], F32)
    wrt = alloc("wrt", [128, 320], F32)
    esink = alloc("esink", [128, 16], F32)
    R_const = Res("const")
    R_mod = Res("mod")
    R_vecs = Res("vecs")
    base0 = cur[0]

    PS = [es.enter_context(nc.psum_tensor("ps%d" % i, [128, 1024], F32)) for i in range(4)]
    RB = [Res("bank%d" % i) for i in range(8)]

    def bank(k):
        return PS[k // 2][:, (k % 2) * 512:(k % 2) * 512 + 512]

    esem = {e: es.enter_context(nc.semaphore("es_" + e)) for e in ENGS}
    dsems = [es.enter_context(nc.semaphore("ds%d" % i)) for i in range(N_DMA_SEMS)]

    def smc(name, j, n=1):
        o = SMO[name] + j
        return sm[:, o:o + n]

    def modc(l, k, ch, col):
        o = l * 144 + (k * 8 + ch) * 3 + col
        return mod[:, o:o + 1]

    def mp1c(l, k, ch, col):
        o = l * 144 + (k * 8 + ch) * 3 + col
        return mp1[:, o:o + 1]

    VK = {}

    def vslot(kind, ch):
        key = (kind, ch)
        if key not in VK:
            VK[key] = len(VK)
            assert len(VK) <= 128
        o = VK[key]
        return vecs[:, o:o + 1]

    S.add("sp", lambda e: e.dma_start(out=sm[:], in_=smd), writes=[R_const], dma=True)
    S.add("sp", lambda e: e.dma_start(out=id32[:], in_=ident), writes=[R_const], dma=True)
    S.add("sp", lambda e: e.dma_start(out=cv32[:], in_=cvec), writes=[R_const], dma=True)
    S.add("sp", lambda e: e.dma_start(out=selt[:], in_=sel), writes=[R_const], dma=True)
    S.add("sp", lambda e: e.dma_start(out=wrt[:].rearrange("p (l n) -> p l n", l=2), in_=wr.rearrange("l p n -> p l n")), writes=[R_const], dma=True)
    S.add("sp", lambda e: e.dma_start(out=esink[:], in_=sinkbc), writes=[R_const], dma=True)
    S.add("pool", lambda e: e.memset(ones1k[:], 1.0 / 1024.0), writes=[R_const])
    S.add("pool", lambda e: e.memset(ones512[:], 1.0 / 512.0), writes=[R_const])
    S.add("act", lambda e: e.activation(out=csil[:], in_=cv32[:], func=AF.Silu), reads=[R_const], writes=[R_const])
    S.add("act", lambda e: e.activation(out=esink[:], in_=esink[:], func=AF.Exp), reads=[R_const], writes=[R_const])

    adaw = [alloc("adaw%d" % i, [128, 8, 1024], BF16) for i in range(2)]
    R_adaw = [Res("adaw0"), Res("adaw1")]
    pi = 0
    for l in range(nlayers):
        awl = ada_w[l].rearrange("(kc p) n -> p kc n", p=128)
        for piece in range(6):
            s = pi % 2
            pi += 1
            S.add("pool", (lambda e, s=s, awl=awl, piece=piece: e.dma_start(out=adaw[s][:], in_=awl[:, :, piece * 1024:(piece + 1) * 1024])),
                  writes=[R_adaw[s]], dma=True)
            for oc8 in range(8):
                oc = piece * 8 + oc8
                for kc in range(8):
                    S.add("pe", (lambda e, s=s, oc=oc, oc8=oc8, kc=kc: e.matmul(bank(0)[:, oc * 3:oc * 3 + 3], lhsT=adaw[s][:, kc, oc8 * 128:(oc8 + 1) * 128],
                                                                                    rhs=csil[:, kc * 3:kc * 3 + 3], start=(kc == 0), stop=(kc == 7))),
                          reads=[R_adaw[s], R_const], writes=[RB[0]])
        ab = smc("ada_b%d" % l, 0, 48)
        S.add("dve", (lambda e, l=l, ab=ab: e.tensor_tensor(out=mod[:, l * 144:(l + 1) * 144].rearrange("p (a b) -> p a b", b=3),
                                                             in0=bank(0)[:, 0:144].rearrange("p (a b) -> p a b", b=3),
                                                             in1=ab.unsqueeze(2).broadcast_to([128, 48, 3]), op=ALU.add)),
              reads=[RB[0], R_const], writes=[R_mod])
        S.add("dve", (lambda e, l=l: e.tensor_scalar_add(out=mp1[:, l * 144:(l + 1) * 144], in0=mod[:, l * 144:(l + 1) * 144], scalar1=1.0)),
              reads=[R_mod], writes=[R_mod])
    S.barrier()
    cur[0] = base0

    A0 = cur[0]
    hres = alloc("hres", [128, 8, NT], F32)
    qT = alloc("qT", [128, 4, NT], BF16, at=A0)
    kT = alloc("kT", [128, 4, NT], BF16, at=A0 + 18432)
    vaug = alloc("vaug", [128, 18, 4, 192], BF16, at=A0 + 36864)
    qT1 = alloc("qT1", [128, 8, SEQ], BF16, at=A0)
    kT1 = alloc("kT1", [128, 2, NT], BF16, at=A0 + 32768)
    vaug1 = alloc("vaug1", [128, 18, 2, 192], BF16, at=A0 + 41984)
    rC = alloc("rC", [128, SEQ], F32, at=A0 + 55808)
    rS = alloc("rS", [128, SEQ], F32, at=A0 + 55808 + 8192)
    bvb = alloc("bvb", [128, 512], F32, at=A0 + 64512)
    idb = alloc("idb", [128, 128], BF16, at=A0 + 64512 + 2048)
    cmean = alloc("cmean", [128, 256], F32, at=A0 + 64512 + 2304)
    crstd = alloc("crstd", [128, 256], F32, at=A0 + 64512 + 3328)
    UT = alloc("UT", [128, 8, NT], BF16)
    OT0 = cur[0]
    oT = alloc("oT", [128, 8, NT], BF16)
    C0 = cur[0]
    RT = {}

    def rtok(name, t0, t1):
        out = []
        for tt in range(t0 // 128, (t1 + 127) // 128):
            key = (name, tt)
            if key not in RT:
                RT[key] = Res("%s_%d" % key)
            out.append(RT[key])
        return out

    R_hpad = [Res("hpad%d" % c) for c in range(4)]

    def derive_vecs(l, col, tag):
        ops = []
        for ch in range(8):
            g1 = smc("ln_g", (l * 2 + 0) * 8 + ch)
            b1 = smc("ln_b", (l * 2 + 0) * 8 + ch)
            g2 = smc("ln_g", (l * 2 + 1) * 8 + ch)
            b2 = smc("ln_b", (l * 2 + 1) * 8 + ch)
            S.add("dve", (lambda e, ch=ch, g1=g1: e.tensor_tensor(out=vslot((tag, "G4"), ch), in0=g1, in1=mp1c(l, 4, ch, col), op=ALU.mult)),
                  reads=[R_const, R_mod], writes=[R_vecs])
            S.add("dve", (lambda e, ch=ch, b1=b1: e.scalar_tensor_tensor(out=vslot((tag, "B4"), ch), in0=b1, scalar=mp1c(l, 4, ch, col), in1=modc(l, 3, ch, col),
                                                                         op0=ALU.mult, op1=ALU.add)),
                  reads=[R_const, R_mod], writes=[R_vecs])
            S.add("dve", (lambda e, ch=ch, g1=g1: e.tensor_scalar_mul(out=vslot((tag, "GA1"), ch), in0=g1, scalar1=ALPHA)), reads=[R_const], writes=[R_vecs])
            S.add("dve", (lambda e, ch=ch, b1=b1: e.tensor_scalar_mul(out=vslot((tag, "BA1"), ch), in0=b1, scalar1=ALPHA)), reads=[R_const], writes=[R_vecs])
            if l == 0:
                S.add("dve", (lambda e, ch=ch: e.tensor_tensor(out=vslot((tag, "M2B"), ch), in0=modc(l, 2, ch, col), in1=smc("b_out", ch), op=ALU.mult)),
                      reads=[R_const, R_mod], writes=[R_vecs])
                S.add("dve", (lambda e, ch=ch, g2=g2: e.tensor_tensor(out=vslot((tag, "GU"), ch), in0=g2, in1=mp1c(1, 1, ch, col), op=ALU.mult)),
                      reads=[R_const, R_mod], writes=[R_vecs])
                S.add("dve", (lambda e, ch=ch, b2=b2: e.scalar_tensor_tensor(out=vslot((tag, "BU"), ch), in0=b2, scalar=mp1c(1, 1, ch, col), in1=modc(1, 0, ch, col),
                                                                             op0=ALU.mult, op1=ALU.add)),
                      reads=[R_const, R_mod], writes=[R_vecs])

    def ln_part1(pre_ap, nch, N, ones_t, prebf, presq, bk, r_pre, r_tmp):
        S.add("dve", lambda e: e.tensor_copy(out=prebf[:, 0:nch, 0:N], in_=pre_ap), reads=r_pre, writes=[r_tmp[0]])
        S.add("act", lambda e: e.activation(out=presq[:, 0:nch, 0:N], in_=pre_ap, func=AF.Square), reads=r_pre, writes=[r_tmp[1]])
        for c in range(nch):
            S.add("pe", (lambda e, c=c: e.matmul(bank(bk)[:, 0:N], lhsT=ones_t[:], rhs=prebf[:, c, 0:N], start=(c == 0), stop=(c == nch - 1))),
                  reads=[r_tmp[0], R_const], writes=[RB[bk]])
        for c in range(nch):
            S.add("pe", (lambda e, c=c: e.matmul(bank(bk)[:, 256:256 + N], lhsT=ones_t[:], rhs=presq[:, c, 0:N], start=(c == 0), stop=(c == nch - 1))),
                  reads=[r_tmp[1], R_const], writes=[RB[bk]])

    def ln_part2(N, mean_sb, rstd_sb, bk, r_tmp):
        S.add("act", lambda e: e.copy(out=mean_sb[:, 0:N], in_=bank(bk)[:, 0:N]), reads=[RB[bk]], writes=[r_tmp[2]])
        S.add("dve", lambda e: e.tensor_tensor(out=rstd_sb[:, 0:N], in0=mean_sb[:, 0:N], in1=mean_sb[:, 0:N], op=ALU.mult), reads=[r_tmp[2]], writes=[r_tmp[3]])
        S.add("dve", lambda e: e.tensor_tensor(out=rstd_sb[:, 0:N], in0=bank(bk)[:, 256:256 + N], in1=rstd_sb[:, 0:N], op=ALU.subtract),
              reads=[RB[bk], r_tmp[3]], writes=[r_tmp[3]])
        S.add("act", lambda e: e.activation(out=rstd_sb[:, 0:N], in_=rstd_sb[:, 0:N], func=AF.Ln, bias=smc("eps", 0), scale=1.0),
              reads=[r_tmp[3], R_const], writes=[r_tmp[3]])
        S.add("act", lambda e: e.activation(out=rstd_sb[:, 0:N], in_=rstd_sb[:, 0:N], func=AF.Exp, scale=-0.5), reads=[r_tmp[3]], writes=[r_tmp[3]])

    def ln_stats(pre_ap, nch, N, ones_t, prebf, presq, mean_sb, rstd_sb, bk, r_pre, r_tmp):
        ln_part1(pre_ap, nch, N, ones_t, prebf, presq, bk, r_pre, r_tmp)
        ln_part2(N, mean_sb, rstd_sb, bk, r_tmp)

    def normalize(pre_ap, nch, N, mean_sb, rstd_sb, r_pre, r_tmp):
        S.add("dve", lambda e: e.tensor_tensor(out=pre_ap, in0=pre_ap, in1=mean_sb[:, 0:N].unsqueeze(1).broadcast_to([128, nch, N]), op=ALU.subtract),
              reads=r_pre + [r_tmp[2]], writes=r_pre)
        S.add("dve", lambda e: e.tensor_tensor(out=pre_ap, in0=pre_ap, in1=rstd_sb[:, 0:N].unsqueeze(1).broadcast_to([128, nch, N]), op=ALU.mult),
              reads=r_pre + [r_tmp[3]], writes=r_pre)

    aff_rr = [0]

    def affine(out_ap, in_ap, sc, bi, reads, writes, psum_in=False):
        k = aff_rr[0] % 2
        aff_rr[0] += 1
        if k == 0:
            S.add("act", lambda e: e.activation(out=out_ap, in_=in_ap, func=AF.Identity, bias=bi, scale=sc), reads=reads, writes=writes)
        else:
            S.add("dve" if k == 1 else "pool", lambda e: e.tensor_scalar(out=out_ap, in0=in_ap, scalar1=sc, scalar2=bi, op0=ALU.mult, op1=ALU.add),
                  reads=reads, writes=writes)

    def dump(name, src_ap3):
        if stop != name and stop != "%s@%d" % (name, cur_l[0]):
            return
        S.barrier()
        c, t = src_ap3.shape[1], src_ap3.shape[2]
        dst = dbg if src_ap3.dtype == F32 else dbgb
        for ci in range(c):
            S.add("sp", (lambda e, ci=ci: e.dma_start(out=dst[:, ci * t:(ci + 1) * t], in_=src_ap3[:, ci, :])), writes=[Res("dbgo")], dma=True)
        raise _Stop()

    cur_l = [0]
    try:
      for b in range(nb):
        for l in range(nlayers):
              cur_l[0] = l
              lat_only = (l == nlayers - 1) and l == 1
              col = b
              S.barrier()
              derive_vecs(l, b, "lat")
              if l == 0:
                  derive_vecs(l, 2, "ctx")

              def V(kind, ch, t0):
                  return vslot((("ctx" if (t0 < NCTX and l == 0) else "lat"), kind), ch)

              def mcol(t0):
                  return 2 if t0 < NCTX else b

              cur[0] = C0
              if l == 0:
                  xin = [alloc("xin%d" % i, [128, D], F32) for i in range(2)]
                  hblk = [alloc("hblk%d" % i, [128, 8, 128], F32) for i in range(2)]
                  R_xin = [Res("xin0"), Res("xin1")]
                  R_hblk = [Res("hblk0"), Res("hblk1")]
                  for tt in range(18):
                      s = tt % 2
                      src = ctx2[b, tt * 128:(tt + 1) * 128, :] if tt < 2 else x2[b, (tt - 2) * 128:(tt - 1) * 128, :]
                      S.add("sp", (lambda e, s=s, src=src: e.dma_start(out=xin[s][:], in_=src)), writes=[R_xin[s]], dma=True)
                      for ch in range(8):
                          bk = (tt % 2) * 2 + ch // 4
                          S.add("pe", (lambda e, s=s, ch=ch, bk=bk: e.transpose(bank(bk)[:, (ch % 4) * 128:(ch % 4 + 1) * 128], xin[s][:, ch * 128:(ch + 1) * 128], id32[:])),
                                reads=[R_xin[s], R_const], writes=[RB[bk]])
                      for hf in range(2):
                          bk = (tt % 2) * 2 + hf
                          S.add("act", (lambda e, s=s, hf=hf, bk=bk: e.copy(out=hblk[s][:, hf * 4:(hf + 1) * 4, :], in_=bank(bk).rearrange("p (c t) -> p c t", c=4))),
                                reads=[RB[bk]], writes=[R_hblk[s]])
                      mc = mcol(tt * 128)
                      for ch in range(8):
                          bk = (tt % 2) * 2 + ch // 4
                          affine(UT[:, ch, tt * 128:(tt + 1) * 128], bank(bk)[:, (ch % 4) * 128:(ch % 4 + 1) * 128], mp1c(0, 1, ch, mc), modc(0, 0, ch, mc),
                                 [RB[bk], R_mod], rtok("UT", tt * 128, tt * 128 + 128), psum_in=True)
                      S.add("sp", (lambda e, s=s, tt=tt: e.dma_start(out=Hs3[:, :, tt * 128:(tt + 1) * 128], in_=hblk[s][:])), reads=[R_hblk[s]],
                            writes=rtok("Hs", tt * 128, tt * 128 + 128), dma=True)

              if l == 0:
                  dump("S0", UT[:])
              blocks512 = [(0, 256)] + [(256 + 512 * i, 512) for i in range(4)]
              blocks256 = [(256 * i, 256) for i in range(9)]
              if l == 1:
                  lat512 = [(256 + 512 * i, 512) for i in range(4)]
                  lat256 = [(256 * i, 256) for i in range(1, 9)]

              if l == 0:
                  S.barrier()
                  cur[0] = C0
                  wAB = alloc("wAB", [128, 8, 1536], BF16)
                  wA = alloc("wA", [128, 8, 1024], BF16, at=C0)
                  hpd = [alloc("hpd%d" % i, [128, 2368], BF16, at=C0 + 16384 + i * 4736) for i in range(2)]
                  dg0 = alloc("diag0", [128, 31, 128], BF16, at=C0 + 25856)
                  diag = [dg0, dg0]
                  cur[0] = C0 + 33792
                  sgt = [alloc("sgt%d" % i, [128, 512], F32) for i in range(2)]
                  czsq = alloc("czsq", [128, 4, 256], BF16)
                  cz = alloc("cz", [128, 4, 256], F32)
                  R_wAB, R_misc = Res("wAB"), Res("misc0")
                  R_hpd = [Res("hpd0"), Res("hpd1")]
                  R_dg = [Res("dg0")] * 2
                  R_sgt = [Res("sgt0"), Res("sgt1")]
                  R_cz, R_ct = Res("cz"), [Res("czbf"), Res("czsq"), Res("cmean"), Res("crstd")]
                  w0 = w_in0.rearrange("(kc p) n -> p kc n", p=128)
                  S.add("pool", lambda e: e.dma_start(out=wA[:], in_=w0[:, :, 0:1024]), writes=[R_wAB], dma=True)
                  S.add("pool", lambda e: e.dma_start(out=idb[:], in_=ident), writes=[R_misc], dma=True)
                  S.add("sp", lambda e: e.dma_start(out=bvb[:], in_=bvbc), writes=[R_misc], dma=True)
                  S.add("pool", lambda e: e.memset(hpd[0][:], 0.0), writes=[R_hpd[0]])
                  S.add("pool", lambda e: e.memset(hpd[1][:], 0.0), writes=[R_hpd[1]])
                  S.add("pool", lambda e: e.memset(vaug[:], 1.0), writes=rtok("vaug", 0, NT))

                  def hoff(t0):
                      return 15 + t0 if t0 < NCTX else 286 + 15 + (t0 - NCTX)

                  it = 0
                  for cc in range(4):
                      hs_ = cc % 2
                      for k in range(31):
                          S.add("dve", (lambda e, cc=cc, k=k, hs_=hs_: e.tensor_scalar_mul(out=diag[hs_][:, k, :], in0=idb[:], scalar1=smc("conv_w", cc * 31 + k))),
                                reads=[R_misc, R_const], writes=[R_dg[hs_]])
                      for (t0, N) in blocks512:
                          s = it % 2
                          it += 1
                          b1, b2 = 2 * s, 2 * s + 1
                          for kc in range(8):
                              S.add("pe", (lambda e, kc=kc, cc=cc, t0=t0, N=N, b1=b1: e.matmul(bank(b1)[:, 0:N], lhsT=wA[:, kc, cc * 128:(cc + 1) * 128], rhs=UT[:, kc, t0:t0 + N],
                                                                                             start=(kc == 0), stop=(kc == 7))),
                                    reads=[R_wAB] + rtok("UT", t0, t0 + N), writes=[RB[b1]])
                          for kc in range(8):
                              S.add("pe", (lambda e, kc=kc, cc=cc, t0=t0, N=N, b2=b2: e.matmul(bank(b2)[:, 0:N], lhsT=wA[:, kc, 512 + cc * 128:512 + (cc + 1) * 128], rhs=UT[:, kc, t0:t0 + N],
                                                                                             start=(kc == 0), stop=(kc == 7))),
                                    reads=[R_wAB] + rtok("UT", t0, t0 + N), writes=[RB[b2]])
                          S.add("act", (lambda e, s=s, cc=cc, N=N, b2=b2: e.activation(out=sgt[s][:, 0:N], in_=bank(b2)[:, 0:N], func=AF.Sigmoid, bias=smc("b_in", 4 + cc), scale=1.0)),
                                reads=[RB[b2], R_const], writes=[R_sgt[s]])
                          ho = hoff(t0)
                          S.add("dve", (lambda e, s=s, cc=cc, N=N, b1=b1, ho=ho, hs_=hs_: e.scalar_tensor_tensor(out=hpd[hs_][:, ho:ho + N], in0=bank(b1)[:, 0:N], scalar=smc("b_in", cc),
                                                                                                                 in1=sgt[s][:, 0:N], op0=ALU.add, op1=ALU.mult)),
                                reads=[RB[b1], R_sgt[s], R_const], writes=[R_hpd[hs_]])
                      for bi, (t0, N) in enumerate(blocks512):
                          ho = hoff(t0) - 15
                          bk = 4 + bi % 2
                          for k in range(31):
                              S.add("pe", (lambda e, k=k, ho=ho, N=N, bk=bk, hs_=hs_: e.matmul(bank(bk)[:, 0:N], lhsT=diag[hs_][:, k, :], rhs=hpd[hs_][:, ho + k:ho + k + N],
                                                                                           start=(k == 0), stop=(k == 30))),
                                    reads=[R_dg[hs_], R_hpd[hs_]], writes=[RB[bk]])
                          S.add("act", (lambda e, cc=cc, N=N, t0=t0, bk=bk: e.activation(out=oT[:, cc, t0:t0 + N], in_=bank(bk)[:, 0:N], func=AF.Identity, bias=smc("conv_b", cc), scale=1.0)),
                                reads=[RB[bk], R_const], writes=rtok("oT", t0, t0 + N))
                  dump("S1z", oT[:, 0:4, :])
                  dump("S1h", hpd[1][:].unsqueeze(1))
                  dump("S1d", diag[0][:])
                  S.add("pool", lambda e: e.dma_start(out=wAB[:], in_=w0[:, :, 1024:2560]), writes=[R_wAB] + R_hpd + [R_dg[0]], dma=True)
                  for (t0, N) in blocks256:
                      zin = oT[:, 0:4, t0:t0 + 256]
                      rz = rtok("oT", t0, t0 + 256)
                      S.add("act", (lambda e, zin=zin: e.activation(out=czsq[:], in_=zin, func=AF.Square)), reads=rz, writes=[R_ct[1]])
                      for c4 in range(4):
                          S.add("pe", (lambda e, c4=c4, t0=t0: e.matmul(bank(6)[:, 0:256], lhsT=ones512[:], rhs=oT[:, c4, t0:t0 + 256], start=(c4 == 0), stop=(c4 == 3))),
                                reads=rz + [R_const], writes=[RB[6]])
                      for c4 in range(4):
                          S.add("pe", (lambda e, c4=c4: e.matmul(bank(6)[:, 256:512], lhsT=ones512[:], rhs=czsq[:, c4, :], start=(c4 == 0), stop=(c4 == 3))),
                                reads=[R_ct[1], R_const], writes=[RB[6]])
                      S.add("act", lambda e: e.copy(out=cmean[:], in_=bank(6)[:, 0:256]), reads=[RB[6]], writes=[R_ct[2]])
                      S.add("dve", lambda e: e.tensor_tensor(out=crstd[:], in0=cmean[:], in1=cmean[:], op=ALU.mult), reads=[R_ct[2]], writes=[R_ct[3]])
                      S.add("dve", lambda e: e.tensor_tensor(out=crstd[:], in0=bank(6)[:, 256:512], in1=crstd[:], op=ALU.subtract), reads=[RB[6], R_ct[3]], writes=[R_ct[3]])
                      S.add("act", lambda e: e.activation(out=crstd[:], in_=crstd[:], func=AF.Sqrt, bias=smc("eps", 0), scale=1.0), reads=[R_ct[3], R_const], writes=[R_ct[3]])
                      S.add("dve", lambda e: e.reciprocal(out=crstd[:], in_=crstd[:]), reads=[R_ct[3]], writes=[R_ct[3]])
                      S.add("dve", (lambda e, zin=zin: e.tensor_tensor(out=cz[:], in0=zin, in1=cmean[:].unsqueeze(1).broadcast_to([128, 4, 256]), op=ALU.subtract)),
                            reads=rz + [R_ct[2]], writes=[R_cz])
                      S.add("dve", lambda e: e.tensor_tensor(out=cz[:], in0=cz[:], in1=crstd[:].unsqueeze(1).broadcast_to([128, 4, 256]), op=ALU.mult),
                            reads=[R_cz, R_ct[3]], writes=[R_cz])
                      for c4 in range(4):
                          S.add("act", (lambda e, c4=c4, t0=t0: e.activation(out=oT[:, c4, t0:t0 + 256], in_=cz[:, c4, :], func=AF.Silu, bias=smc("cln_b", c4), scale=smc("cln_g", c4))),
                                reads=[R_cz, R_const], writes=rz)
                  it = 0
                  for (t0, N) in blocks512:
                      for c in range(8):
                          bk = it % 4
                          it += 1
                          for kc in range(8):
                              S.add("pe", (lambda e, kc=kc, c=c, t0=t0, N=N, bk=bk: e.matmul(bank(bk)[:, 0:N], lhsT=wAB[:, kc, c * 128:(c + 1) * 128], rhs=UT[:, kc, t0:t0 + N],
                                                                                           start=(kc == 0), stop=(kc == 7))),
                                    reads=[R_wAB] + rtok("UT", t0, t0 + N), writes=[RB[bk]])
                          dst = qT if c < 4 else kT
                          S.add("act", (lambda e, c=c, t0=t0, N=N, bk=bk, dst=dst: e.activation(out=dst[:, c % 4, t0:t0 + N], in_=bank(bk)[:, 0:N], func=AF.Identity,
                                                                                              bias=smc("b_in", 8 + c), scale=1.0)),
                                reads=[RB[bk], R_const], writes=rtok("qk", t0, t0 + N))
                  for tt in range(18):
                      bk = it % 4
                      it += 1
                      for kc in range(8):
                          S.add("pe", (lambda e, kc=kc, tt=tt, bk=bk: e.matmul(bank(bk)[:, 0:512], lhsT=UT[:, kc, tt * 128:(tt + 1) * 128], rhs=wAB[:, kc, 1024:1536],
                                                                             start=(kc == 0), stop=(kc == 7))),
                                reads=[R_wAB] + rtok("UT", tt * 128, tt * 128 + 128), writes=[RB[bk]])
                      for x in range(2):
                          S.add("dve", (lambda e, tt=tt, bk=bk, x=x: e.tensor_tensor(out=vaug[:, tt, :, x * 128:x * 128 + 64],
                                                                                   in0=bank(bk).rearrange("p (c x d) -> p c x d", c=4, x=2)[:, :, x, :],
                                                                                   in1=bvb[:].rearrange("p (c x d) -> p c x d", c=4, x=2)[:, :, x, :], op=ALU.add)),
                                reads=[RB[bk], R_misc], writes=rtok("vaug", tt * 128, tt * 128 + 128))

                  dump("S1o", oT[:, 0:4, :])
                  dump("S1q", qT[:])
                  dump("S1k", kT[:])
                  dump("S1v", vaug[:].rearrange("p t c x -> p t (c x)"))
                  S.barrier()
                  cur[0] = C0
                  nabt = [alloc("nabt%d" % i, [128, 21, 128], F32) for i in range(2)]
                  sbt = [alloc("sbt%d" % i, [128, 640], F32) for i in range(2)]
                  PT = [alloc("PT%d" % i, [128, 896], BF16) for i in range(2)]
                  rec = [alloc("rec%d" % i, [128, 256], F32) for i in range(2)]
                  R_nabt = [Res("nabt0"), Res("nabt1")]
                  R_sbt = [Res("sbt0"), Res("sbt1")]
                  R_PT = [Res("PT0"), Res("PT1")]
                  R_rec = [Res("rec0"), Res("rec1")]
                  it = 0
                  na_pend = [None]
                  for h in range(8):
                      if na_pend[0] is not None:
                          na_pend[0]()
                          na_pend[0] = None
                      c, sh = h // 2, h % 2
                      p0, p1 = sh * 64, sh * 64 + 64
                      nh0, nh1 = (0, 64) if sh == 0 else (64, 128)
                      dh0, dh1 = (64, 128) if sh == 0 else (0, 64)
                      hs = h % 2
                      S.add("sp", (lambda e, h=h, hs=hs: e.dma_start(out=nabt[hs][:], in_=nab[h].rearrange("p (a q) -> p a q", a=21))), writes=[R_nabt[hs]], dma=True)
                      vcol = sh * 64
                      s = it % 2
                      it += 1
                      sb0 = 2 * s
                      for i in range(2):
                          S.add("pe", (lambda e, i=i, c=c, p0=p0, p1=p1, sb0=sb0: e.matmul(bank(sb0)[:, i * 256:(i + 1) * 256], lhsT=kT[p0:p1, c, i * 128:(i + 1) * 128],
                                                                                          rhs=qT[p0:p1, c, 0:256], start=True, stop=True)),
                                reads=rtok("qk", 0, 256), writes=[RB[sb0]])
                      S.add("act", (lambda e, s=s, sb0=sb0: e.activation(out=PT[s][:, 0:512], in_=bank(sb0)[:, 0:512], func=AF.Exp, scale=0.125)), reads=[RB[sb0]], writes=[R_PT[s]])
                      ob = 4 + s
                      for i in range(2):
                          S.add("pe", (lambda e, i=i, c=c, s=s, ob=ob, vcol=vcol: e.matmul(bank(ob)[:, 0:256], lhsT=vaug[:, i, c, vcol:vcol + 128], rhs=PT[s][:, i * 256:(i + 1) * 256],
                                                                                          start=(i == 0), stop=(i == 1))),
                                reads=[R_PT[s]] + rtok("vaug", 0, 256), writes=[RB[ob]])
                      S.add("dve", (lambda e, s=s, ob=ob, dh0=dh0, dh1=dh1: e.reciprocal(out=rec[s][dh0:dh1, 0:256], in_=bank(ob)[dh0:dh1, 0:256])), reads=[RB[ob]], writes=[R_rec[s]])
                      S.add("dve", (lambda e, s=s, ob=ob, c=c, nh0=nh0, nh1=nh1, dh0=dh0, dh1=dh1: e.tensor_tensor(out=oT[nh0:nh1, 4 + c, 0:256], in0=bank(ob)[nh0:nh1, 0:256],
                                                                                                               in1=rec[s][dh0:dh1, 0:256], op=ALU.mult)),
                            reads=[RB[ob], R_rec[s]], writes=rtok("oT", 0, 256))
                      for j in range(16):
                          s = it % 2
                          it += 1
                          sb0 = 2 * s
                          tl = _na_tiles(j)
                          nl = len(tl)
                          ti0 = _na_tile_index(j)
                          q0 = NCTX + j * 128
                          ktoks = [NCTX + a * 128 for a in tl] + [0, 128]
                          for i, kt0 in enumerate(ktoks):
                              bk = sb0 + (i // 4)
                              S.add("pe", (lambda e, i=i, kt0=kt0, bk=bk, c=c, p0=p0, p1=p1, q0=q0: e.matmul(bank(bk)[:, (i % 4) * 128:(i % 4 + 1) * 128], lhsT=kT[p0:p1, c, kt0:kt0 + 128],
                                                                                                        rhs=qT[p0:p1, c, q0:q0 + 128], start=True, stop=True)),
                                    reads=rtok("qk", kt0, kt0 + 128) + rtok("qk", q0, q0 + 128), writes=[RB[bk]])
                          S.add("dve", (lambda e, s=s, nl=nl, ti0=ti0, hs=hs: e.scalar_tensor_tensor(out=sbt[s][:, 0:nl * 128], in0=PS[s][:, 0:nl * 128], scalar=0.125,
                                                                                                    in1=nabt[hs][:, ti0:ti0 + nl, :].rearrange("p a q -> p (a q)"),
                                                                                                    op0=ALU.mult, op1=ALU.add)),
                                reads=[RB[sb0], RB[sb0 + 1], R_nabt[hs]], writes=[R_sbt[s]])
                          S.add("act", (lambda e, s=s, nl=nl: e.activation(out=PT[s][:, 0:nl * 128], in_=sbt[s][:, 0:nl * 128], func=AF.Exp)), reads=[R_sbt[s]], writes=[R_PT[s]])
                          S.add("act", (lambda e, s=s, nl=nl: e.activation(out=PT[s][:, nl * 128:(nl + 2) * 128], in_=PS[s][:, nl * 128:(nl + 2) * 128], func=AF.Exp, scale=0.125)),
                                reads=[RB[sb0], RB[sb0 + 1]], writes=[R_PT[s]])
                          ob = 4 + s

                          def na_back(ktoks=ktoks, c=c, s=s, ob=ob, vcol=vcol, nl=nl, q0=q0, nh0=nh0, nh1=nh1, dh0=dh0, dh1=dh1):
                              for i, kt0 in enumerate(ktoks):
                                  S.add("pe", (lambda e, i=i, kt0=kt0: e.matmul(bank(ob)[:, 0:128], lhsT=vaug[:, kt0 // 128, c, vcol:vcol + 128],
                                                                                rhs=PT[s][:, i * 128:(i + 1) * 128], start=(i == 0), stop=(i == nl + 1))),
                                        reads=[R_PT[s]] + rtok("vaug", kt0, kt0 + 128), writes=[RB[ob]])
                              S.add("act", (lambda e: e.activation(out=rec[s][dh0:dh1, 0:128], in_=bank(ob)[dh0:dh1, 0:128], func=AF.Ln)), reads=[RB[ob]], writes=[R_rec[s]])
                              S.add("act", (lambda e: e.activation(out=rec[s][dh0:dh1, 0:128], in_=rec[s][dh0:dh1, 0:128], func=AF.Exp, scale=-1.0)), reads=[R_rec[s]], writes=[R_rec[s]])
                              S.add("dve", (lambda e: e.tensor_tensor(out=oT[nh0:nh1, 4 + c, q0:q0 + 128], in0=bank(ob)[nh0:nh1, 0:128],
                                                                      in1=rec[s][dh0:dh1, 0:128], op=ALU.mult)),
                                    reads=[RB[ob], R_rec[s]], writes=rtok("oT", q0, q0 + 128))
                          if na_pend[0] is not None:
                              na_pend[0]()
                          na_pend[0] = na_back
                  if na_pend[0] is not None:
                      na_pend[0]()
                      na_pend[0] = None
                  dump("S3", oT[:, 4:8, :])
                  w_out_d = w_out0
                  tok_blocks = blocks256
              else:
                  S.barrier()
                  cur[0] = C0
                  wq = alloc("wq", [128, 8, 512], BF16)
                  wqs = alloc("wqs", [128, 8, 512], BF16)
                  wk = alloc("wk", [128, 8, 256], BF16)
                  wks = alloc("wks", [128, 8, 256], BF16)
                  wv = alloc("wv", [128, 8, 256], BF16)
                  rt = [alloc("rt%d" % i, [128, 2, 512], F32) for i in range(2)]
                  R_w1, R_rope = Res("w1"), Res("rope")
                  R_rt = [Res("rt0"), Res("rt1")]
                  R_wq = Res("wq")
                  for dst, src in ((wk, wk1), (wks, wks1), (wv, wv1)):
                      S.add("pool", (lambda e, dst=dst, src=src: e.dma_start(out=dst[:], in_=src.rearrange("(kc p) n -> p kc n", p=128))), writes=[R_w1], dma=True)
                  S.add("sp", lambda e: e.dma_start(out=rC[:], in_=ropeC), writes=[R_rope], dma=True)
                  S.add("sp", lambda e: e.dma_start(out=rS[:], in_=ropeS), writes=[R_rope], dma=True)
                  if True:
                      S.add("pool", lambda e: e.memset(vaug1[:], 1.0), writes=rtok("vaug", 0, NT))
                  it = 0
                  for half, (t0, N) in [(hf_, blk_) for hf_ in range(3) for blk_ in lat512]:
                      l0 = t0 - NCTX
                      if half < 2 and t0 == NCTX:
                          for dst, src in ((wq, wq1), (wqs, wqs1)):
                              S.add("pool", (lambda e, dst=dst, src=src, half=half: e.dma_start(out=dst[:], in_=src.rearrange("(kc p) n -> p kc n", p=128)[:, :, half * 512:(half + 1) * 512])),
                                    writes=[R_wq], dma=True)
                      for c in (range(half * 4, half * 4 + 4) if half < 2 else range(8, 10)):
                          s = it % 2
                          it += 1
                          b1, b2 = 2 * s, 2 * s + 1
                          wa, wb = (wq, wqs) if c < 8 else (wk, wks)
                          cc = (c % 4) if c < 8 else c - 8
                          for kc in range(8):
                              S.add("pe", (lambda e, kc=kc, cc=cc, wa=wa, t0=t0, N=N, b1=b1: e.matmul(bank(b1)[:, 0:N], lhsT=wa[:, kc, cc * 128:(cc + 1) * 128], rhs=UT[:, kc, t0:t0 + N],
                                                                                                 start=(kc == 0), stop=(kc == 7))),
                                    reads=[R_w1, R_wq] + rtok("UT", t0, t0 + N), writes=[RB[b1]])
                          for kc in range(8):
                              S.add("pe", (lambda e, kc=kc, cc=cc, wb=wb, t0=t0, N=N, b2=b2: e.matmul(bank(b2)[:, 0:N], lhsT=wb[:, kc, cc * 128:(cc + 1) * 128], rhs=UT[:, kc, t0:t0 + N],
                                                                                                 start=(kc == 0), stop=(kc == 7))),
                                    reads=[R_w1, R_wq] + rtok("UT", t0, t0 + N), writes=[RB[b2]])
                          S.add("dve", (lambda e, s=s, l0=l0, N=N, b1=b1: e.tensor_tensor(out=rt[s][:, 0, 0:N], in0=bank(b1)[:, 0:N], in1=rC[:, l0:l0 + N], op=ALU.mult)),
                                reads=[RB[b1], R_rope], writes=[R_rt[s]])
                          S.add("dve", (lambda e, s=s, l0=l0, N=N, b2=b2: e.tensor_tensor(out=rt[s][:, 1, 0:N], in0=bank(b2)[:, 0:N], in1=rS[:, l0:l0 + N], op=ALU.mult)),
                                reads=[RB[b2], R_rope], writes=[R_rt[s]])
                          if c < 8:
                              dst = qT1[:, c, l0:l0 + N]
                          else:
                              dst = kT1[:, c - 8, t0:t0 + N]
                          S.add("dve", (lambda e, s=s, N=N, dst=dst: e.tensor_tensor(out=dst, in0=rt[s][:, 0, 0:N], in1=rt[s][:, 1, 0:N], op=ALU.add)),
                                reads=[R_rt[s]], writes=rtok("qk", t0, t0 + N))
                  for cc in range(2):
                      s = it % 2
                      it += 1
                      b1 = 2 * s
                      for kc in range(8):
                          S.add("pe", (lambda e, kc=kc, cc=cc, b1=b1: e.matmul(bank(b1)[:, 0:256], lhsT=wk[:, kc, cc * 128:(cc + 1) * 128], rhs=UT[:, kc, 0:256], start=(kc == 0), stop=(kc == 7))),
                                reads=[R_w1] + rtok("UT", 0, 256), writes=[RB[b1]])
                      S.add("act", (lambda e, cc=cc, b1=b1: e.copy(out=kT1[:, cc, 0:256], in_=bank(b1)[:, 0:256])), reads=[RB[b1]], writes=rtok("qk", 0, 256))
                  for tt in range(18):
                      bk = 4 + tt % 2
                      for kc in range(8):
                          S.add("pe", (lambda e, kc=kc, tt=tt, bk=bk: e.matmul(bank(bk)[:, 0:256], lhsT=UT[:, kc, tt * 128:(tt + 1) * 128], rhs=wv[:, kc, :], start=(kc == 0), stop=(kc == 7))),
                                reads=[R_w1] + rtok("UT", tt * 128, tt * 128 + 128), writes=[RB[bk]])
                      for x in range(2):
                          S.add("act", (lambda e, tt=tt, bk=bk, x=x: e.copy(out=vaug1[:, tt, :, x * 128:x * 128 + 64],
                                                                          in_=bank(bk)[:, 0:256].rearrange("p (c x d) -> p c x d", c=2, x=2)[:, :, x, :])),
                                reads=[RB[bk]], writes=rtok("vaug", tt * 128, tt * 128 + 128))
                  dump("P1", kT1[:])
                  S.barrier()
                  cur[0] = C0
                  mlu = alloc("mlu", [128, 256], F32)
                  S.add("sp", lambda e: e.dma_start(out=mlu[:], in_=maskLU), writes=[R_const], dma=True)
                  sbm = [alloc("sbm%d" % i, [128, 512], F32) for i in range(2)]
                  PT1 = [alloc("PT1_%d" % i, [128, 5, 512], BF16) for i in range(2)]
                  rec1 = [alloc("rec1_%d" % i, [128, 512], F32) for i in range(2)]
                  R_sbm = [Res("sbm0"), Res("sbm1")]
                  R_PT1 = [[Res("PT1_%d_%d" % (i, k)) for k in range(5)] for i in range(2)]
                  R_rec1 = [Res("rec1_0"), Res("rec1_1")]
                  it = 0
                  sbr = 0
                  mi = 0
                  gq_pend = [None]
                  for g in range(4):
                      m, sh = g // 2, g % 2
                      p0, p1 = sh * 64, sh * 64 + 64
                      nh0, nh1 = (0, 64) if sh == 0 else (64, 128)
                      dh0, dh1 = (64, 128) if sh == 0 else (0, 64)
                      vcol = sh * 64
                      for qb in range(16):
                          s = it % 2
                          it += 1
                          tiles = []
                          if qb > 0:
                              tiles.append((NCTX + (qb - 1) * 128, 0))
                          tiles.append((NCTX + qb * 128, None))
                          if qb < 15:
                              tiles.append((NCTX + (qb + 1) * 128, 1))
                          tiles += [(0, None), (128, None)]
                          nt = len(tiles)
                          for i, (kt0, mk) in enumerate(tiles):
                              bk = sbr % 4
                              sbr += 1
                              S.add("pe", (lambda e, kt0=kt0, bk=bk, m=m, p0=p0, p1=p1, qb=qb: e.matmul(bank(bk).rearrange("p (h q) -> p h q", h=4), lhsT=kT1[p0:p1, m, kt0:kt0 + 128],
                                                                                                   rhs=qT1[p0:p1, 4 * m:4 * m + 4, qb * 128:(qb + 1) * 128], start=True, stop=True)),
                                    reads=rtok("qk", kt0, kt0 + 128) + rtok("qk", NCTX + qb * 128, NCTX + qb * 128 + 128), writes=[RB[bk]])
                              if mk is None:
                                  S.add("act", (lambda e, s=s, i=i, bk=bk: e.activation(out=PT1[s][:, i, :], in_=bank(bk), func=AF.Exp, scale=0.125)), reads=[RB[bk]], writes=[R_PT1[s][i]])
                              else:
                                  ms = mi % 2
                                  mi += 1
                                  S.add("dve", (lambda e, ms=ms, mk=mk, bk=bk: e.scalar_tensor_tensor(out=sbm[ms][:].rearrange("p (h q) -> p h q", h=4), in0=bank(bk).rearrange("p (h q) -> p h q", h=4),
                                                                                                    scalar=0.125, in1=mlu[:, mk * 128:(mk + 1) * 128].unsqueeze(1).broadcast_to([128, 4, 128]),
                                                                                                    op0=ALU.mult, op1=ALU.add)),
                                        reads=[RB[bk], R_const], writes=[R_sbm[ms]])
                                  S.add("act", (lambda e, s=s, i=i, ms=ms: e.activation(out=PT1[s][:, i, :], in_=sbm[ms][:], func=AF.Exp)), reads=[R_sbm[ms]], writes=[R_PT1[s][i]])
                          ob = 4 + s
                          q0 = NCTX + qb * 128

                          def gq_back(tiles=tiles, s=s, ob=ob, m=m, sh=sh, vcol=vcol, nt=nt, q0=q0, nh0=nh0, nh1=nh1, dh0=dh0, dh1=dh1):
                              for i, (kt0, mk) in enumerate(tiles):
                                  S.add("pe", (lambda e, i=i, kt0=kt0: e.matmul(bank(ob), lhsT=vaug1[:, kt0 // 128, m, vcol:vcol + 128], rhs=PT1[s][:, i, :],
                                                                                start=(i == 0), stop=(i == nt - 1))),
                                        reads=[R_PT1[s][i]] + rtok("vaug", kt0, kt0 + 128), writes=[RB[ob]])
                              S.add("dve", (lambda e: e.tensor_tensor(out=rec1[s][dh0:dh1, :].rearrange("p (h q) -> p h q", h=4),
                                                                      in0=bank(ob)[dh0:dh1, :].rearrange("p (h q) -> p h q", h=4),
                                                                      in1=esink[dh0:dh1, (m * 2 + sh) * 4:(m * 2 + sh) * 4 + 4].unsqueeze(2).broadcast_to([64, 4, 128]),
                                                                      op=ALU.add)),
                                    reads=[RB[ob], R_const], writes=[R_rec1[s]])
                              S.add("act", (lambda e: e.activation(out=rec1[s][dh0:dh1, :], in_=rec1[s][dh0:dh1, :], func=AF.Ln)), reads=[R_rec1[s]], writes=[R_rec1[s]])
                              S.add("act", (lambda e: e.activation(out=rec1[s][dh0:dh1, :], in_=rec1[s][dh0:dh1, :], func=AF.Exp, scale=-1.0)), reads=[R_rec1[s]], writes=[R_rec1[s]])
                              S.add("dve", (lambda e: e.tensor_tensor(out=oT[nh0:nh1, 4 * m:4 * m + 4, q0:q0 + 128],
                                                                      in0=bank(ob)[nh0:nh1, :].rearrange("p (h q) -> p h q", h=4),
                                                                      in1=rec1[s][dh0:dh1, :].rearrange("p (h q) -> p h q", h=4), op=ALU.mult)),
                                    reads=[RB[ob], R_rec1[s]], writes=rtok("oT", q0, q0 + 128))
                          if gq_pend[0] is not None:
                              gq_pend[0]()
                          gq_pend[0] = gq_back
                  gq_pend[0]()
                  gq_pend[0] = None
                  dump("A1", oT[:])
                  w_out_d = w_out1
                  tok_blocks = lat256

              S.barrier()
              cur[0] = C0
              gatesT = alloc("gatesT", [16, NT], F32)
              wo = alloc("wo", [128, 8, D], BF16)
              hold0_at = cur[0]
              hold = [alloc("hold%d" % i, [128, 8, 128], F32) for i in range(2)]
              pre = alloc("pre", [128, 8, 128], F32)
              prebf = alloc("prebf", [128, 8, 128], BF16)
              presq = alloc("presq", [128, 8, 128], BF16)
              t32 = alloc("t32", [128, 8, 128], F32)
              tmpo = [alloc("tmpo%d" % i, [128, 128], F32) for i in range(2)]
              mean_sb = alloc("mean_sb", [128, 128], F32)
              rstd_sb = alloc("rstd_sb", [128, 128], F32)
              lsb = alloc("lsb", [128, 18, 20], F32, at=hold0_at)
              rw = alloc("rw", [128, 18 * 96], F32, at=hold0_at + 1472)
              assert hold0_at + 1472 + 18 * 96 * 4 <= cur[0]
              R_wo = Res("wo")
              R_hold = [Res("hold0"), Res("hold1")]
              R_pre, R_t32 = Res("pre"), Res("t32")
              R_tmpo = [Res("tmpo0"), Res("tmpo1")]
              R_lt = [Res("prebf"), Res("presq"), Res("mean"), Res("rstd")]
              R_rout = Res("rout")
              S.add("pool", (lambda e, w_out_d=w_out_d: e.dma_start(out=wo[:], in_=w_out_d.rearrange("(kc p) n -> p kc n", p=128))), writes=[R_wo], dma=True)
              tiles4 = [t for (t0_, n_) in tok_blocks for t in range(t0_, t0_ + n_, 128)]
              pre2 = alloc("pre2x", [128, 8, 128], F32, at=C0)
              pre_b = [pre, pre2]
              R_pre_b = [R_pre, Res("pre2x")]
              def s4_frontPE(bi):
                  t0 = tiles4[bi]
                  s = bi % 2
                  S.add("sp", (lambda e: e.dma_start(out=hold[s][:], in_=Hs3[:, :, t0:t0 + 128])), reads=rtok("Hs", t0, t0 + 128), writes=[R_hold[s]], dma=True)
                  for oc in range(8):
                      bk = 2 * s + oc // 4
                      co = (oc % 4) * 128
                      for kc in range(8):
                          S.add("pe", (lambda e, kc=kc, oc=oc, bk=bk, co=co: e.matmul(bank(bk)[:, co:co + 128], lhsT=wo[:, kc, oc * 128:(oc + 1) * 128], rhs=oT[:, kc, t0:t0 + 128],
                                                                                    start=(kc == 0), stop=(kc == 7))),
                                reads=[R_wo] + rtok("oT", t0, t0 + 128), writes=[RB[bk]])

              def s4_frontEV(bi, l=l):
                  t0 = tiles4[bi]
                  s = bi % 2
                  mc = mcol(t0)
                  pb, rpb = pre_b[s], R_pre_b[s]
                  for oc in range(8):
                      bk = 2 * s + oc // 4
                      co = (oc % 4) * 128
                      ts = oc % 2
                      if l == 0:
                          vb = V("M2B", oc, t0)
                          S.add("act", (lambda e, oc=oc, bk=bk, co=co, ts=ts, vb=vb: e.activation(out=tmpo[ts][:], in_=bank(bk)[:, co:co + 128], func=AF.Identity,
                                                                                            bias=vb, scale=modc(0, 2, oc, mc))),
                                reads=[RB[bk], R_mod, R_vecs], writes=[R_tmpo[ts]])
                      else:
                          S.add("act", (lambda e, oc=oc, bk=bk, co=co, ts=ts: e.activation(out=tmpo[ts][:], in_=bank(bk)[:, co:co + 128], func=AF.Identity,
                                                                                     scale=modc(1, 2, oc, mc))),
                                reads=[RB[bk], R_mod], writes=[R_tmpo[ts]])
                      S.add("dve", (lambda e, oc=oc, ts=ts: e.scalar_tensor_tensor(out=pb[:, oc, :], in0=hold[s][:, oc, :], scalar=ALPHA, in1=tmpo[ts][:], op0=ALU.mult, op1=ALU.add)),
                            reads=[R_hold[s], R_tmpo[ts]], writes=[rpb])

              def s4_part1(bi):
                  s = bi % 2
                  ln_part1(pre_b[s][:], 8, 128, ones1k, prebf, presq, 4, [R_pre_b[s]], R_lt)

              def s4_part2(bi, l=l):
                  t0 = tiles4[bi]
                  tt = t0 // 128
                  s = bi % 2
                  pb, rpb = pre_b[s], R_pre_b[s]
                  ln_part2(128, mean_sb, rstd_sb, 4, R_lt)
                  normalize(pb[:], 8, 128, mean_sb, rstd_sb, [rpb], R_lt)
                  for ch in range(8):
                      affine(UT[:, ch, t0:t0 + 128], pb[:, ch, :], V("G4", ch, t0), V("B4", ch, t0), [rpb, R_vecs], rtok("UT", t0, t0 + 128))
                      affine(t32[:, ch, :], pb[:, ch, :], V("G4", ch, t0), V("B4", ch, t0), [rpb, R_vecs], [R_t32])
                      affine(hres[:, ch, t0:t0 + 128], pb[:, ch, :], V("GA1", ch, t0), V("BA1", ch, t0), [rpb, R_vecs], rtok("hres%d" % ch, t0, t0 + 128))
                  for kc in range(8):
                      S.add("pe", (lambda e, kc=kc: e.matmul(bank(5)[:, tt * 20:tt * 20 + 20], lhsT=t32[:, kc, :], rhs=wrt[:, l * 160 + kc * 20:l * 160 + kc * 20 + 20],
                                                             start=(kc == 0), stop=(kc == 7))),
                            reads=[R_t32, R_const], writes=[RB[5]])

              n4 = len(tiles4)
              s4_frontPE(0)
              for bi in range(n4):
                  if bi > 0:
                      s4_part2(bi - 1)
                  s4_frontEV(bi)
                  if bi + 1 < n4:
                      s4_frontPE(bi + 1)
                  s4_part1(bi)
              s4_part2(n4 - 1)

              dump("S4", hres[:])
              dump("S4u", UT[:])
              S.barrier()
              T0 = tok_blocks[0][0] // 128
              T1 = 18
              nT = T1 - T0
              S.add("dve", lambda e: e.tensor_copy(out=lsb[:, T0:T1, :], in_=bank(5)[:, T0 * 20:T1 * 20].rearrange("p (t n) -> p t n", n=20)), reads=[RB[5]], writes=[R_rout])

              def rwv(i, n):
                  return rw[:, i * 18 * 4:(i * 18 * 4) + 18 * n].rearrange("p (t n) -> p t n", n=n)[:, T0:T1, :]

              def rop(fn):
                  S.add("dve", fn, reads=[R_rout], writes=[R_rout])

              lg = lsb[:, T0:T1, 0:4]
              le = lsb[:, T0:T1, 4:20].rearrange("p t (g x) -> p t g x", g=4)
              gmax, gsum, gp, m1, m2, dd, w1, w2 = [rwv(i, 1) for i in range(8)]
              gsh, gmask, elsel, mask1, el2, mask2, within, wa_ = [rwv(8 + i, 4) for i in range(8)]
              t44 = rw[:, 18 * 64:18 * 80].rearrange("p (t g x) -> p t g x", g=4, x=4)[:, T0:T1]
              gates = rw[:, 18 * 80:18 * 96].rearrange("p (t g x) -> p t g x", g=4, x=4)
              bc4 = lambda a: a.broadcast_to([128, nT, 4])
              rop(lambda e: e.tensor_reduce(out=gmax, in_=lg, axis=AX.X, op=ALU.max))
              rop(lambda e: e.tensor_tensor(out=gsh, in0=lg, in1=bc4(gmax), op=ALU.subtract))
              rop(lambda e: e.tensor_tensor(out=gmask, in0=lg, in1=bc4(gmax), op=ALU.is_equal))
              S.add("act", lambda e: e.activation(out=gsh, in_=gsh, func=AF.Exp), reads=[R_rout], writes=[R_rout])
              rop(lambda e: e.tensor_reduce(out=gsum, in_=gsh, axis=AX.X, op=ALU.add))
              rop(lambda e: e.reciprocal(out=gp, in_=gsum))
              rop(lambda e: e.tensor_tensor(out=t44, in0=le, in1=gmask.unsqueeze(3).broadcast_to([128, nT, 4, 4]), op=ALU.mult))
              rop(lambda e: e.tensor_reduce(out=elsel, in_=t44.rearrange("p t g x -> p t x g"), axis=AX.X, op=ALU.add))
              rop(lambda e: e.tensor_reduce(out=m1, in_=elsel, axis=AX.X, op=ALU.max))
              rop(lambda e: e.tensor_tensor(out=mask1, in0=elsel, in1=bc4(m1), op=ALU.is_equal))
              rop(lambda e: e.scalar_tensor_tensor(out=el2, in0=mask1, scalar=NEG, in1=elsel, op0=ALU.mult, op1=ALU.add))
              rop(lambda e: e.tensor_reduce(out=m2, in_=el2, axis=AX.X, op=ALU.max))
              rop(lambda e: e.tensor_tensor(out=mask2, in0=el2, in1=bc4(m2), op=ALU.is_equal))
              rop(lambda e: e.tensor_tensor(out=dd, in0=m2, in1=m1, op=ALU.subtract))
              S.add("act", lambda e: e.activation(out=dd, in_=dd, func=AF.Exp), reads=[R_rout], writes=[R_rout])
              rop(lambda e: e.tensor_scalar_add(out=w1, in0=dd, scalar1=1.0))
              rop(lambda e: e.reciprocal(out=w1, in_=w1))
              rop(lambda e: e.tensor_tensor(out=w1, in0=w1, in1=gp, op=ALU.mult))
              rop(lambda e: e.tensor_tensor(out=w2, in0=dd, in1=w1, op=ALU.mult))
              rop(lambda e: e.tensor_tensor(out=within, in0=mask1, in1=bc4(w1), op=ALU.mult))
              rop(lambda e: e.tensor_tensor(out=wa_, in0=mask2, in1=bc4(w2), op=ALU.mult))
              rop(lambda e: e.tensor_tensor(out=within, in0=within, in1=wa_, op=ALU.add))
              rop(lambda e: e.tensor_tensor(out=gates[:, T0:T1], in0=gmask.unsqueeze(3).broadcast_to([128, nT, 4, 4]), in1=within.unsqueeze(2).broadcast_to([128, nT, 4, 4]), op=ALU.mult))
              for tt in range(T0, T1):
                  bk = 6 + (tt // 4) % 2
                  S.add("pe", (lambda e, tt=tt, bk=bk: e.transpose(bank(bk)[0:16, (tt % 4) * 128:(tt % 4 + 1) * 128], gates[:, tt].rearrange("p g x -> p (g x)"), id32[:])),
                        reads=[R_rout, R_const], writes=[RB[bk]])
                  if tt % 4 == 3 or tt == T1 - 1:
                      ta = (tt // 4) * 4
                      ta0 = max(ta, T0)
                      S.add("act", (lambda e, bk=bk, ta=ta, ta0=ta0, tt=tt: e.copy(out=gatesT[0:16, ta0 * 128:(tt + 1) * 128], in_=bank(bk)[0:16, (ta0 - ta) * 128:(tt + 1 - ta) * 128])),
                            reads=[RB[bk]], writes=[R_rout])

              S.barrier()
              cur[0] = C0
              gatesT = alloc("gatesT", [16, NT], F32)
              wgs = [alloc("wgs%d" % i, [128, 8, 256], BF16) for i in range(2)]
              wus = [alloc("wus%d" % i, [128, 8, 256], BF16) for i in range(2)]
              wds = [alloc("wds%d" % i, [128, 2, D], BF16) for i in range(2)]
              sgm = [alloc("sgm0", [128, 2, 512], F32)]
              hgm = [alloc("hgm%d" % i, [128, 2, 512], BF16) for i in range(4)]
              o_ = OT0
              for i in range(2, 4):
                  wgs.append(alloc("wgs%d" % i, [128, 8, 256], BF16, at=o_)); o_ += 4096
                  wus.append(alloc("wus%d" % i, [128, 8, 256], BF16, at=o_)); o_ += 4096
                  wds.append(alloc("wds%d" % i, [128, 2, D], BF16, at=o_)); o_ += 4096
              sgm.append(alloc("sgm1", [128, 2, 512], F32, at=o_)); o_ += 4096
              gsb = []
              for i in range(2):
                  gsb.append(alloc("gsb%d" % i, [128, 512], F32, at=o_)); o_ += 2048
              assert o_ <= OT0 + 36864
              R_ewg = [Res("ewg%d" % i) for i in range(4)]
              R_ewu = [Res("ewu%d" % i) for i in range(4)]
              R_ewd = [Res("ewd%d" % i) for i in range(4)]
              R_sgm = [Res("sgm0"), Res("sgm1")]
              R_hgm = [Res("hgm%d" % i) for i in range(4)]
              R_gsb = [Res("gsb0"), Res("gsb1")]
              mblocks = blocks512 if l == 0 else lat512

              def load_expert(ex, slot, l=l):
                  S.add("pool", (lambda e: e.dma_start(out=wgs[slot][:], in_=ewg[l, ex].rearrange("(kc p) f -> p kc f", p=128))), writes=[R_ewg[slot]], dma=True)
                  S.add("pool", (lambda e: e.dma_start(out=wus[slot][:], in_=ewu[l, ex].rearrange("(kc p) f -> p kc f", p=128))), writes=[R_ewu[slot]], dma=True)
                  S.add("pool", (lambda e: e.dma_start(out=wds[slot][:], in_=ewd[l, ex].rearrange("(kc p) f -> p kc f", p=128))), writes=[R_ewd[slot]], dma=True)

              def emit_front(ex, slot, t0, N, si):
                  S.add("pe", (lambda e: e.matmul(bank(4)[:, 0:N], lhsT=selt[0:16, ex * 128:(ex + 1) * 128], rhs=gatesT[0:16, t0:t0 + N], start=True, stop=True)),
                        reads=[R_rout, R_const], writes=[RB[4]])
                  S.add("act", (lambda e: e.copy(out=gsb[si][:, 0:N], in_=bank(4)[:, 0:N])), reads=[RB[4]], writes=[R_gsb[si]])
                  for oc in range(4):
                      wsrc = wgs[slot] if oc < 2 else wus[slot]
                      rw_ = R_ewg[slot] if oc < 2 else R_ewu[slot]
                      for kc in range(8):
                          S.add("pe", (lambda e, kc=kc, oc=oc, wsrc=wsrc: e.matmul(bank(oc)[:, 0:N], lhsT=wsrc[:, kc, (oc % 2) * 128:(oc % 2 + 1) * 128], rhs=UT[:, kc, t0:t0 + N],
                                                                                    start=(kc == 0), stop=(kc == 7))),
                                reads=[rw_] + rtok("UT", t0, t0 + N), writes=[RB[oc]])
                  S.add("act", (lambda e: e.activation(out=sgm[si][:, :, 0:N], in_=PS[0][:].rearrange("p (j n) -> p j n", j=2)[:, :, 0:N], func=AF.Silu)),
                        reads=[RB[0], RB[1]], writes=[R_sgm[si]])
                  S.add("dve", (lambda e: e.tensor_tensor(out=sgm[si][:, :, 0:N], in0=sgm[si][:, :, 0:N], in1=PS[1][:].rearrange("p (j n) -> p j n", j=2)[:, :, 0:N], op=ALU.mult)),
                        reads=[RB[2], RB[3], R_sgm[si]], writes=[R_sgm[si]])

              def emit_gate(si, q, N):
                  S.add("dve", (lambda e: e.tensor_tensor(out=hgm[q][:, :, 0:N], in0=sgm[si][:, :, 0:N], in1=gsb[si][:, 0:N].unsqueeze(1).broadcast_to([128, 2, N]), op=ALU.mult)),
                        reads=[R_gsb[si], R_sgm[si]], writes=[R_hgm[q]])

              ybc = [0]

              def make_yhalf(half, slots, qs, t0, N, mc, l=l):
                  def f():
                      for dc in range(half * 4, half * 4 + 4):
                          bk = 5 + ybc[0] % 3
                          ybc[0] += 1
                          for j in range(2):
                              for k2 in range(2):
                                  S.add("pe", (lambda e, j=j, k2=k2, dc=dc, bk=bk: e.matmul(bank(bk)[:, 0:N], lhsT=wds[slots[j]][:, k2, dc * 128:(dc + 1) * 128], rhs=hgm[qs[j]][:, k2, 0:N],
                                                                                           start=(j == 0 and k2 == 0), stop=(j == 1 and k2 == 1))),
                                        reads=[R_ewd[slots[j]], R_hgm[qs[j]]], writes=[RB[bk]])
                          S.add("dve", (lambda e, dc=dc, bk=bk: e.scalar_tensor_tensor(out=hres[:, dc, t0:t0 + N], in0=bank(bk)[:, 0:N], scalar=modc(l, 5, dc, mc),
                                                                                      in1=hres[:, dc, t0:t0 + N], op0=ALU.mult, op1=ALU.add)),
                                reads=[RB[bk], R_mod] + rtok("hres%d" % dc, t0, t0 + N), writes=rtok("hres%d" % dc, t0, t0 + N))
                  return f

              load_expert(0, 0)
              load_expert(1, 1)
              pendA = pendB = None
              it = 0
              for pr in range(8):
                  slots = (2 * (pr % 2), 2 * (pr % 2) + 1)
                  for bi, (t0, N) in enumerate(mblocks):
                      qs = ((it % 2) * 2, (it % 2) * 2 + 1)
                      it += 1
                      mc = mcol(t0)
                      emit_front(2 * pr, slots[0], t0, N, 0)
                      if pendA is not None:
                          pendA()
                      emit_gate(0, qs[0], N)
                      emit_front(2 * pr + 1, slots[1], t0, N, 1)
                      if pendB is not None:
                          pendB()
                      emit_gate(1, qs[1], N)
                      pendA = make_yhalf(0, slots, qs, t0, N, mc)
                      pendB = make_yhalf(1, slots, qs, t0, N, mc)
                      if bi == 0 and pr + 1 < 8:
                          nslots = (2 * ((pr + 1) % 2), 2 * ((pr + 1) % 2) + 1)
                          load_expert(2 * pr + 2, nslots[0])
                          load_expert(2 * pr + 3, nslots[1])
              pendA()
              pendB()

              dump("S5", hres[:])
              S.barrier()
              cur[0] = C0
              pre2 = alloc("pre2", [128, 8, 256], BF16)
              presq2 = alloc("presq2", [128, 8, 256], BF16)
              mean2 = alloc("mean2", [128, 256], F32)
              rstd2 = alloc("rstd2", [128, 256], F32)
              otile = [alloc("otile%d" % i, [128, D], F32) for i in range(2)]
              R_l2 = [Res("pre2"), Res("presq2"), Res("mean2"), Res("rstd2")]
              R_ot = [Res("ot0"), Res("ot1")]
              oi = 0
              for bi, (t0, N) in enumerate(tok_blocks):
                  hap = hres[:, :, t0:t0 + 256]
                  rh = [r_ for ch_ in range(8) for r_ in rtok("hres%d" % ch_, t0, t0 + 256)]
                  ln_stats(hap, 8, 256, ones1k, pre2, presq2, mean2, rstd2, 4, rh, R_l2)
                  normalize(hap, 8, 256, mean2, rstd2, rh, R_l2)
                  if l == 0:
                      for ch in range(8):
                          affine(UT[:, ch, t0:t0 + 256], hres[:, ch, t0:t0 + 256], V("GU", ch, t0), V("BU", ch, t0), rh + [R_vecs], rtok("UT", t0, t0 + 256))
                      for ch in range(8):
                          affine(hres[:, ch, t0:t0 + 256], hres[:, ch, t0:t0 + 256], smc("ln_g", 8 + ch), smc("ln_b", 8 + ch), rh + [R_const], rh)
                      S.add("sp", (lambda e, t0=t0: e.dma_start(out=Hs3[:, :, t0:t0 + 256], in_=hres[:, :, t0:t0 + 256])), reads=rh, writes=rtok("Hs", t0, t0 + 256), dma=True)
                      if debug and nlayers == 1:
                          S.add("sp", (lambda e, t0=t0: e.dma_start(out=dbg.rearrange("p (c t) -> p c t", c=8)[:, :, t0:t0 + 256], in_=hres[:, :, t0:t0 + 256])), reads=rh,
                                writes=[Res("dbgo")], dma=True)
                  else:
                      for ch in range(8):
                          affine(hres[:, ch, t0:t0 + 256], hres[:, ch, t0:t0 + 256], smc("ln_g", 24 + ch), smc("ln_b", 24 + ch), rh + [R_const], rh)
                      for hh in range(2):
                          tk = t0 + hh * 128
                          so = oi % 2
                          oi += 1
                          for ch in range(8):
                              bk = so * 2 + ch // 4
                              S.add("pe", (lambda e, ch=ch, bk=bk, tk=tk: e.transpose(bank(bk)[:, (ch % 4) * 128:(ch % 4 + 1) * 128], hres[:, ch, tk:tk + 128], id32[:])),
                                    reads=rh + [R_const], writes=[RB[bk]])
                          for hf in range(2):
                              bk = so * 2 + hf
                              S.add("act" if hf else "dve", (lambda e, so=so, hf=hf, bk=bk: (e.copy if hf else e.tensor_copy)(out=otile[so][:, hf * 512:(hf + 1) * 512], in_=bank(bk))),
                                    reads=[RB[bk]], writes=[R_ot[so]])
                          S.add("sp", (lambda e, so=so, tk=tk, b=b: e.dma_start(out=outd[b, tk - NCTX:tk - NCTX + 128, :], in_=otile[so][:])), reads=[R_ot[so]], writes=[Res("outw")], dma=True)
    except _Stop:
        pass
    S.barrier()

    with nc.Block() as block:
        @block.tensor
        def _(e):
            S.emit_one("pe", e, esem, dsems)

        @block.scalar
        def _(e):
            S.emit_one("act", e, esem, dsems)

        @block.vector
        def _(e):
            S.emit_one("dve", e, esem, dsems)

        @block.gpsimd
        def _(e):
            S.emit_one("pool", e, esem, dsems)

        @block.sync
        def _(e):
            S.emit_one("sp", e, esem, dsems)
    es.close()
    return nc


def _prep_shared(inp):
    f = lambda a: np.ascontiguousarray(np.asarray(a, np.float32))
    sm = np.zeros((128, SMN), np.float32)

    def put(name, arr):
        arr = np.asarray(arr, np.float32)
        sm[:, SMO[name]:SMO[name] + arr.shape[1]] = arr

    put("ada_b0", _fm(inp["ada_b"][0]))
    put("ada_b1", _fm(inp["ada_b"][1]))
    put("ln_g", np.concatenate([_fm(inp["ln_g"][l, k]) for l in range(2) for k in range(2)], axis=1))
    put("ln_b", np.concatenate([_fm(inp["ln_b"][l, k]) for l in range(2) for k in range(2)], axis=1))
    b_in = np.asarray(inp["ab_b_in"][0], np.float32)
    put("b_in", _fm(b_in[:2048]))
    cw = np.asarray(inp["conv_w"][0], np.float32)
    put("conv_w", np.ascontiguousarray(cw.T.reshape(4, 128, 31).transpose(1, 0, 2).reshape(128, 124)))
    put("conv_b", _fm(inp["conv_b"][0]))
    put("cln_g", _fm(inp["conv_ln_g"][0]))
    put("cln_b", _fm(inp["conv_ln_b"][0]))
    put("b_out", _fm(inp["ab_b_out"][0]))
    sm[:, SMO["eps"]] = EPS
    qidx = _gqa_qidx()
    gw = np.asarray(inp["gqa_w_in"][0], np.float32)
    wq = gw[:, :1024]
    wkk = gw[:, 1024:1280]
    wvv = gw[:, 1280:1536]
    C, Sg = _rope_tables()
    kk = np.arange(128)[:, None]
    qq = np.arange(128)[None, :]
    maskL = np.where(kk >= qq, 0.0, NEG).astype(np.float32)
    maskU = np.where(kk <= qq, 0.0, NEG).astype(np.float32)
    sink = np.asarray(inp["gqa_sink"][0], np.float32)
    sperm = np.array([8 * m + 4 * sh + j for m in range(2) for sh in range(2) for j in range(4)])
    sel = np.zeros((16, 16, 128), np.float32)
    for ex in range(16):
        sel[ex, ex, :] = 1.0
    wr = np.stack([np.concatenate([np.asarray(inp["router_group"][l], np.float32), np.asarray(inp["router_expert"][l], np.float32)], axis=1)
                   .reshape(8, 128, 20).transpose(1, 0, 2).reshape(128, 160) for l in range(2)])
    bv = b_in[2048:2560]
    shared = {
        "ada_w": f(inp["ada_w"]),
        "sm": sm,
        "w_in0": f(inp["ab_w_in"][0]),
        "bvbc": np.ascontiguousarray(np.broadcast_to(bv[None, :], (128, 512))),
        "nab": np.ascontiguousarray(_na_bias_table(np.asarray(inp["na_rpb"][0], np.float32)).reshape(8, 128, 21 * 128)),
        "w_out0": f(inp["ab_w_out"][0]),
        "wq1": f(wq[:, qidx]),
        "wqs1": f(wq[:, qidx][:, _swap64(1024)]),
        "wk1": f(wkk),
        "wks1": f(wkk[:, _swap64(256)]),
        "wv1": f(wvv),
        "w_out1": f(np.asarray(inp["gqa_w_out"][0], np.float32)[qidx, :]),
        "ropeC": C,
        "ropeS": Sg,
        "maskLU": np.ascontiguousarray(np.concatenate([maskL, maskU], axis=1)),
        "sinkbc": np.ascontiguousarray(np.broadcast_to(sink[sperm][None, :], (128, 16))),
        "wr": f(wr),
        "sel": np.ascontiguousarray(sel.reshape(16, 2048)),
        "ident": np.eye(128, dtype=np.float32),
        "ewg": f(inp["exp_w_gate"]),
        "ewu": f(inp["exp_w_up"]),
        "ewd": f(inp["exp_w_down"]),
    }
    return shared


def _core_inputs(inp, shared, i):
    x = np.asarray(inp["x"], np.float32)
    ctx = np.asarray(inp["ctx"], np.float32)
    c = np.asarray(inp["c"], np.float32)
    cc = np.stack([c[2 * i], c[2 * i + 1], np.asarray(inp["c_ctx"], np.float32)])
    cvec = np.ascontiguousarray(cc.reshape(3, 8, 128).transpose(2, 1, 0).reshape(128, 24))
    m = dict(shared)
    m["x2"] = np.ascontiguousarray(x[2 * i:2 * i + 2])
    m["ctx2"] = np.ascontiguousarray(ctx[2 * i:2 * i + 2])
    m["cvec"] = cvec
    return m


_NC_CACHE = {}


def kernel(**inputs):
    n = 8
    if "nc" not in _NC_CACHE:
        _NC_CACHE["nc"] = build()
    nc = _NC_CACHE["nc"]
    shared = _prep_shared(inputs)
    in_maps = [_core_inputs(inputs, shared, i) for i in range(n)]
    res = run_bass_kernel_spmd(nc, in_maps, core_ids=list(range(n)))
    out = np.concatenate([np.asarray(r["out"], np.float32) for r in res.results], axis=0)
    return out
```

```python
import numpy as np
from contextlib import ExitStack
import concourse.bass as bass
import concourse.mybir as mybir
from concourse.bass_utils import run_bass_kernel_spmd

F32 = mybir.dt.float32
BF16 = mybir.dt.bfloat16
AF = mybir.ActivationFunctionType
ALU = mybir.AluOpType
AX = mybir.AxisListType

D = 1024
SEQ = 2048
NCTX = 256
NT = SEQ + NCTX
GW = 64
ALPHA = 4.0 ** 0.25
EPS = 1e-5
NEG = -1e30

ENGS = ("pe", "act", "dve", "pool", "sp")
N_DMA_SEMS = 40


class Res:
    __slots__ = ("name", "last_w", "readers")

    def __init__(self, name):
        self.name = name
        self.last_w = None
        self.readers = []


class Op:
    __slots__ = ("eng", "fn", "idx", "deps", "dma", "sig", "semval", "dsem", "dval", "dprev")

    def __init__(self, eng, fn, idx, dma):
        self.eng = eng
        self.fn = fn
        self.idx = idx
        self.deps = []
        self.dma = dma
        self.sig = False
        self.semval = 0
        self.dsem = -1
        self.dval = 0
        self.dprev = 0


class Sched:
    def __init__(self):
        self.ops = {e: [] for e in ENGS}
        self.ndma = 0
        self.dma_tot = [0] * N_DMA_SEMS
        self.last_dma = [None] * N_DMA_SEMS
        self._assigned = False

    def add(self, eng, fn, reads=(), writes=(), dma=False, extra=()):
        lst = self.ops[eng]
        op = Op(eng, fn, len(lst), dma)
        deps = {}
        for r in reads:
            if r.last_w is not None:
                deps[id(r.last_w)] = r.last_w
        for w in writes:
            if w.last_w is not None:
                deps[id(w.last_w)] = w.last_w
            for rd in w.readers:
                deps[id(rd)] = rd
        for x in extra:
            deps[id(x)] = x
        for r in reads:
            r.readers.append(op)
        for w in writes:
            w.last_w = op
            w.readers = []
        if dma:
            s = self.ndma % N_DMA_SEMS
            self.ndma += 1
            op.dsem = s
            op.dprev = self.dma_tot[s]
            self.dma_tot[s] += 16
            op.dval = self.dma_tot[s]
            self.last_dma[s] = op
        for d in deps.values():
            if d is op:
                continue
            if d.eng == eng and not d.dma and not dma:
                if eng == "pe":
                    continue
                if op.idx - d.idx > 2:
                    continue
            op.deps.append(d)
            if not d.dma:
                d.sig = True
        lst.append(op)
        return op

    def barrier(self):
        lasts = []
        for e in ENGS:
            for op in reversed(self.ops[e]):
                if not op.dma:
                    lasts.append(op)
                    break
        dl = [o for o in self.last_dma if o is not None]
        for e in ENGS:
            self.add(e, lambda eng: eng.nop(), extra=[o for o in lasts if o.eng != e] + dl)

    def emit_one(self, e, eng, esem, dsems):
        if not self._assigned:
            for ee in ENGS:
                c = 0
                for op in self.ops[ee]:
                    if op.sig and not op.dma:
                        c += 1
                        op.semval = c
            self._assigned = True
        seen = {}
        for op in self.ops[e]:
            need = {}
            for d in op.deps:
                if d.dma:
                    key = ("d", d.dsem)
                    val = d.dval
                else:
                    key = ("e", d.eng)
                    val = d.semval
                if val > need.get(key, 0):
                    need[key] = val
            if op.dma and op.dprev > 0:
                key = ("d", op.dsem)
                if op.dprev > need.get(key, 0):
                    need[key] = op.dprev
            for key, val in need.items():
                if seen.get(key, 0) >= val:
                    continue
                seen[key] = val
                sem = dsems[key[1]] if key[0] == "d" else esem[key[1]]
                eng.wait_ge(sem, val)
            ins = op.fn(eng)
            if op.dma:
                ins.then_inc(dsems[op.dsem], 16)
            elif op.sig:
                ins.then_inc(esem[e], 1)


def _sm_layout():
    off = {}
    n = 0
    for name, cols in (("ada_b0", 48), ("ada_b1", 48), ("ln_g", 32), ("ln_b", 32), ("b_in", 16),
                       ("conv_w", 124), ("conv_b", 4), ("cln_g", 4), ("cln_b", 4), ("b_out", 8), ("eps", 1)):
        off[name] = n
        n += cols
    return off, n


SMO, SMN = _sm_layout()


def _fm(v):
    v = np.asarray(v, np.float32)
    return np.ascontiguousarray(v.reshape(-1, 128).T)


def _gqa_qidx():
    idx = np.zeros(1024, np.int64)
    for c in range(8):
        m, j = divmod(c, 4)
        h0 = 8 * m + j
        h1 = 8 * m + 4 + j
        idx[c * 128:c * 128 + 64] = h0 * 64 + np.arange(64)
        idx[c * 128 + 64:c * 128 + 128] = h1 * 64 + np.arange(64)
    return idx


def _swap64(n):
    d = np.arange(n)
    dd = d % 64
    sw = np.where(dd % 32 < 16, dd + 16, dd - 16)
    return (d // 64) * 64 + sw


def _na_tiles(j):
    if j in (0, 1):
        return [0, 1, 2, 3]
    if j in (14, 15):
        return [12, 13, 14, 15]
    return [j - 2, j - 1, j, j + 1, j + 2]


def _na_tile_index(j):
    if j == 0:
        return 5
    if j == 1:
        return 9
    if j == 14:
        return 13
    if j == 15:
        return 17
    return 0


def _na_bias_table(rpb):
    rows = 32
    r = np.arange(rows)
    row_start = np.clip(r - 4, 0, rows - 8)
    jj = np.arange(GW)
    col_start = np.clip(jj - 8, 0, GW - 16)
    col_in = (jj[None, :] >= col_start[:, None]) & (jj[None, :] < col_start[:, None] + 16)
    col_off = np.clip(jj[None, :] - jj[:, None], -15, 15) + 15
    out = np.full((8, 21, 128, 128), NEG, np.float32)

    def tile(j, a):
        t = np.full((8, 128, 128), NEG, np.float32)
        for pk in range(2):
            rk = 2 * a + pk
            for pq in range(2):
                rq = 2 * j + pq
                if not (row_start[rq] <= rk < row_start[rq] + 8):
                    continue
                ro = rk - rq + 7
                blk = rpb[:, ro][:, col_off]
                blk = np.where(col_in[None], blk, np.float32(NEG))
                t[:, pk * 64:(pk + 1) * 64, pq * 64:(pq + 1) * 64] = blk.transpose(0, 2, 1)
        return t

    for i, a in enumerate(_na_tiles(5)):
        out[:, i] = tile(5, a)
    for j in (0, 1, 14, 15):
        base = _na_tile_index(j)
        for i, a in enumerate(_na_tiles(j)):
            out[:, base + i] = tile(j, a)
    return np.ascontiguousarray(out.transpose(0, 2, 1, 3))


def _rope_tables():
    t = np.arange(SEQ)
    row = (t // GW).astype(np.float32)
    col = (t % GW).astype(np.float32)
    inv = (np.float32(10000.0) ** (-np.arange(0, 32, 2, dtype=np.float32) / np.float32(32))).astype(np.float32)
    ang = np.concatenate([row[:, None] * inv, col[:, None] * inv], axis=-1).astype(np.float32)
    cos = np.cos(ang).astype(np.float32)
    sin = np.sin(ang).astype(np.float32)
    p = np.arange(128)
    d = p % 64
    ai = (d // 32) * 16 + d % 16
    sgn = np.where(d % 32 < 16, -1.0, 1.0).astype(np.float32)
    C = np.ascontiguousarray(cos[:, ai].T)
    S = np.ascontiguousarray((sin[:, ai] * sgn[None, :]).T)
    return C.astype(np.float32), S.astype(np.float32)


class _Stop(Exception):
    pass


def build(nlayers=2, nb=2, debug=False, stop=None):
    nc = bass.Bass("TRN2", target_bir_lowering=False)
    S = Sched()

    def din(name, shape):
        return nc.dram_tensor(name, list(shape), F32, kind="ExternalInput").ap()

    x2 = din("x2", [2, SEQ, D])
    ctx2 = din("ctx2", [2, NCTX, D])
    cvec = din("cvec", [128, 24])
    ada_w = din("ada_w", [2, D, 6 * D])
    smd = din("sm", [128, SMN])
    w_in0 = din("w_in0", [D, 2560])
    bvbc = din("bvbc", [128, 512])
    nab = din("nab", [8, 128, 21 * 128])
    w_out0 = din("w_out0", [D, D])
    wq1 = din("wq1", [D, 1024])
    wqs1 = din("wqs1", [D, 1024])
    wk1 = din("wk1", [D, 256])
    wks1 = din("wks1", [D, 256])
    wv1 = din("wv1", [D, 256])
    w_out1 = din("w_out1", [D, D])
    ropeC = din("ropeC", [128, SEQ])
    ropeS = din("ropeS", [128, SEQ])
    maskLU = din("maskLU", [128, 256])
    sinkbc = din("sinkbc", [128, 16])
    wr = din("wr", [2, 128, 160])
    sel = din("sel", [16, 2048])
    ident = din("ident", [128, 128])
    ewg = din("ewg", [2, 16, D, 256])
    ewu = din("ewu", [2, 16, D, 256])
    ewd = din("ewd", [2, 16, 256, D])
    outd = nc.dram_tensor("out", [2, SEQ, D], F32, kind="ExternalOutput").ap()
    Hs = nc.dram_tensor("Hs", [128, 8 * NT], F32, kind="Internal").ap()
    dbg = nc.dram_tensor("dbg", [128, 8 * NT], F32, kind="ExternalOutput").ap() if debug else None
    dbgb = nc.dram_tensor("dbgb", [128, 8 * NT], BF16, kind="ExternalOutput").ap() if debug else None
    Hs3 = Hs.rearrange("p (c t) -> p c t", c=8)

    es = ExitStack()
    cur = [16640]

    acache = {}

    def alloc(name, shape, dt, at=None):
        nbytes = int(np.prod(shape[1:])) * (4 if dt == F32 else 2)
        if at is None:
            at = cur[0]
            cur[0] = (at + nbytes + 63) // 64 * 64
        assert at + nbytes <= 229376, (name, at, nbytes)
        key = (name, at, tuple(shape))
        if key not in acache:
            acache[key] = nc.alloc_sbuf_tensor_at("%s_%d" % (name, len(acache)), list(shape), dt, offset=at)
        return acache[key]

    sm = alloc("sm", [128, SMN], F32)
    id32 = alloc("id32", [128, 128], F32)
    ones1k = alloc("ones1k", [128, 128], BF16)
    ones512 = alloc("ones512", [128, 128], BF16)
    csil = alloc("csil", [128, 24], BF16)
    cv32 = alloc("cv32", [128, 24], F32)
    mod = alloc("mod", [128, 2 * 144], F32)
    mp1 = alloc("mp1", [128, 2 * 144], F32)
    vecs = alloc("vecs", [128, 128], F32)
    selt = alloc("selt", [16, 2048], F32)
    wrt = alloc("wrt", [128, 320], F32)
    esink = alloc("esink", [128, 16], F32)
    R_const = Res("const")
    R_mod = Res("mod")
    R_vecs = Res("vecs")
    base0 = cur[0]

    PS = [es.enter_context(nc.psum_tensor("ps%d" % i, [128, 1024], F32)) for i in range(4)]
    RB = [Res("bank%d" % i) for i in range(8)]

    def bank(k):
        return PS[k // 2][:, (k % 2) * 512:(k % 2) * 512 + 512]

    esem = {e: es.enter_context(nc.semaphore("es_" + e)) for e in ENGS}
    dsems = [es.enter_context(nc.semaphore("ds%d" % i)) for i in range(N_DMA_SEMS)]

    def smc(name, j, n=1):
        o = SMO[name] + j
        return sm[:, o:o + n]

    def modc(l, k, ch, col):
        o = l * 144 + (k * 8 + ch) * 3 + col
        return mod[:, o:o + 1]

    def mp1c(l, k, ch, col):
        o = l * 144 + (k * 8 + ch) * 3 + col
        return mp1[:, o:o + 1]

    VK = {}

    def vslot(kind, ch):
        key = (kind, ch)
        if key not in VK:
            VK[key] = len(VK)
            assert len(VK) <= 128
        o = VK[key]
        return vecs[:, o:o + 1]

    S.add("sp", lambda e: e.dma_start(out=sm[:], in_=smd), writes=[R_const], dma=True)
    S.add("sp", lambda e: e.dma_start(out=id32[:], in_=ident), writes=[R_const], dma=True)
    S.add("sp", lambda e: e.dma_start(out=cv32[:], in_=cvec), writes=[R_const], dma=True)
    S.add("sp", lambda e: e.dma_start(out=selt[:], in_=sel), writes=[R_const], dma=True)
    S.add("sp", lambda e: e.dma_start(out=wrt[:].rearrange("p (l n) -> p l n", l=2), in_=wr.rearrange("l p n -> p l n")), writes=[R_const], dma=True)
    S.add("sp", lambda e: e.dma_start(out=esink[:], in_=sinkbc), writes=[R_const], dma=True)
    S.add("pool", lambda e: e.memset(ones1k[:], 1.0 / 1024.0), writes=[R_const])
    S.add("pool", lambda e: e.memset(ones512[:], 1.0 / 512.0), writes=[R_const])
    S.add("act", lambda e: e.activation(out=csil[:], in_=cv32[:], func=AF.Silu), reads=[R_const], writes=[R_const])
    S.add("act", lambda e: e.activation(out=esink[:], in_=esink[:], func=AF.Exp), reads=[R_const], writes=[R_const])

    adaw = [alloc("adaw%d" % i, [128, 8, 1024], BF16) for i in range(2)]
    R_adaw = [Res("adaw0"), Res("adaw1")]
    pi = 0
    for l in range(nlayers):
        awl = ada_w[l].rearrange("(kc p) n -> p kc n", p=128)
        for piece in range(6):
            s = pi % 2
            pi += 1
            S.add("pool", (lambda e, s=s, awl=awl, piece=piece: e.dma_start(out=adaw[s][:], in_=awl[:, :, piece * 1024:(piece + 1) * 1024])),
                  writes=[R_adaw[s]], dma=True)
            for oc8 in range(8):
                oc = piece * 8 + oc8
                for kc in range(8):
                    S.add("pe", (lambda e, s=s, oc=oc, oc8=oc8, kc=kc: e.matmul(bank(0)[:, oc * 3:oc * 3 + 3], lhsT=adaw[s][:, kc, oc8 * 128:(oc8 + 1) * 128],
                                                                                    rhs=csil[:, kc * 3:kc * 3 + 3], start=(kc == 0), stop=(kc == 7))),
                          reads=[R_adaw[s], R_const], writes=[RB[0]])
        ab = smc("ada_b%d" % l, 0, 48)
        S.add("dve", (lambda e, l=l, ab=ab: e.tensor_tensor(out=mod[:, l * 144:(l + 1) * 144].rearrange("p (a b) -> p a b", b=3),
                                                             in0=bank(0)[:, 0:144].rearrange("p (a b) -> p a b", b=3),
                                                             in1=ab.unsqueeze(2).broadcast_to([128, 48, 3]), op=ALU.add)),
              reads=[RB[0], R_const], writes=[R_mod])
        S.add("dve", (lambda e, l=l: e.tensor_scalar_add(out=mp1[:, l * 144:(l + 1) * 144], in0=mod[:, l * 144:(l + 1) * 144], scalar1=1.0)),
              reads=[R_mod], writes=[R_mod])
    S.barrier()
    cur[0] = base0

    A0 = cur[0]
    hres = alloc("hres", [128, 8, NT], F32)
    qT = alloc("qT", [128, 4, NT], BF16, at=A0)
    kT = alloc("kT", [128, 4, NT], BF16, at=A0 + 18432)
    vaug = alloc("vaug", [128, 18, 4, 192], BF16, at=A0 + 36864)
    qT1 = alloc("qT1", [128, 8, SEQ], BF16, at=A0)
    kT1 = alloc("kT1", [128, 2, NT], BF16, at=A0 + 32768)
    vaug1 = alloc("vaug1", [128, 18, 2, 192], BF16, at=A0 + 41984)
    rC = alloc("rC", [128, SEQ], F32, at=A0 + 55808)
    rS = alloc("rS", [128, SEQ], F32, at=A0 + 55808 + 8192)
    bvb = alloc("bvb", [128, 512], F32, at=A0 + 64512)
    idb = alloc("idb", [128, 128], BF16, at=A0 + 64512 + 2048)
    cmean = alloc("cmean", [128, 256], F32, at=A0 + 64512 + 2304)
    crstd = alloc("crstd", [128, 256], F32, at=A0 + 64512 + 3328)
    UT = alloc("UT", [128, 8, NT], BF16)
    OT0 = cur[0]
    oT = alloc("oT", [128, 8, NT], BF16)
    C0 = cur[0]
    RT = {}

    def rtok(name, t0, t1):
        out = []
        for tt in range(t0 // 128, (t1 + 127) // 128):
            key = (name, tt)
            if key not in RT:
                RT[key] = Res("%s_%d" % key)
            out.append(RT[key])
        return out

    R_hpad = [Res("hpad%d" % c) for c in range(4)]

    def derive_vecs(l, col, tag):
        ops = []
        for ch in range(8):
            g1 = smc("ln_g", (l * 2 + 0) * 8 + ch)
            b1 = smc("ln_b", (l * 2 + 0) * 8 + ch)
            g2 = smc("ln_g", (l * 2 + 1) * 8 + ch)
            b2 = smc("ln_b", (l * 2 + 1) * 8 + ch)
            S.add("dve", (lambda e, ch=ch, g1=g1: e.tensor_tensor(out=vslot((tag, "G4"), ch), in0=g1, in1=mp1c(l, 4, ch, col), op=ALU.mult)),
                  reads=[R_const, R_mod], writes=[R_vecs])
            S.add("dve", (lambda e, ch=ch, b1=b1: e.scalar_tensor_tensor(out=vslot((tag, "B4"), ch), in0=b1, scalar=mp1c(l, 4, ch, col), in1=modc(l, 3, ch, col),
                                                                         op0=ALU.mult, op1=ALU.add)),
                  reads=[R_const, R_mod], writes=[R_vecs])
            S.add("dve", (lambda e, ch=ch, g1=g1: e.tensor_scalar_mul(out=vslot((tag, "GA1"), ch), in0=g1, scalar1=ALPHA)), reads=[R_const], writes=[R_vecs])
            S.add("dve", (lambda e, ch=ch, b1=b1: e.tensor_scalar_mul(out=vslot((tag, "BA1"), ch), in0=b1, scalar1=ALPHA)), reads=[R_const], writes=[R_vecs])
            if l == 0:
                S.add("dve", (lambda e, ch=ch: e.tensor_tensor(out=vslot((tag, "M2B"), ch), in0=modc(l, 2, ch, col), in1=smc("b_out", ch), op=ALU.mult)),
                      reads=[R_const, R_mod], writes=[R_vecs])
                S.add("dve", (lambda e, ch=ch, g2=g2: e.tensor_tensor(out=vslot((tag, "GU"), ch), in0=g2, in1=mp1c(1, 1, ch, col), op=ALU.mult)),
                      reads=[R_const, R_mod], writes=[R_vecs])
                S.add("dve", (lambda e, ch=ch, b2=b2: e.scalar_tensor_tensor(out=vslot((tag, "BU"), ch), in0=b2, scalar=mp1c(1, 1, ch, col), in1=modc(1, 0, ch, col),
                                                                             op0=ALU.mult, op1=ALU.add)),
                      reads=[R_const, R_mod], writes=[R_vecs])

    def ln_part1(pre_ap, nch, N, ones_t, prebf, presq, bk, r_pre, r_tmp):
        S.add("dve", lambda e: e.tensor_copy(out=prebf[:, 0:nch, 0:N], in_=pre_ap), reads=r_pre, writes=[r_tmp[0]])
        S.add("act", lambda e: e.activation(out=presq[:, 0:nch, 0:N], in_=pre_ap, func=AF.Square), reads=r_pre, writes=[r_tmp[1]])
        for c in range(nch):
            S.add("pe", (lambda e, c=c: e.matmul(bank(bk)[:, 0:N], lhsT=ones_t[:], rhs=prebf[:, c, 0:N], start=(c == 0), stop=(c == nch - 1))),
                  reads=[r_tmp[0], R_const], writes=[RB[bk]])
        for c in range(nch):
            S.add("pe", (lambda e, c=c: e.matmul(bank(bk)[:, 256:256 + N], lhsT=ones_t[:], rhs=presq[:, c, 0:N], start=(c == 0), stop=(c == nch - 1))),
                  reads=[r_tmp[1], R_const], writes=[RB[bk]])

    def ln_part2(N, mean_sb, rstd_sb, bk, r_tmp):
        S.add("act", lambda e: e.copy(out=mean_sb[:, 0:N], in_=bank(bk)[:, 0:N]), reads=[RB[bk]], writes=[r_tmp[2]])
        S.add("dve", lambda e: e.tensor_tensor(out=rstd_sb[:, 0:N], in0=mean_sb[:, 0:N], in1=mean_sb[:, 0:N], op=ALU.mult), reads=[r_tmp[2]], writes=[r_tmp[3]])
        S.add("dve", lambda e: e.tensor_tensor(out=rstd_sb[:, 0:N], in0=bank(bk)[:, 256:256 + N], in1=rstd_sb[:, 0:N], op=ALU.subtract),
              reads=[RB[bk], r_tmp[3]], writes=[r_tmp[3]])
        S.add("act", lambda e: e.activation(out=rstd_sb[:, 0:N], in_=rstd_sb[:, 0:N], func=AF.Ln, bias=smc("eps", 0), scale=1.0),
              reads=[r_tmp[3], R_const], writes=[r_tmp[3]])
        S.add("act", lambda e: e.activation(out=rstd_sb[:, 0:N], in_=rstd_sb[:, 0:N], func=AF.Exp, scale=-0.5), reads=[r_tmp[3]], writes=[r_tmp[3]])

    def ln_stats(pre_ap, nch, N, ones_t, prebf, presq, mean_sb, rstd_sb, bk, r_pre, r_tmp):
        ln_part1(pre_ap, nch, N, ones_t, prebf, presq, bk, r_pre, r_tmp)
        ln_part2(N, mean_sb, rstd_sb, bk, r_tmp)

    def normalize(pre_ap, nch, N, mean_sb, rstd_sb, r_pre, r_tmp):
        S.add("dve", lambda e: e.tensor_tensor(out=pre_ap, in0=pre_ap, in1=mean_sb[:, 0:N].unsqueeze(1).broadcast_to([128, nch, N]), op=ALU.subtract),
              reads=r_pre + [r_tmp[2]], writes=r_pre)
        S.add("dve", lambda e: e.tensor_tensor(out=pre_ap, in0=pre_ap, in1=rstd_sb[:, 0:N].unsqueeze(1).broadcast_to([128, nch, N]), op=ALU.mult),
              reads=r_pre + [r_tmp[3]], writes=r_pre)

    aff_rr = [0]

    def affine(out_ap, in_ap, sc, bi, reads, writes, psum_in=False):
        k = aff_rr[0] % 2
        aff_rr[0] += 1
        if k == 0:
            S.add("act", lambda e: e.activation(out=out_ap, in_=in_ap, func=AF.Identity, bias=bi, scale=sc), reads=reads, writes=writes)
        else:
            S.add("dve" if k == 1 else "pool", lambda e: e.tensor_scalar(out=out_ap, in0=in_ap, scalar1=sc, scalar2=bi, op0=ALU.mult, op1=ALU.add),
                  reads=reads, writes=writes)

    def dump(name, src_ap3):
        if stop != name and stop != "%s@%d" % (name, cur_l[0]):
            return
        S.barrier()
        c, t = src_ap3.shape[1], src_ap3.shape[2]
        dst = dbg if src_ap3.dtype == F32 else dbgb
        for ci in range(c):
            S.add("sp", (lambda e, ci=ci: e.dma_start(out=dst[:, ci * t:(ci + 1) * t], in_=src_ap3[:, ci, :])), writes=[Res("dbgo")], dma=True)
        raise _Stop()

    cur_l = [0]
    try:
      for b in range(nb):
        for l in range(nlayers):
              cur_l[0] = l
              lat_only = (l == nlayers - 1) and l == 1
              col = b
              S.barrier()
              derive_vecs(l, b, "lat")
              if l == 0:
                  derive_vecs(l, 2, "ctx")

              def V(kind, ch, t0):
                  return vslot((("ctx" if (t0 < NCTX and l == 0) else "lat"), kind), ch)

              def mcol(t0):
                  return 2 if t0 < NCTX else b

              cur[0] = C0
              if l == 0:
                  xin = [alloc("xin%d" % i, [128, D], F32) for i in range(2)]
                  hblk = [alloc("hblk%d" % i, [128, 8, 128], F32) for i in range(2)]
                  R_xin = [Res("xin0"), Res("xin1")]
                  R_hblk = [Res("hblk0"), Res("hblk1")]
                  for tt in range(18):
                      s = tt % 2
                      src = ctx2[b, tt * 128:(tt + 1) * 128, :] if tt < 2 else x2[b, (tt - 2) * 128:(tt - 1) * 128, :]
                      S.add("sp", (lambda e, s=s, src=src: e.dma_start(out=xin[s][:], in_=src)), writes=[R_xin[s]], dma=True)
                      for ch in range(8):
                          bk = (tt % 2) * 2 + ch // 4
                          S.add("pe", (lambda e, s=s, ch=ch, bk=bk: e.transpose(bank(bk)[:, (ch % 4) * 128:(ch % 4 + 1) * 128], xin[s][:, ch * 128:(ch + 1) * 128], id32[:])),
                                reads=[R_xin[s], R_const], writes=[RB[bk]])
                      for hf in range(2):
                          bk = (tt % 2) * 2 + hf
                          S.add("act", (lambda e, s=s, hf=hf, bk=bk: e.copy(out=hblk[s][:, hf * 4:(hf + 1) * 4, :], in_=bank(bk).rearrange("p (c t) -> p c t", c=4))),
                                reads=[RB[bk]], writes=[R_hblk[s]])
                      mc = mcol(tt * 128)
                      for ch in range(8):
                          bk = (tt % 2) * 2 + ch // 4
                          affine(UT[:, ch, tt * 128:(tt + 1) * 128], bank(bk)[:, (ch % 4) * 128:(ch % 4 + 1) * 128], mp1c(0, 1, ch, mc), modc(0, 0, ch, mc),
                                 [RB[bk], R_mod], rtok("UT", tt * 128, tt * 128 + 128), psum_in=True)
                      S.add("sp", (lambda e, s=s, tt=tt: e.dma_start(out=Hs3[:, :, tt * 128:(tt + 1) * 128], in_=hblk[s][:])), reads=[R_hblk[s]],
                            writes=rtok("Hs", tt * 128, tt * 128 + 128), dma=True)

              if l == 0:
                  dump("S0", UT[:])
              blocks512 = [(0, 256)] + [(256 + 512 * i, 512) for i in range(4)]
              blocks256 = [(256 * i, 256) for i in range(9)]
              if l == 1:
                  lat512 = [(256 + 512 * i, 512) for i in range(4)]
                  lat256 = [(256 * i, 256) for i in range(1, 9)]

              if l == 0:
                  S.barrier()
                  cur[0] = C0
                  wAB = alloc("wAB", [128, 8, 1536], BF16)
                  wA = alloc("wA", [128, 8, 1024], BF16, at=C0)
                  hpd = [alloc("hpd%d" % i, [128, 2368], BF16, at=C0 + 16384 + i * 4736) for i in range(2)]
                  dg0 = alloc("diag0", [128, 31, 128], BF16, at=C0 + 25856)
                  diag = [dg0, dg0]
                  cur[0] = C0 + 33792
                  sgt = [alloc("sgt%d" % i, [128, 512], F32) for i in range(2)]
                  czsq = alloc("czsq", [128, 4, 256], BF16)
                  cz = alloc("cz", [128, 4, 256], F32)
                  R_wAB, R_misc = Res("wAB"), Res("misc0")
                  R_hpd = [Res("hpd0"), Res("hpd1")]
                  R_dg = [Res("dg0")] * 2
                  R_sgt = [Res("sgt0"), Res("sgt1")]
                  R_cz, R_ct = Res("cz"), [Res("czbf"), Res("czsq"), Res("cmean"), Res("crstd")]
                  w0 = w_in0.rearrange("(kc p) n -> p kc n", p=128)
                  S.add("pool", lambda e: e.dma_start(out=wA[:], in_=w0[:, :, 0:1024]), writes=[R_wAB], dma=True)
                  S.add("pool", lambda e: e.dma_start(out=idb[:], in_=ident), writes=[R_misc], dma=True)
                  S.add("sp", lambda e: e.dma_start(out=bvb[:], in_=bvbc), writes=[R_misc], dma=True)
                  S.add("pool", lambda e: e.memset(hpd[0][:], 0.0), writes=[R_hpd[0]])
                  S.add("pool", lambda e: e.memset(hpd[1][:], 0.0), writes=[R_hpd[1]])
                  S.add("pool", lambda e: e.memset(vaug[:], 1.0), writes=rtok("vaug", 0, NT))

                  def hoff(t0):
                      return 15 + t0 if t0 < NCTX else 286 + 15 + (t0 - NCTX)

                  it = 0
                  for cc in range(4):
                      hs_ = cc % 2
                      for k in range(31):
                          S.add("dve", (lambda e, cc=cc, k=k, hs_=hs_: e.tensor_scalar_mul(out=diag[hs_][:, k, :], in0=idb[:], scalar1=smc("conv_w", cc * 31 + k))),
                                reads=[R_misc, R_const], writes=[R_dg[hs_]])
                      for (t0, N) in blocks512:
                          s = it % 2
                          it += 1
                          b1, b2 = 2 * s, 2 * s + 1
                          for kc in range(8):
                              S.add("pe", (lambda e, kc=kc, cc=cc, t0=t0, N=N, b1=b1: e.matmul(bank(b1)[:, 0:N], lhsT=wA[:, kc, cc * 128:(cc + 1) * 128], rhs=UT[:, kc, t0:t0 + N],
                                                                                             start=(kc == 0), stop=(kc == 7))),
                                    reads=[R_wAB] + rtok("UT", t0, t0 + N), writes=[RB[b1]])
                          for kc in range(8):
                              S.add("pe", (lambda e, kc=kc, cc=cc, t0=t0, N=N, b2=b2: e.matmul(bank(b2)[:, 0:N], lhsT=wA[:, kc, 512 + cc * 128:512 + (cc + 1) * 128], rhs=UT[:, kc, t0:t0 + N],
                                                                                             start=(kc == 0), stop=(kc == 7))),
                                    reads=[R_wAB] + rtok("UT", t0, t0 + N), writes=[RB[b2]])
                          S.add("act", (lambda e, s=s, cc=cc, N=N, b2=b2: e.activation(out=sgt[s][:, 0:N], in_=bank(b2)[:, 0:N], func=AF.Sigmoid, bias=smc("b_in", 4 + cc), scale=1.0)),
                                reads=[RB[b2], R_const], writes=[R_sgt[s]])
                          ho = hoff(t0)
                          S.add("dve", (lambda e, s=s, cc=cc, N=N, b1=b1, ho=ho, hs_=hs_: e.scalar_tensor_tensor(out=hpd[hs_][:, ho:ho + N], in0=bank(b1)[:, 0:N], scalar=smc("b_in", cc),
                                                                                                                 in1=sgt[s][:, 0:N], op0=ALU.add, op1=ALU.mult)),
                                reads=[RB[b1], R_sgt[s], R_const], writes=[R_hpd[hs_]])
                      for bi, (t0, N) in enumerate(blocks512):
                          ho = hoff(t0) - 15
                          bk = 4 + bi % 2
                          for k in range(31):
                              S.add("pe", (lambda e, k=k, ho=ho, N=N, bk=bk, hs_=hs_: e.matmul(bank(bk)[:, 0:N], lhsT=diag[hs_][:, k, :], rhs=hpd[hs_][:, ho + k:ho + k + N],
                                                                                           start=(k == 0), stop=(k == 30))),
                                    reads=[R_dg[hs_], R_hpd[hs_]], writes=[RB[bk]])
                          S.add("act", (lambda e, cc=cc, N=N, t0=t0, bk=bk: e.activation(out=oT[:, cc, t0:t0 + N], in_=bank(bk)[:, 0:N], func=AF.Identity, bias=smc("conv_b", cc), scale=1.0)),
                                reads=[RB[bk], R_const], writes=rtok("oT", t0, t0 + N))
                  dump("S1z", oT[:, 0:4, :])
                  dump("S1h", hpd[1][:].unsqueeze(1))
                  dump("S1d", diag[0][:])
                  S.add("pool", lambda e: e.dma_start(out=wAB[:], in_=w0[:, :, 1024:2560]), writes=[R_wAB] + R_hpd + [R_dg[0]], dma=True)
                  for (t0, N) in blocks256:
                      zin = oT[:, 0:4, t0:t0 + 256]
                      rz = rtok("oT", t0, t0 + 256)
                      S.add("act", (lambda e, zin=zin: e.activation(out=czsq[:], in_=zin, func=AF.Square)), reads=rz, writes=[R_ct[1]])
                      for c4 in range(4):
                          S.add("pe", (lambda e, c4=c4, t0=t0: e.matmul(bank(6)[:, 0:256], lhsT=ones512[:], rhs=oT[:, c4, t0:t0 + 256], start=(c4 == 0), stop=(c4 == 3))),
                                reads=rz + [R_const], writes=[RB[6]])
                      for c4 in range(4):
                          S.add("pe", (lambda e, c4=c4: e.matmul(bank(6)[:, 256:512], lhsT=ones512[:], rhs=czsq[:, c4, :], start=(c4 == 0), stop=(c4 == 3))),
                                reads=[R_ct[1], R_const], writes=[RB[6]])
                      S.add("act", lambda e: e.copy(out=cmean[:], in_=bank(6)[:, 0:256]), reads=[RB[6]], writes=[R_ct[2]])
                      S.add("dve", lambda e: e.tensor_tensor(out=crstd[:], in0=cmean[:], in1=cmean[:], op=ALU.mult), reads=[R_ct[2]], writes=[R_ct[3]])
                      S.add("dve", lambda e: e.tensor_tensor(out=crstd[:], in0=bank(6)[:, 256:512], in1=crstd[:], op=ALU.subtract), reads=[RB[6], R_ct[3]], writes=[R_ct[3]])
                      S.add("act", lambda e: e.activation(out=crstd[:], in_=crstd[:], func=AF.Sqrt, bias=smc("eps", 0), scale=1.0), reads=[R_ct[3], R_const], writes=[R_ct[3]])
                      S.add("dve", lambda e: e.reciprocal(out=crstd[:], in_=crstd[:]), reads=[R_ct[3]], writes=[R_ct[3]])
                      S.add("dve", (lambda e, zin=zin: e.tensor_tensor(out=cz[:], in0=zin, in1=cmean[:].unsqueeze(1).broadcast_to([128, 4, 256]), op=ALU.subtract)),
                            reads=rz + [R_ct[2]], writes=[R_cz])
                      S.add("dve", lambda e: e.tensor_tensor(out=cz[:], in0=cz[:], in1=crstd[:].unsqueeze(1).broadcast_to([128, 4, 256]), op=ALU.mult),
                            reads=[R_cz, R_ct[3]], writes=[R_cz])
                      for c4 in range(4):
                          S.add("act", (lambda e, c4=c4, t0=t0: e.activation(out=oT[:, c4, t0:t0 + 256], in_=cz[:, c4, :], func=AF.Silu, bias=smc("cln_b", c4), scale=smc("cln_g", c4))),
                                reads=[R_cz, R_const], writes=rz)
                  it = 0
                  for (t0, N) in blocks512:
                      for c in range(8):
                          bk = it % 4
                          it += 1
                          for kc in range(8):
                              S.add("pe", (lambda e, kc=kc, c=c, t0=t0, N=N, bk=bk: e.matmul(bank(bk)[:, 0:N], lhsT=wAB[:, kc, c * 128:(c + 1) * 128], rhs=UT[:, kc, t0:t0 + N],
                                                                                           start=(kc == 0), stop=(kc == 7))),
                                    reads=[R_wAB] + rtok("UT", t0, t0 + N), writes=[RB[bk]])
                          dst = qT if c < 4 else kT
                          S.add("act", (lambda e, c=c, t0=t0, N=N, bk=bk, dst=dst: e.activation(out=dst[:, c % 4, t0:t0 + N], in_=bank(bk)[:, 0:N], func=AF.Identity,
                                                                                              bias=smc("b_in", 8 + c), scale=1.0)),
                                reads=[RB[bk], R_const], writes=rtok("qk", t0, t0 + N))
                  for tt in range(18):
                      bk = it % 4
                      it += 1
                      for kc in range(8):
                          S.add("pe", (lambda e, kc=kc, tt=tt, bk=bk: e.matmul(bank(bk)[:, 0:512], lhsT=UT[:, kc, tt * 128:(tt + 1) * 128], rhs=wAB[:, kc, 1024:1536],
                                                                             start=(kc == 0), stop=(kc == 7))),
                                reads=[R_wAB] + rtok("UT", tt * 128, tt * 128 + 128), writes=[RB[bk]])
                      for x in range(2):
                          S.add("dve", (lambda e, tt=tt, bk=bk, x=x: e.tensor_tensor(out=vaug[:, tt, :, x * 128:x * 128 + 64],
                                                                                   in0=bank(bk).rearrange("p (c x d) -> p c x d", c=4, x=2)[:, :, x, :],
                                                                                   in1=bvb[:].rearrange("p (c x d) -> p c x d", c=4, x=2)[:, :, x, :], op=ALU.add)),
                                reads=[RB[bk], R_misc], writes=rtok("vaug", tt * 128, tt * 128 + 128))

                  dump("S1o", oT[:, 0:4, :])
                  dump("S1q", qT[:])
                  dump("S1k", kT[:])
                  dump("S1v", vaug[:].rearrange("p t c x -> p t (c x)"))
                  S.barrier()
                  cur[0] = C0
                  nabt = [alloc("nabt%d" % i, [128, 21, 128], F32) for i in range(2)]
                  sbt = [alloc("sbt%d" % i, [128, 640], F32) for i in range(3)]
                  PT = [alloc("PT%d" % i, [128, 896], BF16) for i in range(3)]
                  rec = [alloc("rec%d" % i, [128, 256], F32) for i in range(2)]
                  R_nabt = [Res("nabt0"), Res("nabt1")]
                  R_sbt = [Res("sbt0"), Res("sbt1"), Res("sbt2")]
                  R_PT = [Res("PT0"), Res("PT1"), Res("PT2")]
                  PSl = [PS[0], PS[1], PS[3]]
                  PSb = [0, 2, 6]
                  itl = 0
                  na_q = []
                  na_qB = []
                  R_rec = [Res("rec0"), Res("rec1")]
                  it = 0
                  na_pend = [None]
                  for h in range(8):
                      while na_q or na_qB:
                          if na_qB:
                              na_qB.pop(0)()
                          if na_q:
                              na_q.pop(0)()
                      c, sh = h // 2, h % 2
                      p0, p1 = sh * 64, sh * 64 + 64
                      nh0, nh1 = (0, 64) if sh == 0 else (64, 128)
                      dh0, dh1 = (64, 128) if sh == 0 else (0, 64)
                      hs = h % 2
                      S.add("sp", (lambda e, h=h, hs=hs: e.dma_start(out=nabt[hs][:], in_=nab[h].rearrange("p (a q) -> p a q", a=21))), writes=[R_nabt[hs]], dma=True)
                      vcol = sh * 64
                      s = 0
                      sb0 = 0
                      for i in range(2):
                          S.add("pe", (lambda e, i=i, c=c, p0=p0, p1=p1, sb0=sb0: e.matmul(bank(sb0)[:, i * 256:(i + 1) * 256], lhsT=kT[p0:p1, c, i * 128:(i + 1) * 128],
                                                                                          rhs=qT[p0:p1, c, 0:256], start=True, stop=True)),
                                reads=rtok("qk", 0, 256), writes=[RB[sb0]])
                      S.add("act", (lambda e, s=s, sb0=sb0: e.activation(out=PT[s][:, 0:512], in_=bank(sb0)[:, 0:512], func=AF.Exp, scale=0.125)), reads=[RB[sb0]], writes=[R_PT[s]])
                      ob = 4 + s
                      for i in range(2):
                          S.add("pe", (lambda e, i=i, c=c, s=s, ob=ob, vcol=vcol: e.matmul(bank(ob)[:, 0:256], lhsT=vaug[:, i, c, vcol:vcol + 128], rhs=PT[s][:, i * 256:(i + 1) * 256],
                                                                                          start=(i == 0), stop=(i == 1))),
                                reads=[R_PT[s]] + rtok("vaug", 0, 256), writes=[RB[ob]])
                      S.add("dve", (lambda e, s=s, ob=ob, dh0=dh0, dh1=dh1: e.reciprocal(out=rec[s][dh0:dh1, 0:256], in_=bank(ob)[dh0:dh1, 0:256])), reads=[RB[ob]], writes=[R_rec[s]])
                      S.add("dve", (lambda e, s=s, ob=ob, c=c, nh0=nh0, nh1=nh1, dh0=dh0, dh1=dh1: e.tensor_tensor(out=oT[nh0:nh1, 4 + c, 0:256], in0=bank(ob)[nh0:nh1, 0:256],
                                                                                                               in1=rec[s][dh0:dh1, 0:256], op=ALU.mult)),
                            reads=[RB[ob], R_rec[s]], writes=rtok("oT", 0, 256))
                      for j in range(16):
                          s = itl % 3
                          so = itl % 2
                          itl += 1
                          sb0 = PSb[s]
                          tl = _na_tiles(j)
                          nl = len(tl)
                          ti0 = _na_tile_index(j)
                          q0 = NCTX + j * 128
                          ktoks = [NCTX + a * 128 for a in tl] + [0, 128]
                          for i, kt0 in enumerate(ktoks):
                              bk = sb0 + (i // 4)
                              S.add("pe", (lambda e, i=i, kt0=kt0, bk=bk, c=c, p0=p0, p1=p1, q0=q0: e.matmul(bank(bk)[:, (i % 4) * 128:(i % 4 + 1) * 128], lhsT=kT[p0:p1, c, kt0:kt0 + 128],
                                                                                                        rhs=qT[p0:p1, c, q0:q0 + 128], start=True, stop=True)),
                                    reads=rtok("qk", kt0, kt0 + 128) + rtok("qk", q0, q0 + 128), writes=[RB[bk]])
                          S.add("dve", (lambda e, s=s, nl=nl, ti0=ti0, hs=hs: e.scalar_tensor_tensor(out=sbt[s][:, 0:nl * 128], in0=PSl[s][:, 0:nl * 128], scalar=0.125,
                                                                                                    in1=nabt[hs][:, ti0:ti0 + nl, :].rearrange("p a q -> p (a q)"),
                                                                                                    op0=ALU.mult, op1=ALU.add)),
                                reads=[RB[sb0], RB[sb0 + 1], R_nabt[hs]], writes=[R_sbt[s]])
                          S.add("act", (lambda e, s=s, nl=nl: e.activation(out=PT[s][:, 0:nl * 128], in_=sbt[s][:, 0:nl * 128], func=AF.Exp)), reads=[R_sbt[s]], writes=[R_PT[s]])
                          S.add("act", (lambda e, s=s, nl=nl: e.activation(out=PT[s][:, nl * 128:(nl + 2) * 128], in_=PSl[s][:, nl * 128:(nl + 2) * 128], func=AF.Exp, scale=0.125)),
                                reads=[RB[sb0], RB[sb0 + 1]], writes=[R_PT[s]])
                          ob = 4 + so

                          def na_back(ktoks=ktoks, c=c, s=s, so=so, ob=ob, vcol=vcol, nl=nl, q0=q0, nh0=nh0, nh1=nh1, dh0=dh0, dh1=dh1):
                              for i, kt0 in enumerate(ktoks):
                                  S.add("pe", (lambda e, i=i, kt0=kt0: e.matmul(bank(ob)[:, 0:128], lhsT=vaug[:, kt0 // 128, c, vcol:vcol + 128],
                                                                                rhs=PT[s][:, i * 128:(i + 1) * 128], start=(i == 0), stop=(i == nl + 1))),
                                        reads=[R_PT[s]] + rtok("vaug", kt0, kt0 + 128), writes=[RB[ob]])
                              S.add("act", (lambda e: e.activation(out=rec[so][dh0:dh1, 0:128], in_=bank(ob)[dh0:dh1, 0:128], func=AF.Ln)), reads=[RB[ob]], writes=[R_rec[so]])
                              S.add("act", (lambda e: e.activation(out=rec[so][dh0:dh1, 0:128], in_=rec[so][dh0:dh1, 0:128], func=AF.Exp, scale=-1.0)), reads=[R_rec[so]], writes=[R_rec[so]])

                              def na_backB():
                                  S.add("dve", (lambda e: e.tensor_tensor(out=oT[nh0:nh1, 4 + c, q0:q0 + 128], in0=bank(ob)[nh0:nh1, 0:128],
                                                                          in1=rec[so][dh0:dh1, 0:128], op=ALU.mult)),
                                        reads=[RB[ob], R_rec[so]], writes=rtok("oT", q0, q0 + 128))
                              na_qB.append(na_backB)
                          if na_qB:
                              na_qB.pop(0)()
                          na_q.append(na_back)
                          if len(na_q) > 1:
                              na_q.pop(0)()
                  while na_q or na_qB:
                      if na_qB:
                          na_qB.pop(0)()
                      if na_q:
                          na_q.pop(0)()
                  dump("S3", oT[:, 4:8, :])
                  w_out_d = w_out0
                  tok_blocks = blocks256
              else:
                  S.barrier()
                  cur[0] = C0
                  wq = alloc("wq", [128, 8, 512], BF16)
                  wqs = alloc("wqs", [128, 8, 512], BF16)
                  wk = alloc("wk", [128, 8, 256], BF16)
                  wks = alloc("wks", [128, 8, 256], BF16)
                  wv = alloc("wv", [128, 8, 256], BF16)
                  rt = [alloc("rt%d" % i, [128, 2, 512], F32) for i in range(2)]
                  R_w1, R_rope = Res("w1"), Res("rope")
                  R_rt = [Res("rt0"), Res("rt1")]
                  R_wq = Res("wq")
                  for dst, src in ((wk, wk1), (wks, wks1), (wv, wv1)):
                      S.add("pool", (lambda e, dst=dst, src=src: e.dma_start(out=dst[:], in_=src.rearrange("(kc p) n -> p kc n", p=128))), writes=[R_w1], dma=True)
                  S.add("sp", lambda e: e.dma_start(out=rC[:], in_=ropeC), writes=[R_rope], dma=True)
                  S.add("sp", lambda e: e.dma_start(out=rS[:], in_=ropeS), writes=[R_rope], dma=True)
                  if True:
                      S.add("pool", lambda e: e.memset(vaug1[:], 1.0), writes=rtok("vaug", 0, NT))
                  it = 0
                  for half, (t0, N) in [(hf_, blk_) for hf_ in range(3) for blk_ in lat512]:
                      l0 = t0 - NCTX
                      if half < 2 and t0 == NCTX:
                          for dst, src in ((wq, wq1), (wqs, wqs1)):
                              S.add("pool", (lambda e, dst=dst, src=src, half=half: e.dma_start(out=dst[:], in_=src.rearrange("(kc p) n -> p kc n", p=128)[:, :, half * 512:(half + 1) * 512])),
                                    writes=[R_wq], dma=True)
                      for c in (range(half * 4, half * 4 + 4) if half < 2 else range(8, 10)):
                          s = it % 2
                          it += 1
                          b1, b2 = 2 * s, 2 * s + 1
                          wa, wb = (wq, wqs) if c < 8 else (wk, wks)
                          cc = (c % 4) if c < 8 else c - 8
                          for kc in range(8):
                              S.add("pe", (lambda e, kc=kc, cc=cc, wa=wa, t0=t0, N=N, b1=b1: e.matmul(bank(b1)[:, 0:N], lhsT=wa[:, kc, cc * 128:(cc + 1) * 128], rhs=UT[:, kc, t0:t0 + N],
                                                                                                 start=(kc == 0), stop=(kc == 7))),
                                    reads=[R_w1, R_wq] + rtok("UT", t0, t0 + N), writes=[RB[b1]])
                          for kc in range(8):
                              S.add("pe", (lambda e, kc=kc, cc=cc, wb=wb, t0=t0, N=N, b2=b2: e.matmul(bank(b2)[:, 0:N], lhsT=wb[:, kc, cc * 128:(cc + 1) * 128], rhs=UT[:, kc, t0:t0 + N],
                                                                                                 start=(kc == 0), stop=(kc == 7))),
                                    reads=[R_w1, R_wq] + rtok("UT", t0, t0 + N), writes=[RB[b2]])
                          S.add("dve", (lambda e, s=s, l0=l0, N=N, b1=b1: e.tensor_tensor(out=rt[s][:, 0, 0:N], in0=bank(b1)[:, 0:N], in1=rC[:, l0:l0 + N], op=ALU.mult)),
                                reads=[RB[b1], R_rope], writes=[R_rt[s]])
                          S.add("dve", (lambda e, s=s, l0=l0, N=N, b2=b2: e.tensor_tensor(out=rt[s][:, 1, 0:N], in0=bank(b2)[:, 0:N], in1=rS[:, l0:l0 + N], op=ALU.mult)),
                                reads=[RB[b2], R_rope], writes=[R_rt[s]])
                          if c < 8:
                              dst = qT1[:, c, l0:l0 + N]
                          else:
                              dst = kT1[:, c - 8, t0:t0 + N]
                          S.add("dve", (lambda e, s=s, N=N, dst=dst: e.tensor_tensor(out=dst, in0=rt[s][:, 0, 0:N], in1=rt[s][:, 1, 0:N], op=ALU.add)),
                                reads=[R_rt[s]], writes=rtok("qk", t0, t0 + N))
                  for cc in range(2):
                      s = it % 2
                      it += 1
                      b1 = 2 * s
                      for kc in range(8):
                          S.add("pe", (lambda e, kc=kc, cc=cc, b1=b1: e.matmul(bank(b1)[:, 0:256], lhsT=wk[:, kc, cc * 128:(cc + 1) * 128], rhs=UT[:, kc, 0:256], start=(kc == 0), stop=(kc == 7))),
                                reads=[R_w1] + rtok("UT", 0, 256), writes=[RB[b1]])
                      S.add("act", (lambda e, cc=cc, b1=b1: e.copy(out=kT1[:, cc, 0:256], in_=bank(b1)[:, 0:256])), reads=[RB[b1]], writes=rtok("qk", 0, 256))
                  for tt in range(18):
                      bk = 4 + tt % 2
                      for kc in range(8):
                          S.add("pe", (lambda e, kc=kc, tt=tt, bk=bk: e.matmul(bank(bk)[:, 0:256], lhsT=UT[:, kc, tt * 128:(tt + 1) * 128], rhs=wv[:, kc, :], start=(kc == 0), stop=(kc == 7))),
                                reads=[R_w1] + rtok("UT", tt * 128, tt * 128 + 128), writes=[RB[bk]])
                      for x in range(2):
                          S.add("act", (lambda e, tt=tt, bk=bk, x=x: e.copy(out=vaug1[:, tt, :, x * 128:x * 128 + 64],
                                                                          in_=bank(bk)[:, 0:256].rearrange("p (c x d) -> p c x d", c=2, x=2)[:, :, x, :])),
                                reads=[RB[bk]], writes=rtok("vaug", tt * 128, tt * 128 + 128))
                  dump("P1", kT1[:])
                  S.barrier()
                  cur[0] = C0
                  mlu = alloc("mlu", [128, 256], F32)
                  S.add("sp", lambda e: e.dma_start(out=mlu[:], in_=maskLU), writes=[R_const], dma=True)
                  sbm = [alloc("sbm%d" % i, [128, 512], F32) for i in range(2)]
                  PT1 = [alloc("PT1_%d" % i, [128, 5, 512], BF16) for i in range(2)]
                  rec1 = [alloc("rec1_%d" % i, [128, 512], F32) for i in range(2)]
                  R_sbm = [Res("sbm0"), Res("sbm1")]
                  R_PT1 = [[Res("PT1_%d_%d" % (i, k)) for k in range(5)] for i in range(2)]
                  R_rec1 = [Res("rec1_0"), Res("rec1_1")]
                  it = 0
                  sbr = 0
                  mi = 0
                  gq_pend = [None]
                  gq_qB = []
                  for g in range(4):
                      m, sh = g // 2, g % 2
                      p0, p1 = sh * 64, sh * 64 + 64
                      nh0, nh1 = (0, 64) if sh == 0 else (64, 128)
                      dh0, dh1 = (64, 128) if sh == 0 else (0, 64)
                      vcol = sh * 64
                      for qb in range(16):
                          s = it % 2
                          it += 1
                          tiles = []
                          if qb > 0:
                              tiles.append((NCTX + (qb - 1) * 128, 0))
                          tiles.append((NCTX + qb * 128, None))
                          if qb < 15:
                              tiles.append((NCTX + (qb + 1) * 128, 1))
                          tiles += [(0, None), (128, None)]
                          nt = len(tiles)
                          for i, (kt0, mk) in enumerate(tiles):
                              bk = sbr % 4
                              sbr += 1
                              S.add("pe", (lambda e, kt0=kt0, bk=bk, m=m, p0=p0, p1=p1, qb=qb: e.matmul(bank(bk).rearrange("p (h q) -> p h q", h=4), lhsT=kT1[p0:p1, m, kt0:kt0 + 128],
                                                                                                   rhs=qT1[p0:p1, 4 * m:4 * m + 4, qb * 128:(qb + 1) * 128], start=True, stop=True)),
                                    reads=rtok("qk", kt0, kt0 + 128) + rtok("qk", NCTX + qb * 128, NCTX + qb * 128 + 128), writes=[RB[bk]])
                              if mk is None:
                                  S.add("act", (lambda e, s=s, i=i, bk=bk: e.activation(out=PT1[s][:, i, :], in_=bank(bk), func=AF.Exp, scale=0.125)), reads=[RB[bk]], writes=[R_PT1[s][i]])
                              else:
                                  ms = mi % 2
                                  mi += 1
                                  S.add("dve", (lambda e, ms=ms, mk=mk, bk=bk: e.scalar_tensor_tensor(out=sbm[ms][:].rearrange("p (h q) -> p h q", h=4), in0=bank(bk).rearrange("p (h q) -> p h q", h=4),
                                                                                                    scalar=0.125, in1=mlu[:, mk * 128:(mk + 1) * 128].unsqueeze(1).broadcast_to([128, 4, 128]),
                                                                                                    op0=ALU.mult, op1=ALU.add)),
                                        reads=[RB[bk], R_const], writes=[R_sbm[ms]])
                                  S.add("act", (lambda e, s=s, i=i, ms=ms: e.activation(out=PT1[s][:, i, :], in_=sbm[ms][:], func=AF.Exp)), reads=[R_sbm[ms]], writes=[R_PT1[s][i]])
                          ob = 4 + s
                          q0 = NCTX + qb * 128

                          def gq_back(tiles=tiles, s=s, ob=ob, m=m, sh=sh, vcol=vcol, nt=nt, q0=q0, nh0=nh0, nh1=nh1, dh0=dh0, dh1=dh1):
                              for i, (kt0, mk) in enumerate(tiles):
                                  S.add("pe", (lambda e, i=i, kt0=kt0: e.matmul(bank(ob), lhsT=vaug1[:, kt0 // 128, m, vcol:vcol + 128], rhs=PT1[s][:, i, :],
                                                                                start=(i == 0), stop=(i == nt - 1))),
                                        reads=[R_PT1[s][i]] + rtok("vaug", kt0, kt0 + 128), writes=[RB[ob]])
                              S.add("dve", (lambda e: e.tensor_tensor(out=rec1[s][dh0:dh1, :].rearrange("p (h q) -> p h q", h=4),
                                                                      in0=bank(ob)[dh0:dh1, :].rearrange("p (h q) -> p h q", h=4),
                                                                      in1=esink[dh0:dh1, (m * 2 + sh) * 4:(m * 2 + sh) * 4 + 4].unsqueeze(2).broadcast_to([64, 4, 128]),
                                                                      op=ALU.add)),
                                    reads=[RB[ob], R_const], writes=[R_rec1[s]])
                              S.add("act", (lambda e: e.activation(out=rec1[s][dh0:dh1, :], in_=rec1[s][dh0:dh1, :], func=AF.Ln)), reads=[R_rec1[s]], writes=[R_rec1[s]])
                              S.add("act", (lambda e: e.activation(out=rec1[s][dh0:dh1, :], in_=rec1[s][dh0:dh1, :], func=AF.Exp, scale=-1.0)), reads=[R_rec1[s]], writes=[R_rec1[s]])
                              def gq_backB():
                                  S.add("dve", (lambda e: e.tensor_tensor(out=oT[nh0:nh1, 4 * m:4 * m + 4, q0:q0 + 128],
                                                                          in0=bank(ob)[nh0:nh1, :].rearrange("p (h q) -> p h q", h=4),
                                                                          in1=rec1[s][dh0:dh1, :].rearrange("p (h q) -> p h q", h=4), op=ALU.mult)),
                                        reads=[RB[ob], R_rec1[s]], writes=rtok("oT", q0, q0 + 128))
                              gq_qB.append(gq_backB)
                          if gq_qB:
                              gq_qB.pop(0)()
                          if gq_pend[0] is not None:
                              gq_pend[0]()
                          gq_pend[0] = gq_back
                  if gq_qB:
                      gq_qB.pop(0)()
                  gq_pend[0]()
                  gq_pend[0] = None
                  while gq_qB:
                      gq_qB.pop(0)()
                  dump("A1", oT[:])
                  w_out_d = w_out1
                  tok_blocks = lat256

              S.barrier()
              cur[0] = C0
              gatesT = alloc("gatesT", [16, NT], F32)
              wo = alloc("wo", [128, 8, D], BF16)
              hold0_at = cur[0]
              hold = [alloc("hold%d" % i, [128, 8, 128], F32) for i in range(2)]
              pre = alloc("pre", [128, 8, 128], F32)
              prebf = alloc("prebf", [128, 8, 128], BF16)
              presq = alloc("presq", [128, 8, 128], BF16)
              t32 = alloc("t32", [128, 8, 128], F32)
              tmpo = [alloc("tmpo%d" % i, [128, 128], F32) for i in range(2)]
              mean_sb = alloc("mean_sb", [128, 128], F32)
              rstd_sb = alloc("rstd_sb", [128, 128], F32)
              lsb = alloc("lsb", [128, 18, 20], F32, at=hold0_at)
              rw = alloc("rw", [128, 18 * 96], F32, at=hold0_at + 1472)
              assert hold0_at + 1472 + 18 * 96 * 4 <= cur[0]
              R_wo = Res("wo")
              R_hold = [Res("hold0"), Res("hold1")]
              R_pre, R_t32 = Res("pre"), Res("t32")
              R_tmpo = [Res("tmpo0"), Res("tmpo1")]
              R_lt = [Res("prebf"), Res("presq"), Res("mean"), Res("rstd")]
              R_rout = Res("rout")
              S.add("pool", (lambda e, w_out_d=w_out_d: e.dma_start(out=wo[:], in_=w_out_d.rearrange("(kc p) n -> p kc n", p=128))), writes=[R_wo], dma=True)
              tiles4 = [t for (t0_, n_) in tok_blocks for t in range(t0_, t0_ + n_, 128)]
              pre2 = alloc("pre2x", [128, 8, 128], F32, at=C0)
              pre_b = [pre, pre2]
              R_pre_b = [R_pre, Res("pre2x")]
              def s4_frontPE(bi):
                  t0 = tiles4[bi]
                  s = bi % 2
                  S.add("sp", (lambda e: e.dma_start(out=hold[s][:], in_=Hs3[:, :, t0:t0 + 128])), reads=rtok("Hs", t0, t0 + 128), writes=[R_hold[s]], dma=True)
                  for oc in range(8):
                      bk = 2 * s + oc // 4
                      co = (oc % 4) * 128
                      for kc in range(8):
                          S.add("pe", (lambda e, kc=kc, oc=oc, bk=bk, co=co: e.matmul(bank(bk)[:, co:co + 128], lhsT=wo[:, kc, oc * 128:(oc + 1) * 128], rhs=oT[:, kc, t0:t0 + 128],
                                                                                    start=(kc == 0), stop=(kc == 7))),
                                reads=[R_wo] + rtok("oT", t0, t0 + 128), writes=[RB[bk]])

              def s4_frontEV(bi, l=l):
                  t0 = tiles4[bi]
                  s = bi % 2
                  mc = mcol(t0)
                  pb, rpb = pre_b[s], R_pre_b[s]
                  for oc in range(8):
                      bk = 2 * s + oc // 4
                      co = (oc % 4) * 128
                      ts = oc % 2
                      if l == 0:
                          vb = V("M2B", oc, t0)
                          S.add("act", (lambda e, oc=oc, bk=bk, co=co, ts=ts, vb=vb: e.activation(out=tmpo[ts][:], in_=bank(bk)[:, co:co + 128], func=AF.Identity,
                                                                                            bias=vb, scale=modc(0, 2, oc, mc))),
                                reads=[RB[bk], R_mod, R_vecs], writes=[R_tmpo[ts]])
                      else:
                          S.add("act", (lambda e, oc=oc, bk=bk, co=co, ts=ts: e.activation(out=tmpo[ts][:], in_=bank(bk)[:, co:co + 128], func=AF.Identity,
                                                                                     scale=modc(1, 2, oc, mc))),
                                reads=[RB[bk], R_mod], writes=[R_tmpo[ts]])
                      S.add("dve", (lambda e, oc=oc, ts=ts: e.scalar_tensor_tensor(out=pb[:, oc, :], in0=hold[s][:, oc, :], scalar=ALPHA, in1=tmpo[ts][:], op0=ALU.mult, op1=ALU.add)),
                            reads=[R_hold[s], R_tmpo[ts]], writes=[rpb])

              def s4_part1(bi):
                  s = bi % 2
                  ln_part1(pre_b[s][:], 8, 128, ones1k, prebf, presq, 4, [R_pre_b[s]], R_lt)

              def s4_part2(bi, l=l):
                  t0 = tiles4[bi]
                  tt = t0 // 128
                  s = bi % 2
                  pb, rpb = pre_b[s], R_pre_b[s]
                  ln_part2(128, mean_sb, rstd_sb, 4, R_lt)
                  normalize(pb[:], 8, 128, mean_sb, rstd_sb, [rpb], R_lt)
                  for ch in range(8):
                      affine(UT[:, ch, t0:t0 + 128], pb[:, ch, :], V("G4", ch, t0), V("B4", ch, t0), [rpb, R_vecs], rtok("UT", t0, t0 + 128))
                      affine(t32[:, ch, :], pb[:, ch, :], V("G4", ch, t0), V("B4", ch, t0), [rpb, R_vecs], [R_t32])
                      affine(hres[:, ch, t0:t0 + 128], pb[:, ch, :], V("GA1", ch, t0), V("BA1", ch, t0), [rpb, R_vecs], rtok("hres%d" % ch, t0, t0 + 128))
                  for kc in range(8):
                      S.add("pe", (lambda e, kc=kc: e.matmul(bank(5)[:, tt * 20:tt * 20 + 20], lhsT=t32[:, kc, :], rhs=wrt[:, l * 160 + kc * 20:l * 160 + kc * 20 + 20],
                                                             start=(kc == 0), stop=(kc == 7))),
                            reads=[R_t32, R_const], writes=[RB[5]])

              n4 = len(tiles4)
              s4_frontPE(0)
              for bi in range(n4):
                  if bi > 0:
                      s4_part2(bi - 1)
                  s4_frontEV(bi)
                  if bi + 1 < n4:
                      s4_frontPE(bi + 1)
                  s4_part1(bi)
              s4_part2(n4 - 1)

              dump("S4", hres[:])
              dump("S4u", UT[:])
              S.barrier()
              T0 = tok_blocks[0][0] // 128
              T1 = 18
              nT = T1 - T0
              S.add("dve", lambda e: e.tensor_copy(out=lsb[:, T0:T1, :], in_=bank(5)[:, T0 * 20:T1 * 20].rearrange("p (t n) -> p t n", n=20)), reads=[RB[5]], writes=[R_rout])

              def rwv(i, n):
                  return rw[:, i * 18 * 4:(i * 18 * 4) + 18 * n].rearrange("p (t n) -> p t n", n=n)[:, T0:T1, :]

              def rop(fn):
                  S.add("dve", fn, reads=[R_rout], writes=[R_rout])

              lg = lsb[:, T0:T1, 0:4]
              le = lsb[:, T0:T1, 4:20].rearrange("p t (g x) -> p t g x", g=4)
              gmax, gsum, gp, m1, m2, dd, w1, w2 = [rwv(i, 1) for i in range(8)]
              gsh, gmask, elsel, mask1, el2, mask2, within, wa_ = [rwv(8 + i, 4) for i in range(8)]
              t44 = rw[:, 18 * 64:18 * 80].rearrange("p (t g x) -> p t g x", g=4, x=4)[:, T0:T1]
              gates = rw[:, 18 * 80:18 * 96].rearrange("p (t g x) -> p t g x", g=4, x=4)
              bc4 = lambda a: a.broadcast_to([128, nT, 4])
              rop(lambda e: e.tensor_reduce(out=gmax, in_=lg, axis=AX.X, op=ALU.max))
              rop(lambda e: e.tensor_tensor(out=gsh, in0=lg, in1=bc4(gmax), op=ALU.subtract))
              rop(lambda e: e.tensor_tensor(out=gmask, in0=lg, in1=bc4(gmax), op=ALU.is_equal))
              S.add("act", lambda e: e.activation(out=gsh, in_=gsh, func=AF.Exp), reads=[R_rout], writes=[R_rout])
              rop(lambda e: e.tensor_reduce(out=gsum, in_=gsh, axis=AX.X, op=ALU.add))
              rop(lambda e: e.reciprocal(out=gp, in_=gsum))
              rop(lambda e: e.tensor_tensor(out=t44, in0=le, in1=gmask.unsqueeze(3).broadcast_to([128, nT, 4, 4]), op=ALU.mult))
              rop(lambda e: e.tensor_reduce(out=elsel, in_=t44.rearrange("p t g x -> p t x g"), axis=AX.X, op=ALU.add))
              rop(lambda e: e.tensor_reduce(out=m1, in_=elsel, axis=AX.X, op=ALU.max))
              rop(lambda e: e.tensor_tensor(out=mask1, in0=elsel, in1=bc4(m1), op=ALU.is_equal))
              rop(lambda e: e.scalar_tensor_tensor(out=el2, in0=mask1, scalar=NEG, in1=elsel, op0=ALU.mult, op1=ALU.add))
              rop(lambda e: e.tensor_reduce(out=m2, in_=el2, axis=AX.X, op=ALU.max))
              rop(lambda e: e.tensor_tensor(out=mask2, in0=el2, in1=bc4(m2), op=ALU.is_equal))
              rop(lambda e: e.tensor_tensor(out=dd, in0=m2, in1=m1, op=ALU.subtract))
              S.add("act", lambda e: e.activation(out=dd, in_=dd, func=AF.Exp), reads=[R_rout], writes=[R_rout])
              rop(lambda e: e.tensor_scalar_add(out=w1, in0=dd, scalar1=1.0))
              rop(lambda e: e.reciprocal(out=w1, in_=w1))
              rop(lambda e: e.tensor_tensor(out=w1, in0=w1, in1=gp, op=ALU.mult))
              rop(lambda e: e.tensor_tensor(out=w2, in0=dd, in1=w1, op=ALU.mult))
              rop(lambda e: e.tensor_tensor(out=within, in0=mask1, in1=bc4(w1), op=ALU.mult))
              rop(lambda e: e.tensor_tensor(out=wa_, in0=mask2, in1=bc4(w2), op=ALU.mult))
              rop(lambda e: e.tensor_tensor(out=within, in0=within, in1=wa_, op=ALU.add))
              rop(lambda e: e.tensor_tensor(out=gates[:, T0:T1], in0=gmask.unsqueeze(3).broadcast_to([128, nT, 4, 4]), in1=within.unsqueeze(2).broadcast_to([128, nT, 4, 4]), op=ALU.mult))
              for tt in range(T0, T1):
                  bk = 6 + (tt // 4) % 2
                  S.add("pe", (lambda e, tt=tt, bk=bk: e.transpose(bank(bk)[0:16, (tt % 4) * 128:(tt % 4 + 1) * 128], gates[:, tt].rearrange("p g x -> p (g x)"), id32[:])),
                        reads=[R_rout, R_const], writes=[RB[bk]])
                  if tt % 4 == 3 or tt == T1 - 1:
                      ta = (tt // 4) * 4
                      ta0 = max(ta, T0)
                      S.add("act", (lambda e, bk=bk, ta=ta, ta0=ta0, tt=tt: e.copy(out=gatesT[0:16, ta0 * 128:(tt + 1) * 128], in_=bank(bk)[0:16, (ta0 - ta) * 128:(tt + 1 - ta) * 128])),
                            reads=[RB[bk]], writes=[R_rout])

              S.barrier()
              cur[0] = C0
              gatesT = alloc("gatesT", [16, NT], F32)
              wgs = [alloc("wgs%d" % i, [128, 8, 256], BF16) for i in range(2)]
              wus = [alloc("wus%d" % i, [128, 8, 256], BF16) for i in range(2)]
              wds = [alloc("wds%d" % i, [128, 2, D], BF16) for i in range(2)]
              sgm = [alloc("sgm0", [128, 2, 512], F32)]
              hgm = [alloc("hgm%d" % i, [128, 2, 512], BF16) for i in range(4)]
              o_ = OT0
              for i in range(2, 4):
                  wgs.append(alloc("wgs%d" % i, [128, 8, 256], BF16, at=o_)); o_ += 4096
                  wus.append(alloc("wus%d" % i, [128, 8, 256], BF16, at=o_)); o_ += 4096
                  wds.append(alloc("wds%d" % i, [128, 2, D], BF16, at=o_)); o_ += 4096
              sgm.append(alloc("sgm1", [128, 2, 512], F32, at=o_)); o_ += 4096
              gsb = []
              for i in range(2):
                  gsb.append(alloc("gsb%d" % i, [128, 512], F32, at=o_)); o_ += 2048
              assert o_ <= OT0 + 36864
              R_ewg = [Res("ewg%d" % i) for i in range(4)]
              R_ewu = [Res("ewu%d" % i) for i in range(4)]
              R_ewd = [Res("ewd%d" % i) for i in range(4)]
              R_sgm = [Res("sgm0"), Res("sgm1")]
              R_hgm = [Res("hgm%d" % i) for i in range(4)]
              R_gsb = [Res("gsb0"), Res("gsb1")]
              mblocks = blocks512 if l == 0 else lat512

              def load_expert(ex, slot, l=l):
                  S.add("pool", (lambda e: e.dma_start(out=wgs[slot][:], in_=ewg[l, ex].rearrange("(kc p) f -> p kc f", p=128))), writes=[R_ewg[slot]], dma=True)
                  S.add("pool", (lambda e: e.dma_start(out=wus[slot][:], in_=ewu[l, ex].rearrange("(kc p) f -> p kc f", p=128))), writes=[R_ewu[slot]], dma=True)
                  S.add("pool", (lambda e: e.dma_start(out=wds[slot][:], in_=ewd[l, ex].rearrange("(kc p) f -> p kc f", p=128))), writes=[R_ewd[slot]], dma=True)

              def emit_front(ex, slot, t0, N, si):
                  S.add("pe", (lambda e: e.matmul(bank(4)[:, 0:N], lhsT=selt[0:16, ex * 128:(ex + 1) * 128], rhs=gatesT[0:16, t0:t0 + N], start=True, stop=True)),
                        reads=[R_rout, R_const], writes=[RB[4]])
                  S.add("act", (lambda e: e.copy(out=gsb[si][:, 0:N], in_=bank(4)[:, 0:N])), reads=[RB[4]], writes=[R_gsb[si]])
                  for oc in range(4):
                      wsrc = wgs[slot] if oc < 2 else wus[slot]
                      rw_ = R_ewg[slot] if oc < 2 else R_ewu[slot]
                      for kc in range(8):
                          S.add("pe", (lambda e, kc=kc, oc=oc, wsrc=wsrc: e.matmul(bank(oc)[:, 0:N], lhsT=wsrc[:, kc, (oc % 2) * 128:(oc % 2 + 1) * 128], rhs=UT[:, kc, t0:t0 + N],
                                                                                    start=(kc == 0), stop=(kc == 7))),
                                reads=[rw_] + rtok("UT", t0, t0 + N), writes=[RB[oc]])
                  S.add("act", (lambda e: e.activation(out=sgm[si][:, :, 0:N], in_=PS[0][:].rearrange("p (j n) -> p j n", j=2)[:, :, 0:N], func=AF.Silu)),
                        reads=[RB[0], RB[1]], writes=[R_sgm[si]])
                  S.add("dve", (lambda e: e.tensor_tensor(out=sgm[si][:, :, 0:N], in0=sgm[si][:, :, 0:N], in1=PS[1][:].rearrange("p (j n) -> p j n", j=2)[:, :, 0:N], op=ALU.mult)),
                        reads=[RB[2], RB[3], R_sgm[si]], writes=[R_sgm[si]])

              def emit_gate(si, q, N):
                  S.add("dve", (lambda e: e.tensor_tensor(out=hgm[q][:, :, 0:N], in0=sgm[si][:, :, 0:N], in1=gsb[si][:, 0:N].unsqueeze(1).broadcast_to([128, 2, N]), op=ALU.mult)),
                        reads=[R_gsb[si], R_sgm[si]], writes=[R_hgm[q]])

              ybc = [0]

              def make_yhalf(half, slots, qs, t0, N, mc, l=l):
                  def f():
                      for dc in range(half * 4, half * 4 + 4):
                          bk = 5 + ybc[0] % 3
                          ybc[0] += 1
                          for j in range(2):
                              for k2 in range(2):
                                  S.add("pe", (lambda e, j=j, k2=k2, dc=dc, bk=bk: e.matmul(bank(bk)[:, 0:N], lhsT=wds[slots[j]][:, k2, dc * 128:(dc + 1) * 128], rhs=hgm[qs[j]][:, k2, 0:N],
                                                                                           start=(j == 0 and k2 == 0), stop=(j == 1 and k2 == 1))),
                                        reads=[R_ewd[slots[j]], R_hgm[qs[j]]], writes=[RB[bk]])
                          S.add("dve", (lambda e, dc=dc, bk=bk: e.scalar_tensor_tensor(out=hres[:, dc, t0:t0 + N], in0=bank(bk)[:, 0:N], scalar=modc(l, 5, dc, mc),
                                                                                      in1=hres[:, dc, t0:t0 + N], op0=ALU.mult, op1=ALU.add)),
                                reads=[RB[bk], R_mod] + rtok("hres%d" % dc, t0, t0 + N), writes=rtok("hres%d" % dc, t0, t0 + N))
                  return f

              load_expert(0, 0)
              load_expert(1, 1)
              pendA = pendB = None
              it = 0
              for pr in range(8):
                  slots = (2 * (pr % 2), 2 * (pr % 2) + 1)
                  for bi, (t0, N) in enumerate(mblocks):
                      qs = ((it % 2) * 2, (it % 2) * 2 + 1)
                      it += 1
                      mc = mcol(t0)
                      emit_front(2 * pr, slots[0], t0, N, 0)
                      if pendA is not None:
                          pendA()
                      emit_gate(0, qs[0], N)
                      emit_front(2 * pr + 1, slots[1], t0, N, 1)
                      if pendB is not None:
                          pendB()
                      emit_gate(1, qs[1], N)
                      pendA = make_yhalf(0, slots, qs, t0, N, mc)
                      pendB = make_yhalf(1, slots, qs, t0, N, mc)
                      if bi == 0 and pr + 1 < 8:
                          nslots = (2 * ((pr + 1) % 2), 2 * ((pr + 1) % 2) + 1)
                          load_expert(2 * pr + 2, nslots[0])
                          load_expert(2 * pr + 3, nslots[1])
              pendA()
              pendB()

              dump("S5", hres[:])
              S.barrier()
              cur[0] = C0
              pre2 = alloc("pre2", [128, 8, 256], BF16)
              presq2 = alloc("presq2", [128, 8, 256], BF16)
              mean2 = alloc("mean2", [128, 256], F32)
              rstd2 = alloc("rstd2", [128, 256], F32)
              otile = [alloc("otile%d" % i, [128, D], F32) for i in range(2)]
              R_l2 = [Res("pre2"), Res("presq2"), Res("mean2"), Res("rstd2")]
              R_ot = [Res("ot0"), Res("ot1")]
              oi = 0
              def s6_part1(bi):
                  t0_ = tok_blocks[bi][0]
                  rh_ = [r_ for ch_ in range(8) for r_ in rtok("hres%d" % ch_, t0_, t0_ + 256)]
                  ln_part1(hres[:, :, t0_:t0_ + 256], 8, 256, ones1k, pre2, presq2, 4 + bi % 2, rh_, R_l2)

              s6_part1(0)
              for bi, (t0, N) in enumerate(tok_blocks):
                  hap = hres[:, :, t0:t0 + 256]
                  rh = [r_ for ch_ in range(8) for r_ in rtok("hres%d" % ch_, t0, t0 + 256)]
                  if bi + 1 < len(tok_blocks):
                      s6_part1(bi + 1)
                  ln_part2(256, mean2, rstd2, 4 + bi % 2, R_l2)
                  normalize(hap, 8, 256, mean2, rstd2, rh, R_l2)
                  if l == 0:
                      for ch in range(8):
                          affine(UT[:, ch, t0:t0 + 256], hres[:, ch, t0:t0 + 256], V("GU", ch, t0), V("BU", ch, t0), rh + [R_vecs], rtok("UT", t0, t0 + 256))
                      for ch in range(8):
                          affine(hres[:, ch, t0:t0 + 256], hres[:, ch, t0:t0 + 256], smc("ln_g", 8 + ch), smc("ln_b", 8 + ch), rh + [R_const], rh)
                      S.add("sp", (lambda e, t0=t0: e.dma_start(out=Hs3[:, :, t0:t0 + 256], in_=hres[:, :, t0:t0 + 256])), reads=rh, writes=rtok("Hs", t0, t0 + 256), dma=True)
                      if debug and nlayers == 1:
                          S.add("sp", (lambda e, t0=t0: e.dma_start(out=dbg.rearrange("p (c t) -> p c t", c=8)[:, :, t0:t0 + 256], in_=hres[:, :, t0:t0 + 256])), reads=rh,
                                writes=[Res("dbgo")], dma=True)
                  else:
                      for ch in range(8):
                          affine(hres[:, ch, t0:t0 + 256], hres[:, ch, t0:t0 + 256], smc("ln_g", 24 + ch), smc("ln_b", 24 + ch), rh + [R_const], rh)
                      for hh in range(2):
                          tk = t0 + hh * 128
                          so = oi % 2
                          oi += 1
                          for ch in range(8):
                              bk = so * 2 + ch // 4
                              S.add("pe", (lambda e, ch=ch, bk=bk, tk=tk: e.transpose(bank(bk)[:, (ch % 4) * 128:(ch % 4 + 1) * 128], hres[:, ch, tk:tk + 128], id32[:])),
                                    reads=rh + [R_const], writes=[RB[bk]])
                          for hf in range(2):
                              bk = so * 2 + hf
                              S.add("act" if hf else "dve", (lambda e, so=so, hf=hf, bk=bk: (e.copy if hf else e.tensor_copy)(out=otile[so][:, hf * 512:(hf + 1) * 512], in_=bank(bk))),
                                    reads=[RB[bk]], writes=[R_ot[so]])
                          S.add("sp", (lambda e, so=so, tk=tk, b=b: e.dma_start(out=outd[b, tk - NCTX:tk - NCTX + 128, :], in_=otile[so][:])), reads=[R_ot[so]], writes=[Res("outw")], dma=True)
    except _Stop:
        pass
    S.barrier()

    with nc.Block() as block:
        @block.tensor
        def _(e):
            S.emit_one("pe", e, esem, dsems)

        @block.scalar
        def _(e):
            S.emit_one("act", e, esem, dsems)

        @block.vector
        def _(e):
            S.emit_one("dve", e, esem, dsems)

        @block.gpsimd
        def _(e):
            S.emit_one("pool", e, esem, dsems)

        @block.sync
        def _(e):
            S.emit_one("sp", e, esem, dsems)
    es.close()
    return nc


def _prep_shared(inp):
    f = lambda a: np.ascontiguousarray(np.asarray(a, np.float32))
    sm = np.zeros((128, SMN), np.float32)

    def put(name, arr):
        arr = np.asarray(arr, np.float32)
        sm[:, SMO[name]:SMO[name] + arr.shape[1]] = arr

    put("ada_b0", _fm(inp["ada_b"][0]))
    put("ada_b1", _fm(inp["ada_b"][1]))
    put("ln_g", np.concatenate([_fm(inp["ln_g"][l, k]) for l in range(2) for k in range(2)], axis=1))
    put("ln_b", np.concatenate([_fm(inp["ln_b"][l, k]) for l in range(2) for k in range(2)], axis=1))
    b_in = np.asarray(inp["ab_b_in"][0], np.float32)
    put("b_in", _fm(b_in[:2048]))
    cw = np.asarray(inp["conv_w"][0], np.float32)
    put("conv_w", np.ascontiguousarray(cw.T.reshape(4, 128, 31).transpose(1, 0, 2).reshape(128, 124)))
    put("conv_b", _fm(inp["conv_b"][0]))
    put("cln_g", _fm(inp["conv_ln_g"][0]))
    put("cln_b", _fm(inp["conv_ln_b"][0]))
    put("b_out", _fm(inp["ab_b_out"][0]))
    sm[:, SMO["eps"]] = EPS
    qidx = _gqa_qidx()
    gw = np.asarray(inp["gqa_w_in"][0], np.float32)
    wq = gw[:, :1024]
    wkk = gw[:, 1024:1280]
    wvv = gw[:, 1280:1536]
    C, Sg = _rope_tables()
    kk = np.arange(128)[:, None]
    qq = np.arange(128)[None, :]
    maskL = np.where(kk >= qq, 0.0, NEG).astype(np.float32)
    maskU = np.where(kk <= qq, 0.0, NEG).astype(np.float32)
    sink = np.asarray(inp["gqa_sink"][0], np.float32)
    sperm = np.array([8 * m + 4 * sh + j for m in range(2) for sh in range(2) for j in range(4)])
    sel = np.zeros((16, 16, 128), np.float32)
    for ex in range(16):
        sel[ex, ex, :] = 1.0
    wr = np.stack([np.concatenate([np.asarray(inp["router_group"][l], np.float32), np.asarray(inp["router_expert"][l], np.float32)], axis=1)
                   .reshape(8, 128, 20).transpose(1, 0, 2).reshape(128, 160) for l in range(2)])
    bv = b_in[2048:2560]
    shared = {
        "ada_w": f(inp["ada_w"]),
        "sm": sm,
        "w_in0": f(inp["ab_w_in"][0]),
        "bvbc": np.ascontiguousarray(np.broadcast_to(bv[None, :], (128, 512))),
        "nab": np.ascontiguousarray(_na_bias_table(np.asarray(inp["na_rpb"][0], np.float32)).reshape(8, 128, 21 * 128)),
        "w_out0": f(inp["ab_w_out"][0]),
        "wq1": f(wq[:, qidx]),
        "wqs1": f(wq[:, qidx][:, _swap64(1024)]),
        "wk1": f(wkk),
        "wks1": f(wkk[:, _swap64(256)]),
        "wv1": f(wvv),
        "w_out1": f(np.asarray(inp["gqa_w_out"][0], np.float32)[qidx, :]),
        "ropeC": C,
        "ropeS": Sg,
        "maskLU": np.ascontiguousarray(np.concatenate([maskL, maskU], axis=1)),
        "sinkbc": np.ascontiguousarray(np.broadcast_to(sink[sperm][None, :], (128, 16))),
        "wr": f(wr),
        "sel": np.ascontiguousarray(sel.reshape(16, 2048)),
        "ident": np.eye(128, dtype=np.float32),
        "ewg": f(inp["exp_w_gate"]),
        "ewu": f(inp["exp_w_up"]),
        "ewd": f(inp["exp_w_down"]),
    }
    return shared


def _core_inputs(inp, shared, i):
    x = np.asarray(inp["x"], np.float32)
    ctx = np.asarray(inp["ctx"], np.float32)
    c = np.asarray(inp["c"], np.float32)
    cc = np.stack([c[2 * i], c[2 * i + 1], np.asarray(inp["c_ctx"], np.float32)])
    cvec = np.ascontiguousarray(cc.reshape(3, 8, 128).transpose(2, 1, 0).reshape(128, 24))
    m = dict(shared)
    m["x2"] = np.ascontiguousarray(x[2 * i:2 * i + 2])
    m["ctx2"] = np.ascontiguousarray(ctx[2 * i:2 * i + 2])
    m["cvec"] = cvec
    return m


_NC_CACHE = {}


def kernel(**inputs):
    n = 8
    if "nc" not in _NC_CACHE:
        _NC_CACHE["nc"] = build()
    nc = _NC_CACHE["nc"]
    shared = _prep_shared(inputs)
    in_maps = [_core_inputs(inputs, shared, i) for i in range(n)]
    res = run_bass_kernel_spmd(nc, in_maps, core_ids=list(range(n)))
    out = np.concatenate([np.asarray(r["out"], np.float32) for r in res.results], axis=0)
    return out
```

```python
import numpy as np
from contextlib import ExitStack
import concourse.bass as bass
import concourse.mybir as mybir
from concourse.bass_utils import run_bass_kernel_spmd

F32 = mybir.dt.float32
BF16 = mybir.dt.bfloat16
AF = mybir.ActivationFunctionType
ALU = mybir.AluOpType
AX = mybir.AxisListType

D = 1024
SEQ = 2048
NCTX = 256
NT = SEQ + NCTX
GW = 64
ALPHA = 4.0 ** 0.25
EPS = 1e-5
NEG = -1e30

ENGS = ("pe", "act", "dve", "pool", "sp")
N_DMA_SEMS = 40


class Res:
    __slots__ = ("name", "last_w", "readers")

    def __init__(self, name):
        self.name = name
        self.last_w = None
        self.readers = []


class Op:
    __slots__ = ("eng", "fn", "idx", "deps", "dma", "sig", "semval", "dsem", "dval", "dprev")

    def __init__(self, eng, fn, idx, dma):
        self.eng = eng
        self.fn = fn
        self.idx = idx
        self.deps = []
        self.dma = dma
        self.sig = False
        self.semval = 0
        self.dsem = -1
        self.dval = 0
        self.dprev = 0


class Sched:
    def __init__(self):
        self.ops = {e: [] for e in ENGS}
        self.ndma = 0
        self.dma_tot = [0] * N_DMA_SEMS
        self.last_dma = [None] * N_DMA_SEMS
        self._assigned = False

    def add(self, eng, fn, reads=(), writes=(), dma=False, extra=()):
        lst = self.ops[eng]
        op = Op(eng, fn, len(lst), dma)
        deps = {}
        for r in reads:
            if r.last_w is not None:
                deps[id(r.last_w)] = r.last_w
        for w in writes:
            if w.last_w is not None:
                deps[id(w.last_w)] = w.last_w
            for rd in w.readers:
                deps[id(rd)] = rd
        for x in extra:
            deps[id(x)] = x
        for r in reads:
            r.readers.append(op)
        for w in writes:
            w.last_w = op
            w.readers = []
        if dma:
            s = self.ndma % N_DMA_SEMS
            self.ndma += 1
            op.dsem = s
            op.dprev = self.dma_tot[s]
            self.dma_tot[s] += 16
            op.dval = self.dma_tot[s]
            self.last_dma[s] = op
        for d in deps.values():
            if d is op:
                continue
            if d.eng == eng and not d.dma and not dma:
                if eng == "pe":
                    continue
                if op.idx - d.idx > 2:
                    continue
            op.deps.append(d)
            if not d.dma:
                d.sig = True
        lst.append(op)
        return op

    def barrier(self):
        lasts = []
        for e in ENGS:
            for op in reversed(self.ops[e]):
                if not op.dma:
                    lasts.append(op)
                    break
        dl = [o for o in self.last_dma if o is not None]
        for e in ENGS:
            self.add(e, lambda eng: eng.nop(), extra=[o for o in lasts if o.eng != e] + dl)

    def emit_one(self, e, eng, esem, dsems):
        if not self._assigned:
            for ee in ENGS:
                c = 0
                for op in self.ops[ee]:
                    if op.sig and not op.dma:
                        c += 1
                        op.semval = c
            self._assigned = True
        seen = {}
        for op in self.ops[e]:
            need = {}
            for d in op.deps:
                if d.dma:
                    key = ("d", d.dsem)
                    val = d.dval
                else:
                    key = ("e", d.eng)
                    val = d.semval
                if val > need.get(key, 0):
                    need[key] = val
            if op.dma and op.dprev > 0:
                key = ("d", op.dsem)
                if op.dprev > need.get(key, 0):
                    need[key] = op.dprev
            for key, val in need.items():
                if seen.get(key, 0) >= val:
                    continue
                seen[key] = val
                sem = dsems[key[1]] if key[0] == "d" else esem[key[1]]
                eng.wait_ge(sem, val)
            ins = op.fn(eng)
            if op.dma:
                ins.then_inc(dsems[op.dsem], 16)
            elif op.sig:
                ins.then_inc(esem[e], 1)


def _sm_layout():
    off = {}
    n = 0
    for name, cols in (("ada_b0", 48), ("ada_b1", 48), ("ln_g", 32), ("ln_b", 32), ("b_in", 16),
                       ("conv_w", 124), ("conv_b", 4), ("cln_g", 4), ("cln_b", 4), ("b_out", 8), ("eps", 1)):
        off[name] = n
        n += cols
    return off, n


SMO, SMN = _sm_layout()


def _fm(v):
    v = np.asarray(v, np.float32)
    return np.ascontiguousarray(v.reshape(-1, 128).T)


def _gqa_qidx():
    idx = np.zeros(1024, np.int64)
    for c in range(8):
        m, j = divmod(c, 4)
        h0 = 8 * m + j
        h1 = 8 * m + 4 + j
        idx[c * 128:c * 128 + 64] = h0 * 64 + np.arange(64)
        idx[c * 128 + 64:c * 128 + 128] = h1 * 64 + np.arange(64)
    return idx


def _swap64(n):
    d = np.arange(n)
    dd = d % 64
    sw = np.where(dd % 32 < 16, dd + 16, dd - 16)
    return (d // 64) * 64 + sw


def _na_tiles(j):
    if j in (0, 1):
        return [0, 1, 2, 3]
    if j in (14, 15):
        return [12, 13, 14, 15]
    return [j - 2, j - 1, j, j + 1, j + 2]


def _na_tile_index(j):
    if j == 0:
        return 5
    if j == 1:
        return 9
    if j == 14:
        return 13
    if j == 15:
        return 17
    return 0


def _na_bias_table(rpb):
    rows = 32
    r = np.arange(rows)
    row_start = np.clip(r - 4, 0, rows - 8)
    jj = np.arange(GW)
    col_start = np.clip(jj - 8, 0, GW - 16)
    col_in = (jj[None, :] >= col_start[:, None]) & (jj[None, :] < col_start[:, None] + 16)
    col_off = np.clip(jj[None, :] - jj[:, None], -15, 15) + 15
    out = np.full((8, 21, 128, 128), NEG, np.float32)

    def tile(j, a):
        t = np.full((8, 128, 128), NEG, np.float32)
        for pk in range(2):
            rk = 2 * a + pk
            for pq in range(2):
                rq = 2 * j + pq
                if not (row_start[rq] <= rk < row_start[rq] + 8):
                    continue
                ro = rk - rq + 7
                blk = rpb[:, ro][:, col_off]
                blk = np.where(col_in[None], blk, np.float32(NEG))
                t[:, pk * 64:(pk + 1) * 64, pq * 64:(pq + 1) * 64] = blk.transpose(0, 2, 1)
        return t

    for i, a in enumerate(_na_tiles(5)):
        out[:, i] = tile(5, a)
    for j in (0, 1, 14, 15):
        base = _na_tile_index(j)
        for i, a in enumerate(_na_tiles(j)):
            out[:, base + i] = tile(j, a)
    return np.ascontiguousarray(out.transpose(0, 2, 1, 3))


def _rope_tables():
    t = np.arange(SEQ)
    row = (t // GW).astype(np.float32)
    col = (t % GW).astype(np.float32)
    inv = (np.float32(10000.0) ** (-np.arange(0, 32, 2, dtype=np.float32) / np.float32(32))).astype(np.float32)
    ang = np.concatenate([row[:, None] * inv, col[:, None] * inv], axis=-1).astype(np.float32)
    cos = np.cos(ang).astype(np.float32)
    sin = np.sin(ang).astype(np.float32)
    p = np.arange(128)
    d = p % 64
    ai = (d // 32) * 16 + d % 16
    sgn = np.where(d % 32 < 16, -1.0, 1.0).astype(np.float32)
    C = np.ascontiguousarray(cos[:, ai].T)
    S = np.ascontiguousarray((sin[:, ai] * sgn[None, :]).T)
    return C.astype(np.float32), S.astype(np.float32)


class _Stop(Exception):
    pass


def build(nlayers=2, nb=2, debug=False, stop=None):
    nc = bass.Bass("TRN2", target_bir_lowering=False)
    S = Sched()

    def din(name, shape):
        return nc.dram_tensor(name, list(shape), F32, kind="ExternalInput").ap()

    x2 = din("x2", [2, SEQ, D])
    ctx2 = din("ctx2", [2, NCTX, D])
    cvec = din("cvec", [128, 24])
    ada_w = din("ada_w", [2, D, 6 * D])
    smd = din("sm", [128, SMN])
    w_in0 = din("w_in0", [D, 2560])
    bvbc = din("bvbc", [128, 512])
    nab = din("nab", [8, 128, 21 * 128])
    w_out0 = din("w_out0", [D, D])
    wq1 = din("wq1", [D, 1024])
    wqs1 = din("wqs1", [D, 1024])
    wk1 = din("wk1", [D, 256])
    wks1 = din("wks1", [D, 256])
    wv1 = din("wv1", [D, 256])
    w_out1 = din("w_out1", [D, D])
    ropeC = din("ropeC", [128, SEQ])
    ropeS = din("ropeS", [128, SEQ])
    maskLU = din("maskLU", [128, 256])
    sinkbc = din("sinkbc", [128, 16])
    wr = din("wr", [2, 128, 160])
    sel = din("sel", [16, 2048])
    ident = din("ident", [128, 128])
    ewg = din("ewg", [2, 16, D, 256])
    ewu = din("ewu", [2, 16, D, 256])
    ewd = din("ewd", [2, 16, 256, D])
    outd = nc.dram_tensor("out", [2, SEQ, D], F32, kind="ExternalOutput").ap()
    Hs = nc.dram_tensor("Hs", [128, 8 * NT], F32, kind="Internal").ap()
    dbg = nc.dram_tensor("dbg", [128, 8 * NT], F32, kind="ExternalOutput").ap() if debug else None
    dbgb = nc.dram_tensor("dbgb", [128, 8 * NT], BF16, kind="ExternalOutput").ap() if debug else None
    Hs3 = Hs.rearrange("p (c t) -> p c t", c=8)

    es = ExitStack()
    cur = [16640]

    acache = {}

    def alloc(name, shape, dt, at=None):
        nbytes = int(np.prod(shape[1:])) * (4 if dt == F32 else 2)
        if at is None:
            at = cur[0]
            cur[0] = (at + nbytes + 63) // 64 * 64
        assert at + nbytes <= 229376, (name, at, nbytes)
        key = (name, at, tuple(shape))
        if key not in acache:
            acache[key] = nc.alloc_sbuf_tensor_at("%s_%d" % (name, len(acache)), list(shape), dt, offset=at)
        return acache[key]

    sm = alloc("sm", [128, SMN], F32)
    id32 = alloc("id32", [128, 128], F32)
    ones1k = alloc("ones1k", [128, 128], BF16)
    ones512 = alloc("ones512", [128, 128], BF16)
    csil = alloc("csil", [128, 24], BF16)
    cv32 = alloc("cv32", [128, 24], F32)
    mod = alloc("mod", [128, 2 * 144], F32)
    mp1 = alloc("mp1", [128, 2 * 144], F32)
    vecs = alloc("vecs", [128, 128], F32)
    selt = alloc("selt", [16, 2048], F32)
    wrt = alloc("wrt", [128, 320], F32)
    esink = alloc("esink", [128, 16], F32)
    R_const = Res("const")
    R_mod = Res("mod")
    R_vecs = Res("vecs")
    base0 = cur[0]

    PS = [es.enter_context(nc.psum_tensor("ps%d" % i, [128, 1024], F32)) for i in range(4)]
    RB = [Res("bank%d" % i) for i in range(8)]

    def bank(k):
        return PS[k // 2][:, (k % 2) * 512:(k % 2) * 512 + 512]

    esem = {e: es.enter_context(nc.semaphore("es_" + e)) for e in ENGS}
    dsems = [es.enter_context(nc.semaphore("ds%d" % i)) for i in range(N_DMA_SEMS)]

    def smc(name, j, n=1):
        o = SMO[name] + j
        return sm[:, o:o + n]

    def modc(l, k, ch, col):
        o = l * 144 + (k * 8 + ch) * 3 + col
        return mod[:, o:o + 1]

    def mp1c(l, k, ch, col):
        o = l * 144 + (k * 8 + ch) * 3 + col
        return mp1[:, o:o + 1]

    VK = {}

    def vslot(kind, ch):
        key = (kind, ch)
        if key not in VK:
            VK[key] = len(VK)
            assert len(VK) <= 128
        o = VK[key]
        return vecs[:, o:o + 1]

    S.add("sp", lambda e: e.dma_start(out=sm[:], in_=smd), writes=[R_const], dma=True)
    S.add("sp", lambda e: e.dma_start(out=id32[:], in_=ident), writes=[R_const], dma=True)
    S.add("sp", lambda e: e.dma_start(out=cv32[:], in_=cvec), writes=[R_const], dma=True)
    S.add("sp", lambda e: e.dma_start(out=selt[:], in_=sel), writes=[R_const], dma=True)
    S.add("sp", lambda e: e.dma_start(out=wrt[:].rearrange("p (l n) -> p l n", l=2), in_=wr.rearrange("l p n -> p l n")), writes=[R_const], dma=True)
    S.add("sp", lambda e: e.dma_start(out=esink[:], in_=sinkbc), writes=[R_const], dma=True)
    S.add("pool", lambda e: e.memset(ones1k[:], 1.0 / 1024.0), writes=[R_const])
    S.add("pool", lambda e: e.memset(ones512[:], 1.0 / 512.0), writes=[R_const])
    S.add("act", lambda e: e.activation(out=csil[:], in_=cv32[:], func=AF.Silu), reads=[R_const], writes=[R_const])
    S.add("act", lambda e: e.activation(out=esink[:], in_=esink[:], func=AF.Exp), reads=[R_const], writes=[R_const])

    adaw = [alloc("adaw%d" % i, [128, 8, 1024], BF16) for i in range(2)]
    R_adaw = [Res("adaw0"), Res("adaw1")]
    pi = 0
    for l in range(nlayers):
        awl = ada_w[l].rearrange("(kc p) n -> p kc n", p=128)
        for piece in range(6):
            s = pi % 2
            pi += 1
            S.add("pool", (lambda e, s=s, awl=awl, piece=piece: e.dma_start(out=adaw[s][:], in_=awl[:, :, piece * 1024:(piece + 1) * 1024])),
                  writes=[R_adaw[s]], dma=True)
            for oc8 in range(8):
                oc = piece * 8 + oc8
                for kc in range(8):
                    S.add("pe", (lambda e, s=s, oc=oc, oc8=oc8, kc=kc: e.matmul(bank(0)[:, oc * 3:oc * 3 + 3], lhsT=adaw[s][:, kc, oc8 * 128:(oc8 + 1) * 128],
                                                                                    rhs=csil[:, kc * 3:kc * 3 + 3], start=(kc == 0), stop=(kc == 7))),
                          reads=[R_adaw[s], R_const], writes=[RB[0]])
        ab = smc("ada_b%d" % l, 0, 48)
        S.add("dve", (lambda e, l=l, ab=ab: e.tensor_tensor(out=mod[:, l * 144:(l + 1) * 144].rearrange("p (a b) -> p a b", b=3),
                                                             in0=bank(0)[:, 0:144].rearrange("p (a b) -> p a b", b=3),
                                                             in1=ab.unsqueeze(2).broadcast_to([128, 48, 3]), op=ALU.add)),
              reads=[RB[0], R_const], writes=[R_mod])
        S.add("dve", (lambda e, l=l: e.tensor_scalar_add(out=mp1[:, l * 144:(l + 1) * 144], in0=mod[:, l * 144:(l + 1) * 144], scalar1=1.0)),
              reads=[R_mod], writes=[R_mod])
    S.barrier()
    cur[0] = base0

    A0 = cur[0]
    hres = alloc("hres", [128, 8, NT], F32)
    qT = alloc("qT", [128, 4, NT], BF16, at=A0)
    kT = alloc("kT", [128, 4, NT], BF16, at=A0 + 18432)
    vaug = alloc("vaug", [128, 18, 4, 192], BF16, at=A0 + 36864)
    qT1 = alloc("qT1", [128, 8, SEQ], BF16, at=A0)
    kT1 = alloc("kT1", [128, 2, NT], BF16, at=A0 + 32768)
    vaug1 = alloc("vaug1", [128, 18, 2, 192], BF16, at=A0 + 41984)
    rC = alloc("rC", [128, SEQ], F32, at=A0 + 55808)
    rS = alloc("rS", [128, SEQ], F32, at=A0 + 55808 + 8192)
    bvb = alloc("bvb", [128, 512], F32, at=A0 + 64512)
    idb = alloc("idb", [128, 128], BF16, at=A0 + 64512 + 2048)
    cmean = alloc("cmean", [128, 256], F32, at=A0 + 64512 + 2304)
    crstd = alloc("crstd", [128, 256], F32, at=A0 + 64512 + 3328)
    UT = alloc("UT", [128, 8, NT], BF16)
    OT0 = cur[0]
    oT = alloc("oT", [128, 8, NT], BF16)
    C0 = cur[0]
    RT = {}

    def rtok(name, t0, t1):
        out = []
        for tt in range(t0 // 128, (t1 + 127) // 128):
            key = (name, tt)
            if key not in RT:
                RT[key] = Res("%s_%d" % key)
            out.append(RT[key])
        return out

    R_hpad = [Res("hpad%d" % c) for c in range(4)]

    def derive_vecs(l, col, tag):
        ops = []
        for ch in range(8):
            g1 = smc("ln_g", (l * 2 + 0) * 8 + ch)
            b1 = smc("ln_b", (l * 2 + 0) * 8 + ch)
            g2 = smc("ln_g", (l * 2 + 1) * 8 + ch)
            b2 = smc("ln_b", (l * 2 + 1) * 8 + ch)
            S.add("dve", (lambda e, ch=ch, g1=g1: e.tensor_tensor(out=vslot((tag, "G4"), ch), in0=g1, in1=mp1c(l, 4, ch, col), op=ALU.mult)),
                  reads=[R_const, R_mod], writes=[R_vecs])
            S.add("dve", (lambda e, ch=ch, b1=b1: e.scalar_tensor_tensor(out=vslot((tag, "B4"), ch), in0=b1, scalar=mp1c(l, 4, ch, col), in1=modc(l, 3, ch, col),
                                                                         op0=ALU.mult, op1=ALU.add)),
                  reads=[R_const, R_mod], writes=[R_vecs])
            S.add("dve", (lambda e, ch=ch, g1=g1: e.tensor_scalar_mul(out=vslot((tag, "GA1"), ch), in0=g1, scalar1=ALPHA)), reads=[R_const], writes=[R_vecs])
            S.add("dve", (lambda e, ch=ch, b1=b1: e.tensor_scalar_mul(out=vslot((tag, "BA1"), ch), in0=b1, scalar1=ALPHA)), reads=[R_const], writes=[R_vecs])
            if l == 0:
                S.add("dve", (lambda e, ch=ch: e.tensor_tensor(out=vslot((tag, "M2B"), ch), in0=modc(l, 2, ch, col), in1=smc("b_out", ch), op=ALU.mult)),
                      reads=[R_const, R_mod], writes=[R_vecs])
                S.add("dve", (lambda e, ch=ch, g2=g2: e.tensor_tensor(out=vslot((tag, "GU"), ch), in0=g2, in1=mp1c(1, 1, ch, col), op=ALU.mult)),
                      reads=[R_const, R_mod], writes=[R_vecs])
                S.add("dve", (lambda e, ch=ch, b2=b2: e.scalar_tensor_tensor(out=vslot((tag, "BU"), ch), in0=b2, scalar=mp1c(1, 1, ch, col), in1=modc(1, 0, ch, col),
                                                                             op0=ALU.mult, op1=ALU.add)),
                      reads=[R_const, R_mod], writes=[R_vecs])

    def ln_part1(pre_ap, nch, N, ones_t, prebf, presq, bk, r_pre, r_tmp):
        S.add("dve", lambda e: e.tensor_copy(out=prebf[:, 0:nch, 0:N], in_=pre_ap), reads=r_pre, writes=[r_tmp[0]])
        S.add("act", lambda e: e.activation(out=presq[:, 0:nch, 0:N], in_=pre_ap, func=AF.Square), reads=r_pre, writes=[r_tmp[1]])
        for c in range(nch):
            S.add("pe", (lambda e, c=c: e.matmul(bank(bk)[:, 0:N], lhsT=ones_t[:], rhs=prebf[:, c, 0:N], start=(c == 0), stop=(c == nch - 1))),
                  reads=[r_tmp[0], R_const], writes=[RB[bk]])
        for c in range(nch):
            S.add("pe", (lambda e, c=c: e.matmul(bank(bk)[:, 256:256 + N], lhsT=ones_t[:], rhs=presq[:, c, 0:N], start=(c == 0), stop=(c == nch - 1))),
                  reads=[r_tmp[1], R_const], writes=[RB[bk]])

    def ln_part2(N, mean_sb, rstd_sb, bk, r_tmp):
        S.add("act", lambda e: e.copy(out=mean_sb[:, 0:N], in_=bank(bk)[:, 0:N]), reads=[RB[bk]], writes=[r_tmp[2]])
        S.add("dve", lambda e: e.tensor_tensor(out=rstd_sb[:, 0:N], in0=mean_sb[:, 0:N], in1=mean_sb[:, 0:N], op=ALU.mult), reads=[r_tmp[2]], writes=[r_tmp[3]])
        S.add("dve", lambda e: e.tensor_tensor(out=rstd_sb[:, 0:N], in0=bank(bk)[:, 256:256 + N], in1=rstd_sb[:, 0:N], op=ALU.subtract),
              reads=[RB[bk], r_tmp[3]], writes=[r_tmp[3]])
        S.add("act", lambda e: e.activation(out=rstd_sb[:, 0:N], in_=rstd_sb[:, 0:N], func=AF.Ln, bias=smc("eps", 0), scale=1.0),
              reads=[r_tmp[3], R_const], writes=[r_tmp[3]])
        S.add("act", lambda e: e.activation(out=rstd_sb[:, 0:N], in_=rstd_sb[:, 0:N], func=AF.Exp, scale=-0.5), reads=[r_tmp[3]], writes=[r_tmp[3]])

    def ln_stats(pre_ap, nch, N, ones_t, prebf, presq, mean_sb, rstd_sb, bk, r_pre, r_tmp):
        ln_part1(pre_ap, nch, N, ones_t, prebf, presq, bk, r_pre, r_tmp)
        ln_part2(N, mean_sb, rstd_sb, bk, r_tmp)

    def normalize(pre_ap, nch, N, mean_sb, rstd_sb, r_pre, r_tmp):
        S.add("dve", lambda e: e.tensor_tensor(out=pre_ap, in0=pre_ap, in1=mean_sb[:, 0:N].unsqueeze(1).broadcast_to([128, nch, N]), op=ALU.subtract),
              reads=r_pre + [r_tmp[2]], writes=r_pre)
        S.add("dve", lambda e: e.tensor_tensor(out=pre_ap, in0=pre_ap, in1=rstd_sb[:, 0:N].unsqueeze(1).broadcast_to([128, nch, N]), op=ALU.mult),
              reads=r_pre + [r_tmp[3]], writes=r_pre)

    aff_rr = [0]

    def affine(out_ap, in_ap, sc, bi, reads, writes, psum_in=False):
        k = aff_rr[0] % 2
        aff_rr[0] += 1
        if k == 0:
            S.add("act", lambda e: e.activation(out=out_ap, in_=in_ap, func=AF.Identity, bias=bi, scale=sc), reads=reads, writes=writes)
        else:
            S.add("dve" if k == 1 else "pool", lambda e: e.tensor_scalar(out=out_ap, in0=in_ap, scalar1=sc, scalar2=bi, op0=ALU.mult, op1=ALU.add),
                  reads=reads, writes=writes)

    def dump(name, src_ap3):
        if stop != name and stop != "%s@%d" % (name, cur_l[0]):
            return
        S.barrier()
        c, t = src_ap3.shape[1], src_ap3.shape[2]
        dst = dbg if src_ap3.dtype == F32 else dbgb
        for ci in range(c):
            S.add("sp", (lambda e, ci=ci: e.dma_start(out=dst[:, ci * t:(ci + 1) * t], in_=src_ap3[:, ci, :])), writes=[Res("dbgo")], dma=True)
        raise _Stop()

    cur_l = [0]
    try:
      for b in range(nb):
        for l in range(nlayers):
              cur_l[0] = l
              lat_only = (l == nlayers - 1) and l == 1
              col = b
              S.barrier()
              derive_vecs(l, b, "lat")
              if l == 0:
                  derive_vecs(l, 2, "ctx")

              def V(kind, ch, t0):
                  return vslot((("ctx" if (t0 < NCTX and l == 0) else "lat"), kind), ch)

              def mcol(t0):
                  return 2 if t0 < NCTX else b

              cur[0] = C0
              if l == 0:
                  xin = [alloc("xin%d" % i, [128, D], F32) for i in range(2)]
                  hblk = [alloc("hblk%d" % i, [128, 8, 128], F32) for i in range(2)]
                  R_xin = [Res("xin0"), Res("xin1")]
                  R_hblk = [Res("hblk0"), Res("hblk1")]
                  for tt in range(18):
                      s = tt % 2
                      src = ctx2[b, tt * 128:(tt + 1) * 128, :] if tt < 2 else x2[b, (tt - 2) * 128:(tt - 1) * 128, :]
                      S.add("sp", (lambda e, s=s, src=src: e.dma_start(out=xin[s][:], in_=src)), writes=[R_xin[s]], dma=True)
                      for ch in range(8):
                          bk = (tt % 2) * 2 + ch // 4
                          S.add("pe", (lambda e, s=s, ch=ch, bk=bk: e.transpose(bank(bk)[:, (ch % 4) * 128:(ch % 4 + 1) * 128], xin[s][:, ch * 128:(ch + 1) * 128], id32[:])),
                                reads=[R_xin[s], R_const], writes=[RB[bk]])
                      for hf in range(2):
                          bk = (tt % 2) * 2 + hf
                          S.add("act", (lambda e, s=s, hf=hf, bk=bk: e.copy(out=hblk[s][:, hf * 4:(hf + 1) * 4, :], in_=bank(bk).rearrange("p (c t) -> p c t", c=4))),
                                reads=[RB[bk]], writes=[R_hblk[s]])
                      mc = mcol(tt * 128)
                      for ch in range(8):
                          bk = (tt % 2) * 2 + ch // 4
                          affine(UT[:, ch, tt * 128:(tt + 1) * 128], bank(bk)[:, (ch % 4) * 128:(ch % 4 + 1) * 128], mp1c(0, 1, ch, mc), modc(0, 0, ch, mc),
                                 [RB[bk], R_mod], rtok("UT", tt * 128, tt * 128 + 128), psum_in=True)
                      S.add("sp", (lambda e, s=s, tt=tt: e.dma_start(out=Hs3[:, :, tt * 128:(tt + 1) * 128], in_=hblk[s][:])), reads=[R_hblk[s]],
                            writes=rtok("Hs", tt * 128, tt * 128 + 128), dma=True)

              if l == 0:
                  dump("S0", UT[:])
              blocks512 = [(0, 256)] + [(256 + 512 * i, 512) for i in range(4)]
              blocks256 = [(256 * i, 256) for i in range(9)]
              if l == 1:
                  lat512 = [(256 + 512 * i, 512) for i in range(4)]
                  lat256 = [(256 * i, 256) for i in range(1, 9)]

              if l == 0:
                  S.barrier()
                  cur[0] = C0
                  wAB = alloc("wAB", [128, 8, 1536], BF16)
                  wA = alloc("wA", [128, 8, 1024], BF16, at=C0)
                  hpd = [alloc("hpd%d" % i, [128, 2368], BF16, at=C0 + 16384 + i * 4736) for i in range(2)]
                  dg0 = alloc("diag0", [128, 31, 128], BF16, at=C0 + 25856)
                  diag = [dg0, dg0]
                  cur[0] = C0 + 33792
                  sgt = [alloc("sgt%d" % i, [128, 512], F32) for i in range(2)]
                  czsq = alloc("czsq", [128, 4, 256], BF16)
                  cz = alloc("cz", [128, 4, 256], F32)
                  R_wAB, R_misc = Res("wAB"), Res("misc0")
                  R_hpd = [Res("hpd0"), Res("hpd1")]
                  R_dg = [Res("dg0")] * 2
                  R_sgt = [Res("sgt0"), Res("sgt1")]
                  R_cz, R_ct = Res("cz"), [Res("czbf"), Res("czsq"), Res("cmean"), Res("crstd")]
                  w0 = w_in0.rearrange("(kc p) n -> p kc n", p=128)
                  S.add("pool", lambda e: e.dma_start(out=wA[:], in_=w0[:, :, 0:1024]), writes=[R_wAB], dma=True)
                  S.add("pool", lambda e: e.dma_start(out=idb[:], in_=ident), writes=[R_misc], dma=True)
                  S.add("sp", lambda e: e.dma_start(out=bvb[:], in_=bvbc), writes=[R_misc], dma=True)
                  S.add("pool", lambda e: e.memset(hpd[0][:], 0.0), writes=[R_hpd[0]])
                  S.add("pool", lambda e: e.memset(hpd[1][:], 0.0), writes=[R_hpd[1]])
                  S.add("pool", lambda e: e.memset(vaug[:], 1.0), writes=rtok("vaug", 0, NT))

                  def hoff(t0):
                      return 15 + t0 if t0 < NCTX else 286 + 15 + (t0 - NCTX)

                  it = 0
                  for cc in range(4):
                      hs_ = cc % 2
                      for k in range(31):
                          S.add("dve", (lambda e, cc=cc, k=k, hs_=hs_: e.tensor_scalar_mul(out=diag[hs_][:, k, :], in0=idb[:], scalar1=smc("conv_w", cc * 31 + k))),
                                reads=[R_misc, R_const], writes=[R_dg[hs_]])
                      for (t0, N) in blocks512:
                          s = it % 2
                          it += 1
                          b1, b2 = 2 * s, 2 * s + 1
                          for kc in range(8):
                              S.add("pe", (lambda e, kc=kc, cc=cc, t0=t0, N=N, b1=b1: e.matmul(bank(b1)[:, 0:N], lhsT=wA[:, kc, cc * 128:(cc + 1) * 128], rhs=UT[:, kc, t0:t0 + N],
                                                                                             start=(kc == 0), stop=(kc == 7))),
                                    reads=[R_wAB] + rtok("UT", t0, t0 + N), writes=[RB[b1]])
                          for kc in range(8):
                              S.add("pe", (lambda e, kc=kc, cc=cc, t0=t0, N=N, b2=b2: e.matmul(bank(b2)[:, 0:N], lhsT=wA[:, kc, 512 + cc * 128:512 + (cc + 1) * 128], rhs=UT[:, kc, t0:t0 + N],
                                                                                             start=(kc == 0), stop=(kc == 7))),
                                    reads=[R_wAB] + rtok("UT", t0, t0 + N), writes=[RB[b2]])
                          S.add("act", (lambda e, s=s, cc=cc, N=N, b2=b2: e.activation(out=sgt[s][:, 0:N], in_=bank(b2)[:, 0:N], func=AF.Sigmoid, bias=smc("b_in", 4 + cc), scale=1.0)),
                                reads=[RB[b2], R_const], writes=[R_sgt[s]])
                          ho = hoff(t0)
                          S.add("dve", (lambda e, s=s, cc=cc, N=N, b1=b1, ho=ho, hs_=hs_: e.scalar_tensor_tensor(out=hpd[hs_][:, ho:ho + N], in0=bank(b1)[:, 0:N], scalar=smc("b_in", cc),
                                                                                                                 in1=sgt[s][:, 0:N], op0=ALU.add, op1=ALU.mult)),
                                reads=[RB[b1], R_sgt[s], R_const], writes=[R_hpd[hs_]])
                      for bi, (t0, N) in enumerate(blocks512):
                          ho = hoff(t0) - 15
                          bk = 4 + bi % 2
                          for k in range(31):
                              S.add("pe", (lambda e, k=k, ho=ho, N=N, bk=bk, hs_=hs_: e.matmul(bank(bk)[:, 0:N], lhsT=diag[hs_][:, k, :], rhs=hpd[hs_][:, ho + k:ho + k + N],
                                                                                           start=(k == 0), stop=(k == 30))),
                                    reads=[R_dg[hs_], R_hpd[hs_]], writes=[RB[bk]])
                          S.add("act", (lambda e, cc=cc, N=N, t0=t0, bk=bk: e.activation(out=oT[:, cc, t0:t0 + N], in_=bank(bk)[:, 0:N], func=AF.Identity, bias=smc("conv_b", cc), scale=1.0)),
                                reads=[RB[bk], R_const], writes=rtok("oT", t0, t0 + N))
                  dump("S1z", oT[:, 0:4, :])
                  dump("S1h", hpd[1][:].unsqueeze(1))
                  dump("S1d", diag[0][:])
                  S.add("pool", lambda e: e.dma_start(out=wAB[:], in_=w0[:, :, 1024:2560]), writes=[R_wAB] + R_hpd + [R_dg[0]], dma=True)
                  for (t0, N) in blocks256:
                      zin = oT[:, 0:4, t0:t0 + 256]
                      rz = rtok("oT", t0, t0 + 256)
                      S.add("act", (lambda e, zin=zin: e.activation(out=czsq[:], in_=zin, func=AF.Square)), reads=rz, writes=[R_ct[1]])
                      for c4 in range(4):
                          S.add("pe", (lambda e, c4=c4, t0=t0: e.matmul(bank(6)[:, 0:256], lhsT=ones512[:], rhs=oT[:, c4, t0:t0 + 256], start=(c4 == 0), stop=(c4 == 3))),
                                reads=rz + [R_const], writes=[RB[6]])
                      for c4 in range(4):
                          S.add("pe", (lambda e, c4=c4: e.matmul(bank(6)[:, 256:512], lhsT=ones512[:], rhs=czsq[:, c4, :], start=(c4 == 0), stop=(c4 == 3))),
                                reads=[R_ct[1], R_const], writes=[RB[6]])
                      S.add("act", lambda e: e.copy(out=cmean[:], in_=bank(6)[:, 0:256]), reads=[RB[6]], writes=[R_ct[2]])
                      S.add("dve", lambda e: e.tensor_tensor(out=crstd[:], in0=cmean[:], in1=cmean[:], op=ALU.mult), reads=[R_ct[2]], writes=[R_ct[3]])
                      S.add("dve", lambda e: e.tensor_tensor(out=crstd[:], in0=bank(6)[:, 256:512], in1=crstd[:], op=ALU.subtract), reads=[RB[6], R_ct[3]], writes=[R_ct[3]])
                      S.add("act", lambda e: e.activation(out=crstd[:], in_=crstd[:], func=AF.Sqrt, bias=smc("eps", 0), scale=1.0), reads=[R_ct[3], R_const], writes=[R_ct[3]])
                      S.add("dve", lambda e: e.reciprocal(out=crstd[:], in_=crstd[:]), reads=[R_ct[3]], writes=[R_ct[3]])
                      S.add("dve", (lambda e, zin=zin: e.tensor_tensor(out=cz[:], in0=zin, in1=cmean[:].unsqueeze(1).broadcast_to([128, 4, 256]), op=ALU.subtract)),
                            reads=rz + [R_ct[2]], writes=[R_cz])
                      S.add("dve", lambda e: e.tensor_tensor(out=cz[:], in0=cz[:], in1=crstd[:].unsqueeze(1).broadcast_to([128, 4, 256]), op=ALU.mult),
                            reads=[R_cz, R_ct[3]], writes=[R_cz])
                      for c4 in range(4):
                          S.add("act", (lambda e, c4=c4, t0=t0: e.activation(out=oT[:, c4, t0:t0 + 256], in_=cz[:, c4, :], func=AF.Silu, bias=smc("cln_b", c4), scale=smc("cln_g", c4))),
                                reads=[R_cz, R_const], writes=rz)
                  it = 0
                  for (t0, N) in blocks512:
                      for c in range(8):
                          bk = it % 4
                          it += 1
                          for kc in range(8):
                              S.add("pe", (lambda e, kc=kc, c=c, t0=t0, N=N, bk=bk: e.matmul(bank(bk)[:, 0:N], lhsT=wAB[:, kc, c * 128:(c + 1) * 128], rhs=UT[:, kc, t0:t0 + N],
                                                                                           start=(kc == 0), stop=(kc == 7))),
                                    reads=[R_wAB] + rtok("UT", t0, t0 + N), writes=[RB[bk]])
                          dst = qT if c < 4 else kT
                          S.add("act", (lambda e, c=c, t0=t0, N=N, bk=bk, dst=dst: e.activation(out=dst[:, c % 4, t0:t0 + N], in_=bank(bk)[:, 0:N], func=AF.Identity,
                                                                                              bias=smc("b_in", 8 + c), scale=1.0)),
                                reads=[RB[bk], R_const], writes=rtok("qk", t0, t0 + N))
                  for tt in range(18):
                      bk = it % 4
                      it += 1
                      for kc in range(8):
                          S.add("pe", (lambda e, kc=kc, tt=tt, bk=bk: e.matmul(bank(bk)[:, 0:512], lhsT=UT[:, kc, tt * 128:(tt + 1) * 128], rhs=wAB[:, kc, 1024:1536],
                                                                             start=(kc == 0), stop=(kc == 7))),
                                reads=[R_wAB] + rtok("UT", tt * 128, tt * 128 + 128), writes=[RB[bk]])
                      for x in range(2):
                          S.add("dve", (lambda e, tt=tt, bk=bk, x=x: e.tensor_tensor(out=vaug[:, tt, :, x * 128:x * 128 + 64],
                                                                                   in0=bank(bk).rearrange("p (c x d) -> p c x d", c=4, x=2)[:, :, x, :],
                                                                                   in1=bvb[:].rearrange("p (c x d) -> p c x d", c=4, x=2)[:, :, x, :], op=ALU.add)),
                                reads=[RB[bk], R_misc], writes=rtok("vaug", tt * 128, tt * 128 + 128))

                  dump("S1o", oT[:, 0:4, :])
                  dump("S1q", qT[:])
                  dump("S1k", kT[:])
                  dump("S1v", vaug[:].rearrange("p t c x -> p t (c x)"))
                  S.barrier()
                  cur[0] = C0
                  nabt = [alloc("nabt%d" % i, [128, 21, 128], F32) for i in range(2)]
                  sbt = [alloc("sbt%d" % i, [128, 640], F32) for i in range(3)]
                  PT = [alloc("PT%d" % i, [128, 896], BF16) for i in range(3)]
                  rec = [alloc("rec%d" % i, [128, 256], F32) for i in range(2)]
                  R_nabt = [Res("nabt0"), Res("nabt1")]
                  R_sbt = [Res("sbt0"), Res("sbt1"), Res("sbt2")]
                  R_PT = [Res("PT0"), Res("PT1"), Res("PT2")]
                  PSl = [PS[0], PS[1], PS[3]]
                  PSb = [0, 2, 6]
                  itl = 0
                  na_q = []
                  na_qB = []
                  R_rec = [Res("rec0"), Res("rec1")]
                  it = 0
                  na_pend = [None]
                  for h in range(8):
                      while na_q or na_qB:
                          if na_qB:
                              na_qB.pop(0)()
                          if na_q:
                              na_q.pop(0)()
                      c, sh = h // 2, h % 2
                      p0, p1 = sh * 64, sh * 64 + 64
                      nh0, nh1 = (0, 64) if sh == 0 else (64, 128)
                      dh0, dh1 = (64, 128) if sh == 0 else (0, 64)
                      hs = h % 2
                      S.add("sp", (lambda e, h=h, hs=hs: e.dma_start(out=nabt[hs][:], in_=nab[h].rearrange("p (a q) -> p a q", a=21))), writes=[R_nabt[hs]], dma=True)
                      vcol = sh * 64
                      s = 0
                      sb0 = 0
                      for i in range(2):
                          S.add("pe", (lambda e, i=i, c=c, p0=p0, p1=p1, sb0=sb0: e.matmul(bank(sb0)[:, i * 256:(i + 1) * 256], lhsT=kT[p0:p1, c, i * 128:(i + 1) * 128],
                                                                                          rhs=qT[p0:p1, c, 0:256], start=True, stop=True)),
                                reads=rtok("qk", 0, 256), writes=[RB[sb0]])
                      S.add("act", (lambda e, s=s, sb0=sb0: e.activation(out=PT[s][:, 0:512], in_=bank(sb0)[:, 0:512], func=AF.Exp, scale=0.125)), reads=[RB[sb0]], writes=[R_PT[s]])
                      ob = 4 + s
                      for i in range(2):
                          S.add("pe", (lambda e, i=i, c=c, s=s, ob=ob, vcol=vcol: e.matmul(bank(ob)[:, 0:256], lhsT=vaug[:, i, c, vcol:vcol + 128], rhs=PT[s][:, i * 256:(i + 1) * 256],
                                                                                          start=(i == 0), stop=(i == 1))),
                                reads=[R_PT[s]] + rtok("vaug", 0, 256), writes=[RB[ob]])
                      S.add("dve", (lambda e, s=s, ob=ob, dh0=dh0, dh1=dh1: e.reciprocal(out=rec[s][dh0:dh1, 0:256], in_=bank(ob)[dh0:dh1, 0:256])), reads=[RB[ob]], writes=[R_rec[s]])
                      S.add("dve", (lambda e, s=s, ob=ob, c=c, nh0=nh0, nh1=nh1, dh0=dh0, dh1=dh1: e.tensor_tensor(out=oT[nh0:nh1, 4 + c, 0:256], in0=bank(ob)[nh0:nh1, 0:256],
                                                                                                               in1=rec[s][dh0:dh1, 0:256], op=ALU.mult)),
                            reads=[RB[ob], R_rec[s]], writes=rtok("oT", 0, 256))
                      for j in range(16):
                          s = itl % 3
                          so = itl % 2
                          itl += 1
                          sb0 = PSb[s]
                          tl = _na_tiles(j)
                          nl = len(tl)
                          ti0 = _na_tile_index(j)
                          q0 = NCTX + j * 128
                          ktoks = [NCTX + a * 128 for a in tl] + [0, 128]
                          for i, kt0 in enumerate(ktoks):
                              bk = sb0 + (i // 4)
                              S.add("pe", (lambda e, i=i, kt0=kt0, bk=bk, c=c, p0=p0, p1=p1, q0=q0: e.matmul(bank(bk)[:, (i % 4) * 128:(i % 4 + 1) * 128], lhsT=kT[p0:p1, c, kt0:kt0 + 128],
                                                                                                        rhs=qT[p0:p1, c, q0:q0 + 128], start=True, stop=True)),
                                    reads=rtok("qk", kt0, kt0 + 128) + rtok("qk", q0, q0 + 128), writes=[RB[bk]])
                          S.add("dve", (lambda e, s=s, nl=nl, ti0=ti0, hs=hs: e.scalar_tensor_tensor(out=sbt[s][:, 0:nl * 128], in0=PSl[s][:, 0:nl * 128], scalar=0.125,
                                                                                                    in1=nabt[hs][:, ti0:ti0 + nl, :].rearrange("p a q -> p (a q)"),
                                                                                                    op0=ALU.mult, op1=ALU.add)),
                                reads=[RB[sb0], RB[sb0 + 1], R_nabt[hs]], writes=[R_sbt[s]])
                          S.add("act", (lambda e, s=s, nl=nl: e.activation(out=PT[s][:, 0:nl * 128], in_=sbt[s][:, 0:nl * 128], func=AF.Exp)), reads=[R_sbt[s]], writes=[R_PT[s]])
                          S.add("act", (lambda e, s=s, nl=nl: e.activation(out=PT[s][:, nl * 128:(nl + 2) * 128], in_=PSl[s][:, nl * 128:(nl + 2) * 128], func=AF.Exp, scale=0.125)),
                                reads=[RB[sb0], RB[sb0 + 1]], writes=[R_PT[s]])
                          ob = 4 + so

                          def na_back(ktoks=ktoks, c=c, s=s, so=so, ob=ob, vcol=vcol, nl=nl, q0=q0, nh0=nh0, nh1=nh1, dh0=dh0, dh1=dh1):
                              for i, kt0 in enumerate(ktoks):
                                  S.add("pe", (lambda e, i=i, kt0=kt0: e.matmul(bank(ob)[:, 0:128], lhsT=vaug[:, kt0 // 128, c, vcol:vcol + 128],
                                                                                rhs=PT[s][:, i * 128:(i + 1) * 128], start=(i == 0), stop=(i == nl + 1))),
                                        reads=[R_PT[s]] + rtok("vaug", kt0, kt0 + 128), writes=[RB[ob]])
                              S.add("act", (lambda e: e.activation(out=rec[so][dh0:dh1, 0:128], in_=bank(ob)[dh0:dh1, 0:128], func=AF.Ln)), reads=[RB[ob]], writes=[R_rec[so]])
                              S.add("act", (lambda e: e.activation(out=rec[so][dh0:dh1, 0:128], in_=rec[so][dh0:dh1, 0:128], func=AF.Exp, scale=-1.0)), reads=[R_rec[so]], writes=[R_rec[so]])

                              def na_backB():
                                  S.add("dve", (lambda e: e.tensor_tensor(out=oT[nh0:nh1, 4 + c, q0:q0 + 128], in0=bank(ob)[nh0:nh1, 0:128],
                                                                          in1=rec[so][dh0:dh1, 0:128], op=ALU.mult)),
                                        reads=[RB[ob], R_rec[so]], writes=rtok("oT", q0, q0 + 128))
                              na_qB.append(na_backB)
                          if na_qB:
                              na_qB.pop(0)()
                          na_q.append(na_back)
                          if len(na_q) > 1:
                              na_q.pop(0)()
                  while na_q or na_qB:
                      if na_qB:
                          na_qB.pop(0)()
                      if na_q:
                          na_q.pop(0)()
                  dump("S3", oT[:, 4:8, :])
                  w_out_d = w_out0
                  tok_blocks = blocks256
              else:
                  S.barrier()
                  cur[0] = C0
                  wq = alloc("wq", [128, 8, 512], BF16)
                  wqs = alloc("wqs", [128, 8, 512], BF16)
                  wk = alloc("wk", [128, 8, 256], BF16)
                  wks = alloc("wks", [128, 8, 256], BF16)
                  wv = alloc("wv", [128, 8, 256], BF16)
                  rt = [alloc("rt%d" % i, [128, 2, 512], F32) for i in range(2)]
                  R_w1, R_rope = Res("w1"), Res("rope")
                  R_rt = [Res("rt0"), Res("rt1")]
                  R_wq = Res("wq")
                  for dst, src in ((wk, wk1), (wks, wks1), (wv, wv1)):
                      S.add("pool", (lambda e, dst=dst, src=src: e.dma_start(out=dst[:], in_=src.rearrange("(kc p) n -> p kc n", p=128))), writes=[R_w1], dma=True)
                  S.add("sp", lambda e: e.dma_start(out=rC[:], in_=ropeC), writes=[R_rope], dma=True)
                  S.add("sp", lambda e: e.dma_start(out=rS[:], in_=ropeS), writes=[R_rope], dma=True)
                  if True:
                      S.add("pool", lambda e: e.memset(vaug1[:], 1.0), writes=rtok("vaug", 0, NT))
                  it = 0
                  for half, (t0, N) in [(hf_, blk_) for hf_ in range(3) for blk_ in lat512]:
                      l0 = t0 - NCTX
                      if half < 2 and t0 == NCTX:
                          for dst, src in ((wq, wq1), (wqs, wqs1)):
                              S.add("pool", (lambda e, dst=dst, src=src, half=half: e.dma_start(out=dst[:], in_=src.rearrange("(kc p) n -> p kc n", p=128)[:, :, half * 512:(half + 1) * 512])),
                                    writes=[R_wq], dma=True)
                      for c in (range(half * 4, half * 4 + 4) if half < 2 else range(8, 10)):
                          s = it % 2
                          it += 1
                          b1, b2 = 2 * s, 2 * s + 1
                          wa, wb = (wq, wqs) if c < 8 else (wk, wks)
                          cc = (c % 4) if c < 8 else c - 8
                          for kc in range(8):
                              S.add("pe", (lambda e, kc=kc, cc=cc, wa=wa, t0=t0, N=N, b1=b1: e.matmul(bank(b1)[:, 0:N], lhsT=wa[:, kc, cc * 128:(cc + 1) * 128], rhs=UT[:, kc, t0:t0 + N],
                                                                                                 start=(kc == 0), stop=(kc == 7))),
                                    reads=[R_w1, R_wq] + rtok("UT", t0, t0 + N), writes=[RB[b1]])
                          for kc in range(8):
                              S.add("pe", (lambda e, kc=kc, cc=cc, wb=wb, t0=t0, N=N, b2=b2: e.matmul(bank(b2)[:, 0:N], lhsT=wb[:, kc, cc * 128:(cc + 1) * 128], rhs=UT[:, kc, t0:t0 + N],
                                                                                                 start=(kc == 0), stop=(kc == 7))),
                                    reads=[R_w1, R_wq] + rtok("UT", t0, t0 + N), writes=[RB[b2]])
                          S.add("dve", (lambda e, s=s, l0=l0, N=N, b1=b1: e.tensor_tensor(out=rt[s][:, 0, 0:N], in0=bank(b1)[:, 0:N], in1=rC[:, l0:l0 + N], op=ALU.mult)),
                                reads=[RB[b1], R_rope], writes=[R_rt[s]])
                          S.add("dve", (lambda e, s=s, l0=l0, N=N, b2=b2: e.tensor_tensor(out=rt[s][:, 1, 0:N], in0=bank(b2)[:, 0:N], in1=rS[:, l0:l0 + N], op=ALU.mult)),
                                reads=[RB[b2], R_rope], writes=[R_rt[s]])
                          if c < 8:
                              dst = qT1[:, c, l0:l0 + N]
                          else:
                              dst = kT1[:, c - 8, t0:t0 + N]
                          S.add("dve", (lambda e, s=s, N=N, dst=dst: e.tensor_tensor(out=dst, in0=rt[s][:, 0, 0:N], in1=rt[s][:, 1, 0:N], op=ALU.add)),
                                reads=[R_rt[s]], writes=rtok("qk", t0, t0 + N))
                  for cc in range(2):
                      s = it % 2
                      it += 1
                      b1 = 2 * s
                      for kc in range(8):
                          S.add("pe", (lambda e, kc=kc, cc=cc, b1=b1: e.matmul(bank(b1)[:, 0:256], lhsT=wk[:, kc, cc * 128:(cc + 1) * 128], rhs=UT[:, kc, 0:256], start=(kc == 0), stop=(kc == 7))),
                                reads=[R_w1] + rtok("UT", 0, 256), writes=[RB[b1]])
                      S.add("act", (lambda e, cc=cc, b1=b1: e.copy(out=kT1[:, cc, 0:256], in_=bank(b1)[:, 0:256])), reads=[RB[b1]], writes=rtok("qk", 0, 256))
                  for tt in range(18):
                      bk = 4 + tt % 2
                      for kc in range(8):
                          S.add("pe", (lambda e, kc=kc, tt=tt, bk=bk: e.matmul(bank(bk)[:, 0:256], lhsT=UT[:, kc, tt * 128:(tt + 1) * 128], rhs=wv[:, kc, :], start=(kc == 0), stop=(kc == 7))),
                                reads=[R_w1] + rtok("UT", tt * 128, tt * 128 + 128), writes=[RB[bk]])
                      for x in range(2):
                          S.add("act", (lambda e, tt=tt, bk=bk, x=x: e.copy(out=vaug1[:, tt, :, x * 128:x * 128 + 64],
                                                                          in_=bank(bk)[:, 0:256].rearrange("p (c x d) -> p c x d", c=2, x=2)[:, :, x, :])),
                                reads=[RB[bk]], writes=rtok("vaug", tt * 128, tt * 128 + 128))
                  dump("P1", kT1[:])
                  S.barrier()
                  cur[0] = C0
                  mlu = alloc("mlu", [128, 256], F32)
                  S.add("sp", lambda e: e.dma_start(out=mlu[:], in_=maskLU), writes=[R_const], dma=True)
                  sbm = [alloc("sbm%d" % i, [128, 512], F32) for i in range(2)]
                  PT1 = [alloc("PT1_%d" % i, [128, 5, 512], BF16) for i in range(2)]
                  rec1 = [alloc("rec1_%d" % i, [128, 512], F32) for i in range(2)]
                  R_sbm = [Res("sbm0"), Res("sbm1")]
                  R_PT1 = [[Res("PT1_%d_%d" % (i, k)) for k in range(5)] for i in range(2)]
                  R_rec1 = [Res("rec1_0"), Res("rec1_1")]
                  it = 0
                  sbr = 0
                  mi = 0
                  gq_pend = [None]
                  gq_qB = []
                  for g in range(4):
                      m, sh = g // 2, g % 2
                      p0, p1 = sh * 64, sh * 64 + 64
                      nh0, nh1 = (0, 64) if sh == 0 else (64, 128)
                      dh0, dh1 = (64, 128) if sh == 0 else (0, 64)
                      vcol = sh * 64
                      for qb in range(16):
                          s = it % 2
                          it += 1
                          tiles = []
                          if qb > 0:
                              tiles.append((NCTX + (qb - 1) * 128, 0))
                          tiles.append((NCTX + qb * 128, None))
                          if qb < 15:
                              tiles.append((NCTX + (qb + 1) * 128, 1))
                          tiles += [(0, None), (128, None)]
                          nt = len(tiles)
                          for i, (kt0, mk) in enumerate(tiles):
                              bk = sbr % 4
                              sbr += 1
                              S.add("pe", (lambda e, kt0=kt0, bk=bk, m=m, p0=p0, p1=p1, qb=qb: e.matmul(bank(bk).rearrange("p (h q) -> p h q", h=4), lhsT=kT1[p0:p1, m, kt0:kt0 + 128],
                                                                                                   rhs=qT1[p0:p1, 4 * m:4 * m + 4, qb * 128:(qb + 1) * 128], start=True, stop=True)),
                                    reads=rtok("qk", kt0, kt0 + 128) + rtok("qk", NCTX + qb * 128, NCTX + qb * 128 + 128), writes=[RB[bk]])
                              if mk is None:
                                  S.add("act", (lambda e, s=s, i=i, bk=bk: e.activation(out=PT1[s][:, i, :], in_=bank(bk), func=AF.Exp, scale=0.125)), reads=[RB[bk]], writes=[R_PT1[s][i]])
                              else:
                                  ms = mi % 2
                                  mi += 1
                                  S.add("dve", (lambda e, ms=ms, mk=mk, bk=bk: e.scalar_tensor_tensor(out=sbm[ms][:].rearrange("p (h q) -> p h q", h=4), in0=bank(bk).rearrange("p (h q) -> p h q", h=4),
                                                                                                    scalar=0.125, in1=mlu[:, mk * 128:(mk + 1) * 128].unsqueeze(1).broadcast_to([128, 4, 128]),
                                                                                                    op0=ALU.mult, op1=ALU.add)),
                                        reads=[RB[bk], R_const], writes=[R_sbm[ms]])
                                  S.add("act", (lambda e, s=s, i=i, ms=ms: e.activation(out=PT1[s][:, i, :], in_=sbm[ms][:], func=AF.Exp)), reads=[R_sbm[ms]], writes=[R_PT1[s][i]])
                          ob = 4 + s
                          q0 = NCTX + qb * 128

                          def gq_back(tiles=tiles, s=s, ob=ob, m=m, sh=sh, vcol=vcol, nt=nt, q0=q0, nh0=nh0, nh1=nh1, dh0=dh0, dh1=dh1):
                              for i, (kt0, mk) in enumerate(tiles):
                                  S.add("pe", (lambda e, i=i, kt0=kt0: e.matmul(bank(ob), lhsT=vaug1[:, kt0 // 128, m, vcol:vcol + 128], rhs=PT1[s][:, i, :],
                                                                                start=(i == 0), stop=(i == nt - 1))),
                                        reads=[R_PT1[s][i]] + rtok("vaug", kt0, kt0 + 128), writes=[RB[ob]])
                              S.add("dve", (lambda e: e.tensor_tensor(out=rec1[s][dh0:dh1, :].rearrange("p (h q) -> p h q", h=4),
                                                                      in0=bank(ob)[dh0:dh1, :].rearrange("p (h q) -> p h q", h=4),
                                                                      in1=esink[dh0:dh1, (m * 2 + sh) * 4:(m * 2 + sh) * 4 + 4].unsqueeze(2).broadcast_to([64, 4, 128]),
                                                                      op=ALU.add)),
                                    reads=[RB[ob], R_const], writes=[R_rec1[s]])
                              S.add("act", (lambda e: e.activation(out=rec1[s][dh0:dh1, :], in_=rec1[s][dh0:dh1, :], func=AF.Ln)), reads=[R_rec1[s]], writes=[R_rec1[s]])
                              S.add("act", (lambda e: e.activation(out=rec1[s][dh0:dh1, :], in_=rec1[s][dh0:dh1, :], func=AF.Exp, scale=-1.0)), reads=[R_rec1[s]], writes=[R_rec1[s]])
                              def gq_backB():
                                  S.add("dve", (lambda e: e.tensor_tensor(out=oT[nh0:nh1, 4 * m:4 * m + 4, q0:q0 + 128],
                                                                          in0=bank(ob)[nh0:nh1, :].rearrange("p (h q) -> p h q", h=4),
                                                                          in1=rec1[s][dh0:dh1, :].rearrange("p (h q) -> p h q", h=4), op=ALU.mult)),
                                        reads=[RB[ob], R_rec1[s]], writes=rtok("oT", q0, q0 + 128))
                              gq_qB.append(gq_backB)
                          if gq_qB:
                              gq_qB.pop(0)()
                          if gq_pend[0] is not None:
                              gq_pend[0]()
                          gq_pend[0] = gq_back
                  if gq_qB:
                      gq_qB.pop(0)()
                  gq_pend[0]()
                  gq_pend[0] = None
                  while gq_qB:
                      gq_qB.pop(0)()
                  dump("A1", oT[:])
                  w_out_d = w_out1
                  tok_blocks = lat256

              S.barrier()
              cur[0] = C0
              gatesT = alloc("gatesT", [16, NT], F32)
              wo = alloc("wo", [128, 8, D], BF16)
              hold0_at = cur[0]
              hold = [alloc("hold%d" % i, [128, 8, 128], F32) for i in range(2)]
              pre = alloc("pre", [128, 8, 128], F32)
              prebf = alloc("prebf", [128, 8, 128], BF16)
              presq = alloc("presq", [128, 8, 128], BF16)
              t32 = alloc("t32", [128, 8, 128], F32)
              tmpo = [alloc("tmpo%d" % i, [128, 128], F32) for i in range(2)]
              mean_sb = alloc("mean_sb", [128, 128], F32)
              rstd_sb = alloc("rstd_sb", [128, 128], F32)
              lsb = alloc("lsb", [128, 18, 20], F32, at=hold0_at)
              rw = alloc("rw", [128, 18 * 96], F32, at=hold0_at + 1472)
              assert hold0_at + 1472 + 18 * 96 * 4 <= cur[0]
              R_wo = Res("wo")
              R_hold = [Res("hold0"), Res("hold1")]
              R_pre, R_t32 = Res("pre"), Res("t32")
              R_tmpo = [Res("tmpo0"), Res("tmpo1")]
              R_lt = [Res("prebf"), Res("presq"), Res("mean"), Res("rstd")]
              R_rout = Res("rout")
              S.add("pool", (lambda e, w_out_d=w_out_d: e.dma_start(out=wo[:], in_=w_out_d.rearrange("(kc p) n -> p kc n", p=128))), writes=[R_wo], dma=True)
              tiles4 = [t for (t0_, n_) in tok_blocks for t in range(t0_, t0_ + n_, 128)]
              pre2 = alloc("pre2x", [128, 8, 128], F32, at=C0)
              pre_b = [pre, pre2]
              R_pre_b = [R_pre, Res("pre2x")]
              def s4_frontPE(bi):
                  t0 = tiles4[bi]
                  s = bi % 2
                  S.add("sp", (lambda e: e.dma_start(out=hold[s][:], in_=Hs3[:, :, t0:t0 + 128])), reads=rtok("Hs", t0, t0 + 128), writes=[R_hold[s]], dma=True)
                  for oc in range(8):
                      bk = 2 * s + oc // 4
                      co = (oc % 4) * 128
                      for kc in range(8):
                          S.add("pe", (lambda e, kc=kc, oc=oc, bk=bk, co=co: e.matmul(bank(bk)[:, co:co + 128], lhsT=wo[:, kc, oc * 128:(oc + 1) * 128], rhs=oT[:, kc, t0:t0 + 128],
                                                                                    start=(kc == 0), stop=(kc == 7))),
                                reads=[R_wo] + rtok("oT", t0, t0 + 128), writes=[RB[bk]])

              def s4_frontEV(bi, l=l):
                  t0 = tiles4[bi]
                  s = bi % 2
                  mc = mcol(t0)
                  pb, rpb = pre_b[s], R_pre_b[s]
                  for oc in range(8):
                      bk = 2 * s + oc // 4
                      co = (oc % 4) * 128
                      ts = oc % 2
                      if l == 0:
                          vb = V("M2B", oc, t0)
                          S.add("act", (lambda e, oc=oc, bk=bk, co=co, ts=ts, vb=vb: e.activation(out=tmpo[ts][:], in_=bank(bk)[:, co:co + 128], func=AF.Identity,
                                                                                            bias=vb, scale=modc(0, 2, oc, mc))),
                                reads=[RB[bk], R_mod, R_vecs], writes=[R_tmpo[ts]])
                      else:
                          S.add("act", (lambda e, oc=oc, bk=bk, co=co, ts=ts: e.activation(out=tmpo[ts][:], in_=bank(bk)[:, co:co + 128], func=AF.Identity,
                                                                                     scale=modc(1, 2, oc, mc))),
                                reads=[RB[bk], R_mod], writes=[R_tmpo[ts]])
                      S.add("dve", (lambda e, oc=oc, ts=ts: e.scalar_tensor_tensor(out=pb[:, oc, :], in0=hold[s][:, oc, :], scalar=ALPHA, in1=tmpo[ts][:], op0=ALU.mult, op1=ALU.add)),
                            reads=[R_hold[s], R_tmpo[ts]], writes=[rpb])

              def s4_part1(bi):
                  s = bi % 2
                  ln_part1(pre_b[s][:], 8, 128, ones1k, prebf, presq, 4 + 2 * s, [R_pre_b[s]], R_lt)

              def s4_part2(bi, l=l):
                  t0 = tiles4[bi]
                  tt = t0 // 128
                  s = bi % 2
                  pb, rpb = pre_b[s], R_pre_b[s]
                  ln_part2(128, mean_sb, rstd_sb, 4 + 2 * s, R_lt)
                  normalize(pb[:], 8, 128, mean_sb, rstd_sb, [rpb], R_lt)
                  for ch in range(8):
                      affine(UT[:, ch, t0:t0 + 128], pb[:, ch, :], V("G4", ch, t0), V("B4", ch, t0), [rpb, R_vecs], rtok("UT", t0, t0 + 128))
                      affine(t32[:, ch, :], pb[:, ch, :], V("G4", ch, t0), V("B4", ch, t0), [rpb, R_vecs], [R_t32])
                      affine(hres[:, ch, t0:t0 + 128], pb[:, ch, :], V("GA1", ch, t0), V("BA1", ch, t0), [rpb, R_vecs], rtok("hres%d" % ch, t0, t0 + 128))
                  for kc in range(8):
                      S.add("pe", (lambda e, kc=kc: e.matmul(bank(5)[:, tt * 20:tt * 20 + 20], lhsT=t32[:, kc, :], rhs=wrt[:, l * 160 + kc * 20:l * 160 + kc * 20 + 20],
                                                             start=(kc == 0), stop=(kc == 7))),
                            reads=[R_t32, R_const], writes=[RB[5]])

              n4 = len(tiles4)
              s4_frontPE(0)
              for bi in range(n4):
                  s4_frontEV(bi)
                  s4_part1(bi)
                  if bi + 1 < n4:
                      s4_frontPE(bi + 1)
                  if bi > 0:
                      s4_part2(bi - 1)
              s4_part2(n4 - 1)

              dump("S4", hres[:])
              dump("S4u", UT[:])
              S.barrier()
              T0 = tok_blocks[0][0] // 128
              T1 = 18
              nT = T1 - T0
              S.add("dve", lambda e: e.tensor_copy(out=lsb[:, T0:T1, :], in_=bank(5)[:, T0 * 20:T1 * 20].rearrange("p (t n) -> p t n", n=20)), reads=[RB[5]], writes=[R_rout])

              def rwv(i, n):
                  return rw[:, i * 18 * 4:(i * 18 * 4) + 18 * n].rearrange("p (t n) -> p t n", n=n)[:, T0:T1, :]

              def rop(fn):
                  S.add("dve", fn, reads=[R_rout], writes=[R_rout])

              lg = lsb[:, T0:T1, 0:4]
              le = lsb[:, T0:T1, 4:20].rearrange("p t (g x) -> p t g x", g=4)
              gmax, gsum, gp, m1, m2, dd, w1, w2 = [rwv(i, 1) for i in range(8)]
              gsh, gmask, elsel, mask1, el2, mask2, within, wa_ = [rwv(8 + i, 4) for i in range(8)]
              t44 = rw[:, 18 * 64:18 * 80].rearrange("p (t g x) -> p t g x", g=4, x=4)[:, T0:T1]
              gates = rw[:, 18 * 80:18 * 96].rearrange("p (t g x) -> p t g x", g=4, x=4)
              bc4 = lambda a: a.broadcast_to([128, nT, 4])
              rop(lambda e: e.tensor_reduce(out=gmax, in_=lg, axis=AX.X, op=ALU.max))
              rop(lambda e: e.tensor_tensor(out=gsh, in0=lg, in1=bc4(gmax), op=ALU.subtract))
              rop(lambda e: e.tensor_tensor(out=gmask, in0=lg, in1=bc4(gmax), op=ALU.is_equal))
              S.add("act", lambda e: e.activation(out=gsh, in_=gsh, func=AF.Exp), reads=[R_rout], writes=[R_rout])
              rop(lambda e: e.tensor_reduce(out=gsum, in_=gsh, axis=AX.X, op=ALU.add))
              rop(lambda e: e.reciprocal(out=gp, in_=gsum))
              rop(lambda e: e.tensor_tensor(out=t44, in0=le, in1=gmask.unsqueeze(3).broadcast_to([128, nT, 4, 4]), op=ALU.mult))
              rop(lambda e: e.tensor_reduce(out=elsel, in_=t44.rearrange("p t g x -> p t x g"), axis=AX.X, op=ALU.add))
              rop(lambda e: e.tensor_reduce(out=m1, in_=elsel, axis=AX.X, op=ALU.max))
              rop(lambda e: e.tensor_tensor(out=mask1, in0=elsel, in1=bc4(m1), op=ALU.is_equal))
              rop(lambda e: e.scalar_tensor_tensor(out=el2, in0=mask1, scalar=NEG, in1=elsel, op0=ALU.mult, op1=ALU.add))
              rop(lambda e: e.tensor_reduce(out=m2, in_=el2, axis=AX.X, op=ALU.max))
              rop(lambda e: e.tensor_tensor(out=mask2, in0=el2, in1=bc4(m2), op=ALU.is_equal))
              rop(lambda e: e.tensor_tensor(out=dd, in0=m2, in1=m1, op=ALU.subtract))
              S.add("act", lambda e: e.activation(out=dd, in_=dd, func=AF.Exp), reads=[R_rout], writes=[R_rout])
              rop(lambda e: e.tensor_scalar_add(out=w1, in0=dd, scalar1=1.0))
              rop(lambda e: e.reciprocal(out=w1, in_=w1))
              rop(lambda e: e.tensor_tensor(out=w1, in0=w1, in1=gp, op=ALU.mult))
              rop(lambda e: e.tensor_tensor(out=w2, in0=dd, in1=w1, op=ALU.mult))
              rop(lambda e: e.tensor_tensor(out=within, in0=mask1, in1=bc4(w1), op=ALU.mult))
              rop(lambda e: e.tensor_tensor(out=wa_, in0=mask2, in1=bc4(w2), op=ALU.mult))
              rop(lambda e: e.tensor_tensor(out=within, in0=within, in1=wa_, op=ALU.add))
              rop(lambda e: e.tensor_tensor(out=gates[:, T0:T1], in0=gmask.unsqueeze(3).broadcast_to([128, nT, 4, 4]), in1=within.unsqueeze(2).broadcast_to([128, nT, 4, 4]), op=ALU.mult))
              for tt in range(T0, T1):
                  bk = 6 + (tt // 4) % 2
                  S.add("pe", (lambda e, tt=tt, bk=bk: e.transpose(bank(bk)[0:16, (tt % 4) * 128:(tt % 4 + 1) * 128], gates[:, tt].rearrange("p g x -> p (g x)"), id32[:])),
                        reads=[R_rout, R_const], writes=[RB[bk]])
                  if tt % 4 == 3 or tt == T1 - 1:
                      ta = (tt // 4) * 4
                      ta0 = max(ta, T0)
                      S.add("act", (lambda e, bk=bk, ta=ta, ta0=ta0, tt=tt: e.copy(out=gatesT[0:16, ta0 * 128:(tt + 1) * 128], in_=bank(bk)[0:16, (ta0 - ta) * 128:(tt + 1 - ta) * 128])),
                            reads=[RB[bk]], writes=[R_rout])

              S.barrier()
              cur[0] = C0
              gatesT = alloc("gatesT", [16, NT], F32)
              wgs = [alloc("wgs%d" % i, [128, 8, 256], BF16) for i in range(2)]
              wus = [alloc("wus%d" % i, [128, 8, 256], BF16) for i in range(2)]
              wds = [alloc("wds%d" % i, [128, 2, D], BF16) for i in range(2)]
              sgm = [alloc("sgm0", [128, 2, 512], F32)]
              hgm = [alloc("hgm%d" % i, [128, 2, 512], BF16) for i in range(4)]
              o_ = OT0
              for i in range(2, 4):
                  wgs.append(alloc("wgs%d" % i, [128, 8, 256], BF16, at=o_)); o_ += 4096
                  wus.append(alloc("wus%d" % i, [128, 8, 256], BF16, at=o_)); o_ += 4096
                  wds.append(alloc("wds%d" % i, [128, 2, D], BF16, at=o_)); o_ += 4096
              sgm.append(alloc("sgm1", [128, 2, 512], F32, at=o_)); o_ += 4096
              gsb = []
              for i in range(2):
                  gsb.append(alloc("gsb%d" % i, [128, 512], F32, at=o_)); o_ += 2048
              assert o_ <= OT0 + 36864
              R_ewg = [Res("ewg%d" % i) for i in range(4)]
              R_ewu = [Res("ewu%d" % i) for i in range(4)]
              R_ewd = [Res("ewd%d" % i) for i in range(4)]
              R_sgm = [Res("sgm0"), Res("sgm1")]
              R_hgm = [Res("hgm%d" % i) for i in range(4)]
              R_gsb = [Res("gsb0"), Res("gsb1")]
              mblocks = blocks512 if l == 0 else lat512

              def load_expert(ex, slot, l=l):
                  S.add("pool", (lambda e: e.dma_start(out=wgs[slot][:], in_=ewg[l, ex].rearrange("(kc p) f -> p kc f", p=128))), writes=[R_ewg[slot]], dma=True)
                  S.add("pool", (lambda e: e.dma_start(out=wus[slot][:], in_=ewu[l, ex].rearrange("(kc p) f -> p kc f", p=128))), writes=[R_ewu[slot]], dma=True)
                  S.add("pool", (lambda e: e.dma_start(out=wds[slot][:], in_=ewd[l, ex].rearrange("(kc p) f -> p kc f", p=128))), writes=[R_ewd[slot]], dma=True)

              def emit_front(ex, slot, t0, N, si):
                  S.add("pe", (lambda e: e.matmul(bank(4)[:, 0:N], lhsT=selt[0:16, ex * 128:(ex + 1) * 128], rhs=gatesT[0:16, t0:t0 + N], start=True, stop=True)),
                        reads=[R_rout, R_const], writes=[RB[4]])
                  S.add("act", (lambda e: e.copy(out=gsb[si][:, 0:N], in_=bank(4)[:, 0:N])), reads=[RB[4]], writes=[R_gsb[si]])
                  for oc in range(4):
                      wsrc = wgs[slot] if oc < 2 else wus[slot]
                      rw_ = R_ewg[slot] if oc < 2 else R_ewu[slot]
                      for kc in range(8):
                          S.add("pe", (lambda e, kc=kc, oc=oc, wsrc=wsrc: e.matmul(bank(oc)[:, 0:N], lhsT=wsrc[:, kc, (oc % 2) * 128:(oc % 2 + 1) * 128], rhs=UT[:, kc, t0:t0 + N],
                                                                                    start=(kc == 0), stop=(kc == 7))),
                                reads=[rw_] + rtok("UT", t0, t0 + N), writes=[RB[oc]])
                  S.add("act", (lambda e: e.activation(out=sgm[si][:, :, 0:N], in_=PS[0][:].rearrange("p (j n) -> p j n", j=2)[:, :, 0:N], func=AF.Silu)),
                        reads=[RB[0], RB[1]], writes=[R_sgm[si]])
                  S.add("dve", (lambda e: e.tensor_tensor(out=sgm[si][:, :, 0:N], in0=sgm[si][:, :, 0:N], in1=PS[1][:].rearrange("p (j n) -> p j n", j=2)[:, :, 0:N], op=ALU.mult)),
                        reads=[RB[2], RB[3], R_sgm[si]], writes=[R_sgm[si]])

              def emit_gate(si, q, N):
                  S.add("dve", (lambda e: e.tensor_tensor(out=hgm[q][:, :, 0:N], in0=sgm[si][:, :, 0:N], in1=gsb[si][:, 0:N].unsqueeze(1).broadcast_to([128, 2, N]), op=ALU.mult)),
                        reads=[R_gsb[si], R_sgm[si]], writes=[R_hgm[q]])

              ybc = [0]

              def make_yhalf(half, slots, qs, t0, N, mc, l=l):
                  def f():
                      for dc in range(half * 4, half * 4 + 4):
                          bk = 5 + ybc[0] % 3
                          ybc[0] += 1
                          for j in range(2):
                              for k2 in range(2):
                                  S.add("pe", (lambda e, j=j, k2=k2, dc=dc, bk=bk: e.matmul(bank(bk)[:, 0:N], lhsT=wds[slots[j]][:, k2, dc * 128:(dc + 1) * 128], rhs=hgm[qs[j]][:, k2, 0:N],
                                                                                           start=(j == 0 and k2 == 0), stop=(j == 1 and k2 == 1))),
                                        reads=[R_ewd[slots[j]], R_hgm[qs[j]]], writes=[RB[bk]])
                          S.add("dve", (lambda e, dc=dc, bk=bk: e.scalar_tensor_tensor(out=hres[:, dc, t0:t0 + N], in0=bank(bk)[:, 0:N], scalar=modc(l, 5, dc, mc),
                                                                                      in1=hres[:, dc, t0:t0 + N], op0=ALU.mult, op1=ALU.add)),
                                reads=[RB[bk], R_mod] + rtok("hres%d" % dc, t0, t0 + N), writes=rtok("hres%d" % dc, t0, t0 + N))
                  return f

              load_expert(0, 0)
              load_expert(1, 1)
              pendA = pendB = None
              it = 0
              for pr in range(8):
                  slots = (2 * (pr % 2), 2 * (pr % 2) + 1)
                  for bi, (t0, N) in enumerate(mblocks):
                      qs = ((it % 2) * 2, (it % 2) * 2 + 1)
                      it += 1
                      mc = mcol(t0)
                      emit_front(2 * pr, slots[0], t0, N, 0)
                      if pendA is not None:
                          pendA()
                      emit_gate(0, qs[0], N)
                      emit_front(2 * pr + 1, slots[1], t0, N, 1)
                      if pendB is not None:
                          pendB()
                      emit_gate(1, qs[1], N)
                      pendA = make_yhalf(0, slots, qs, t0, N, mc)
                      pendB = make_yhalf(1, slots, qs, t0, N, mc)
                      if bi == 0 and pr + 1 < 8:
                          nslots = (2 * ((pr + 1) % 2), 2 * ((pr + 1) % 2) + 1)
                          load_expert(2 * pr + 2, nslots[0])
                          load_expert(2 * pr + 3, nslots[1])
              pendA()
              pendB()

              dump("S5", hres[:])
              S.barrier()
              cur[0] = C0
              pre2 = alloc("pre2", [128, 8, 256], BF16)
              presq2 = alloc("presq2", [128, 8, 256], BF16)
              mean2 = alloc("mean2", [128, 256], F32)
              rstd2 = alloc("rstd2", [128, 256], F32)
              otile = [alloc("otile%d" % i, [128, D], F32) for i in range(2)]
              R_l2 = [Res("pre2"), Res("presq2"), Res("mean2"), Res("rstd2")]
              R_ot = [Res("ot0"), Res("ot1")]
              oi = 0
              def s6_part1(bi):
                  t0_ = tok_blocks[bi][0]
                  rh_ = [r_ for ch_ in range(8) for r_ in rtok("hres%d" % ch_, t0_, t0_ + 256)]
                  ln_part1(hres[:, :, t0_:t0_ + 256], 8, 256, ones1k, pre2, presq2, 4 + bi % 2, rh_, R_l2)

              s6_part1(0)
              for bi, (t0, N) in enumerate(tok_blocks):
                  hap = hres[:, :, t0:t0 + 256]
                  rh = [r_ for ch_ in range(8) for r_ in rtok("hres%d" % ch_, t0, t0 + 256)]
                  if bi + 1 < len(tok_blocks):
                      s6_part1(bi + 1)
                  ln_part2(256, mean2, rstd2, 4 + bi % 2, R_l2)
                  normalize(hap, 8, 256, mean2, rstd2, rh, R_l2)
                  if l == 0:
                      for ch in range(8):
                          affine(UT[:, ch, t0:t0 + 256], hres[:, ch, t0:t0 + 256], V("GU", ch, t0), V("BU", ch, t0), rh + [R_vecs], rtok("UT", t0, t0 + 256))
                      for ch in range(8):
                          affine(hres[:, ch, t0:t0 + 256], hres[:, ch, t0:t0 + 256], smc("ln_g", 8 + ch), smc("ln_b", 8 + ch), rh + [R_const], rh)
                      S.add("sp", (lambda e, t0=t0: e.dma_start(out=Hs3[:, :, t0:t0 + 256], in_=hres[:, :, t0:t0 + 256])), reads=rh, writes=rtok("Hs", t0, t0 + 256), dma=True)
                      if debug and nlayers == 1:
                          S.add("sp", (lambda e, t0=t0: e.dma_start(out=dbg.rearrange("p (c t) -> p c t", c=8)[:, :, t0:t0 + 256], in_=hres[:, :, t0:t0 + 256])), reads=rh,
                                writes=[Res("dbgo")], dma=True)
                  else:
                      for ch in range(8):
                          affine(hres[:, ch, t0:t0 + 256], hres[:, ch, t0:t0 + 256], smc("ln_g", 24 + ch), smc("ln_b", 24 + ch), rh + [R_const], rh)
                      for hh in range(2):
                          tk = t0 + hh * 128
                          so = oi % 2
                          oi += 1
                          for ch in range(8):
                              bk = so * 2 + ch // 4
                              S.add("pe", (lambda e, ch=ch, bk=bk, tk=tk: e.transpose(bank(bk)[:, (ch % 4) * 128:(ch % 4 + 1) * 128], hres[:, ch, tk:tk + 128], id32[:])),
                                    reads=rh + [R_const], writes=[RB[bk]])
                          for hf in range(2):
                              bk = so * 2 + hf
                              S.add("act" if hf else "dve", (lambda e, so=so, hf=hf, bk=bk: (e.copy if hf else e.tensor_copy)(out=otile[so][:, hf * 512:(hf + 1) * 512], in_=bank(bk))),
                                    reads=[RB[bk]], writes=[R_ot[so]])
                          S.add("sp", (lambda e, so=so, tk=tk, b=b: e.dma_start(out=outd[b, tk - NCTX:tk - NCTX + 128, :], in_=otile[so][:])), reads=[R_ot[so]], writes=[Res("outw")], dma=True)
    except _Stop:
        pass
    S.barrier()

    with nc.Block() as block:
        @block.tensor
        def _(e):
            S.emit_one("pe", e, esem, dsems)

        @block.scalar
        def _(e):
            S.emit_one("act", e, esem, dsems)

        @block.vector
        def _(e):
            S.emit_one("dve", e, esem, dsems)

        @block.gpsimd
        def _(e):
            S.emit_one("pool", e, esem, dsems)

        @block.sync
        def _(e):
            S.emit_one("sp", e, esem, dsems)
    es.close()
    return nc


def _prep_shared(inp):
    f = lambda a: np.ascontiguousarray(np.asarray(a, np.float32))
    sm = np.zeros((128, SMN), np.float32)

    def put(name, arr):
        arr = np.asarray(arr, np.float32)
        sm[:, SMO[name]:SMO[name] + arr.shape[1]] = arr

    put("ada_b0", _fm(inp["ada_b"][0]))
    put("ada_b1", _fm(inp["ada_b"][1]))
    put("ln_g", np.concatenate([_fm(inp["ln_g"][l, k]) for l in range(2) for k in range(2)], axis=1))
    put("ln_b", np.concatenate([_fm(inp["ln_b"][l, k]) for l in range(2) for k in range(2)], axis=1))
    b_in = np.asarray(inp["ab_b_in"][0], np.float32)
    put("b_in", _fm(b_in[:2048]))
    cw = np.asarray(inp["conv_w"][0], np.float32)
    put("conv_w", np.ascontiguousarray(cw.T.reshape(4, 128, 31).transpose(1, 0, 2).reshape(128, 124)))
    put("conv_b", _fm(inp["conv_b"][0]))
    put("cln_g", _fm(inp["conv_ln_g"][0]))
    put("cln_b", _fm(inp["conv_ln_b"][0]))
    put("b_out", _fm(inp["ab_b_out"][0]))
    sm[:, SMO["eps"]] = EPS
    qidx = _gqa_qidx()
    gw = np.asarray(inp["gqa_w_in"][0], np.float32)
    wq = gw[:, :1024]
    wkk = gw[:, 1024:1280]
    wvv = gw[:, 1280:1536]
    C, Sg = _rope_tables()
    kk = np.arange(128)[:, None]
    qq = np.arange(128)[None, :]
    maskL = np.where(kk >= qq, 0.0, NEG).astype(np.float32)
    maskU = np.where(kk <= qq, 0.0, NEG).astype(np.float32)
    sink = np.asarray(inp["gqa_sink"][0], np.float32)
    sperm = np.array([8 * m + 4 * sh + j for m in range(2) for sh in range(2) for j in range(4)])
    sel = np.zeros((16, 16, 128), np.float32)
    for ex in range(16):
        sel[ex, ex, :] = 1.0
    wr = np.stack([np.concatenate([np.asarray(inp["router_group"][l], np.float32), np.asarray(inp["router_expert"][l], np.float32)], axis=1)
                   .reshape(8, 128, 20).transpose(1, 0, 2).reshape(128, 160) for l in range(2)])
    bv = b_in[2048:2560]
    shared = {
        "ada_w": f(inp["ada_w"]),
        "sm": sm,
        "w_in0": f(inp["ab_w_in"][0]),
        "bvbc": np.ascontiguousarray(np.broadcast_to(bv[None, :], (128, 512))),
        "nab": np.ascontiguousarray(_na_bias_table(np.asarray(inp["na_rpb"][0], np.float32)).reshape(8, 128, 21 * 128)),
        "w_out0": f(inp["ab_w_out"][0]),
        "wq1": f(wq[:, qidx]),
        "wqs1": f(wq[:, qidx][:, _swap64(1024)]),
        "wk1": f(wkk),
        "wks1": f(wkk[:, _swap64(256)]),
        "wv1": f(wvv),
        "w_out1": f(np.asarray(inp["gqa_w_out"][0], np.float32)[qidx, :]),
        "ropeC": C,
        "ropeS": Sg,
        "maskLU": np.ascontiguousarray(np.concatenate([maskL, maskU], axis=1)),
        "sinkbc": np.ascontiguousarray(np.broadcast_to(sink[sperm][None, :], (128, 16))),
        "wr": f(wr),
        "sel": np.ascontiguousarray(sel.reshape(16, 2048)),
        "ident": np.eye(128, dtype=np.float32),
        "ewg": f(inp["exp_w_gate"]),
        "ewu": f(inp["exp_w_up"]),
        "ewd": f(inp["exp_w_down"]),
    }
    return shared


def _core_inputs(inp, shared, i):
    x = np.asarray(inp["x"], np.float32)
    ctx = np.asarray(inp["ctx"], np.float32)
    c = np.asarray(inp["c"], np.float32)
    cc = np.stack([c[2 * i], c[2 * i + 1], np.asarray(inp["c_ctx"], np.float32)])
    cvec = np.ascontiguousarray(cc.reshape(3, 8, 128).transpose(2, 1, 0).reshape(128, 24))
    m = dict(shared)
    m["x2"] = np.ascontiguousarray(x[2 * i:2 * i + 2])
    m["ctx2"] = np.ascontiguousarray(ctx[2 * i:2 * i + 2])
    m["cvec"] = cvec
    return m


_NC_CACHE = {}


def kernel(**inputs):
    n = 8
    if "nc" not in _NC_CACHE:
        _NC_CACHE["nc"] = build()
    nc = _NC_CACHE["nc"]
    shared = _prep_shared(inputs)
    in_maps = [_core_inputs(inputs, shared, i) for i in range(n)]
    res = run_bass_kernel_spmd(nc, in_maps, core_ids=list(range(n)))
    out = np.concatenate([np.asarray(r["out"], np.float32) for r in res.results], axis=0)
    return out
```

```python
import numpy as np
from contextlib import ExitStack
import concourse.bass as bass
import concourse.mybir as mybir
from concourse.bass_utils import run_bass_kernel_spmd

F32 = mybir.dt.float32
BF16 = mybir.dt.bfloat16
AF = mybir.ActivationFunctionType
ALU = mybir.AluOpType
AX = mybir.AxisListType

D = 1024
SEQ = 2048
NCTX = 256
NT = SEQ + NCTX
GW = 64
ALPHA = 4.0 ** 0.25
EPS = 1e-5
NEG = -1e30

ENGS = ("pe", "act", "dve", "pool", "sp")
N_DMA_SEMS = 40


class Res:
    __slots__ = ("name", "last_w", "readers")

    def __init__(self, name):
        self.name = name
        self.last_w = None
        self.readers = []


class Op:
    __slots__ = ("eng", "fn", "idx", "deps", "dma", "sig", "semval", "dsem", "dval", "dprev")

    def __init__(self, eng, fn, idx, dma):
        self.eng = eng
        self.fn = fn
        self.idx = idx
        self.deps = []
        self.dma = dma
        self.sig = False
        self.semval = 0
        self.dsem = -1
        self.dval = 0
        self.dprev = 0


class Sched:
    def __init__(self):
        self.ops = {e: [] for e in ENGS}
        self.ndma = 0
        self.dma_tot = [0] * N_DMA_SEMS
        self.last_dma = [None] * N_DMA_SEMS
        self._assigned = False

    def add(self, eng, fn, reads=(), writes=(), dma=False, extra=()):
        lst = self.ops[eng]
        op = Op(eng, fn, len(lst), dma)
        deps = {}
        for r in reads:
            if r.last_w is not None:
                deps[id(r.last_w)] = r.last_w
        for w in writes:
            if w.last_w is not None:
                deps[id(w.last_w)] = w.last_w
            for rd in w.readers:
                deps[id(rd)] = rd
        for x in extra:
            deps[id(x)] = x
        for r in reads:
            r.readers.append(op)
        for w in writes:
            w.last_w = op
            w.readers = []
        if dma:
            s = self.ndma % N_DMA_SEMS
            self.ndma += 1
            op.dsem = s
            op.dprev = self.dma_tot[s]
            self.dma_tot[s] += 16
            op.dval = self.dma_tot[s]
            self.last_dma[s] = op
        for d in deps.values():
            if d is op:
                continue
            if d.eng == eng and not d.dma and not dma:
                if eng == "pe":
                    continue
                if op.idx - d.idx > 2:
                    continue
            op.deps.append(d)
            if not d.dma:
                d.sig = True
        lst.append(op)
        return op

    def barrier(self):
        lasts = []
        for e in ENGS:
            for op in reversed(self.ops[e]):
                if not op.dma:
                    lasts.append(op)
                    break
        dl = [o for o in self.last_dma if o is not None]
        for e in ENGS:
            self.add(e, lambda eng: eng.nop(), extra=[o for o in lasts if o.eng != e] + dl)

    def emit_one(self, e, eng, esem, dsems):
        if not self._assigned:
            for ee in ENGS:
                c = 0
                for op in self.ops[ee]:
                    if op.sig and not op.dma:
                        c += 1
                        op.semval = c
            self._assigned = True
        seen = {}
        for op in self.ops[e]:
            need = {}
            for d in op.deps:
                if d.dma:
                    key = ("d", d.dsem)
                    val = d.dval
                else:
                    key = ("e", d.eng)
                    val = d.semval
                if val > need.get(key, 0):
                    need[key] = val
            if op.dma and op.dprev > 0:
                key = ("d", op.dsem)
                if op.dprev > need.get(key, 0):
                    need[key] = op.dprev
            for key, val in need.items():
                if seen.get(key, 0) >= val:
                    continue
                seen[key] = val
                sem = dsems[key[1]] if key[0] == "d" else esem[key[1]]
                eng.wait_ge(sem, val)
            ins = op.fn(eng)
            if op.dma:
                ins.then_inc(dsems[op.dsem], 16)
            elif op.sig:
                ins.then_inc(esem[e], 1)


def _sm_layout():
    off = {}
    n = 0
    for name, cols in (("ada_b0", 48), ("ada_b1", 48), ("ln_g", 32), ("ln_b", 32), ("b_in", 16),
                       ("conv_w", 124), ("conv_b", 4), ("cln_g", 4), ("cln_b", 4), ("b_out", 8), ("eps", 1)):
        off[name] = n
        n += cols
    return off, n


SMO, SMN = _sm_layout()


def _fm(v):
    v = np.asarray(v, np.float32)
    return np.ascontiguousarray(v.reshape(-1, 128).T)


def _gqa_qidx():
    idx = np.zeros(1024, np.int64)
    for c in range(8):
        m, j = divmod(c, 4)
        h0 = 8 * m + j
        h1 = 8 * m + 4 + j
        idx[c * 128:c * 128 + 64] = h0 * 64 + np.arange(64)
        idx[c * 128 + 64:c * 128 + 128] = h1 * 64 + np.arange(64)
    return idx


def _swap64(n):
    d = np.arange(n)
    dd = d % 64
    sw = np.where(dd % 32 < 16, dd + 16, dd - 16)
    return (d // 64) * 64 + sw


def _na_tiles(j):
    if j in (0, 1):
        return [0, 1, 2, 3]
    if j in (14, 15):
        return [12, 13, 14, 15]
    return [j - 2, j - 1, j, j + 1, j + 2]


def _na_tile_index(j):
    if j == 0:
        return 5
    if j == 1:
        return 9
    if j == 14:
        return 13
    if j == 15:
        return 17
    return 0


def _na_bias_table(rpb):
    rows = 32
    r = np.arange(rows)
    row_start = np.clip(r - 4, 0, rows - 8)
    jj = np.arange(GW)
    col_start = np.clip(jj - 8, 0, GW - 16)
    col_in = (jj[None, :] >= col_start[:, None]) & (jj[None, :] < col_start[:, None] + 16)
    col_off = np.clip(jj[None, :] - jj[:, None], -15, 15) + 15
    out = np.full((8, 21, 128, 128), NEG, np.float32)

    def tile(j, a):
        t = np.full((8, 128, 128), NEG, np.float32)
        for pk in range(2):
            rk = 2 * a + pk
            for pq in range(2):
                rq = 2 * j + pq
                if not (row_start[rq] <= rk < row_start[rq] + 8):
                    continue
                ro = rk - rq + 7
                blk = rpb[:, ro][:, col_off]
                blk = np.where(col_in[None], blk, np.float32(NEG))
                t[:, pk * 64:(pk + 1) * 64, pq * 64:(pq + 1) * 64] = blk.transpose(0, 2, 1)
        return t

    for i, a in enumerate(_na_tiles(5)):
        out[:, i] = tile(5, a)
    for j in (0, 1, 14, 15):
        base = _na_tile_index(j)
        for i, a in enumerate(_na_tiles(j)):
            out[:, base + i] = tile(j, a)
    return np.ascontiguousarray(out.transpose(0, 2, 1, 3))


def _rope_tables():
    t = np.arange(SEQ)
    row = (t // GW).astype(np.float32)
    col = (t % GW).astype(np.float32)
    inv = (np.float32(10000.0) ** (-np.arange(0, 32, 2, dtype=np.float32) / np.float32(32))).astype(np.float32)
    ang = np.concatenate([row[:, None] * inv, col[:, None] * inv], axis=-1).astype(np.float32)
    cos = np.cos(ang).astype(np.float32)
    sin = np.sin(ang).astype(np.float32)
    p = np.arange(128)
    d = p % 64
    ai = (d // 32) * 16 + d % 16
    sgn = np.where(d % 32 < 16, -1.0, 1.0).astype(np.float32)
    C = np.ascontiguousarray(cos[:, ai].T)
    S = np.ascontiguousarray((sin[:, ai] * sgn[None, :]).T)
    return C.astype(np.float32), S.astype(np.float32)


class _Stop(Exception):
    pass


def build(nlayers=2, nb=2, debug=False, stop=None):
    nc = bass.Bass("TRN2", target_bir_lowering=False)
    S = Sched()

    def din(name, shape):
        return nc.dram_tensor(name, list(shape), F32, kind="ExternalInput").ap()

    x2 = din("x2", [2, SEQ, D])
    ctx2 = din("ctx2", [2, NCTX, D])
    cvec = din("cvec", [128, 24])
    ada_w = din("ada_w", [2, D, 6 * D])
    smd = din("sm", [128, SMN])
    w_in0 = din("w_in0", [D, 2560])
    bvbc = din("bvbc", [128, 512])
    nab = din("nab", [8, 128, 21 * 128])
    w_out0 = din("w_out0", [D, D])
    wq1 = din("wq1", [D, 1024])
    wqs1 = din("wqs1", [D, 1024])
    wk1 = din("wk1", [D, 256])
    wks1 = din("wks1", [D, 256])
    wv1 = din("wv1", [D, 256])
    w_out1 = din("w_out1", [D, D])
    ropeC = din("ropeC", [128, SEQ])
    ropeS = din("ropeS", [128, SEQ])
    maskLU = din("maskLU", [128, 256])
    sinkbc = din("sinkbc", [128, 16])
    wr = din("wr", [2, 128, 160])
    sel = din("sel", [16, 2048])
    ident = din("ident", [128, 128])
    ewg = din("ewg", [2, 16, D, 256])
    ewu = din("ewu", [2, 16, D, 256])
    ewd = din("ewd", [2, 16, 256, D])
    outd = nc.dram_tensor("out", [2, SEQ, D], F32, kind="ExternalOutput").ap()
    Hs = nc.dram_tensor("Hs", [128, 8 * NT], F32, kind="Internal").ap()
    dbg = nc.dram_tensor("dbg", [128, 8 * NT], F32, kind="ExternalOutput").ap() if debug else None
    dbgb = nc.dram_tensor("dbgb", [128, 8 * NT], BF16, kind="ExternalOutput").ap() if debug else None
    Hs3 = Hs.rearrange("p (c t) -> p c t", c=8)

    es = ExitStack()
    cur = [16640]

    acache = {}

    def alloc(name, shape, dt, at=None):
        nbytes = int(np.prod(shape[1:])) * (4 if dt == F32 else 2)
        if at is None:
            at = cur[0]
            cur[0] = (at + nbytes + 63) // 64 * 64
        assert at + nbytes <= 229376, (name, at, nbytes)
        key = (name, at, tuple(shape))
        if key not in acache:
            acache[key] = nc.alloc_sbuf_tensor_at("%s_%d" % (name, len(acache)), list(shape), dt, offset=at)
        return acache[key]

    sm = alloc("sm", [128, SMN], F32)
    id32 = alloc("id32", [128, 128], F32)
    ones1k = alloc("ones1k", [128, 128], BF16)
    ones512 = alloc("ones512", [128, 128], BF16)
    csil = alloc("csil", [128, 24], BF16)
    cv32 = alloc("cv32", [128, 24], F32)
    mod = alloc("mod", [128, 2 * 144], F32)
    mp1 = alloc("mp1", [128, 2 * 144], F32)
    vecs = alloc("vecs", [128, 128], F32)
    selt = alloc("selt", [16, 2048], BF16)
    wrt = alloc("wrt", [128, 320], F32)
    esink = alloc("esink", [128, 16], F32)
    R_const = Res("const")
    R_mod = Res("mod")
    R_vecs = Res("vecs")
    base0 = cur[0]

    PS = [es.enter_context(nc.psum_tensor("ps%d" % i, [128, 1024], F32)) for i in range(4)]
    RB = [Res("bank%d" % i) for i in range(8)]

    def bank(k):
        return PS[k // 2][:, (k % 2) * 512:(k % 2) * 512 + 512]

    esem = {e: es.enter_context(nc.semaphore("es_" + e)) for e in ENGS}
    dsems = [es.enter_context(nc.semaphore("ds%d" % i)) for i in range(N_DMA_SEMS)]

    def smc(name, j, n=1):
        o = SMO[name] + j
        return sm[:, o:o + n]

    def modc(l, k, ch, col):
        o = l * 144 + (k * 8 + ch) * 3 + col
        return mod[:, o:o + 1]

    def mp1c(l, k, ch, col):
        o = l * 144 + (k * 8 + ch) * 3 + col
        return mp1[:, o:o + 1]

    VK = {}

    def vslot(kind, ch):
        key = (kind, ch)
        if key not in VK:
            VK[key] = len(VK)
            assert len(VK) <= 128
        o = VK[key]
        return vecs[:, o:o + 1]

    S.add("sp", lambda e: e.dma_start(out=sm[:], in_=smd), writes=[R_const], dma=True)
    S.add("sp", lambda e: e.dma_start(out=id32[:], in_=ident), writes=[R_const], dma=True)
    S.add("sp", lambda e: e.dma_start(out=cv32[:], in_=cvec), writes=[R_const], dma=True)
    S.add("pool", lambda e: e.dma_start(out=selt[:], in_=sel), writes=[R_const], dma=True)
    S.add("sp", lambda e: e.dma_start(out=wrt[:].rearrange("p (l n) -> p l n", l=2), in_=wr.rearrange("l p n -> p l n")), writes=[R_const], dma=True)
    S.add("sp", lambda e: e.dma_start(out=esink[:], in_=sinkbc), writes=[R_const], dma=True)
    S.add("pool", lambda e: e.memset(ones1k[:], 1.0 / 1024.0), writes=[R_const])
    S.add("pool", lambda e: e.memset(ones512[:], 1.0 / 512.0), writes=[R_const])
    S.add("act", lambda e: e.activation(out=csil[:], in_=cv32[:], func=AF.Silu), reads=[R_const], writes=[R_const])
    S.add("act", lambda e: e.activation(out=esink[:], in_=esink[:], func=AF.Exp), reads=[R_const], writes=[R_const])

    adaw = [alloc("adaw%d" % i, [128, 8, 1024], BF16) for i in range(2)]
    R_adaw = [Res("adaw0"), Res("adaw1")]
    pi = 0
    for l in range(nlayers):
        awl = ada_w[l].rearrange("(kc p) n -> p kc n", p=128)
        for piece in range(6):
            s = pi % 2
            pi += 1
            S.add("pool", (lambda e, s=s, awl=awl, piece=piece: e.dma_start(out=adaw[s][:], in_=awl[:, :, piece * 1024:(piece + 1) * 1024])),
                  writes=[R_adaw[s]], dma=True)
            for oc8 in range(8):
                oc = piece * 8 + oc8
                for kc in range(8):
                    S.add("pe", (lambda e, s=s, oc=oc, oc8=oc8, kc=kc: e.matmul(bank(0)[:, oc * 3:oc * 3 + 3], lhsT=adaw[s][:, kc, oc8 * 128:(oc8 + 1) * 128],
                                                                                    rhs=csil[:, kc * 3:kc * 3 + 3], start=(kc == 0), stop=(kc == 7))),
                          reads=[R_adaw[s], R_const], writes=[RB[0]])
        ab = smc("ada_b%d" % l, 0, 48)
        S.add("dve", (lambda e, l=l, ab=ab: e.tensor_tensor(out=mod[:, l * 144:(l + 1) * 144].rearrange("p (a b) -> p a b", b=3),
                                                             in0=bank(0)[:, 0:144].rearrange("p (a b) -> p a b", b=3),
                                                             in1=ab.unsqueeze(2).broadcast_to([128, 48, 3]), op=ALU.add)),
              reads=[RB[0], R_const], writes=[R_mod])
        S.add("dve", (lambda e, l=l: e.tensor_scalar_add(out=mp1[:, l * 144:(l + 1) * 144], in0=mod[:, l * 144:(l + 1) * 144], scalar1=1.0)),
              reads=[R_mod], writes=[R_mod])
    S.barrier()
    cur[0] = base0

    A0 = cur[0]
    hres = alloc("hres", [128, 8, NT], F32)
    qT = alloc("qT", [128, 4, NT], BF16, at=A0)
    kT = alloc("kT", [128, 4, NT], BF16, at=A0 + 18432)
    vaug = alloc("vaug", [128, 18, 4, 192], BF16, at=A0 + 36864)
    qT1 = alloc("qT1", [128, 8, SEQ], BF16, at=A0)
    kT1 = alloc("kT1", [128, 2, NT], BF16, at=A0 + 32768)
    vaug1 = alloc("vaug1", [128, 18, 2, 192], BF16, at=A0 + 41984)
    rC = alloc("rC", [128, SEQ], F32, at=A0 + 55808)
    rS = alloc("rS", [128, SEQ], F32, at=A0 + 55808 + 8192)
    bvb = alloc("bvb", [128, 512], F32, at=A0 + 64512)
    idb = alloc("idb", [128, 128], BF16, at=A0 + 64512 + 2048)
    cmean = alloc("cmean", [128, 256], F32, at=A0 + 64512 + 2304)
    crstd = alloc("crstd", [128, 256], F32, at=A0 + 64512 + 3328)
    UT = alloc("UT", [128, 8, NT], BF16)
    OT0 = cur[0]
    oT = alloc("oT", [128, 8, NT], BF16)
    C0 = cur[0]
    RT = {}

    def rtok(name, t0, t1):
        out = []
        for tt in range(t0 // 128, (t1 + 127) // 128):
            key = (name, tt)
            if key not in RT:
                RT[key] = Res("%s_%d" % key)
            out.append(RT[key])
        return out

    R_hpad = [Res("hpad%d" % c) for c in range(4)]

    def derive_vecs(l, col, tag):
        ops = []
        for ch in range(8):
            g1 = smc("ln_g", (l * 2 + 0) * 8 + ch)
            b1 = smc("ln_b", (l * 2 + 0) * 8 + ch)
            g2 = smc("ln_g", (l * 2 + 1) * 8 + ch)
            b2 = smc("ln_b", (l * 2 + 1) * 8 + ch)
            S.add("dve", (lambda e, ch=ch, g1=g1: e.tensor_tensor(out=vslot((tag, "G4"), ch), in0=g1, in1=mp1c(l, 4, ch, col), op=ALU.mult)),
                  reads=[R_const, R_mod], writes=[R_vecs])
            S.add("dve", (lambda e, ch=ch, b1=b1: e.scalar_tensor_tensor(out=vslot((tag, "B4"), ch), in0=b1, scalar=mp1c(l, 4, ch, col), in1=modc(l, 3, ch, col),
                                                                         op0=ALU.mult, op1=ALU.add)),
                  reads=[R_const, R_mod], writes=[R_vecs])
            S.add("dve", (lambda e, ch=ch, g1=g1: e.tensor_scalar_mul(out=vslot((tag, "GA1"), ch), in0=g1, scalar1=ALPHA)), reads=[R_const], writes=[R_vecs])
            S.add("dve", (lambda e, ch=ch, b1=b1: e.tensor_scalar_mul(out=vslot((tag, "BA1"), ch), in0=b1, scalar1=ALPHA)), reads=[R_const], writes=[R_vecs])
            if l == 0:
                S.add("dve", (lambda e, ch=ch: e.tensor_tensor(out=vslot((tag, "M2B"), ch), in0=modc(l, 2, ch, col), in1=smc("b_out", ch), op=ALU.mult)),
                      reads=[R_const, R_mod], writes=[R_vecs])
                S.add("dve", (lambda e, ch=ch, g2=g2: e.tensor_tensor(out=vslot((tag, "GU"), ch), in0=g2, in1=mp1c(1, 1, ch, col), op=ALU.mult)),
                      reads=[R_const, R_mod], writes=[R_vecs])
                S.add("dve", (lambda e, ch=ch, b2=b2: e.scalar_tensor_tensor(out=vslot((tag, "BU"), ch), in0=b2, scalar=mp1c(1, 1, ch, col), in1=modc(1, 0, ch, col),
                                                                             op0=ALU.mult, op1=ALU.add)),
                      reads=[R_const, R_mod], writes=[R_vecs])

    def ln_part1(pre_ap, nch, N, ones_t, prebf, presq, bk, r_pre, r_tmp):
        S.add("dve", lambda e: e.tensor_copy(out=prebf[:, 0:nch, 0:N], in_=pre_ap), reads=r_pre, writes=[r_tmp[0]])
        S.add("act", lambda e: e.activation(out=presq[:, 0:nch, 0:N], in_=pre_ap, func=AF.Square), reads=r_pre, writes=[r_tmp[1]])
        for c in range(nch):
            S.add("pe", (lambda e, c=c: e.matmul(bank(bk)[:, 0:N], lhsT=ones_t[:], rhs=prebf[:, c, 0:N], start=(c == 0), stop=(c == nch - 1))),
                  reads=[r_tmp[0], R_const], writes=[RB[bk]])
        for c in range(nch):
            S.add("pe", (lambda e, c=c: e.matmul(bank(bk)[:, 256:256 + N], lhsT=ones_t[:], rhs=presq[:, c, 0:N], start=(c == 0), stop=(c == nch - 1))),
                  reads=[r_tmp[1], R_const], writes=[RB[bk]])

    def ln_part2(N, mean_sb, rstd_sb, bk, r_tmp):
        S.add("act", lambda e: e.copy(out=mean_sb[:, 0:N], in_=bank(bk)[:, 0:N]), reads=[RB[bk]], writes=[r_tmp[2]])
        S.add("dve", lambda e: e.tensor_tensor(out=rstd_sb[:, 0:N], in0=mean_sb[:, 0:N], in1=mean_sb[:, 0:N], op=ALU.mult), reads=[r_tmp[2]], writes=[r_tmp[3]])
        S.add("dve", lambda e: e.tensor_tensor(out=rstd_sb[:, 0:N], in0=bank(bk)[:, 256:256 + N], in1=rstd_sb[:, 0:N], op=ALU.subtract),
              reads=[RB[bk], r_tmp[3]], writes=[r_tmp[3]])
        S.add("act", lambda e: e.activation(out=rstd_sb[:, 0:N], in_=rstd_sb[:, 0:N], func=AF.Ln, bias=smc("eps", 0), scale=1.0),
              reads=[r_tmp[3], R_const], writes=[r_tmp[3]])
        S.add("act", lambda e: e.activation(out=rstd_sb[:, 0:N], in_=rstd_sb[:, 0:N], func=AF.Exp, scale=-0.5), reads=[r_tmp[3]], writes=[r_tmp[3]])

    def ln_stats(pre_ap, nch, N, ones_t, prebf, presq, mean_sb, rstd_sb, bk, r_pre, r_tmp):
        ln_part1(pre_ap, nch, N, ones_t, prebf, presq, bk, r_pre, r_tmp)
        ln_part2(N, mean_sb, rstd_sb, bk, r_tmp)

    def normalize(pre_ap, nch, N, mean_sb, rstd_sb, r_pre, r_tmp):
        S.add("dve", lambda e: e.tensor_tensor(out=pre_ap, in0=pre_ap, in1=mean_sb[:, 0:N].unsqueeze(1).broadcast_to([128, nch, N]), op=ALU.subtract),
              reads=r_pre + [r_tmp[2]], writes=r_pre)
        S.add("dve", lambda e: e.tensor_tensor(out=pre_ap, in0=pre_ap, in1=rstd_sb[:, 0:N].unsqueeze(1).broadcast_to([128, nch, N]), op=ALU.mult),
              reads=r_pre + [r_tmp[3]], writes=r_pre)

    aff_rr = [0]

    def affine(out_ap, in_ap, sc, bi, reads, writes, psum_in=False):
        k = aff_rr[0] % 2
        aff_rr[0] += 1
        if k == 0:
            S.add("act", lambda e: e.activation(out=out_ap, in_=in_ap, func=AF.Identity, bias=bi, scale=sc), reads=reads, writes=writes)
        else:
            S.add("dve" if k == 1 else "pool", lambda e: e.tensor_scalar(out=out_ap, in0=in_ap, scalar1=sc, scalar2=bi, op0=ALU.mult, op1=ALU.add),
                  reads=reads, writes=writes)

    def dump(name, src_ap3):
        if stop != name and stop != "%s@%d" % (name, cur_l[0]):
            return
        S.barrier()
        c, t = src_ap3.shape[1], src_ap3.shape[2]
        dst = dbg if src_ap3.dtype == F32 else dbgb
        for ci in range(c):
            S.add("sp", (lambda e, ci=ci: e.dma_start(out=dst[:, ci * t:(ci + 1) * t], in_=src_ap3[:, ci, :])), writes=[Res("dbgo")], dma=True)
        raise _Stop()

    cur_l = [0]
    try:
      for b in range(nb):
        for l in range(nlayers):
              cur_l[0] = l
              lat_only = (l == nlayers - 1) and l == 1
              col = b
              S.barrier()
              derive_vecs(l, b, "lat")
              if l == 0:
                  derive_vecs(l, 2, "ctx")

              def V(kind, ch, t0):
                  return vslot((("ctx" if (t0 < NCTX and l == 0) else "lat"), kind), ch)

              def mcol(t0):
                  return 2 if t0 < NCTX else b

              cur[0] = C0
              if l == 0:
                  xin = [alloc("xin%d" % i, [128, D], F32) for i in range(2)]
                  hblk = [alloc("hblk%d" % i, [128, 8, 128], F32) for i in range(2)]
                  R_xin = [Res("xin0"), Res("xin1")]
                  R_hblk = [Res("hblk0"), Res("hblk1")]
                  for tt in range(18):
                      s = tt % 2
                      src = ctx2[b, tt * 128:(tt + 1) * 128, :] if tt < 2 else x2[b, (tt - 2) * 128:(tt - 1) * 128, :]
                      S.add("sp", (lambda e, s=s, src=src: e.dma_start(out=xin[s][:], in_=src)), writes=[R_xin[s]], dma=True)
                      for ch in range(8):
                          bk = (tt % 2) * 2 + ch // 4
                          S.add("pe", (lambda e, s=s, ch=ch, bk=bk: e.transpose(bank(bk)[:, (ch % 4) * 128:(ch % 4 + 1) * 128], xin[s][:, ch * 128:(ch + 1) * 128], id32[:])),
                                reads=[R_xin[s], R_const], writes=[RB[bk]])
                      for hf in range(2):
                          bk = (tt % 2) * 2 + hf
                          S.add("act", (lambda e, s=s, hf=hf, bk=bk: e.copy(out=hblk[s][:, hf * 4:(hf + 1) * 4, :], in_=bank(bk).rearrange("p (c t) -> p c t", c=4))),
                                reads=[RB[bk]], writes=[R_hblk[s]])
                      mc = mcol(tt * 128)
                      for ch in range(8):
                          bk = (tt % 2) * 2 + ch // 4
                          affine(UT[:, ch, tt * 128:(tt + 1) * 128], bank(bk)[:, (ch % 4) * 128:(ch % 4 + 1) * 128], mp1c(0, 1, ch, mc), modc(0, 0, ch, mc),
                                 [RB[bk], R_mod], rtok("UT", tt * 128, tt * 128 + 128), psum_in=True)
                      S.add("sp", (lambda e, s=s, tt=tt: e.dma_start(out=Hs3[:, :, tt * 128:(tt + 1) * 128], in_=hblk[s][:])), reads=[R_hblk[s]],
                            writes=rtok("Hs", tt * 128, tt * 128 + 128), dma=True)

              if l == 0:
                  dump("S0", UT[:])
              blocks512 = [(0, 256)] + [(256 + 512 * i, 512) for i in range(4)]
              blocks256 = [(256 * i, 256) for i in range(9)]
              if l == 1:
                  lat512 = [(256 + 512 * i, 512) for i in range(4)]
                  lat256 = [(256 * i, 256) for i in range(1, 9)]

              if l == 0:
                  S.barrier()
                  cur[0] = C0
                  wAB = alloc("wAB", [128, 8, 1536], BF16)
                  wA = alloc("wA", [128, 8, 1024], BF16, at=C0)
                  hpd = [alloc("hpd%d" % i, [128, 2368], BF16, at=C0 + 16384 + i * 4736) for i in range(2)]
                  dg0 = alloc("diag0", [128, 31, 128], BF16, at=C0 + 25856)
                  diag = [dg0, dg0]
                  cur[0] = C0 + 33792
                  sgt = [alloc("sgt%d" % i, [128, 512], F32) for i in range(2)]
                  czsq = alloc("czsq", [128, 4, 256], BF16)
                  cz = alloc("cz", [128, 4, 256], F32)
                  R_wAB, R_misc = Res("wAB"), Res("misc0")
                  R_hpd = [Res("hpd0"), Res("hpd1")]
                  R_dg = [Res("dg0")] * 2
                  R_sgt = [Res("sgt0"), Res("sgt1")]
                  R_cz, R_ct = Res("cz"), [Res("czbf"), Res("czsq"), Res("cmean"), Res("crstd")]
                  w0 = w_in0.rearrange("(kc p) n -> p kc n", p=128)
                  S.add("pool", lambda e: e.dma_start(out=wA[:], in_=w0[:, :, 0:1024]), writes=[R_wAB], dma=True)
                  S.add("pool", lambda e: e.dma_start(out=idb[:], in_=ident), writes=[R_misc], dma=True)
                  S.add("sp", lambda e: e.dma_start(out=bvb[:], in_=bvbc), writes=[R_misc], dma=True)
                  S.add("pool", lambda e: e.memset(hpd[0][:], 0.0), writes=[R_hpd[0]])
                  S.add("pool", lambda e: e.memset(hpd[1][:], 0.0), writes=[R_hpd[1]])
                  S.add("pool", lambda e: e.memset(vaug[:], 1.0), writes=rtok("vaug", 0, NT))

                  def hoff(t0):
                      return 15 + t0 if t0 < NCTX else 286 + 15 + (t0 - NCTX)

                  it = 0
                  for cc in range(4):
                      hs_ = cc % 2
                      for k in range(31):
                          S.add("dve", (lambda e, cc=cc, k=k, hs_=hs_: e.tensor_scalar_mul(out=diag[hs_][:, k, :], in0=idb[:], scalar1=smc("conv_w", cc * 31 + k))),
                                reads=[R_misc, R_const], writes=[R_dg[hs_]])
                      for (t0, N) in blocks512:
                          s = it % 2
                          it += 1
                          b1, b2 = 2 * s, 2 * s + 1
                          for kc in range(8):
                              S.add("pe", (lambda e, kc=kc, cc=cc, t0=t0, N=N, b1=b1: e.matmul(bank(b1)[:, 0:N], lhsT=wA[:, kc, cc * 128:(cc + 1) * 128], rhs=UT[:, kc, t0:t0 + N],
                                                                                             start=(kc == 0), stop=(kc == 7))),
                                    reads=[R_wAB] + rtok("UT", t0, t0 + N), writes=[RB[b1]])
                          for kc in range(8):
                              S.add("pe", (lambda e, kc=kc, cc=cc, t0=t0, N=N, b2=b2: e.matmul(bank(b2)[:, 0:N], lhsT=wA[:, kc, 512 + cc * 128:512 + (cc + 1) * 128], rhs=UT[:, kc, t0:t0 + N],
                                                                                             start=(kc == 0), stop=(kc == 7))),
                                    reads=[R_wAB] + rtok("UT", t0, t0 + N), writes=[RB[b2]])
                          S.add("act", (lambda e, s=s, cc=cc, N=N, b2=b2: e.activation(out=sgt[s][:, 0:N], in_=bank(b2)[:, 0:N], func=AF.Sigmoid, bias=smc("b_in", 4 + cc), scale=1.0)),
                                reads=[RB[b2], R_const], writes=[R_sgt[s]])
                          ho = hoff(t0)
                          S.add("dve", (lambda e, s=s, cc=cc, N=N, b1=b1, ho=ho, hs_=hs_: e.scalar_tensor_tensor(out=hpd[hs_][:, ho:ho + N], in0=bank(b1)[:, 0:N], scalar=smc("b_in", cc),
                                                                                                                 in1=sgt[s][:, 0:N], op0=ALU.add, op1=ALU.mult)),
                                reads=[RB[b1], R_sgt[s], R_const], writes=[R_hpd[hs_]])
                      for bi, (t0, N) in enumerate(blocks512):
                          ho = hoff(t0) - 15
                          bk = 4 + bi % 2
                          for k in range(31):
                              S.add("pe", (lambda e, k=k, ho=ho, N=N, bk=bk, hs_=hs_: e.matmul(bank(bk)[:, 0:N], lhsT=diag[hs_][:, k, :], rhs=hpd[hs_][:, ho + k:ho + k + N],
                                                                                           start=(k == 0), stop=(k == 30))),
                                    reads=[R_dg[hs_], R_hpd[hs_]], writes=[RB[bk]])
                          S.add("act", (lambda e, cc=cc, N=N, t0=t0, bk=bk: e.activation(out=oT[:, cc, t0:t0 + N], in_=bank(bk)[:, 0:N], func=AF.Identity, bias=smc("conv_b", cc), scale=1.0)),
                                reads=[RB[bk], R_const], writes=rtok("oT", t0, t0 + N))
                  dump("S1z", oT[:, 0:4, :])
                  dump("S1h", hpd[1][:].unsqueeze(1))
                  dump("S1d", diag[0][:])
                  S.add("pool", lambda e: e.dma_start(out=wAB[:], in_=w0[:, :, 1024:2560]), writes=[R_wAB] + R_hpd + [R_dg[0]], dma=True)
                  for (t0, N) in blocks256:
                      zin = oT[:, 0:4, t0:t0 + 256]
                      rz = rtok("oT", t0, t0 + 256)
                      S.add("act", (lambda e, zin=zin: e.activation(out=czsq[:], in_=zin, func=AF.Square)), reads=rz, writes=[R_ct[1]])
                      for c4 in range(4):
                          S.add("pe", (lambda e, c4=c4, t0=t0: e.matmul(bank(6)[:, 0:256], lhsT=ones512[:], rhs=oT[:, c4, t0:t0 + 256], start=(c4 == 0), stop=(c4 == 3))),
                                reads=rz + [R_const], writes=[RB[6]])
                      for c4 in range(4):
                          S.add("pe", (lambda e, c4=c4: e.matmul(bank(6)[:, 256:512], lhsT=ones512[:], rhs=czsq[:, c4, :], start=(c4 == 0), stop=(c4 == 3))),
                                reads=[R_ct[1], R_const], writes=[RB[6]])
                      S.add("act", lambda e: e.copy(out=cmean[:], in_=bank(6)[:, 0:256]), reads=[RB[6]], writes=[R_ct[2]])
                      S.add("dve", lambda e: e.tensor_tensor(out=crstd[:], in0=cmean[:], in1=cmean[:], op=ALU.mult), reads=[R_ct[2]], writes=[R_ct[3]])
                      S.add("dve", lambda e: e.tensor_tensor(out=crstd[:], in0=bank(6)[:, 256:512], in1=crstd[:], op=ALU.subtract), reads=[RB[6], R_ct[3]], writes=[R_ct[3]])
                      S.add("act", lambda e: e.activation(out=crstd[:], in_=crstd[:], func=AF.Sqrt, bias=smc("eps", 0), scale=1.0), reads=[R_ct[3], R_const], writes=[R_ct[3]])
                      S.add("dve", lambda e: e.reciprocal(out=crstd[:], in_=crstd[:]), reads=[R_ct[3]], writes=[R_ct[3]])
                      S.add("dve", (lambda e, zin=zin: e.tensor_tensor(out=cz[:], in0=zin, in1=cmean[:].unsqueeze(1).broadcast_to([128, 4, 256]), op=ALU.subtract)),
                            reads=rz + [R_ct[2]], writes=[R_cz])
                      S.add("dve", lambda e: e.tensor_tensor(out=cz[:], in0=cz[:], in1=crstd[:].unsqueeze(1).broadcast_to([128, 4, 256]), op=ALU.mult),
                            reads=[R_cz, R_ct[3]], writes=[R_cz])
                      for c4 in range(4):
                          S.add("act", (lambda e, c4=c4, t0=t0: e.activation(out=oT[:, c4, t0:t0 + 256], in_=cz[:, c4, :], func=AF.Silu, bias=smc("cln_b", c4), scale=smc("cln_g", c4))),
                                reads=[R_cz, R_const], writes=rz)
                  it = 0
                  for (t0, N) in blocks512:
                      for c in range(8):
                          bk = it % 4
                          it += 1
                          for kc in range(8):
                              S.add("pe", (lambda e, kc=kc, c=c, t0=t0, N=N, bk=bk: e.matmul(bank(bk)[:, 0:N], lhsT=wAB[:, kc, c * 128:(c + 1) * 128], rhs=UT[:, kc, t0:t0 + N],
                                                                                           start=(kc == 0), stop=(kc == 7))),
                                    reads=[R_wAB] + rtok("UT", t0, t0 + N), writes=[RB[bk]])
                          dst = qT if c < 4 else kT
                          S.add("act", (lambda e, c=c, t0=t0, N=N, bk=bk, dst=dst: e.activation(out=dst[:, c % 4, t0:t0 + N], in_=bank(bk)[:, 0:N], func=AF.Identity,
                                                                                              bias=smc("b_in", 8 + c), scale=1.0)),
                                reads=[RB[bk], R_const], writes=rtok("qk", t0, t0 + N))
                  for tt in range(18):
                      bk = it % 4
                      it += 1
                      for kc in range(8):
                          S.add("pe", (lambda e, kc=kc, tt=tt, bk=bk: e.matmul(bank(bk)[:, 0:512], lhsT=UT[:, kc, tt * 128:(tt + 1) * 128], rhs=wAB[:, kc, 1024:1536],
                                                                             start=(kc == 0), stop=(kc == 7))),
                                reads=[R_wAB] + rtok("UT", tt * 128, tt * 128 + 128), writes=[RB[bk]])
                      for x in range(2):
                          S.add("dve", (lambda e, tt=tt, bk=bk, x=x: e.tensor_tensor(out=vaug[:, tt, :, x * 128:x * 128 + 64],
                                                                                   in0=bank(bk).rearrange("p (c x d) -> p c x d", c=4, x=2)[:, :, x, :],
                                                                                   in1=bvb[:].rearrange("p (c x d) -> p c x d", c=4, x=2)[:, :, x, :], op=ALU.add)),
                                reads=[RB[bk], R_misc], writes=rtok("vaug", tt * 128, tt * 128 + 128))

                  dump("S1o", oT[:, 0:4, :])
                  dump("S1q", qT[:])
                  dump("S1k", kT[:])
                  dump("S1v", vaug[:].rearrange("p t c x -> p t (c x)"))
                  S.barrier()
                  cur[0] = C0
                  nabt = [alloc("nabt%d" % i, [128, 21, 128], F32) for i in range(2)]
                  sbt = [alloc("sbt%d" % i, [128, 640], F32) for i in range(3)]
                  PT = [alloc("PT%d" % i, [128, 896], BF16) for i in range(3)]
                  rec = [alloc("rec%d" % i, [128, 256], F32) for i in range(2)]
                  R_nabt = [Res("nabt0"), Res("nabt1")]
                  R_sbt = [Res("sbt0"), Res("sbt1"), Res("sbt2")]
                  R_PT = [Res("PT0"), Res("PT1"), Res("PT2")]
                  PSl = [PS[0], PS[1], PS[3]]
                  PSb = [0, 2, 6]
                  itl = 0
                  na_q = []
                  na_qB = []
                  R_rec = [Res("rec0"), Res("rec1")]
                  it = 0
                  na_pend = [None]
                  for h in range(8):
                      while na_q or na_qB:
                          if na_qB:
                              na_qB.pop(0)()
                          if na_q:
                              na_q.pop(0)()
                      c, sh = h // 2, h % 2
                      p0, p1 = sh * 64, sh * 64 + 64
                      nh0, nh1 = (0, 64) if sh == 0 else (64, 128)
                      dh0, dh1 = (64, 128) if sh == 0 else (0, 64)
                      hs = h % 2
                      S.add("sp", (lambda e, h=h, hs=hs: e.dma_start(out=nabt[hs][:], in_=nab[h].rearrange("p (a q) -> p a q", a=21))), writes=[R_nabt[hs]], dma=True)
                      vcol = sh * 64
                      s = 0
                      sb0 = 0
                      for i in range(2):
                          S.add("pe", (lambda e, i=i, c=c, p0=p0, p1=p1, sb0=sb0: e.matmul(bank(sb0)[:, i * 256:(i + 1) * 256], lhsT=kT[p0:p1, c, i * 128:(i + 1) * 128],
                                                                                          rhs=qT[p0:p1, c, 0:256], start=True, stop=True)),
                                reads=rtok("qk", 0, 256), writes=[RB[sb0]])
                      S.add("act", (lambda e, s=s, sb0=sb0: e.activation(out=PT[s][:, 0:512], in_=bank(sb0)[:, 0:512], func=AF.Exp, scale=0.125)), reads=[RB[sb0]], writes=[R_PT[s]])
                      ob = 4 + s
                      for i in range(2):
                          S.add("pe", (lambda e, i=i, c=c, s=s, ob=ob, vcol=vcol: e.matmul(bank(ob)[:, 0:256], lhsT=vaug[:, i, c, vcol:vcol + 128], rhs=PT[s][:, i * 256:(i + 1) * 256],
                                                                                          start=(i == 0), stop=(i == 1))),
                                reads=[R_PT[s]] + rtok("vaug", 0, 256), writes=[RB[ob]])
                      S.add("dve", (lambda e, s=s, ob=ob, dh0=dh0, dh1=dh1: e.reciprocal(out=rec[s][dh0:dh1, 0:256], in_=bank(ob)[dh0:dh1, 0:256])), reads=[RB[ob]], writes=[R_rec[s]])
                      S.add("dve", (lambda e, s=s, ob=ob, c=c, nh0=nh0, nh1=nh1, dh0=dh0, dh1=dh1: e.tensor_tensor(out=oT[nh0:nh1, 4 + c, 0:256], in0=bank(ob)[nh0:nh1, 0:256],
                                                                                                               in1=rec[s][dh0:dh1, 0:256], op=ALU.mult)),
                            reads=[RB[ob], R_rec[s]], writes=rtok("oT", 0, 256))
                      for j in range(16):
                          s = itl % 3
                          so = itl % 2
                          itl += 1
                          sb0 = PSb[s]
                          tl = _na_tiles(j)
                          nl = len(tl)
                          ti0 = _na_tile_index(j)
                          q0 = NCTX + j * 128
                          ktoks = [NCTX + a * 128 for a in tl] + [0, 128]
                          for i, kt0 in enumerate(ktoks):
                              bk = sb0 + (i // 4)
                              S.add("pe", (lambda e, i=i, kt0=kt0, bk=bk, c=c, p0=p0, p1=p1, q0=q0: e.matmul(bank(bk)[:, (i % 4) * 128:(i % 4 + 1) * 128], lhsT=kT[p0:p1, c, kt0:kt0 + 128],
                                                                                                        rhs=qT[p0:p1, c, q0:q0 + 128], start=True, stop=True)),
                                    reads=rtok("qk", kt0, kt0 + 128) + rtok("qk", q0, q0 + 128), writes=[RB[bk]])
                          S.add("dve", (lambda e, s=s, nl=nl, ti0=ti0, hs=hs: e.scalar_tensor_tensor(out=sbt[s][:, 0:nl * 128], in0=PSl[s][:, 0:nl * 128], scalar=0.125,
                                                                                                    in1=nabt[hs][:, ti0:ti0 + nl, :].rearrange("p a q -> p (a q)"),
                                                                                                    op0=ALU.mult, op1=ALU.add)),
                                reads=[RB[sb0], RB[sb0 + 1], R_nabt[hs]], writes=[R_sbt[s]])
                          S.add("act", (lambda e, s=s, nl=nl: e.activation(out=PT[s][:, 0:nl * 128], in_=sbt[s][:, 0:nl * 128], func=AF.Exp)), reads=[R_sbt[s]], writes=[R_PT[s]])
                          S.add("act", (lambda e, s=s, nl=nl: e.activation(out=PT[s][:, nl * 128:(nl + 2) * 128], in_=PSl[s][:, nl * 128:(nl + 2) * 128], func=AF.Exp, scale=0.125)),
                                reads=[RB[sb0], RB[sb0 + 1]], writes=[R_PT[s]])
                          ob = 4 + so

                          def na_back(ktoks=ktoks, c=c, s=s, so=so, ob=ob, vcol=vcol, nl=nl, q0=q0, nh0=nh0, nh1=nh1, dh0=dh0, dh1=dh1):
                              for i, kt0 in enumerate(ktoks):
                                  S.add("pe", (lambda e, i=i, kt0=kt0: e.matmul(bank(ob)[:, 0:128], lhsT=vaug[:, kt0 // 128, c, vcol:vcol + 128],
                                                                                rhs=PT[s][:, i * 128:(i + 1) * 128], start=(i == 0), stop=(i == nl + 1))),
                                        reads=[R_PT[s]] + rtok("vaug", kt0, kt0 + 128), writes=[RB[ob]])
                              S.add("act", (lambda e: e.activation(out=rec[so][dh0:dh1, 0:128], in_=bank(ob)[dh0:dh1, 0:128], func=AF.Ln)), reads=[RB[ob]], writes=[R_rec[so]])
                              S.add("act", (lambda e: e.activation(out=rec[so][dh0:dh1, 0:128], in_=rec[so][dh0:dh1, 0:128], func=AF.Exp, scale=-1.0)), reads=[R_rec[so]], writes=[R_rec[so]])

                              def na_backB():
                                  S.add("dve", (lambda e: e.tensor_tensor(out=oT[nh0:nh1, 4 + c, q0:q0 + 128], in0=bank(ob)[nh0:nh1, 0:128],
                                                                          in1=rec[so][dh0:dh1, 0:128], op=ALU.mult)),
                                        reads=[RB[ob], R_rec[so]], writes=rtok("oT", q0, q0 + 128))
                              na_qB.append(na_backB)
                          if na_qB:
                              na_qB.pop(0)()
                          na_q.append(na_back)
                          if len(na_q) > 1:
                              na_q.pop(0)()
                  while na_q or na_qB:
                      if na_qB:
                          na_qB.pop(0)()
                      if na_q:
                          na_q.pop(0)()
                  dump("S3", oT[:, 4:8, :])
                  w_out_d = w_out0
                  tok_blocks = blocks256
              else:
                  S.barrier()
                  cur[0] = C0
                  wq = alloc("wq", [128, 8, 512], BF16)
                  wqs = alloc("wqs", [128, 8, 512], BF16)
                  wk = alloc("wk", [128, 8, 256], BF16)
                  wks = alloc("wks", [128, 8, 256], BF16)
                  wv = alloc("wv", [128, 8, 256], BF16)
                  rt = [alloc("rt%d" % i, [128, 2, 512], F32) for i in range(2)]
                  R_w1, R_rope = Res("w1"), Res("rope")
                  R_rt = [Res("rt0"), Res("rt1")]
                  R_wq = Res("wq")
                  for dst, src in ((wk, wk1), (wks, wks1), (wv, wv1)):
                      S.add("pool", (lambda e, dst=dst, src=src: e.dma_start(out=dst[:], in_=src.rearrange("(kc p) n -> p kc n", p=128))), writes=[R_w1], dma=True)
                  S.add("sp", lambda e: e.dma_start(out=rC[:], in_=ropeC), writes=[R_rope], dma=True)
                  S.add("sp", lambda e: e.dma_start(out=rS[:], in_=ropeS), writes=[R_rope], dma=True)
                  if True:
                      S.add("pool", lambda e: e.memset(vaug1[:], 1.0), writes=rtok("vaug", 0, NT))
                  it = 0
                  for half, (t0, N) in [(hf_, blk_) for hf_ in range(3) for blk_ in lat512]:
                      l0 = t0 - NCTX
                      if half < 2 and t0 == NCTX:
                          for dst, src in ((wq, wq1), (wqs, wqs1)):
                              S.add("pool", (lambda e, dst=dst, src=src, half=half: e.dma_start(out=dst[:], in_=src.rearrange("(kc p) n -> p kc n", p=128)[:, :, half * 512:(half + 1) * 512])),
                                    writes=[R_wq], dma=True)
                      for c in (range(half * 4, half * 4 + 4) if half < 2 else range(8, 10)):
                          s = it % 2
                          it += 1
                          b1, b2 = 2 * s, 2 * s + 1
                          wa, wb = (wq, wqs) if c < 8 else (wk, wks)
                          cc = (c % 4) if c < 8 else c - 8
                          for kc in range(8):
                              S.add("pe", (lambda e, kc=kc, cc=cc, wa=wa, t0=t0, N=N, b1=b1: e.matmul(bank(b1)[:, 0:N], lhsT=wa[:, kc, cc * 128:(cc + 1) * 128], rhs=UT[:, kc, t0:t0 + N],
                                                                                                 start=(kc == 0), stop=(kc == 7))),
                                    reads=[R_w1, R_wq] + rtok("UT", t0, t0 + N), writes=[RB[b1]])
                          for kc in range(8):
                              S.add("pe", (lambda e, kc=kc, cc=cc, wb=wb, t0=t0, N=N, b2=b2: e.matmul(bank(b2)[:, 0:N], lhsT=wb[:, kc, cc * 128:(cc + 1) * 128], rhs=UT[:, kc, t0:t0 + N],
                                                                                                 start=(kc == 0), stop=(kc == 7))),
                                    reads=[R_w1, R_wq] + rtok("UT", t0, t0 + N), writes=[RB[b2]])
                          S.add("dve", (lambda e, s=s, l0=l0, N=N, b1=b1: e.tensor_tensor(out=rt[s][:, 0, 0:N], in0=bank(b1)[:, 0:N], in1=rC[:, l0:l0 + N], op=ALU.mult)),
                                reads=[RB[b1], R_rope], writes=[R_rt[s]])
                          S.add("dve", (lambda e, s=s, l0=l0, N=N, b2=b2: e.tensor_tensor(out=rt[s][:, 1, 0:N], in0=bank(b2)[:, 0:N], in1=rS[:, l0:l0 + N], op=ALU.mult)),
                                reads=[RB[b2], R_rope], writes=[R_rt[s]])
                          if c < 8:
                              dst = qT1[:, c, l0:l0 + N]
                          else:
                              dst = kT1[:, c - 8, t0:t0 + N]
                          S.add("dve", (lambda e, s=s, N=N, dst=dst: e.tensor_tensor(out=dst, in0=rt[s][:, 0, 0:N], in1=rt[s][:, 1, 0:N], op=ALU.add)),
                                reads=[R_rt[s]], writes=rtok("qk", t0, t0 + N))
                  for cc in range(2):
                      s = it % 2
                      it += 1
                      b1 = 2 * s
                      for kc in range(8):
                          S.add("pe", (lambda e, kc=kc, cc=cc, b1=b1: e.matmul(bank(b1)[:, 0:256], lhsT=wk[:, kc, cc * 128:(cc + 1) * 128], rhs=UT[:, kc, 0:256], start=(kc == 0), stop=(kc == 7))),
                                reads=[R_w1] + rtok("UT", 0, 256), writes=[RB[b1]])
                      S.add("act", (lambda e, cc=cc, b1=b1: e.copy(out=kT1[:, cc, 0:256], in_=bank(b1)[:, 0:256])), reads=[RB[b1]], writes=rtok("qk", 0, 256))
                  for tt in range(18):
                      bk = 4 + tt % 2
                      for kc in range(8):
                          S.add("pe", (lambda e, kc=kc, tt=tt, bk=bk: e.matmul(bank(bk)[:, 0:256], lhsT=UT[:, kc, tt * 128:(tt + 1) * 128], rhs=wv[:, kc, :], start=(kc == 0), stop=(kc == 7))),
                                reads=[R_w1] + rtok("UT", tt * 128, tt * 128 + 128), writes=[RB[bk]])
                      for x in range(2):
                          S.add("act", (lambda e, tt=tt, bk=bk, x=x: e.copy(out=vaug1[:, tt, :, x * 128:x * 128 + 64],
                                                                          in_=bank(bk)[:, 0:256].rearrange("p (c x d) -> p c x d", c=2, x=2)[:, :, x, :])),
                                reads=[RB[bk]], writes=rtok("vaug", tt * 128, tt * 128 + 128))
                  dump("P1", kT1[:])
                  S.barrier()
                  cur[0] = C0
                  mlu = alloc("mlu", [128, 256], F32)
                  S.add("sp", lambda e: e.dma_start(out=mlu[:], in_=maskLU), writes=[R_const], dma=True)
                  sbm = [alloc("sbm%d" % i, [128, 512], F32) for i in range(2)]
                  PT1 = [alloc("PT1_%d" % i, [128, 5, 512], BF16) for i in range(2)]
                  rec1 = [alloc("rec1_%d" % i, [128, 512], F32) for i in range(2)]
                  R_sbm = [Res("sbm0"), Res("sbm1")]
                  R_PT1 = [[Res("PT1_%d_%d" % (i, k)) for k in range(5)] for i in range(2)]
                  R_rec1 = [Res("rec1_0"), Res("rec1_1")]
                  it = 0
                  sbr = 0
                  mi = 0
                  gq_pend = [None]
                  gq_qB = []
                  for g in range(4):
                      m, sh = g // 2, g % 2
                      p0, p1 = sh * 64, sh * 64 + 64
                      nh0, nh1 = (0, 64) if sh == 0 else (64, 128)
                      dh0, dh1 = (64, 128) if sh == 0 else (0, 64)
                      vcol = sh * 64
                      for qb in range(16):
                          s = it % 2
                          it += 1
                          tiles = []
                          if qb > 0:
                              tiles.append((NCTX + (qb - 1) * 128, 0))
                          tiles.append((NCTX + qb * 128, None))
                          if qb < 15:
                              tiles.append((NCTX + (qb + 1) * 128, 1))
                          tiles += [(0, None), (128, None)]
                          nt = len(tiles)
                          for i, (kt0, mk) in enumerate(tiles):
                              bk = sbr % 4
                              sbr += 1
                              S.add("pe", (lambda e, kt0=kt0, bk=bk, m=m, p0=p0, p1=p1, qb=qb: e.matmul(bank(bk).rearrange("p (h q) -> p h q", h=4), lhsT=kT1[p0:p1, m, kt0:kt0 + 128],
                                                                                                   rhs=qT1[p0:p1, 4 * m:4 * m + 4, qb * 128:(qb + 1) * 128], start=True, stop=True)),
                                    reads=rtok("qk", kt0, kt0 + 128) + rtok("qk", NCTX + qb * 128, NCTX + qb * 128 + 128), writes=[RB[bk]])
                              if mk is None:
                                  S.add("act", (lambda e, s=s, i=i, bk=bk: e.activation(out=PT1[s][:, i, :], in_=bank(bk), func=AF.Exp, scale=0.125)), reads=[RB[bk]], writes=[R_PT1[s][i]])
                              else:
                                  ms = mi % 2
                                  mi += 1
                                  S.add("dve", (lambda e, ms=ms, mk=mk, bk=bk: e.scalar_tensor_tensor(out=sbm[ms][:].rearrange("p (h q) -> p h q", h=4), in0=bank(bk).rearrange("p (h q) -> p h q", h=4),
                                                                                                    scalar=0.125, in1=mlu[:, mk * 128:(mk + 1) * 128].unsqueeze(1).broadcast_to([128, 4, 128]),
                                                                                                    op0=ALU.mult, op1=ALU.add)),
                                        reads=[RB[bk], R_const], writes=[R_sbm[ms]])
                                  S.add("act", (lambda e, s=s, i=i, ms=ms: e.activation(out=PT1[s][:, i, :], in_=sbm[ms][:], func=AF.Exp)), reads=[R_sbm[ms]], writes=[R_PT1[s][i]])
                          ob = 4 + s
                          q0 = NCTX + qb * 128

                          def gq_back(tiles=tiles, s=s, ob=ob, m=m, sh=sh, vcol=vcol, nt=nt, q0=q0, nh0=nh0, nh1=nh1, dh0=dh0, dh1=dh1):
                              for i, (kt0, mk) in enumerate(tiles):
                                  S.add("pe", (lambda e, i=i, kt0=kt0: e.matmul(bank(ob), lhsT=vaug1[:, kt0 // 128, m, vcol:vcol + 128], rhs=PT1[s][:, i, :],
                                                                                start=(i == 0), stop=(i == nt - 1))),
                                        reads=[R_PT1[s][i]] + rtok("vaug", kt0, kt0 + 128), writes=[RB[ob]])
                              S.add("dve", (lambda e: e.tensor_tensor(out=rec1[s][dh0:dh1, :].rearrange("p (h q) -> p h q", h=4),
                                                                      in0=bank(ob)[dh0:dh1, :].rearrange("p (h q) -> p h q", h=4),
                                                                      in1=esink[dh0:dh1, (m * 2 + sh) * 4:(m * 2 + sh) * 4 + 4].unsqueeze(2).broadcast_to([64, 4, 128]),
                                                                      op=ALU.add)),
                                    reads=[RB[ob], R_const], writes=[R_rec1[s]])
                              S.add("act", (lambda e: e.activation(out=rec1[s][dh0:dh1, :], in_=rec1[s][dh0:dh1, :], func=AF.Ln)), reads=[R_rec1[s]], writes=[R_rec1[s]])
                              S.add("act", (lambda e: e.activation(out=rec1[s][dh0:dh1, :], in_=rec1[s][dh0:dh1, :], func=AF.Exp, scale=-1.0)), reads=[R_rec1[s]], writes=[R_rec1[s]])
                              def gq_backB():
                                  S.add("dve", (lambda e: e.tensor_tensor(out=oT[nh0:nh1, 4 * m:4 * m + 4, q0:q0 + 128],
                                                                          in0=bank(ob)[nh0:nh1, :].rearrange("p (h q) -> p h q", h=4),
                                                                          in1=rec1[s][dh0:dh1, :].rearrange("p (h q) -> p h q", h=4), op=ALU.mult)),
                                        reads=[RB[ob], R_rec1[s]], writes=rtok("oT", q0, q0 + 128))
                              gq_qB.append(gq_backB)
                          if gq_qB:
                              gq_qB.pop(0)()
                          if gq_pend[0] is not None:
                              gq_pend[0]()
                          gq_pend[0] = gq_back
                  if gq_qB:
                      gq_qB.pop(0)()
                  gq_pend[0]()
                  gq_pend[0] = None
                  while gq_qB:
                      gq_qB.pop(0)()
                  dump("A1", oT[:])
                  w_out_d = w_out1
                  tok_blocks = lat256

              S.barrier()
              cur[0] = C0
              gTh = alloc("gTh", [16, NT], BF16)
              gTl = alloc("gTl", [16, NT], BF16)
              assert cur[0] == C0 + 9216
              gatesT = alloc("gatesT", [16, NT], F32, at=C0 + 9216)
              wo = alloc("wo", [128, 8, D], BF16)
              hold0_at = cur[0]
              hold = [alloc("hold%d" % i, [128, 8, 128], F32) for i in range(2)]
              pre = alloc("pre", [128, 8, 128], F32)
              prebf = alloc("prebf", [128, 8, 128], BF16)
              presq = alloc("presq", [128, 8, 128], BF16)
              t32 = alloc("t32", [128, 8, 128], F32)
              tmpo = [alloc("tmpo%d" % i, [128, 128], F32) for i in range(2)]
              mean_sb = alloc("mean_sb", [128, 128], F32)
              rstd_sb = alloc("rstd_sb", [128, 128], F32)
              lsb = alloc("lsb", [128, 18, 20], F32, at=hold0_at)
              rw = alloc("rw", [128, 18 * 96], F32, at=hold0_at + 1472)
              assert hold0_at + 1472 + 18 * 96 * 4 <= cur[0]
              R_wo = Res("wo")
              R_hold = [Res("hold0"), Res("hold1")]
              R_pre, R_t32 = Res("pre"), Res("t32")
              R_tmpo = [Res("tmpo0"), Res("tmpo1")]
              R_lt = [Res("prebf"), Res("presq"), Res("mean"), Res("rstd")]
              R_rout = Res("rout")
              S.add("pool", (lambda e, w_out_d=w_out_d: e.dma_start(out=wo[:], in_=w_out_d.rearrange("(kc p) n -> p kc n", p=128))), writes=[R_wo], dma=True)
              tiles4 = [t for (t0_, n_) in tok_blocks for t in range(t0_, t0_ + n_, 128)]
              pre2 = alloc("pre2x", [128, 8, 128], F32, at=C0)
              pre_b = [pre, pre2]
              R_pre_b = [R_pre, Res("pre2x")]
              def s4_frontPE(bi):
                  t0 = tiles4[bi]
                  s = bi % 2
                  S.add("sp", (lambda e: e.dma_start(out=hold[s][:], in_=Hs3[:, :, t0:t0 + 128])), reads=rtok("Hs", t0, t0 + 128), writes=[R_hold[s]], dma=True)
                  for oc in range(8):
                      bk = 2 * s + oc // 4
                      co = (oc % 4) * 128
                      for kc in range(8):
                          S.add("pe", (lambda e, kc=kc, oc=oc, bk=bk, co=co: e.matmul(bank(bk)[:, co:co + 128], lhsT=wo[:, kc, oc * 128:(oc + 1) * 128], rhs=oT[:, kc, t0:t0 + 128],
                                                                                    start=(kc == 0), stop=(kc == 7))),
                                reads=[R_wo] + rtok("oT", t0, t0 + 128), writes=[RB[bk]])

              def s4_frontEV(bi, l=l):
                  t0 = tiles4[bi]
                  s = bi % 2
                  mc = mcol(t0)
                  pb, rpb = pre_b[s], R_pre_b[s]
                  for oc in range(8):
                      bk = 2 * s + oc // 4
                      co = (oc % 4) * 128
                      ts = oc % 2
                      if l == 0:
                          vb = V("M2B", oc, t0)
                          S.add("act", (lambda e, oc=oc, bk=bk, co=co, ts=ts, vb=vb: e.activation(out=tmpo[ts][:], in_=bank(bk)[:, co:co + 128], func=AF.Identity,
                                                                                            bias=vb, scale=modc(0, 2, oc, mc))),
                                reads=[RB[bk], R_mod, R_vecs], writes=[R_tmpo[ts]])
                      else:
                          S.add("act", (lambda e, oc=oc, bk=bk, co=co, ts=ts: e.activation(out=tmpo[ts][:], in_=bank(bk)[:, co:co + 128], func=AF.Identity,
                                                                                     scale=modc(1, 2, oc, mc))),
                                reads=[RB[bk], R_mod], writes=[R_tmpo[ts]])
                      S.add("dve", (lambda e, oc=oc, ts=ts: e.scalar_tensor_tensor(out=pb[:, oc, :], in0=hold[s][:, oc, :], scalar=ALPHA, in1=tmpo[ts][:], op0=ALU.mult, op1=ALU.add)),
                            reads=[R_hold[s], R_tmpo[ts]], writes=[rpb])

              def s4_part1(bi):
                  s = bi % 2
                  ln_part1(pre_b[s][:], 8, 128, ones1k, prebf, presq, 4 + 2 * s, [R_pre_b[s]], R_lt)

              def s4_part2(bi, l=l):
                  t0 = tiles4[bi]
                  tt = t0 // 128
                  s = bi % 2
                  pb, rpb = pre_b[s], R_pre_b[s]
                  ln_part2(128, mean_sb, rstd_sb, 4 + 2 * s, R_lt)
                  normalize(pb[:], 8, 128, mean_sb, rstd_sb, [rpb], R_lt)
                  for ch in range(8):
                      affine(UT[:, ch, t0:t0 + 128], pb[:, ch, :], V("G4", ch, t0), V("B4", ch, t0), [rpb, R_vecs], rtok("UT", t0, t0 + 128))
                      affine(t32[:, ch, :], pb[:, ch, :], V("G4", ch, t0), V("B4", ch, t0), [rpb, R_vecs], [R_t32])
                      affine(hres[:, ch, t0:t0 + 128], pb[:, ch, :], V("GA1", ch, t0), V("BA1", ch, t0), [rpb, R_vecs], rtok("hres%d" % ch, t0, t0 + 128))
                  for kc in range(8):
                      S.add("pe", (lambda e, kc=kc: e.matmul(bank(5)[:, tt * 20:tt * 20 + 20], lhsT=t32[:, kc, :], rhs=wrt[:, l * 160 + kc * 20:l * 160 + kc * 20 + 20],
                                                             start=(kc == 0), stop=(kc == 7))),
                            reads=[R_t32, R_const], writes=[RB[5]])

              n4 = len(tiles4)
              s4_frontPE(0)
              for bi in range(n4):
                  s4_frontEV(bi)
                  s4_part1(bi)
                  if bi + 1 < n4:
                      s4_frontPE(bi + 1)
                  if bi > 0:
                      s4_part2(bi - 1)
              s4_part2(n4 - 1)

              dump("S4", hres[:])
              dump("S4u", UT[:])
              S.barrier()
              def routing_block(T0, T1, lsb=lsb, rw=rw, gatesT=gatesT, gTh=gTh, gTl=gTl, R_rout=R_rout):
                  nT = T1 - T0
                  S.add("dve", lambda e: e.tensor_copy(out=lsb[:, T0:T1, :], in_=bank(5)[:, T0 * 20:T1 * 20].rearrange("p (t n) -> p t n", n=20)), reads=[RB[5]], writes=[R_rout])

                  def rwv(i, n):
                      return rw[:, i * 18 * 4:(i * 18 * 4) + 18 * n].rearrange("p (t n) -> p t n", n=n)[:, T0:T1, :]

                  def rop(fn):
                      S.add("dve", fn, reads=[R_rout], writes=[R_rout])

                  lg = lsb[:, T0:T1, 0:4]
                  le = lsb[:, T0:T1, 4:20].rearrange("p t (g x) -> p t g x", g=4)
                  gmax, gsum, gp, m1, m2, dd, w1, w2 = [rwv(i, 1) for i in range(8)]
                  gsh, gmask, elsel, mask1, el2, mask2, within, wa_ = [rwv(8 + i, 4) for i in range(8)]
                  t44 = rw[:, 18 * 64:18 * 80].rearrange("p (t g x) -> p t g x", g=4, x=4)[:, T0:T1]
                  gates = rw[:, 18 * 80:18 * 96].rearrange("p (t g x) -> p t g x", g=4, x=4)
                  bc4 = lambda a: a.broadcast_to([128, nT, 4])
                  rop(lambda e: e.tensor_reduce(out=gmax, in_=lg, axis=AX.X, op=ALU.max))
                  rop(lambda e: e.tensor_tensor(out=gsh, in0=lg, in1=bc4(gmax), op=ALU.subtract))
                  rop(lambda e: e.tensor_tensor(out=gmask, in0=lg, in1=bc4(gmax), op=ALU.is_equal))
                  S.add("act", lambda e: e.activation(out=gsh, in_=gsh, func=AF.Exp), reads=[R_rout], writes=[R_rout])
                  rop(lambda e: e.tensor_reduce(out=gsum, in_=gsh, axis=AX.X, op=ALU.add))
                  rop(lambda e: e.reciprocal(out=gp, in_=gsum))
                  rop(lambda e: e.tensor_tensor(out=t44, in0=le, in1=gmask.unsqueeze(3).broadcast_to([128, nT, 4, 4]), op=ALU.mult))
                  rop(lambda e: e.tensor_reduce(out=elsel, in_=t44.rearrange("p t g x -> p t x g"), axis=AX.X, op=ALU.add))
                  rop(lambda e: e.tensor_reduce(out=m1, in_=elsel, axis=AX.X, op=ALU.max))
                  rop(lambda e: e.tensor_tensor(out=mask1, in0=elsel, in1=bc4(m1), op=ALU.is_equal))
                  rop(lambda e: e.scalar_tensor_tensor(out=el2, in0=mask1, scalar=NEG, in1=elsel, op0=ALU.mult, op1=ALU.add))
                  rop(lambda e: e.tensor_reduce(out=m2, in_=el2, axis=AX.X, op=ALU.max))
                  rop(lambda e: e.tensor_tensor(out=mask2, in0=el2, in1=bc4(m2), op=ALU.is_equal))
                  rop(lambda e: e.tensor_tensor(out=dd, in0=m2, in1=m1, op=ALU.subtract))
                  S.add("act", lambda e: e.activation(out=dd, in_=dd, func=AF.Exp), reads=[R_rout], writes=[R_rout])
                  rop(lambda e: e.tensor_scalar_add(out=w1, in0=dd, scalar1=1.0))
                  rop(lambda e: e.reciprocal(out=w1, in_=w1))
                  rop(lambda e: e.tensor_tensor(out=w1, in0=w1, in1=gp, op=ALU.mult))
                  rop(lambda e: e.tensor_tensor(out=w2, in0=dd, in1=w1, op=ALU.mult))
                  rop(lambda e: e.tensor_tensor(out=within, in0=mask1, in1=bc4(w1), op=ALU.mult))
                  rop(lambda e: e.tensor_tensor(out=wa_, in0=mask2, in1=bc4(w2), op=ALU.mult))
                  rop(lambda e: e.tensor_tensor(out=within, in0=within, in1=wa_, op=ALU.add))
                  rop(lambda e: e.tensor_tensor(out=gates[:, T0:T1], in0=gmask.unsqueeze(3).broadcast_to([128, nT, 4, 4]), in1=within.unsqueeze(2).broadcast_to([128, nT, 4, 4]), op=ALU.mult))
                  for tt in range(T0, T1):
                      bk = 6 + (tt // 4) % 2
                      S.add("pe", (lambda e, tt=tt, bk=bk: e.transpose(bank(bk)[0:16, (tt % 4) * 128:(tt % 4 + 1) * 128], gates[:, tt].rearrange("p g x -> p (g x)"), id32[:])),
                            reads=[R_rout, R_const], writes=[RB[bk]])
                      if tt % 4 == 3 or tt == T1 - 1:
                          ta = (tt // 4) * 4
                          ta0 = max(ta, T0)
                          S.add("act", (lambda e, bk=bk, ta=ta, ta0=ta0, tt=tt: e.copy(out=gatesT[0:16, ta0 * 128:(tt + 1) * 128], in_=bank(bk)[0:16, (ta0 - ta) * 128:(tt + 1 - ta) * 128])),
                                reads=[RB[bk]], writes=[R_rout])

                  g0, g1 = T0 * 128, T1 * 128
                  S.add("act", (lambda e, g0=g0, g1=g1: e.copy(out=gTh[0:16, g0:g1], in_=gatesT[0:16, g0:g1])), reads=[R_rout], writes=[R_rout])
                  S.add("dve", (lambda e, g0=g0, g1=g1: e.tensor_tensor(out=gTl[0:16, g0:g1], in0=gatesT[0:16, g0:g1], in1=gTh[0:16, g0:g1], op=ALU.subtract)), reads=[R_rout], writes=[R_rout])
              routing_block(tok_blocks[0][0] // 128, 18)
              S.barrier()
              cur[0] = C0
              gTh = alloc("gTh", [16, NT], BF16)
              gTl = alloc("gTl", [16, NT], BF16)
              wgs = [alloc("wgs%d" % i, [128, 8, 256], BF16) for i in range(2)]
              wus = [alloc("wus%d" % i, [128, 8, 256], BF16) for i in range(2)]
              wds = [alloc("wds%d" % i, [128, 2, D], BF16) for i in range(2)]
              sgm = [alloc("sgm0", [128, 2, 512], F32)]
              hgm = [alloc("hgm%d" % i, [128, 2, 512], BF16) for i in range(4)]
              o_ = OT0
              for i in range(2, 4):
                  wgs.append(alloc("wgs%d" % i, [128, 8, 256], BF16, at=o_)); o_ += 4096
                  wus.append(alloc("wus%d" % i, [128, 8, 256], BF16, at=o_)); o_ += 4096
                  wds.append(alloc("wds%d" % i, [128, 2, D], BF16, at=o_)); o_ += 4096
              sgm.append(alloc("sgm1", [128, 2, 512], F32, at=o_)); o_ += 4096
              gsb = []
              for i in range(2):
                  gsb.append(alloc("gsb%d" % i, [128, 512], F32, at=o_)); o_ += 2048
              assert o_ <= OT0 + 36864
              R_ewg = [Res("ewg%d" % i) for i in range(4)]
              R_ewu = [Res("ewu%d" % i) for i in range(4)]
              R_ewd = [Res("ewd%d" % i) for i in range(4)]
              R_sgm = [Res("sgm0"), Res("sgm1")]
              R_hgm = [Res("hgm%d" % i) for i in range(4)]
              R_gsb = [Res("gsb0"), Res("gsb1")]
              mblocks = blocks512 if l == 0 else lat512

              def load_expert(ex, slot, l=l):
                  S.add("pool", (lambda e: e.dma_start(out=wgs[slot][:], in_=ewg[l, ex].rearrange("(kc p) f -> p kc f", p=128))), writes=[R_ewg[slot]], dma=True)
                  S.add("pool", (lambda e: e.dma_start(out=wus[slot][:], in_=ewu[l, ex].rearrange("(kc p) f -> p kc f", p=128))), writes=[R_ewu[slot]], dma=True)
                  S.add("pool", (lambda e: e.dma_start(out=wds[slot][:], in_=ewd[l, ex].rearrange("(kc p) f -> p kc f", p=128))), writes=[R_ewd[slot]], dma=True)

              def emit_front(ex, slot, t0, N, si):
                  S.add("pe", (lambda e: e.matmul(bank(4)[:, 0:N], lhsT=selt[0:16, ex * 128:(ex + 1) * 128], rhs=gTh[0:16, t0:t0 + N], start=True, stop=False)),
                        reads=[R_rout, R_const], writes=[RB[4]])
                  S.add("pe", (lambda e: e.matmul(bank(4)[:, 0:N], lhsT=selt[0:16, ex * 128:(ex + 1) * 128], rhs=gTl[0:16, t0:t0 + N], start=False, stop=True)),
                        reads=[R_rout, R_const], writes=[RB[4]])
                  S.add("act", (lambda e: e.copy(out=gsb[si][:, 0:N], in_=bank(4)[:, 0:N])), reads=[RB[4]], writes=[R_gsb[si]])
                  for oc in range(4):
                      wsrc = wgs[slot] if oc < 2 else wus[slot]
                      rw_ = R_ewg[slot] if oc < 2 else R_ewu[slot]
                      for kc in range(8):
                          S.add("pe", (lambda e, kc=kc, oc=oc, wsrc=wsrc: e.matmul(bank(oc)[:, 0:N], lhsT=wsrc[:, kc, (oc % 2) * 128:(oc % 2 + 1) * 128], rhs=UT[:, kc, t0:t0 + N],
                                                                                    start=(kc == 0), stop=(kc == 7))),
                                reads=[rw_] + rtok("UT", t0, t0 + N), writes=[RB[oc]])
                  S.add("act", (lambda e: e.activation(out=sgm[si][:, :, 0:N], in_=PS[0][:].rearrange("p (j n) -> p j n", j=2)[:, :, 0:N], func=AF.Silu)),
                        reads=[RB[0], RB[1]], writes=[R_sgm[si]])
                  S.add("dve", (lambda e: e.tensor_tensor(out=sgm[si][:, :, 0:N], in0=sgm[si][:, :, 0:N], in1=PS[1][:].rearrange("p (j n) -> p j n", j=2)[:, :, 0:N], op=ALU.mult)),
                        reads=[RB[2], RB[3], R_sgm[si]], writes=[R_sgm[si]])

              def emit_gate(si, q, N):
                  S.add("dve", (lambda e: e.tensor_tensor(out=hgm[q][:, :, 0:N], in0=sgm[si][:, :, 0:N], in1=gsb[si][:, 0:N].unsqueeze(1).broadcast_to([128, 2, N]), op=ALU.mult)),
                        reads=[R_gsb[si], R_sgm[si]], writes=[R_hgm[q]])

              ybc = [0]

              def make_yhalf(half, slots, qs, t0, N, mc, l=l):
                  def f():
                      for dc in range(half * 4, half * 4 + 4):
                          bk = 5 + ybc[0] % 3
                          ybc[0] += 1
                          for j in range(2):
                              for k2 in range(2):
                                  S.add("pe", (lambda e, j=j, k2=k2, dc=dc, bk=bk: e.matmul(bank(bk)[:, 0:N], lhsT=wds[slots[j]][:, k2, dc * 128:(dc + 1) * 128], rhs=hgm[qs[j]][:, k2, 0:N],
                                                                                           start=(j == 0 and k2 == 0), stop=(j == 1 and k2 == 1))),
                                        reads=[R_ewd[slots[j]], R_hgm[qs[j]]], writes=[RB[bk]])
                          S.add("dve", (lambda e, dc=dc, bk=bk: e.scalar_tensor_tensor(out=hres[:, dc, t0:t0 + N], in0=bank(bk)[:, 0:N], scalar=modc(l, 5, dc, mc),
                                                                                      in1=hres[:, dc, t0:t0 + N], op0=ALU.mult, op1=ALU.add)),
                                reads=[RB[bk], R_mod] + rtok("hres%d" % dc, t0, t0 + N), writes=rtok("hres%d" % dc, t0, t0 + N))
                  return f

              load_expert(0, 0)
              load_expert(1, 1)
              pendA = pendB = None
              it = 0
              for pr in range(8):
                  slots = (2 * (pr % 2), 2 * (pr % 2) + 1)
                  for bi, (t0, N) in enumerate(mblocks):
                      qs = ((it % 2) * 2, (it % 2) * 2 + 1)
                      it += 1
                      mc = mcol(t0)
                      emit_front(2 * pr, slots[0], t0, N, 0)
                      if pendA is not None:
                          pendA()
                      emit_gate(0, qs[0], N)
                      emit_front(2 * pr + 1, slots[1], t0, N, 1)
                      if pendB is not None:
                          pendB()
                      emit_gate(1, qs[1], N)
                      pendA = make_yhalf(0, slots, qs, t0, N, mc)
                      pendB = make_yhalf(1, slots, qs, t0, N, mc)
                      if bi == 0 and pr + 1 < 8:
                          nslots = (2 * ((pr + 1) % 2), 2 * ((pr + 1) % 2) + 1)
                          load_expert(2 * pr + 2, nslots[0])
                          load_expert(2 * pr + 3, nslots[1])
              pendA()
              pendB()

              dump("S5", hres[:])
              S.barrier()
              cur[0] = C0
              pre2 = alloc("pre2", [128, 8, 256], BF16)
              presq2 = alloc("presq2", [128, 8, 256], BF16)
              mean2 = alloc("mean2", [128, 256], F32)
              rstd2 = alloc("rstd2", [128, 256], F32)
              otile = [alloc("otile%d" % i, [128, D], F32) for i in range(2)]
              R_l2 = [Res("pre2"), Res("presq2"), Res("mean2"), Res("rstd2")]
              R_ot = [Res("ot0"), Res("ot1")]
              oi = 0
              def s6_part1(bi):
                  t0_ = tok_blocks[bi][0]
                  rh_ = [r_ for ch_ in range(8) for r_ in rtok("hres%d" % ch_, t0_, t0_ + 256)]
                  ln_part1(hres[:, :, t0_:t0_ + 256], 8, 256, ones1k, pre2, presq2, 4 + bi % 2, rh_, R_l2)

              s6_part1(0)
              for bi, (t0, N) in enumerate(tok_blocks):
                  hap = hres[:, :, t0:t0 + 256]
                  rh = [r_ for ch_ in range(8) for r_ in rtok("hres%d" % ch_, t0, t0 + 256)]
                  if bi + 1 < len(tok_blocks):
                      s6_part1(bi + 1)
                  ln_part2(256, mean2, rstd2, 4 + bi % 2, R_l2)
                  normalize(hap, 8, 256, mean2, rstd2, rh, R_l2)
                  if l == 0:
                      for ch in range(8):
                          affine(UT[:, ch, t0:t0 + 256], hres[:, ch, t0:t0 + 256], V("GU", ch, t0), V("BU", ch, t0), rh + [R_vecs], rtok("UT", t0, t0 + 256))
                      for ch in range(8):
                          affine(hres[:, ch, t0:t0 + 256], hres[:, ch, t0:t0 + 256], smc("ln_g", 8 + ch), smc("ln_b", 8 + ch), rh + [R_const], rh)
                      S.add("sp", (lambda e, t0=t0: e.dma_start(out=Hs3[:, :, t0:t0 + 256], in_=hres[:, :, t0:t0 + 256])), reads=rh, writes=rtok("Hs", t0, t0 + 256), dma=True)
                      if debug and nlayers == 1:
                          S.add("sp", (lambda e, t0=t0: e.dma_start(out=dbg.rearrange("p (c t) -> p c t", c=8)[:, :, t0:t0 + 256], in_=hres[:, :, t0:t0 + 256])), reads=rh,
                                writes=[Res("dbgo")], dma=True)
                  else:
                      for ch in range(8):
                          affine(hres[:, ch, t0:t0 + 256], hres[:, ch, t0:t0 + 256], smc("ln_g", 24 + ch), smc("ln_b", 24 + ch), rh + [R_const], rh)
                      for hh in range(2):
                          tk = t0 + hh * 128
                          so = oi % 2
                          oi += 1
                          for ch in range(8):
                              bk = so * 2 + ch // 4
                              S.add("pe", (lambda e, ch=ch, bk=bk, tk=tk: e.transpose(bank(bk)[:, (ch % 4) * 128:(ch % 4 + 1) * 128], hres[:, ch, tk:tk + 128], id32[:])),
                                    reads=rh + [R_const], writes=[RB[bk]])
                          for hf in range(2):
                              bk = so * 2 + hf
                              S.add("act" if hf else "dve", (lambda e, so=so, hf=hf, bk=bk: (e.copy if hf else e.tensor_copy)(out=otile[so][:, hf * 512:(hf + 1) * 512], in_=bank(bk))),
                                    reads=[RB[bk]], writes=[R_ot[so]])
                          S.add("sp", (lambda e, so=so, tk=tk, b=b: e.dma_start(out=outd[b, tk - NCTX:tk - NCTX + 128, :], in_=otile[so][:])), reads=[R_ot[so]], writes=[Res("outw")], dma=True)
    except _Stop:
        pass
    S.barrier()

    with nc.Block() as block:
        @block.tensor
        def _(e):
            S.emit_one("pe", e, esem, dsems)

        @block.scalar
        def _(e):
            S.emit_one("act", e, esem, dsems)

        @block.vector
        def _(e):
            S.emit_one("dve", e, esem, dsems)

        @block.gpsimd
        def _(e):
            S.emit_one("pool", e, esem, dsems)

        @block.sync
        def _(e):
            S.emit_one("sp", e, esem, dsems)
    es.close()
    return nc


def _prep_shared(inp):
    f = lambda a: np.ascontiguousarray(np.asarray(a, np.float32))
    sm = np.zeros((128, SMN), np.float32)

    def put(name, arr):
        arr = np.asarray(arr, np.float32)
        sm[:, SMO[name]:SMO[name] + arr.shape[1]] = arr

    put("ada_b0", _fm(inp["ada_b"][0]))
    put("ada_b1", _fm(inp["ada_b"][1]))
    put("ln_g", np.concatenate([_fm(inp["ln_g"][l, k]) for l in range(2) for k in range(2)], axis=1))
    put("ln_b", np.concatenate([_fm(inp["ln_b"][l, k]) for l in range(2) for k in range(2)], axis=1))
    b_in = np.asarray(inp["ab_b_in"][0], np.float32)
    put("b_in", _fm(b_in[:2048]))
    cw = np.asarray(inp["conv_w"][0], np.float32)
    put("conv_w", np.ascontiguousarray(cw.T.reshape(4, 128, 31).transpose(1, 0, 2).reshape(128, 124)))
    put("conv_b", _fm(inp["conv_b"][0]))
    put("cln_g", _fm(inp["conv_ln_g"][0]))
    put("cln_b", _fm(inp["conv_ln_b"][0]))
    put("b_out", _fm(inp["ab_b_out"][0]))
    sm[:, SMO["eps"]] = EPS
    qidx = _gqa_qidx()
    gw = np.asarray(inp["gqa_w_in"][0], np.float32)
    wq = gw[:, :1024]
    wkk = gw[:, 1024:1280]
    wvv = gw[:, 1280:1536]
    C, Sg = _rope_tables()
    kk = np.arange(128)[:, None]
    qq = np.arange(128)[None, :]
    maskL = np.where(kk >= qq, 0.0, NEG).astype(np.float32)
    maskU = np.where(kk <= qq, 0.0, NEG).astype(np.float32)
    sink = np.asarray(inp["gqa_sink"][0], np.float32)
    sperm = np.array([8 * m + 4 * sh + j for m in range(2) for sh in range(2) for j in range(4)])
    sel = np.zeros((16, 16, 128), np.float32)
    for ex in range(16):
        sel[ex, ex, :] = 1.0
    wr = np.stack([np.concatenate([np.asarray(inp["router_group"][l], np.float32), np.asarray(inp["router_expert"][l], np.float32)], axis=1)
                   .reshape(8, 128, 20).transpose(1, 0, 2).reshape(128, 160) for l in range(2)])
    bv = b_in[2048:2560]
    shared = {
        "ada_w": f(inp["ada_w"]),
        "sm": sm,
        "w_in0": f(inp["ab_w_in"][0]),
        "bvbc": np.ascontiguousarray(np.broadcast_to(bv[None, :], (128, 512))),
        "nab": np.ascontiguousarray(_na_bias_table(np.asarray(inp["na_rpb"][0], np.float32)).reshape(8, 128, 21 * 128)),
        "w_out0": f(inp["ab_w_out"][0]),
        "wq1": f(wq[:, qidx]),
        "wqs1": f(wq[:, qidx][:, _swap64(1024)]),
        "wk1": f(wkk),
        "wks1": f(wkk[:, _swap64(256)]),
        "wv1": f(wvv),
        "w_out1": f(np.asarray(inp["gqa_w_out"][0], np.float32)[qidx, :]),
        "ropeC": C,
        "ropeS": Sg,
        "maskLU": np.ascontiguousarray(np.concatenate([maskL, maskU], axis=1)),
        "sinkbc": np.ascontiguousarray(np.broadcast_to(sink[sperm][None, :], (128, 16))),
        "wr": f(wr),
        "sel": np.ascontiguousarray(sel.reshape(16, 2048)),
        "ident": np.eye(128, dtype=np.float32),
        "ewg": f(inp["exp_w_gate"]),
        "ewu": f(inp["exp_w_up"]),
        "ewd": f(inp["exp_w_down"]),
    }
    return shared


def _core_inputs(inp, shared, i):
    x = np.asarray(inp["x"], np.float32)
    ctx = np.asarray(inp["ctx"], np.float32)
    c = np.asarray(inp["c"], np.float32)
    cc = np.stack([c[2 * i], c[2 * i + 1], np.asarray(inp["c_ctx"], np.float32)])
    cvec = np.ascontiguousarray(cc.reshape(3, 8, 128).transpose(2, 1, 0).reshape(128, 24))
    m = dict(shared)
    m["x2"] = np.ascontiguousarray(x[2 * i:2 * i + 2])
    m["ctx2"] = np.ascontiguousarray(ctx[2 * i:2 * i + 2])
    m["cvec"] = cvec
    return m


_NC_CACHE = {}


def kernel(**inputs):
    n = 8
    if "nc" not in _NC_CACHE:
        _NC_CACHE["nc"] = build()
    nc = _NC_CACHE["nc"]
    shared = _prep_shared(inputs)
    in_maps = [_core_inputs(inputs, shared, i) for i in range(n)]
    res = run_bass_kernel_spmd(nc, in_maps, core_ids=list(range(n)))
    out = np.concatenate([np.asarray(r["out"], np.float32) for r in res.results], axis=0)
    return out
```

```python
import numpy as np
from contextlib import ExitStack
import concourse.bass as bass
import concourse.mybir as mybir
from concourse.bass_utils import run_bass_kernel_spmd

F32 = mybir.dt.float32
BF16 = mybir.dt.bfloat16
AF = mybir.ActivationFunctionType
ALU = mybir.AluOpType
AX = mybir.AxisListType

D = 1024
SEQ = 2048
NCTX = 256
NT = SEQ + NCTX
GW = 64
ALPHA = 4.0 ** 0.25
EPS = 1e-5
NEG = -1e30

ENGS = ("pe", "act", "dve", "pool", "sp")
N_DMA_SEMS = 40


class Res:
    __slots__ = ("name", "last_w", "readers")

    def __init__(self, name):
        self.name = name
        self.last_w = None
        self.readers = []


class Op:
    __slots__ = ("eng", "fn", "idx", "deps", "dma", "sig", "semval", "dsem", "dval", "dprev", "seq")

    def __init__(self, eng, fn, idx, dma):
        self.eng = eng
        self.fn = fn
        self.idx = idx
        self.deps = []
        self.dma = dma
        self.sig = False
        self.semval = 0
        self.dsem = -1
        self.dval = 0
        self.dprev = 0


class Sched:
    def __init__(self):
        self.ops = {e: [] for e in ENGS}
        self.ndma = 0
        self.nseq = 0
        self.dma_tot = [0] * N_DMA_SEMS
        self.last_dma = [None] * N_DMA_SEMS
        self._assigned = False

    def add(self, eng, fn, reads=(), writes=(), dma=False, extra=()):
        lst = self.ops[eng]
        op = Op(eng, fn, len(lst), dma)
        op.seq = self.nseq
        self.nseq += 1
        deps = {}
        for r in reads:
            if r.last_w is not None:
                deps[id(r.last_w)] = r.last_w
        for w in writes:
            if w.last_w is not None:
                deps[id(w.last_w)] = w.last_w
            for rd in w.readers:
                deps[id(rd)] = rd
        for x in extra:
            deps[id(x)] = x
        for r in reads:
            r.readers.append(op)
        for w in writes:
            w.last_w = op
            w.readers = []
        if dma:
            s = self.ndma % N_DMA_SEMS
            self.ndma += 1
            op.dsem = s
            op.dprev = self.dma_tot[s]
            self.dma_tot[s] += 16
            op.dval = self.dma_tot[s]
            self.last_dma[s] = op
        for d in deps.values():
            if d is op:
                continue
            if d.eng == eng and not d.dma and not dma:
                if eng == "pe":
                    continue
                if op.idx - d.idx > 2:
                    continue
            op.deps.append(d)
            if not d.dma:
                d.sig = True
        lst.append(op)
        return op

    def mark(self):
        return self.nseq

    def interleave(self, m0, m1, m2):
        assert m2 == self.nseq
        na, nb_ = m1 - m0, m2 - m1
        if na == 0 or nb_ == 0:
            return
        newseq = {}
        ia = ib = 0
        k = m0
        while ia < na or ib < nb_:
            if ib >= nb_ or (ia < na and ia * nb_ <= ib * na):
                newseq[m0 + ia] = k
                ia += 1
            else:
                newseq[m1 + ib] = k
                ib += 1
            k += 1
        for e in ENGS:
            lst = self.ops[e]
            j0 = len(lst)
            while j0 > 0 and lst[j0 - 1].seq >= m0:
                j0 -= 1
            seg = lst[j0:]
            for op in seg:
                op.seq = newseq[op.seq]
            seg.sort(key=lambda o: o.seq)
            lst[j0:] = seg
            for i, op in enumerate(lst[j0:], start=j0):
                op.idx = i

    def barrier(self):
        lasts = []
        for e in ENGS:
            for op in reversed(self.ops[e]):
                if not op.dma:
                    lasts.append(op)
                    break
        dl = [o for o in self.last_dma if o is not None]
        for e in ENGS:
            self.add(e, lambda eng: eng.nop(), extra=[o for o in lasts if o.eng != e] + dl)

    def emit_one(self, e, eng, esem, dsems):
        if not self._assigned:
            for ee in ENGS:
                c = 0
                for op in self.ops[ee]:
                    if op.sig and not op.dma:
                        c += 1
                        op.semval = c
            self._assigned = True
        seen = {}
        for op in self.ops[e]:
            need = {}
            for d in op.deps:
                if d.dma:
                    key = ("d", d.dsem)
                    val = d.dval
                else:
                    key = ("e", d.eng)
                    val = d.semval
                if val > need.get(key, 0):
                    need[key] = val
            if op.dma and op.dprev > 0:
                key = ("d", op.dsem)
                if op.dprev > need.get(key, 0):
                    need[key] = op.dprev
            for key, val in need.items():
                if seen.get(key, 0) >= val:
                    continue
                seen[key] = val
                sem = dsems[key[1]] if key[0] == "d" else esem[key[1]]
                eng.wait_ge(sem, val)
            ins = op.fn(eng)
            if op.dma:
                ins.then_inc(dsems[op.dsem], 16)
            elif op.sig:
                ins.then_inc(esem[e], 1)


def _sm_layout():
    off = {}
    n = 0
    for name, cols in (("ada_b0", 48), ("ada_b1", 48), ("ln_g", 32), ("ln_b", 32), ("b_in", 16),
                       ("conv_w", 124), ("conv_b", 4), ("cln_g", 4), ("cln_b", 4), ("b_out", 8), ("eps", 1)):
        off[name] = n
        n += cols
    return off, n


SMO, SMN = _sm_layout()


def _fm(v):
    v = np.asarray(v, np.float32)
    return np.ascontiguousarray(v.reshape(-1, 128).T)


def _gqa_qidx():
    idx = np.zeros(1024, np.int64)
    for c in range(8):
        m, j = divmod(c, 4)
        h0 = 8 * m + j
        h1 = 8 * m + 4 + j
        idx[c * 128:c * 128 + 64] = h0 * 64 + np.arange(64)
        idx[c * 128 + 64:c * 128 + 128] = h1 * 64 + np.arange(64)
    return idx


def _swap64(n):
    d = np.arange(n)
    dd = d % 64
    sw = np.where(dd % 32 < 16, dd + 16, dd - 16)
    return (d // 64) * 64 + sw


def _na_tiles(j):
    if j in (0, 1):
        return [0, 1, 2, 3]
    if j in (14, 15):
        return [12, 13, 14, 15]
    return [j - 2, j - 1, j, j + 1, j + 2]


def _na_tile_index(j):
    if j == 0:
        return 5
    if j == 1:
        return 9
    if j == 14:
        return 13
    if j == 15:
        return 17
    return 0


def _na_bias_table(rpb):
    rows = 32
    r = np.arange(rows)
    row_start = np.clip(r - 4, 0, rows - 8)
    jj = np.arange(GW)
    col_start = np.clip(jj - 8, 0, GW - 16)
    col_in = (jj[None, :] >= col_start[:, None]) & (jj[None, :] < col_start[:, None] + 16)
    col_off = np.clip(jj[None, :] - jj[:, None], -15, 15) + 15
    out = np.full((8, 21, 128, 128), NEG, np.float32)

    def tile(j, a):
        t = np.full((8, 128, 128), NEG, np.float32)
        for pk in range(2):
            rk = 2 * a + pk
            for pq in range(2):
                rq = 2 * j + pq
                if not (row_start[rq] <= rk < row_start[rq] + 8):
                    continue
                ro = rk - rq + 7
                blk = rpb[:, ro][:, col_off]
                blk = np.where(col_in[None], blk, np.float32(NEG))
                t[:, pk * 64:(pk + 1) * 64, pq * 64:(pq + 1) * 64] = blk.transpose(0, 2, 1)
        return t

    for i, a in enumerate(_na_tiles(5)):
        out[:, i] = tile(5, a)
    for j in (0, 1, 14, 15):
        base = _na_tile_index(j)
        for i, a in enumerate(_na_tiles(j)):
            out[:, base + i] = tile(j, a)
    return np.ascontiguousarray(out.transpose(0, 2, 1, 3))


def _rope_tables():
    t = np.arange(SEQ)
    row = (t // GW).astype(np.float32)
    col = (t % GW).astype(np.float32)
    inv = (np.float32(10000.0) ** (-np.arange(0, 32, 2, dtype=np.float32) / np.float32(32))).astype(np.float32)
    ang = np.concatenate([row[:, None] * inv, col[:, None] * inv], axis=-1).astype(np.float32)
    cos = np.cos(ang).astype(np.float32)
    sin = np.sin(ang).astype(np.float32)
    p = np.arange(128)
    d = p % 64
    ai = (d // 32) * 16 + d % 16
    sgn = np.where(d % 32 < 16, -1.0, 1.0).astype(np.float32)
    C = np.ascontiguousarray(cos[:, ai].T)
    S = np.ascontiguousarray((sin[:, ai] * sgn[None, :]).T)
    return C.astype(np.float32), S.astype(np.float32)


class _Stop(Exception):
    pass


def build(nlayers=2, nb=2, debug=False, stop=None):
    nc = bass.Bass("TRN2", target_bir_lowering=False)
    S = Sched()

    def din(name, shape):
        return nc.dram_tensor(name, list(shape), F32, kind="ExternalInput").ap()

    x2 = din("x2", [2, SEQ, D])
    ctx2 = din("ctx2", [2, NCTX, D])
    cvec = din("cvec", [128, 24])
    ada_w = din("ada_w", [2, D, 6 * D])
    smd = din("sm", [128, SMN])
    w_in0 = din("w_in0", [D, 2560])
    bvbc = din("bvbc", [128, 512])
    nab = din("nab", [8, 128, 21 * 128])
    w_out0 = din("w_out0", [D, D])
    wq1 = din("wq1", [D, 1024])
    wqs1 = din("wqs1", [D, 1024])
    wk1 = din("wk1", [D, 256])
    wks1 = din("wks1", [D, 256])
    wv1 = din("wv1", [D, 256])
    w_out1 = din("w_out1", [D, D])
    ropeC = din("ropeC", [128, SEQ])
    ropeS = din("ropeS", [128, SEQ])
    maskLU = din("maskLU", [128, 256])
    sinkbc = din("sinkbc", [128, 16])
    wr = din("wr", [2, 128, 160])
    sel = din("sel", [16, 2048])
    ident = din("ident", [128, 128])
    ewg = din("ewg", [2, 16, D, 256])
    ewu = din("ewu", [2, 16, D, 256])
    ewd = din("ewd", [2, 16, 256, D])
    outd = nc.dram_tensor("out", [2, SEQ, D], F32, kind="ExternalOutput").ap()
    Hs = nc.dram_tensor("Hs", [128, 8 * NT], F32, kind="Internal").ap()
    dbg = nc.dram_tensor("dbg", [128, 8 * NT], F32, kind="ExternalOutput").ap() if debug else None
    dbgb = nc.dram_tensor("dbgb", [128, 8 * NT], BF16, kind="ExternalOutput").ap() if debug else None
    Hs3 = Hs.rearrange("p (c t) -> p c t", c=8)

    es = ExitStack()
    cur = [16640]

    acache = {}

    def alloc(name, shape, dt, at=None):
        nbytes = int(np.prod(shape[1:])) * (4 if dt == F32 else 2)
        if at is None:
            at = cur[0]
            cur[0] = (at + nbytes + 63) // 64 * 64
        assert at + nbytes <= 229376, (name, at, nbytes)
        key = (name, at, tuple(shape))
        if key not in acache:
            acache[key] = nc.alloc_sbuf_tensor_at("%s_%d" % (name, len(acache)), list(shape), dt, offset=at)
        return acache[key]

    sm = alloc("sm", [128, SMN], F32)
    id32 = alloc("id32", [128, 128], F32)
    ones1k = alloc("ones1k", [128, 128], BF16)
    ones512 = alloc("ones512", [128, 128], BF16)
    csil = alloc("csil", [128, 24], BF16)
    cv32 = alloc("cv32", [128, 24], F32)
    mod = alloc("mod", [128, 2 * 144], F32)
    mp1 = alloc("mp1", [128, 2 * 144], F32)
    vecs = alloc("vecs", [128, 128], F32)
    selt = alloc("selt", [16, 2048], BF16)
    wrt = alloc("wrt", [128, 320], F32)
    esink = alloc("esink", [128, 16], F32)
    R_const = Res("const")
    R_mod = Res("mod")
    R_vecs = Res("vecs")
    base0 = cur[0]

    PS = [es.enter_context(nc.psum_tensor("ps%d" % i, [128, 1024], F32)) for i in range(4)]
    RB = [Res("bank%d" % i) for i in range(8)]

    def bank(k):
        return PS[k // 2][:, (k % 2) * 512:(k % 2) * 512 + 512]

    esem = {e: es.enter_context(nc.semaphore("es_" + e)) for e in ENGS}
    dsems = [es.enter_context(nc.semaphore("ds%d" % i)) for i in range(N_DMA_SEMS)]

    def smc(name, j, n=1):
        o = SMO[name] + j
        return sm[:, o:o + n]

    def modc(l, k, ch, col):
        o = l * 144 + (k * 8 + ch) * 3 + col
        return mod[:, o:o + 1]

    def mp1c(l, k, ch, col):
        o = l * 144 + (k * 8 + ch) * 3 + col
        return mp1[:, o:o + 1]

    VK = {}

    def vslot(kind, ch):
        key = (kind, ch)
        if key not in VK:
            VK[key] = len(VK)
            assert len(VK) <= 128
        o = VK[key]
        return vecs[:, o:o + 1]

    S.add("sp", lambda e: e.dma_start(out=sm[:], in_=smd), writes=[R_const], dma=True)
    S.add("sp", lambda e: e.dma_start(out=id32[:], in_=ident), writes=[R_const], dma=True)
    S.add("sp", lambda e: e.dma_start(out=cv32[:], in_=cvec), writes=[R_const], dma=True)
    S.add("pool", lambda e: e.dma_start(out=selt[:], in_=sel), writes=[R_const], dma=True)
    S.add("sp", lambda e: e.dma_start(out=wrt[:].rearrange("p (l n) -> p l n", l=2), in_=wr.rearrange("l p n -> p l n")), writes=[R_const], dma=True)
    S.add("sp", lambda e: e.dma_start(out=esink[:], in_=sinkbc), writes=[R_const], dma=True)
    S.add("pool", lambda e: e.memset(ones1k[:], 1.0 / 1024.0), writes=[R_const])
    S.add("pool", lambda e: e.memset(ones512[:], 1.0 / 512.0), writes=[R_const])
    S.add("act", lambda e: e.activation(out=csil[:], in_=cv32[:], func=AF.Silu), reads=[R_const], writes=[R_const])
    S.add("act", lambda e: e.activation(out=esink[:], in_=esink[:], func=AF.Exp), reads=[R_const], writes=[R_const])

    adaw = [alloc("adaw%d" % i, [128, 8, 1024], BF16) for i in range(2)]
    R_adaw = [Res("adaw0"), Res("adaw1")]
    pi = 0
    for l in range(nlayers):
        awl = ada_w[l].rearrange("(kc p) n -> p kc n", p=128)
        for piece in range(6):
            s = pi % 2
            pi += 1
            S.add("pool", (lambda e, s=s, awl=awl, piece=piece: e.dma_start(out=adaw[s][:], in_=awl[:, :, piece * 1024:(piece + 1) * 1024])),
                  writes=[R_adaw[s]], dma=True)
            for oc8 in range(8):
                oc = piece * 8 + oc8
                for kc in range(8):
                    S.add("pe", (lambda e, s=s, oc=oc, oc8=oc8, kc=kc: e.matmul(bank(0)[:, oc * 3:oc * 3 + 3], lhsT=adaw[s][:, kc, oc8 * 128:(oc8 + 1) * 128],
                                                                                    rhs=csil[:, kc * 3:kc * 3 + 3], start=(kc == 0), stop=(kc == 7))),
                          reads=[R_adaw[s], R_const], writes=[RB[0]])
        ab = smc("ada_b%d" % l, 0, 48)
        S.add("dve", (lambda e, l=l, ab=ab: e.tensor_tensor(out=mod[:, l * 144:(l + 1) * 144].rearrange("p (a b) -> p a b", b=3),
                                                             in0=bank(0)[:, 0:144].rearrange("p (a b) -> p a b", b=3),
                                                             in1=ab.unsqueeze(2).broadcast_to([128, 48, 3]), op=ALU.add)),
              reads=[RB[0], R_const], writes=[R_mod])
        S.add("dve", (lambda e, l=l: e.tensor_scalar_add(out=mp1[:, l * 144:(l + 1) * 144], in0=mod[:, l * 144:(l + 1) * 144], scalar1=1.0)),
              reads=[R_mod], writes=[R_mod])
    S.barrier()
    cur[0] = base0

    A0 = cur[0]
    hres = alloc("hres", [128, 8, NT], F32)
    qT = alloc("qT", [128, 4, NT], BF16, at=A0)
    kT = alloc("kT", [128, 4, NT], BF16, at=A0 + 18432)
    vaug = alloc("vaug", [128, 18, 4, 192], BF16, at=A0 + 36864)
    qT1 = alloc("qT1", [128, 8, SEQ], BF16, at=A0)
    kT1 = alloc("kT1", [128, 2, NT], BF16, at=A0 + 32768)
    vaug1 = alloc("vaug1", [128, 18, 2, 192], BF16, at=A0 + 41984)
    rC = alloc("rC", [128, SEQ], F32, at=A0 + 55808)
    rS = alloc("rS", [128, SEQ], F32, at=A0 + 55808 + 8192)
    bvb = alloc("bvb", [128, 512], F32, at=A0 + 64512)
    idb = alloc("idb", [128, 128], BF16, at=A0 + 64512 + 2048)
    cmean = alloc("cmean", [128, 256], F32, at=A0 + 64512 + 2304)
    crstd = alloc("crstd", [128, 256], F32, at=A0 + 64512 + 3328)
    UT = alloc("UT", [128, 8, NT], BF16)
    OT0 = cur[0]
    oT = alloc("oT", [128, 8, NT], BF16)
    C0 = cur[0]
    RT = {}

    def rtok(name, t0, t1):
        out = []
        for tt in range(t0 // 128, (t1 + 127) // 128):
            key = (name, tt)
            if key not in RT:
                RT[key] = Res("%s_%d" % key)
            out.append(RT[key])
        return out

    R_hpad = [Res("hpad%d" % c) for c in range(4)]

    def derive_vecs(l, col, tag):
        ops = []
        for ch in range(8):
            g1 = smc("ln_g", (l * 2 + 0) * 8 + ch)
            b1 = smc("ln_b", (l * 2 + 0) * 8 + ch)
            g2 = smc("ln_g", (l * 2 + 1) * 8 + ch)
            b2 = smc("ln_b", (l * 2 + 1) * 8 + ch)
            S.add("dve", (lambda e, ch=ch, g1=g1: e.tensor_tensor(out=vslot((tag, "G4"), ch), in0=g1, in1=mp1c(l, 4, ch, col), op=ALU.mult)),
                  reads=[R_const, R_mod], writes=[R_vecs])
            S.add("dve", (lambda e, ch=ch, b1=b1: e.scalar_tensor_tensor(out=vslot((tag, "B4"), ch), in0=b1, scalar=mp1c(l, 4, ch, col), in1=modc(l, 3, ch, col),
                                                                         op0=ALU.mult, op1=ALU.add)),
                  reads=[R_const, R_mod], writes=[R_vecs])
            S.add("dve", (lambda e, ch=ch, g1=g1: e.tensor_scalar_mul(out=vslot((tag, "GA1"), ch), in0=g1, scalar1=ALPHA)), reads=[R_const], writes=[R_vecs])
            S.add("dve", (lambda e, ch=ch, b1=b1: e.tensor_scalar_mul(out=vslot((tag, "BA1"), ch), in0=b1, scalar1=ALPHA)), reads=[R_const], writes=[R_vecs])
            if l == 0:
                S.add("dve", (lambda e, ch=ch: e.tensor_tensor(out=vslot((tag, "M2B"), ch), in0=modc(l, 2, ch, col), in1=smc("b_out", ch), op=ALU.mult)),
                      reads=[R_const, R_mod], writes=[R_vecs])
                S.add("dve", (lambda e, ch=ch, g2=g2: e.tensor_tensor(out=vslot((tag, "GU"), ch), in0=g2, in1=mp1c(1, 1, ch, col), op=ALU.mult)),
                      reads=[R_const, R_mod], writes=[R_vecs])
                S.add("dve", (lambda e, ch=ch, b2=b2: e.scalar_tensor_tensor(out=vslot((tag, "BU"), ch), in0=b2, scalar=mp1c(1, 1, ch, col), in1=modc(1, 0, ch, col),
                                                                             op0=ALU.mult, op1=ALU.add)),
                      reads=[R_const, R_mod], writes=[R_vecs])

    def ln_part1(pre_ap, nch, N, ones_t, prebf, presq, bk, r_pre, r_tmp):
        S.add("dve", lambda e: e.tensor_copy(out=prebf[:, 0:nch, 0:N], in_=pre_ap), reads=r_pre, writes=[r_tmp[0]])
        S.add("act", lambda e: e.activation(out=presq[:, 0:nch, 0:N], in_=pre_ap, func=AF.Square), reads=r_pre, writes=[r_tmp[1]])
        for c in range(nch):
            S.add("pe", (lambda e, c=c: e.matmul(bank(bk)[:, 0:N], lhsT=ones_t[:], rhs=prebf[:, c, 0:N], start=(c == 0), stop=(c == nch - 1))),
                  reads=[r_tmp[0], R_const], writes=[RB[bk]])
        for c in range(nch):
            S.add("pe", (lambda e, c=c: e.matmul(bank(bk)[:, 256:256 + N], lhsT=ones_t[:], rhs=presq[:, c, 0:N], start=(c == 0), stop=(c == nch - 1))),
                  reads=[r_tmp[1], R_const], writes=[RB[bk]])

    def ln_part2(N, mean_sb, rstd_sb, bk, r_tmp):
        S.add("act", lambda e: e.copy(out=mean_sb[:, 0:N], in_=bank(bk)[:, 0:N]), reads=[RB[bk]], writes=[r_tmp[2]])
        S.add("dve", lambda e: e.tensor_tensor(out=rstd_sb[:, 0:N], in0=mean_sb[:, 0:N], in1=mean_sb[:, 0:N], op=ALU.mult), reads=[r_tmp[2]], writes=[r_tmp[3]])
        S.add("dve", lambda e: e.tensor_tensor(out=rstd_sb[:, 0:N], in0=bank(bk)[:, 256:256 + N], in1=rstd_sb[:, 0:N], op=ALU.subtract),
              reads=[RB[bk], r_tmp[3]], writes=[r_tmp[3]])
        S.add("act", lambda e: e.activation(out=rstd_sb[:, 0:N], in_=rstd_sb[:, 0:N], func=AF.Ln, bias=smc("eps", 0), scale=1.0),
              reads=[r_tmp[3], R_const], writes=[r_tmp[3]])
        S.add("act", lambda e: e.activation(out=rstd_sb[:, 0:N], in_=rstd_sb[:, 0:N], func=AF.Exp, scale=-0.5), reads=[r_tmp[3]], writes=[r_tmp[3]])

    def ln_stats(pre_ap, nch, N, ones_t, prebf, presq, mean_sb, rstd_sb, bk, r_pre, r_tmp):
        ln_part1(pre_ap, nch, N, ones_t, prebf, presq, bk, r_pre, r_tmp)
        ln_part2(N, mean_sb, rstd_sb, bk, r_tmp)

    def normalize(pre_ap, nch, N, mean_sb, rstd_sb, r_pre, r_tmp):
        S.add("dve", lambda e: e.tensor_tensor(out=pre_ap, in0=pre_ap, in1=mean_sb[:, 0:N].unsqueeze(1).broadcast_to([128, nch, N]), op=ALU.subtract),
              reads=r_pre + [r_tmp[2]], writes=r_pre)
        S.add("dve", lambda e: e.tensor_tensor(out=pre_ap, in0=pre_ap, in1=rstd_sb[:, 0:N].unsqueeze(1).broadcast_to([128, nch, N]), op=ALU.mult),
              reads=r_pre + [r_tmp[3]], writes=r_pre)

    aff_rr = [0]

    def affine(out_ap, in_ap, sc, bi, reads, writes, psum_in=False):
        k = aff_rr[0] % 2
        aff_rr[0] += 1
        if k == 0:
            S.add("act", lambda e: e.activation(out=out_ap, in_=in_ap, func=AF.Identity, bias=bi, scale=sc), reads=reads, writes=writes)
        else:
            S.add("dve" if k == 1 else "pool", lambda e: e.tensor_scalar(out=out_ap, in0=in_ap, scalar1=sc, scalar2=bi, op0=ALU.mult, op1=ALU.add),
                  reads=reads, writes=writes)

    def dump(name, src_ap3):
        if stop != name and stop != "%s@%d" % (name, cur_l[0]):
            return
        S.barrier()
        c, t = src_ap3.shape[1], src_ap3.shape[2]
        dst = dbg if src_ap3.dtype == F32 else dbgb
        for ci in range(c):
            S.add("sp", (lambda e, ci=ci: e.dma_start(out=dst[:, ci * t:(ci + 1) * t], in_=src_ap3[:, ci, :])), writes=[Res("dbgo")], dma=True)
        raise _Stop()

    cur_l = [0]
    try:
      for b in range(nb):
        for l in range(nlayers):
              cur_l[0] = l
              lat_only = (l == nlayers - 1) and l == 1
              col = b
              S.barrier()
              derive_vecs(l, b, "lat")
              if l == 0:
                  derive_vecs(l, 2, "ctx")

              def V(kind, ch, t0):
                  return vslot((("ctx" if (t0 < NCTX and l == 0) else "lat"), kind), ch)

              def mcol(t0):
                  return 2 if t0 < NCTX else b

              cur[0] = C0
              if l == 0:
                  xin = [alloc("xin%d" % i, [128, D], F32) for i in range(2)]
                  hblk = [alloc("hblk%d" % i, [128, 8, 128], F32) for i in range(2)]
                  R_xin = [Res("xin0"), Res("xin1")]
                  R_hblk = [Res("hblk0"), Res("hblk1")]
                  for tt in range(18):
                      s = tt % 2
                      src = ctx2[b, tt * 128:(tt + 1) * 128, :] if tt < 2 else x2[b, (tt - 2) * 128:(tt - 1) * 128, :]
                      S.add("sp", (lambda e, s=s, src=src: e.dma_start(out=xin[s][:], in_=src)), writes=[R_xin[s]], dma=True)
                      for ch in range(8):
                          bk = (tt % 2) * 2 + ch // 4
                          S.add("pe", (lambda e, s=s, ch=ch, bk=bk: e.transpose(bank(bk)[:, (ch % 4) * 128:(ch % 4 + 1) * 128], xin[s][:, ch * 128:(ch + 1) * 128], id32[:])),
                                reads=[R_xin[s], R_const], writes=[RB[bk]])
                      for hf in range(2):
                          bk = (tt % 2) * 2 + hf
                          S.add("act", (lambda e, s=s, hf=hf, bk=bk: e.copy(out=hblk[s][:, hf * 4:(hf + 1) * 4, :], in_=bank(bk).rearrange("p (c t) -> p c t", c=4))),
                                reads=[RB[bk]], writes=[R_hblk[s]])
                      mc = mcol(tt * 128)
                      for ch in range(8):
                          bk = (tt % 2) * 2 + ch // 4
                          affine(UT[:, ch, tt * 128:(tt + 1) * 128], bank(bk)[:, (ch % 4) * 128:(ch % 4 + 1) * 128], mp1c(0, 1, ch, mc), modc(0, 0, ch, mc),
                                 [RB[bk], R_mod], rtok("UT", tt * 128, tt * 128 + 128), psum_in=True)
                      S.add("sp", (lambda e, s=s, tt=tt: e.dma_start(out=Hs3[:, :, tt * 128:(tt + 1) * 128], in_=hblk[s][:])), reads=[R_hblk[s]],
                            writes=rtok("Hs", tt * 128, tt * 128 + 128), dma=True)

              if l == 0:
                  dump("S0", UT[:])
              blocks512 = [(0, 256)] + [(256 + 512 * i, 512) for i in range(4)]
              blocks256 = [(256 * i, 256) for i in range(9)]
              if l == 1:
                  lat512 = [(256 + 512 * i, 512) for i in range(4)]
                  lat256 = [(256 * i, 256) for i in range(1, 9)]

              if l == 0:
                  S.barrier()
                  cur[0] = C0
                  wAB = alloc("wAB", [128, 8, 1536], BF16)
                  wA = alloc("wA", [128, 8, 1024], BF16, at=C0)
                  hpd = [alloc("hpd%d" % i, [128, 2368], BF16, at=C0 + 16384 + i * 4736) for i in range(2)]
                  dg0 = alloc("diag0", [128, 31, 128], BF16, at=C0 + 25856)
                  diag = [dg0, dg0]
                  cur[0] = C0 + 33792
                  sgt = [alloc("sgt%d" % i, [128, 512], F32) for i in range(2)]
                  czsq = alloc("czsq", [128, 4, 256], BF16)
                  cz = alloc("cz", [128, 4, 256], F32)
                  R_wAB, R_misc = Res("wAB"), Res("misc0")
                  R_hpd = [Res("hpd0"), Res("hpd1")]
                  R_dg = [Res("dg0")] * 2
                  R_sgt = [Res("sgt0"), Res("sgt1")]
                  R_cz, R_ct = Res("cz"), [Res("czbf"), Res("czsq"), Res("cmean"), Res("crstd")]
                  w0 = w_in0.rearrange("(kc p) n -> p kc n", p=128)
                  S.add("pool", lambda e: e.dma_start(out=wA[:], in_=w0[:, :, 0:1024]), writes=[R_wAB], dma=True)
                  S.add("pool", lambda e: e.dma_start(out=idb[:], in_=ident), writes=[R_misc], dma=True)
                  S.add("sp", lambda e: e.dma_start(out=bvb[:], in_=bvbc), writes=[R_misc], dma=True)
                  S.add("pool", lambda e: e.memset(hpd[0][:], 0.0), writes=[R_hpd[0]])
                  S.add("pool", lambda e: e.memset(hpd[1][:], 0.0), writes=[R_hpd[1]])
                  S.add("pool", lambda e: e.memset(vaug[:], 1.0), writes=rtok("vaug", 0, NT))

                  def hoff(t0):
                      return 15 + t0 if t0 < NCTX else 286 + 15 + (t0 - NCTX)

                  it = 0
                  for cc in range(4):
                      hs_ = cc % 2
                      for k in range(31):
                          S.add("dve", (lambda e, cc=cc, k=k, hs_=hs_: e.tensor_scalar_mul(out=diag[hs_][:, k, :], in0=idb[:], scalar1=smc("conv_w", cc * 31 + k))),
                                reads=[R_misc, R_const], writes=[R_dg[hs_]])
                      for (t0, N) in blocks512:
                          s = it % 2
                          it += 1
                          b1, b2 = 2 * s, 2 * s + 1
                          for kc in range(8):
                              S.add("pe", (lambda e, kc=kc, cc=cc, t0=t0, N=N, b1=b1: e.matmul(bank(b1)[:, 0:N], lhsT=wA[:, kc, cc * 128:(cc + 1) * 128], rhs=UT[:, kc, t0:t0 + N],
                                                                                             start=(kc == 0), stop=(kc == 7))),
                                    reads=[R_wAB] + rtok("UT", t0, t0 + N), writes=[RB[b1]])
                          for kc in range(8):
                              S.add("pe", (lambda e, kc=kc, cc=cc, t0=t0, N=N, b2=b2: e.matmul(bank(b2)[:, 0:N], lhsT=wA[:, kc, 512 + cc * 128:512 + (cc + 1) * 128], rhs=UT[:, kc, t0:t0 + N],
                                                                                             start=(kc == 0), stop=(kc == 7))),
                                    reads=[R_wAB] + rtok("UT", t0, t0 + N), writes=[RB[b2]])
                          S.add("act", (lambda e, s=s, cc=cc, N=N, b2=b2: e.activation(out=sgt[s][:, 0:N], in_=bank(b2)[:, 0:N], func=AF.Sigmoid, bias=smc("b_in", 4 + cc), scale=1.0)),
                                reads=[RB[b2], R_const], writes=[R_sgt[s]])
                          ho = hoff(t0)
                          S.add("dve", (lambda e, s=s, cc=cc, N=N, b1=b1, ho=ho, hs_=hs_: e.scalar_tensor_tensor(out=hpd[hs_][:, ho:ho + N], in0=bank(b1)[:, 0:N], scalar=smc("b_in", cc),
                                                                                                                 in1=sgt[s][:, 0:N], op0=ALU.add, op1=ALU.mult)),
                                reads=[RB[b1], R_sgt[s], R_const], writes=[R_hpd[hs_]])
                      for bi, (t0, N) in enumerate(blocks512):
                          ho = hoff(t0) - 15
                          bk = 4 + bi % 2
                          for k in range(31):
                              S.add("pe", (lambda e, k=k, ho=ho, N=N, bk=bk, hs_=hs_: e.matmul(bank(bk)[:, 0:N], lhsT=diag[hs_][:, k, :], rhs=hpd[hs_][:, ho + k:ho + k + N],
                                                                                           start=(k == 0), stop=(k == 30))),
                                    reads=[R_dg[hs_], R_hpd[hs_]], writes=[RB[bk]])
                          S.add("act", (lambda e, cc=cc, N=N, t0=t0, bk=bk: e.activation(out=oT[:, cc, t0:t0 + N], in_=bank(bk)[:, 0:N], func=AF.Identity, bias=smc("conv_b", cc), scale=1.0)),
                                reads=[RB[bk], R_const], writes=rtok("oT", t0, t0 + N))
                  dump("S1z", oT[:, 0:4, :])
                  dump("S1h", hpd[1][:].unsqueeze(1))
                  dump("S1d", diag[0][:])
                  S.add("pool", lambda e: e.dma_start(out=wAB[:], in_=w0[:, :, 1024:2560]), writes=[R_wAB] + R_hpd + [R_dg[0]], dma=True)
                  mk0 = S.mark()
                  for (t0, N) in blocks256:
                      zin = oT[:, 0:4, t0:t0 + 256]
                      rz = rtok("oT", t0, t0 + 256)
                      S.add("act", (lambda e, zin=zin: e.activation(out=czsq[:], in_=zin, func=AF.Square)), reads=rz, writes=[R_ct[1]])
                      for c4 in range(4):
                          S.add("pe", (lambda e, c4=c4, t0=t0: e.matmul(bank(6)[:, 0:256], lhsT=ones512[:], rhs=oT[:, c4, t0:t0 + 256], start=(c4 == 0), stop=(c4 == 3))),
                                reads=rz + [R_const], writes=[RB[6]])
                      for c4 in range(4):
                          S.add("pe", (lambda e, c4=c4: e.matmul(bank(6)[:, 256:512], lhsT=ones512[:], rhs=czsq[:, c4, :], start=(c4 == 0), stop=(c4 == 3))),
                                reads=[R_ct[1], R_const], writes=[RB[6]])
                      S.add("act", lambda e: e.copy(out=cmean[:], in_=bank(6)[:, 0:256]), reads=[RB[6]], writes=[R_ct[2]])
                      S.add("dve", lambda e: e.tensor_tensor(out=crstd[:], in0=cmean[:], in1=cmean[:], op=ALU.mult), reads=[R_ct[2]], writes=[R_ct[3]])
                      S.add("dve", lambda e: e.tensor_tensor(out=crstd[:], in0=bank(6)[:, 256:512], in1=crstd[:], op=ALU.subtract), reads=[RB[6], R_ct[3]], writes=[R_ct[3]])
                      S.add("act", lambda e: e.activation(out=crstd[:], in_=crstd[:], func=AF.Sqrt, bias=smc("eps", 0), scale=1.0), reads=[R_ct[3], R_const], writes=[R_ct[3]])
                      S.add("dve", lambda e: e.reciprocal(out=crstd[:], in_=crstd[:]), reads=[R_ct[3]], writes=[R_ct[3]])
                      S.add("dve", (lambda e, zin=zin: e.tensor_tensor(out=cz[:], in0=zin, in1=cmean[:].unsqueeze(1).broadcast_to([128, 4, 256]), op=ALU.subtract)),
                            reads=rz + [R_ct[2]], writes=[R_cz])
                      S.add("dve", lambda e: e.tensor_tensor(out=cz[:], in0=cz[:], in1=crstd[:].unsqueeze(1).broadcast_to([128, 4, 256]), op=ALU.mult),
                            reads=[R_cz, R_ct[3]], writes=[R_cz])
                      for c4 in range(4):
                          S.add("act", (lambda e, c4=c4, t0=t0: e.activation(out=oT[:, c4, t0:t0 + 256], in_=cz[:, c4, :], func=AF.Silu, bias=smc("cln_b", c4), scale=smc("cln_g", c4))),
                                reads=[R_cz, R_const], writes=rz)
                  mk1 = S.mark()
                  it = 0
                  for (t0, N) in blocks512:
                      for c in range(8):
                          bk = it % 4
                          it += 1
                          for kc in range(8):
                              S.add("pe", (lambda e, kc=kc, c=c, t0=t0, N=N, bk=bk: e.matmul(bank(bk)[:, 0:N], lhsT=wAB[:, kc, c * 128:(c + 1) * 128], rhs=UT[:, kc, t0:t0 + N],
                                                                                           start=(kc == 0), stop=(kc == 7))),
                                    reads=[R_wAB] + rtok("UT", t0, t0 + N), writes=[RB[bk]])
                          dst = qT if c < 4 else kT
                          S.add("act", (lambda e, c=c, t0=t0, N=N, bk=bk, dst=dst: e.activation(out=dst[:, c % 4, t0:t0 + N], in_=bank(bk)[:, 0:N], func=AF.Identity,
                                                                                              bias=smc("b_in", 8 + c), scale=1.0)),
                                reads=[RB[bk], R_const], writes=rtok("qk", t0, t0 + N))
                  for tt in range(18):
                      bk = it % 4
                      it += 1
                      for kc in range(8):
                          S.add("pe", (lambda e, kc=kc, tt=tt, bk=bk: e.matmul(bank(bk)[:, 0:512], lhsT=UT[:, kc, tt * 128:(tt + 1) * 128], rhs=wAB[:, kc, 1024:1536],
                                                                             start=(kc == 0), stop=(kc == 7))),
                                reads=[R_wAB] + rtok("UT", tt * 128, tt * 128 + 128), writes=[RB[bk]])
                      for x in range(2):
                          S.add("dve", (lambda e, tt=tt, bk=bk, x=x: e.tensor_tensor(out=vaug[:, tt, :, x * 128:x * 128 + 64],
                                                                                   in0=bank(bk).rearrange("p (c x d) -> p c x d", c=4, x=2)[:, :, x, :],
                                                                                   in1=bvb[:].rearrange("p (c x d) -> p c x d", c=4, x=2)[:, :, x, :], op=ALU.add)),
                                reads=[RB[bk], R_misc], writes=rtok("vaug", tt * 128, tt * 128 + 128))

                  S.interleave(mk0, mk1, S.mark())
                  dump("S1o", oT[:, 0:4, :])
                  dump("S1q", qT[:])
                  dump("S1k", kT[:])
                  dump("S1v", vaug[:].rearrange("p t c x -> p t (c x)"))
                  S.barrier()
                  cur[0] = C0
                  nabt = [alloc("nabt%d" % i, [128, 21, 128], F32) for i in range(2)]
                  sbt = [alloc("sbt%d" % i, [128, 640], F32) for i in range(3)]
                  PT = [alloc("PT%d" % i, [128, 896], BF16) for i in range(3)]
                  rec = [alloc("rec%d" % i, [128, 256], F32) for i in range(2)]
                  R_nabt = [Res("nabt0"), Res("nabt1")]
                  R_sbt = [Res("sbt0"), Res("sbt1"), Res("sbt2")]
                  R_PT = [Res("PT0"), Res("PT1"), Res("PT2")]
                  PSl = [PS[0], PS[1], PS[3]]
                  PSb = [0, 2, 6]
                  itl = 0
                  na_q = []
                  na_qB = []
                  R_rec = [Res("rec0"), Res("rec1")]
                  it = 0
                  na_pend = [None]
                  for h in range(8):
                      while na_q or na_qB:
                          if na_qB:
                              na_qB.pop(0)()
                          if na_q:
                              na_q.pop(0)()
                      c, sh = h // 2, h % 2
                      p0, p1 = sh * 64, sh * 64 + 64
                      nh0, nh1 = (0, 64) if sh == 0 else (64, 128)
                      dh0, dh1 = (64, 128) if sh == 0 else (0, 64)
                      hs = h % 2
                      S.add("sp", (lambda e, h=h, hs=hs: e.dma_start(out=nabt[hs][:], in_=nab[h].rearrange("p (a q) -> p a q", a=21))), writes=[R_nabt[hs]], dma=True)
                      vcol = sh * 64
                      s = 0
                      sb0 = 0
                      for i in range(2):
                          S.add("pe", (lambda e, i=i, c=c, p0=p0, p1=p1, sb0=sb0: e.matmul(bank(sb0)[:, i * 256:(i + 1) * 256], lhsT=kT[p0:p1, c, i * 128:(i + 1) * 128],
                                                                                          rhs=qT[p0:p1, c, 0:256], start=True, stop=True)),
                                reads=rtok("qk", 0, 256), writes=[RB[sb0]])
                      S.add("act", (lambda e, s=s, sb0=sb0: e.activation(out=PT[s][:, 0:512], in_=bank(sb0)[:, 0:512], func=AF.Exp, scale=0.125)), reads=[RB[sb0]], writes=[R_PT[s]])
                      ob = 4 + s
                      for i in range(2):
                          S.add("pe", (lambda e, i=i, c=c, s=s, ob=ob, vcol=vcol: e.matmul(bank(ob)[:, 0:256], lhsT=vaug[:, i, c, vcol:vcol + 128], rhs=PT[s][:, i * 256:(i + 1) * 256],
                                                                                          start=(i == 0), stop=(i == 1))),
                                reads=[R_PT[s]] + rtok("vaug", 0, 256), writes=[RB[ob]])
                      S.add("dve", (lambda e, s=s, ob=ob, dh0=dh0, dh1=dh1: e.reciprocal(out=rec[s][dh0:dh1, 0:256], in_=bank(ob)[dh0:dh1, 0:256])), reads=[RB[ob]], writes=[R_rec[s]])
                      S.add("dve", (lambda e, s=s, ob=ob, c=c, nh0=nh0, nh1=nh1, dh0=dh0, dh1=dh1: e.tensor_tensor(out=oT[nh0:nh1, 4 + c, 0:256], in0=bank(ob)[nh0:nh1, 0:256],
                                                                                                               in1=rec[s][dh0:dh1, 0:256], op=ALU.mult)),
                            reads=[RB[ob], R_rec[s]], writes=rtok("oT", 0, 256))
                      for j in range(16):
                          s = itl % 3
                          so = itl % 2
                          itl += 1
                          sb0 = PSb[s]
                          tl = _na_tiles(j)
                          nl = len(tl)
                          ti0 = _na_tile_index(j)
                          q0 = NCTX + j * 128
                          ktoks = [NCTX + a * 128 for a in tl] + [0, 128]
                          for i, kt0 in enumerate(ktoks):
                              bk = sb0 + (i // 4)
                              S.add("pe", (lambda e, i=i, kt0=kt0, bk=bk, c=c, p0=p0, p1=p1, q0=q0: e.matmul(bank(bk)[:, (i % 4) * 128:(i % 4 + 1) * 128], lhsT=kT[p0:p1, c, kt0:kt0 + 128],
                                                                                                        rhs=qT[p0:p1, c, q0:q0 + 128], start=True, stop=True)),
                                    reads=rtok("qk", kt0, kt0 + 128) + rtok("qk", q0, q0 + 128), writes=[RB[bk]])
                          S.add("dve", (lambda e, s=s, nl=nl, ti0=ti0, hs=hs: e.scalar_tensor_tensor(out=sbt[s][:, 0:nl * 128], in0=PSl[s][:, 0:nl * 128], scalar=0.125,
                                                                                                    in1=nabt[hs][:, ti0:ti0 + nl, :].rearrange("p a q -> p (a q)"),
                                                                                                    op0=ALU.mult, op1=ALU.add)),
                                reads=[RB[sb0], RB[sb0 + 1], R_nabt[hs]], writes=[R_sbt[s]])
                          S.add("act", (lambda e, s=s, nl=nl: e.activation(out=PT[s][:, 0:nl * 128], in_=sbt[s][:, 0:nl * 128], func=AF.Exp)), reads=[R_sbt[s]], writes=[R_PT[s]])
                          S.add("act", (lambda e, s=s, nl=nl: e.activation(out=PT[s][:, nl * 128:(nl + 2) * 128], in_=PSl[s][:, nl * 128:(nl + 2) * 128], func=AF.Exp, scale=0.125)),
                                reads=[RB[sb0], RB[sb0 + 1]], writes=[R_PT[s]])
                          ob = 4 + so

                          def na_back(ktoks=ktoks, c=c, s=s, so=so, ob=ob, vcol=vcol, nl=nl, q0=q0, nh0=nh0, nh1=nh1, dh0=dh0, dh1=dh1):
                              for i, kt0 in enumerate(ktoks):
                                  S.add("pe", (lambda e, i=i, kt0=kt0: e.matmul(bank(ob)[:, 0:128], lhsT=vaug[:, kt0 // 128, c, vcol:vcol + 128],
                                                                                rhs=PT[s][:, i * 128:(i + 1) * 128], start=(i == 0), stop=(i == nl + 1))),
                                        reads=[R_PT[s]] + rtok("vaug", kt0, kt0 + 128), writes=[RB[ob]])
                              S.add("act", (lambda e: e.activation(out=rec[so][dh0:dh1, 0:128], in_=bank(ob)[dh0:dh1, 0:128], func=AF.Ln)), reads=[RB[ob]], writes=[R_rec[so]])
                              S.add("act", (lambda e: e.activation(out=rec[so][dh0:dh1, 0:128], in_=rec[so][dh0:dh1, 0:128], func=AF.Exp, scale=-1.0)), reads=[R_rec[so]], writes=[R_rec[so]])

                              def na_backB():
                                  S.add("dve", (lambda e: e.tensor_tensor(out=oT[nh0:nh1, 4 + c, q0:q0 + 128], in0=bank(ob)[nh0:nh1, 0:128],
                                                                          in1=rec[so][dh0:dh1, 0:128], op=ALU.mult)),
                                        reads=[RB[ob], R_rec[so]], writes=rtok("oT", q0, q0 + 128))
                              na_qB.append(na_backB)
                          if na_qB:
                              na_qB.pop(0)()
                          na_q.append(na_back)
                          if len(na_q) > 1:
                              na_q.pop(0)()
                  while na_q or na_qB:
                      if na_qB:
                          na_qB.pop(0)()
                      if na_q:
                          na_q.pop(0)()
                  dump("S3", oT[:, 4:8, :])
                  w_out_d = w_out0
                  tok_blocks = blocks256
              else:
                  S.barrier()
                  cur[0] = C0
                  wq = alloc("wq", [128, 8, 512], BF16)
                  wqs = alloc("wqs", [128, 8, 512], BF16)
                  wk = alloc("wk", [128, 8, 256], BF16)
                  wks = alloc("wks", [128, 8, 256], BF16)
                  wv = alloc("wv", [128, 8, 256], BF16)
                  rt = [alloc("rt%d" % i, [128, 2, 512], F32) for i in range(2)]
                  R_w1, R_rope = Res("w1"), Res("rope")
                  R_rt = [Res("rt0"), Res("rt1")]
                  R_wq = Res("wq")
                  for dst, src in ((wk, wk1), (wks, wks1), (wv, wv1)):
                      S.add("pool", (lambda e, dst=dst, src=src: e.dma_start(out=dst[:], in_=src.rearrange("(kc p) n -> p kc n", p=128))), writes=[R_w1], dma=True)
                  S.add("sp", lambda e: e.dma_start(out=rC[:], in_=ropeC), writes=[R_rope], dma=True)
                  S.add("sp", lambda e: e.dma_start(out=rS[:], in_=ropeS), writes=[R_rope], dma=True)
                  if True:
                      S.add("pool", lambda e: e.memset(vaug1[:], 1.0), writes=rtok("vaug", 0, NT))
                  it = 0
                  for half, (t0, N) in [(hf_, blk_) for hf_ in range(3) for blk_ in lat512]:
                      l0 = t0 - NCTX
                      if half < 2 and t0 == NCTX:
                          for dst, src in ((wq, wq1), (wqs, wqs1)):
                              S.add("pool", (lambda e, dst=dst, src=src, half=half: e.dma_start(out=dst[:], in_=src.rearrange("(kc p) n -> p kc n", p=128)[:, :, half * 512:(half + 1) * 512])),
                                    writes=[R_wq], dma=True)
                      for c in (range(half * 4, half * 4 + 4) if half < 2 else range(8, 10)):
                          s = it % 2
                          it += 1
                          b1, b2 = 2 * s, 2 * s + 1
                          wa, wb = (wq, wqs) if c < 8 else (wk, wks)
                          cc = (c % 4) if c < 8 else c - 8
                          for kc in range(8):
                              S.add("pe", (lambda e, kc=kc, cc=cc, wa=wa, t0=t0, N=N, b1=b1: e.matmul(bank(b1)[:, 0:N], lhsT=wa[:, kc, cc * 128:(cc + 1) * 128], rhs=UT[:, kc, t0:t0 + N],
                                                                                                 start=(kc == 0), stop=(kc == 7))),
                                    reads=[R_w1, R_wq] + rtok("UT", t0, t0 + N), writes=[RB[b1]])
                          for kc in range(8):
                              S.add("pe", (lambda e, kc=kc, cc=cc, wb=wb, t0=t0, N=N, b2=b2: e.matmul(bank(b2)[:, 0:N], lhsT=wb[:, kc, cc * 128:(cc + 1) * 128], rhs=UT[:, kc, t0:t0 + N],
                                                                                                 start=(kc == 0), stop=(kc == 7))),
                                    reads=[R_w1, R_wq] + rtok("UT", t0, t0 + N), writes=[RB[b2]])
                          S.add("dve", (lambda e, s=s, l0=l0, N=N, b1=b1: e.tensor_tensor(out=rt[s][:, 0, 0:N], in0=bank(b1)[:, 0:N], in1=rC[:, l0:l0 + N], op=ALU.mult)),
                                reads=[RB[b1], R_rope], writes=[R_rt[s]])
                          S.add("dve", (lambda e, s=s, l0=l0, N=N, b2=b2: e.tensor_tensor(out=rt[s][:, 1, 0:N], in0=bank(b2)[:, 0:N], in1=rS[:, l0:l0 + N], op=ALU.mult)),
                                reads=[RB[b2], R_rope], writes=[R_rt[s]])
                          if c < 8:
                              dst = qT1[:, c, l0:l0 + N]
                          else:
                              dst = kT1[:, c - 8, t0:t0 + N]
                          S.add("dve", (lambda e, s=s, N=N, dst=dst: e.tensor_tensor(out=dst, in0=rt[s][:, 0, 0:N], in1=rt[s][:, 1, 0:N], op=ALU.add)),
                                reads=[R_rt[s]], writes=rtok("qk", t0, t0 + N))
                  for cc in range(2):
                      s = it % 2
                      it += 1
                      b1 = 2 * s
                      for kc in range(8):
                          S.add("pe", (lambda e, kc=kc, cc=cc, b1=b1: e.matmul(bank(b1)[:, 0:256], lhsT=wk[:, kc, cc * 128:(cc + 1) * 128], rhs=UT[:, kc, 0:256], start=(kc == 0), stop=(kc == 7))),
                                reads=[R_w1] + rtok("UT", 0, 256), writes=[RB[b1]])
                      S.add("act", (lambda e, cc=cc, b1=b1: e.copy(out=kT1[:, cc, 0:256], in_=bank(b1)[:, 0:256])), reads=[RB[b1]], writes=rtok("qk", 0, 256))
                  for tt in range(18):
                      bk = 4 + tt % 2
                      for kc in range(8):
                          S.add("pe", (lambda e, kc=kc, tt=tt, bk=bk: e.matmul(bank(bk)[:, 0:256], lhsT=UT[:, kc, tt * 128:(tt + 1) * 128], rhs=wv[:, kc, :], start=(kc == 0), stop=(kc == 7))),
                                reads=[R_w1] + rtok("UT", tt * 128, tt * 128 + 128), writes=[RB[bk]])
                      for x in range(2):
                          S.add("act", (lambda e, tt=tt, bk=bk, x=x: e.copy(out=vaug1[:, tt, :, x * 128:x * 128 + 64],
                                                                          in_=bank(bk)[:, 0:256].rearrange("p (c x d) -> p c x d", c=2, x=2)[:, :, x, :])),
                                reads=[RB[bk]], writes=rtok("vaug", tt * 128, tt * 128 + 128))
                  dump("P1", kT1[:])
                  S.barrier()
                  cur[0] = C0
                  mlu = alloc("mlu", [128, 256], F32)
                  S.add("sp", lambda e: e.dma_start(out=mlu[:], in_=maskLU), writes=[R_const], dma=True)
                  sbm = [alloc("sbm%d" % i, [128, 512], F32) for i in range(2)]
                  PT1 = [alloc("PT1_%d" % i, [128, 5, 512], BF16) for i in range(2)]
                  rec1 = [alloc("rec1_%d" % i, [128, 512], F32) for i in range(2)]
                  R_sbm = [Res("sbm0"), Res("sbm1")]
                  R_PT1 = [[Res("PT1_%d_%d" % (i, k)) for k in range(5)] for i in range(2)]
                  R_rec1 = [Res("rec1_0"), Res("rec1_1")]
                  it = 0
                  sbr = 0
                  mi = 0
                  gq_pend = [None]
                  gq_qB = []
                  for g in range(4):
                      m, sh = g // 2, g % 2
                      p0, p1 = sh * 64, sh * 64 + 64
                      nh0, nh1 = (0, 64) if sh == 0 else (64, 128)
                      dh0, dh1 = (64, 128) if sh == 0 else (0, 64)
                      vcol = sh * 64
                      for qb in range(16):
                          s = it % 2
                          it += 1
                          tiles = []
                          if qb > 0:
                              tiles.append((NCTX + (qb - 1) * 128, 0))
                          tiles.append((NCTX + qb * 128, None))
                          if qb < 15:
                              tiles.append((NCTX + (qb + 1) * 128, 1))
                          tiles += [(0, None), (128, None)]
                          nt = len(tiles)
                          for i, (kt0, mk) in enumerate(tiles):
                              bk = sbr % 4
                              sbr += 1
                              S.add("pe", (lambda e, kt0=kt0, bk=bk, m=m, p0=p0, p1=p1, qb=qb: e.matmul(bank(bk).rearrange("p (h q) -> p h q", h=4), lhsT=kT1[p0:p1, m, kt0:kt0 + 128],
                                                                                                   rhs=qT1[p0:p1, 4 * m:4 * m + 4, qb * 128:(qb + 1) * 128], start=True, stop=True)),
                                    reads=rtok("qk", kt0, kt0 + 128) + rtok("qk", NCTX + qb * 128, NCTX + qb * 128 + 128), writes=[RB[bk]])
                              if mk is None:
                                  S.add("act", (lambda e, s=s, i=i, bk=bk: e.activation(out=PT1[s][:, i, :], in_=bank(bk), func=AF.Exp, scale=0.125)), reads=[RB[bk]], writes=[R_PT1[s][i]])
                              else:
                                  ms = mi % 2
                                  mi += 1
                                  S.add("dve", (lambda e, ms=ms, mk=mk, bk=bk: e.scalar_tensor_tensor(out=sbm[ms][:].rearrange("p (h q) -> p h q", h=4), in0=bank(bk).rearrange("p (h q) -> p h q", h=4),
                                                                                                    scalar=0.125, in1=mlu[:, mk * 128:(mk + 1) * 128].unsqueeze(1).broadcast_to([128, 4, 128]),
                                                                                                    op0=ALU.mult, op1=ALU.add)),
                                        reads=[RB[bk], R_const], writes=[R_sbm[ms]])
                                  S.add("act", (lambda e, s=s, i=i, ms=ms: e.activation(out=PT1[s][:, i, :], in_=sbm[ms][:], func=AF.Exp)), reads=[R_sbm[ms]], writes=[R_PT1[s][i]])
                          ob = 4 + s
                          q0 = NCTX + qb * 128

                          def gq_back(tiles=tiles, s=s, ob=ob, m=m, sh=sh, vcol=vcol, nt=nt, q0=q0, nh0=nh0, nh1=nh1, dh0=dh0, dh1=dh1):
                              for i, (kt0, mk) in enumerate(tiles):
                                  S.add("pe", (lambda e, i=i, kt0=kt0: e.matmul(bank(ob), lhsT=vaug1[:, kt0 // 128, m, vcol:vcol + 128], rhs=PT1[s][:, i, :],
                                                                                start=(i == 0), stop=(i == nt - 1))),
                                        reads=[R_PT1[s][i]] + rtok("vaug", kt0, kt0 + 128), writes=[RB[ob]])
                              S.add("dve", (lambda e: e.tensor_tensor(out=rec1[s][dh0:dh1, :].rearrange("p (h q) -> p h q", h=4),
                                                                      in0=bank(ob)[dh0:dh1, :].rearrange("p (h q) -> p h q", h=4),
                                                                      in1=esink[dh0:dh1, (m * 2 + sh) * 4:(m * 2 + sh) * 4 + 4].unsqueeze(2).broadcast_to([64, 4, 128]),
                                                                      op=ALU.add)),
                                    reads=[RB[ob], R_const], writes=[R_rec1[s]])
                              S.add("act", (lambda e: e.activation(out=rec1[s][dh0:dh1, :], in_=rec1[s][dh0:dh1, :], func=AF.Ln)), reads=[R_rec1[s]], writes=[R_rec1[s]])
                              S.add("act", (lambda e: e.activation(out=rec1[s][dh0:dh1, :], in_=rec1[s][dh0:dh1, :], func=AF.Exp, scale=-1.0)), reads=[R_rec1[s]], writes=[R_rec1[s]])
                              def gq_backB():
                                  S.add("dve", (lambda e: e.tensor_tensor(out=oT[nh0:nh1, 4 * m:4 * m + 4, q0:q0 + 128],
                                                                          in0=bank(ob)[nh0:nh1, :].rearrange("p (h q) -> p h q", h=4),
                                                                          in1=rec1[s][dh0:dh1, :].rearrange("p (h q) -> p h q", h=4), op=ALU.mult)),
                                        reads=[RB[ob], R_rec1[s]], writes=rtok("oT", q0, q0 + 128))
                              gq_qB.append(gq_backB)
                          if gq_qB:
                              gq_qB.pop(0)()
                          if gq_pend[0] is not None:
                              gq_pend[0]()
                          gq_pend[0] = gq_back
                  if gq_qB:
                      gq_qB.pop(0)()
                  gq_pend[0]()
                  gq_pend[0] = None
                  while gq_qB:
                      gq_qB.pop(0)()
                  dump("A1", oT[:])
                  w_out_d = w_out1
                  tok_blocks = lat256

              S.barrier()
              cur[0] = C0
              gTh = alloc("gTh", [16, NT], BF16)
              gTl = alloc("gTl", [16, NT], BF16)
              assert cur[0] == C0 + 9216
              gatesT = alloc("gatesT", [16, NT], F32, at=C0 + 9216)
              wo = alloc("wo", [128, 8, D], BF16)
              hold0_at = cur[0]
              hold = [alloc("hold%d" % i, [128, 8, 128], F32) for i in range(2)]
              pre = alloc("pre", [128, 8, 128], F32)
              prebf = alloc("prebf", [128, 8, 128], BF16)
              presq = alloc("presq", [128, 8, 128], BF16)
              t32 = alloc("t32", [128, 8, 128], F32)
              tmpo = [alloc("tmpo%d" % i, [128, 128], F32) for i in range(2)]
              mean_sb = alloc("mean_sb", [128, 128], F32)
              rstd_sb = alloc("rstd_sb", [128, 128], F32)
              lsb = alloc("lsb", [128, 18, 20], F32, at=hold0_at)
              rw = alloc("rw", [128, 18 * 96], F32, at=hold0_at + 1472)
              assert hold0_at + 1472 + 18 * 96 * 4 <= cur[0]
              R_wo = Res("wo")
              R_hold = [Res("hold0"), Res("hold1")]
              R_pre, R_t32 = Res("pre"), Res("t32")
              R_tmpo = [Res("tmpo0"), Res("tmpo1")]
              R_lt = [Res("prebf"), Res("presq"), Res("mean"), Res("rstd")]
              R_rout = Res("rout")
              S.add("pool", (lambda e, w_out_d=w_out_d: e.dma_start(out=wo[:], in_=w_out_d.rearrange("(kc p) n -> p kc n", p=128))), writes=[R_wo], dma=True)
              tiles4 = [t for (t0_, n_) in tok_blocks for t in range(t0_, t0_ + n_, 128)]
              pre2 = alloc("pre2x", [128, 8, 128], F32, at=C0)
              pre_b = [pre, pre2]
              R_pre_b = [R_pre, Res("pre2x")]
              def s4_frontPE(bi):
                  t0 = tiles4[bi]
                  s = bi % 2
                  S.add("sp", (lambda e: e.dma_start(out=hold[s][:], in_=Hs3[:, :, t0:t0 + 128])), reads=rtok("Hs", t0, t0 + 128), writes=[R_hold[s]], dma=True)
                  for oc in range(8):
                      bk = 2 * s + oc // 4
                      co = (oc % 4) * 128
                      for kc in range(8):
                          S.add("pe", (lambda e, kc=kc, oc=oc, bk=bk, co=co: e.matmul(bank(bk)[:, co:co + 128], lhsT=wo[:, kc, oc * 128:(oc + 1) * 128], rhs=oT[:, kc, t0:t0 + 128],
                                                                                    start=(kc == 0), stop=(kc == 7))),
                                reads=[R_wo] + rtok("oT", t0, t0 + 128), writes=[RB[bk]])

              def s4_frontEV(bi, l=l):
                  t0 = tiles4[bi]
                  s = bi % 2
                  mc = mcol(t0)
                  pb, rpb = pre_b[s], R_pre_b[s]
                  for oc in range(8):
                      bk = 2 * s + oc // 4
                      co = (oc % 4) * 128
                      ts = oc % 2
                      if l == 0:
                          vb = V("M2B", oc, t0)
                          S.add("act", (lambda e, oc=oc, bk=bk, co=co, ts=ts, vb=vb: e.activation(out=tmpo[ts][:], in_=bank(bk)[:, co:co + 128], func=AF.Identity,
                                                                                            bias=vb, scale=modc(0, 2, oc, mc))),
                                reads=[RB[bk], R_mod, R_vecs], writes=[R_tmpo[ts]])
                      else:
                          S.add("act", (lambda e, oc=oc, bk=bk, co=co, ts=ts: e.activation(out=tmpo[ts][:], in_=bank(bk)[:, co:co + 128], func=AF.Identity,
                                                                                     scale=modc(1, 2, oc, mc))),
                                reads=[RB[bk], R_mod], writes=[R_tmpo[ts]])
                      S.add("dve", (lambda e, oc=oc, ts=ts: e.scalar_tensor_tensor(out=pb[:, oc, :], in0=hold[s][:, oc, :], scalar=ALPHA, in1=tmpo[ts][:], op0=ALU.mult, op1=ALU.add)),
                            reads=[R_hold[s], R_tmpo[ts]], writes=[rpb])

              def s4_part1(bi):
                  s = bi % 2
                  ln_part1(pre_b[s][:], 8, 128, ones1k, prebf, presq, 4 + 2 * s, [R_pre_b[s]], R_lt)

              def s4_part2(bi, l=l):
                  t0 = tiles4[bi]
                  tt = t0 // 128
                  s = bi % 2
                  pb, rpb = pre_b[s], R_pre_b[s]
                  ln_part2(128, mean_sb, rstd_sb, 4 + 2 * s, R_lt)
                  normalize(pb[:], 8, 128, mean_sb, rstd_sb, [rpb], R_lt)
                  for ch in range(8):
                      affine(UT[:, ch, t0:t0 + 128], pb[:, ch, :], V("G4", ch, t0), V("B4", ch, t0), [rpb, R_vecs], rtok("UT", t0, t0 + 128))
                      affine(t32[:, ch, :], pb[:, ch, :], V("G4", ch, t0), V("B4", ch, t0), [rpb, R_vecs], [R_t32])
                      affine(hres[:, ch, t0:t0 + 128], pb[:, ch, :], V("GA1", ch, t0), V("BA1", ch, t0), [rpb, R_vecs], rtok("hres%d" % ch, t0, t0 + 128))
                  for kc in range(8):
                      S.add("pe", (lambda e, kc=kc: e.matmul(bank(5)[:, tt * 20:tt * 20 + 20], lhsT=t32[:, kc, :], rhs=wrt[:, l * 160 + kc * 20:l * 160 + kc * 20 + 20],
                                                             start=(kc == 0), stop=(kc == 7))),
                            reads=[R_t32, R_const], writes=[RB[5]])

              n4 = len(tiles4)
              s4_frontPE(0)
              for bi in range(n4):
                  s4_frontEV(bi)
                  s4_part1(bi)
                  if bi + 1 < n4:
                      s4_frontPE(bi + 1)
                  if bi > 0:
                      s4_part2(bi - 1)
              s4_part2(n4 - 1)

              dump("S4", hres[:])
              dump("S4u", UT[:])
              S.barrier()
              def routing_block(T0, T1, lsb=lsb, rw=rw, gatesT=gatesT, gTh=gTh, gTl=gTl, R_rout=R_rout):
                  nT = T1 - T0
                  S.add("dve", lambda e: e.tensor_copy(out=lsb[:, T0:T1, :], in_=bank(5)[:, T0 * 20:T1 * 20].rearrange("p (t n) -> p t n", n=20)), reads=[RB[5]], writes=[R_rout])

                  def rwv(i, n):
                      return rw[:, i * 18 * 4:(i * 18 * 4) + 18 * n].rearrange("p (t n) -> p t n", n=n)[:, T0:T1, :]

                  def rop(fn):
                      S.add("dve", fn, reads=[R_rout], writes=[R_rout])

                  lg = lsb[:, T0:T1, 0:4]
                  le = lsb[:, T0:T1, 4:20].rearrange("p t (g x) -> p t g x", g=4)
                  gmax, gsum, gp, m1, m2, dd, w1, w2 = [rwv(i, 1) for i in range(8)]
                  gsh, gmask, elsel, mask1, el2, mask2, within, wa_ = [rwv(8 + i, 4) for i in range(8)]
                  t44 = rw[:, 18 * 64:18 * 80].rearrange("p (t g x) -> p t g x", g=4, x=4)[:, T0:T1]
                  gates = rw[:, 18 * 80:18 * 96].rearrange("p (t g x) -> p t g x", g=4, x=4)
                  bc4 = lambda a: a.broadcast_to([128, nT, 4])
                  rop(lambda e: e.tensor_reduce(out=gmax, in_=lg, axis=AX.X, op=ALU.max))
                  rop(lambda e: e.tensor_tensor(out=gsh, in0=lg, in1=bc4(gmax), op=ALU.subtract))
                  rop(lambda e: e.tensor_tensor(out=gmask, in0=lg, in1=bc4(gmax), op=ALU.is_equal))
                  S.add("act", lambda e: e.activation(out=gsh, in_=gsh, func=AF.Exp), reads=[R_rout], writes=[R_rout])
                  rop(lambda e: e.tensor_reduce(out=gsum, in_=gsh, axis=AX.X, op=ALU.add))
                  rop(lambda e: e.reciprocal(out=gp, in_=gsum))
                  rop(lambda e: e.tensor_tensor(out=t44, in0=le, in1=gmask.unsqueeze(3).broadcast_to([128, nT, 4, 4]), op=ALU.mult))
                  rop(lambda e: e.tensor_reduce(out=elsel, in_=t44.rearrange("p t g x -> p t x g"), axis=AX.X, op=ALU.add))
                  rop(lambda e: e.tensor_reduce(out=m1, in_=elsel, axis=AX.X, op=ALU.max))
                  rop(lambda e: e.tensor_tensor(out=mask1, in0=elsel, in1=bc4(m1), op=ALU.is_equal))
                  rop(lambda e: e.scalar_tensor_tensor(out=el2, in0=mask1, scalar=NEG, in1=elsel, op0=ALU.mult, op1=ALU.add))
                  rop(lambda e: e.tensor_reduce(out=m2, in_=el2, axis=AX.X, op=ALU.max))
                  rop(lambda e: e.tensor_tensor(out=mask2, in0=el2, in1=bc4(m2), op=ALU.is_equal))
                  rop(lambda e: e.tensor_tensor(out=dd, in0=m2, in1=m1, op=ALU.subtract))
                  S.add("act", lambda e: e.activation(out=dd, in_=dd, func=AF.Exp), reads=[R_rout], writes=[R_rout])
                  rop(lambda e: e.tensor_scalar_add(out=w1, in0=dd, scalar1=1.0))
                  rop(lambda e: e.reciprocal(out=w1, in_=w1))
                  rop(lambda e: e.tensor_tensor(out=w1, in0=w1, in1=gp, op=ALU.mult))
                  rop(lambda e: e.tensor_tensor(out=w2, in0=dd, in1=w1, op=ALU.mult))
                  rop(lambda e: e.tensor_tensor(out=within, in0=mask1, in1=bc4(w1), op=ALU.mult))
                  rop(lambda e: e.tensor_tensor(out=wa_, in0=mask2, in1=bc4(w2), op=ALU.mult))
                  rop(lambda e: e.tensor_tensor(out=within, in0=within, in1=wa_, op=ALU.add))
                  rop(lambda e: e.tensor_tensor(out=gates[:, T0:T1], in0=gmask.unsqueeze(3).broadcast_to([128, nT, 4, 4]), in1=within.unsqueeze(2).broadcast_to([128, nT, 4, 4]), op=ALU.mult))
                  for tt in range(T0, T1):
                      bk = 6 + (tt // 4) % 2
                      S.add("pe", (lambda e, tt=tt, bk=bk: e.transpose(bank(bk)[0:16, (tt % 4) * 128:(tt % 4 + 1) * 128], gates[:, tt].rearrange("p g x -> p (g x)"), id32[:])),
                            reads=[R_rout, R_const], writes=[RB[bk]])
                      if tt % 4 == 3 or tt == T1 - 1:
                          ta = (tt // 4) * 4
                          ta0 = max(ta, T0)
                          S.add("act", (lambda e, bk=bk, ta=ta, ta0=ta0, tt=tt: e.copy(out=gatesT[0:16, ta0 * 128:(tt + 1) * 128], in_=bank(bk)[0:16, (ta0 - ta) * 128:(tt + 1 - ta) * 128])),
                                reads=[RB[bk]], writes=[R_rout])

                  g0, g1 = T0 * 128, T1 * 128
                  S.add("act", (lambda e, g0=g0, g1=g1: e.copy(out=gTh[0:16, g0:g1], in_=gatesT[0:16, g0:g1])), reads=[R_rout], writes=[R_rout])
                  S.add("dve", (lambda e, g0=g0, g1=g1: e.tensor_tensor(out=gTl[0:16, g0:g1], in0=gatesT[0:16, g0:g1], in1=gTh[0:16, g0:g1], op=ALU.subtract)), reads=[R_rout], writes=[R_rout])
              routing_block(tok_blocks[0][0] // 128, 18)
              S.barrier()
              cur[0] = C0
              gTh = alloc("gTh", [16, NT], BF16)
              gTl = alloc("gTl", [16, NT], BF16)
              wgs = [alloc("wgs%d" % i, [128, 8, 256], BF16) for i in range(2)]
              wus = [alloc("wus%d" % i, [128, 8, 256], BF16) for i in range(2)]
              wds = [alloc("wds%d" % i, [128, 2, D], BF16) for i in range(2)]
              sgm = [alloc("sgm0", [128, 2, 512], F32)]
              hgm = [alloc("hgm%d" % i, [128, 2, 512], BF16) for i in range(4)]
              o_ = OT0
              for i in range(2, 4):
                  wgs.append(alloc("wgs%d" % i, [128, 8, 256], BF16, at=o_)); o_ += 4096
                  wus.append(alloc("wus%d" % i, [128, 8, 256], BF16, at=o_)); o_ += 4096
                  wds.append(alloc("wds%d" % i, [128, 2, D], BF16, at=o_)); o_ += 4096
              sgm.append(alloc("sgm1", [128, 2, 512], F32, at=o_)); o_ += 4096
              gsb = []
              for i in range(2):
                  gsb.append(alloc("gsb%d" % i, [128, 512], F32, at=o_)); o_ += 2048
              assert o_ <= OT0 + 36864
              R_ewg = [Res("ewg%d" % i) for i in range(4)]
              R_ewu = [Res("ewu%d" % i) for i in range(4)]
              R_ewd = [Res("ewd%d" % i) for i in range(4)]
              R_sgm = [Res("sgm0"), Res("sgm1")]
              R_hgm = [Res("hgm%d" % i) for i in range(4)]
              R_gsb = [Res("gsb0"), Res("gsb1")]
              mblocks = blocks512 if l == 0 else lat512

              def load_expert(ex, slot, l=l):
                  S.add("pool", (lambda e: e.dma_start(out=wgs[slot][:], in_=ewg[l, ex].rearrange("(kc p) f -> p kc f", p=128))), writes=[R_ewg[slot]], dma=True)
                  S.add("pool", (lambda e: e.dma_start(out=wus[slot][:], in_=ewu[l, ex].rearrange("(kc p) f -> p kc f", p=128))), writes=[R_ewu[slot]], dma=True)
                  S.add("pool", (lambda e: e.dma_start(out=wds[slot][:], in_=ewd[l, ex].rearrange("(kc p) f -> p kc f", p=128))), writes=[R_ewd[slot]], dma=True)

              def emit_front(ex, slot, t0, N, si):
                  S.add("pe", (lambda e: e.matmul(bank(4)[:, 0:N], lhsT=selt[0:16, ex * 128:(ex + 1) * 128], rhs=gTh[0:16, t0:t0 + N], start=True, stop=False)),
                        reads=[R_rout, R_const], writes=[RB[4]])
                  S.add("pe", (lambda e: e.matmul(bank(4)[:, 0:N], lhsT=selt[0:16, ex * 128:(ex + 1) * 128], rhs=gTl[0:16, t0:t0 + N], start=False, stop=True)),
                        reads=[R_rout, R_const], writes=[RB[4]])
                  S.add("act", (lambda e: e.copy(out=gsb[si][:, 0:N], in_=bank(4)[:, 0:N])), reads=[RB[4]], writes=[R_gsb[si]])
                  for oc in range(4):
                      wsrc = wgs[slot] if oc < 2 else wus[slot]
                      rw_ = R_ewg[slot] if oc < 2 else R_ewu[slot]
                      for kc in range(8):
                          S.add("pe", (lambda e, kc=kc, oc=oc, wsrc=wsrc: e.matmul(bank(oc)[:, 0:N], lhsT=wsrc[:, kc, (oc % 2) * 128:(oc % 2 + 1) * 128], rhs=UT[:, kc, t0:t0 + N],
                                                                                    start=(kc == 0), stop=(kc == 7))),
                                reads=[rw_] + rtok("UT", t0, t0 + N), writes=[RB[oc]])
                  S.add("act", (lambda e: e.activation(out=sgm[si][:, :, 0:N], in_=PS[0][:].rearrange("p (j n) -> p j n", j=2)[:, :, 0:N], func=AF.Silu)),
                        reads=[RB[0], RB[1]], writes=[R_sgm[si]])
                  S.add("dve", (lambda e: e.tensor_tensor(out=sgm[si][:, :, 0:N], in0=sgm[si][:, :, 0:N], in1=PS[1][:].rearrange("p (j n) -> p j n", j=2)[:, :, 0:N], op=ALU.mult)),
                        reads=[RB[2], RB[3], R_sgm[si]], writes=[R_sgm[si]])

              def emit_gate(si, q, N):
                  S.add("dve", (lambda e: e.tensor_tensor(out=hgm[q][:, :, 0:N], in0=sgm[si][:, :, 0:N], in1=gsb[si][:, 0:N].unsqueeze(1).broadcast_to([128, 2, N]), op=ALU.mult)),
                        reads=[R_gsb[si], R_sgm[si]], writes=[R_hgm[q]])

              ybc = [0]

              def make_yhalf(half, slots, qs, t0, N, mc, l=l):
                  def f():
                      for dc in range(half * 4, half * 4 + 4):
                          bk = 5 + ybc[0] % 3
                          ybc[0] += 1
                          for j in range(2):
                              for k2 in range(2):
                                  S.add("pe", (lambda e, j=j, k2=k2, dc=dc, bk=bk: e.matmul(bank(bk)[:, 0:N], lhsT=wds[slots[j]][:, k2, dc * 128:(dc + 1) * 128], rhs=hgm[qs[j]][:, k2, 0:N],
                                                                                           start=(j == 0 and k2 == 0), stop=(j == 1 and k2 == 1))),
                                        reads=[R_ewd[slots[j]], R_hgm[qs[j]]], writes=[RB[bk]])
                          S.add("dve", (lambda e, dc=dc, bk=bk: e.scalar_tensor_tensor(out=hres[:, dc, t0:t0 + N], in0=bank(bk)[:, 0:N], scalar=modc(l, 5, dc, mc),
                                                                                      in1=hres[:, dc, t0:t0 + N], op0=ALU.mult, op1=ALU.add)),
                                reads=[RB[bk], R_mod] + rtok("hres%d" % dc, t0, t0 + N), writes=rtok("hres%d" % dc, t0, t0 + N))
                  return f

              load_expert(0, 0)
              load_expert(1, 1)
              pendA = pendB = None
              it = 0
              for pr in range(8):
                  slots = (2 * (pr % 2), 2 * (pr % 2) + 1)
                  for bi, (t0, N) in enumerate(mblocks):
                      qs = ((it % 2) * 2, (it % 2) * 2 + 1)
                      it += 1
                      mc = mcol(t0)
                      emit_front(2 * pr, slots[0], t0, N, 0)
                      if pendA is not None:
                          pendA()
                      emit_gate(0, qs[0], N)
                      emit_front(2 * pr + 1, slots[1], t0, N, 1)
                      if pendB is not None:
                          pendB()
                      emit_gate(1, qs[1], N)
                      pendA = make_yhalf(0, slots, qs, t0, N, mc)
                      pendB = make_yhalf(1, slots, qs, t0, N, mc)
                      if bi == 0 and pr + 1 < 8:
                          nslots = (2 * ((pr + 1) % 2), 2 * ((pr + 1) % 2) + 1)
                          load_expert(2 * pr + 2, nslots[0])
                          load_expert(2 * pr + 3, nslots[1])
              pendA()
              pendB()

              dump("S5", hres[:])
              S.barrier()
              cur[0] = C0
              pre2 = alloc("pre2", [128, 8, 256], BF16)
              presq2 = alloc("presq2", [128, 8, 256], BF16)
              mean2 = alloc("mean2", [128, 256], F32)
              rstd2 = alloc("rstd2", [128, 256], F32)
              otile = [alloc("otile%d" % i, [128, D], F32) for i in range(2)]
              R_l2 = [Res("pre2"), Res("presq2"), Res("mean2"), Res("rstd2")]
              R_ot = [Res("ot0"), Res("ot1")]
              oi = 0
              def s6_part1(bi):
                  t0_ = tok_blocks[bi][0]
                  rh_ = [r_ for ch_ in range(8) for r_ in rtok("hres%d" % ch_, t0_, t0_ + 256)]
                  ln_part1(hres[:, :, t0_:t0_ + 256], 8, 256, ones1k, pre2, presq2, 4 + bi % 2, rh_, R_l2)

              s6_part1(0)
              for bi, (t0, N) in enumerate(tok_blocks):
                  hap = hres[:, :, t0:t0 + 256]
                  rh = [r_ for ch_ in range(8) for r_ in rtok("hres%d" % ch_, t0, t0 + 256)]
                  if bi + 1 < len(tok_blocks):
                      s6_part1(bi + 1)
                  ln_part2(256, mean2, rstd2, 4 + bi % 2, R_l2)
                  normalize(hap, 8, 256, mean2, rstd2, rh, R_l2)
                  if l == 0:
                      for ch in range(8):
                          affine(UT[:, ch, t0:t0 + 256], hres[:, ch, t0:t0 + 256], V("GU", ch, t0), V("BU", ch, t0), rh + [R_vecs], rtok("UT", t0, t0 + 256))
                      for ch in range(8):
                          affine(hres[:, ch, t0:t0 + 256], hres[:, ch, t0:t0 + 256], smc("ln_g", 8 + ch), smc("ln_b", 8 + ch), rh + [R_const], rh)
                      S.add("sp", (lambda e, t0=t0: e.dma_start(out=Hs3[:, :, t0:t0 + 256], in_=hres[:, :, t0:t0 + 256])), reads=rh, writes=rtok("Hs", t0, t0 + 256), dma=True)
                      if debug and nlayers == 1:
                          S.add("sp", (lambda e, t0=t0: e.dma_start(out=dbg.rearrange("p (c t) -> p c t", c=8)[:, :, t0:t0 + 256], in_=hres[:, :, t0:t0 + 256])), reads=rh,
                                writes=[Res("dbgo")], dma=True)
                  else:
                      for ch in range(8):
                          affine(hres[:, ch, t0:t0 + 256], hres[:, ch, t0:t0 + 256], smc("ln_g", 24 + ch), smc("ln_b", 24 + ch), rh + [R_const], rh)
                      for hh in range(2):
                          tk = t0 + hh * 128
                          so = oi % 2
                          oi += 1
                          for ch in range(8):
                              bk = so * 2 + ch // 4
                              S.add("pe", (lambda e, ch=ch, bk=bk, tk=tk: e.transpose(bank(bk)[:, (ch % 4) * 128:(ch % 4 + 1) * 128], hres[:, ch, tk:tk + 128], id32[:])),
                                    reads=rh + [R_const], writes=[RB[bk]])
                          for hf in range(2):
                              bk = so * 2 + hf
                              S.add("act" if hf else "dve", (lambda e, so=so, hf=hf, bk=bk: (e.copy if hf else e.tensor_copy)(out=otile[so][:, hf * 512:(hf + 1) * 512], in_=bank(bk))),
                                    reads=[RB[bk]], writes=[R_ot[so]])
                          S.add("sp", (lambda e, so=so, tk=tk, b=b: e.dma_start(out=outd[b, tk - NCTX:tk - NCTX + 128, :], in_=otile[so][:])), reads=[R_ot[so]], writes=[Res("outw")], dma=True)
    except _Stop:
        pass
    S.barrier()

    with nc.Block() as block:
        @block.tensor
        def _(e):
            S.emit_one("pe", e, esem, dsems)

        @block.scalar
        def _(e):
            S.emit_one("act", e, esem, dsems)

        @block.vector
        def _(e):
            S.emit_one("dve", e, esem, dsems)

        @block.gpsimd
        def _(e):
            S.emit_one("pool", e, esem, dsems)

        @block.sync
        def _(e):
            S.emit_one("sp", e, esem, dsems)
    es.close()
    return nc


def _prep_shared(inp):
    f = lambda a: np.ascontiguousarray(np.asarray(a, np.float32))
    sm = np.zeros((128, SMN), np.float32)

    def put(name, arr):
        arr = np.asarray(arr, np.float32)
        sm[:, SMO[name]:SMO[name] + arr.shape[1]] = arr

    put("ada_b0", _fm(inp["ada_b"][0]))
    put("ada_b1", _fm(inp["ada_b"][1]))
    put("ln_g", np.concatenate([_fm(inp["ln_g"][l, k]) for l in range(2) for k in range(2)], axis=1))
    put("ln_b", np.concatenate([_fm(inp["ln_b"][l, k]) for l in range(2) for k in range(2)], axis=1))
    b_in = np.asarray(inp["ab_b_in"][0], np.float32)
    put("b_in", _fm(b_in[:2048]))
    cw = np.asarray(inp["conv_w"][0], np.float32)
    put("conv_w", np.ascontiguousarray(cw.T.reshape(4, 128, 31).transpose(1, 0, 2).reshape(128, 124)))
    put("conv_b", _fm(inp["conv_b"][0]))
    put("cln_g", _fm(inp["conv_ln_g"][0]))
    put("cln_b", _fm(inp["conv_ln_b"][0]))
    put("b_out", _fm(inp["ab_b_out"][0]))
    sm[:, SMO["eps"]] = EPS
    qidx = _gqa_qidx()
    gw = np.asarray(inp["gqa_w_in"][0], np.float32)
    wq = gw[:, :1024]
    wkk = gw[:, 1024:1280]
    wvv = gw[:, 1280:1536]
    C, Sg = _rope_tables()
    kk = np.arange(128)[:, None]
    qq = np.arange(128)[None, :]
    maskL = np.where(kk >= qq, 0.0, NEG).astype(np.float32)
    maskU = np.where(kk <= qq, 0.0, NEG).astype(np.float32)
    sink = np.asarray(inp["gqa_sink"][0], np.float32)
    sperm = np.array([8 * m + 4 * sh + j for m in range(2) for sh in range(2) for j in range(4)])
    sel = np.zeros((16, 16, 128), np.float32)
    for ex in range(16):
        sel[ex, ex, :] = 1.0
    wr = np.stack([np.concatenate([np.asarray(inp["router_group"][l], np.float32), np.asarray(inp["router_expert"][l], np.float32)], axis=1)
                   .reshape(8, 128, 20).transpose(1, 0, 2).reshape(128, 160) for l in range(2)])
    bv = b_in[2048:2560]
    shared = {
        "ada_w": f(inp["ada_w"]),
        "sm": sm,
        "w_in0": f(inp["ab_w_in"][0]),
        "bvbc": np.ascontiguousarray(np.broadcast_to(bv[None, :], (128, 512))),
        "nab": np.ascontiguousarray(_na_bias_table(np.asarray(inp["na_rpb"][0], np.float32)).reshape(8, 128, 21 * 128)),
        "w_out0": f(inp["ab_w_out"][0]),
        "wq1": f(wq[:, qidx]),
        "wqs1": f(wq[:, qidx][:, _swap64(1024)]),
        "wk1": f(wkk),
        "wks1": f(wkk[:, _swap64(256)]),
        "wv1": f(wvv),
        "w_out1": f(np.asarray(inp["gqa_w_out"][0], np.float32)[qidx, :]),
        "ropeC": C,
        "ropeS": Sg,
        "maskLU": np.ascontiguousarray(np.concatenate([maskL, maskU], axis=1)),
        "sinkbc": np.ascontiguousarray(np.broadcast_to(sink[sperm][None, :], (128, 16))),
        "wr": f(wr),
        "sel": np.ascontiguousarray(sel.reshape(16, 2048)),
        "ident": np.eye(128, dtype=np.float32),
        "ewg": f(inp["exp_w_gate"]),
        "ewu": f(inp["exp_w_up"]),
        "ewd": f(inp["exp_w_down"]),
    }
    return shared


def _core_inputs(inp, shared, i):
    x = np.asarray(inp["x"], np.float32)
    ctx = np.asarray(inp["ctx"], np.float32)
    c = np.asarray(inp["c"], np.float32)
    cc = np.stack([c[2 * i], c[2 * i + 1], np.asarray(inp["c_ctx"], np.float32)])
    cvec = np.ascontiguousarray(cc.reshape(3, 8, 128).transpose(2, 1, 0).reshape(128, 24))
    m = dict(shared)
    m["x2"] = np.ascontiguousarray(x[2 * i:2 * i + 2])
    m["ctx2"] = np.ascontiguousarray(ctx[2 * i:2 * i + 2])
    m["cvec"] = cvec
    return m


_NC_CACHE = {}


def kernel(**inputs):
    n = 8
    if "nc" not in _NC_CACHE:
        _NC_CACHE["nc"] = build()
    nc = _NC_CACHE["nc"]
    shared = _prep_shared(inputs)
    in_maps = [_core_inputs(inputs, shared, i) for i in range(n)]
    res = run_bass_kernel_spmd(nc, in_maps, core_ids=list(range(n)))
    out = np.concatenate([np.asarray(r["out"], np.float32) for r in res.results], axis=0)
    return out
```

```python
import numpy as np
from contextlib import ExitStack
import concourse.bass as bass
import concourse.mybir as mybir
from concourse.bass_utils import run_bass_kernel_spmd

F32 = mybir.dt.float32
BF16 = mybir.dt.bfloat16
AF = mybir.ActivationFunctionType
ALU = mybir.AluOpType
AX = mybir.AxisListType

D = 1024
SEQ = 2048
NCTX = 256
NT = SEQ + NCTX
GW = 64
ALPHA = 4.0 ** 0.25
EPS = 1e-5
NEG = -1e30

ENGS = ("pe", "act", "dve", "pool", "sp")
N_DMA_SEMS = 40


class Res:
    __slots__ = ("name", "last_w", "readers")

    def __init__(self, name):
        self.name = name
        self.last_w = None
        self.readers = []


class Op:
    __slots__ = ("eng", "fn", "idx", "deps", "dma", "sig", "semval", "dsem", "dval", "dprev", "seq")

    def __init__(self, eng, fn, idx, dma):
        self.eng = eng
        self.fn = fn
        self.idx = idx
        self.deps = []
        self.dma = dma
        self.sig = False
        self.semval = 0
        self.dsem = -1
        self.dval = 0
        self.dprev = 0


class Sched:
    def __init__(self):
        self.ops = {e: [] for e in ENGS}
        self.ndma = 0
        self.nseq = 0
        self.dma_tot = [0] * N_DMA_SEMS
        self.last_dma = [None] * N_DMA_SEMS
        self._assigned = False

    def add(self, eng, fn, reads=(), writes=(), dma=False, extra=()):
        lst = self.ops[eng]
        op = Op(eng, fn, len(lst), dma)
        op.seq = self.nseq
        self.nseq += 1
        deps = {}
        for r in reads:
            if r.last_w is not None:
                deps[id(r.last_w)] = r.last_w
        for w in writes:
            if w.last_w is not None:
                deps[id(w.last_w)] = w.last_w
            for rd in w.readers:
                deps[id(rd)] = rd
        for x in extra:
            deps[id(x)] = x
        for r in reads:
            r.readers.append(op)
        for w in writes:
            w.last_w = op
            w.readers = []
        if dma:
            s = self.ndma % N_DMA_SEMS
            self.ndma += 1
            op.dsem = s
            op.dprev = self.dma_tot[s]
            self.dma_tot[s] += 16
            op.dval = self.dma_tot[s]
            self.last_dma[s] = op
        for d in deps.values():
            if d is op:
                continue
            if d.eng == eng and not d.dma and not dma:
                if eng == "pe":
                    continue
                if op.idx - d.idx > 2:
                    continue
            op.deps.append(d)
            if not d.dma:
                d.sig = True
        lst.append(op)
        return op

    def mark(self):
        return self.nseq

    def interleave(self, m0, m1, m2):
        assert m2 == self.nseq
        na, nb_ = m1 - m0, m2 - m1
        if na == 0 or nb_ == 0:
            return
        newseq = {}
        ia = ib = 0
        k = m0
        while ia < na or ib < nb_:
            if ib >= nb_ or (ia < na and ia * nb_ <= ib * na):
                newseq[m0 + ia] = k
                ia += 1
            else:
                newseq[m1 + ib] = k
                ib += 1
            k += 1
        for e in ENGS:
            lst = self.ops[e]
            j0 = len(lst)
            while j0 > 0 and lst[j0 - 1].seq >= m0:
                j0 -= 1
            seg = lst[j0:]
            for op in seg:
                op.seq = newseq[op.seq]
            seg.sort(key=lambda o: o.seq)
            lst[j0:] = seg
            for i, op in enumerate(lst[j0:], start=j0):
                op.idx = i

    def barrier(self):
        lasts = []
        for e in ENGS:
            for op in reversed(self.ops[e]):
                if not op.dma:
                    lasts.append(op)
                    break
        dl = [o for o in self.last_dma if o is not None]
        for e in ENGS:
            self.add(e, lambda eng: eng.nop(), extra=[o for o in lasts if o.eng != e] + dl)

    def emit_one(self, e, eng, esem, dsems):
        if not self._assigned:
            for ee in ENGS:
                c = 0
                for op in self.ops[ee]:
                    if op.sig and not op.dma:
                        c += 1
                        op.semval = c
            self._assigned = True
        seen = {}
        for op in self.ops[e]:
            need = {}
            for d in op.deps:
                if d.dma:
                    key = ("d", d.dsem)
                    val = d.dval
                else:
                    key = ("e", d.eng)
                    val = d.semval
                if val > need.get(key, 0):
                    need[key] = val
            if op.dma and op.dprev > 0:
                key = ("d", op.dsem)
                if op.dprev > need.get(key, 0):
                    need[key] = op.dprev
            for key, val in need.items():
                if seen.get(key, 0) >= val:
                    continue
                seen[key] = val
                sem = dsems[key[1]] if key[0] == "d" else esem[key[1]]
                eng.wait_ge(sem, val)
            ins = op.fn(eng)
            if op.dma:
                ins.then_inc(dsems[op.dsem], 16)
            elif op.sig:
                ins.then_inc(esem[e], 1)


def _sm_layout():
    off = {}
    n = 0
    for name, cols in (("ada_b0", 48), ("ada_b1", 48), ("ln_g", 32), ("ln_b", 32), ("b_in", 16),
                       ("conv_w", 124), ("conv_b", 4), ("cln_g", 4), ("cln_b", 4), ("b_out", 8), ("eps", 1)):
        off[name] = n
        n += cols
    return off, n


SMO, SMN = _sm_layout()


def _fm(v):
    v = np.asarray(v, np.float32)
    return np.ascontiguousarray(v.reshape(-1, 128).T)


def _gqa_qidx():
    idx = np.zeros(1024, np.int64)
    for c in range(8):
        m, j = divmod(c, 4)
        h0 = 8 * m + j
        h1 = 8 * m + 4 + j
        idx[c * 128:c * 128 + 64] = h0 * 64 + np.arange(64)
        idx[c * 128 + 64:c * 128 + 128] = h1 * 64 + np.arange(64)
    return idx


def _swap64(n):
    d = np.arange(n)
    dd = d % 64
    sw = np.where(dd % 32 < 16, dd + 16, dd - 16)
    return (d // 64) * 64 + sw


def _na_tiles(j):
    if j in (0, 1):
        return [0, 1, 2, 3]
    if j in (14, 15):
        return [12, 13, 14, 15]
    return [j - 2, j - 1, j, j + 1, j + 2]


def _na_tile_index(j):
    if j == 0:
        return 5
    if j == 1:
        return 9
    if j == 14:
        return 13
    if j == 15:
        return 17
    return 0


def _na_bias_table(rpb):
    rows = 32
    r = np.arange(rows)
    row_start = np.clip(r - 4, 0, rows - 8)
    jj = np.arange(GW)
    col_start = np.clip(jj - 8, 0, GW - 16)
    col_in = (jj[None, :] >= col_start[:, None]) & (jj[None, :] < col_start[:, None] + 16)
    col_off = np.clip(jj[None, :] - jj[:, None], -15, 15) + 15
    out = np.full((8, 21, 128, 128), NEG, np.float32)

    def tile(j, a):
        t = np.full((8, 128, 128), NEG, np.float32)
        for pk in range(2):
            rk = 2 * a + pk
            for pq in range(2):
                rq = 2 * j + pq
                if not (row_start[rq] <= rk < row_start[rq] + 8):
                    continue
                ro = rk - rq + 7
                blk = rpb[:, ro][:, col_off]
                blk = np.where(col_in[None], blk, np.float32(NEG))
                t[:, pk * 64:(pk + 1) * 64, pq * 64:(pq + 1) * 64] = blk.transpose(0, 2, 1)
        return t

    for i, a in enumerate(_na_tiles(5)):
        out[:, i] = tile(5, a)
    for j in (0, 1, 14, 15):
        base = _na_tile_index(j)
        for i, a in enumerate(_na_tiles(j)):
            out[:, base + i] = tile(j, a)
    return np.ascontiguousarray(out.transpose(0, 2, 1, 3))


def _rope_tables():
    t = np.arange(SEQ)
    row = (t // GW).astype(np.float32)
    col = (t % GW).astype(np.float32)
    inv = (np.float32(10000.0) ** (-np.arange(0, 32, 2, dtype=np.float32) / np.float32(32))).astype(np.float32)
    ang = np.concatenate([row[:, None] * inv, col[:, None] * inv], axis=-1).astype(np.float32)
    cos = np.cos(ang).astype(np.float32)
    sin = np.sin(ang).astype(np.float32)
    p = np.arange(128)
    d = p % 64
    ai = (d // 32) * 16 + d % 16
    sgn = np.where(d % 32 < 16, -1.0, 1.0).astype(np.float32)
    C = np.ascontiguousarray(cos[:, ai].T)
    S = np.ascontiguousarray((sin[:, ai] * sgn[None, :]).T)
    return C.astype(np.float32), S.astype(np.float32)


class _Stop(Exception):
    pass


def build(nlayers=2, nb=2, debug=False, stop=None):
    nc = bass.Bass("TRN2", target_bir_lowering=False)
    S = Sched()

    def din(name, shape):
        return nc.dram_tensor(name, list(shape), F32, kind="ExternalInput").ap()

    x2 = din("x2", [2, SEQ, D])
    ctx2 = din("ctx2", [2, NCTX, D])
    cvec = din("cvec", [128, 24])
    ada_w = din("ada_w", [2, D, 6 * D])
    smd = din("sm", [128, SMN])
    w_in0 = din("w_in0", [D, 2560])
    bvbc = din("bvbc", [128, 512])
    nab = din("nab", [8, 128, 21 * 128])
    w_out0 = din("w_out0", [D, D])
    wq1 = din("wq1", [D, 1024])
    wqs1 = din("wqs1", [D, 1024])
    wk1 = din("wk1", [D, 256])
    wks1 = din("wks1", [D, 256])
    wv1 = din("wv1", [D, 256])
    w_out1 = din("w_out1", [D, D])
    ropeC = din("ropeC", [128, SEQ])
    ropeS = din("ropeS", [128, SEQ])
    maskLU = din("maskLU", [128, 256])
    sinkbc = din("sinkbc", [128, 16])
    wr = din("wr", [2, 128, 160])
    sel = din("sel", [16, 2048])
    ident = din("ident", [128, 128])
    ewg = din("ewg", [2, 16, D, 256])
    ewu = din("ewu", [2, 16, D, 256])
    ewd = din("ewd", [2, 16, 256, D])
    outd = nc.dram_tensor("out", [2, SEQ, D], F32, kind="ExternalOutput").ap()
    Hs = nc.dram_tensor("Hs", [128, 8 * NT], F32, kind="Internal").ap()
    dbg = nc.dram_tensor("dbg", [128, 8 * NT], F32, kind="ExternalOutput").ap() if debug else None
    dbgb = nc.dram_tensor("dbgb", [128, 8 * NT], BF16, kind="ExternalOutput").ap() if debug else None
    Hs3 = Hs.rearrange("p (c t) -> p c t", c=8)

    es = ExitStack()
    cur = [16640]

    acache = {}

    def alloc(name, shape, dt, at=None):
        nbytes = int(np.prod(shape[1:])) * (4 if dt == F32 else 2)
        if at is None:
            at = cur[0]
            cur[0] = (at + nbytes + 63) // 64 * 64
        assert at + nbytes <= 229376, (name, at, nbytes)
        key = (name, at, tuple(shape))
        if key not in acache:
            acache[key] = nc.alloc_sbuf_tensor_at("%s_%d" % (name, len(acache)), list(shape), dt, offset=at)
        return acache[key]

    sm = alloc("sm", [128, SMN], F32)
    id32 = alloc("id32", [128, 128], F32)
    ones1k = alloc("ones1k", [128, 128], BF16)
    ones512 = alloc("ones512", [128, 128], BF16)
    csil = alloc("csil", [128, 24], BF16)
    cv32 = alloc("cv32", [128, 24], F32)
    mod = alloc("mod", [128, 2 * 144], F32)
    mp1 = alloc("mp1", [128, 2 * 144], F32)
    vecs = alloc("vecs", [128, 128], F32)
    selt = alloc("selt", [16, 2048], BF16)
    wrt = alloc("wrt", [128, 320], F32)
    esink = alloc("esink", [128, 16], F32)
    R_const = Res("const")
    R_mod = Res("mod")
    R_vecs = Res("vecs")
    base0 = cur[0]

    PS = [es.enter_context(nc.psum_tensor("ps%d" % i, [128, 1024], F32)) for i in range(4)]
    RB = [Res("bank%d" % i) for i in range(8)]

    def bank(k):
        return PS[k // 2][:, (k % 2) * 512:(k % 2) * 512 + 512]

    esem = {e: es.enter_context(nc.semaphore("es_" + e)) for e in ENGS}
    dsems = [es.enter_context(nc.semaphore("ds%d" % i)) for i in range(N_DMA_SEMS)]

    def smc(name, j, n=1):
        o = SMO[name] + j
        return sm[:, o:o + n]

    def modc(l, k, ch, col):
        o = l * 144 + (k * 8 + ch) * 3 + col
        return mod[:, o:o + 1]

    def mp1c(l, k, ch, col):
        o = l * 144 + (k * 8 + ch) * 3 + col
        return mp1[:, o:o + 1]

    VK = {}

    def vslot(kind, ch):
        key = (kind, ch)
        if key not in VK:
            VK[key] = len(VK)
            assert len(VK) <= 128
        o = VK[key]
        return vecs[:, o:o + 1]

    S.add("sp", lambda e: e.dma_start(out=sm[:], in_=smd), writes=[R_const], dma=True)
    S.add("sp", lambda e: e.dma_start(out=id32[:], in_=ident), writes=[R_const], dma=True)
    S.add("sp", lambda e: e.dma_start(out=cv32[:], in_=cvec), writes=[R_const], dma=True)
    S.add("pool", lambda e: e.dma_start(out=selt[:], in_=sel), writes=[R_const], dma=True)
    S.add("sp", lambda e: e.dma_start(out=wrt[:].rearrange("p (l n) -> p l n", l=2), in_=wr.rearrange("l p n -> p l n")), writes=[R_const], dma=True)
    S.add("sp", lambda e: e.dma_start(out=esink[:], in_=sinkbc), writes=[R_const], dma=True)
    S.add("pool", lambda e: e.memset(ones1k[:], 1.0 / 1024.0), writes=[R_const])
    S.add("pool", lambda e: e.memset(ones512[:], 1.0 / 512.0), writes=[R_const])
    S.add("act", lambda e: e.activation(out=csil[:], in_=cv32[:], func=AF.Silu), reads=[R_const], writes=[R_const])
    S.add("act", lambda e: e.activation(out=esink[:], in_=esink[:], func=AF.Exp), reads=[R_const], writes=[R_const])

    adaw = [alloc("adaw%d" % i, [128, 8, 1024], BF16) for i in range(2)]
    R_adaw = [Res("adaw0"), Res("adaw1")]
    pi = 0
    for l in range(nlayers):
        awl = ada_w[l].rearrange("(kc p) n -> p kc n", p=128)
        for piece in range(6):
            s = pi % 2
            pi += 1
            S.add("pool", (lambda e, s=s, awl=awl, piece=piece: e.dma_start(out=adaw[s][:], in_=awl[:, :, piece * 1024:(piece + 1) * 1024])),
                  writes=[R_adaw[s]], dma=True)
            for oc8 in range(8):
                oc = piece * 8 + oc8
                for kc in range(8):
                    S.add("pe", (lambda e, s=s, oc=oc, oc8=oc8, kc=kc: e.matmul(bank(0)[:, oc * 3:oc * 3 + 3], lhsT=adaw[s][:, kc, oc8 * 128:(oc8 + 1) * 128],
                                                                                    rhs=csil[:, kc * 3:kc * 3 + 3], start=(kc == 0), stop=(kc == 7))),
                          reads=[R_adaw[s], R_const], writes=[RB[0]])
        ab = smc("ada_b%d" % l, 0, 48)
        S.add("dve", (lambda e, l=l, ab=ab: e.tensor_tensor(out=mod[:, l * 144:(l + 1) * 144].rearrange("p (a b) -> p a b", b=3),
                                                             in0=bank(0)[:, 0:144].rearrange("p (a b) -> p a b", b=3),
                                                             in1=ab.unsqueeze(2).broadcast_to([128, 48, 3]), op=ALU.add)),
              reads=[RB[0], R_const], writes=[R_mod])
        S.add("dve", (lambda e, l=l: e.tensor_scalar_add(out=mp1[:, l * 144:(l + 1) * 144], in0=mod[:, l * 144:(l + 1) * 144], scalar1=1.0)),
              reads=[R_mod], writes=[R_mod])
    S.barrier()
    cur[0] = base0

    A0 = cur[0]
    hres = alloc("hres", [128, 8, NT], F32)
    qT = alloc("qT", [128, 4, NT], BF16, at=A0)
    kT = alloc("kT", [128, 4, NT], BF16, at=A0 + 18432)
    vaug = alloc("vaug", [128, 18, 4, 192], BF16, at=A0 + 36864)
    qT1 = alloc("qT1", [128, 8, SEQ], BF16, at=A0)
    kT1 = alloc("kT1", [128, 2, NT], BF16, at=A0 + 32768)
    vaug1 = alloc("vaug1", [128, 18, 2, 192], BF16, at=A0 + 41984)
    rC = alloc("rC", [128, SEQ], F32, at=A0 + 55808)
    rS = alloc("rS", [128, SEQ], F32, at=A0 + 55808 + 8192)
    bvb = alloc("bvb", [128, 512], F32, at=A0 + 64512)
    idb = alloc("idb", [128, 128], BF16, at=A0 + 64512 + 2048)
    cmean = alloc("cmean", [128, 256], F32, at=A0 + 64512 + 2304)
    crstd = alloc("crstd", [128, 256], F32, at=A0 + 64512 + 3328)
    UT = alloc("UT", [128, 8, NT], BF16)
    OT0 = cur[0]
    oT = alloc("oT", [128, 8, NT], BF16)
    C0 = cur[0]
    RT = {}

    def rtok(name, t0, t1):
        out = []
        for tt in range(t0 // 128, (t1 + 127) // 128):
            key = (name, tt)
            if key not in RT:
                RT[key] = Res("%s_%d" % key)
            out.append(RT[key])
        return out

    R_hpad = [Res("hpad%d" % c) for c in range(4)]

    def derive_vecs(l, col, tag):
        ops = []
        for ch in range(8):
            g1 = smc("ln_g", (l * 2 + 0) * 8 + ch)
            b1 = smc("ln_b", (l * 2 + 0) * 8 + ch)
            g2 = smc("ln_g", (l * 2 + 1) * 8 + ch)
            b2 = smc("ln_b", (l * 2 + 1) * 8 + ch)
            S.add("dve", (lambda e, ch=ch, g1=g1: e.tensor_tensor(out=vslot((tag, "G4"), ch), in0=g1, in1=mp1c(l, 4, ch, col), op=ALU.mult)),
                  reads=[R_const, R_mod], writes=[R_vecs])
            S.add("dve", (lambda e, ch=ch, b1=b1: e.scalar_tensor_tensor(out=vslot((tag, "B4"), ch), in0=b1, scalar=mp1c(l, 4, ch, col), in1=modc(l, 3, ch, col),
                                                                         op0=ALU.mult, op1=ALU.add)),
                  reads=[R_const, R_mod], writes=[R_vecs])
            S.add("dve", (lambda e, ch=ch, g1=g1: e.tensor_scalar_mul(out=vslot((tag, "GA1"), ch), in0=g1, scalar1=ALPHA)), reads=[R_const], writes=[R_vecs])
            S.add("dve", (lambda e, ch=ch, b1=b1: e.tensor_scalar_mul(out=vslot((tag, "BA1"), ch), in0=b1, scalar1=ALPHA)), reads=[R_const], writes=[R_vecs])
            if l == 0:
                S.add("dve", (lambda e, ch=ch: e.tensor_tensor(out=vslot((tag, "M2B"), ch), in0=modc(l, 2, ch, col), in1=smc("b_out", ch), op=ALU.mult)),
                      reads=[R_const, R_mod], writes=[R_vecs])
                S.add("dve", (lambda e, ch=ch, g2=g2: e.tensor_tensor(out=vslot((tag, "GU"), ch), in0=g2, in1=mp1c(1, 1, ch, col), op=ALU.mult)),
                      reads=[R_const, R_mod], writes=[R_vecs])
                S.add("dve", (lambda e, ch=ch, b2=b2: e.scalar_tensor_tensor(out=vslot((tag, "BU"), ch), in0=b2, scalar=mp1c(1, 1, ch, col), in1=modc(1, 0, ch, col),
                                                                             op0=ALU.mult, op1=ALU.add)),
                      reads=[R_const, R_mod], writes=[R_vecs])

    def ln_part1(pre_ap, nch, N, ones_t, prebf, presq, bk, r_pre, r_tmp):
        S.add("dve", lambda e: e.tensor_copy(out=prebf[:, 0:nch, 0:N], in_=pre_ap), reads=r_pre, writes=[r_tmp[0]])
        S.add("act", lambda e: e.activation(out=presq[:, 0:nch, 0:N], in_=pre_ap, func=AF.Square), reads=r_pre, writes=[r_tmp[1]])
        for c in range(nch):
            S.add("pe", (lambda e, c=c: e.matmul(bank(bk)[:, 0:N], lhsT=ones_t[:], rhs=prebf[:, c, 0:N], start=(c == 0), stop=(c == nch - 1))),
                  reads=[r_tmp[0], R_const], writes=[RB[bk]])
        for c in range(nch):
            S.add("pe", (lambda e, c=c: e.matmul(bank(bk)[:, 256:256 + N], lhsT=ones_t[:], rhs=presq[:, c, 0:N], start=(c == 0), stop=(c == nch - 1))),
                  reads=[r_tmp[1], R_const], writes=[RB[bk]])

    def ln_part2(N, mean_sb, rstd_sb, bk, r_tmp):
        S.add("act", lambda e: e.copy(out=mean_sb[:, 0:N], in_=bank(bk)[:, 0:N]), reads=[RB[bk]], writes=[r_tmp[2]])
        S.add("dve", lambda e: e.tensor_tensor(out=rstd_sb[:, 0:N], in0=mean_sb[:, 0:N], in1=mean_sb[:, 0:N], op=ALU.mult), reads=[r_tmp[2]], writes=[r_tmp[3]])
        S.add("dve", lambda e: e.tensor_tensor(out=rstd_sb[:, 0:N], in0=bank(bk)[:, 256:256 + N], in1=rstd_sb[:, 0:N], op=ALU.subtract),
              reads=[RB[bk], r_tmp[3]], writes=[r_tmp[3]])
        S.add("act", lambda e: e.activation(out=rstd_sb[:, 0:N], in_=rstd_sb[:, 0:N], func=AF.Ln, bias=smc("eps", 0), scale=1.0),
              reads=[r_tmp[3], R_const], writes=[r_tmp[3]])
        S.add("act", lambda e: e.activation(out=rstd_sb[:, 0:N], in_=rstd_sb[:, 0:N], func=AF.Exp, scale=-0.5), reads=[r_tmp[3]], writes=[r_tmp[3]])

    def ln_stats(pre_ap, nch, N, ones_t, prebf, presq, mean_sb, rstd_sb, bk, r_pre, r_tmp):
        ln_part1(pre_ap, nch, N, ones_t, prebf, presq, bk, r_pre, r_tmp)
        ln_part2(N, mean_sb, rstd_sb, bk, r_tmp)

    def normalize(pre_ap, nch, N, mean_sb, rstd_sb, r_pre, r_tmp):
        S.add("dve", lambda e: e.tensor_tensor(out=pre_ap, in0=pre_ap, in1=mean_sb[:, 0:N].unsqueeze(1).broadcast_to([128, nch, N]), op=ALU.subtract),
              reads=r_pre + [r_tmp[2]], writes=r_pre)
        S.add("dve", lambda e: e.tensor_tensor(out=pre_ap, in0=pre_ap, in1=rstd_sb[:, 0:N].unsqueeze(1).broadcast_to([128, nch, N]), op=ALU.mult),
              reads=r_pre + [r_tmp[3]], writes=r_pre)

    aff_rr = [0]

    def affine(out_ap, in_ap, sc, bi, reads, writes, psum_in=False):
        k = aff_rr[0] % 2
        aff_rr[0] += 1
        if k == 0:
            S.add("act", lambda e: e.activation(out=out_ap, in_=in_ap, func=AF.Identity, bias=bi, scale=sc), reads=reads, writes=writes)
        else:
            S.add("dve" if k == 1 else "pool", lambda e: e.tensor_scalar(out=out_ap, in0=in_ap, scalar1=sc, scalar2=bi, op0=ALU.mult, op1=ALU.add),
                  reads=reads, writes=writes)

    def dump(name, src_ap3):
        if stop != name and stop != "%s@%d" % (name, cur_l[0]):
            return
        S.barrier()
        c, t = src_ap3.shape[1], src_ap3.shape[2]
        dst = dbg if src_ap3.dtype == F32 else dbgb
        for ci in range(c):
            S.add("sp", (lambda e, ci=ci: e.dma_start(out=dst[:, ci * t:(ci + 1) * t], in_=src_ap3[:, ci, :])), writes=[Res("dbgo")], dma=True)
        raise _Stop()

    cur_l = [0]
    try:
      for b in range(nb):
        for l in range(nlayers):
              cur_l[0] = l
              lat_only = (l == nlayers - 1) and l == 1
              col = b
              S.barrier()
              derive_vecs(l, b, "lat")
              if l == 0:
                  derive_vecs(l, 2, "ctx")

              def V(kind, ch, t0):
                  return vslot((("ctx" if (t0 < NCTX and l == 0) else "lat"), kind), ch)

              def mcol(t0):
                  return 2 if t0 < NCTX else b

              cur[0] = C0
              if l == 0:
                  xin = [alloc("xin%d" % i, [128, D], F32) for i in range(2)]
                  hblk = [alloc("hblk%d" % i, [128, 8, 128], F32) for i in range(2)]
                  R_xin = [Res("xin0"), Res("xin1")]
                  R_hblk = [Res("hblk0"), Res("hblk1")]
                  for tt in range(18):
                      s = tt % 2
                      src = ctx2[b, tt * 128:(tt + 1) * 128, :] if tt < 2 else x2[b, (tt - 2) * 128:(tt - 1) * 128, :]
                      S.add("sp", (lambda e, s=s, src=src: e.dma_start(out=xin[s][:], in_=src)), writes=[R_xin[s]], dma=True)
                      for ch in range(8):
                          bk = (tt % 2) * 2 + ch // 4
                          S.add("pe", (lambda e, s=s, ch=ch, bk=bk: e.transpose(bank(bk)[:, (ch % 4) * 128:(ch % 4 + 1) * 128], xin[s][:, ch * 128:(ch + 1) * 128], id32[:])),
                                reads=[R_xin[s], R_const], writes=[RB[bk]])
                      for hf in range(2):
                          bk = (tt % 2) * 2 + hf
                          S.add("act", (lambda e, s=s, hf=hf, bk=bk: e.copy(out=hblk[s][:, hf * 4:(hf + 1) * 4, :], in_=bank(bk).rearrange("p (c t) -> p c t", c=4))),
                                reads=[RB[bk]], writes=[R_hblk[s]])
                      mc = mcol(tt * 128)
                      for ch in range(8):
                          bk = (tt % 2) * 2 + ch // 4
                          affine(UT[:, ch, tt * 128:(tt + 1) * 128], bank(bk)[:, (ch % 4) * 128:(ch % 4 + 1) * 128], mp1c(0, 1, ch, mc), modc(0, 0, ch, mc),
                                 [RB[bk], R_mod], rtok("UT", tt * 128, tt * 128 + 128), psum_in=True)
                      S.add("sp", (lambda e, s=s, tt=tt: e.dma_start(out=Hs3[:, :, tt * 128:(tt + 1) * 128], in_=hblk[s][:])), reads=[R_hblk[s]],
                            writes=rtok("Hs", tt * 128, tt * 128 + 128), dma=True)

              if l == 0:
                  dump("S0", UT[:])
              blocks512 = [(0, 256)] + [(256 + 512 * i, 512) for i in range(4)]
              blocks256 = [(256 * i, 256) for i in range(9)]
              if l == 1:
                  lat512 = [(256 + 512 * i, 512) for i in range(4)]
                  lat256 = [(256 * i, 256) for i in range(1, 9)]

              if l == 0:
                  S.barrier()
                  cur[0] = C0
                  wAB = alloc("wAB", [128, 8, 1536], BF16)
                  wA = alloc("wA", [128, 8, 1024], BF16, at=C0)
                  hpd = [alloc("hpd%d" % i, [128, 2368], BF16, at=C0 + 16384 + i * 4736) for i in range(2)]
                  dg0 = alloc("diag0", [128, 31, 128], BF16, at=C0 + 25856)
                  diag = [dg0, dg0]
                  cur[0] = C0 + 33792
                  sgt = [alloc("sgt%d" % i, [128, 512], F32) for i in range(2)]
                  czsq = alloc("czsq", [128, 4, 256], BF16)
                  cz = alloc("cz", [128, 4, 256], F32)
                  R_wAB, R_misc = Res("wAB"), Res("misc0")
                  R_hpd = [Res("hpd0"), Res("hpd1")]
                  R_dg = [Res("dg0")] * 2
                  R_sgt = [Res("sgt0"), Res("sgt1")]
                  R_cz, R_ct = Res("cz"), [Res("czbf"), Res("czsq"), Res("cmean"), Res("crstd")]
                  w0 = w_in0.rearrange("(kc p) n -> p kc n", p=128)
                  S.add("pool", lambda e: e.dma_start(out=wA[:], in_=w0[:, :, 0:1024]), writes=[R_wAB], dma=True)
                  S.add("pool", lambda e: e.dma_start(out=idb[:], in_=ident), writes=[R_misc], dma=True)
                  S.add("sp", lambda e: e.dma_start(out=bvb[:], in_=bvbc), writes=[R_misc], dma=True)
                  S.add("pool", lambda e: e.memset(hpd[0][:], 0.0), writes=[R_hpd[0]])
                  S.add("pool", lambda e: e.memset(hpd[1][:], 0.0), writes=[R_hpd[1]])
                  S.add("pool", lambda e: e.memset(vaug[:], 1.0), writes=rtok("vaug", 0, NT))

                  def hoff(t0):
                      return 15 + t0 if t0 < NCTX else 286 + 15 + (t0 - NCTX)

                  it = 0
                  for cc in range(4):
                      hs_ = cc % 2
                      for k in range(31):
                          S.add("dve", (lambda e, cc=cc, k=k, hs_=hs_: e.tensor_scalar_mul(out=diag[hs_][:, k, :], in0=idb[:], scalar1=smc("conv_w", cc * 31 + k))),
                                reads=[R_misc, R_const], writes=[R_dg[hs_]])
                      for (t0, N) in blocks512:
                          s = it % 2
                          it += 1
                          b1, b2 = 2 * s, 2 * s + 1
                          for kc in range(8):
                              S.add("pe", (lambda e, kc=kc, cc=cc, t0=t0, N=N, b1=b1: e.matmul(bank(b1)[:, 0:N], lhsT=wA[:, kc, cc * 128:(cc + 1) * 128], rhs=UT[:, kc, t0:t0 + N],
                                                                                             start=(kc == 0), stop=(kc == 7))),
                                    reads=[R_wAB] + rtok("UT", t0, t0 + N), writes=[RB[b1]])
                          for kc in range(8):
                              S.add("pe", (lambda e, kc=kc, cc=cc, t0=t0, N=N, b2=b2: e.matmul(bank(b2)[:, 0:N], lhsT=wA[:, kc, 512 + cc * 128:512 + (cc + 1) * 128], rhs=UT[:, kc, t0:t0 + N],
                                                                                             start=(kc == 0), stop=(kc == 7))),
                                    reads=[R_wAB] + rtok("UT", t0, t0 + N), writes=[RB[b2]])
                          S.add("act", (lambda e, s=s, cc=cc, N=N, b2=b2: e.activation(out=sgt[s][:, 0:N], in_=bank(b2)[:, 0:N], func=AF.Sigmoid, bias=smc("b_in", 4 + cc), scale=1.0)),
                                reads=[RB[b2], R_const], writes=[R_sgt[s]])
                          ho = hoff(t0)
                          S.add("dve", (lambda e, s=s, cc=cc, N=N, b1=b1, ho=ho, hs_=hs_: e.scalar_tensor_tensor(out=hpd[hs_][:, ho:ho + N], in0=bank(b1)[:, 0:N], scalar=smc("b_in", cc),
                                                                                                                 in1=sgt[s][:, 0:N], op0=ALU.add, op1=ALU.mult)),
                                reads=[RB[b1], R_sgt[s], R_const], writes=[R_hpd[hs_]])
                      for bi, (t0, N) in enumerate(blocks512):
                          ho = hoff(t0) - 15
                          bk = 4 + bi % 2
                          for k in range(31):
                              S.add("pe", (lambda e, k=k, ho=ho, N=N, bk=bk, hs_=hs_: e.matmul(bank(bk)[:, 0:N], lhsT=diag[hs_][:, k, :], rhs=hpd[hs_][:, ho + k:ho + k + N],
                                                                                           start=(k == 0), stop=(k == 30))),
                                    reads=[R_dg[hs_], R_hpd[hs_]], writes=[RB[bk]])
                          S.add("act", (lambda e, cc=cc, N=N, t0=t0, bk=bk: e.activation(out=oT[:, cc, t0:t0 + N], in_=bank(bk)[:, 0:N], func=AF.Identity, bias=smc("conv_b", cc), scale=1.0)),
                                reads=[RB[bk], R_const], writes=rtok("oT", t0, t0 + N))
                  dump("S1z", oT[:, 0:4, :])
                  dump("S1h", hpd[1][:].unsqueeze(1))
                  dump("S1d", diag[0][:])
                  S.add("pool", lambda e: e.dma_start(out=wAB[:], in_=w0[:, :, 1024:2560]), writes=[R_wAB] + R_hpd + [R_dg[0]], dma=True)
                  mk0 = S.mark()
                  for (t0, N) in blocks256:
                      zin = oT[:, 0:4, t0:t0 + 256]
                      rz = rtok("oT", t0, t0 + 256)
                      S.add("act", (lambda e, zin=zin: e.activation(out=czsq[:], in_=zin, func=AF.Square)), reads=rz, writes=[R_ct[1]])
                      for c4 in range(4):
                          S.add("pe", (lambda e, c4=c4, t0=t0: e.matmul(bank(6)[:, 0:256], lhsT=ones512[:], rhs=oT[:, c4, t0:t0 + 256], start=(c4 == 0), stop=(c4 == 3))),
                                reads=rz + [R_const], writes=[RB[6]])
                      for c4 in range(4):
                          S.add("pe", (lambda e, c4=c4: e.matmul(bank(6)[:, 256:512], lhsT=ones512[:], rhs=czsq[:, c4, :], start=(c4 == 0), stop=(c4 == 3))),
                                reads=[R_ct[1], R_const], writes=[RB[6]])
                      S.add("act", lambda e: e.copy(out=cmean[:], in_=bank(6)[:, 0:256]), reads=[RB[6]], writes=[R_ct[2]])
                      S.add("dve", lambda e: e.tensor_tensor(out=crstd[:], in0=cmean[:], in1=cmean[:], op=ALU.mult), reads=[R_ct[2]], writes=[R_ct[3]])
                      S.add("dve", lambda e: e.tensor_tensor(out=crstd[:], in0=bank(6)[:, 256:512], in1=crstd[:], op=ALU.subtract), reads=[RB[6], R_ct[3]], writes=[R_ct[3]])
                      S.add("act", lambda e: e.activation(out=crstd[:], in_=crstd[:], func=AF.Sqrt, bias=smc("eps", 0), scale=1.0), reads=[R_ct[3], R_const], writes=[R_ct[3]])
                      S.add("dve", lambda e: e.reciprocal(out=crstd[:], in_=crstd[:]), reads=[R_ct[3]], writes=[R_ct[3]])
                      S.add("dve", (lambda e, zin=zin: e.tensor_tensor(out=cz[:], in0=zin, in1=cmean[:].unsqueeze(1).broadcast_to([128, 4, 256]), op=ALU.subtract)),
                            reads=rz + [R_ct[2]], writes=[R_cz])
                      S.add("dve", lambda e: e.tensor_tensor(out=cz[:], in0=cz[:], in1=crstd[:].unsqueeze(1).broadcast_to([128, 4, 256]), op=ALU.mult),
                            reads=[R_cz, R_ct[3]], writes=[R_cz])
                      for c4 in range(4):
                          S.add("act", (lambda e, c4=c4, t0=t0: e.activation(out=oT[:, c4, t0:t0 + 256], in_=cz[:, c4, :], func=AF.Silu, bias=smc("cln_b", c4), scale=smc("cln_g", c4))),
                                reads=[R_cz, R_const], writes=rz)
                  mk1 = S.mark()
                  it = 0
                  for (t0, N) in blocks512:
                      for c in range(8):
                          bk = it % 4
                          it += 1
                          for kc in range(8):
                              S.add("pe", (lambda e, kc=kc, c=c, t0=t0, N=N, bk=bk: e.matmul(bank(bk)[:, 0:N], lhsT=wAB[:, kc, c * 128:(c + 1) * 128], rhs=UT[:, kc, t0:t0 + N],
                                                                                           start=(kc == 0), stop=(kc == 7))),
                                    reads=[R_wAB] + rtok("UT", t0, t0 + N), writes=[RB[bk]])
                          dst = qT if c < 4 else kT
                          S.add("act", (lambda e, c=c, t0=t0, N=N, bk=bk, dst=dst: e.activation(out=dst[:, c % 4, t0:t0 + N], in_=bank(bk)[:, 0:N], func=AF.Identity,
                                                                                              bias=smc("b_in", 8 + c), scale=1.0)),
                                reads=[RB[bk], R_const], writes=rtok("qk", t0, t0 + N))
                  for tt in range(18):
                      bk = it % 4
                      it += 1
                      for kc in range(8):
                          S.add("pe", (lambda e, kc=kc, tt=tt, bk=bk: e.matmul(bank(bk)[:, 0:512], lhsT=UT[:, kc, tt * 128:(tt + 1) * 128], rhs=wAB[:, kc, 1024:1536],
                                                                             start=(kc == 0), stop=(kc == 7))),
                                reads=[R_wAB] + rtok("UT", tt * 128, tt * 128 + 128), writes=[RB[bk]])
                      for x in range(2):
                          S.add("dve", (lambda e, tt=tt, bk=bk, x=x: e.tensor_tensor(out=vaug[:, tt, :, x * 128:x * 128 + 64],
                                                                                   in0=bank(bk).rearrange("p (c x d) -> p c x d", c=4, x=2)[:, :, x, :],
                                                                                   in1=bvb[:].rearrange("p (c x d) -> p c x d", c=4, x=2)[:, :, x, :], op=ALU.add)),
                                reads=[RB[bk], R_misc], writes=rtok("vaug", tt * 128, tt * 128 + 128))

                  S.interleave(mk0, mk1, S.mark())
                  dump("S1o", oT[:, 0:4, :])
                  dump("S1q", qT[:])
                  dump("S1k", kT[:])
                  dump("S1v", vaug[:].rearrange("p t c x -> p t (c x)"))
                  S.barrier()
                  cur[0] = C0
                  nabt = [alloc("nabt%d" % i, [128, 21, 128], F32) for i in range(2)]
                  sbt = [alloc("sbt%d" % i, [128, 640], F32) for i in range(3)]
                  PT = [alloc("PT%d" % i, [128, 896], BF16) for i in range(3)]
                  rec = [alloc("rec%d" % i, [128, 256], F32) for i in range(2)]
                  R_nabt = [Res("nabt0"), Res("nabt1")]
                  R_sbt = [Res("sbt0"), Res("sbt1"), Res("sbt2")]
                  R_PT = [Res("PT0"), Res("PT1"), Res("PT2")]
                  PSl = [PS[0], PS[1], PS[3]]
                  PSb = [0, 2, 6]
                  itl = 0
                  na_q = []
                  na_qB = []
                  R_rec = [Res("rec0"), Res("rec1")]
                  it = 0
                  na_pend = [None]
                  for h in range(8):
                      while na_q or na_qB:
                          if na_qB:
                              na_qB.pop(0)()
                          if na_q:
                              na_q.pop(0)()
                      c, sh = h // 2, h % 2
                      p0, p1 = sh * 64, sh * 64 + 64
                      nh0, nh1 = (0, 64) if sh == 0 else (64, 128)
                      dh0, dh1 = (64, 128) if sh == 0 else (0, 64)
                      hs = h % 2
                      S.add("sp", (lambda e, h=h, hs=hs: e.dma_start(out=nabt[hs][:], in_=nab[h].rearrange("p (a q) -> p a q", a=21))), writes=[R_nabt[hs]], dma=True)
                      vcol = sh * 64
                      s = 0
                      sb0 = 0
                      for i in range(2):
                          S.add("pe", (lambda e, i=i, c=c, p0=p0, p1=p1, sb0=sb0: e.matmul(bank(sb0)[:, i * 256:(i + 1) * 256], lhsT=kT[p0:p1, c, i * 128:(i + 1) * 128],
                                                                                          rhs=qT[p0:p1, c, 0:256], start=True, stop=True)),
                                reads=rtok("qk", 0, 256), writes=[RB[sb0]])
                      S.add("act", (lambda e, s=s, sb0=sb0: e.activation(out=PT[s][:, 0:512], in_=bank(sb0)[:, 0:512], func=AF.Exp, scale=0.125)), reads=[RB[sb0]], writes=[R_PT[s]])
                      ob = 4 + s
                      for i in range(2):
                          S.add("pe", (lambda e, i=i, c=c, s=s, ob=ob, vcol=vcol: e.matmul(bank(ob)[:, 0:256], lhsT=vaug[:, i, c, vcol:vcol + 128], rhs=PT[s][:, i * 256:(i + 1) * 256],
                                                                                          start=(i == 0), stop=(i == 1))),
                                reads=[R_PT[s]] + rtok("vaug", 0, 256), writes=[RB[ob]])
                      S.add("dve", (lambda e, s=s, ob=ob, dh0=dh0, dh1=dh1: e.reciprocal(out=rec[s][dh0:dh1, 0:256], in_=bank(ob)[dh0:dh1, 0:256])), reads=[RB[ob]], writes=[R_rec[s]])
                      S.add("dve", (lambda e, s=s, ob=ob, c=c, nh0=nh0, nh1=nh1, dh0=dh0, dh1=dh1: e.tensor_tensor(out=oT[nh0:nh1, 4 + c, 0:256], in0=bank(ob)[nh0:nh1, 0:256],
                                                                                                               in1=rec[s][dh0:dh1, 0:256], op=ALU.mult)),
                            reads=[RB[ob], R_rec[s]], writes=rtok("oT", 0, 256))
                      for j in range(16):
                          s = itl % 3
                          so = itl % 2
                          itl += 1
                          sb0 = PSb[s]
                          tl = _na_tiles(j)
                          nl = len(tl)
                          ti0 = _na_tile_index(j)
                          q0 = NCTX + j * 128
                          ktoks = [NCTX + a * 128 for a in tl] + [0, 128]
                          for i, kt0 in enumerate(ktoks):
                              bk = sb0 + (i // 4)
                              S.add("pe", (lambda e, i=i, kt0=kt0, bk=bk, c=c, p0=p0, p1=p1, q0=q0: e.matmul(bank(bk)[:, (i % 4) * 128:(i % 4 + 1) * 128], lhsT=kT[p0:p1, c, kt0:kt0 + 128],
                                                                                                        rhs=qT[p0:p1, c, q0:q0 + 128], start=True, stop=True)),
                                    reads=rtok("qk", kt0, kt0 + 128) + rtok("qk", q0, q0 + 128), writes=[RB[bk]])
                          S.add("dve", (lambda e, s=s, nl=nl, ti0=ti0, hs=hs: e.scalar_tensor_tensor(out=sbt[s][:, 0:nl * 128], in0=PSl[s][:, 0:nl * 128], scalar=0.125,
                                                                                                    in1=nabt[hs][:, ti0:ti0 + nl, :].rearrange("p a q -> p (a q)"),
                                                                                                    op0=ALU.mult, op1=ALU.add)),
                                reads=[RB[sb0], RB[sb0 + 1], R_nabt[hs]], writes=[R_sbt[s]])
                          S.add("act", (lambda e, s=s, nl=nl: e.activation(out=PT[s][:, 0:nl * 128], in_=sbt[s][:, 0:nl * 128], func=AF.Exp)), reads=[R_sbt[s]], writes=[R_PT[s]])
                          S.add("act", (lambda e, s=s, nl=nl: e.activation(out=PT[s][:, nl * 128:(nl + 2) * 128], in_=PSl[s][:, nl * 128:(nl + 2) * 128], func=AF.Exp, scale=0.125)),
                                reads=[RB[sb0], RB[sb0 + 1]], writes=[R_PT[s]])
                          ob = 4 + so

                          def na_back(ktoks=ktoks, c=c, s=s, so=so, ob=ob, vcol=vcol, nl=nl, q0=q0, nh0=nh0, nh1=nh1, dh0=dh0, dh1=dh1):
                              for i, kt0 in enumerate(ktoks):
                                  S.add("pe", (lambda e, i=i, kt0=kt0: e.matmul(bank(ob)[:, 0:128], lhsT=vaug[:, kt0 // 128, c, vcol:vcol + 128],
                                                                                rhs=PT[s][:, i * 128:(i + 1) * 128], start=(i == 0), stop=(i == nl + 1))),
                                        reads=[R_PT[s]] + rtok("vaug", kt0, kt0 + 128), writes=[RB[ob]])
                              S.add("act", (lambda e: e.activation(out=rec[so][dh0:dh1, 0:128], in_=bank(ob)[dh0:dh1, 0:128], func=AF.Ln)), reads=[RB[ob]], writes=[R_rec[so]])
                              S.add("act", (lambda e: e.activation(out=rec[so][dh0:dh1, 0:128], in_=rec[so][dh0:dh1, 0:128], func=AF.Exp, scale=-1.0)), reads=[R_rec[so]], writes=[R_rec[so]])

                              def na_backB():
                                  S.add("dve", (lambda e: e.tensor_tensor(out=oT[nh0:nh1, 4 + c, q0:q0 + 128], in0=bank(ob)[nh0:nh1, 0:128],
                                                                          in1=rec[so][dh0:dh1, 0:128], op=ALU.mult)),
                                        reads=[RB[ob], R_rec[so]], writes=rtok("oT", q0, q0 + 128))
                              na_qB.append(na_backB)
                          if na_qB:
                              na_qB.pop(0)()
                          na_q.append(na_back)
                          if len(na_q) > 1:
                              na_q.pop(0)()
                  while na_q or na_qB:
                      if na_qB:
                          na_qB.pop(0)()
                      if na_q:
                          na_q.pop(0)()
                  dump("S3", oT[:, 4:8, :])
                  w_out_d = w_out0
                  tok_blocks = blocks256
              else:
                  S.barrier()
                  cur[0] = C0
                  wq = alloc("wq", [128, 8, 512], BF16)
                  wqs = alloc("wqs", [128, 8, 512], BF16)
                  wk = alloc("wk", [128, 8, 256], BF16)
                  wks = alloc("wks", [128, 8, 256], BF16)
                  wv = alloc("wv", [128, 8, 256], BF16)
                  rt = [alloc("rt%d" % i, [128, 2, 512], F32) for i in range(2)]
                  R_w1, R_rope = Res("w1"), Res("rope")
                  R_rt = [Res("rt0"), Res("rt1")]
                  R_wq = Res("wq")
                  for dst, src in ((wk, wk1), (wks, wks1), (wv, wv1)):
                      S.add("pool", (lambda e, dst=dst, src=src: e.dma_start(out=dst[:], in_=src.rearrange("(kc p) n -> p kc n", p=128))), writes=[R_w1], dma=True)
                  S.add("sp", lambda e: e.dma_start(out=rC[:], in_=ropeC), writes=[R_rope], dma=True)
                  S.add("sp", lambda e: e.dma_start(out=rS[:], in_=ropeS), writes=[R_rope], dma=True)
                  if True:
                      S.add("pool", lambda e: e.memset(vaug1[:], 1.0), writes=rtok("vaug", 0, NT))
                  it = 0
                  R_wqq = [Res("wqq0"), Res("wqq1")]

                  def load_q(qt):
                      sl = qt % 2
                      for dst, src in ((wq, wq1), (wqs, wqs1)):
                          S.add("pool", (lambda e, dst=dst, src=src: e.dma_start(out=dst[:, :, sl * 256:(sl + 1) * 256],
                                                                                 in_=src.rearrange("(kc p) n -> p kc n", p=128)[:, :, qt * 256:(qt + 1) * 256])),
                                writes=[R_wqq[sl]], dma=True)

                  load_q(0)
                  load_q(1)
                  for half, (t0, N) in [(hf_, blk_) for hf_ in range(5) for blk_ in lat512]:
                      l0 = t0 - NCTX
                      if 1 <= half < 3 and t0 == NCTX:
                          load_q(half + 1)
                      R_wq = R_wqq[half % 2] if half < 4 else R_w1
                      for c in (range(half * 2, half * 2 + 2) if half < 4 else range(8, 10)):
                          s = it % 2
                          it += 1
                          b1, b2 = 2 * s, 2 * s + 1
                          wa, wb = (wq, wqs) if c < 8 else (wk, wks)
                          cc = ((half % 2) * 2 + c % 2) if c < 8 else c - 8
                          for kc in range(8):
                              S.add("pe", (lambda e, kc=kc, cc=cc, wa=wa, t0=t0, N=N, b1=b1: e.matmul(bank(b1)[:, 0:N], lhsT=wa[:, kc, cc * 128:(cc + 1) * 128], rhs=UT[:, kc, t0:t0 + N],
                                                                                                 start=(kc == 0), stop=(kc == 7))),
                                    reads=[R_w1, R_wq] + rtok("UT", t0, t0 + N), writes=[RB[b1]])
                          for kc in range(8):
                              S.add("pe", (lambda e, kc=kc, cc=cc, wb=wb, t0=t0, N=N, b2=b2: e.matmul(bank(b2)[:, 0:N], lhsT=wb[:, kc, cc * 128:(cc + 1) * 128], rhs=UT[:, kc, t0:t0 + N],
                                                                                                 start=(kc == 0), stop=(kc == 7))),
                                    reads=[R_w1, R_wq] + rtok("UT", t0, t0 + N), writes=[RB[b2]])
                          S.add("dve", (lambda e, s=s, l0=l0, N=N, b1=b1: e.tensor_tensor(out=rt[s][:, 0, 0:N], in0=bank(b1)[:, 0:N], in1=rC[:, l0:l0 + N], op=ALU.mult)),
                                reads=[RB[b1], R_rope], writes=[R_rt[s]])
                          S.add("dve", (lambda e, s=s, l0=l0, N=N, b2=b2: e.tensor_tensor(out=rt[s][:, 1, 0:N], in0=bank(b2)[:, 0:N], in1=rS[:, l0:l0 + N], op=ALU.mult)),
                                reads=[RB[b2], R_rope], writes=[R_rt[s]])
                          if c < 8:
                              dst = qT1[:, c, l0:l0 + N]
                          else:
                              dst = kT1[:, c - 8, t0:t0 + N]
                          S.add("dve", (lambda e, s=s, N=N, dst=dst: e.tensor_tensor(out=dst, in0=rt[s][:, 0, 0:N], in1=rt[s][:, 1, 0:N], op=ALU.add)),
                                reads=[R_rt[s]], writes=rtok("qk", t0, t0 + N))
                  for cc in range(2):
                      s = it % 2
                      it += 1
                      b1 = 2 * s
                      for kc in range(8):
                          S.add("pe", (lambda e, kc=kc, cc=cc, b1=b1: e.matmul(bank(b1)[:, 0:256], lhsT=wk[:, kc, cc * 128:(cc + 1) * 128], rhs=UT[:, kc, 0:256], start=(kc == 0), stop=(kc == 7))),
                                reads=[R_w1] + rtok("UT", 0, 256), writes=[RB[b1]])
                      S.add("act", (lambda e, cc=cc, b1=b1: e.copy(out=kT1[:, cc, 0:256], in_=bank(b1)[:, 0:256])), reads=[RB[b1]], writes=rtok("qk", 0, 256))
                  for tt in range(18):
                      bk = 4 + tt % 2
                      for kc in range(8):
                          S.add("pe", (lambda e, kc=kc, tt=tt, bk=bk: e.matmul(bank(bk)[:, 0:256], lhsT=UT[:, kc, tt * 128:(tt + 1) * 128], rhs=wv[:, kc, :], start=(kc == 0), stop=(kc == 7))),
                                reads=[R_w1] + rtok("UT", tt * 128, tt * 128 + 128), writes=[RB[bk]])
                      for x in range(2):
                          S.add("act", (lambda e, tt=tt, bk=bk, x=x: e.copy(out=vaug1[:, tt, :, x * 128:x * 128 + 64],
                                                                          in_=bank(bk)[:, 0:256].rearrange("p (c x d) -> p c x d", c=2, x=2)[:, :, x, :])),
                                reads=[RB[bk]], writes=rtok("vaug", tt * 128, tt * 128 + 128))
                  dump("P1", kT1[:])
                  S.barrier()
                  cur[0] = C0
                  mlu = alloc("mlu", [128, 256], F32)
                  S.add("sp", lambda e: e.dma_start(out=mlu[:], in_=maskLU), writes=[R_const], dma=True)
                  sbm = [alloc("sbm%d" % i, [128, 512], F32) for i in range(2)]
                  PT1 = [alloc("PT1_%d" % i, [128, 5, 512], BF16) for i in range(2)]
                  rec1 = [alloc("rec1_%d" % i, [128, 512], F32) for i in range(2)]
                  R_sbm = [Res("sbm0"), Res("sbm1")]
                  R_PT1 = [[Res("PT1_%d_%d" % (i, k)) for k in range(5)] for i in range(2)]
                  R_rec1 = [Res("rec1_0"), Res("rec1_1")]
                  it = 0
                  sbr = 0
                  mi = 0
                  gq_pend = [None]
                  gq_qB = []
                  for g in range(4):
                      m, sh = g // 2, g % 2
                      p0, p1 = sh * 64, sh * 64 + 64
                      nh0, nh1 = (0, 64) if sh == 0 else (64, 128)
                      dh0, dh1 = (64, 128) if sh == 0 else (0, 64)
                      vcol = sh * 64
                      for qb in range(16):
                          s = it % 2
                          it += 1
                          tiles = []
                          if qb > 0:
                              tiles.append((NCTX + (qb - 1) * 128, 0))
                          tiles.append((NCTX + qb * 128, None))
                          if qb < 15:
                              tiles.append((NCTX + (qb + 1) * 128, 1))
                          tiles += [(0, None), (128, None)]
                          nt = len(tiles)
                          for i, (kt0, mk) in enumerate(tiles):
                              bk = sbr % 4
                              sbr += 1
                              S.add("pe", (lambda e, kt0=kt0, bk=bk, m=m, p0=p0, p1=p1, qb=qb: e.matmul(bank(bk).rearrange("p (h q) -> p h q", h=4), lhsT=kT1[p0:p1, m, kt0:kt0 + 128],
                                                                                                   rhs=qT1[p0:p1, 4 * m:4 * m + 4, qb * 128:(qb + 1) * 128], start=True, stop=True)),
                                    reads=rtok("qk", kt0, kt0 + 128) + rtok("qk", NCTX + qb * 128, NCTX + qb * 128 + 128), writes=[RB[bk]])
                              if mk is None:
                                  S.add("act", (lambda e, s=s, i=i, bk=bk: e.activation(out=PT1[s][:, i, :], in_=bank(bk), func=AF.Exp, scale=0.125)), reads=[RB[bk]], writes=[R_PT1[s][i]])
                              else:
                                  ms = mi % 2
                                  mi += 1
                                  S.add("dve", (lambda e, ms=ms, mk=mk, bk=bk: e.scalar_tensor_tensor(out=sbm[ms][:].rearrange("p (h q) -> p h q", h=4), in0=bank(bk).rearrange("p (h q) -> p h q", h=4),
                                                                                                    scalar=0.125, in1=mlu[:, mk * 128:(mk + 1) * 128].unsqueeze(1).broadcast_to([128, 4, 128]),
                                                                                                    op0=ALU.mult, op1=ALU.add)),
                                        reads=[RB[bk], R_const], writes=[R_sbm[ms]])
                                  S.add("act", (lambda e, s=s, i=i, ms=ms: e.activation(out=PT1[s][:, i, :], in_=sbm[ms][:], func=AF.Exp)), reads=[R_sbm[ms]], writes=[R_PT1[s][i]])
                          ob = 4 + s
                          q0 = NCTX + qb * 128

                          def gq_back(tiles=tiles, s=s, ob=ob, m=m, sh=sh, vcol=vcol, nt=nt, q0=q0, nh0=nh0, nh1=nh1, dh0=dh0, dh1=dh1):
                              for i, (kt0, mk) in enumerate(tiles):
                                  S.add("pe", (lambda e, i=i, kt0=kt0: e.matmul(bank(ob), lhsT=vaug1[:, kt0 // 128, m, vcol:vcol + 128], rhs=PT1[s][:, i, :],
                                                                                start=(i == 0), stop=(i == nt - 1))),
                                        reads=[R_PT1[s][i]] + rtok("vaug", kt0, kt0 + 128), writes=[RB[ob]])
                              S.add("dve", (lambda e: e.tensor_tensor(out=rec1[s][dh0:dh1, :].rearrange("p (h q) -> p h q", h=4),
                                                                      in0=bank(ob)[dh0:dh1, :].rearrange("p (h q) -> p h q", h=4),
                                                                      in1=esink[dh0:dh1, (m * 2 + sh) * 4:(m * 2 + sh) * 4 + 4].unsqueeze(2).broadcast_to([64, 4, 128]),
                                                                      op=ALU.add)),
                                    reads=[RB[ob], R_const], writes=[R_rec1[s]])
                              S.add("act", (lambda e: e.activation(out=rec1[s][dh0:dh1, :], in_=rec1[s][dh0:dh1, :], func=AF.Ln)), reads=[R_rec1[s]], writes=[R_rec1[s]])
                              S.add("act", (lambda e: e.activation(out=rec1[s][dh0:dh1, :], in_=rec1[s][dh0:dh1, :], func=AF.Exp, scale=-1.0)), reads=[R_rec1[s]], writes=[R_rec1[s]])
                              def gq_backB():
                                  S.add("dve", (lambda e: e.tensor_tensor(out=oT[nh0:nh1, 4 * m:4 * m + 4, q0:q0 + 128],
                                                                          in0=bank(ob)[nh0:nh1, :].rearrange("p (h q) -> p h q", h=4),
                                                                          in1=rec1[s][dh0:dh1, :].rearrange("p (h q) -> p h q", h=4), op=ALU.mult)),
                                        reads=[RB[ob], R_rec1[s]], writes=rtok("oT", q0, q0 + 128))
                              gq_qB.append(gq_backB)
                          if gq_qB:
                              gq_qB.pop(0)()
                          if gq_pend[0] is not None:
                              gq_pend[0]()
                          gq_pend[0] = gq_back
                  if gq_qB:
                      gq_qB.pop(0)()
                  gq_pend[0]()
                  gq_pend[0] = None
                  while gq_qB:
                      gq_qB.pop(0)()
                  dump("A1", oT[:])
                  w_out_d = w_out1
                  tok_blocks = lat256

              S.barrier()
              cur[0] = C0
              gTh = alloc("gTh", [16, NT], BF16)
              gTl = alloc("gTl", [16, NT], BF16)
              assert cur[0] == C0 + 9216
              gatesT = alloc("gatesT", [16, NT], F32, at=C0 + 9216)
              wo = alloc("wo", [128, 8, D], BF16)
              hold0_at = cur[0]
              hold = [alloc("hold%d" % i, [128, 8, 128], F32) for i in range(2)]
              pre = alloc("pre", [128, 8, 128], F32)
              prebf = alloc("prebf", [128, 8, 128], BF16)
              presq = alloc("presq", [128, 8, 128], BF16)
              t32 = alloc("t32", [128, 8, 128], F32)
              tmpo = [alloc("tmpo%d" % i, [128, 128], F32) for i in range(2)]
              mean_sb = alloc("mean_sb", [128, 128], F32)
              rstd_sb = alloc("rstd_sb", [128, 128], F32)
              lsb = alloc("lsb", [128, 18, 20], F32, at=hold0_at)
              rw = alloc("rw", [128, 18 * 96], F32, at=hold0_at + 1472)
              assert hold0_at + 1472 + 18 * 96 * 4 <= cur[0]
              R_wo = Res("wo")
              R_hold = [Res("hold0"), Res("hold1")]
              R_pre, R_t32 = Res("pre"), Res("t32")
              R_tmpo = [Res("tmpo0"), Res("tmpo1")]
              R_lt = [Res("prebf"), Res("presq"), Res("mean"), Res("rstd")]
              R_rout = Res("rout")
              S.add("pool", (lambda e, w_out_d=w_out_d: e.dma_start(out=wo[:], in_=w_out_d.rearrange("(kc p) n -> p kc n", p=128))), writes=[R_wo], dma=True)
              tiles4 = [t for (t0_, n_) in tok_blocks for t in range(t0_, t0_ + n_, 128)]
              pre2 = alloc("pre2x", [128, 8, 128], F32, at=C0)
              pre_b = [pre, pre2]
              R_pre_b = [R_pre, Res("pre2x")]
              def s4_frontPE(bi):
                  t0 = tiles4[bi]
                  s = bi % 2
                  S.add("sp", (lambda e: e.dma_start(out=hold[s][:], in_=Hs3[:, :, t0:t0 + 128])), reads=rtok("Hs", t0, t0 + 128), writes=[R_hold[s]], dma=True)
                  for oc in range(8):
                      bk = 2 * s + oc // 4
                      co = (oc % 4) * 128
                      for kc in range(8):
                          S.add("pe", (lambda e, kc=kc, oc=oc, bk=bk, co=co: e.matmul(bank(bk)[:, co:co + 128], lhsT=wo[:, kc, oc * 128:(oc + 1) * 128], rhs=oT[:, kc, t0:t0 + 128],
                                                                                    start=(kc == 0), stop=(kc == 7))),
                                reads=[R_wo] + rtok("oT", t0, t0 + 128), writes=[RB[bk]])

              def s4_frontEV(bi, l=l):
                  t0 = tiles4[bi]
                  s = bi % 2
                  mc = mcol(t0)
                  pb, rpb = pre_b[s], R_pre_b[s]
                  for oc in range(8):
                      bk = 2 * s + oc // 4
                      co = (oc % 4) * 128
                      ts = oc % 2
                      if l == 0:
                          vb = V("M2B", oc, t0)
                          S.add("act", (lambda e, oc=oc, bk=bk, co=co, ts=ts, vb=vb: e.activation(out=tmpo[ts][:], in_=bank(bk)[:, co:co + 128], func=AF.Identity,
                                                                                            bias=vb, scale=modc(0, 2, oc, mc))),
                                reads=[RB[bk], R_mod, R_vecs], writes=[R_tmpo[ts]])
                      else:
                          S.add("act", (lambda e, oc=oc, bk=bk, co=co, ts=ts: e.activation(out=tmpo[ts][:], in_=bank(bk)[:, co:co + 128], func=AF.Identity,
                                                                                     scale=modc(1, 2, oc, mc))),
                                reads=[RB[bk], R_mod], writes=[R_tmpo[ts]])
                      S.add("dve", (lambda e, oc=oc, ts=ts: e.scalar_tensor_tensor(out=pb[:, oc, :], in0=hold[s][:, oc, :], scalar=ALPHA, in1=tmpo[ts][:], op0=ALU.mult, op1=ALU.add)),
                            reads=[R_hold[s], R_tmpo[ts]], writes=[rpb])

              def s4_part1(bi):
                  s = bi % 2
                  ln_part1(pre_b[s][:], 8, 128, ones1k, prebf, presq, 4 + 2 * s, [R_pre_b[s]], R_lt)

              def s4_part2(bi, l=l):
                  t0 = tiles4[bi]
                  tt = t0 // 128
                  s = bi % 2
                  pb, rpb = pre_b[s], R_pre_b[s]
                  ln_part2(128, mean_sb, rstd_sb, 4 + 2 * s, R_lt)
                  normalize(pb[:], 8, 128, mean_sb, rstd_sb, [rpb], R_lt)
                  for ch in range(8):
                      affine(UT[:, ch, t0:t0 + 128], pb[:, ch, :], V("G4", ch, t0), V("B4", ch, t0), [rpb, R_vecs], rtok("UT", t0, t0 + 128))
                      affine(t32[:, ch, :], pb[:, ch, :], V("G4", ch, t0), V("B4", ch, t0), [rpb, R_vecs], [R_t32])
                      affine(hres[:, ch, t0:t0 + 128], pb[:, ch, :], V("GA1", ch, t0), V("BA1", ch, t0), [rpb, R_vecs], rtok("hres%d" % ch, t0, t0 + 128))
                  for kc in range(8):
                      S.add("pe", (lambda e, kc=kc: e.matmul(bank(5)[:, tt * 20:tt * 20 + 20], lhsT=t32[:, kc, :], rhs=wrt[:, l * 160 + kc * 20:l * 160 + kc * 20 + 20],
                                                             start=(kc == 0), stop=(kc == 7))),
                            reads=[R_t32, R_const], writes=[RB[5]])

              n4 = len(tiles4)
              s4_frontPE(0)
              for bi in range(n4):
                  s4_frontEV(bi)
                  s4_part1(bi)
                  if bi + 1 < n4:
                      s4_frontPE(bi + 1)
                  if bi > 0:
                      s4_part2(bi - 1)
              s4_part2(n4 - 1)

              dump("S4", hres[:])
              dump("S4u", UT[:])
              S.barrier()
              def routing_block(T0, T1, lsb=lsb, rw=rw, gatesT=gatesT, gTh=gTh, gTl=gTl, R_rout=R_rout):
                  nT = T1 - T0
                  S.add("dve", lambda e: e.tensor_copy(out=lsb[:, T0:T1, :], in_=bank(5)[:, T0 * 20:T1 * 20].rearrange("p (t n) -> p t n", n=20)), reads=[RB[5]], writes=[R_rout])

                  def rwv(i, n):
                      return rw[:, i * 18 * 4:(i * 18 * 4) + 18 * n].rearrange("p (t n) -> p t n", n=n)[:, T0:T1, :]

                  def rop(fn):
                      S.add("dve", fn, reads=[R_rout], writes=[R_rout])

                  lg = lsb[:, T0:T1, 0:4]
                  le = lsb[:, T0:T1, 4:20].rearrange("p t (g x) -> p t g x", g=4)
                  gmax, gsum, gp, m1, m2, dd, w1, w2 = [rwv(i, 1) for i in range(8)]
                  gsh, gmask, elsel, mask1, el2, mask2, within, wa_ = [rwv(8 + i, 4) for i in range(8)]
                  t44 = rw[:, 18 * 64:18 * 80].rearrange("p (t g x) -> p t g x", g=4, x=4)[:, T0:T1]
                  gates = rw[:, 18 * 80:18 * 96].rearrange("p (t g x) -> p t g x", g=4, x=4)
                  bc4 = lambda a: a.broadcast_to([128, nT, 4])
                  rop(lambda e: e.tensor_reduce(out=gmax, in_=lg, axis=AX.X, op=ALU.max))
                  rop(lambda e: e.tensor_tensor(out=gsh, in0=lg, in1=bc4(gmax), op=ALU.subtract))
                  rop(lambda e: e.tensor_tensor(out=gmask, in0=lg, in1=bc4(gmax), op=ALU.is_equal))
                  S.add("act", lambda e: e.activation(out=gsh, in_=gsh, func=AF.Exp), reads=[R_rout], writes=[R_rout])
                  rop(lambda e: e.tensor_reduce(out=gsum, in_=gsh, axis=AX.X, op=ALU.add))
                  rop(lambda e: e.reciprocal(out=gp, in_=gsum))
                  rop(lambda e: e.tensor_tensor(out=t44, in0=le, in1=gmask.unsqueeze(3).broadcast_to([128, nT, 4, 4]), op=ALU.mult))
                  rop(lambda e: e.tensor_reduce(out=elsel, in_=t44.rearrange("p t g x -> p t x g"), axis=AX.X, op=ALU.add))
                  rop(lambda e: e.tensor_reduce(out=m1, in_=elsel, axis=AX.X, op=ALU.max))
                  rop(lambda e: e.tensor_tensor(out=mask1, in0=elsel, in1=bc4(m1), op=ALU.is_equal))
                  rop(lambda e: e.scalar_tensor_tensor(out=el2, in0=mask1, scalar=NEG, in1=elsel, op0=ALU.mult, op1=ALU.add))
                  rop(lambda e: e.tensor_reduce(out=m2, in_=el2, axis=AX.X, op=ALU.max))
                  rop(lambda e: e.tensor_tensor(out=mask2, in0=el2, in1=bc4(m2), op=ALU.is_equal))
                  rop(lambda e: e.tensor_tensor(out=dd, in0=m2, in1=m1, op=ALU.subtract))
                  S.add("act", lambda e: e.activation(out=dd, in_=dd, func=AF.Exp), reads=[R_rout], writes=[R_rout])
                  rop(lambda e: e.tensor_scalar_add(out=w1, in0=dd, scalar1=1.0))
                  rop(lambda e: e.reciprocal(out=w1, in_=w1))
                  rop(lambda e: e.tensor_tensor(out=w1, in0=w1, in1=gp, op=ALU.mult))
                  rop(lambda e: e.tensor_tensor(out=w2, in0=dd, in1=w1, op=ALU.mult))
                  rop(lambda e: e.tensor_tensor(out=within, in0=mask1, in1=bc4(w1), op=ALU.mult))
                  rop(lambda e: e.tensor_tensor(out=wa_, in0=mask2, in1=bc4(w2), op=ALU.mult))
                  rop(lambda e: e.tensor_tensor(out=within, in0=within, in1=wa_, op=ALU.add))
                  rop(lambda e: e.tensor_tensor(out=gates[:, T0:T1], in0=gmask.unsqueeze(3).broadcast_to([128, nT, 4, 4]), in1=within.unsqueeze(2).broadcast_to([128, nT, 4, 4]), op=ALU.mult))
                  for tt in range(T0, T1):
                      bk = 6 + (tt // 4) % 2
                      S.add("pe", (lambda e, tt=tt, bk=bk: e.transpose(bank(bk)[0:16, (tt % 4) * 128:(tt % 4 + 1) * 128], gates[:, tt].rearrange("p g x -> p (g x)"), id32[:])),
                            reads=[R_rout, R_const], writes=[RB[bk]])
                      if tt % 4 == 3 or tt == T1 - 1:
                          ta = (tt // 4) * 4
                          ta0 = max(ta, T0)
                          S.add("act", (lambda e, bk=bk, ta=ta, ta0=ta0, tt=tt: e.copy(out=gatesT[0:16, ta0 * 128:(tt + 1) * 128], in_=bank(bk)[0:16, (ta0 - ta) * 128:(tt + 1 - ta) * 128])),
                                reads=[RB[bk]], writes=[R_rout])

                  g0, g1 = T0 * 128, T1 * 128
                  S.add("act", (lambda e, g0=g0, g1=g1: e.copy(out=gTh[0:16, g0:g1], in_=gatesT[0:16, g0:g1])), reads=[R_rout], writes=[R_rout])
                  S.add("dve", (lambda e, g0=g0, g1=g1: e.tensor_tensor(out=gTl[0:16, g0:g1], in0=gatesT[0:16, g0:g1], in1=gTh[0:16, g0:g1], op=ALU.subtract)), reads=[R_rout], writes=[R_rout])
              routing_block(tok_blocks[0][0] // 128, 18)
              S.barrier()
              cur[0] = C0
              gTh = alloc("gTh", [16, NT], BF16)
              gTl = alloc("gTl", [16, NT], BF16)
              wgs = [alloc("wgs%d" % i, [128, 8, 256], BF16) for i in range(2)]
              wus = [alloc("wus%d" % i, [128, 8, 256], BF16) for i in range(2)]
              wds = [alloc("wds%d" % i, [128, 2, D], BF16) for i in range(2)]
              sgm = [alloc("sgm0", [128, 2, 512], F32)]
              hgm = [alloc("hgm%d" % i, [128, 2, 512], BF16) for i in range(4)]
              o_ = OT0
              for i in range(2, 4):
                  wgs.append(alloc("wgs%d" % i, [128, 8, 256], BF16, at=o_)); o_ += 4096
                  wus.append(alloc("wus%d" % i, [128, 8, 256], BF16, at=o_)); o_ += 4096
                  wds.append(alloc("wds%d" % i, [128, 2, D], BF16, at=o_)); o_ += 4096
              sgm.append(alloc("sgm1", [128, 2, 512], F32, at=o_)); o_ += 4096
              gsb = []
              for i in range(2):
                  gsb.append(alloc("gsb%d" % i, [128, 512], F32, at=o_)); o_ += 2048
              assert o_ <= OT0 + 36864
              R_ewg = [Res("ewg%d" % i) for i in range(4)]
              R_ewu = [Res("ewu%d" % i) for i in range(4)]
              R_ewd = [Res("ewd%d" % i) for i in range(4)]
              R_sgm = [Res("sgm0"), Res("sgm1")]
              R_hgm = [Res("hgm%d" % i) for i in range(4)]
              R_gsb = [Res("gsb0"), Res("gsb1")]
              mblocks = blocks512 if l == 0 else lat512

              def load_expert(ex, slot, l=l):
                  S.add("pool", (lambda e: e.dma_start(out=wgs[slot][:], in_=ewg[l, ex].rearrange("(kc p) f -> p kc f", p=128))), writes=[R_ewg[slot]], dma=True)
                  S.add("pool", (lambda e: e.dma_start(out=wus[slot][:], in_=ewu[l, ex].rearrange("(kc p) f -> p kc f", p=128))), writes=[R_ewu[slot]], dma=True)
                  S.add("pool", (lambda e: e.dma_start(out=wds[slot][:], in_=ewd[l, ex].rearrange("(kc p) f -> p kc f", p=128))), writes=[R_ewd[slot]], dma=True)

              def emit_front(ex, slot, t0, N, si):
                  S.add("pe", (lambda e: e.matmul(bank(4)[:, 0:N], lhsT=selt[0:16, ex * 128:(ex + 1) * 128], rhs=gTh[0:16, t0:t0 + N], start=True, stop=False)),
                        reads=[R_rout, R_const], writes=[RB[4]])
                  S.add("pe", (lambda e: e.matmul(bank(4)[:, 0:N], lhsT=selt[0:16, ex * 128:(ex + 1) * 128], rhs=gTl[0:16, t0:t0 + N], start=False, stop=True)),
                        reads=[R_rout, R_const], writes=[RB[4]])
                  S.add("act", (lambda e: e.copy(out=gsb[si][:, 0:N], in_=bank(4)[:, 0:N])), reads=[RB[4]], writes=[R_gsb[si]])
                  for oc in range(4):
                      wsrc = wgs[slot] if oc < 2 else wus[slot]
                      rw_ = R_ewg[slot] if oc < 2 else R_ewu[slot]
                      for kc in range(8):
                          S.add("pe", (lambda e, kc=kc, oc=oc, wsrc=wsrc: e.matmul(bank(oc)[:, 0:N], lhsT=wsrc[:, kc, (oc % 2) * 128:(oc % 2 + 1) * 128], rhs=UT[:, kc, t0:t0 + N],
                                                                                    start=(kc == 0), stop=(kc == 7))),
                                reads=[rw_] + rtok("UT", t0, t0 + N), writes=[RB[oc]])
                  S.add("act", (lambda e: e.activation(out=sgm[si][:, :, 0:N], in_=PS[0][:].rearrange("p (j n) -> p j n", j=2)[:, :, 0:N], func=AF.Silu)),
                        reads=[RB[0], RB[1]], writes=[R_sgm[si]])
                  S.add("dve", (lambda e: e.tensor_tensor(out=sgm[si][:, :, 0:N], in0=sgm[si][:, :, 0:N], in1=PS[1][:].rearrange("p (j n) -> p j n", j=2)[:, :, 0:N], op=ALU.mult)),
                        reads=[RB[2], RB[3], R_sgm[si]], writes=[R_sgm[si]])

              def emit_gate(si, q, N):
                  S.add("dve", (lambda e: e.tensor_tensor(out=hgm[q][:, :, 0:N], in0=sgm[si][:, :, 0:N], in1=gsb[si][:, 0:N].unsqueeze(1).broadcast_to([128, 2, N]), op=ALU.mult)),
                        reads=[R_gsb[si], R_sgm[si]], writes=[R_hgm[q]])

              ybc = [0]

              def make_yhalf(half, slots, qs, t0, N, mc, l=l):
                  def f():
                      for dc in range(half * 4, half * 4 + 4):
                          bk = 5 + ybc[0] % 3
                          ybc[0] += 1
                          for j in range(2):
                              for k2 in range(2):
                                  S.add("pe", (lambda e, j=j, k2=k2, dc=dc, bk=bk: e.matmul(bank(bk)[:, 0:N], lhsT=wds[slots[j]][:, k2, dc * 128:(dc + 1) * 128], rhs=hgm[qs[j]][:, k2, 0:N],
                                                                                           start=(j == 0 and k2 == 0), stop=(j == 1 and k2 == 1))),
                                        reads=[R_ewd[slots[j]], R_hgm[qs[j]]], writes=[RB[bk]])
                          S.add("dve", (lambda e, dc=dc, bk=bk: e.scalar_tensor_tensor(out=hres[:, dc, t0:t0 + N], in0=bank(bk)[:, 0:N], scalar=modc(l, 5, dc, mc),
                                                                                      in1=hres[:, dc, t0:t0 + N], op0=ALU.mult, op1=ALU.add)),
                                reads=[RB[bk], R_mod] + rtok("hres%d" % dc, t0, t0 + N), writes=rtok("hres%d" % dc, t0, t0 + N))
                  return f

              load_expert(0, 0)
              load_expert(1, 1)
              pendA = pendB = None
              it = 0
              for pr in range(8):
                  slots = (2 * (pr % 2), 2 * (pr % 2) + 1)
                  for bi, (t0, N) in enumerate(mblocks):
                      qs = ((it % 2) * 2, (it % 2) * 2 + 1)
                      it += 1
                      mc = mcol(t0)
                      emit_front(2 * pr, slots[0], t0, N, 0)
                      if pendA is not None:
                          pendA()
                      emit_gate(0, qs[0], N)
                      emit_front(2 * pr + 1, slots[1], t0, N, 1)
                      if pendB is not None:
                          pendB()
                      emit_gate(1, qs[1], N)
                      pendA = make_yhalf(0, slots, qs, t0, N, mc)
                      pendB = make_yhalf(1, slots, qs, t0, N, mc)
                      if bi == 0 and pr + 1 < 8:
                          nslots = (2 * ((pr + 1) % 2), 2 * ((pr + 1) % 2) + 1)
                          load_expert(2 * pr + 2, nslots[0])
                          load_expert(2 * pr + 3, nslots[1])
              pendA()
              pendB()

              dump("S5", hres[:])
              S.barrier()
              cur[0] = C0
              pre2 = alloc("pre2", [128, 8, 256], BF16)
              presq2 = alloc("presq2", [128, 8, 256], BF16)
              mean2 = alloc("mean2", [128, 256], F32)
              rstd2 = alloc("rstd2", [128, 256], F32)
              otile = [alloc("otile%d" % i, [128, D], F32) for i in range(2)]
              R_l2 = [Res("pre2"), Res("presq2"), Res("mean2"), Res("rstd2")]
              R_ot = [Res("ot0"), Res("ot1")]
              oi = 0
              def s6_part1(bi):
                  t0_ = tok_blocks[bi][0]
                  rh_ = [r_ for ch_ in range(8) for r_ in rtok("hres%d" % ch_, t0_, t0_ + 256)]
                  ln_part1(hres[:, :, t0_:t0_ + 256], 8, 256, ones1k, pre2, presq2, 4 + bi % 2, rh_, R_l2)

              s6_part1(0)
              for bi, (t0, N) in enumerate(tok_blocks):
                  hap = hres[:, :, t0:t0 + 256]
                  rh = [r_ for ch_ in range(8) for r_ in rtok("hres%d" % ch_, t0, t0 + 256)]
                  if bi + 1 < len(tok_blocks):
                      s6_part1(bi + 1)
                  ln_part2(256, mean2, rstd2, 4 + bi % 2, R_l2)
                  normalize(hap, 8, 256, mean2, rstd2, rh, R_l2)
                  if l == 0:
                      for ch in range(8):
                          affine(UT[:, ch, t0:t0 + 256], hres[:, ch, t0:t0 + 256], V("GU", ch, t0), V("BU", ch, t0), rh + [R_vecs], rtok("UT", t0, t0 + 256))
                      for ch in range(8):
                          affine(hres[:, ch, t0:t0 + 256], hres[:, ch, t0:t0 + 256], smc("ln_g", 8 + ch), smc("ln_b", 8 + ch), rh + [R_const], rh)
                      S.add("sp", (lambda e, t0=t0: e.dma_start(out=Hs3[:, :, t0:t0 + 256], in_=hres[:, :, t0:t0 + 256])), reads=rh, writes=rtok("Hs", t0, t0 + 256), dma=True)
                      if debug and nlayers == 1:
                          S.add("sp", (lambda e, t0=t0: e.dma_start(out=dbg.rearrange("p (c t) -> p c t", c=8)[:, :, t0:t0 + 256], in_=hres[:, :, t0:t0 + 256])), reads=rh,
                                writes=[Res("dbgo")], dma=True)
                  else:
                      for ch in range(8):
                          affine(hres[:, ch, t0:t0 + 256], hres[:, ch, t0:t0 + 256], smc("ln_g", 24 + ch), smc("ln_b", 24 + ch), rh + [R_const], rh)
                      for hh in range(2):
                          tk = t0 + hh * 128
                          so = oi % 2
                          oi += 1
                          for ch in range(8):
                              bk = so * 2 + ch // 4
                              S.add("pe", (lambda e, ch=ch, bk=bk, tk=tk: e.transpose(bank(bk)[:, (ch % 4) * 128:(ch % 4 + 1) * 128], hres[:, ch, tk:tk + 128], id32[:])),
                                    reads=rh + [R_const], writes=[RB[bk]])
                          for hf in range(2):
                              bk = so * 2 + hf
                              S.add("act" if hf else "dve", (lambda e, so=so, hf=hf, bk=bk: (e.copy if hf else e.tensor_copy)(out=otile[so][:, hf * 512:(hf + 1) * 512], in_=bank(bk))),
                                    reads=[RB[bk]], writes=[R_ot[so]])
                          S.add("sp", (lambda e, so=so, tk=tk, b=b: e.dma_start(out=outd[b, tk - NCTX:tk - NCTX + 128, :], in_=otile[so][:])), reads=[R_ot[so]], writes=[Res("outw")], dma=True)
    except _Stop:
        pass
    S.barrier()

    with nc.Block() as block:
        @block.tensor
        def _(e):
            S.emit_one("pe", e, esem, dsems)

        @block.scalar
        def _(e):
            S.emit_one("act", e, esem, dsems)

        @block.vector
        def _(e):
            S.emit_one("dve", e, esem, dsems)

        @block.gpsimd
        def _(e):
            S.emit_one("pool", e, esem, dsems)

        @block.sync
        def _(e):
            S.emit_one("sp", e, esem, dsems)
    es.close()
    return nc


def _prep_shared(inp):
    f = lambda a: np.ascontiguousarray(np.asarray(a, np.float32))
    sm = np.zeros((128, SMN), np.float32)

    def put(name, arr):
        arr = np.asarray(arr, np.float32)
        sm[:, SMO[name]:SMO[name] + arr.shape[1]] = arr

    put("ada_b0", _fm(inp["ada_b"][0]))
    put("ada_b1", _fm(inp["ada_b"][1]))
    put("ln_g", np.concatenate([_fm(inp["ln_g"][l, k]) for l in range(2) for k in range(2)], axis=1))
    put("ln_b", np.concatenate([_fm(inp["ln_b"][l, k]) for l in range(2) for k in range(2)], axis=1))
    b_in = np.asarray(inp["ab_b_in"][0], np.float32)
    put("b_in", _fm(b_in[:2048]))
    cw = np.asarray(inp["conv_w"][0], np.float32)
    put("conv_w", np.ascontiguousarray(cw.T.reshape(4, 128, 31).transpose(1, 0, 2).reshape(128, 124)))
    put("conv_b", _fm(inp["conv_b"][0]))
    put("cln_g", _fm(inp["conv_ln_g"][0]))
    put("cln_b", _fm(inp["conv_ln_b"][0]))
    put("b_out", _fm(inp["ab_b_out"][0]))
    sm[:, SMO["eps"]] = EPS
    qidx = _gqa_qidx()
    gw = np.asarray(inp["gqa_w_in"][0], np.float32)
    wq = gw[:, :1024]
    wkk = gw[:, 1024:1280]
    wvv = gw[:, 1280:1536]
    C, Sg = _rope_tables()
    kk = np.arange(128)[:, None]
    qq = np.arange(128)[None, :]
    maskL = np.where(kk >= qq, 0.0, NEG).astype(np.float32)
    maskU = np.where(kk <= qq, 0.0, NEG).astype(np.float32)
    sink = np.asarray(inp["gqa_sink"][0], np.float32)
    sperm = np.array([8 * m + 4 * sh + j for m in range(2) for sh in range(2) for j in range(4)])
    sel = np.zeros((16, 16, 128), np.float32)
    for ex in range(16):
        sel[ex, ex, :] = 1.0
    wr = np.stack([np.concatenate([np.asarray(inp["router_group"][l], np.float32), np.asarray(inp["router_expert"][l], np.float32)], axis=1)
                   .reshape(8, 128, 20).transpose(1, 0, 2).reshape(128, 160) for l in range(2)])
    bv = b_in[2048:2560]
    shared = {
        "ada_w": f(inp["ada_w"]),
        "sm": sm,
        "w_in0": f(inp["ab_w_in"][0]),
        "bvbc": np.ascontiguousarray(np.broadcast_to(bv[None, :], (128, 512))),
        "nab": np.ascontiguousarray(_na_bias_table(np.asarray(inp["na_rpb"][0], np.float32)).reshape(8, 128, 21 * 128)),
        "w_out0": f(inp["ab_w_out"][0]),
        "wq1": f(wq[:, qidx]),
        "wqs1": f(wq[:, qidx][:, _swap64(1024)]),
        "wk1": f(wkk),
        "wks1": f(wkk[:, _swap64(256)]),
        "wv1": f(wvv),
        "w_out1": f(np.asarray(inp["gqa_w_out"][0], np.float32)[qidx, :]),
        "ropeC": C,
        "ropeS": Sg,
        "maskLU": np.ascontiguousarray(np.concatenate([maskL, maskU], axis=1)),
        "sinkbc": np.ascontiguousarray(np.broadcast_to(sink[sperm][None, :], (128, 16))),
        "wr": f(wr),
        "sel": np.ascontiguousarray(sel.reshape(16, 2048)),
        "ident": np.eye(128, dtype=np.float32),
        "ewg": f(inp["exp_w_gate"]),
        "ewu": f(inp["exp_w_up"]),
        "ewd": f(inp["exp_w_down"]),
    }
    return shared


def _core_inputs(inp, shared, i):
    x = np.asarray(inp["x"], np.float32)
    ctx = np.asarray(inp["ctx"], np.float32)
    c = np.asarray(inp["c"], np.float32)
    cc = np.stack([c[2 * i], c[2 * i + 1], np.asarray(inp["c_ctx"], np.float32)])
    cvec = np.ascontiguousarray(cc.reshape(3, 8, 128).transpose(2, 1, 0).reshape(128, 24))
    m = dict(shared)
    m["x2"] = np.ascontiguousarray(x[2 * i:2 * i + 2])
    m["ctx2"] = np.ascontiguousarray(ctx[2 * i:2 * i + 2])
    m["cvec"] = cvec
    return m


_NC_CACHE = {}


def kernel(**inputs):
    n = 8
    if "nc" not in _NC_CACHE:
        _NC_CACHE["nc"] = build()
    nc = _NC_CACHE["nc"]
    shared = _prep_shared(inputs)
    in_maps = [_core_inputs(inputs, shared, i) for i in range(n)]
    res = run_bass_kernel_spmd(nc, in_maps, core_ids=list(range(n)))
    out = np.concatenate([np.asarray(r["out"], np.float32) for r in res.results], axis=0)
    return out
```
